# Optimizing a Trainium2 kernel written in Bass

```python
import jax, jax.numpy as jnp
from jax import lax
import numpy as np

D_MODEL = 1024
BATCH = 4
SEQ = 8192
DEPTH = 1

GRID_W = 64
CTX_LEN = 256
W_CONV = 1024
CONV_K = 31
W_LRU = 1024
LRU_HEADS = 8
LRU_HEAD_DIM = W_LRU // LRU_HEADS
LRU_CONV_K = 4
LRU_C = 8.0
N_GROUPS = 4
EXPERTS_PER_GROUP = 4
N_EXPERTS = N_GROUPS * EXPERTS_PER_GROUP
TOP_K_IN_GROUP = 2
D_EXPERT = 512
EPS = 1e-6
SPLITS = (W_CONV, 2 * W_CONV, 2 * W_CONV + W_LRU, 2 * W_CONV + 2 * W_LRU, 2 * W_CONV + 2 * W_LRU + D_MODEL)
REC0 = 2 * W_CONV + W_LRU
REC1 = 2 * W_CONV + 2 * W_LRU
IN_COLS = 2 * W_CONV + 2 * W_LRU + 2 * D_MODEL

kernel_name = "hybrid_conformer_rglru_hmoe_prefix_block"


def rms_norm(x, g):
    xf = x.astype(jnp.float32)
    y = xf * lax.rsqrt(jnp.mean(xf * xf, axis=-1, keepdims=True) + EPS)
    return (y * g.astype(jnp.float32)).astype(x.dtype)


def layer_norm(x, g, b):
    xf = x.astype(jnp.float32)
    mu = jnp.mean(xf, axis=-1, keepdims=True)
    xc = xf - mu
    y = xc * lax.rsqrt(jnp.mean(xc * xc, axis=-1, keepdims=True) + EPS)
    return (y * g.astype(jnp.float32) + b.astype(jnp.float32)).astype(x.dtype)


def ada_split(cond, w, b):
    m = jax.nn.silu(cond) @ w + b
    return jnp.split(m[..., None, :], 6, axis=-1)


def modulate(xn, shift, scale):
    return xn * (1 + scale) + shift


def depthwise_conv(x, w, b, pad_lo, pad_hi):
    y = lax.conv_general_dilated(
        x, w[:, None, :].astype(x.dtype), window_strides=(1,),
        padding=[(pad_lo, pad_hi)], dimension_numbers=("NWC", "WIO", "NWC"),
        feature_group_count=x.shape[-1])
    return y + b


def conformer_conv(u, v, w_dw, b_dw, ln_g, ln_b, w_pa):
    z = u * jax.nn.sigmoid(v)
    z = depthwise_conv(z, w_dw, b_dw, CONV_K // 2, CONV_K // 2)
    z = jax.nn.silu(layer_norm(z, ln_g, ln_b))
    return z @ w_pa


def rglru_coeffs(xc, w_r, b_r, w_i, b_i, lam):
    bsz, n = xc.shape[0], xc.shape[1]
    xh = xc.reshape(bsz, n, LRU_HEADS, LRU_HEAD_DIM)
    r = jax.nn.sigmoid(jnp.einsum("blhi,hij->blhj", xh, w_r) + b_r).reshape(bsz, n, W_LRU)
    i = jax.nn.sigmoid(jnp.einsum("blhi,hij->blhj", xh, w_i) + b_i).reshape(bsz, n, W_LRU)
    log_a = -LRU_C * r.astype(jnp.float32) * jax.nn.softplus(-lam.astype(jnp.float32))
    a = jnp.exp(log_a)
    b = jnp.sqrt(-jnp.expm1(2.0 * log_a)) * (i * xc).astype(jnp.float32)
    return a, b


def linear_scan(a, b, h0):
    b = b.at[:, 0].add(a[:, 0] * h0)

    def combine(left, right):
        a_l, b_l = left
        a_r, b_r = right
        return a_l * a_r, a_r * b_l + b_r

    _, h = lax.associative_scan(combine, (a, b), axis=1)
    return h


def rglru_bidir(xc, pf, pb, h0f, h0b):
    af, bf = rglru_coeffs(xc, *pf)
    ab, bb = rglru_coeffs(jnp.flip(xc, axis=1), *pb)
    return linear_scan(af, bf, h0f), linear_scan(ab, bb, h0b)


def split_proj(p):
    return jnp.split(p, SPLITS, axis=-1)


def merge_branches(br_a, br_b, g_a, g_b, w_o):
    return (jax.nn.sigmoid(g_a) * br_a + jax.nn.sigmoid(g_b) * br_b) @ w_o


def hier_moe(h, w_grp, b_grp, w_er, b_er, w_gate, w_up, w_down):
    n_tok = h.shape[0]
    g_logits = (h @ w_grp + b_grp).astype(jnp.float32)
    g_prob = jax.nn.softmax(g_logits, axis=-1)
    g_sel = jnp.argmax(g_logits, axis=-1)
    p_grp = jnp.max(g_prob, axis=-1, keepdims=True)
    e_logits = (h @ w_er + b_er).astype(jnp.float32).reshape(n_tok, N_GROUPS, EXPERTS_PER_GROUP)
    e_sel = jnp.einsum("nge,ng->ne", e_logits, jax.nn.one_hot(g_sel, N_GROUPS, dtype=jnp.float32))
    top_v, top_i = lax.top_k(e_sel, TOP_K_IN_GROUP)
    wts = p_grp * jax.nn.softmax(top_v, axis=-1)
    expert_id = g_sel[:, None] * EXPERTS_PER_GROUP + top_i
    combine = jnp.sum(jax.nn.one_hot(expert_id, N_EXPERTS, dtype=jnp.float32) * wts[..., None], axis=1)
    combine = combine.astype(h.dtype)
    out = jnp.zeros_like(h)
    for g in range(N_GROUPS):
        sl = slice(g * EXPERTS_PER_GROUP, (g + 1) * EXPERTS_PER_GROUP)
        hg = jnp.einsum("nd,edf->nef", h, w_gate[sl])
        hu = jnp.einsum("nd,edf->nef", h, w_up[sl])
        act = jax.nn.silu(hg) * hu * combine[:, sl, None]
        out = out + jnp.einsum("nef,efd->nd", act, w_down[sl])
    return out


def setup_inputs(seed: int = 0) -> dict:
    key = jax.random.key(seed)
    ks = jax.random.split(key, 40)
    f32 = jnp.float32
    D, L = D_MODEL, DEPTH
    H, hd = LRU_HEADS, LRU_HEAD_DIM

    def nrm(k, shape, scale):
        return jax.random.normal(k, shape, f32) * scale

    def lam_init(k):
        a_c = jax.random.uniform(k, (L, W_LRU), f32, minval=0.9, maxval=0.999)
        a = a_c ** (1.0 / LRU_C)
        return jnp.log(a) - jnp.log1p(-a)

    return {
        "x": nrm(ks[0], (BATCH, SEQ, D), 1.0),
        "c": nrm(ks[1], (BATCH, D), 1.0),
        "ctx": nrm(ks[2], (BATCH, CTX_LEN, D), 1.0),
        "c_ctx": nrm(ks[3], (D,), 1.0),
        "w_ada": nrm(ks[4], (L, D, 6 * D), 0.5 * D ** -0.5),
        "b_ada": nrm(ks[5], (L, 6 * D), 0.01),
        "g_mix": 1.0 + nrm(ks[6], (L, D), 0.01),
        "w_in": nrm(ks[7], (L, D, IN_COLS), D ** -0.5),
        "b_in": nrm(ks[8], (L, IN_COLS), 0.01),
        "conv_w": nrm(ks[9], (L, CONV_K, W_CONV), CONV_K ** -0.5),
        "conv_b": nrm(ks[10], (L, W_CONV), 0.01),
        "ln_g": 1.0 + nrm(ks[11], (L, W_CONV), 0.01),
        "ln_b": nrm(ks[12], (L, W_CONV), 0.01),
        "w_pa": nrm(ks[13], (L, W_CONV, D), W_CONV ** -0.5),
        "lru_conv_w": nrm(ks[14], (L, LRU_CONV_K, W_LRU), LRU_CONV_K ** -0.5),
        "lru_conv_b": nrm(ks[15], (L, W_LRU), 0.01),
        "w_r_f": nrm(ks[16], (L, H, hd, hd), hd ** -0.5),
        "b_r_f": nrm(ks[17], (L, H, hd), 0.01),
        "w_i_f": nrm(ks[18], (L, H, hd, hd), hd ** -0.5),
        "b_i_f": nrm(ks[19], (L, H, hd), 0.01),
        "lam_f": lam_init(ks[20]),
        "w_r_b": nrm(ks[21], (L, H, hd, hd), hd ** -0.5),
        "b_r_b": nrm(ks[22], (L, H, hd), 0.01),
        "w_i_b": nrm(ks[23], (L, H, hd, hd), hd ** -0.5),
        "b_i_b": nrm(ks[24], (L, H, hd), 0.01),
        "lam_b": lam_init(ks[25]),
        "w_pb": nrm(ks[26], (L, W_LRU, D), W_LRU ** -0.5),
        "w_o": nrm(ks[27], (L, D, D), D ** -0.5),
        "g_ffn": 1.0 + nrm(ks[28], (L, D), 0.01),
        "w_grp": nrm(ks[29], (L, D, N_GROUPS), D ** -0.5),
        "b_grp": nrm(ks[30], (L, N_GROUPS), 0.01),
        "w_er": nrm(ks[31], (L, D, N_EXPERTS), D ** -0.5),
        "b_er": nrm(ks[32], (L, N_EXPERTS), 0.01),
        "w_gate": nrm(ks[33], (L, N_EXPERTS, D, D_EXPERT), D ** -0.5),
        "w_up": nrm(ks[34], (L, N_EXPERTS, D, D_EXPERT), D ** -0.5),
        "w_down": nrm(ks[35], (L, N_EXPERTS, D_EXPERT, D), D_EXPERT ** -0.5),
        "g_final": 1.0 + nrm(ks[36], (D,), 0.01),
    }


def reference(x, c, ctx, c_ctx, w_ada, b_ada, g_mix, w_in, b_in, conv_w, conv_b, ln_g, ln_b, w_pa,
              lru_conv_w, lru_conv_b, w_r_f, b_r_f, w_i_f, b_i_f, lam_f, w_r_b, b_r_b, w_i_b, b_i_b, lam_b,
              w_pb, w_o, g_ffn, w_grp, b_grp, w_er, b_er, w_gate, w_up, w_down, g_final):
    bsz, n_lat, d = x.shape
    rows = n_lat // GRID_W
    n_ctx = ctx.shape[1]
    zero_state = jnp.zeros((bsz, W_LRU), jnp.float32)
    for l in range(DEPTH):
        last = l == DEPTH - 1
        sh1, sc1, gt1, sh2, sc2, gt2 = ada_split(c, w_ada[l], b_ada[l])
        csh1, csc1, cgt1, csh2, csc2, cgt2 = ada_split(c_ctx, w_ada[l], b_ada[l])
        pf = (w_r_f[l], b_r_f[l], w_i_f[l], b_i_f[l], lam_f[l])
        pb = (w_r_b[l], b_r_b[l], w_i_b[l], b_i_b[l], lam_b[l])

        hc = modulate(rms_norm(ctx, g_mix[l]), csh1, csc1)
        if last:
            xr_c = hc @ w_in[l][:, REC0:REC1] + b_in[l][REC0:REC1]
        else:
            u_c, v_c, yg_c, xr_c, ga_c, gb_c = split_proj(hc @ w_in[l] + b_in[l])
        xc_c = depthwise_conv(xr_c, lru_conv_w[l], lru_conv_b[l], LRU_CONV_K // 2, LRU_CONV_K - 1 - LRU_CONV_K // 2)
        hf_c, hb_c = rglru_bidir(xc_c, pf, pb, zero_state, zero_state)
        h0f, h0b = hf_c[:, -1], hb_c[:, -1]
        if not last:
            a_c = conformer_conv(u_c, v_c, conv_w[l], conv_b[l], ln_g[l], ln_b[l], w_pa[l])
            y_c = (hf_c + jnp.flip(hb_c, axis=1)).astype(ctx.dtype)
            b_c = (jax.nn.gelu(yg_c) * y_c) @ w_pb[l]
            ctx_new = ctx + cgt1 * merge_branches(a_c, b_c, ga_c, gb_c, w_o[l])
            hcf = modulate(rms_norm(ctx_new, g_ffn[l]), csh2, csc2)
            ctx_new = ctx_new + cgt2 * hier_moe(hcf.reshape(-1, d), w_grp[l], b_grp[l], w_er[l], b_er[l],
                                                w_gate[l], w_up[l], w_down[l]).reshape(bsz, n_ctx, d)

        hx = modulate(rms_norm(x, g_mix[l]), sh1, sc1)
        u, v, yg, xr, ga, gb = split_proj(hx @ w_in[l] + b_in[l])
        a_lat = conformer_conv(u.reshape(bsz * rows, GRID_W, W_CONV), v.reshape(bsz * rows, GRID_W, W_CONV),
                               conv_w[l], conv_b[l], ln_g[l], ln_b[l], w_pa[l]).reshape(bsz, n_lat, d)
        xc = depthwise_conv(xr, lru_conv_w[l], lru_conv_b[l], LRU_CONV_K // 2, LRU_CONV_K - 1 - LRU_CONV_K // 2)
        hf, hb = rglru_bidir(xc, pf, pb, h0f, h0b)
        y_rec = (hf + jnp.flip(hb, axis=1)).astype(x.dtype)
        b_lat = (jax.nn.gelu(yg) * y_rec) @ w_pb[l]
        x = x + gt1 * merge_branches(a_lat, b_lat, ga, gb, w_o[l])
        hm = modulate(rms_norm(x, g_ffn[l]), sh2, sc2)
        x = x + gt2 * hier_moe(hm.reshape(-1, d), w_grp[l], b_grp[l], w_er[l], b_er[l],
                               w_gate[l], w_up[l], w_down[l]).reshape(bsz, n_lat, d)
        if not last:
            ctx = ctx_new
    return rms_norm(x, g_final)
```

```python
from contextlib import ExitStack
import os
import numpy as np
import concourse.bass as bass
import concourse.mybir as mybir
from concourse.bass_utils import run_bass_kernel_spmd

F32 = mybir.dt.float32
BF16 = mybir.dt.bfloat16
AF = mybir.ActivationFunctionType
ALU = mybir.AluOpType
AX = mybir.AxisListType
EPS = 1e-6
NB = 512
NOWN = 4096
DEBUG = bool(int(os.environ.get("MK_DEBUG", "0")))


class Sched:
    def __init__(self, nc, es):
        self.nc = nc
        self.es = es
        self.E = dict(pe=nc.tensor, act=nc.scalar, dve=nc.vector, pool=nc.gpsimd, sp=nc.sync)
        self.sem = {e: es.enter_context(nc.semaphore("c_" + e)) for e in self.E}
        self.cnt = {e: 0 for e in self.E}
        self.seen = {e: {} for e in self.E}
        self.lastw = {}
        self.readers = {}
        self.dsem = {}
        self.dcnt = {}

    def _wait(self, e, tok, same_ok=False):
        if tok is None:
            return
        name, sem, val, src = tok
        if same_ok and src == e:
            return
        d = self.seen[e]
        if d.get(name, 0) >= val:
            return
        self.E[e].wait_ge(sem, val)
        d[name] = val

    def deps(self, e, reads, writes):
        for k in reads:
            self._wait(e, self.lastw.get(k))
        for k in writes:
            self._wait(e, self.lastw.get(k), same_ok=True)
            for t in self.readers.get(k, {}).values():
                self._wait(e, t, same_ok=True)

    def commit(self, tok, reads, writes):
        for k in reads:
            self.readers.setdefault(k, {})[tok[0]] = tok
        for k in writes:
            self.lastw[k] = tok
            self.readers[k] = {}

    def op(self, e, fn, reads=(), writes=()):
        self.deps(e, reads, writes)
        ins = fn(self.E[e])
        self.cnt[e] += 1
        ins.then_inc(self.sem[e], 1)
        self.commit(("c_" + e, self.sem[e], self.cnt[e], e), reads, writes)

    def group(self, e, fns, reads=(), writes=()):
        self.deps(e, reads, writes)
        ins = None
        for f in fns:
            ins = f(self.E[e])
        self.cnt[e] += 1
        ins.then_inc(self.sem[e], 1)
        self.commit(("c_" + e, self.sem[e], self.cnt[e], e), reads, writes)

    def dma(self, q, out, in_, reads=(), writes=(), key=None):
        self.deps(q, reads, writes)
        if key not in self.dsem:
            self.dsem[key] = self.es.enter_context(self.nc.semaphore("d_" + key))
            self.dcnt[key] = 0
        ins = self.E[q].dma_start(out=out, in_=in_)
        self.dcnt[key] += 16
        ins.then_inc(self.dsem[key], 16)
        self.commit(("d_" + key, self.dsem[key], self.dcnt[key], "dma"), reads, writes)

    def final_wait(self, e, keys):
        for k in keys:
            self._wait(e, self.lastw.get(k))

    def barrier(self):
        for e in self.E:
            for e2 in self.E:
                if self.cnt[e2] > 0:
                    self._wait(e, ("c_" + e2, self.sem[e2], self.cnt[e2], e2))
            for k, sem in self.dsem.items():
                self._wait(e, ("d_" + k, sem, self.dcnt[k], "dma"))


def build_program():
    nc = bass.Bass("TRN2", target_bir_lowering=False)

    def din(name, shape):
        return nc.dram_tensor(name, list(shape), F32, kind="ExternalInput").ap()

    xp = din("xp", [8196, 1024])
    ctxp = din("ctxp", [260, 1024])
    cvec = din("cvec", [128, 8, 2])
    w_ada = din("w_ada", [1024, 6144])
    b_ada_fm = din("b_ada_fm", [128, 48])
    b_ada_gt = din("b_ada_gt", [128, 2, 1024])
    w_in = din("w_in", [1024, 6144])
    b_in_fm = din("b_in_fm", [128, 48])
    cw = din("cw", [128, 8, 31])
    cb = din("cb", [128, 8])
    lng = din("lng", [128, 8])
    lnb = din("lnb", [128, 8])
    w_pa = din("w_pa", [1024, 1024])
    w_pb = din("w_pb", [1024, 1024])
    w_o = din("w_o", [1024, 1024])
    lw5 = din("lw5", [128, 8, 5])
    lb = din("lb", [128, 8])
    wg = din("wg", [4, 8, 128, 128])
    bg = din("bg", [128, 4, 8])
    lam = din("lam", [128, 2, 8])
    gmix = din("gmix", [128, 8])
    gffn = din("gffn", [128, 8])
    gfin = din("gfin", [128, 1024])
    w_rt = din("w_rt", [1024, 20])
    b_rt = din("b_rt", [128, 20])
    w_gate = din("w_gate", [16, 1024, 512])
    w_up = din("w_up", [16, 1024, 512])
    w_down = din("w_down", [16, 512, 1024])
    ident = din("ident", [128, 128])
    out = nc.dram_tensor("out", [NOWN, 1024], F32, kind="ExternalOutput").ap()
    if DEBUG:
        hs_scr = nc.dram_tensor("hs_scr", [8, 128, 8, NB], BF16, kind="ExternalOutput").ap()
        x1_scr = nc.dram_tensor("x1_scr", [NOWN, 1024], F32, kind="ExternalOutput").ap()
    else:
        hs_scr = nc.dram_tensor("hs_scr", [8, 128, 8, NB], BF16, kind="Internal").ap()
        x1_scr = nc.dram_tensor("x1_scr", [NOWN, 1024], F32, kind="Internal").ap()

    with ExitStack() as es:
        S = Sched(nc, es)

        def sb(name, shape, dt=F32, stack=es):
            return stack.enter_context(nc.sbuf_tensor(name, list(shape), dt))

        psA = es.enter_context(nc.psum_tensor("psA", [128, 2048], F32))
        psB = es.enter_context(nc.psum_tensor("psB", [128, 2048], F32))

        def bank(i):
            t = psA if i < 4 else psB
            return t[:, 512 * (i % 4):512 * (i % 4) + 512]

        def pk(i):
            return "ps%d" % i

        tpv = psA[:, :].bitcast(BF16).rearrange("p (c t) -> p c t", t=512)
        TPK = ("ps0", "ps1", "ps2", "ps3")

        identb = sb("identb", [128, 128], BF16)
        ones_m = sb("ones_m", [128, 128], BF16)
        ones1 = sb("ones1", [128, 128], BF16)
        b_in_sb = sb("b_in_sb", [128, 48])
        hb_in = sb("hb_in", [128, 48])
        cw_sb = sb("cw_sb", [128, 8, 31])
        cb_sb = sb("cb_sb", [128, 8])
        lng_sb = sb("lng_sb", [128, 8])
        lnb_sb = sb("lnb_sb", [128, 8])
        lw5_sb = sb("lw5_sb", [128, 8, 5])
        lb_sb = sb("lb_sb", [128, 8])
        bg_sb = sb("bg_sb", [128, 4, 8])
        hbg = sb("hbg", [128, 4, 8])
        lam_sb = sb("lam_sb", [128, 2, 8])
        gmix_sb = sb("gmix_sb", [128, 8])
        gffn_sb = sb("gffn_sb", [128, 8])
        gf32 = sb("gf32", [128, 1024])
        b_rt_sb = sb("b_rt_sb", [128, 20])
        b_ada_fm_sb = sb("b_ada_fm_sb", [128, 48])
        cvec_sb = sb("cvec_sb", [128, 8, 2])
        mods = sb("mods", [128, 48, 2])
        s1 = sb("s1", [128, 8])
        s1c = sb("s1c", [128, 8])
        s2 = sb("s2", [128, 8])
        gt1h = sb("gt1h", [128, 1024])
        gt2b = sb("gt2b", [128, 1024])
        cl = sb("cl", [128, 2, 8])
        hcl = sb("hcl", [128, 2, 8])
        state = sb("state", [128, 2, 8])
        ss = sb("ss", [128, 8])
        rs = sb("rs", [128, 8])
        w_rt_b = sb("w_rt_b", [128, 8, 20], BF16)
        half_t = sb("half_t", [128, NB], F32)
        mhalf_t = sb("mhalf_t", [128, NB], F32)

        def pload(t, src):
            S.dma("sp", t, src, writes=("params",), key="params")

        pload(b_in_sb[:], b_in_fm)
        pload(cw_sb[:], cw)
        pload(cb_sb[:], cb)
        pload(lng_sb[:], lng)
        pload(lnb_sb[:], lnb)
        pload(lw5_sb[:], lw5)
        pload(lb_sb[:], lb)
        pload(bg_sb[:], bg)
        pload(lam_sb[:], lam)
        pload(gmix_sb[:], gmix)
        pload(gffn_sb[:], gffn)
        pload(gf32[:], gfin)
        pload(b_rt_sb[:], b_rt)
        pload(b_ada_fm_sb[:], b_ada_fm)
        pload(cvec_sb[:], cvec)
        S.dma("pool", identb[:], ident, writes=("identb",), key="identb")
        S.dma("pool", w_rt_b[:], w_rt.rearrange("(k p) n -> p k n", p=128), writes=("w_rt_b",), key="w_rt_b")
        S.op("pool", lambda e: e.memset(ones_m[:], 1.0 / 1024.0), writes=("ones_m",))
        S.op("pool", lambda e: e.memset(ones1[:], 1.0), writes=("ones1",))
        S.op("pool", lambda e: e.memset(half_t[:], 0.5), writes=("half_t",))
        S.op("pool", lambda e: e.memset(mhalf_t[:], -0.5), writes=("half_t",))
        S.op("pool", lambda e: e.memset(state[:], 0.0), writes=("state",))
        S.op("pool", lambda e: e.memset(ss[:], 0.0), writes=("ss",))

        with ExitStack() as p0:
            cs = sb("cs", [128, 8, 2], BF16, p0)
            cs_rep = sb("cs_rep", [128, 8, 128], BF16, p0)
            b_ada_gt_sb = sb("b_ada_gt_sb", [128, 2, 1024], F32, p0)
            wa = [sb("wa%d" % i, [128, 8, 512], BF16, p0) for i in range(3)]
            e_t = sb("e_t", [128, 16], F32, p0)
            t_t = sb("t_t", [128, 16], F32, p0)
            l_t = sb("l_t", [128, 16], F32, p0)
            m_t = sb("m_t", [128, 16], F32, p0)
            pload(b_ada_gt_sb[:], b_ada_gt)

            S.op("act", lambda e: e.activation(out=cs[:], in_=cvec_sb[:], func=AF.Silu), reads=("params",), writes=("cs",))
            S.op("dve", lambda e: e.tensor_copy(out=cs_rep[:], in_=cs[:, :, 0:1].to_broadcast([128, 8, 128])),
                 reads=("cs",), writes=("cs_rep",))
            psm = bank(0)[:, 0:96].rearrange("p (j t) -> p j t", t=2)
            for q in range(12):
                s = q % 3
                S.dma("pool", wa[s][:], w_ada[:, 512 * q:512 * q + 512].rearrange("(k p) n -> p k n", p=128),
                      writes=("wa%d" % s,), key="wa%d" % s)
                fns = []
                for jj in range(4):
                    for k in range(8):
                        fns.append(lambda e, jj=jj, k=k, s=s, q=q: e.matmul(
                            psm[:, 4 * q + jj, :], lhsT=wa[s][:, k, 128 * jj:128 * jj + 128], rhs=cs[:, k, :],
                            start=(k == 0), stop=(k == 7)))
                S.group("pe", fns, reads=("wa%d" % s, "cs"), writes=("ps0",))
                if q in (4, 5, 10, 11):
                    bk = 1 + (q % 2)
                    S.group("pe", [lambda e, k=k, s=s, bk=bk: e.matmul(bank(bk), lhsT=cs_rep[:, k, :], rhs=wa[s][:, k, :],
                                                                      start=(k == 0), stop=(k == 7)) for k in range(8)],
                            reads=("wa%d" % s, "cs_rep"), writes=(pk(bk),))
                    dst = gt1h if q < 6 else gt2b
                    gi = 0 if q < 6 else 1
                    cols = slice(512 * (q % 2), 512 * (q % 2) + 512)
                    S.op("dve", lambda e, dst=dst, gi=gi, cols=cols, bk=bk: e.tensor_tensor(
                        out=dst[:, cols], in0=bank(bk), in1=b_ada_gt_sb[:, gi, cols], op=ALU.add),
                        reads=(pk(bk), "params"), writes=("gt",))
            S.op("dve", lambda e: e.tensor_scalar_mul(out=gt1h[:], in0=gt1h[:], scalar1=0.5), reads=("gt",), writes=("gt",))
            S.op("dve", lambda e: e.tensor_tensor(out=mods[:], in0=psm, in1=b_ada_fm_sb[:].unsqueeze(2).to_broadcast([128, 48, 2]),
                                                  op=ALU.add), reads=("ps0", "params"), writes=("mods",))
            for (dst, col, j0, gsb) in ((s1, 0, 8, gmix_sb), (s1c, 1, 8, gmix_sb), (s2, 0, 32, gffn_sb)):
                S.op("dve", lambda e, dst=dst, col=col, j0=j0, gsb=gsb: e.scalar_tensor_tensor(
                    out=dst[:], in0=mods[:, j0:j0 + 8, col], scalar=1.0, in1=gsb[:], op0=ALU.add, op1=ALU.mult),
                    reads=("mods", "params"), writes=("sc",))
                S.op("dve", lambda e, dst=dst: e.tensor_scalar_mul(out=dst[:], in0=dst[:], scalar1=32.0),
                     reads=("sc",), writes=("sc",))
            S.op("dve", lambda e: e.tensor_scalar_mul(out=gf32[:], in0=gf32[:], scalar1=32.0), reads=("params",), writes=("gf32",))
            S.op("dve", lambda e: e.tensor_scalar_mul(out=hb_in[:], in0=b_in_sb[:], scalar1=0.5), reads=("params",), writes=("hb_in",))
            S.op("dve", lambda e: e.tensor_scalar_mul(out=hbg[:], in0=bg_sb[:], scalar1=0.5), reads=("params",), writes=("hbg",))
            lamf = lam_sb[:].rearrange("p a b -> p (a b)")
            S.op("act", lambda e: e.activation(out=e_t[:], in_=lamf, func=AF.Exp, scale=-1.0), reads=("params",), writes=("e_t",))
            S.op("dve", lambda e: e.tensor_scalar(out=t_t[:], in0=e_t[:], scalar1=-0.25, scalar2=1.0 / 3.0, op0=ALU.mult, op1=ALU.add),
                 reads=("e_t",), writes=("t_t",))
            S.op("dve", lambda e: e.tensor_tensor(out=t_t[:], in0=t_t[:], in1=e_t[:], op=ALU.mult), reads=("t_t", "e_t"), writes=("t_t",))
            S.op("dve", lambda e: e.tensor_scalar_add(out=t_t[:], in0=t_t[:], scalar1=-0.5), reads=("t_t",), writes=("t_t",))
            S.op("dve", lambda e: e.tensor_tensor(out=t_t[:], in0=t_t[:], in1=e_t[:], op=ALU.mult), reads=("t_t", "e_t"), writes=("t_t",))
            S.op("dve", lambda e: e.tensor_scalar_add(out=t_t[:], in0=t_t[:], scalar1=1.0), reads=("t_t",), writes=("t_t",))
            S.op("dve", lambda e: e.tensor_tensor(out=t_t[:], in0=t_t[:], in1=e_t[:], op=ALU.mult), reads=("t_t", "e_t"), writes=("t_t",))
            S.op("dve", lambda e: e.tensor_scalar_add(out=l_t[:], in0=e_t[:], scalar1=1.0), reads=("e_t",), writes=("l_t",))
            S.op("act", lambda e: e.activation(out=l_t[:], in_=l_t[:], func=AF.Ln), reads=("l_t",), writes=("l_t",))
            S.op("dve", lambda e: e.tensor_single_scalar(out=m_t[:], in_=e_t[:], scalar=0.1, op=ALU.is_lt), reads=("e_t",), writes=("m_t",))
            S.op("dve", lambda e: e.tensor_tensor(out=t_t[:], in0=t_t[:], in1=l_t[:], op=ALU.subtract), reads=("t_t", "l_t"), writes=("t_t",))
            S.op("dve", lambda e: e.tensor_tensor(out=t_t[:], in0=t_t[:], in1=m_t[:], op=ALU.mult), reads=("t_t", "m_t"), writes=("t_t",))
            S.op("dve", lambda e: e.tensor_tensor(out=t_t[:], in0=t_t[:], in1=l_t[:], op=ALU.add), reads=("t_t", "l_t"), writes=("t_t",))
            clf = cl[:].rearrange("p a b -> p (a b)")
            hclf = hcl[:].rearrange("p a b -> p (a b)")
            S.op("dve", lambda e: e.tensor_scalar_mul(out=clf, in0=t_t[:], scalar1=-8.0), reads=("t_t",), writes=("cl",))
            S.op("dve", lambda e: e.tensor_scalar_mul(out=hclf, in0=t_t[:], scalar1=-4.0), reads=("t_t",), writes=("cl",))
            S.barrier()

        mixer = ExitStack()
        wxr = sb("wxr", [128, 8, 1024], BF16, mixer)
        wgb = sb("wgb", [128, 4, 8, 128], BF16, mixer)
        dg5 = sb("dg5", [128, 8, 5, 128], BF16, mixer)
        S.dma("pool", wxr[:], w_in[:, 3072:4096].rearrange("(k p) n -> p k n", p=128), writes=("wxr",), key="wxr")
        S.dma("pool", wgb[:], wg.rearrange("g h p n -> p g h n"), writes=("wgb",), key="wgb")
        for c in range(8):
            S.op("pool", lambda e, c=c: e.tensor_tensor(
                out=dg5[:, c, :, :], in0=identb[:].unsqueeze(1).to_broadcast([128, 5, 128]),
                in1=lw5_sb[:, c, :].unsqueeze(2).to_broadcast([128, 5, 128]), op=ALU.mult),
                reads=("identb", "params"), writes=("dg5",))

        xt = sb("xt", [128, 4, 1024], F32, mixer)
        xh = sb("xh", [4, 1024], F32, mixer)
        xn = sb("xn", [128, 4, 1024], BF16, mixer)
        xnh = sb("xnh", [4, 1024], BF16, mixer)
        hxT = sb("hxT", [128, 8, NB + 4], BF16, mixer)
        xrp = [sb("xrp%d" % i, [128, NB + 4], BF16, mixer) for i in range(2)]
        xcb = [sb("xcb%d" % i, [128, NB], BF16, mixer) for i in range(2)]
        tr = sb("tr", [128, NB], F32, mixer)
        ti = sb("ti", [128, NB], F32, mixer)
        a_t = sb("a_t", [128, NB], F32, mixer)
        a2_t = sb("a2_t", [128, NB], F32, mixer)
        tmp1 = sb("tmp1", [128, NB], F32, mixer)
        w_t = sb("w_t", [128, NB], F32, mixer)
        bb_t = sb("bb_t", [128, NB], F32, mixer)
        hf = sb("hf", [128, NB], F32, mixer)
        hsb = sb("hsb", [128, 8, NB], BF16, mixer)

        tph = bank(4).bitcast(BF16)[:, 0:32].rearrange("p (c t) -> p c t", t=4)

        def prep(xsrc, r0, N, sc, bcol, keep_key):
            nt = N // 128
            S.dma("sp", xt[:, 0:nt, :], xsrc[r0:r0 + N, :].rearrange("(j p) d -> p j d", p=128), writes=(keep_key,), key="xt")
            S.dma("sp", xh[0:2, :], xsrc[r0 - 2:r0, :], writes=("xh",), key="xh")
            S.dma("sp", xh[2:4, :], xsrc[r0 + N:r0 + N + 2, :], writes=("xh",), key="xh")
            S.op("pool", lambda e: e.memset(ss[:], 0.0), writes=("ss",))
            for j in range(nt):
                S.op("act", lambda e, j=j: e.activation(out=xn[:, j, :], in_=xt[:, j, :], func=AF.Square, accum_out=ss[:, j:j + 1]),
                     reads=(keep_key,), writes=("xn", "ss"))
            S.op("act", lambda e: e.activation(out=xnh[:], in_=xh[:], func=AF.Square, accum_out=ss[0:4, 4:5]),
                 reads=("xh",), writes=("xnh", "ss"))
            S.op("dve", lambda e: e.tensor_scalar_add(out=rs[:, 0:5], in0=ss[:, 0:5], scalar1=1024.0 * EPS), reads=("ss",), writes=("rs",))
            S.op("pool", lambda e: e.tensor_tensor(out=rs[:, 0:5], in0=rs[:, 0:5], in1=mhalf_t[:, 0:5], op=ALU.pow),
                 reads=("rs",), writes=("rs",))
            for j in range(nt):
                S.op("pool", lambda e, j=j: e.tensor_scalar_mul(out=xn[:, j, :], in0=xt[:, j, :], scalar1=rs[:, j:j + 1]),
                     reads=(keep_key, "rs"), writes=("xn",))
            S.op("pool", lambda e: e.tensor_scalar_mul(out=xnh[:], in0=xh[:], scalar1=rs[0:4, 4:5]),
                 reads=("xh", "rs"), writes=("xnh",))
            for j in range(nt):
                S.group("pe", [lambda e, j=j, c=c: e.transpose(out=tpv[:, c, 128 * j:128 * j + 128],
                                                               in_=xn[:, j, 128 * c:128 * c + 128], identity=identb[:])
                               for c in range(8)], reads=("xn", "identb"), writes=TPK)
            S.group("pe", [lambda e, c=c: e.transpose(out=tph[:, c, :], in_=xnh[:, 128 * c:128 * c + 128], identity=identb[0:4, 0:4])
                           for c in range(8)], reads=("xnh", "identb"), writes=("ps4",))
            for c in range(8):
                S.op("act", lambda e, c=c: e.activation(out=hxT[:, c, 0:N], in_=tpv[:, c, 0:N], func=AF.Identity,
                                                        scale=sc[:, c:c + 1], bias=mods[:, c, bcol:bcol + 1]),
                     reads=TPK + ("sc", "mods"), writes=("hxT",))
            S.op("dve", lambda e: e.tensor_tensor(out=hxT[:, :, N:N + 4], in0=tph, in1=sc[:].unsqueeze(2).to_broadcast([128, 8, 4]),
                                                  op=ALU.mult), reads=("ps4", "sc"), writes=("hxTh",))
            S.op("dve", lambda e: e.tensor_tensor(out=hxT[:, :, N:N + 4], in0=hxT[:, :, N:N + 4],
                                                  in1=mods[:, 0:8, bcol:bcol + 1].to_broadcast([128, 8, 4]), op=ALU.add),
                 reads=("hxTh", "mods"), writes=("hxTh",))

        def rglru_chunk(c, N, d, reverse, has_lo, has_hi):
            par = c % 2
            bxr = bank(par)[:, 0:N]
            bxh = bank(2 + par)[:, 0:4]
            S.group("pe", [lambda e, k=k: e.matmul(bxr, lhsT=wxr[:, k, 128 * c:128 * c + 128], rhs=hxT[:, k, 0:N],
                                                   start=(k == 0), stop=(k == 7)) for k in range(8)],
                    reads=("hxT", "wxr"), writes=(pk(par),))
            S.group("pe", [lambda e, k=k: e.matmul(bxh, lhsT=wxr[:, k, 128 * c:128 * c + 128], rhs=hxT[:, k, N:N + 4],
                                                   start=(k == 0), stop=(k == 7)) for k in range(8)],
                    reads=("hxTh", "wxr"), writes=(pk(2 + par),))
            xk = "xrp%d" % par
            bia = b_in_sb[:, 24 + c:25 + c]
            S.op("act", lambda e: e.activation(out=xrp[par][:, 2:2 + N], in_=bxr, func=AF.Identity, bias=bia),
                 reads=(pk(par), "params"), writes=(xk,))
            if has_lo:
                S.op("act", lambda e: e.activation(out=xrp[par][:, 0:2], in_=bxh[:, 0:2], func=AF.Identity, bias=bia),
                     reads=(pk(2 + par), "params"), writes=(xk,))
            else:
                S.op("pool", lambda e: e.memset(xrp[par][:, 0:2], 0.0), writes=(xk,))
            if has_hi:
                S.op("act", lambda e: e.activation(out=xrp[par][:, 2 + N:4 + N], in_=bxh[:, 2:4], func=AF.Identity, bias=bia),
                     reads=(pk(2 + par), "params"), writes=(xk,))
            else:
                S.op("pool", lambda e: e.memset(xrp[par][:, 2 + N:4 + N], 0.0), writes=(xk,))
            bcv = bank(4 + par)[:, 0:N]
            S.group("pe", [lambda e, j=j: e.matmul(bcv, lhsT=dg5[:, c, j, :], rhs=xrp[par][:, j:j + N],
                                                   start=(j == 0), stop=(j == 4)) for j in range(5)],
                    reads=(xk, "dg5"), writes=(pk(4 + par),))
            ck = "xcb%d" % par
            S.op("act", lambda e: e.activation(out=xcb[par][:, 0:N], in_=bcv, func=AF.Identity, bias=lb_sb[:, c:c + 1]),
                 reads=(pk(4 + par), "params"), writes=(ck,))
            br_ = bank(6)[:, 0:N]
            bi_ = bank(7)[:, 0:N]
            S.group("pe", [lambda e: e.matmul(br_, lhsT=wgb[:, 2 * d, c, :], rhs=xcb[par][:, 0:N], start=True, stop=True)],
                    reads=(ck, "wgb"), writes=("ps6",))
            S.group("pe", [lambda e: e.matmul(bi_, lhsT=wgb[:, 2 * d + 1, c, :], rhs=xcb[par][:, 0:N], start=True, stop=True)],
                    reads=(ck, "wgb"), writes=("ps7",))
            S.op("act", lambda e: e.activation(out=tr[:, 0:N], in_=br_, func=AF.Tanh, scale=0.5, bias=hbg[:, 2 * d, c:c + 1]),
                 reads=("ps6", "hbg"), writes=("tr",))
            S.op("act", lambda e: e.activation(out=ti[:, 0:N], in_=bi_, func=AF.Tanh, scale=0.5, bias=hbg[:, 2 * d + 1, c:c + 1]),
                 reads=("ps7", "hbg"), writes=("ti",))
            S.op("act", lambda e: e.activation(out=a_t[:, 0:N], in_=tr[:, 0:N], func=AF.Exp, scale=hcl[:, d, c:c + 1],
                                               bias=hcl[:, d, c:c + 1]), reads=("tr", "cl"), writes=("a_t",))
            S.op("act", lambda e: e.activation(out=a2_t[:, 0:N], in_=tr[:, 0:N], func=AF.Exp, scale=cl[:, d, c:c + 1],
                                               bias=cl[:, d, c:c + 1]), reads=("tr", "cl"), writes=("a2_t",))
            S.op("dve", lambda e: e.scalar_tensor_tensor(out=tmp1[:, 0:N], in0=ti[:, 0:N], scalar=1.0, in1=xcb[par][:, 0:N],
                                                         op0=ALU.add, op1=ALU.mult), reads=("ti", ck), writes=("tmp1",))
            S.op("dve", lambda e: e.tensor_scalar(out=w_t[:, 0:N], in0=a2_t[:, 0:N], scalar1=-0.25, scalar2=0.25,
                                                  op0=ALU.mult, op1=ALU.add), reads=("a2_t",), writes=("w_t",))
            S.op("pool", lambda e: e.tensor_tensor(out=w_t[:, 0:N], in0=w_t[:, 0:N], in1=half_t[:, 0:N], op=ALU.pow),
                 reads=("w_t",), writes=("w_t",))
            S.op("dve", lambda e: e.tensor_tensor(out=bb_t[:, 0:N], in0=w_t[:, 0:N], in1=tmp1[:, 0:N], op=ALU.mult),
                 reads=("w_t", "tmp1"), writes=("bb_t",))
            if reverse:
                S.op("dve", lambda e: e.tensor_tensor_scan(out=hf[:, 0:N][:, ::-1], data0=a_t[:, 0:N][:, ::-1],
                                                           data1=bb_t[:, 0:N][:, ::-1], initial=state[:, d, c:c + 1],
                                                           op0=ALU.mult, op1=ALU.add),
                     reads=("a_t", "bb_t", "state"), writes=("hf",))
                S.op("act", lambda e: e.activation(out=state[:, d, c:c + 1], in_=hf[:, 0:1], func=AF.Copy),
                     reads=("hf",), writes=("state",))
            else:
                S.op("dve", lambda e: e.tensor_tensor_scan(out=hf[:, 0:N], data0=a_t[:, 0:N], data1=bb_t[:, 0:N],
                                                           initial=state[:, d, c:c + 1], op0=ALU.mult, op1=ALU.add),
                     reads=("a_t", "bb_t", "state"), writes=("hf",))
                S.op("act", lambda e: e.activation(out=state[:, d, c:c + 1], in_=hf[:, N - 1:N], func=AF.Copy),
                     reads=("hf",), writes=("state",))

        prep(ctxp, 2, 256, s1c, 1, "xt")
        for c in range(8):
            rglru_chunk(c, 256, 0, False, False, False)
        for c in range(8):
            rglru_chunk(c, 256, 1, True, False, False)
        for blk in range(15, -1, -1):
            prep(xp, 2 + NB * blk, NB, s1, 0, "xt")
            for c in range(8):
                rglru_chunk(c, NB, 1, True, blk != 0, blk != 15)
                if blk < 8:
                    S.op("pool", lambda e, c=c: e.tensor_copy(out=hsb[:, c, :], in_=hf[:]), reads=("hf",), writes=("hsb",))
            if blk < 8:
                S.dma("sp", hs_scr[blk], hsb[:], reads=("hsb",), writes=("hs_scr%d" % blk,), key="hs_scr")

        pb_ = ExitStack()
        cwh = sb("cwh", [128, 8, 31], F32, pb_)
        S.op("dve", lambda e: e.tensor_scalar_mul(out=cwh[:], in0=cw_sb[:], scalar1=0.5), reads=("params",), writes=("cwh",))
        wsl = [sb("wsl%d" % i, [128, 8, 512], BF16, pb_) for i in range(3)]
        dgc = [sb("dgc0", [128, 31, 128], BF16, pb_)] * 2
        tv, uu, mean, rstd, mr = tr, ti, a_t, a2_t, w_t
        zb = [sb("zb0", [128, NB], BF16, pb_)] * 2
        zc = sb("zc", [128, 8, NB], BF16, pb_)
        aa = zc
        zsq = [sb("zsq0", [128, NB], BF16, pb_)] * 2
        A_t = sb("A_t", [128, 8, NB], BF16, pb_)
        gy = sb("gy", [128, 8, NB], BF16, pb_)
        mg = gy
        yb = sb("yb", [128, 8, NB], BF16, pb_)
        hs_in = hsb
        x1 = xt

        wctr = [0]

        def wpiece(col0, src=None):
            src = w_in if src is None else src
            i = wctr[0] % 3
            wctr[0] += 1
            S.dma("pool", wsl[i][:], src[:, col0:col0 + 512].rearrange("(k p) n -> p k n", p=128),
                  writes=("wsl%d" % i,), key="wsl%d" % i)
            return wsl[i], "wsl%d" % i

        def inproj(dstbank, wt, wk, j4):
            S.group("pe", [lambda e, k=k: e.matmul(bank(dstbank), lhsT=wt[:, k, 128 * j4:128 * j4 + 128], rhs=hxT[:, k, 0:NB],
                                                   start=(k == 0), stop=(k == 7)) for k in range(8)],
                    reads=("hxT", wk), writes=(pk(dstbank),))

        for blk in range(8):
            prep(xp, 2 + NB * blk, NB, s1, 0, "xt")
            S.dma("sp", hs_in[:], hs_scr[blk], reads=("hs_scr%d" % blk,), writes=("hsb",), key="hs_in")
            for half in range(2):
                wu, wuk = wpiece(512 * half)
                wv, wvk = wpiece(1024 + 512 * half)
                for c4 in range(4):
                    c = 4 * half + c4
                    par = c % 2
                    inproj(par, wu, wuk, c4)
                    inproj(2 + par, wv, wvk, c4)
                    S.op("act", lambda e, c=c, par=par: e.activation(out=tv[:], in_=bank(2 + par), func=AF.Tanh, scale=0.5,
                                                                     bias=hb_in[:, 8 + c:9 + c]),
                         reads=(pk(2 + par), "hb_in"), writes=("tv",))
                    S.op("act", lambda e, c=c, par=par: e.activation(out=uu[:], in_=bank(par), func=AF.Identity,
                                                                     bias=b_in_sb[:, c:c + 1]),
                         reads=(pk(par), "params"), writes=("uu",))
                    zk = "zb0"
                    S.op("dve", lambda e, par=par: e.scalar_tensor_tensor(out=zb[par][:], in0=tv[:], scalar=1.0, in1=uu[:],
                                                                          op0=ALU.add, op1=ALU.mult),
                         reads=("tv", "uu"), writes=(zk,))
                    dk = "dgc0"
                    S.op("pool", lambda e, c=c, par=par: e.tensor_tensor(
                        out=dgc[par][:], in0=identb[:].unsqueeze(1).to_broadcast([128, 31, 128]),
                        in1=cwh[:, c, :].unsqueeze(2).to_broadcast([128, 31, 128]), op=ALU.mult),
                        reads=("identb", "cwh"), writes=(dk,))
                    zv = zb[par][:].rearrange("p (r t) -> p r t", t=64)
                    pcv = bank(4 + par).rearrange("p (r t) -> p r t", t=64)
                    fns = []
                    order = [15] + [k for k in range(31) if k != 15]
                    for idx, k in enumerate(order):
                        o = k - 15
                        t0, t1 = max(0, -o), 64 - max(0, o)
                        fns.append(lambda e, k=k, o=o, t0=t0, t1=t1, idx=idx, par=par, pcv=pcv, zv=zv: e.matmul(
                            pcv[:, :, t0:t1], lhsT=dgc[par][:, k, :], rhs=zv[:, :, t0 + o:t1 + o],
                            start=(idx == 0), stop=(idx == 30)))
                    S.group("pe", fns, reads=(zk, dk), writes=(pk(4 + par),))
                    S.op("act", lambda e, c=c, par=par: e.activation(out=zc[:, c, :], in_=bank(4 + par), func=AF.Identity,
                                                                     bias=cb_sb[:, c:c + 1]),
                         reads=(pk(4 + par), "params"), writes=("zc",))
                    qk = "zsq0"
                    S.op("act", lambda e, c=c, par=par: e.activation(out=zsq[par][:], in_=bank(4 + par), func=AF.Square,
                                                                     bias=cb_sb[:, c:c + 1]),
                         reads=(pk(4 + par), "params"), writes=(qk,))
                    S.group("pe", [lambda e, c=c: e.matmul(bank(6), lhsT=ones_m[:], rhs=zc[:, c, :], start=(c == 0), stop=(c == 7))],
                            reads=("zc", "ones_m"), writes=("ps6",))
                    S.group("pe", [lambda e, c=c, par=par: e.matmul(bank(7), lhsT=ones_m[:], rhs=zsq[par][:], start=(c == 0),
                                                                    stop=(c == 7))], reads=(qk, "ones_m"), writes=("ps7",))
            S.op("act", lambda e: e.activation(out=mean[:], in_=bank(6), func=AF.Copy), reads=("ps6",), writes=("mean",))
            S.op("dve", lambda e: e.tensor_tensor(out=mr[:], in0=mean[:], in1=mean[:], op=ALU.mult), reads=("mean",), writes=("mr",))
            S.op("dve", lambda e: e.tensor_tensor(out=rstd[:], in0=bank(7), in1=mr[:], op=ALU.subtract), reads=("ps7", "mr"),
                 writes=("rstd",))
            S.op("dve", lambda e: e.tensor_scalar_add(out=rstd[:], in0=rstd[:], scalar1=EPS), reads=("rstd",), writes=("rstd",))
            S.op("pool", lambda e: e.tensor_tensor(out=rstd[:], in0=rstd[:], in1=mhalf_t[:], op=ALU.pow),
                 reads=("rstd",), writes=("rstd",))
            S.op("dve", lambda e: e.tensor_tensor(out=mr[:], in0=mean[:], in1=rstd[:], op=ALU.mult), reads=("mean", "rstd"),
                 writes=("mr",))
            for c in range(8):
                S.op("dve", lambda e, c=c: e.tensor_tensor(out=tv[:], in0=zc[:, c, :], in1=rstd[:], op=ALU.mult),
                     reads=("zc", "rstd"), writes=("tv",))
                S.op("dve", lambda e: e.tensor_tensor(out=uu[:], in0=tv[:], in1=mr[:], op=ALU.subtract), reads=("tv", "mr"),
                     writes=("uu",))
                S.op("act", lambda e, c=c: e.activation(out=aa[:, c, :], in_=uu[:], func=AF.Silu, scale=lng_sb[:, c:c + 1],
                                                        bias=lnb_sb[:, c:c + 1]), reads=("uu", "params"), writes=("zc",))
            for half in range(2):
                wga, wgak = wpiece(4096 + 512 * half)
                wpa, wpak = wpiece(512 * half, w_pa)
                for m4 in range(4):
                    m = 4 * half + m4
                    par = m % 2
                    S.group("pe", [lambda e, k=k, m4=m4, par=par, wpa=wpa: e.matmul(bank(par), lhsT=wpa[:, k, 128 * m4:128 * m4 + 128],
                                                                         rhs=aa[:, k, :], start=(k == 0), stop=(k == 7))
                                   for k in range(8)], reads=("zc", wpak), writes=(pk(par),))
                    inproj(2 + par, wga, wgak, m4)
                    S.op("act", lambda e, m=m, par=par: e.activation(out=tv[:], in_=bank(2 + par), func=AF.Tanh, scale=0.5,
                                                                     bias=hb_in[:, 32 + m:33 + m]),
                         reads=(pk(2 + par), "hb_in"), writes=("tv",))
                    S.op("dve", lambda e, m=m, par=par: e.scalar_tensor_tensor(out=A_t[:, m, :], in0=tv[:], scalar=1.0,
                                                                               in1=bank(par), op0=ALU.add, op1=ALU.mult),
                         reads=("tv", pk(par)), writes=("A_t",))
            for half in range(2):
                wy, wyk = wpiece(2048 + 512 * half)
                for c4 in range(4):
                    c = 4 * half + c4
                    par = c % 2
                    inproj(par, wy, wyk, c4)
                    S.op("act", lambda e, c=c, par=par: e.activation(out=gy[:, c, :], in_=bank(par), func=AF.Gelu_apprx_tanh,
                                                                     bias=b_in_sb[:, 16 + c:17 + c]),
                         reads=(pk(par), "params"), writes=("gy",))
            for c in range(8):
                rglru_chunk(c, NB, 0, False, blk != 0, True)
                S.op("dve", lambda e, c=c: e.tensor_tensor(out=tmp1[:], in0=hf[:], in1=hs_in[:, c, :], op=ALU.add),
                     reads=("hf", "hsb"), writes=("tmp1",))
                S.op("dve", lambda e, c=c: e.tensor_tensor(out=yb[:, c, :], in0=tmp1[:], in1=gy[:, c, :], op=ALU.mult),
                     reads=("tmp1", "gy"), writes=("yb",))
            for half in range(2):
                wgb_, wgbk = wpiece(5120 + 512 * half)
                wpb, wpbk = wpiece(512 * half, w_pb)
                for m4 in range(4):
                    m = 4 * half + m4
                    par = m % 2
                    S.group("pe", [lambda e, k=k, m4=m4, par=par, wpb=wpb: e.matmul(bank(par), lhsT=wpb[:, k, 128 * m4:128 * m4 + 128],
                                                                         rhs=yb[:, k, :], start=(k == 0), stop=(k == 7))
                                   for k in range(8)], reads=("yb", wpbk), writes=(pk(par),))
                    inproj(2 + par, wgb_, wgbk, m4)
                    S.op("act", lambda e, m=m, par=par: e.activation(out=tv[:], in_=bank(2 + par), func=AF.Tanh, scale=0.5,
                                                                     bias=hb_in[:, 40 + m:41 + m]),
                         reads=(pk(2 + par), "hb_in"), writes=("tv",))
                    S.op("dve", lambda e, par=par: e.scalar_tensor_tensor(out=uu[:], in0=tv[:], scalar=1.0, in1=bank(par),
                                                                          op0=ALU.add, op1=ALU.mult),
                         reads=("tv", pk(par)), writes=("uu",))
                    S.op("dve", lambda e, m=m: e.tensor_tensor(out=mg[:, m, :], in0=uu[:], in1=A_t[:, m, :], op=ALU.add),
                         reads=("uu", "A_t"), writes=("gy",))
            for hh in range(2):
                wo, wok = wpiece(512 * hh, w_o)
                for j in range(4):
                    bk = 4 + j
                    S.group("pe", [lambda e, k=k, j=j, hh=hh, bk=bk, wo=wo: e.matmul(bank(bk), lhsT=mg[:, k, 128 * j:128 * j + 128],
                                                                              rhs=wo[:, k, :],
                                                                              start=(k == 0), stop=(k == 7)) for k in range(8)],
                            reads=("gy", wok), writes=(pk(bk),))
                    S.op("dve", lambda e, hh=hh, bk=bk: e.tensor_tensor(out=tv[:], in0=bank(bk), in1=gt1h[:, 512 * hh:512 * hh + 512],
                                                                        op=ALU.mult), reads=(pk(bk), "gt"), writes=("tv",))
                    S.op("pool", lambda e, j=j, hh=hh: e.tensor_tensor(out=x1[:, j, 512 * hh:512 * hh + 512], in0=tv[:],
                                                                       in1=xt[:, j, 512 * hh:512 * hh + 512], op=ALU.add),
                         reads=("tv", "xt"), writes=("xt",))
            S.dma("sp", x1_scr[NB * blk:NB * blk + NB, :].rearrange("(j p) d -> p j d", p=128), xt[:],
                  reads=("xt",), writes=("x1_scr%d" % blk,), key="x1_scr")
        S.barrier()
        pb_.close()
        mixer.close()

        pc = ExitStack()
        x1t = sb("x1t", [128, 4, 1024], F32, pc)
        xn2 = sb("xn2", [128, 4, 1024], BF16, pc)
        hmT = sb("hmT", [128, 8, NB], BF16, pc)
        accm = sb("accm", [128, 4, 1024], F32, pc)
        cbc = sb("cbc", [128, 16, NB], BF16, pc)
        dgm = sb("dgm", [128, 16, 128], BF16, pc)
        actb = [sb("actb%d" % i, [128, NB], BF16, pc) for i in range(16)]
        wgu = [sb("wgu%d" % i, [128, 2, 8, 512], BF16, pc) for i in range(2)]
        wd = [sb("wd%d" % i, [128, 4, 1024], BF16, pc) for i in range(6)]
        sg = [sb("sg%d" % i, [128, NB], F32, pc) for i in range(2)]
        tt = [sb("tt%d" % i, [128, NB], BF16, pc) for i in range(2)]
        L = sb("L", [128, 4, 20], F32, pc)
        gmax = sb("gmax", [128, 4, 1], F32, pc)
        oh = sb("oh", [128, 4, 4], F32, pc)
        eg = sb("eg", [128, 4, 4], F32, pc)
        pg = sb("pg", [128, 4, 1], F32, pc)
        tmp16 = sb("tmp16", [128, 4, 16], F32, pc)
        esel = sb("esel", [128, 4, 4], F32, pc)
        m1 = sb("m1", [128, 4, 1], F32, pc)
        m2 = sb("m2", [128, 4, 1], F32, pc)
        k1 = sb("k1", [128, 4, 4], F32, pc)
        k2 = sb("k2", [128, 4, 4], F32, pc)
        e2 = sb("e2", [128, 4, 4], F32, pc)
        w1 = sb("w1", [128, 4, 1], F32, pc)
        w2 = sb("w2", [128, 4, 1], F32, pc)
        wsel = sb("wsel", [128, 4, 4], F32, pc)
        comb = sb("comb", [128, 4, 16], F32, pc)
        ofin = accm
        ectr = [0]

        def bc(ap, shape):
            return ap.to_broadcast(shape)

        for blk in range(8):
            S.dma("sp", x1t[:], x1_scr[NB * blk:NB * blk + NB, :].rearrange("(j p) d -> p j d", p=128),
                  reads=("x1_scr%d" % blk,), writes=("x1t",), key="x1t")
            S.op("pool", lambda e: e.memset(ss[:], 0.0), writes=("ss",))
            for j in range(4):
                S.op("act", lambda e, j=j: e.activation(out=xn2[:, j, :], in_=x1t[:, j, :], func=AF.Square, accum_out=ss[:, j:j + 1]),
                     reads=("x1t",), writes=("xn2", "ss"))
            S.op("dve", lambda e: e.tensor_scalar_add(out=rs[:, 0:4], in0=ss[:, 0:4], scalar1=1024.0 * EPS), reads=("ss",), writes=("rs",))
            S.op("pool", lambda e: e.tensor_tensor(out=rs[:, 0:4], in0=rs[:, 0:4], in1=mhalf_t[:, 0:4], op=ALU.pow),
                 reads=("rs",), writes=("rs",))
            for j in range(4):
                S.op("pool", lambda e, j=j: e.tensor_scalar_mul(out=xn2[:, j, :], in0=x1t[:, j, :], scalar1=rs[:, j:j + 1]),
                     reads=("x1t", "rs"), writes=("xn2",))
            for j in range(4):
                S.group("pe", [lambda e, j=j, c=c: e.transpose(out=tpv[:, c, 128 * j:128 * j + 128],
                                                               in_=xn2[:, j, 128 * c:128 * c + 128], identity=identb[:])
                               for c in range(8)], reads=("xn2", "identb"), writes=TPK)
            for c in range(8):
                S.op("act", lambda e, c=c: e.activation(out=hmT[:, c, :], in_=tpv[:, c, :], func=AF.Identity,
                                                        scale=s2[:, c:c + 1], bias=mods[:, 24 + c, 0:1]),
                     reads=TPK + ("sc", "mods"), writes=("hmT",))
            for j in range(4):
                S.group("pe", [lambda e, k=k, j=j: e.matmul(bank(4)[:, 20 * j:20 * j + 20], lhsT=hmT[:, k, 128 * j:128 * j + 128],
                                                            rhs=w_rt_b[:, k, :], start=(k == 0), stop=(k == 7)) for k in range(8)],
                        reads=("hmT", "w_rt_b"), writes=("ps4",))
            S.op("dve", lambda e: e.tensor_tensor(out=L[:], in0=bank(4)[:, 0:80].rearrange("p (j n) -> p j n", n=20),
                                                  in1=bc(b_rt_sb[:].unsqueeze(1), [128, 4, 20]), op=ALU.add),
                 reads=("ps4", "params"), writes=("L",))
            R = ("rt",)
            S.op("dve", lambda e: e.tensor_reduce(out=gmax[:], in_=L[:, :, 0:4], axis=AX.X, op=ALU.max), reads=("L",), writes=R)
            S.op("dve", lambda e: e.tensor_tensor(out=oh[:], in0=L[:, :, 0:4], in1=bc(gmax[:], [128, 4, 4]), op=ALU.is_equal),
                 reads=R + ("L",), writes=R)
            S.op("dve", lambda e: e.tensor_tensor(out=eg[:], in0=L[:, :, 0:4], in1=bc(gmax[:], [128, 4, 4]), op=ALU.subtract),
                 reads=R + ("L",), writes=R)
            S.op("act", lambda e: e.activation(out=eg[:], in_=eg[:], func=AF.Exp), reads=R, writes=R)
            S.op("dve", lambda e: e.tensor_reduce(out=pg[:], in_=eg[:], axis=AX.X, op=ALU.add), reads=R, writes=R)
            S.op("dve", lambda e: e.reciprocal(out=pg[:], in_=pg[:]), reads=R, writes=R)
            S.op("dve", lambda e: e.tensor_tensor(out=tmp16[:].rearrange("p j (g x) -> p j g x", x=4),
                                                  in0=L[:, :, 4:20].rearrange("p j (g x) -> p j g x", x=4),
                                                  in1=bc(oh[:].unsqueeze(3), [128, 4, 4, 4]), op=ALU.mult),
                 reads=R + ("L",), writes=R)
            S.op("dve", lambda e: e.tensor_reduce(out=esel[:].unsqueeze(3), in_=tmp16[:].rearrange("p j (g x) -> p j x g", x=4),
                                                  axis=AX.X, op=ALU.add), reads=R, writes=R)
            S.op("dve", lambda e: e.tensor_reduce(out=m1[:], in_=esel[:], axis=AX.X, op=ALU.max), reads=R, writes=R)
            S.op("dve", lambda e: e.tensor_tensor(out=k1[:], in0=esel[:], in1=bc(m1[:], [128, 4, 4]), op=ALU.is_equal),
                 reads=R, writes=R)
            S.op("dve", lambda e: e.scalar_tensor_tensor(out=e2[:], in0=k1[:], scalar=-1e30, in1=esel[:], op0=ALU.mult, op1=ALU.add),
                 reads=R, writes=R)
            S.op("dve", lambda e: e.tensor_reduce(out=m2[:], in_=e2[:], axis=AX.X, op=ALU.max), reads=R, writes=R)
            S.op("dve", lambda e: e.tensor_tensor(out=k2[:], in0=e2[:], in1=bc(m2[:], [128, 4, 4]), op=ALU.is_equal),
                 reads=R, writes=R)
            S.op("dve", lambda e: e.tensor_tensor(out=w2[:], in0=m2[:], in1=m1[:], op=ALU.subtract), reads=R, writes=R)
            S.op("act", lambda e: e.activation(out=w2[:], in_=w2[:], func=AF.Exp), reads=R, writes=R)
            S.op("dve", lambda e: e.tensor_scalar_add(out=w1[:], in0=w2[:], scalar1=1.0), reads=R, writes=R)
            S.op("dve", lambda e: e.reciprocal(out=w1[:], in_=w1[:]), reads=R, writes=R)
            S.op("dve", lambda e: e.tensor_tensor(out=w2[:], in0=w2[:], in1=w1[:], op=ALU.mult), reads=R, writes=R)
            S.op("dve", lambda e: e.tensor_tensor(out=w1[:], in0=w1[:], in1=pg[:], op=ALU.mult), reads=R, writes=R)
            S.op("dve", lambda e: e.tensor_tensor(out=w2[:], in0=w2[:], in1=pg[:], op=ALU.mult), reads=R, writes=R)
            S.op("dve", lambda e: e.tensor_tensor(out=wsel[:], in0=k1[:], in1=bc(w1[:], [128, 4, 4]), op=ALU.mult), reads=R, writes=R)
            S.op("dve", lambda e: e.tensor_tensor(out=k2[:], in0=k2[:], in1=bc(w2[:], [128, 4, 4]), op=ALU.mult), reads=R, writes=R)
            S.op("dve", lambda e: e.tensor_tensor(out=wsel[:], in0=wsel[:], in1=k2[:], op=ALU.add), reads=R, writes=R)
            S.op("dve", lambda e: e.tensor_tensor(out=comb[:].rearrange("p j (g x) -> p j g x", x=4),
                                                  in0=bc(oh[:].unsqueeze(3), [128, 4, 4, 4]),
                                                  in1=bc(wsel[:].unsqueeze(2), [128, 4, 4, 4]), op=ALU.mult),
                 reads=R, writes=("comb",))
            for j in range(4):
                S.op("pool", lambda e, j=j: e.tensor_tensor(out=dgm[:], in0=bc(identb[:].unsqueeze(1), [128, 16, 128]),
                                                            in1=bc(comb[:, j, :].unsqueeze(2), [128, 16, 128]), op=ALU.mult),
                     reads=("identb", "comb"), writes=("dgm",))
                for q in range(4):
                    S.group("pe", [lambda e, q=q: e.matmul(bank(4 + q), lhsT=ones1[:], rhs=dgm[:, 4 * q:4 * q + 4, :],
                                                           start=True, stop=True)], reads=("dgm", "ones1"), writes=(pk(4 + q),))
                    S.op("act", lambda e, q=q, j=j: e.activation(out=cbc[:, 4 * q:4 * q + 4, 128 * j:128 * j + 128],
                                                                 in_=bank(4 + q).rearrange("p (x t) -> p x t", t=128), func=AF.Copy),
                         reads=(pk(4 + q),), writes=("cbc",))
            for g in range(4):
                for el in range(4):
                    ex = 4 * g + el
                    si = ectr[0] % 2
                    di = ectr[0] % 6
                    ectr[0] += 1
                    gk, dk_ = "wgu%d" % si, "wd%d" % di
                    S.dma("pool", wgu[si][:, 0], w_gate[ex].rearrange("(k p) n -> p k n", p=128), writes=(gk,), key=gk)
                    S.dma("pool", wgu[si][:, 1], w_up[ex].rearrange("(k p) n -> p k n", p=128), writes=(gk,), key=gk)
                    S.dma("pool", wd[di][:], w_down[ex].rearrange("(k p) n -> p k n", p=128), writes=(dk_,), key=dk_)
                    for f in range(4):
                        u = 4 * el + f
                        pp = u % 2
                        S.group("pe", [lambda e, k=k, f=f, si=si, pp=pp: e.matmul(
                            bank(2 * pp), lhsT=wgu[si][:, 0, k, 128 * f:128 * f + 128], rhs=hmT[:, k, :],
                            start=(k == 0), stop=(k == 7)) for k in range(8)], reads=("hmT", gk), writes=(pk(2 * pp),))
                        S.group("pe", [lambda e, k=k, f=f, si=si, pp=pp: e.matmul(
                            bank(2 * pp + 1), lhsT=wgu[si][:, 1, k, 128 * f:128 * f + 128], rhs=hmT[:, k, :],
                            start=(k == 0), stop=(k == 7)) for k in range(8)], reads=("hmT", gk), writes=(pk(2 * pp + 1),))
                        S.op("act", lambda e, pp=pp: e.activation(out=sg[pp][:], in_=bank(2 * pp), func=AF.Silu),
                             reads=(pk(2 * pp),), writes=("sg%d" % pp,))
                        S.op("dve", lambda e, pp=pp: e.tensor_tensor(out=tt[pp][:], in0=bank(2 * pp + 1), in1=sg[pp][:], op=ALU.mult),
                             reads=(pk(2 * pp + 1), "sg%d" % pp), writes=("tt%d" % pp,))
                        S.op("pool", lambda e, pp=pp, u=u, ex=ex: e.tensor_tensor(out=actb[u][:], in0=tt[pp][:], in1=cbc[:, ex, :],
                                                                                 op=ALU.mult),
                             reads=("tt%d" % pp, "cbc"), writes=("actb%d" % u,))
                dbase = ectr[0] - 4
                for tp_ in range(2):
                    fns = []
                    for u in range(16):
                        el, f = divmod(u, 4)
                        di = (dbase + el) % 6
                        for jj in range(2):
                            j = 2 * tp_ + jj
                            for hh in range(2):
                                fns.append(lambda e, u=u, f=f, di=di, j=j, jj=jj, hh=hh: e.matmul(
                                    bank(4 + 2 * jj + hh), lhsT=actb[u][:, 128 * j:128 * j + 128],
                                    rhs=wd[di][:, f, 512 * hh:512 * hh + 512], start=(u == 0), stop=(u == 15)))
                    S.group("pe", fns, reads=tuple("actb%d" % u for u in range(16)) + tuple("wd%d" % ((dbase + el) % 6) for el in range(4)),
                            writes=("ps4", "ps5", "ps6", "ps7"))
                    for jj in range(2):
                        j = 2 * tp_ + jj
                        for hh in range(2):
                            bk = 4 + 2 * jj + hh
                            dst = accm[:, j, 512 * hh:512 * hh + 512]
                            if g == 0:
                                S.op("act", lambda e, dst=dst, bk=bk: e.activation(out=dst, in_=bank(bk), func=AF.Copy),
                                     reads=(pk(bk),), writes=("accm",))
                            else:
                                S.op("dve", lambda e, dst=dst, bk=bk: e.tensor_tensor(out=dst, in0=dst, in1=bank(bk), op=ALU.add),
                                     reads=(pk(bk), "accm"), writes=("accm",))
            S.op("pool", lambda e: e.memset(ss[:], 0.0), writes=("ss",))
            for j in range(4):
                S.op("dve", lambda e, j=j: e.tensor_tensor(out=accm[:, j, :], in0=accm[:, j, :], in1=gt2b[:], op=ALU.mult),
                     reads=("accm", "gt"), writes=("accm",))
                S.op("pool", lambda e, j=j: e.tensor_tensor(out=accm[:, j, :], in0=accm[:, j, :], in1=x1t[:, j, :], op=ALU.add),
                     reads=("accm", "x1t"), writes=("accm",))
                S.op("act", lambda e, j=j: e.activation(out=xn2[:, j, :], in_=accm[:, j, :], func=AF.Square, accum_out=ss[:, j:j + 1]),
                     reads=("accm",), writes=("xn2", "ss"))
            S.op("dve", lambda e: e.tensor_scalar_add(out=rs[:, 0:4], in0=ss[:, 0:4], scalar1=1024.0 * EPS), reads=("ss",), writes=("rs",))
            S.op("pool", lambda e: e.tensor_tensor(out=rs[:, 0:4], in0=rs[:, 0:4], in1=mhalf_t[:, 0:4], op=ALU.pow),
                 reads=("rs",), writes=("rs",))
            for j in range(4):
                S.op("dve", lambda e, j=j: e.scalar_tensor_tensor(out=ofin[:, j, :], in0=accm[:, j, :], scalar=rs[:, j:j + 1],
                                                                  in1=gf32[:], op0=ALU.mult, op1=ALU.mult),
                     reads=("accm", "rs", "gf32"), writes=("accm",))
            S.dma("sp", out[NB * blk:NB * blk + NB, :].rearrange("(j p) d -> p j d", p=128), accm[:],
                  reads=("accm",), writes=("out%d" % blk,), key="out")
        S.barrier()
        pc.close()
    return nc


_NC_CACHE = {}


def _fm(v):
    v = np.asarray(v, np.float32).reshape(-1, 128)
    return np.ascontiguousarray(v.T)


def kernel(x, c, ctx, c_ctx, w_ada, b_ada, g_mix, w_in, b_in, conv_w, conv_b, ln_g, ln_b, w_pa,
           lru_conv_w, lru_conv_b, w_r_f, b_r_f, w_i_f, b_i_f, lam_f, w_r_b, b_r_b, w_i_b, b_i_b, lam_b,
           w_pb, w_o, g_ffn, w_grp, b_grp, w_er, b_er, w_gate, w_up, w_down, g_final):
    f = lambda a: np.ascontiguousarray(np.asarray(a, np.float32))
    x, c, ctx, c_ctx = f(x), f(c), f(ctx), f(c_ctx)
    B = x.shape[0]
    if "nc" not in _NC_CACHE:
        _NC_CACHE["nc"] = build_program()
    nc = _NC_CACHE["nc"]

    common = {
        "w_ada": f(w_ada[0]), "b_ada_fm": _fm(b_ada[0]),
        "b_ada_gt": f(np.broadcast_to(np.stack([b_ada[0][2048:3072], b_ada[0][5120:6144]])[None], (128, 2, 1024))),
        "w_in": f(w_in[0]), "b_in_fm": _fm(b_in[0]),
        "cb": _fm(conv_b[0]), "lng": _fm(ln_g[0]), "lnb": _fm(ln_b[0]),
        "w_pa": f(w_pa[0]), "w_pb": f(w_pb[0]), "w_o": f(w_o[0]),
        "lb": _fm(lru_conv_b[0]),
        "gmix": _fm(g_mix[0]), "gffn": _fm(g_ffn[0]),
        "gfin": f(np.broadcast_to(np.asarray(g_final, np.float32)[None], (128, 1024))),
        "w_rt": f(np.concatenate([w_grp[0], w_er[0]], axis=1)),
        "b_rt": f(np.broadcast_to(np.concatenate([b_grp[0], b_er[0]])[None], (128, 20))),
        "w_gate": f(w_gate[0]), "w_up": f(w_up[0]), "w_down": f(w_down[0]),
        "ident": np.eye(128, dtype=np.float32),
    }
    cwn = np.asarray(conv_w[0], np.float32)
    lwn = np.asarray(lru_conv_w[0], np.float32)
    zero = np.zeros((1, 1024), np.float32)
    lw5_nat = np.concatenate([lwn, zero], axis=0)
    lw5_rev = lw5_nat[::-1]

    def fm3(a):
        T = a.shape[0]
        return np.ascontiguousarray(a.reshape(T, 8, 128).transpose(2, 1, 0))

    pf = (w_r_f[0], b_r_f[0], w_i_f[0], b_i_f[0], lam_f[0])
    pbk = (w_r_b[0], b_r_b[0], w_i_b[0], b_i_b[0], lam_b[0])

    def gates(P, Sd):
        wgs = np.stack([P[0], P[2], Sd[0], Sd[2]]).astype(np.float32)
        bgs = np.stack([np.asarray(t, np.float32) for t in (P[1], P[3], Sd[1], Sd[3])])
        bgs = np.ascontiguousarray(bgs.transpose(2, 0, 1))
        lams = np.stack([np.asarray(P[4], np.float32).reshape(8, 128), np.asarray(Sd[4], np.float32).reshape(8, 128)])
        lams = np.ascontiguousarray(lams.transpose(2, 0, 1))
        return f(wgs), bgs, lams

    per_half = []
    for half in range(2):
        if half == 0:
            wgs, bgs, lams = gates(pf, pbk)
            d = {"cw": fm3(cwn), "lw5": fm3(lw5_nat), "wg": wgs, "bg": bgs, "lam": lams}
        else:
            wgs, bgs, lams = gates(pbk, pf)
            d = {"cw": fm3(cwn[::-1]), "lw5": fm3(lw5_rev), "wg": wgs, "bg": bgs, "lam": lams}
        per_half.append(d)

    in_maps = []
    pad2 = np.zeros((2, 1024), np.float32)
    for b in range(B):
        for half in range(2):
            xs = x[b] if half == 0 else x[b, ::-1]
            cs_ = ctx[b] if half == 0 else ctx[b, ::-1]
            m = dict(common)
            m.update(per_half[half])
            m["xp"] = np.ascontiguousarray(np.concatenate([pad2, xs, pad2], axis=0))
            m["ctxp"] = np.ascontiguousarray(np.concatenate([pad2, cs_, pad2], axis=0))
            m["cvec"] = np.ascontiguousarray(np.stack([_fm(c[b]), _fm(c_ctx)], axis=-1))
            in_maps.append(m)
    res = run_bass_kernel_spmd(nc, in_maps, core_ids=list(range(2 * B)))
    outp = np.empty((B, 2 * NOWN, 1024), np.float32)
    for b in range(B):
        outp[b, :NOWN] = res.results[2 * b]["out"]
        outp[b, NOWN:] = res.results[2 * b + 1]["out"][::-1]
    if DEBUG:
        kernel.last = res
    return outp
```

```python
from contextlib import ExitStack
import os
import numpy as np
import concourse.bass as bass
import concourse.mybir as mybir
from concourse.bass_utils import run_bass_kernel_spmd

F32 = mybir.dt.float32
BF16 = mybir.dt.bfloat16
AF = mybir.ActivationFunctionType
ALU = mybir.AluOpType
AX = mybir.AxisListType
EPS = 1e-6
NB = 512
NOWN = 4096
DEBUG = bool(int(os.environ.get("MK_DEBUG", "0")))


class Sched:
    def __init__(self, nc, es):
        self.nc = nc
        self.es = es
        self.E = dict(pe=nc.tensor, act=nc.scalar, dve=nc.vector, pool=nc.gpsimd, sp=nc.sync)
        self.sem = {e: es.enter_context(nc.semaphore("c_" + e)) for e in self.E}
        self.cnt = {e: 0 for e in self.E}
        self.seen = {e: {} for e in self.E}
        self.lastw = {}
        self.readers = {}
        self.dsem = {}
        self.dcnt = {}

    def _wait(self, e, tok, same_ok=False):
        if tok is None:
            return
        name, sem, val, src = tok
        if same_ok and src == e:
            return
        d = self.seen[e]
        if d.get(name, 0) >= val:
            return
        self.E[e].wait_ge(sem, val)
        d[name] = val

    def deps(self, e, reads, writes):
        for k in reads:
            self._wait(e, self.lastw.get(k))
        for k in writes:
            self._wait(e, self.lastw.get(k), same_ok=True)
            for t in self.readers.get(k, {}).values():
                self._wait(e, t, same_ok=True)

    def commit(self, tok, reads, writes):
        for k in reads:
            self.readers.setdefault(k, {})[tok[0]] = tok
        for k in writes:
            self.lastw[k] = tok
            self.readers[k] = {}

    def op(self, e, fn, reads=(), writes=()):
        self.deps(e, reads, writes)
        ins = fn(self.E[e])
        self.cnt[e] += 1
        ins.then_inc(self.sem[e], 1)
        self.commit(("c_" + e, self.sem[e], self.cnt[e], e), reads, writes)

    def group(self, e, fns, reads=(), writes=()):
        self.deps(e, reads, writes)
        ins = None
        for f in fns:
            ins = f(self.E[e])
        self.cnt[e] += 1
        ins.then_inc(self.sem[e], 1)
        self.commit(("c_" + e, self.sem[e], self.cnt[e], e), reads, writes)

    def dma(self, q, out, in_, reads=(), writes=(), key=None):
        self.deps(q, reads, writes)
        if key not in self.dsem:
            self.dsem[key] = self.es.enter_context(self.nc.semaphore("d_" + key))
            self.dcnt[key] = 0
        ins = self.E[q].dma_start(out=out, in_=in_)
        self.dcnt[key] += 16
        ins.then_inc(self.dsem[key], 16)
        self.commit(("d_" + key, self.dsem[key], self.dcnt[key], "dma"), reads, writes)

    def final_wait(self, e, keys):
        for k in keys:
            self._wait(e, self.lastw.get(k))

    def barrier(self):
        for e in self.E:
            for e2 in self.E:
                if self.cnt[e2] > 0:
                    self._wait(e, ("c_" + e2, self.sem[e2], self.cnt[e2], e2))
            for k, sem in self.dsem.items():
                self._wait(e, ("d_" + k, sem, self.dcnt[k], "dma"))


def build_program():
    nc = bass.Bass("TRN2", target_bir_lowering=False)

    def din(name, shape):
        return nc.dram_tensor(name, list(shape), F32, kind="ExternalInput").ap()

    xp = din("xp", [8196, 1024])
    ctxp = din("ctxp", [260, 1024])
    cvec = din("cvec", [128, 8, 2])
    w_ada = din("w_ada", [1024, 6144])
    b_ada_fm = din("b_ada_fm", [128, 48])
    b_ada_gt = din("b_ada_gt", [128, 2, 1024])
    w_in = din("w_in", [1024, 6144])
    b_in_fm = din("b_in_fm", [128, 48])
    cw = din("cw", [128, 8, 31])
    cb = din("cb", [128, 8])
    lng = din("lng", [128, 8])
    lnb = din("lnb", [128, 8])
    w_pa = din("w_pa", [1024, 1024])
    w_pb = din("w_pb", [1024, 1024])
    w_o = din("w_o", [1024, 1024])
    lw5 = din("lw5", [128, 8, 5])
    lb = din("lb", [128, 8])
    wg = din("wg", [4, 8, 128, 128])
    bg = din("bg", [128, 4, 8])
    lam = din("lam", [128, 2, 8])
    gmix = din("gmix", [128, 8])
    gffn = din("gffn", [128, 8])
    gfin = din("gfin", [128, 1024])
    w_rt = din("w_rt", [1024, 20])
    b_rt = din("b_rt", [128, 20])
    w_gate = din("w_gate", [16, 1024, 512])
    w_up = din("w_up", [16, 1024, 512])
    w_down = din("w_down", [16, 512, 1024])
    ident = din("ident", [128, 128])
    out = nc.dram_tensor("out", [NOWN, 1024], F32, kind="ExternalOutput").ap()
    if DEBUG:
        hs_scr = nc.dram_tensor("hs_scr", [8, 128, 8, NB], BF16, kind="ExternalOutput").ap()
        x1_scr = nc.dram_tensor("x1_scr", [NOWN, 1024], F32, kind="ExternalOutput").ap()
    else:
        hs_scr = nc.dram_tensor("hs_scr", [8, 128, 8, NB], BF16, kind="Internal").ap()
        x1_scr = nc.dram_tensor("x1_scr", [NOWN, 1024], F32, kind="Internal").ap()

    with ExitStack() as es:
        S = Sched(nc, es)

        def sb(name, shape, dt=F32, stack=es):
            return stack.enter_context(nc.sbuf_tensor(name, list(shape), dt))

        psA = es.enter_context(nc.psum_tensor("psA", [128, 2048], F32))
        psB = es.enter_context(nc.psum_tensor("psB", [128, 2048], F32))

        def bank(i):
            t = psA if i < 4 else psB
            return t[:, 512 * (i % 4):512 * (i % 4) + 512]

        def pk(i):
            return "ps%d" % i

        tpv = psA[:, :].bitcast(BF16).rearrange("p (c t) -> p c t", t=512)
        TPK = ("ps0", "ps1", "ps2", "ps3")

        identb = sb("identb", [128, 128], BF16)
        ones_m = sb("ones_m", [128, 128], BF16)
        ones1 = sb("ones1", [128, 128], BF16)
        b_in_sb = sb("b_in_sb", [128, 48])
        hb_in = sb("hb_in", [128, 48])
        cw_sb = sb("cw_sb", [128, 8, 31])
        cb_sb = sb("cb_sb", [128, 8])
        lng_sb = sb("lng_sb", [128, 8])
        lnb_sb = sb("lnb_sb", [128, 8])
        lw5_sb = sb("lw5_sb", [128, 8, 5])
        lb_sb = sb("lb_sb", [128, 8])
        bg_sb = sb("bg_sb", [128, 4, 8])
        hbg = sb("hbg", [128, 4, 8])
        lam_sb = sb("lam_sb", [128, 2, 8])
        gmix_sb = sb("gmix_sb", [128, 8])
        gffn_sb = sb("gffn_sb", [128, 8])
        gf32 = sb("gf32", [128, 1024])
        b_rt_sb = sb("b_rt_sb", [128, 20])
        b_ada_fm_sb = sb("b_ada_fm_sb", [128, 48])
        cvec_sb = sb("cvec_sb", [128, 8, 2])
        mods = sb("mods", [128, 48, 2])
        s1 = sb("s1", [128, 8])
        s1c = sb("s1c", [128, 8])
        s2 = sb("s2", [128, 8])
        gt1h = sb("gt1h", [128, 1024])
        gt2b = sb("gt2b", [128, 1024])
        cl = sb("cl", [128, 2, 8])
        hcl = sb("hcl", [128, 2, 8])
        state = sb("state", [128, 2, 8])
        ss = sb("ss", [128, 8])
        rs = sb("rs", [128, 8])
        w_rt_b = sb("w_rt_b", [128, 8, 20], BF16)
        qtr = sb("qtr", [128, 4], F32)

        def pload(t, src):
            S.dma("sp", t, src, writes=("params",), key="params")

        pload(b_in_sb[:], b_in_fm)
        pload(cw_sb[:], cw)
        pload(cb_sb[:], cb)
        pload(lng_sb[:], lng)
        pload(lnb_sb[:], lnb)
        pload(lw5_sb[:], lw5)
        pload(lb_sb[:], lb)
        pload(bg_sb[:], bg)
        pload(lam_sb[:], lam)
        pload(gmix_sb[:], gmix)
        pload(gffn_sb[:], gffn)
        pload(gf32[:], gfin)
        pload(b_rt_sb[:], b_rt)
        pload(b_ada_fm_sb[:], b_ada_fm)
        pload(cvec_sb[:], cvec)
        S.dma("pool", identb[:], ident, writes=("identb",), key="identb")
        S.dma("pool", w_rt_b[:], w_rt.rearrange("(k p) n -> p k n", p=128), writes=("w_rt_b",), key="w_rt_b")
        S.op("pool", lambda e: e.memset(ones_m[:], 1.0 / 1024.0), writes=("ones_m",))
        S.op("pool", lambda e: e.memset(ones1[:], 1.0), writes=("ones1",))
        S.op("pool", lambda e: e.memset(qtr[:, 0:1], 0.25), writes=("qtr",))
        S.op("pool", lambda e: e.memset(qtr[:, 1:2], 1024.0 * EPS), writes=("qtr",))
        S.op("pool", lambda e: e.memset(qtr[:, 2:3], EPS), writes=("qtr",))
        S.op("pool", lambda e: e.memset(state[:], 0.0), writes=("state",))
        S.op("pool", lambda e: e.memset(ss[:], 0.0), writes=("ss",))

        with ExitStack() as p0:
            cs = sb("cs", [128, 8, 2], BF16, p0)
            cs_rep = sb("cs_rep", [128, 8, 128], BF16, p0)
            b_ada_gt_sb = sb("b_ada_gt_sb", [128, 2, 1024], F32, p0)
            wa = [sb("wa%d" % i, [128, 8, 512], BF16, p0) for i in range(3)]
            e_t = sb("e_t", [128, 16], F32, p0)
            t_t = sb("t_t", [128, 16], F32, p0)
            l_t = sb("l_t", [128, 16], F32, p0)
            m_t = sb("m_t", [128, 16], F32, p0)
            pload(b_ada_gt_sb[:], b_ada_gt)

            S.op("act", lambda e: e.activation(out=cs[:], in_=cvec_sb[:], func=AF.Silu), reads=("params",), writes=("cs",))
            S.op("dve", lambda e: e.tensor_copy(out=cs_rep[:], in_=cs[:, :, 0:1].to_broadcast([128, 8, 128])),
                 reads=("cs",), writes=("cs_rep",))
            psm = bank(0)[:, 0:96].rearrange("p (j t) -> p j t", t=2)
            for q in range(12):
                s = q % 3
                S.dma("pool", wa[s][:], w_ada[:, 512 * q:512 * q + 512].rearrange("(k p) n -> p k n", p=128),
                      writes=("wa%d" % s,), key="wa%d" % s)
                fns = []
                for jj in range(4):
                    for k in range(8):
                        fns.append(lambda e, jj=jj, k=k, s=s, q=q: e.matmul(
                            psm[:, 4 * q + jj, :], lhsT=wa[s][:, k, 128 * jj:128 * jj + 128], rhs=cs[:, k, :],
                            start=(k == 0), stop=(k == 7)))
                S.group("pe", fns, reads=("wa%d" % s, "cs"), writes=("ps0",))
                if q in (4, 5, 10, 11):
                    bk = 1 + (q % 2)
                    S.group("pe", [lambda e, k=k, s=s, bk=bk: e.matmul(bank(bk), lhsT=cs_rep[:, k, :], rhs=wa[s][:, k, :],
                                                                      start=(k == 0), stop=(k == 7)) for k in range(8)],
                            reads=("wa%d" % s, "cs_rep"), writes=(pk(bk),))
                    dst = gt1h if q < 6 else gt2b
                    gi = 0 if q < 6 else 1
                    cols = slice(512 * (q % 2), 512 * (q % 2) + 512)
                    S.op("dve", lambda e, dst=dst, gi=gi, cols=cols, bk=bk: e.tensor_tensor(
                        out=dst[:, cols], in0=bank(bk), in1=b_ada_gt_sb[:, gi, cols], op=ALU.add),
                        reads=(pk(bk), "params"), writes=("gt",))
            S.op("dve", lambda e: e.tensor_scalar_mul(out=gt1h[:], in0=gt1h[:], scalar1=0.5), reads=("gt",), writes=("gt",))
            S.op("dve", lambda e: e.tensor_tensor(out=mods[:], in0=psm, in1=b_ada_fm_sb[:].unsqueeze(2).to_broadcast([128, 48, 2]),
                                                  op=ALU.add), reads=("ps0", "params"), writes=("mods",))
            for (dst, col, j0, gsb) in ((s1, 0, 8, gmix_sb), (s1c, 1, 8, gmix_sb), (s2, 0, 32, gffn_sb)):
                S.op("dve", lambda e, dst=dst, col=col, j0=j0, gsb=gsb: e.scalar_tensor_tensor(
                    out=dst[:], in0=mods[:, j0:j0 + 8, col], scalar=1.0, in1=gsb[:], op0=ALU.add, op1=ALU.mult),
                    reads=("mods", "params"), writes=("sc",))
                S.op("dve", lambda e, dst=dst: e.tensor_scalar_mul(out=dst[:], in0=dst[:], scalar1=32.0),
                     reads=("sc",), writes=("sc",))
            S.op("dve", lambda e: e.tensor_scalar_mul(out=gf32[:], in0=gf32[:], scalar1=32.0), reads=("params",), writes=("gf32",))
            S.op("dve", lambda e: e.tensor_scalar_mul(out=hb_in[:], in0=b_in_sb[:], scalar1=0.5), reads=("params",), writes=("hb_in",))
            S.op("dve", lambda e: e.tensor_scalar_mul(out=hbg[:], in0=bg_sb[:], scalar1=0.5), reads=("params",), writes=("hbg",))
            lamf = lam_sb[:].rearrange("p a b -> p (a b)")
            S.op("act", lambda e: e.activation(out=e_t[:], in_=lamf, func=AF.Exp, scale=-1.0), reads=("params",), writes=("e_t",))
            S.op("dve", lambda e: e.tensor_scalar(out=t_t[:], in0=e_t[:], scalar1=-0.25, scalar2=1.0 / 3.0, op0=ALU.mult, op1=ALU.add),
                 reads=("e_t",), writes=("t_t",))
            S.op("dve", lambda e: e.tensor_tensor(out=t_t[:], in0=t_t[:], in1=e_t[:], op=ALU.mult), reads=("t_t", "e_t"), writes=("t_t",))
            S.op("dve", lambda e: e.tensor_scalar_add(out=t_t[:], in0=t_t[:], scalar1=-0.5), reads=("t_t",), writes=("t_t",))
            S.op("dve", lambda e: e.tensor_tensor(out=t_t[:], in0=t_t[:], in1=e_t[:], op=ALU.mult), reads=("t_t", "e_t"), writes=("t_t",))
            S.op("dve", lambda e: e.tensor_scalar_add(out=t_t[:], in0=t_t[:], scalar1=1.0), reads=("t_t",), writes=("t_t",))
            S.op("dve", lambda e: e.tensor_tensor(out=t_t[:], in0=t_t[:], in1=e_t[:], op=ALU.mult), reads=("t_t", "e_t"), writes=("t_t",))
            S.op("dve", lambda e: e.tensor_scalar_add(out=l_t[:], in0=e_t[:], scalar1=1.0), reads=("e_t",), writes=("l_t",))
            S.op("act", lambda e: e.activation(out=l_t[:], in_=l_t[:], func=AF.Ln), reads=("l_t",), writes=("l_t",))
            S.op("dve", lambda e: e.tensor_single_scalar(out=m_t[:], in_=e_t[:], scalar=0.1, op=ALU.is_lt), reads=("e_t",), writes=("m_t",))
            S.op("dve", lambda e: e.tensor_tensor(out=t_t[:], in0=t_t[:], in1=l_t[:], op=ALU.subtract), reads=("t_t", "l_t"), writes=("t_t",))
            S.op("dve", lambda e: e.tensor_tensor(out=t_t[:], in0=t_t[:], in1=m_t[:], op=ALU.mult), reads=("t_t", "m_t"), writes=("t_t",))
            S.op("dve", lambda e: e.tensor_tensor(out=t_t[:], in0=t_t[:], in1=l_t[:], op=ALU.add), reads=("t_t", "l_t"), writes=("t_t",))
            clf = cl[:].rearrange("p a b -> p (a b)")
            hclf = hcl[:].rearrange("p a b -> p (a b)")
            S.op("dve", lambda e: e.tensor_scalar_mul(out=clf, in0=t_t[:], scalar1=-8.0), reads=("t_t",), writes=("cl",))
            S.op("dve", lambda e: e.tensor_scalar_mul(out=hclf, in0=t_t[:], scalar1=-4.0), reads=("t_t",), writes=("cl",))
            S.barrier()

        mixer = ExitStack()
        wxr = sb("wxr", [128, 8, 1024], BF16, mixer)
        wgb = sb("wgb", [128, 4, 8, 128], BF16, mixer)
        dg5 = sb("dg5", [128, 8, 5, 128], BF16, mixer)
        S.dma("pool", wxr[:], w_in[:, 3072:4096].rearrange("(k p) n -> p k n", p=128), writes=("wxr",), key="wxr")
        S.dma("pool", wgb[:], wg.rearrange("g h p n -> p g h n"), writes=("wgb",), key="wgb")
        for c in range(8):
            S.op("dve", lambda e, c=c: e.tensor_tensor(
                out=dg5[:, c, :, :], in0=identb[:].unsqueeze(1).to_broadcast([128, 5, 128]),
                in1=lw5_sb[:, c, :].unsqueeze(2).to_broadcast([128, 5, 128]), op=ALU.mult),
                reads=("identb", "params"), writes=("dg5",))

        xt = sb("xt", [128, 4, 1024], F32, mixer)
        xh = sb("xh", [4, 1024], F32, mixer)
        xn = sb("xn", [128, 4, 1024], BF16, mixer)
        xnh = sb("xnh", [4, 1024], BF16, mixer)
        hxT = sb("hxT", [128, 8, NB + 4], BF16, mixer)
        xrp = [sb("xrp%d" % i, [128, NB + 4], BF16, mixer) for i in range(2)]
        xcb = [sb("xcb%d" % i, [128, NB], BF16, mixer) for i in range(2)]
        tr = sb("tr", [128, NB], F32, mixer)
        ti = sb("ti", [128, NB], F32, mixer)
        a4 = sb("a4", [128, 4, NB], F32, mixer)
        s4 = sb("s4", [128, 4, NB], F32, mixer)
        t4 = sb("t4", [128, 4, NB], BF16, mixer)
        tmp1 = sb("tmp1", [128, NB], F32, mixer)
        bb_t = sb("bb_t", [128, NB], F32, mixer)
        hf = sb("hf", [128, NB], F32, mixer)
        hsb = sb("hsb", [128, 8, NB], BF16, mixer)

        tph = bank(4).bitcast(BF16)[:, 0:32].rearrange("p (c t) -> p c t", t=4)

        def prep(xsrc, r0, N, sc, bcol, keep_key):
            nt = N // 128
            S.dma("sp", xt[:, 0:nt, :], xsrc[r0:r0 + N, :].rearrange("(j p) d -> p j d", p=128), writes=(keep_key,), key="xt")
            S.dma("sp", xh[0:2, :], xsrc[r0 - 2:r0, :], writes=("xh",), key="xh")
            S.dma("sp", xh[2:4, :], xsrc[r0 + N:r0 + N + 2, :], writes=("xh",), key="xh")
            S.op("pool", lambda e: e.memset(ss[:], 0.0), writes=("ss",))
            for j in range(nt):
                S.op("act", lambda e, j=j: e.activation(out=xn[:, j, :], in_=xt[:, j, :], func=AF.Square, accum_out=ss[:, j:j + 1]),
                     reads=(keep_key,), writes=("xn", "ss"))
            S.op("act", lambda e: e.activation(out=xnh[:], in_=xh[:], func=AF.Square, accum_out=ss[0:4, 4:5]),
                 reads=("xh",), writes=("xnh", "ss"))
            S.op("act", lambda e: e.activation(out=rs[:, 0:5], in_=ss[:, 0:5], func=AF.Sqrt, bias=qtr[:, 1:2]), reads=("ss", "qtr"), writes=("rs",))
            S.op("dve", lambda e: e.reciprocal(out=rs[:, 0:5], in_=rs[:, 0:5]), reads=("rs",), writes=("rs",))
            for j in range(nt):
                S.op("dve", lambda e, j=j: e.tensor_scalar_mul(out=xn[:, j, :], in0=xt[:, j, :], scalar1=rs[:, j:j + 1]),
                     reads=(keep_key, "rs"), writes=("xn",))
            S.op("dve", lambda e: e.tensor_scalar_mul(out=xnh[:], in0=xh[:], scalar1=rs[0:4, 4:5]),
                 reads=("xh", "rs"), writes=("xnh",))
            for j in range(nt):
                S.group("pe", [lambda e, j=j, c=c: e.transpose(out=tpv[:, c, 128 * j:128 * j + 128],
                                                               in_=xn[:, j, 128 * c:128 * c + 128], identity=identb[:])
                               for c in range(8)], reads=("xn", "identb"), writes=TPK)
            S.group("pe", [lambda e, c=c: e.transpose(out=tph[:, c, :], in_=xnh[:, 128 * c:128 * c + 128], identity=identb[0:4, 0:4])
                           for c in range(8)], reads=("xnh", "identb"), writes=("ps4",))
            for c in range(8):
                S.op("act", lambda e, c=c: e.activation(out=hxT[:, c, 0:N], in_=tpv[:, c, 0:N], func=AF.Identity,
                                                        scale=sc[:, c:c + 1], bias=mods[:, c, bcol:bcol + 1]),
                     reads=TPK + ("sc", "mods"), writes=("hxT",))
            S.op("dve", lambda e: e.tensor_tensor(out=hxT[:, :, N:N + 4], in0=tph, in1=sc[:].unsqueeze(2).to_broadcast([128, 8, 4]),
                                                  op=ALU.mult), reads=("ps4", "sc"), writes=("hxTh",))
            S.op("dve", lambda e: e.tensor_tensor(out=hxT[:, :, N:N + 4], in0=hxT[:, :, N:N + 4],
                                                  in1=mods[:, 0:8, bcol:bcol + 1].to_broadcast([128, 8, 4]), op=ALU.add),
                 reads=("hxTh", "mods"), writes=("hxTh",))

        def rglru_block(N, d, reverse, has_lo, has_hi, consumer=None):
            def st1(c):
                par = c % 2
                bxr = bank(par)[:, 0:N]
                bxh = bank(2 + par)[:, 0:4]
                S.group("pe", [lambda e, k=k: e.matmul(bxr, lhsT=wxr[:, k, 128 * c:128 * c + 128], rhs=hxT[:, k, 0:N],
                                                       start=(k == 0), stop=(k == 7)) for k in range(8)],
                        reads=("hxT", "wxr"), writes=(pk(par),))
                S.group("pe", [lambda e, k=k: e.matmul(bxh, lhsT=wxr[:, k, 128 * c:128 * c + 128], rhs=hxT[:, k, N:N + 4],
                                                       start=(k == 0), stop=(k == 7)) for k in range(8)],
                        reads=("hxTh", "wxr"), writes=(pk(2 + par),))
                xk = "xrp%d" % par
                bia = b_in_sb[:, 24 + c:25 + c]
                S.op("dve", lambda e: e.tensor_scalar_add(out=xrp[par][:, 2:2 + N], in0=bxr, scalar1=bia),
                     reads=(pk(par), "params"), writes=(xk,))
                if has_lo:
                    S.op("dve", lambda e: e.tensor_scalar_add(out=xrp[par][:, 0:2], in0=bxh[:, 0:2], scalar1=bia),
                         reads=(pk(2 + par), "params"), writes=(xk,))
                else:
                    S.op("dve", lambda e: e.memset(xrp[par][:, 0:2], 0.0), writes=(xk,))
                if has_hi:
                    S.op("dve", lambda e: e.tensor_scalar_add(out=xrp[par][:, 2 + N:4 + N], in0=bxh[:, 2:4], scalar1=bia),
                         reads=(pk(2 + par), "params"), writes=(xk,))
                else:
                    S.op("dve", lambda e: e.memset(xrp[par][:, 2 + N:4 + N], 0.0), writes=(xk,))

            def st2(c):
                par = c % 2
                xk = "xrp%d" % par
                bcv = bank(4 + par)[:, 0:N]
                S.group("pe", [lambda e, j=j: e.matmul(bcv, lhsT=dg5[:, c, j, :], rhs=xrp[par][:, j:j + N],
                                                       start=(j == 0), stop=(j == 4)) for j in range(5)],
                        reads=(xk, "dg5"), writes=(pk(4 + par),))
                S.op("dve", lambda e: e.tensor_scalar_add(out=xcb[par][:, 0:N], in0=bcv, scalar1=lb_sb[:, c:c + 1]),
                     reads=(pk(4 + par), "params"), writes=("xcb%d" % par,))

            def st3(c):
                par = c % 2
                q = c % 4
                ck = "xcb%d" % par
                br_ = bank(6)[:, 0:N]
                bi_ = bank(7)[:, 0:N]
                S.group("pe", [lambda e: e.matmul(br_, lhsT=wgb[:, 2 * d, c, :], rhs=xcb[par][:, 0:N], start=True, stop=True)],
                        reads=(ck, "wgb"), writes=("ps6",))
                S.group("pe", [lambda e: e.matmul(bi_, lhsT=wgb[:, 2 * d + 1, c, :], rhs=xcb[par][:, 0:N], start=True, stop=True)],
                        reads=(ck, "wgb"), writes=("ps7",))
                S.op("act", lambda e: e.activation(out=tr[:, 0:N], in_=br_, func=AF.Tanh, scale=0.5, bias=hbg[:, 2 * d, c:c + 1]),
                     reads=("ps6", "hbg"), writes=("tr",))
                S.op("act", lambda e: e.activation(out=ti[:, 0:N], in_=bi_, func=AF.Tanh, scale=0.5, bias=hbg[:, 2 * d + 1, c:c + 1]),
                     reads=("ps7", "hbg"), writes=("ti",))
                S.op("act", lambda e: e.activation(out=a4[:, q, 0:N], in_=tr[:, 0:N], func=AF.Exp, scale=hcl[:, d, c:c + 1],
                                                   bias=hcl[:, d, c:c + 1]), reads=("tr", "cl"), writes=("a4_%d" % q,))
                S.op("act", lambda e: e.activation(out=s4[:, q, 0:N], in_=tr[:, 0:N], func=AF.Exp, scale=cl[:, d, c:c + 1],
                                                   bias=cl[:, d, c:c + 1]), reads=("tr", "cl"), writes=("s4_%d" % q,))
                S.op("dve", lambda e: e.scalar_tensor_tensor(out=t4[:, q, 0:N], in0=ti[:, 0:N], scalar=1.0, in1=xcb[par][:, 0:N],
                                                             op0=ALU.add, op1=ALU.mult), reads=("ti", ck), writes=("t4_%d" % q,))

            def st4(c0):
                sk = tuple("s4_%d" % q for q in range(4))
                S.op("act", lambda e: e.activation(out=s4[:, :, 0:N], in_=s4[:, :, 0:N], func=AF.Sqrt, scale=-0.25, bias=qtr[:, 0:1]),
                     reads=sk + ("qtr",), writes=sk)
                for c in range(c0, c0 + 4):
                    q = c % 4
                    S.op("dve", lambda e, q=q: e.tensor_tensor(out=bb_t[:, 0:N], in0=s4[:, q, 0:N], in1=t4[:, q, 0:N], op=ALU.mult),
                         reads=("s4_%d" % q, "t4_%d" % q), writes=("bb_t",))
                    if reverse:
                        S.op("dve", lambda e, q=q, c=c: e.tensor_tensor_scan(
                            out=hf[:, 0:N][:, ::-1], data0=a4[:, q, 0:N][:, ::-1], data1=bb_t[:, 0:N][:, ::-1],
                            initial=state[:, d, c:c + 1], op0=ALU.mult, op1=ALU.add),
                            reads=("a4_%d" % q, "bb_t", "state"), writes=("hf",))
                        S.op("act", lambda e, c=c: e.activation(out=state[:, d, c:c + 1], in_=hf[:, 0:1], func=AF.Copy),
                             reads=("hf",), writes=("state",))
                    else:
                        S.op("dve", lambda e, q=q, c=c: e.tensor_tensor_scan(
                            out=hf[:, 0:N], data0=a4[:, q, 0:N], data1=bb_t[:, 0:N], initial=state[:, d, c:c + 1],
                            op0=ALU.mult, op1=ALU.add), reads=("a4_%d" % q, "bb_t", "state"), writes=("hf",))
                        S.op("act", lambda e, c=c: e.activation(out=state[:, d, c:c + 1], in_=hf[:, N - 1:N], func=AF.Copy),
                             reads=("hf",), writes=("state",))
                    if consumer is not None:
                        consumer(c)

            for s_ in range(10):
                if s_ < 8:
                    st1(s_)
                if 1 <= s_ <= 8:
                    st2(s_ - 1)
                if 2 <= s_ <= 9:
                    st3(s_ - 2)
                    if (s_ - 2) % 4 == 3:
                        st4(s_ - 2 - 3)

        prep(ctxp, 2, 256, s1c, 1, "xt")
        rglru_block(256, 0, False, False, False)
        rglru_block(256, 1, True, False, False)
        for blk in range(15, -1, -1):
            prep(xp, 2 + NB * blk, NB, s1, 0, "xt")
            def cons_a(c):
                S.op("dve", lambda e, c=c: e.tensor_copy(out=hsb[:, c, :], in_=hf[:]), reads=("hf",), writes=("hsb",))
            rglru_block(NB, 1, True, blk != 0, blk != 15, cons_a if blk < 8 else None)
            if blk < 8:
                S.dma("sp", hs_scr[blk], hsb[:], reads=("hsb",), writes=("hs_scr%d" % blk,), key="hs_scr")

        pb_ = ExitStack()
        cwh = sb("cwh", [128, 8, 31], F32, pb_)
        S.op("dve", lambda e: e.tensor_scalar_mul(out=cwh[:], in0=cw_sb[:], scalar1=0.5), reads=("params",), writes=("cwh",))
        wsl = [sb("wsl%d" % i, [128, 8, 512], BF16, pb_) for i in range(3)]
        dgc = [sb("dgc0", [128, 31, 128], BF16, pb_)] * 2
        tv, uu, mean, rstd, mr = tr, ti, a4[:, 0, :], a4[:, 1, :], a4[:, 2, :]
        zb = [sb("zb0", [128, NB], BF16, pb_)] * 2
        zc = sb("zc", [128, 8, NB], BF16, pb_)
        aa = zc
        zsq = [sb("zsq0", [128, NB], BF16, pb_)] * 2
        A_t = sb("A_t", [128, 8, NB], BF16, pb_)
        gy = sb("gy", [128, 8, NB], BF16, pb_)
        mg = gy
        yb = sb("yb", [128, 8, NB], BF16, pb_)
        hs_in = hsb
        x1 = xt

        wctr = [0]

        def wpiece(col0, src=None):
            src = w_in if src is None else src
            i = wctr[0] % 3
            wctr[0] += 1
            S.dma("pool", wsl[i][:], src[:, col0:col0 + 512].rearrange("(k p) n -> p k n", p=128),
                  writes=("wsl%d" % i,), key="wsl%d" % i)
            return wsl[i], "wsl%d" % i

        def inproj(dstbank, wt, wk, j4):
            S.group("pe", [lambda e, k=k: e.matmul(bank(dstbank), lhsT=wt[:, k, 128 * j4:128 * j4 + 128], rhs=hxT[:, k, 0:NB],
                                                   start=(k == 0), stop=(k == 7)) for k in range(8)],
                    reads=("hxT", wk), writes=(pk(dstbank),))

        for blk in range(8):
            prep(xp, 2 + NB * blk, NB, s1, 0, "xt")
            S.dma("sp", hs_in[:], hs_scr[blk], reads=("hs_scr%d" % blk,), writes=("hsb",), key="hs_in")
            for half in range(2):
                wu, wuk = wpiece(512 * half)
                wv, wvk = wpiece(1024 + 512 * half)
                for c4 in range(4):
                    c = 4 * half + c4
                    par = c % 2
                    inproj(par, wu, wuk, c4)
                    inproj(2 + par, wv, wvk, c4)
                    S.op("act", lambda e, c=c, par=par: e.activation(out=tv[:], in_=bank(2 + par), func=AF.Tanh, scale=0.5,
                                                                     bias=hb_in[:, 8 + c:9 + c]),
                         reads=(pk(2 + par), "hb_in"), writes=("tv",))
                    S.op("act", lambda e, c=c, par=par: e.activation(out=uu[:], in_=bank(par), func=AF.Identity,
                                                                     bias=b_in_sb[:, c:c + 1]),
                         reads=(pk(par), "params"), writes=("uu",))
                    zk = "zb0"
                    S.op("dve", lambda e, par=par: e.scalar_tensor_tensor(out=zb[par][:], in0=tv[:], scalar=1.0, in1=uu[:],
                                                                          op0=ALU.add, op1=ALU.mult),
                         reads=("tv", "uu"), writes=(zk,))
                    dk = "dgc0"
                    S.op("dve", lambda e, c=c, par=par: e.tensor_tensor(
                        out=dgc[par][:], in0=identb[:].unsqueeze(1).to_broadcast([128, 31, 128]),
                        in1=cwh[:, c, :].unsqueeze(2).to_broadcast([128, 31, 128]), op=ALU.mult),
                        reads=("identb", "cwh"), writes=(dk,))
                    zv = zb[par][:].rearrange("p (r t) -> p r t", t=64)
                    pcv = bank(4 + par).rearrange("p (r t) -> p r t", t=64)
                    fns = []
                    order = [15] + [k for k in range(31) if k != 15]
                    for idx, k in enumerate(order):
                        o = k - 15
                        t0, t1 = max(0, -o), 64 - max(0, o)
                        fns.append(lambda e, k=k, o=o, t0=t0, t1=t1, idx=idx, par=par, pcv=pcv, zv=zv: e.matmul(
                            pcv[:, :, t0:t1], lhsT=dgc[par][:, k, :], rhs=zv[:, :, t0 + o:t1 + o],
                            start=(idx == 0), stop=(idx == 30)))
                    S.group("pe", fns, reads=(zk, dk), writes=(pk(4 + par),))
                    S.op("act", lambda e, c=c, par=par: e.activation(out=zc[:, c, :], in_=bank(4 + par), func=AF.Identity,
                                                                     bias=cb_sb[:, c:c + 1]),
                         reads=(pk(4 + par), "params"), writes=("zc",))
                    qk = "zsq0"
                    S.op("act", lambda e, c=c, par=par: e.activation(out=zsq[par][:], in_=bank(4 + par), func=AF.Square,
                                                                     bias=cb_sb[:, c:c + 1]),
                         reads=(pk(4 + par), "params"), writes=(qk,))
                    S.group("pe", [lambda e, c=c: e.matmul(bank(6), lhsT=ones_m[:], rhs=zc[:, c, :], start=(c == 0), stop=(c == 7))],
                            reads=("zc", "ones_m"), writes=("ps6",))
                    S.group("pe", [lambda e, c=c, par=par: e.matmul(bank(7), lhsT=ones_m[:], rhs=zsq[par][:], start=(c == 0),
                                                                    stop=(c == 7))], reads=(qk, "ones_m"), writes=("ps7",))
            S.op("act", lambda e: e.activation(out=mean, in_=bank(6), func=AF.Copy), reads=("ps6",), writes=("mean",))
            S.op("dve", lambda e: e.tensor_tensor(out=mr, in0=mean, in1=mean, op=ALU.mult), reads=("mean",), writes=("mr",))
            S.op("dve", lambda e: e.tensor_tensor(out=rstd, in0=bank(7), in1=mr, op=ALU.subtract), reads=("ps7", "mr"),
                 writes=("rstd",))
            S.op("act", lambda e: e.activation(out=rstd, in_=rstd, func=AF.Sqrt, bias=qtr[:, 2:3]), reads=("rstd", "qtr"), writes=("rstd",))
            S.op("dve", lambda e: e.reciprocal(out=rstd, in_=rstd), reads=("rstd",), writes=("rstd",))
            S.op("dve", lambda e: e.tensor_tensor(out=mr, in0=mean, in1=rstd, op=ALU.mult), reads=("mean", "rstd"),
                 writes=("mr",))
            for c in range(8):
                S.op("dve", lambda e, c=c: e.tensor_tensor(out=tv[:], in0=zc[:, c, :], in1=rstd, op=ALU.mult),
                     reads=("zc", "rstd"), writes=("tv",))
                S.op("dve", lambda e: e.tensor_tensor(out=uu[:], in0=tv[:], in1=mr, op=ALU.subtract), reads=("tv", "mr"),
                     writes=("uu",))
                S.op("act", lambda e, c=c: e.activation(out=aa[:, c, :], in_=uu[:], func=AF.Silu, scale=lng_sb[:, c:c + 1],
                                                        bias=lnb_sb[:, c:c + 1]), reads=("uu", "params"), writes=("zc",))
            for half in range(2):
                wga, wgak = wpiece(4096 + 512 * half)
                wpa, wpak = wpiece(512 * half, w_pa)
                for m4 in range(4):
                    m = 4 * half + m4
                    par = m % 2
                    S.group("pe", [lambda e, k=k, m4=m4, par=par, wpa=wpa: e.matmul(bank(par), lhsT=wpa[:, k, 128 * m4:128 * m4 + 128],
                                                                         rhs=aa[:, k, :], start=(k == 0), stop=(k == 7))
                                   for k in range(8)], reads=("zc", wpak), writes=(pk(par),))
                    inproj(2 + par, wga, wgak, m4)
                    S.op("act", lambda e, m=m, par=par: e.activation(out=tv[:], in_=bank(2 + par), func=AF.Tanh, scale=0.5,
                                                                     bias=hb_in[:, 32 + m:33 + m]),
                         reads=(pk(2 + par), "hb_in"), writes=("tv",))
                    S.op("dve", lambda e, m=m, par=par: e.scalar_tensor_tensor(out=A_t[:, m, :], in0=tv[:], scalar=1.0,
                                                                               in1=bank(par), op0=ALU.add, op1=ALU.mult),
                         reads=("tv", pk(par)), writes=("A_t",))
            for half in range(2):
                wy, wyk = wpiece(2048 + 512 * half)
                for c4 in range(4):
                    c = 4 * half + c4
                    par = c % 2
                    inproj(par, wy, wyk, c4)
                    S.op("act", lambda e, c=c, par=par: e.activation(out=gy[:, c, :], in_=bank(par), func=AF.Gelu_apprx_tanh,
                                                                     bias=b_in_sb[:, 16 + c:17 + c]),
                         reads=(pk(par), "params"), writes=("gy",))
            def cons_b(c):
                S.op("dve", lambda e, c=c: e.tensor_tensor(out=tmp1[:], in0=hf[:], in1=hs_in[:, c, :], op=ALU.add),
                     reads=("hf", "hsb"), writes=("tmp1",))
                S.op("dve", lambda e, c=c: e.tensor_tensor(out=yb[:, c, :], in0=tmp1[:], in1=gy[:, c, :], op=ALU.mult),
                     reads=("tmp1", "gy"), writes=("yb",))
            rglru_block(NB, 0, False, blk != 0, True, cons_b)
            for half in range(2):
                wgb_, wgbk = wpiece(5120 + 512 * half)
                wpb, wpbk = wpiece(512 * half, w_pb)
                for m4 in range(4):
                    m = 4 * half + m4
                    par = m % 2
                    S.group("pe", [lambda e, k=k, m4=m4, par=par, wpb=wpb: e.matmul(bank(par), lhsT=wpb[:, k, 128 * m4:128 * m4 + 128],
                                                                         rhs=yb[:, k, :], start=(k == 0), stop=(k == 7))
                                   for k in range(8)], reads=("yb", wpbk), writes=(pk(par),))
                    inproj(2 + par, wgb_, wgbk, m4)
                    S.op("act", lambda e, m=m, par=par: e.activation(out=tv[:], in_=bank(2 + par), func=AF.Tanh, scale=0.5,
                                                                     bias=hb_in[:, 40 + m:41 + m]),
                         reads=(pk(2 + par), "hb_in"), writes=("tv",))
                    S.op("dve", lambda e, par=par: e.scalar_tensor_tensor(out=uu[:], in0=tv[:], scalar=1.0, in1=bank(par),
                                                                          op0=ALU.add, op1=ALU.mult),
                         reads=("tv", pk(par)), writes=("uu",))
                    S.op("dve", lambda e, m=m: e.tensor_tensor(out=mg[:, m, :], in0=uu[:], in1=A_t[:, m, :], op=ALU.add),
                         reads=("uu", "A_t"), writes=("gy",))
            for hh in range(2):
                wo, wok = wpiece(512 * hh, w_o)
                for j in range(4):
                    bk = 4 + j
                    S.group("pe", [lambda e, k=k, j=j, hh=hh, bk=bk, wo=wo: e.matmul(bank(bk), lhsT=mg[:, k, 128 * j:128 * j + 128],
                                                                              rhs=wo[:, k, :],
                                                                              start=(k == 0), stop=(k == 7)) for k in range(8)],
                            reads=("gy", wok), writes=(pk(bk),))
                    S.op("dve", lambda e, hh=hh, bk=bk: e.tensor_tensor(out=tv[:], in0=bank(bk), in1=gt1h[:, 512 * hh:512 * hh + 512],
                                                                        op=ALU.mult), reads=(pk(bk), "gt"), writes=("tv",))
                    S.op("pool", lambda e, j=j, hh=hh: e.tensor_tensor(out=x1[:, j, 512 * hh:512 * hh + 512], in0=tv[:],
                                                                       in1=xt[:, j, 512 * hh:512 * hh + 512], op=ALU.add),
                         reads=("tv", "xt"), writes=("xt",))
            S.dma("sp", x1_scr[NB * blk:NB * blk + NB, :].rearrange("(j p) d -> p j d", p=128), xt[:],
                  reads=("xt",), writes=("x1_scr%d" % blk,), key="x1_scr")
        S.barrier()
        pb_.close()
        mixer.close()

        pc = ExitStack()
        x1t = sb("x1t", [128, 4, 1024], F32, pc)
        xn2 = sb("xn2", [128, 4, 1024], BF16, pc)
        hmT = sb("hmT", [128, 8, NB], BF16, pc)
        accm = sb("accm", [128, 4, 1024], F32, pc)
        cbc = sb("cbc", [128, 16, NB], BF16, pc)
        dgm = sb("dgm", [128, 16, 128], BF16, pc)
        actb = [sb("actb%d" % i, [128, NB], BF16, pc) for i in range(16)]
        wgu = [sb("wgu%d" % i, [128, 2, 8, 512], BF16, pc) for i in range(2)]
        wd = [sb("wd%d" % i, [128, 4, 1024], BF16, pc) for i in range(6)]
        sg = [sb("sg%d" % i, [128, NB], F32, pc) for i in range(2)]
        tt = [sb("tt%d" % i, [128, NB], BF16, pc) for i in range(2)]
        L = sb("L", [128, 4, 20], F32, pc)
        gmax = sb("gmax", [128, 4, 1], F32, pc)
        oh = sb("oh", [128, 4, 4], F32, pc)
        eg = sb("eg", [128, 4, 4], F32, pc)
        pg = sb("pg", [128, 4, 1], F32, pc)
        tmp16 = sb("tmp16", [128, 4, 16], F32, pc)
        esel = sb("esel", [128, 4, 4], F32, pc)
        m1 = sb("m1", [128, 4, 1], F32, pc)
        m2 = sb("m2", [128, 4, 1], F32, pc)
        k1 = sb("k1", [128, 4, 4], F32, pc)
        k2 = sb("k2", [128, 4, 4], F32, pc)
        e2 = sb("e2", [128, 4, 4], F32, pc)
        w1 = sb("w1", [128, 4, 1], F32, pc)
        w2 = sb("w2", [128, 4, 1], F32, pc)
        wsel = sb("wsel", [128, 4, 4], F32, pc)
        comb = sb("comb", [128, 4, 16], F32, pc)
        ofin = accm
        ectr = [0]

        def bc(ap, shape):
            return ap.to_broadcast(shape)

        for blk in range(8):
            S.dma("sp", x1t[:], x1_scr[NB * blk:NB * blk + NB, :].rearrange("(j p) d -> p j d", p=128),
                  reads=("x1_scr%d" % blk,), writes=("x1t",), key="x1t")
            S.op("pool", lambda e: e.memset(ss[:], 0.0), writes=("ss",))
            for j in range(4):
                S.op("act", lambda e, j=j: e.activation(out=xn2[:, j, :], in_=x1t[:, j, :], func=AF.Square, accum_out=ss[:, j:j + 1]),
                     reads=("x1t",), writes=("xn2", "ss"))
            S.op("act", lambda e: e.activation(out=rs[:, 0:4], in_=ss[:, 0:4], func=AF.Sqrt, bias=qtr[:, 1:2]), reads=("ss", "qtr"), writes=("rs",))
            S.op("dve", lambda e: e.reciprocal(out=rs[:, 0:4], in_=rs[:, 0:4]), reads=("rs",), writes=("rs",))
            for j in range(4):
                S.op("dve", lambda e, j=j: e.tensor_scalar_mul(out=xn2[:, j, :], in0=x1t[:, j, :], scalar1=rs[:, j:j + 1]),
                     reads=("x1t", "rs"), writes=("xn2",))
            for j in range(4):
                S.group("pe", [lambda e, j=j, c=c: e.transpose(out=tpv[:, c, 128 * j:128 * j + 128],
                                                               in_=xn2[:, j, 128 * c:128 * c + 128], identity=identb[:])
                               for c in range(8)], reads=("xn2", "identb"), writes=TPK)
            for c in range(8):
                S.op("act", lambda e, c=c: e.activation(out=hmT[:, c, :], in_=tpv[:, c, :], func=AF.Identity,
                                                        scale=s2[:, c:c + 1], bias=mods[:, 24 + c, 0:1]),
                     reads=TPK + ("sc", "mods"), writes=("hmT",))
            for j in range(4):
                S.group("pe", [lambda e, k=k, j=j: e.matmul(bank(4)[:, 20 * j:20 * j + 20], lhsT=hmT[:, k, 128 * j:128 * j + 128],
                                                            rhs=w_rt_b[:, k, :], start=(k == 0), stop=(k == 7)) for k in range(8)],
                        reads=("hmT", "w_rt_b"), writes=("ps4",))
            S.op("dve", lambda e: e.tensor_tensor(out=L[:], in0=bank(4)[:, 0:80].rearrange("p (j n) -> p j n", n=20),
                                                  in1=bc(b_rt_sb[:].unsqueeze(1), [128, 4, 20]), op=ALU.add),
                 reads=("ps4", "params"), writes=("L",))
            R = ("rt",)
            S.op("dve", lambda e: e.tensor_reduce(out=gmax[:], in_=L[:, :, 0:4], axis=AX.X, op=ALU.max), reads=("L",), writes=R)
            S.op("dve", lambda e: e.tensor_tensor(out=oh[:], in0=L[:, :, 0:4], in1=bc(gmax[:], [128, 4, 4]), op=ALU.is_equal),
                 reads=R + ("L",), writes=R)
            S.op("dve", lambda e: e.tensor_tensor(out=eg[:], in0=L[:, :, 0:4], in1=bc(gmax[:], [128, 4, 4]), op=ALU.subtract),
                 reads=R + ("L",), writes=R)
            S.op("act", lambda e: e.activation(out=eg[:], in_=eg[:], func=AF.Exp), reads=R, writes=R)
            S.op("dve", lambda e: e.tensor_reduce(out=pg[:], in_=eg[:], axis=AX.X, op=ALU.add), reads=R, writes=R)
            S.op("dve", lambda e: e.reciprocal(out=pg[:], in_=pg[:]), reads=R, writes=R)
            S.op("dve", lambda e: e.tensor_tensor(out=tmp16[:].rearrange("p j (g x) -> p j g x", x=4),
                                                  in0=L[:, :, 4:20].rearrange("p j (g x) -> p j g x", x=4),
                                                  in1=bc(oh[:].unsqueeze(3), [128, 4, 4, 4]), op=ALU.mult),
                 reads=R + ("L",), writes=R)
            S.op("dve", lambda e: e.tensor_reduce(out=esel[:].unsqueeze(3), in_=tmp16[:].rearrange("p j (g x) -> p j x g", x=4),
                                                  axis=AX.X, op=ALU.add), reads=R, writes=R)
            S.op("dve", lambda e: e.tensor_reduce(out=m1[:], in_=esel[:], axis=AX.X, op=ALU.max), reads=R, writes=R)
            S.op("dve", lambda e: e.tensor_tensor(out=k1[:], in0=esel[:], in1=bc(m1[:], [128, 4, 4]), op=ALU.is_equal),
                 reads=R, writes=R)
            S.op("dve", lambda e: e.scalar_tensor_tensor(out=e2[:], in0=k1[:], scalar=-1e30, in1=esel[:], op0=ALU.mult, op1=ALU.add),
                 reads=R, writes=R)
            S.op("dve", lambda e: e.tensor_reduce(out=m2[:], in_=e2[:], axis=AX.X, op=ALU.max), reads=R, writes=R)
            S.op("dve", lambda e: e.tensor_tensor(out=k2[:], in0=e2[:], in1=bc(m2[:], [128, 4, 4]), op=ALU.is_equal),
                 reads=R, writes=R)
            S.op("dve", lambda e: e.tensor_tensor(out=w2[:], in0=m2[:], in1=m1[:], op=ALU.subtract), reads=R, writes=R)
            S.op("act", lambda e: e.activation(out=w2[:], in_=w2[:], func=AF.Exp), reads=R, writes=R)
            S.op("dve", lambda e: e.tensor_scalar_add(out=w1[:], in0=w2[:], scalar1=1.0), reads=R, writes=R)
            S.op("dve", lambda e: e.reciprocal(out=w1[:], in_=w1[:]), reads=R, writes=R)
            S.op("dve", lambda e: e.tensor_tensor(out=w2[:], in0=w2[:], in1=w1[:], op=ALU.mult), reads=R, writes=R)
            S.op("dve", lambda e: e.tensor_tensor(out=w1[:], in0=w1[:], in1=pg[:], op=ALU.mult), reads=R, writes=R)
            S.op("dve", lambda e: e.tensor_tensor(out=w2[:], in0=w2[:], in1=pg[:], op=ALU.mult), reads=R, writes=R)
            S.op("dve", lambda e: e.tensor_tensor(out=wsel[:], in0=k1[:], in1=bc(w1[:], [128, 4, 4]), op=ALU.mult), reads=R, writes=R)
            S.op("dve", lambda e: e.tensor_tensor(out=k2[:], in0=k2[:], in1=bc(w2[:], [128, 4, 4]), op=ALU.mult), reads=R, writes=R)
            S.op("dve", lambda e: e.tensor_tensor(out=wsel[:], in0=wsel[:], in1=k2[:], op=ALU.add), reads=R, writes=R)
            S.op("dve", lambda e: e.tensor_tensor(out=comb[:].rearrange("p j (g x) -> p j g x", x=4),
                                                  in0=bc(oh[:].unsqueeze(3), [128, 4, 4, 4]),
                                                  in1=bc(wsel[:].unsqueeze(2), [128, 4, 4, 4]), op=ALU.mult),
                 reads=R, writes=("comb",))
            for j in range(4):
                S.op("dve", lambda e, j=j: e.tensor_tensor(out=dgm[:], in0=bc(identb[:].unsqueeze(1), [128, 16, 128]),
                                                            in1=bc(comb[:, j, :].unsqueeze(2), [128, 16, 128]), op=ALU.mult),
                     reads=("identb", "comb"), writes=("dgm",))
                for q in range(4):
                    S.group("pe", [lambda e, q=q: e.matmul(bank(4 + q), lhsT=ones1[:], rhs=dgm[:, 4 * q:4 * q + 4, :],
                                                           start=True, stop=True)], reads=("dgm", "ones1"), writes=(pk(4 + q),))
                    S.op("act", lambda e, q=q, j=j: e.activation(out=cbc[:, 4 * q:4 * q + 4, 128 * j:128 * j + 128],
                                                                 in_=bank(4 + q).rearrange("p (x t) -> p x t", t=128), func=AF.Copy),
                         reads=(pk(4 + q),), writes=("cbc",))
            for g in range(4):
                for el in range(4):
                    ex = 4 * g + el
                    si = ectr[0] % 2
                    di = ectr[0] % 6
                    ectr[0] += 1
                    gk, dk_ = "wgu%d" % si, "wd%d" % di
                    S.dma("pool", wgu[si][:, 0], w_gate[ex].rearrange("(k p) n -> p k n", p=128), writes=(gk,), key=gk)
                    S.dma("pool", wgu[si][:, 1], w_up[ex].rearrange("(k p) n -> p k n", p=128), writes=(gk,), key=gk)
                    S.dma("pool", wd[di][:], w_down[ex].rearrange("(k p) n -> p k n", p=128), writes=(dk_,), key=dk_)
                    for f in range(4):
                        u = 4 * el + f
                        pp = u % 2
                        S.group("pe", [lambda e, k=k, f=f, si=si, pp=pp: e.matmul(
                            bank(2 * pp), lhsT=wgu[si][:, 0, k, 128 * f:128 * f + 128], rhs=hmT[:, k, :],
                            start=(k == 0), stop=(k == 7)) for k in range(8)], reads=("hmT", gk), writes=(pk(2 * pp),))
                        S.group("pe", [lambda e, k=k, f=f, si=si, pp=pp: e.matmul(
                            bank(2 * pp + 1), lhsT=wgu[si][:, 1, k, 128 * f:128 * f + 128], rhs=hmT[:, k, :],
                            start=(k == 0), stop=(k == 7)) for k in range(8)], reads=("hmT", gk), writes=(pk(2 * pp + 1),))
                        S.op("act", lambda e, pp=pp: e.activation(out=sg[pp][:], in_=bank(2 * pp), func=AF.Silu),
                             reads=(pk(2 * pp),), writes=("sg%d" % pp,))
                        S.op("dve", lambda e, pp=pp: e.tensor_tensor(out=tt[pp][:], in0=bank(2 * pp + 1), in1=sg[pp][:], op=ALU.mult),
                             reads=(pk(2 * pp + 1), "sg%d" % pp), writes=("tt%d" % pp,))
                        S.op("dve", lambda e, pp=pp, u=u, ex=ex: e.tensor_tensor(out=actb[u][:], in0=tt[pp][:], in1=cbc[:, ex, :],
                                                                                 op=ALU.mult),
                             reads=("tt%d" % pp, "cbc"), writes=("actb%d" % u,))
                dbase = ectr[0] - 4
                for tp_ in range(2):
                    fns = []
                    for u in range(16):
                        el, f = divmod(u, 4)
                        di = (dbase + el) % 6
                        for jj in range(2):
                            j = 2 * tp_ + jj
                            for hh in range(2):
                                fns.append(lambda e, u=u, f=f, di=di, j=j, jj=jj, hh=hh: e.matmul(
                                    bank(4 + 2 * jj + hh), lhsT=actb[u][:, 128 * j:128 * j + 128],
                                    rhs=wd[di][:, f, 512 * hh:512 * hh + 512], start=(u == 0), stop=(u == 15)))
                    S.group("pe", fns, reads=tuple("actb%d" % u for u in range(16)) + tuple("wd%d" % ((dbase + el) % 6) for el in range(4)),
                            writes=("ps4", "ps5", "ps6", "ps7"))
                    for jj in range(2):
                        j = 2 * tp_ + jj
                        for hh in range(2):
                            bk = 4 + 2 * jj + hh
                            dst = accm[:, j, 512 * hh:512 * hh + 512]
                            if g == 0:
                                S.op("act", lambda e, dst=dst, bk=bk: e.activation(out=dst, in_=bank(bk), func=AF.Copy),
                                     reads=(pk(bk),), writes=("accm",))
                            else:
                                S.op("dve", lambda e, dst=dst, bk=bk: e.tensor_tensor(out=dst, in0=dst, in1=bank(bk), op=ALU.add),
                                     reads=(pk(bk), "accm"), writes=("accm",))
            S.op("pool", lambda e: e.memset(ss[:], 0.0), writes=("ss",))
            for j in range(4):
                S.op("dve", lambda e, j=j: e.tensor_tensor(out=accm[:, j, :], in0=accm[:, j, :], in1=gt2b[:], op=ALU.mult),
                     reads=("accm", "gt"), writes=("accm",))
                S.op("pool", lambda e, j=j: e.tensor_tensor(out=accm[:, j, :], in0=accm[:, j, :], in1=x1t[:, j, :], op=ALU.add),
                     reads=("accm", "x1t"), writes=("accm",))
                S.op("act", lambda e, j=j: e.activation(out=xn2[:, j, :], in_=accm[:, j, :], func=AF.Square, accum_out=ss[:, j:j + 1]),
                     reads=("accm",), writes=("xn2", "ss"))
            S.op("act", lambda e: e.activation(out=rs[:, 0:4], in_=ss[:, 0:4], func=AF.Sqrt, bias=qtr[:, 1:2]), reads=("ss", "qtr"), writes=("rs",))
            S.op("dve", lambda e: e.reciprocal(out=rs[:, 0:4], in_=rs[:, 0:4]), reads=("rs",), writes=("rs",))
            for j in range(4):
                S.op("dve", lambda e, j=j: e.scalar_tensor_tensor(out=ofin[:, j, :], in0=accm[:, j, :], scalar=rs[:, j:j + 1],
                                                                  in1=gf32[:], op0=ALU.mult, op1=ALU.mult),
                     reads=("accm", "rs", "gf32"), writes=("accm",))
            S.dma("sp", out[NB * blk:NB * blk + NB, :].rearrange("(j p) d -> p j d", p=128), accm[:],
                  reads=("accm",), writes=("out%d" % blk,), key="out")
        S.barrier()
        pc.close()
    return nc


_NC_CACHE = {}


def _fm(v):
    v = np.asarray(v, np.float32).reshape(-1, 128)
    return np.ascontiguousarray(v.T)


def kernel(x, c, ctx, c_ctx, w_ada, b_ada, g_mix, w_in, b_in, conv_w, conv_b, ln_g, ln_b, w_pa,
           lru_conv_w, lru_conv_b, w_r_f, b_r_f, w_i_f, b_i_f, lam_f, w_r_b, b_r_b, w_i_b, b_i_b, lam_b,
           w_pb, w_o, g_ffn, w_grp, b_grp, w_er, b_er, w_gate, w_up, w_down, g_final):
    f = lambda a: np.ascontiguousarray(np.asarray(a, np.float32))
    x, c, ctx, c_ctx = f(x), f(c), f(ctx), f(c_ctx)
    B = x.shape[0]
    if "nc" not in _NC_CACHE:
        _NC_CACHE["nc"] = build_program()
    nc = _NC_CACHE["nc"]

    common = {
        "w_ada": f(w_ada[0]), "b_ada_fm": _fm(b_ada[0]),
        "b_ada_gt": f(np.broadcast_to(np.stack([b_ada[0][2048:3072], b_ada[0][5120:6144]])[None], (128, 2, 1024))),
        "w_in": f(w_in[0]), "b_in_fm": _fm(b_in[0]),
        "cb": _fm(conv_b[0]), "lng": _fm(ln_g[0]), "lnb": _fm(ln_b[0]),
        "w_pa": f(w_pa[0]), "w_pb": f(w_pb[0]), "w_o": f(w_o[0]),
        "lb": _fm(lru_conv_b[0]),
        "gmix": _fm(g_mix[0]), "gffn": _fm(g_ffn[0]),
        "gfin": f(np.broadcast_to(np.asarray(g_final, np.float32)[None], (128, 1024))),
        "w_rt": f(np.concatenate([w_grp[0], w_er[0]], axis=1)),
        "b_rt": f(np.broadcast_to(np.concatenate([b_grp[0], b_er[0]])[None], (128, 20))),
        "w_gate": f(w_gate[0]), "w_up": f(w_up[0]), "w_down": f(w_down[0]),
        "ident": np.eye(128, dtype=np.float32),
    }
    cwn = np.asarray(conv_w[0], np.float32)
    lwn = np.asarray(lru_conv_w[0], np.float32)
    zero = np.zeros((1, 1024), np.float32)
    lw5_nat = np.concatenate([lwn, zero], axis=0)
    lw5_rev = lw5_nat[::-1]

    def fm3(a):
        T = a.shape[0]
        return np.ascontiguousarray(a.reshape(T, 8, 128).transpose(2, 1, 0))

    pf = (w_r_f[0], b_r_f[0], w_i_f[0], b_i_f[0], lam_f[0])
    pbk = (w_r_b[0], b_r_b[0], w_i_b[0], b_i_b[0], lam_b[0])

    def gates(P, Sd):
        wgs = np.stack([P[0], P[2], Sd[0], Sd[2]]).astype(np.float32)
        bgs = np.stack([np.asarray(t, np.float32) for t in (P[1], P[3], Sd[1], Sd[3])])
        bgs = np.ascontiguousarray(bgs.transpose(2, 0, 1))
        lams = np.stack([np.asarray(P[4], np.float32).reshape(8, 128), np.asarray(Sd[4], np.float32).reshape(8, 128)])
        lams = np.ascontiguousarray(lams.transpose(2, 0, 1))
        return f(wgs), bgs, lams

    per_half = []
    for half in range(2):
        if half == 0:
            wgs, bgs, lams = gates(pf, pbk)
            d = {"cw": fm3(cwn), "lw5": fm3(lw5_nat), "wg": wgs, "bg": bgs, "lam": lams}
        else:
            wgs, bgs, lams = gates(pbk, pf)
            d = {"cw": fm3(cwn[::-1]), "lw5": fm3(lw5_rev), "wg": wgs, "bg": bgs, "lam": lams}
        per_half.append(d)

    in_maps = []
    pad2 = np.zeros((2, 1024), np.float32)
    for b in range(B):
        for half in range(2):
            xs = x[b] if half == 0 else x[b, ::-1]
            cs_ = ctx[b] if half == 0 else ctx[b, ::-1]
            m = dict(common)
            m.update(per_half[half])
            m["xp"] = np.ascontiguousarray(np.concatenate([pad2, xs, pad2], axis=0))
            m["ctxp"] = np.ascontiguousarray(np.concatenate([pad2, cs_, pad2], axis=0))
            m["cvec"] = np.ascontiguousarray(np.stack([_fm(c[b]), _fm(c_ctx)], axis=-1))
            in_maps.append(m)
    res = run_bass_kernel_spmd(nc, in_maps, core_ids=list(range(2 * B)))
    outp = np.empty((B, 2 * NOWN, 1024), np.float32)
    for b in range(B):
        outp[b, :NOWN] = res.results[2 * b]["out"]
        outp[b, NOWN:] = res.results[2 * b + 1]["out"][::-1]
    if DEBUG:
        kernel.last = res
    return outp
```

```python
from contextlib import ExitStack
import os
import numpy as np
import concourse.bass as bass
import concourse.mybir as mybir
from concourse.bass_utils import run_bass_kernel_spmd

F32 = mybir.dt.float32
BF16 = mybir.dt.bfloat16
AF = mybir.ActivationFunctionType
ALU = mybir.AluOpType
AX = mybir.AxisListType
EPS = 1e-6
NB = 512
NOWN = 4096
DEBUG = bool(int(os.environ.get("MK_DEBUG", "0")))


class _Rec:
    def __init__(self):
        self.calls = []

    def __getattr__(self, name):
        def f(*args, **kw):
            self.calls.append((name, args, kw))
            return self
        return f


_TBL = {"Exp": "exp", "Tanh": None, "Identity": None, "Copy": None, "Square": None, "Sqrt": "sqrt", "Silu": "silu",
        "Gelu_apprx_tanh": "gelu", "Ln": "ln"}


def _fsize(ap):
    n = 1
    for d in ap.shape[1:]:
        n *= int(d)
    return n


class Sched:
    REORDER = True
    WINDOW = 600

    def __init__(self, nc, es):
        self.nc = nc
        self.es = es
        self.E = dict(pe=nc.tensor, act=nc.scalar, dve=nc.vector, pool=nc.gpsimd, sp=nc.sync)
        self.sem = {e: es.enter_context(nc.semaphore("c_" + e)) for e in self.E}
        self.cnt = {e: 0 for e in self.E}
        self.seen = {e: {} for e in self.E}
        self.lastw = {}
        self.readers = {}
        self.dsem = {}
        self.dcnt = {}
        self.ops = []

    def op(self, e, fn, reads=(), writes=()):
        r = _Rec()
        fn(r)
        self._add(e, "op", r.calls, tuple(reads), tuple(writes), None)

    def group(self, e, fns, reads=(), writes=()):
        r = _Rec()
        for f in fns:
            f(r)
        self._add(e, "op", r.calls, tuple(reads), tuple(writes), None)

    def dma(self, q, out, in_, reads=(), writes=(), key=None):
        self._add(q, "dma", [("dma_start", (), dict(out=out, in_=in_))], tuple(reads), tuple(writes), key)

    def _add(self, e, kind, calls, reads, writes, key):
        dur = 0.0
        tbl = None
        if kind == "dma":
            nb = 128 * _fsize(calls[0][2]["out"]) * 4
            dur = 1000.0 if e == "pool" else 150.0
            lat = 2500.0 + nb / 150.0
        else:
            lat = 0.0
            for (name, args, kw) in calls:
                if e == "pe":
                    src = kw.get("rhs", kw.get("in_"))
                    dur += 25.0 + 0.5 * max(_fsize(src), 64)
                else:
                    oap = kw.get("out", kw.get("ap", args[0] if args else None))
                    n = _fsize(oap)
                    if e == "act":
                        dur += 250.0 + 0.73 * n
                        fnm = kw.get("func")
                        tbl = _TBL.get(getattr(fnm, "name", str(fnm)), None) if fnm is not None else None
                    elif e == "dve":
                        dur += 160.0 + 1.04 * n
                    else:
                        dur += 300.0 + 3.1 * n
        self.ops.append(dict(e=e, kind=kind, calls=calls, reads=reads, writes=writes, key=key, dur=dur, lat=lat, tbl=tbl))

    def flush(self):
        ops = self.ops
        self.ops = []
        n = len(ops)
        if n == 0:
            return
        lastw, readers = {}, {}
        preds = [None] * n
        succs = [[] for _ in range(n)]
        for i, o in enumerate(ops):
            p = set()
            for k in o["reads"]:
                if k in lastw:
                    p.add(lastw[k])
            for k in o["writes"]:
                if k in lastw:
                    p.add(lastw[k])
                p.update(readers.get(k, ()))
            p.discard(i)
            preds[i] = p
            for j in p:
                succs[j].append(i)
            for k in o["reads"]:
                readers.setdefault(k, []).append(i)
            for k in o["writes"]:
                lastw[k] = i
                readers[k] = []
        if not self.REORDER:
            order = range(n)
        else:
            indeg = [len(p) for p in preds]
            ready = [i for i in range(n) if indeg[i] == 0]
            finish = [0.0] * n
            efree = {e: 0.0 for e in self.E}
            etbl = [None]
            done = [False] * n
            lo = 0
            order = []
            while len(order) < n:
                while lo < n and done[lo]:
                    lo += 1
                best, bkey = None, None
                for i in ready:
                    if i > lo + self.WINDOW:
                        continue
                    o = ops[i]
                    st = efree[o["e"]]
                    for j in preds[i]:
                        f = finish[j] + (0.0 if ops[j]["e"] == o["e"] else 120.0)
                        if f > st:
                            st = f
                    if o["e"] == "act" and o["tbl"] is not None and o["tbl"] != etbl[0]:
                        st += 1300.0
                    kk = (st, i)
                    if bkey is None or kk < bkey:
                        best, bkey = i, kk
                i = best
                o = ops[i]
                st = bkey[0]
                if o["e"] == "act" and o["tbl"] is not None:
                    etbl[0] = o["tbl"]
                efree[o["e"]] = st + o["dur"]
                finish[i] = st + o["dur"] + o["lat"]
                done[i] = True
                ready.remove(i)
                order.append(i)
                for j in succs[i]:
                    indeg[j] -= 1
                    if indeg[j] == 0:
                        ready.append(j)
        for i in order:
            self._emit(ops[i])

    def _wait(self, e, tok, same_ok=False):
        if tok is None:
            return
        name, sem, val, src = tok
        if same_ok and src == e:
            return
        d = self.seen[e]
        if d.get(name, 0) >= val:
            return
        self.E[e].wait_ge(sem, val)
        d[name] = val

    def _emit(self, o):
        e, reads, writes = o["e"], o["reads"], o["writes"]
        for k in reads:
            self._wait(e, self.lastw.get(k))
        for k in writes:
            self._wait(e, self.lastw.get(k), same_ok=True)
            for t in self.readers.get(k, {}).values():
                self._wait(e, t, same_ok=True)
        ins = None
        for (name, args, kw) in o["calls"]:
            ins = getattr(self.E[e], name)(*args, **kw)
        if o["kind"] == "dma":
            key = o["key"]
            if key not in self.dsem:
                self.dsem[key] = self.es.enter_context(self.nc.semaphore("d_" + key))
                self.dcnt[key] = 0
            self.dcnt[key] += 16
            ins.then_inc(self.dsem[key], 16)
            tok = ("d_" + key, self.dsem[key], self.dcnt[key], "dma")
        else:
            self.cnt[e] += 1
            ins.then_inc(self.sem[e], 1)
            tok = ("c_" + e, self.sem[e], self.cnt[e], e)
        for k in reads:
            self.readers.setdefault(k, {})[tok[0]] = tok
        for k in writes:
            self.lastw[k] = tok
            self.readers[k] = {}

    def barrier(self):
        self.flush()
        for e in self.E:
            for e2 in self.E:
                if self.cnt[e2] > 0:
                    self._wait(e, ("c_" + e2, self.sem[e2], self.cnt[e2], e2))
            for k, sem in self.dsem.items():
                self._wait(e, ("d_" + k, sem, self.dcnt[k], "dma"))


def build_program():
    nc = bass.Bass("TRN2", target_bir_lowering=False)

    def din(name, shape):
        return nc.dram_tensor(name, list(shape), F32, kind="ExternalInput").ap()

    xp = din("xp", [8196, 1024])
    ctxp = din("ctxp", [260, 1024])
    cvec = din("cvec", [128, 8, 2])
    w_ada = din("w_ada", [1024, 6144])
    b_ada_fm = din("b_ada_fm", [128, 48])
    b_ada_gt = din("b_ada_gt", [128, 2, 1024])
    w_in = din("w_in", [1024, 6144])
    b_in_fm = din("b_in_fm", [128, 48])
    cw = din("cw", [128, 8, 31])
    cb = din("cb", [128, 8])
    lng = din("lng", [128, 8])
    lnb = din("lnb", [128, 8])
    w_pa = din("w_pa", [1024, 1024])
    w_pb = din("w_pb", [1024, 1024])
    w_o = din("w_o", [1024, 1024])
    lw5 = din("lw5", [128, 8, 5])
    lb = din("lb", [128, 8])
    wg = din("wg", [4, 8, 128, 128])
    bg = din("bg", [128, 4, 8])
    lam = din("lam", [128, 2, 8])
    gmix = din("gmix", [128, 8])
    gffn = din("gffn", [128, 8])
    gfin = din("gfin", [128, 1024])
    w_rt = din("w_rt", [1024, 20])
    b_rt = din("b_rt", [128, 20])
    w_gate = din("w_gate", [16, 1024, 512])
    w_up = din("w_up", [16, 1024, 512])
    w_down = din("w_down", [16, 512, 1024])
    ident = din("ident", [128, 128])
    out = nc.dram_tensor("out", [NOWN, 1024], F32, kind="ExternalOutput").ap()
    if DEBUG:
        hs_scr = nc.dram_tensor("hs_scr", [8, 128, 8, NB], BF16, kind="ExternalOutput").ap()
        x1_scr = nc.dram_tensor("x1_scr", [NOWN, 1024], F32, kind="ExternalOutput").ap()
    else:
        hs_scr = nc.dram_tensor("hs_scr", [8, 128, 8, NB], BF16, kind="Internal").ap()
        x1_scr = nc.dram_tensor("x1_scr", [NOWN, 1024], F32, kind="Internal").ap()

    with ExitStack() as es:
        S = Sched(nc, es)

        def sb(name, shape, dt=F32, stack=es):
            return stack.enter_context(nc.sbuf_tensor(name, list(shape), dt))

        psA = es.enter_context(nc.psum_tensor("psA", [128, 2048], F32))
        psB = es.enter_context(nc.psum_tensor("psB", [128, 2048], F32))

        def bank(i):
            t = psA if i < 4 else psB
            return t[:, 512 * (i % 4):512 * (i % 4) + 512]

        def pk(i):
            return "ps%d" % i

        tpv = psA[:, :].bitcast(BF16).rearrange("p (c t) -> p c t", t=512)
        TPK = ("ps0", "ps1", "ps2", "ps3")

        identb = sb("identb", [128, 128], BF16)
        ones_m = sb("ones_m", [128, 128], BF16)
        ones1 = sb("ones1", [128, 128], BF16)
        b_in_sb = sb("b_in_sb", [128, 48])
        hb_in = sb("hb_in", [128, 48])
        cw_sb = sb("cw_sb", [128, 8, 31])
        cb_sb = sb("cb_sb", [128, 8])
        lng_sb = sb("lng_sb", [128, 8])
        lnb_sb = sb("lnb_sb", [128, 8])
        lw5_sb = sb("lw5_sb", [128, 8, 5])
        lb_sb = sb("lb_sb", [128, 8])
        bg_sb = sb("bg_sb", [128, 4, 8])
        hbg = sb("hbg", [128, 4, 8])
        lam_sb = sb("lam_sb", [128, 2, 8])
        gmix_sb = sb("gmix_sb", [128, 8])
        gffn_sb = sb("gffn_sb", [128, 8])
        gf32 = sb("gf32", [128, 1024])
        b_rt_sb = sb("b_rt_sb", [128, 20])
        b_ada_fm_sb = sb("b_ada_fm_sb", [128, 48])
        cvec_sb = sb("cvec_sb", [128, 8, 2])
        mods = sb("mods", [128, 48, 2])
        s1 = sb("s1", [128, 8])
        s1c = sb("s1c", [128, 8])
        s2 = sb("s2", [128, 8])
        gt1h = sb("gt1h", [128, 1024])
        gt2b = sb("gt2b", [128, 1024])
        cl = sb("cl", [128, 2, 8])
        hcl = sb("hcl", [128, 2, 8])
        state = sb("state", [128, 2, 8])
        ss = sb("ss", [128, 8])
        rs = sb("rs", [128, 8])
        w_rt_b = sb("w_rt_b", [128, 8, 20], BF16)
        qtr = sb("qtr", [128, 4], F32)

        def pload(t, src):
            S.dma("sp", t, src, writes=("params",), key="params")

        pload(b_in_sb[:], b_in_fm)
        pload(cw_sb[:], cw)
        pload(cb_sb[:], cb)
        pload(lng_sb[:], lng)
        pload(lnb_sb[:], lnb)
        pload(lw5_sb[:], lw5)
        pload(lb_sb[:], lb)
        pload(bg_sb[:], bg)
        pload(lam_sb[:], lam)
        pload(gmix_sb[:], gmix)
        pload(gffn_sb[:], gffn)
        pload(gf32[:], gfin)
        pload(b_rt_sb[:], b_rt)
        pload(b_ada_fm_sb[:], b_ada_fm)
        pload(cvec_sb[:], cvec)
        S.dma("pool", identb[:], ident, writes=("identb",), key="identb")
        S.dma("pool", w_rt_b[:], w_rt.rearrange("(k p) n -> p k n", p=128), writes=("w_rt_b",), key="w_rt_b")
        S.op("pool", lambda e: e.memset(ones_m[:], 1.0 / 1024.0), writes=("ones_m",))
        S.op("pool", lambda e: e.memset(ones1[:], 1.0), writes=("ones1",))
        S.op("pool", lambda e: e.memset(qtr[:, 0:1], 0.25), writes=("qtr",))
        S.op("pool", lambda e: e.memset(qtr[:, 1:2], 1024.0 * EPS), writes=("qtr",))
        S.op("pool", lambda e: e.memset(qtr[:, 2:3], EPS), writes=("qtr",))
        S.op("pool", lambda e: e.memset(state[:], 0.0), writes=("state",))
        S.op("pool", lambda e: e.memset(ss[:], 0.0), writes=("ss",))

        with ExitStack() as p0:
            cs = sb("cs", [128, 8, 2], BF16, p0)
            cs_rep = sb("cs_rep", [128, 8, 128], BF16, p0)
            b_ada_gt_sb = sb("b_ada_gt_sb", [128, 2, 1024], F32, p0)
            wa = [sb("wa%d" % i, [128, 8, 512], BF16, p0) for i in range(3)]
            e_t = sb("e_t", [128, 16], F32, p0)
            t_t = sb("t_t", [128, 16], F32, p0)
            l_t = sb("l_t", [128, 16], F32, p0)
            m_t = sb("m_t", [128, 16], F32, p0)
            pload(b_ada_gt_sb[:], b_ada_gt)

            S.op("act", lambda e: e.activation(out=cs[:], in_=cvec_sb[:], func=AF.Silu), reads=("params",), writes=("cs",))
            S.op("dve", lambda e: e.tensor_copy(out=cs_rep[:], in_=cs[:, :, 0:1].to_broadcast([128, 8, 128])),
                 reads=("cs",), writes=("cs_rep",))
            psm = bank(0)[:, 0:96].rearrange("p (j t) -> p j t", t=2)
            for q in range(12):
                s = q % 3
                S.dma("pool", wa[s][:], w_ada[:, 512 * q:512 * q + 512].rearrange("(k p) n -> p k n", p=128),
                      writes=("wa%d" % s,), key="wa%d" % s)
                fns = []
                for jj in range(4):
                    for k in range(8):
                        fns.append(lambda e, jj=jj, k=k, s=s, q=q: e.matmul(
                            psm[:, 4 * q + jj, :], lhsT=wa[s][:, k, 128 * jj:128 * jj + 128], rhs=cs[:, k, :],
                            start=(k == 0), stop=(k == 7)))
                S.group("pe", fns, reads=("wa%d" % s, "cs"), writes=("ps0",))
                if q in (4, 5, 10, 11):
                    bk = 1 + (q % 2)
                    S.group("pe", [lambda e, k=k, s=s, bk=bk: e.matmul(bank(bk), lhsT=cs_rep[:, k, :], rhs=wa[s][:, k, :],
                                                                      start=(k == 0), stop=(k == 7)) for k in range(8)],
                            reads=("wa%d" % s, "cs_rep"), writes=(pk(bk),))
                    dst = gt1h if q < 6 else gt2b
                    gi = 0 if q < 6 else 1
                    cols = slice(512 * (q % 2), 512 * (q % 2) + 512)
                    S.op("dve", lambda e, dst=dst, gi=gi, cols=cols, bk=bk: e.tensor_tensor(
                        out=dst[:, cols], in0=bank(bk), in1=b_ada_gt_sb[:, gi, cols], op=ALU.add),
                        reads=(pk(bk), "params"), writes=("gt",))
            S.op("dve", lambda e: e.tensor_scalar_mul(out=gt1h[:], in0=gt1h[:], scalar1=0.5), reads=("gt",), writes=("gt",))
            S.op("dve", lambda e: e.tensor_tensor(out=mods[:], in0=psm, in1=b_ada_fm_sb[:].unsqueeze(2).to_broadcast([128, 48, 2]),
                                                  op=ALU.add), reads=("ps0", "params"), writes=("mods",))
            for (dst, col, j0, gsb) in ((s1, 0, 8, gmix_sb), (s1c, 1, 8, gmix_sb), (s2, 0, 32, gffn_sb)):
                S.op("dve", lambda e, dst=dst, col=col, j0=j0, gsb=gsb: e.scalar_tensor_tensor(
                    out=dst[:], in0=mods[:, j0:j0 + 8, col], scalar=1.0, in1=gsb[:], op0=ALU.add, op1=ALU.mult),
                    reads=("mods", "params"), writes=("sc",))
                S.op("dve", lambda e, dst=dst: e.tensor_scalar_mul(out=dst[:], in0=dst[:], scalar1=32.0),
                     reads=("sc",), writes=("sc",))
            S.op("dve", lambda e: e.tensor_scalar_mul(out=gf32[:], in0=gf32[:], scalar1=32.0), reads=("params",), writes=("gf32",))
            S.op("dve", lambda e: e.tensor_scalar_mul(out=hb_in[:], in0=b_in_sb[:], scalar1=0.5), reads=("params",), writes=("hb_in",))
            S.op("dve", lambda e: e.tensor_scalar_mul(out=hbg[:], in0=bg_sb[:], scalar1=0.5), reads=("params",), writes=("hbg",))
            lamf = lam_sb[:].rearrange("p a b -> p (a b)")
            S.op("act", lambda e: e.activation(out=e_t[:], in_=lamf, func=AF.Exp, scale=-1.0), reads=("params",), writes=("e_t",))
            S.op("dve", lambda e: e.tensor_scalar(out=t_t[:], in0=e_t[:], scalar1=-0.25, scalar2=1.0 / 3.0, op0=ALU.mult, op1=ALU.add),
                 reads=("e_t",), writes=("t_t",))
            S.op("dve", lambda e: e.tensor_tensor(out=t_t[:], in0=t_t[:], in1=e_t[:], op=ALU.mult), reads=("t_t", "e_t"), writes=("t_t",))
            S.op("dve", lambda e: e.tensor_scalar_add(out=t_t[:], in0=t_t[:], scalar1=-0.5), reads=("t_t",), writes=("t_t",))
            S.op("dve", lambda e: e.tensor_tensor(out=t_t[:], in0=t_t[:], in1=e_t[:], op=ALU.mult), reads=("t_t", "e_t"), writes=("t_t",))
            S.op("dve", lambda e: e.tensor_scalar_add(out=t_t[:], in0=t_t[:], scalar1=1.0), reads=("t_t",), writes=("t_t",))
            S.op("dve", lambda e: e.tensor_tensor(out=t_t[:], in0=t_t[:], in1=e_t[:], op=ALU.mult), reads=("t_t", "e_t"), writes=("t_t",))
            S.op("dve", lambda e: e.tensor_scalar_add(out=l_t[:], in0=e_t[:], scalar1=1.0), reads=("e_t",), writes=("l_t",))
            S.op("act", lambda e: e.activation(out=l_t[:], in_=l_t[:], func=AF.Ln), reads=("l_t",), writes=("l_t",))
            S.op("dve", lambda e: e.tensor_single_scalar(out=m_t[:], in_=e_t[:], scalar=0.1, op=ALU.is_lt), reads=("e_t",), writes=("m_t",))
            S.op("dve", lambda e: e.tensor_tensor(out=t_t[:], in0=t_t[:], in1=l_t[:], op=ALU.subtract), reads=("t_t", "l_t"), writes=("t_t",))
            S.op("dve", lambda e: e.tensor_tensor(out=t_t[:], in0=t_t[:], in1=m_t[:], op=ALU.mult), reads=("t_t", "m_t"), writes=("t_t",))
            S.op("dve", lambda e: e.tensor_tensor(out=t_t[:], in0=t_t[:], in1=l_t[:], op=ALU.add), reads=("t_t", "l_t"), writes=("t_t",))
            clf = cl[:].rearrange("p a b -> p (a b)")
            hclf = hcl[:].rearrange("p a b -> p (a b)")
            S.op("dve", lambda e: e.tensor_scalar_mul(out=clf, in0=t_t[:], scalar1=-8.0), reads=("t_t",), writes=("cl",))
            S.op("dve", lambda e: e.tensor_scalar_mul(out=hclf, in0=t_t[:], scalar1=-4.0), reads=("t_t",), writes=("cl",))
            S.barrier()

        mixer = ExitStack()
        wxr = sb("wxr", [128, 8, 1024], BF16, mixer)
        wgb = sb("wgb", [128, 4, 8, 128], BF16, mixer)
        dg5 = sb("dg5", [128, 8, 5, 128], BF16, mixer)
        S.dma("pool", wxr[:], w_in[:, 3072:4096].rearrange("(k p) n -> p k n", p=128), writes=("wxr",), key="wxr")
        S.dma("pool", wgb[:], wg.rearrange("g h p n -> p g h n"), writes=("wgb",), key="wgb")
        for c in range(8):
            S.op("dve", lambda e, c=c: e.tensor_tensor(
                out=dg5[:, c, :, :], in0=identb[:].unsqueeze(1).to_broadcast([128, 5, 128]),
                in1=lw5_sb[:, c, :].unsqueeze(2).to_broadcast([128, 5, 128]), op=ALU.mult),
                reads=("identb", "params"), writes=("dg5",))

        xt = sb("xt", [128, 4, 1024], F32, mixer)
        xh = sb("xh", [4, 1024], F32, mixer)
        xn = sb("xn", [128, 4, 1024], BF16, mixer)
        xnh = sb("xnh", [4, 1024], BF16, mixer)
        hxT = sb("hxT", [128, 8, NB + 4], BF16, mixer)
        xrp = [sb("xrp%d" % i, [128, NB + 4], BF16, mixer) for i in range(2)]
        xcb = [sb("xcb%d" % i, [128, NB], BF16, mixer) for i in range(2)]
        tr = sb("tr", [128, NB], F32, mixer)
        ti = sb("ti", [128, NB], F32, mixer)
        a4 = sb("a4", [128, 4, NB], F32, mixer)
        s4 = sb("s4", [128, 4, NB], F32, mixer)
        t4 = sb("t4", [128, 4, NB], BF16, mixer)
        tmp1 = sb("tmp1", [128, NB], F32, mixer)
        bb_t = sb("bb_t", [128, NB], F32, mixer)
        hf = sb("hf", [128, NB], F32, mixer)
        hsb = sb("hsb", [128, 8, NB], BF16, mixer)

        tph = bank(4).bitcast(BF16)[:, 0:32].rearrange("p (c t) -> p c t", t=4)

        def prep(xsrc, r0, N, sc, bcol, keep_key):
            nt = N // 128
            S.dma("sp", xt[:, 0:nt, :], xsrc[r0:r0 + N, :].rearrange("(j p) d -> p j d", p=128), writes=(keep_key,), key="xt")
            S.dma("sp", xh[0:2, :], xsrc[r0 - 2:r0, :], writes=("xh",), key="xh")
            S.dma("sp", xh[2:4, :], xsrc[r0 + N:r0 + N + 2, :], writes=("xh",), key="xh")
            S.op("pool", lambda e: e.memset(ss[:], 0.0), writes=("ss",))
            for j in range(nt):
                S.op("act", lambda e, j=j: e.activation(out=xn[:, j, :], in_=xt[:, j, :], func=AF.Square, accum_out=ss[:, j:j + 1]),
                     reads=(keep_key,), writes=("xn", "ss"))
            S.op("act", lambda e: e.activation(out=xnh[:], in_=xh[:], func=AF.Square, accum_out=ss[0:4, 4:5]),
                 reads=("xh",), writes=("xnh", "ss"))
            S.op("act", lambda e: e.activation(out=rs[:, 0:5], in_=ss[:, 0:5], func=AF.Sqrt, bias=qtr[:, 1:2]), reads=("ss", "qtr"), writes=("rs",))
            S.op("dve", lambda e: e.reciprocal(out=rs[:, 0:5], in_=rs[:, 0:5]), reads=("rs",), writes=("rs",))
            for j in range(nt):
                S.op("dve", lambda e, j=j: e.tensor_scalar_mul(out=xn[:, j, :], in0=xt[:, j, :], scalar1=rs[:, j:j + 1]),
                     reads=(keep_key, "rs"), writes=("xn",))
            S.op("dve", lambda e: e.tensor_scalar_mul(out=xnh[:], in0=xh[:], scalar1=rs[0:4, 4:5]),
                 reads=("xh", "rs"), writes=("xnh",))
            for j in range(nt):
                S.group("pe", [lambda e, j=j, c=c: e.transpose(out=tpv[:, c, 128 * j:128 * j + 128],
                                                               in_=xn[:, j, 128 * c:128 * c + 128], identity=identb[:])
                               for c in range(8)], reads=("xn", "identb"), writes=TPK)
            S.group("pe", [lambda e, c=c: e.transpose(out=tph[:, c, :], in_=xnh[:, 128 * c:128 * c + 128], identity=identb[0:4, 0:4])
                           for c in range(8)], reads=("xnh", "identb"), writes=("ps4",))
            for c in range(8):
                S.op("act", lambda e, c=c: e.activation(out=hxT[:, c, 0:N], in_=tpv[:, c, 0:N], func=AF.Identity,
                                                        scale=sc[:, c:c + 1], bias=mods[:, c, bcol:bcol + 1]),
                     reads=TPK + ("sc", "mods"), writes=("hxT",))
            S.op("dve", lambda e: e.tensor_tensor(out=hxT[:, :, N:N + 4], in0=tph, in1=sc[:].unsqueeze(2).to_broadcast([128, 8, 4]),
                                                  op=ALU.mult), reads=("ps4", "sc"), writes=("hxTh",))
            S.op("dve", lambda e: e.tensor_tensor(out=hxT[:, :, N:N + 4], in0=hxT[:, :, N:N + 4],
                                                  in1=mods[:, 0:8, bcol:bcol + 1].to_broadcast([128, 8, 4]), op=ALU.add),
                 reads=("hxTh", "mods"), writes=("hxTh",))

        def rglru_block(N, d, reverse, has_lo, has_hi, consumer=None):
            def st1(c):
                par = c % 2
                bxr = bank(par)[:, 0:N]
                bxh = bank(2 + par)[:, 0:4]
                S.group("pe", [lambda e, k=k: e.matmul(bxr, lhsT=wxr[:, k, 128 * c:128 * c + 128], rhs=hxT[:, k, 0:N],
                                                       start=(k == 0), stop=(k == 7)) for k in range(8)],
                        reads=("hxT", "wxr"), writes=(pk(par),))
                S.group("pe", [lambda e, k=k: e.matmul(bxh, lhsT=wxr[:, k, 128 * c:128 * c + 128], rhs=hxT[:, k, N:N + 4],
                                                       start=(k == 0), stop=(k == 7)) for k in range(8)],
                        reads=("hxTh", "wxr"), writes=(pk(2 + par),))
                xk = "xrp%d" % par
                bia = b_in_sb[:, 24 + c:25 + c]
                S.op("dve", lambda e: e.tensor_scalar_add(out=xrp[par][:, 2:2 + N], in0=bxr, scalar1=bia),
                     reads=(pk(par), "params"), writes=(xk,))
                if has_lo:
                    S.op("dve", lambda e: e.tensor_scalar_add(out=xrp[par][:, 0:2], in0=bxh[:, 0:2], scalar1=bia),
                         reads=(pk(2 + par), "params"), writes=(xk,))
                else:
                    S.op("dve", lambda e: e.memset(xrp[par][:, 0:2], 0.0), writes=(xk,))
                if has_hi:
                    S.op("dve", lambda e: e.tensor_scalar_add(out=xrp[par][:, 2 + N:4 + N], in0=bxh[:, 2:4], scalar1=bia),
                         reads=(pk(2 + par), "params"), writes=(xk,))
                else:
                    S.op("dve", lambda e: e.memset(xrp[par][:, 2 + N:4 + N], 0.0), writes=(xk,))

            def st2(c):
                par = c % 2
                xk = "xrp%d" % par
                bcv = bank(4 + par)[:, 0:N]
                S.group("pe", [lambda e, j=j: e.matmul(bcv, lhsT=dg5[:, c, j, :], rhs=xrp[par][:, j:j + N],
                                                       start=(j == 0), stop=(j == 4)) for j in range(5)],
                        reads=(xk, "dg5"), writes=(pk(4 + par),))
                S.op("dve", lambda e: e.tensor_scalar_add(out=xcb[par][:, 0:N], in0=bcv, scalar1=lb_sb[:, c:c + 1]),
                     reads=(pk(4 + par), "params"), writes=("xcb%d" % par,))

            def st3(c):
                par = c % 2
                q = c % 4
                ck = "xcb%d" % par
                br_ = bank(6)[:, 0:N]
                bi_ = bank(7)[:, 0:N]
                S.group("pe", [lambda e: e.matmul(br_, lhsT=wgb[:, 2 * d, c, :], rhs=xcb[par][:, 0:N], start=True, stop=True)],
                        reads=(ck, "wgb"), writes=("ps6",))
                S.group("pe", [lambda e: e.matmul(bi_, lhsT=wgb[:, 2 * d + 1, c, :], rhs=xcb[par][:, 0:N], start=True, stop=True)],
                        reads=(ck, "wgb"), writes=("ps7",))
                S.op("act", lambda e: e.activation(out=tr[:, 0:N], in_=br_, func=AF.Tanh, scale=0.5, bias=hbg[:, 2 * d, c:c + 1]),
                     reads=("ps6", "hbg"), writes=("tr",))
                S.op("act", lambda e: e.activation(out=ti[:, 0:N], in_=bi_, func=AF.Tanh, scale=0.5, bias=hbg[:, 2 * d + 1, c:c + 1]),
                     reads=("ps7", "hbg"), writes=("ti",))
                S.op("act", lambda e: e.activation(out=a4[:, q, 0:N], in_=tr[:, 0:N], func=AF.Exp, scale=hcl[:, d, c:c + 1],
                                                   bias=hcl[:, d, c:c + 1]), reads=("tr", "cl"), writes=("a4_%d" % q,))
                S.op("act", lambda e: e.activation(out=s4[:, q, 0:N], in_=tr[:, 0:N], func=AF.Exp, scale=cl[:, d, c:c + 1],
                                                   bias=cl[:, d, c:c + 1]), reads=("tr", "cl"), writes=("s4_%d" % q,))
                S.op("dve", lambda e: e.scalar_tensor_tensor(out=t4[:, q, 0:N], in0=ti[:, 0:N], scalar=1.0, in1=xcb[par][:, 0:N],
                                                             op0=ALU.add, op1=ALU.mult), reads=("ti", ck), writes=("t4_%d" % q,))

            def st4(c0):
                sk = tuple("s4_%d" % q for q in range(4))
                S.op("act", lambda e: e.activation(out=s4[:, :, 0:N], in_=s4[:, :, 0:N], func=AF.Sqrt, scale=-0.25, bias=qtr[:, 0:1]),
                     reads=sk + ("qtr",), writes=sk)
                for c in range(c0, c0 + 4):
                    q = c % 4
                    S.op("dve", lambda e, q=q: e.tensor_tensor(out=bb_t[:, 0:N], in0=s4[:, q, 0:N], in1=t4[:, q, 0:N], op=ALU.mult),
                         reads=("s4_%d" % q, "t4_%d" % q), writes=("bb_t",))
                    if reverse:
                        S.op("dve", lambda e, q=q, c=c: e.tensor_tensor_scan(
                            out=hf[:, 0:N][:, ::-1], data0=a4[:, q, 0:N][:, ::-1], data1=bb_t[:, 0:N][:, ::-1],
                            initial=state[:, d, c:c + 1], op0=ALU.mult, op1=ALU.add),
                            reads=("a4_%d" % q, "bb_t", "state"), writes=("hf",))
                        S.op("act", lambda e, c=c: e.activation(out=state[:, d, c:c + 1], in_=hf[:, 0:1], func=AF.Copy),
                             reads=("hf",), writes=("state",))
                    else:
                        S.op("dve", lambda e, q=q, c=c: e.tensor_tensor_scan(
                            out=hf[:, 0:N], data0=a4[:, q, 0:N], data1=bb_t[:, 0:N], initial=state[:, d, c:c + 1],
                            op0=ALU.mult, op1=ALU.add), reads=("a4_%d" % q, "bb_t", "state"), writes=("hf",))
                        S.op("act", lambda e, c=c: e.activation(out=state[:, d, c:c + 1], in_=hf[:, N - 1:N], func=AF.Copy),
                             reads=("hf",), writes=("state",))
                    if consumer is not None:
                        consumer(c)

            for s_ in range(10):
                if s_ < 8:
                    st1(s_)
                if 1 <= s_ <= 8:
                    st2(s_ - 1)
                if 2 <= s_ <= 9:
                    st3(s_ - 2)
                    if (s_ - 2) % 4 == 3:
                        st4(s_ - 2 - 3)

        prep(ctxp, 2, 256, s1c, 1, "xt")
        rglru_block(256, 0, False, False, False)
        rglru_block(256, 1, True, False, False)
        for blk in range(15, -1, -1):
            prep(xp, 2 + NB * blk, NB, s1, 0, "xt")
            def cons_a(c):
                S.op("dve", lambda e, c=c: e.tensor_copy(out=hsb[:, c, :], in_=hf[:]), reads=("hf",), writes=("hsb",))
            rglru_block(NB, 1, True, blk != 0, blk != 15, cons_a if blk < 8 else None)
            if blk < 8:
                S.dma("sp", hs_scr[blk], hsb[:], reads=("hsb",), writes=("hs_scr%d" % blk,), key="hs_scr")

        pb_ = ExitStack()
        cwh = sb("cwh", [128, 8, 31], F32, pb_)
        S.op("dve", lambda e: e.tensor_scalar_mul(out=cwh[:], in0=cw_sb[:], scalar1=0.5), reads=("params",), writes=("cwh",))
        wsl = [sb("wsl%d" % i, [128, 8, 512], BF16, pb_) for i in range(3)]
        dgc = [sb("dgc0", [128, 31, 128], BF16, pb_)] * 2
        tv, uu, mean, rstd, mr = tr, ti, a4[:, 0, :], a4[:, 1, :], a4[:, 2, :]
        zb = [sb("zb0", [128, NB], BF16, pb_)] * 2
        zc = sb("zc", [128, 8, NB], BF16, pb_)
        aa = zc
        zsq = [sb("zsq0", [128, NB], BF16, pb_)] * 2
        A_t = sb("A_t", [128, 8, NB], BF16, pb_)
        gy = sb("gy", [128, 8, NB], BF16, pb_)
        mg = gy
        yb = sb("yb", [128, 8, NB], BF16, pb_)
        hs_in = hsb
        x1 = xt

        wctr = [0]

        def wpiece(col0, src=None):
            src = w_in if src is None else src
            i = wctr[0] % 3
            wctr[0] += 1
            S.dma("pool", wsl[i][:], src[:, col0:col0 + 512].rearrange("(k p) n -> p k n", p=128),
                  writes=("wsl%d" % i,), key="wsl%d" % i)
            return wsl[i], "wsl%d" % i

        def inproj(dstbank, wt, wk, j4):
            S.group("pe", [lambda e, k=k: e.matmul(bank(dstbank), lhsT=wt[:, k, 128 * j4:128 * j4 + 128], rhs=hxT[:, k, 0:NB],
                                                   start=(k == 0), stop=(k == 7)) for k in range(8)],
                    reads=("hxT", wk), writes=(pk(dstbank),))

        for blk in range(8):
            prep(xp, 2 + NB * blk, NB, s1, 0, "xt")
            S.dma("sp", hs_in[:], hs_scr[blk], reads=("hs_scr%d" % blk,), writes=("hsb",), key="hs_in")
            for half in range(2):
                wu, wuk = wpiece(512 * half)
                wv, wvk = wpiece(1024 + 512 * half)
                for c4 in range(4):
                    c = 4 * half + c4
                    par = c % 2
                    inproj(par, wu, wuk, c4)
                    inproj(2 + par, wv, wvk, c4)
                    S.op("act", lambda e, c=c, par=par: e.activation(out=tv[:], in_=bank(2 + par), func=AF.Tanh, scale=0.5,
                                                                     bias=hb_in[:, 8 + c:9 + c]),
                         reads=(pk(2 + par), "hb_in"), writes=("tr",))
                    S.op("act", lambda e, c=c, par=par: e.activation(out=uu[:], in_=bank(par), func=AF.Identity,
                                                                     bias=b_in_sb[:, c:c + 1]),
                         reads=(pk(par), "params"), writes=("ti",))
                    zk = "zb0"
                    S.op("dve", lambda e, par=par: e.scalar_tensor_tensor(out=zb[par][:], in0=tv[:], scalar=1.0, in1=uu[:],
                                                                          op0=ALU.add, op1=ALU.mult),
                         reads=("tr", "ti"), writes=(zk,))
                    dk = "dgc0"
                    S.op("dve", lambda e, c=c, par=par: e.tensor_tensor(
                        out=dgc[par][:], in0=identb[:].unsqueeze(1).to_broadcast([128, 31, 128]),
                        in1=cwh[:, c, :].unsqueeze(2).to_broadcast([128, 31, 128]), op=ALU.mult),
                        reads=("identb", "cwh"), writes=(dk,))
                    zv = zb[par][:].rearrange("p (r t) -> p r t", t=64)
                    pcv = bank(4 + par).rearrange("p (r t) -> p r t", t=64)
                    fns = []
                    order = [15] + [k for k in range(31) if k != 15]
                    for idx, k in enumerate(order):
                        o = k - 15
                        t0, t1 = max(0, -o), 64 - max(0, o)
                        fns.append(lambda e, k=k, o=o, t0=t0, t1=t1, idx=idx, par=par, pcv=pcv, zv=zv: e.matmul(
                            pcv[:, :, t0:t1], lhsT=dgc[par][:, k, :], rhs=zv[:, :, t0 + o:t1 + o],
                            start=(idx == 0), stop=(idx == 30)))
                    S.group("pe", fns, reads=(zk, dk), writes=(pk(4 + par),))
                    S.op("act", lambda e, c=c, par=par: e.activation(out=zc[:, c, :], in_=bank(4 + par), func=AF.Identity,
                                                                     bias=cb_sb[:, c:c + 1]),
                         reads=(pk(4 + par), "params"), writes=("zc",))
                    qk = "zsq0"
                    S.op("act", lambda e, c=c, par=par: e.activation(out=zsq[par][:], in_=bank(4 + par), func=AF.Square,
                                                                     bias=cb_sb[:, c:c + 1]),
                         reads=(pk(4 + par), "params"), writes=(qk,))
                    S.group("pe", [lambda e, c=c: e.matmul(bank(6), lhsT=ones_m[:], rhs=zc[:, c, :], start=(c == 0), stop=(c == 7))],
                            reads=("zc", "ones_m"), writes=("ps6",))
                    S.group("pe", [lambda e, c=c, par=par: e.matmul(bank(7), lhsT=ones_m[:], rhs=zsq[par][:], start=(c == 0),
                                                                    stop=(c == 7))], reads=(qk, "ones_m"), writes=("ps7",))
            S.op("act", lambda e: e.activation(out=mean, in_=bank(6), func=AF.Copy), reads=("ps6",), writes=("a4_0",))
            S.op("dve", lambda e: e.tensor_tensor(out=mr, in0=mean, in1=mean, op=ALU.mult), reads=("a4_0",), writes=("a4_2",))
            S.op("dve", lambda e: e.tensor_tensor(out=rstd, in0=bank(7), in1=mr, op=ALU.subtract), reads=("ps7", "a4_2"),
                 writes=("a4_1",))
            S.op("act", lambda e: e.activation(out=rstd, in_=rstd, func=AF.Sqrt, bias=qtr[:, 2:3]), reads=("a4_1", "qtr"), writes=("a4_1",))
            S.op("dve", lambda e: e.reciprocal(out=rstd, in_=rstd), reads=("a4_1",), writes=("a4_1",))
            S.op("dve", lambda e: e.tensor_tensor(out=mr, in0=mean, in1=rstd, op=ALU.mult), reads=("a4_0", "a4_1"),
                 writes=("a4_2",))
            for c in range(8):
                S.op("dve", lambda e, c=c: e.tensor_tensor(out=tv[:], in0=zc[:, c, :], in1=rstd, op=ALU.mult),
                     reads=("zc", "a4_1"), writes=("tr",))
                S.op("dve", lambda e: e.tensor_tensor(out=uu[:], in0=tv[:], in1=mr, op=ALU.subtract), reads=("tr", "a4_2"),
                     writes=("ti",))
                S.op("act", lambda e, c=c: e.activation(out=aa[:, c, :], in_=uu[:], func=AF.Silu, scale=lng_sb[:, c:c + 1],
                                                        bias=lnb_sb[:, c:c + 1]), reads=("ti", "params"), writes=("zc",))
            for half in range(2):
                wga, wgak = wpiece(4096 + 512 * half)
                wpa, wpak = wpiece(512 * half, w_pa)
                for m4 in range(4):
                    m = 4 * half + m4
                    par = m % 2
                    S.group("pe", [lambda e, k=k, m4=m4, par=par, wpa=wpa: e.matmul(bank(par), lhsT=wpa[:, k, 128 * m4:128 * m4 + 128],
                                                                         rhs=aa[:, k, :], start=(k == 0), stop=(k == 7))
                                   for k in range(8)], reads=("zc", wpak), writes=(pk(par),))
                    inproj(2 + par, wga, wgak, m4)
                    S.op("act", lambda e, m=m, par=par: e.activation(out=tv[:], in_=bank(2 + par), func=AF.Tanh, scale=0.5,
                                                                     bias=hb_in[:, 32 + m:33 + m]),
                         reads=(pk(2 + par), "hb_in"), writes=("tr",))
                    S.op("dve", lambda e, m=m, par=par: e.scalar_tensor_tensor(out=A_t[:, m, :], in0=tv[:], scalar=1.0,
                                                                               in1=bank(par), op0=ALU.add, op1=ALU.mult),
                         reads=("tr", pk(par)), writes=("A_t",))
            for half in range(2):
                wy, wyk = wpiece(2048 + 512 * half)
                for c4 in range(4):
                    c = 4 * half + c4
                    par = c % 2
                    inproj(par, wy, wyk, c4)
                    S.op("act", lambda e, c=c, par=par: e.activation(out=gy[:, c, :], in_=bank(par), func=AF.Gelu_apprx_tanh,
                                                                     bias=b_in_sb[:, 16 + c:17 + c]),
                         reads=(pk(par), "params"), writes=("gy",))
            def cons_b(c):
                S.op("dve", lambda e, c=c: e.tensor_tensor(out=tmp1[:], in0=hf[:], in1=hs_in[:, c, :], op=ALU.add),
                     reads=("hf", "hsb"), writes=("tmp1",))
                S.op("dve", lambda e, c=c: e.tensor_tensor(out=yb[:, c, :], in0=tmp1[:], in1=gy[:, c, :], op=ALU.mult),
                     reads=("tmp1", "gy"), writes=("yb",))
            rglru_block(NB, 0, False, blk != 0, True, cons_b)
            for half in range(2):
                wgb_, wgbk = wpiece(5120 + 512 * half)
                wpb, wpbk = wpiece(512 * half, w_pb)
                for m4 in range(4):
                    m = 4 * half + m4
                    par = m % 2
                    S.group("pe", [lambda e, k=k, m4=m4, par=par, wpb=wpb: e.matmul(bank(par), lhsT=wpb[:, k, 128 * m4:128 * m4 + 128],
                                                                         rhs=yb[:, k, :], start=(k == 0), stop=(k == 7))
                                   for k in range(8)], reads=("yb", wpbk), writes=(pk(par),))
                    inproj(2 + par, wgb_, wgbk, m4)
                    S.op("act", lambda e, m=m, par=par: e.activation(out=tv[:], in_=bank(2 + par), func=AF.Tanh, scale=0.5,
                                                                     bias=hb_in[:, 40 + m:41 + m]),
                         reads=(pk(2 + par), "hb_in"), writes=("tr",))
                    S.op("dve", lambda e, par=par: e.scalar_tensor_tensor(out=uu[:], in0=tv[:], scalar=1.0, in1=bank(par),
                                                                          op0=ALU.add, op1=ALU.mult),
                         reads=("tr", pk(par)), writes=("ti",))
                    S.op("dve", lambda e, m=m: e.tensor_tensor(out=mg[:, m, :], in0=uu[:], in1=A_t[:, m, :], op=ALU.add),
                         reads=("ti", "A_t"), writes=("gy",))
            for hh in range(2):
                wo, wok = wpiece(512 * hh, w_o)
                for j in range(4):
                    bk = 4 + j
                    S.group("pe", [lambda e, k=k, j=j, hh=hh, bk=bk, wo=wo: e.matmul(bank(bk), lhsT=mg[:, k, 128 * j:128 * j + 128],
                                                                              rhs=wo[:, k, :],
                                                                              start=(k == 0), stop=(k == 7)) for k in range(8)],
                            reads=("gy", wok), writes=(pk(bk),))
                    S.op("dve", lambda e, hh=hh, bk=bk: e.tensor_tensor(out=tv[:], in0=bank(bk), in1=gt1h[:, 512 * hh:512 * hh + 512],
                                                                        op=ALU.mult), reads=(pk(bk), "gt"), writes=("tr",))
                    S.op("pool", lambda e, j=j, hh=hh: e.tensor_tensor(out=x1[:, j, 512 * hh:512 * hh + 512], in0=tv[:],
                                                                       in1=xt[:, j, 512 * hh:512 * hh + 512], op=ALU.add),
                         reads=("tr", "xt"), writes=("xt",))
            S.dma("sp", x1_scr[NB * blk:NB * blk + NB, :].rearrange("(j p) d -> p j d", p=128), xt[:],
                  reads=("xt",), writes=("x1_scr%d" % blk,), key="x1_scr")
        S.barrier()
        pb_.close()
        mixer.close()

        pc = ExitStack()
        x1t = sb("x1t", [128, 4, 1024], F32, pc)
        xn2 = sb("xn2", [128, 4, 1024], BF16, pc)
        hmT = sb("hmT", [128, 8, NB], BF16, pc)
        accm = sb("accm", [128, 4, 1024], F32, pc)
        cbc = sb("cbc", [128, 16, NB], BF16, pc)
        dgm = sb("dgm", [128, 16, 128], BF16, pc)
        actb = [sb("actb%d" % i, [128, NB], BF16, pc) for i in range(16)]
        wgu = [sb("wgu%d" % i, [128, 2, 8, 512], BF16, pc) for i in range(2)]
        wd = [sb("wd%d" % i, [128, 4, 1024], BF16, pc) for i in range(6)]
        sg = [sb("sg%d" % i, [128, NB], F32, pc) for i in range(2)]
        tt = [sb("tt%d" % i, [128, NB], BF16, pc) for i in range(2)]
        L = sb("L", [128, 4, 20], F32, pc)
        gmax = sb("gmax", [128, 4, 1], F32, pc)
        oh = sb("oh", [128, 4, 4], F32, pc)
        eg = sb("eg", [128, 4, 4], F32, pc)
        pg = sb("pg", [128, 4, 1], F32, pc)
        tmp16 = sb("tmp16", [128, 4, 16], F32, pc)
        esel = sb("esel", [128, 4, 4], F32, pc)
        m1 = sb("m1", [128, 4, 1], F32, pc)
        m2 = sb("m2", [128, 4, 1], F32, pc)
        k1 = sb("k1", [128, 4, 4], F32, pc)
        k2 = sb("k2", [128, 4, 4], F32, pc)
        e2 = sb("e2", [128, 4, 4], F32, pc)
        w1 = sb("w1", [128, 4, 1], F32, pc)
        w2 = sb("w2", [128, 4, 1], F32, pc)
        wsel = sb("wsel", [128, 4, 4], F32, pc)
        comb = sb("comb", [128, 4, 16], F32, pc)
        ofin = accm
        ectr = [0]

        def bc(ap, shape):
            return ap.to_broadcast(shape)

        for blk in range(8):
            S.dma("sp", x1t[:], x1_scr[NB * blk:NB * blk + NB, :].rearrange("(j p) d -> p j d", p=128),
                  reads=("x1_scr%d" % blk,), writes=("x1t",), key="x1t")
            S.op("pool", lambda e: e.memset(ss[:], 0.0), writes=("ss",))
            for j in range(4):
                S.op("act", lambda e, j=j: e.activation(out=xn2[:, j, :], in_=x1t[:, j, :], func=AF.Square, accum_out=ss[:, j:j + 1]),
                     reads=("x1t",), writes=("xn2", "ss"))
            S.op("act", lambda e: e.activation(out=rs[:, 0:4], in_=ss[:, 0:4], func=AF.Sqrt, bias=qtr[:, 1:2]), reads=("ss", "qtr"), writes=("rs",))
            S.op("dve", lambda e: e.reciprocal(out=rs[:, 0:4], in_=rs[:, 0:4]), reads=("rs",), writes=("rs",))
            for j in range(4):
                S.op("dve", lambda e, j=j: e.tensor_scalar_mul(out=xn2[:, j, :], in0=x1t[:, j, :], scalar1=rs[:, j:j + 1]),
                     reads=("x1t", "rs"), writes=("xn2",))
            for j in range(4):
                S.group("pe", [lambda e, j=j, c=c: e.transpose(out=tpv[:, c, 128 * j:128 * j + 128],
                                                               in_=xn2[:, j, 128 * c:128 * c + 128], identity=identb[:])
                               for c in range(8)], reads=("xn2", "identb"), writes=TPK)
            for c in range(8):
                S.op("act", lambda e, c=c: e.activation(out=hmT[:, c, :], in_=tpv[:, c, :], func=AF.Identity,
                                                        scale=s2[:, c:c + 1], bias=mods[:, 24 + c, 0:1]),
                     reads=TPK + ("sc", "mods"), writes=("hmT",))
            for j in range(4):
                S.group("pe", [lambda e, k=k, j=j: e.matmul(bank(4)[:, 20 * j:20 * j + 20], lhsT=hmT[:, k, 128 * j:128 * j + 128],
                                                            rhs=w_rt_b[:, k, :], start=(k == 0), stop=(k == 7)) for k in range(8)],
                        reads=("hmT", "w_rt_b"), writes=("ps4",))
            S.op("dve", lambda e: e.tensor_tensor(out=L[:], in0=bank(4)[:, 0:80].rearrange("p (j n) -> p j n", n=20),
                                                  in1=bc(b_rt_sb[:].unsqueeze(1), [128, 4, 20]), op=ALU.add),
                 reads=("ps4", "params"), writes=("L",))
            R = ("rt",)
            S.op("dve", lambda e: e.tensor_reduce(out=gmax[:], in_=L[:, :, 0:4], axis=AX.X, op=ALU.max), reads=("L",), writes=R)
            S.op("dve", lambda e: e.tensor_tensor(out=oh[:], in0=L[:, :, 0:4], in1=bc(gmax[:], [128, 4, 4]), op=ALU.is_equal),
                 reads=R + ("L",), writes=R)
            S.op("dve", lambda e: e.tensor_tensor(out=eg[:], in0=L[:, :, 0:4], in1=bc(gmax[:], [128, 4, 4]), op=ALU.subtract),
                 reads=R + ("L",), writes=R)
            S.op("act", lambda e: e.activation(out=eg[:], in_=eg[:], func=AF.Exp), reads=R, writes=R)
            S.op("dve", lambda e: e.tensor_reduce(out=pg[:], in_=eg[:], axis=AX.X, op=ALU.add), reads=R, writes=R)
            S.op("dve", lambda e: e.reciprocal(out=pg[:], in_=pg[:]), reads=R, writes=R)
            S.op("dve", lambda e: e.tensor_tensor(out=tmp16[:].rearrange("p j (g x) -> p j g x", x=4),
                                                  in0=L[:, :, 4:20].rearrange("p j (g x) -> p j g x", x=4),
                                                  in1=bc(oh[:].unsqueeze(3), [128, 4, 4, 4]), op=ALU.mult),
                 reads=R + ("L",), writes=R)
            S.op("dve", lambda e: e.tensor_reduce(out=esel[:].unsqueeze(3), in_=tmp16[:].rearrange("p j (g x) -> p j x g", x=4),
                                                  axis=AX.X, op=ALU.add), reads=R, writes=R)
            S.op("dve", lambda e: e.tensor_reduce(out=m1[:], in_=esel[:], axis=AX.X, op=ALU.max), reads=R, writes=R)
            S.op("dve", lambda e: e.tensor_tensor(out=k1[:], in0=esel[:], in1=bc(m1[:], [128, 4, 4]), op=ALU.is_equal),
                 reads=R, writes=R)
            S.op("dve", lambda e: e.scalar_tensor_tensor(out=e2[:], in0=k1[:], scalar=-1e30, in1=esel[:], op0=ALU.mult, op1=ALU.add),
                 reads=R, writes=R)
            S.op("dve", lambda e: e.tensor_reduce(out=m2[:], in_=e2[:], axis=AX.X, op=ALU.max), reads=R, writes=R)
            S.op("dve", lambda e: e.tensor_tensor(out=k2[:], in0=e2[:], in1=bc(m2[:], [128, 4, 4]), op=ALU.is_equal),
                 reads=R, writes=R)
            S.op("dve", lambda e: e.tensor_tensor(out=w2[:], in0=m2[:], in1=m1[:], op=ALU.subtract), reads=R, writes=R)
            S.op("act", lambda e: e.activation(out=w2[:], in_=w2[:], func=AF.Exp), reads=R, writes=R)
            S.op("dve", lambda e: e.tensor_scalar_add(out=w1[:], in0=w2[:], scalar1=1.0), reads=R, writes=R)
            S.op("dve", lambda e: e.reciprocal(out=w1[:], in_=w1[:]), reads=R, writes=R)
            S.op("dve", lambda e: e.tensor_tensor(out=w2[:], in0=w2[:], in1=w1[:], op=ALU.mult), reads=R, writes=R)
            S.op("dve", lambda e: e.tensor_tensor(out=w1[:], in0=w1[:], in1=pg[:], op=ALU.mult), reads=R, writes=R)
            S.op("dve", lambda e: e.tensor_tensor(out=w2[:], in0=w2[:], in1=pg[:], op=ALU.mult), reads=R, writes=R)
            S.op("dve", lambda e: e.tensor_tensor(out=wsel[:], in0=k1[:], in1=bc(w1[:], [128, 4, 4]), op=ALU.mult), reads=R, writes=R)
            S.op("dve", lambda e: e.tensor_tensor(out=k2[:], in0=k2[:], in1=bc(w2[:], [128, 4, 4]), op=ALU.mult), reads=R, writes=R)
            S.op("dve", lambda e: e.tensor_tensor(out=wsel[:], in0=wsel[:], in1=k2[:], op=ALU.add), reads=R, writes=R)
            S.op("dve", lambda e: e.tensor_tensor(out=comb[:].rearrange("p j (g x) -> p j g x", x=4),
                                                  in0=bc(oh[:].unsqueeze(3), [128, 4, 4, 4]),
                                                  in1=bc(wsel[:].unsqueeze(2), [128, 4, 4, 4]), op=ALU.mult),
                 reads=R, writes=("comb",))
            for j in range(4):
                S.op("dve", lambda e, j=j: e.tensor_tensor(out=dgm[:], in0=bc(identb[:].unsqueeze(1), [128, 16, 128]),
                                                            in1=bc(comb[:, j, :].unsqueeze(2), [128, 16, 128]), op=ALU.mult),
                     reads=("identb", "comb"), writes=("dgm",))
                for q in range(4):
                    S.group("pe", [lambda e, q=q: e.matmul(bank(4 + q), lhsT=ones1[:], rhs=dgm[:, 4 * q:4 * q + 4, :],
                                                           start=True, stop=True)], reads=("dgm", "ones1"), writes=(pk(4 + q),))
                    S.op("act", lambda e, q=q, j=j: e.activation(out=cbc[:, 4 * q:4 * q + 4, 128 * j:128 * j + 128],
                                                                 in_=bank(4 + q).rearrange("p (x t) -> p x t", t=128), func=AF.Copy),
                         reads=(pk(4 + q),), writes=("cbc",))
            for g in range(4):
                for el in range(4):
                    ex = 4 * g + el
                    si = ectr[0] % 2
                    di = ectr[0] % 6
                    ectr[0] += 1
                    gk, dk_ = "wgu%d" % si, "wd%d" % di
                    S.dma("pool", wgu[si][:, 0], w_gate[ex].rearrange("(k p) n -> p k n", p=128), writes=(gk,), key=gk)
                    S.dma("pool", wgu[si][:, 1], w_up[ex].rearrange("(k p) n -> p k n", p=128), writes=(gk,), key=gk)
                    S.dma("pool", wd[di][:], w_down[ex].rearrange("(k p) n -> p k n", p=128), writes=(dk_,), key=dk_)
                    for f in range(4):
                        u = 4 * el + f
                        pp = u % 2
                        S.group("pe", [lambda e, k=k, f=f, si=si, pp=pp: e.matmul(
                            bank(2 * pp), lhsT=wgu[si][:, 0, k, 128 * f:128 * f + 128], rhs=hmT[:, k, :],
                            start=(k == 0), stop=(k == 7)) for k in range(8)], reads=("hmT", gk), writes=(pk(2 * pp),))
                        S.group("pe", [lambda e, k=k, f=f, si=si, pp=pp: e.matmul(
                            bank(2 * pp + 1), lhsT=wgu[si][:, 1, k, 128 * f:128 * f + 128], rhs=hmT[:, k, :],
                            start=(k == 0), stop=(k == 7)) for k in range(8)], reads=("hmT", gk), writes=(pk(2 * pp + 1),))
                        S.op("act", lambda e, pp=pp: e.activation(out=sg[pp][:], in_=bank(2 * pp), func=AF.Silu),
                             reads=(pk(2 * pp),), writes=("sg%d" % pp,))
                        S.op("dve", lambda e, pp=pp: e.tensor_tensor(out=tt[pp][:], in0=bank(2 * pp + 1), in1=sg[pp][:], op=ALU.mult),
                             reads=(pk(2 * pp + 1), "sg%d" % pp), writes=("tt%d" % pp,))
                        S.op("dve", lambda e, pp=pp, u=u, ex=ex: e.tensor_tensor(out=actb[u][:], in0=tt[pp][:], in1=cbc[:, ex, :],
                                                                                 op=ALU.mult),
                             reads=("tt%d" % pp, "cbc"), writes=("actb%d" % u,))
                dbase = ectr[0] - 4
                for tp_ in range(2):
                    fns = []
                    for u in range(16):
                        el, f = divmod(u, 4)
                        di = (dbase + el) % 6
                        for jj in range(2):
                            j = 2 * tp_ + jj
                            for hh in range(2):
                                fns.append(lambda e, u=u, f=f, di=di, j=j, jj=jj, hh=hh: e.matmul(
                                    bank(4 + 2 * jj + hh), lhsT=actb[u][:, 128 * j:128 * j + 128],
                                    rhs=wd[di][:, f, 512 * hh:512 * hh + 512], start=(u == 0), stop=(u == 15)))
                    S.group("pe", fns, reads=tuple("actb%d" % u for u in range(16)) + tuple("wd%d" % ((dbase + el) % 6) for el in range(4)),
                            writes=("ps4", "ps5", "ps6", "ps7"))
                    for jj in range(2):
                        j = 2 * tp_ + jj
                        for hh in range(2):
                            bk = 4 + 2 * jj + hh
                            dst = accm[:, j, 512 * hh:512 * hh + 512]
                            if g == 0:
                                S.op("act", lambda e, dst=dst, bk=bk: e.activation(out=dst, in_=bank(bk), func=AF.Copy),
                                     reads=(pk(bk),), writes=("accm",))
                            else:
                                S.op("dve", lambda e, dst=dst, bk=bk: e.tensor_tensor(out=dst, in0=dst, in1=bank(bk), op=ALU.add),
                                     reads=(pk(bk), "accm"), writes=("accm",))
            S.op("pool", lambda e: e.memset(ss[:], 0.0), writes=("ss",))
            for j in range(4):
                S.op("dve", lambda e, j=j: e.tensor_tensor(out=accm[:, j, :], in0=accm[:, j, :], in1=gt2b[:], op=ALU.mult),
                     reads=("accm", "gt"), writes=("accm",))
                S.op("pool", lambda e, j=j: e.tensor_tensor(out=accm[:, j, :], in0=accm[:, j, :], in1=x1t[:, j, :], op=ALU.add),
                     reads=("accm", "x1t"), writes=("accm",))
                S.op("act", lambda e, j=j: e.activation(out=xn2[:, j, :], in_=accm[:, j, :], func=AF.Square, accum_out=ss[:, j:j + 1]),
                     reads=("accm",), writes=("xn2", "ss"))
            S.op("act", lambda e: e.activation(out=rs[:, 0:4], in_=ss[:, 0:4], func=AF.Sqrt, bias=qtr[:, 1:2]), reads=("ss", "qtr"), writes=("rs",))
            S.op("dve", lambda e: e.reciprocal(out=rs[:, 0:4], in_=rs[:, 0:4]), reads=("rs",), writes=("rs",))
            for j in range(4):
                S.op("dve", lambda e, j=j: e.scalar_tensor_tensor(out=ofin[:, j, :], in0=accm[:, j, :], scalar=rs[:, j:j + 1],
                                                                  in1=gf32[:], op0=ALU.mult, op1=ALU.mult),
                     reads=("accm", "rs", "gf32"), writes=("accm",))
            S.dma("sp", out[NB * blk:NB * blk + NB, :].rearrange("(j p) d -> p j d", p=128), accm[:],
                  reads=("accm",), writes=("out%d" % blk,), key="out")
        S.barrier()
        pc.close()
    return nc


_NC_CACHE = {}


def _fm(v):
    v = np.asarray(v, np.float32).reshape(-1, 128)
    return np.ascontiguousarray(v.T)


def kernel(x, c, ctx, c_ctx, w_ada, b_ada, g_mix, w_in, b_in, conv_w, conv_b, ln_g, ln_b, w_pa,
           lru_conv_w, lru_conv_b, w_r_f, b_r_f, w_i_f, b_i_f, lam_f, w_r_b, b_r_b, w_i_b, b_i_b, lam_b,
           w_pb, w_o, g_ffn, w_grp, b_grp, w_er, b_er, w_gate, w_up, w_down, g_final):
    f = lambda a: np.ascontiguousarray(np.asarray(a, np.float32))
    x, c, ctx, c_ctx = f(x), f(c), f(ctx), f(c_ctx)
    B = x.shape[0]
    if "nc" not in _NC_CACHE:
        _NC_CACHE["nc"] = build_program()
    nc = _NC_CACHE["nc"]

    common = {
        "w_ada": f(w_ada[0]), "b_ada_fm": _fm(b_ada[0]),
        "b_ada_gt": f(np.broadcast_to(np.stack([b_ada[0][2048:3072], b_ada[0][5120:6144]])[None], (128, 2, 1024))),
        "w_in": f(w_in[0]), "b_in_fm": _fm(b_in[0]),
        "cb": _fm(conv_b[0]), "lng": _fm(ln_g[0]), "lnb": _fm(ln_b[0]),
        "w_pa": f(w_pa[0]), "w_pb": f(w_pb[0]), "w_o": f(w_o[0]),
        "lb": _fm(lru_conv_b[0]),
        "gmix": _fm(g_mix[0]), "gffn": _fm(g_ffn[0]),
        "gfin": f(np.broadcast_to(np.asarray(g_final, np.float32)[None], (128, 1024))),
        "w_rt": f(np.concatenate([w_grp[0], w_er[0]], axis=1)),
        "b_rt": f(np.broadcast_to(np.concatenate([b_grp[0], b_er[0]])[None], (128, 20))),
        "w_gate": f(w_gate[0]), "w_up": f(w_up[0]), "w_down": f(w_down[0]),
        "ident": np.eye(128, dtype=np.float32),
    }
    cwn = np.asarray(conv_w[0], np.float32)
    lwn = np.asarray(lru_conv_w[0], np.float32)
    zero = np.zeros((1, 1024), np.float32)
    lw5_nat = np.concatenate([lwn, zero], axis=0)
    lw5_rev = lw5_nat[::-1]

    def fm3(a):
        T = a.shape[0]
        return np.ascontiguousarray(a.reshape(T, 8, 128).transpose(2, 1, 0))

    pf = (w_r_f[0], b_r_f[0], w_i_f[0], b_i_f[0], lam_f[0])
    pbk = (w_r_b[0], b_r_b[0], w_i_b[0], b_i_b[0], lam_b[0])

    def gates(P, Sd):
        wgs = np.stack([P[0], P[2], Sd[0], Sd[2]]).astype(np.float32)
        bgs = np.stack([np.asarray(t, np.float32) for t in (P[1], P[3], Sd[1], Sd[3])])
        bgs = np.ascontiguousarray(bgs.transpose(2, 0, 1))
        lams = np.stack([np.asarray(P[4], np.float32).reshape(8, 128), np.asarray(Sd[4], np.float32).reshape(8, 128)])
        lams = np.ascontiguousarray(lams.transpose(2, 0, 1))
        return f(wgs), bgs, lams

    per_half = []
    for half in range(2):
        if half == 0:
            wgs, bgs, lams = gates(pf, pbk)
            d = {"cw": fm3(cwn), "lw5": fm3(lw5_nat), "wg": wgs, "bg": bgs, "lam": lams}
        else:
            wgs, bgs, lams = gates(pbk, pf)
            d = {"cw": fm3(cwn[::-1]), "lw5": fm3(lw5_rev), "wg": wgs, "bg": bgs, "lam": lams}
        per_half.append(d)

    in_maps = []
    pad2 = np.zeros((2, 1024), np.float32)
    for b in range(B):
        for half in range(2):
            xs = x[b] if half == 0 else x[b, ::-1]
            cs_ = ctx[b] if half == 0 else ctx[b, ::-1]
            m = dict(common)
            m.update(per_half[half])
            m["xp"] = np.ascontiguousarray(np.concatenate([pad2, xs, pad2], axis=0))
            m["ctxp"] = np.ascontiguousarray(np.concatenate([pad2, cs_, pad2], axis=0))
            m["cvec"] = np.ascontiguousarray(np.stack([_fm(c[b]), _fm(c_ctx)], axis=-1))
            in_maps.append(m)
    res = run_bass_kernel_spmd(nc, in_maps, core_ids=list(range(2 * B)))
    outp = np.empty((B, 2 * NOWN, 1024), np.float32)
    for b in range(B):
        outp[b, :NOWN] = res.results[2 * b]["out"]
        outp[b, NOWN:] = res.results[2 * b + 1]["out"][::-1]
    if DEBUG:
        kernel.last = res
    return outp
```

```python
from contextlib import ExitStack
import os
import numpy as np
import concourse.bass as bass
import concourse.mybir as mybir
from concourse.bass_utils import run_bass_kernel_spmd

F32 = mybir.dt.float32
BF16 = mybir.dt.bfloat16
AF = mybir.ActivationFunctionType
ALU = mybir.AluOpType
AX = mybir.AxisListType
EPS = 1e-6
NB = 512
NOWN = 4096
DEBUG = bool(int(os.environ.get("MK_DEBUG", "0")))


class _Rec:
    def __init__(self):
        self.calls = []

    def __getattr__(self, name):
        def f(*args, **kw):
            self.calls.append((name, args, kw))
            return self
        return f


_TBL = {"Exp": "exp", "Tanh": None, "Identity": None, "Copy": None, "Square": None, "Sqrt": "sqrt", "Silu": "silu",
        "Gelu_apprx_tanh": "gelu", "Ln": "ln"}


def _fsize(ap):
    n = 1
    for d in ap.shape[1:]:
        n *= int(d)
    return n


class Sched:
    REORDER = True
    WINDOW = 600

    def __init__(self, nc, es):
        self.nc = nc
        self.es = es
        self.E = dict(pe=nc.tensor, act=nc.scalar, dve=nc.vector, pool=nc.gpsimd, sp=nc.sync)
        self.sem = {e: es.enter_context(nc.semaphore("c_" + e)) for e in self.E}
        self.cnt = {e: 0 for e in self.E}
        self.seen = {e: {} for e in self.E}
        self.lastw = {}
        self.readers = {}
        self.dsem = {}
        self.dcnt = {}
        self.ops = []

    def op(self, e, fn, reads=(), writes=()):
        r = _Rec()
        fn(r)
        self._add(e, "op", r.calls, tuple(reads), tuple(writes), None)

    def group(self, e, fns, reads=(), writes=()):
        r = _Rec()
        for f in fns:
            f(r)
        self._add(e, "op", r.calls, tuple(reads), tuple(writes), None)

    def dma(self, q, out, in_, reads=(), writes=(), key=None):
        self._add(q, "dma", [("dma_start", (), dict(out=out, in_=in_))], tuple(reads), tuple(writes), key)

    def _add(self, e, kind, calls, reads, writes, key):
        dur = 0.0
        tbl = None
        if kind == "dma":
            nb = 128 * _fsize(calls[0][2]["out"]) * 4
            dur = 1000.0 if e == "pool" else 150.0
            lat = 2000.0 + nb / 300.0
        else:
            lat = 0.0
            for (name, args, kw) in calls:
                if e == "pe":
                    src = kw.get("rhs", kw.get("in_"))
                    dur += 25.0 + 0.5 * max(_fsize(src), 64)
                else:
                    oap = kw.get("out", kw.get("ap", args[0] if args else None))
                    n = _fsize(oap)
                    if e == "act":
                        dur += 250.0 + 0.73 * n
                        fnm = kw.get("func")
                        tbl = _TBL.get(getattr(fnm, "name", str(fnm)), None) if fnm is not None else None
                    elif e == "dve":
                        dur += 160.0 + 1.04 * n
                    else:
                        dur += 300.0 + 3.1 * n
        self.ops.append(dict(e=e, kind=kind, calls=calls, reads=reads, writes=writes, key=key, dur=dur, lat=lat, tbl=tbl))

    def flush(self):
        ops = self.ops
        self.ops = []
        n = len(ops)
        if n == 0:
            return
        lastw, readers = {}, {}
        preds = [None] * n
        succs = [[] for _ in range(n)]
        for i, o in enumerate(ops):
            p = set()
            for k in o["reads"]:
                if k in lastw:
                    p.add(lastw[k])
            for k in o["writes"]:
                if k in lastw:
                    p.add(lastw[k])
                p.update(readers.get(k, ()))
            p.discard(i)
            preds[i] = p
            for j in p:
                succs[j].append(i)
            for k in o["reads"]:
                readers.setdefault(k, []).append(i)
            for k in o["writes"]:
                lastw[k] = i
                readers[k] = []
        if not self.REORDER:
            order = range(n)
        else:
            indeg = [len(p) for p in preds]
            ready = [i for i in range(n) if indeg[i] == 0]
            finish = [0.0] * n
            efree = {e: 0.0 for e in self.E}
            etbl = [None]
            done = [False] * n
            lo = 0
            order = []
            while len(order) < n:
                while lo < n and done[lo]:
                    lo += 1
                best, bkey = None, None
                for i in ready:
                    if i > lo + self.WINDOW:
                        continue
                    o = ops[i]
                    st = efree[o["e"]]
                    for j in preds[i]:
                        f = finish[j] + (0.0 if ops[j]["e"] == o["e"] else 120.0)
                        if f > st:
                            st = f
                    if o["e"] == "act" and o["tbl"] is not None and o["tbl"] != etbl[0]:
                        st += 1300.0
                    kk = (st, i)
                    if bkey is None or kk < bkey:
                        best, bkey = i, kk
                i = best
                o = ops[i]
                st = bkey[0]
                if os.environ.get("MK_TL") and n > 3000 and len(ops) == int(os.environ.get("MK_TL")):
                    lim = None
                    for j in preds[i]:
                        f = finish[j]
                        if lim is None or f > lim[0]:
                            lim = (f, j)
                    gap = st - efree[o["e"]]
                    if o["e"] == "pe" and gap > 300:
                        print("PE gap %.1fus at t=%.1fus op#%d writes=%s waits for %s op#%d writes=%s" % (
                            gap / 1e3, st / 1e3, i, o["writes"][:2], ops[lim[1]]["e"], lim[1], ops[lim[1]]["writes"][:2]))
                if o["e"] == "act" and o["tbl"] is not None:
                    etbl[0] = o["tbl"]
                efree[o["e"]] = st + o["dur"]
                finish[i] = st + o["dur"] + o["lat"]
                done[i] = True
                ready.remove(i)
                order.append(i)
                for j in succs[i]:
                    indeg[j] -= 1
                    if indeg[j] == 0:
                        ready.append(j)
        if self.REORDER and os.environ.get("MK_STATS"):
            busy = {e: 0.0 for e in self.E}
            for o in ops:
                busy[o["e"]] += o["dur"]
            print("phase: n=%d est_makespan=%.0fus busy(us): %s" % (n, max(finish) / 1e3, {e: int(v / 1e3) for e, v in busy.items()}))
        for i in order:
            self._emit(ops[i])

    def _wait(self, e, tok, same_ok=False):
        if tok is None:
            return
        name, sem, val, src = tok
        if same_ok and src == e:
            return
        d = self.seen[e]
        if d.get(name, 0) >= val:
            return
        self.E[e].wait_ge(sem, val)
        d[name] = val

    def _emit(self, o):
        e, reads, writes = o["e"], o["reads"], o["writes"]
        for k in reads:
            self._wait(e, self.lastw.get(k))
        for k in writes:
            self._wait(e, self.lastw.get(k), same_ok=True)
            for t in self.readers.get(k, {}).values():
                self._wait(e, t, same_ok=True)
        ins = None
        for (name, args, kw) in o["calls"]:
            ins = getattr(self.E[e], name)(*args, **kw)
        if o["kind"] == "dma":
            key = o["key"]
            if key not in self.dsem:
                self.dsem[key] = self.es.enter_context(self.nc.semaphore("d_" + key))
                self.dcnt[key] = 0
            self.dcnt[key] += 16
            ins.then_inc(self.dsem[key], 16)
            tok = ("d_" + key, self.dsem[key], self.dcnt[key], "dma")
        else:
            self.cnt[e] += 1
            ins.then_inc(self.sem[e], 1)
            tok = ("c_" + e, self.sem[e], self.cnt[e], e)
        for k in reads:
            self.readers.setdefault(k, {})[tok[0]] = tok
        for k in writes:
            self.lastw[k] = tok
            self.readers[k] = {}

    def barrier(self):
        self.flush()
        for e in self.E:
            for e2 in self.E:
                if self.cnt[e2] > 0:
                    self._wait(e, ("c_" + e2, self.sem[e2], self.cnt[e2], e2))
            for k, sem in self.dsem.items():
                self._wait(e, ("d_" + k, sem, self.dcnt[k], "dma"))


def build_program():
    nc = bass.Bass("TRN2", target_bir_lowering=False)

    def din(name, shape):
        return nc.dram_tensor(name, list(shape), F32, kind="ExternalInput").ap()

    xp = din("xp", [8196, 1024])
    ctxp = din("ctxp", [260, 1024])
    cvec = din("cvec", [128, 8, 2])
    w_ada = din("w_ada", [1024, 6144])
    b_ada_fm = din("b_ada_fm", [128, 48])
    b_ada_gt = din("b_ada_gt", [128, 2, 1024])
    w_in = din("w_in", [1024, 6144])
    b_in_fm = din("b_in_fm", [128, 48])
    cw = din("cw", [128, 8, 31])
    cb = din("cb", [128, 8])
    lng = din("lng", [128, 8])
    lnb = din("lnb", [128, 8])
    w_pa = din("w_pa", [1024, 1024])
    w_pb = din("w_pb", [1024, 1024])
    w_o = din("w_o", [1024, 1024])
    lw5 = din("lw5", [128, 8, 5])
    lb = din("lb", [128, 8])
    wg = din("wg", [4, 8, 128, 128])
    bg = din("bg", [128, 4, 8])
    lam = din("lam", [128, 2, 8])
    gmix = din("gmix", [128, 8])
    gffn = din("gffn", [128, 8])
    gfin = din("gfin", [128, 1024])
    w_rt = din("w_rt", [1024, 20])
    b_rt = din("b_rt", [128, 20])
    w_gate = din("w_gate", [16, 1024, 512])
    w_up = din("w_up", [16, 1024, 512])
    w_down = din("w_down", [16, 512, 1024])
    ident = din("ident", [128, 128])
    out = nc.dram_tensor("out", [NOWN, 1024], F32, kind="ExternalOutput").ap()
    if DEBUG:
        hs_scr = nc.dram_tensor("hs_scr", [8, 128, 8, NB], BF16, kind="ExternalOutput").ap()
        x1_scr = nc.dram_tensor("x1_scr", [NOWN, 1024], F32, kind="ExternalOutput").ap()
    else:
        hs_scr = nc.dram_tensor("hs_scr", [8, 128, 8, NB], BF16, kind="Internal").ap()
        x1_scr = nc.dram_tensor("x1_scr", [NOWN, 1024], F32, kind="Internal").ap()
    gt2_scr = nc.dram_tensor("gt2_scr", [128, 1024], F32, kind="Internal").ap()

    with ExitStack() as es:
        S = Sched(nc, es)

        def sb(name, shape, dt=F32, stack=es):
            return stack.enter_context(nc.sbuf_tensor(name, list(shape), dt))

        psA = es.enter_context(nc.psum_tensor("psA", [128, 2048], F32))
        psB = es.enter_context(nc.psum_tensor("psB", [128, 2048], F32))

        def bank(i):
            t = psA if i < 4 else psB
            return t[:, 512 * (i % 4):512 * (i % 4) + 512]

        def pk(i):
            return "ps%d" % i

        tpv = psA[:, :].bitcast(BF16).rearrange("p (c t) -> p c t", t=512)
        TPK = ("ps0", "ps1", "ps2", "ps3")

        identb = sb("identb", [128, 128], BF16)
        ones_m = sb("ones_m", [128, 128], BF16)
        ones1 = sb("ones1", [128, 128], BF16)
        b_in_sb = sb("b_in_sb", [128, 48])
        hb_in = sb("hb_in", [128, 48])
        cw_sb = sb("cw_sb", [128, 8, 31])
        cb_sb = sb("cb_sb", [128, 8])
        lng_sb = sb("lng_sb", [128, 8])
        lnb_sb = sb("lnb_sb", [128, 8])
        lw5_sb = sb("lw5_sb", [128, 8, 5])
        lb_sb = sb("lb_sb", [128, 8])
        bg_sb = sb("bg_sb", [128, 4, 8])
        hbg = sb("hbg", [128, 4, 8])
        lam_sb = sb("lam_sb", [128, 2, 8])
        gmix_sb = sb("gmix_sb", [128, 8])
        gffn_sb = sb("gffn_sb", [128, 8])
        b_rt_sb = sb("b_rt_sb", [128, 20])
        b_ada_fm_sb = sb("b_ada_fm_sb", [128, 48])
        cvec_sb = sb("cvec_sb", [128, 8, 2])
        mods = sb("mods", [128, 48, 2])
        s1 = sb("s1", [128, 8])
        s1c = sb("s1c", [128, 8])
        s2 = sb("s2", [128, 8])
        gt1h = sb("gt1h", [128, 1024])
        cl = sb("cl", [128, 2, 8])
        hcl = sb("hcl", [128, 2, 8])
        state = sb("state", [128, 2, 8])
        ss = sb("ss", [128, 8])
        rs = sb("rs", [128, 8])
        w_rt_b = sb("w_rt_b", [128, 8, 20], BF16)
        qtr = sb("qtr", [128, 4], F32)

        def pload(t, src):
            S.dma("sp", t, src, writes=("params",), key="params")

        pload(b_in_sb[:], b_in_fm)
        pload(cw_sb[:], cw)
        pload(cb_sb[:], cb)
        pload(lng_sb[:], lng)
        pload(lnb_sb[:], lnb)
        pload(lw5_sb[:], lw5)
        pload(lb_sb[:], lb)
        pload(bg_sb[:], bg)
        pload(lam_sb[:], lam)
        pload(gmix_sb[:], gmix)
        pload(gffn_sb[:], gffn)
        pload(b_rt_sb[:], b_rt)
        pload(b_ada_fm_sb[:], b_ada_fm)
        pload(cvec_sb[:], cvec)
        S.dma("pool", identb[:], ident, writes=("identb",), key="identb")
        S.dma("pool", w_rt_b[:], w_rt.rearrange("(k p) n -> p k n", p=128), writes=("w_rt_b",), key="w_rt_b")
        S.op("pool", lambda e: e.memset(ones_m[:], 1.0 / 1024.0), writes=("ones_m",))
        S.op("pool", lambda e: e.memset(ones1[:], 1.0), writes=("ones1",))
        S.op("pool", lambda e: e.memset(qtr[:, 0:1], 0.25), writes=("qtr",))
        S.op("pool", lambda e: e.memset(qtr[:, 1:2], 1024.0 * EPS), writes=("qtr",))
        S.op("pool", lambda e: e.memset(qtr[:, 2:3], EPS), writes=("qtr",))
        S.op("pool", lambda e: e.memset(state[:], 0.0), writes=("state",))
        S.op("pool", lambda e: e.memset(ss[:], 0.0), writes=("ss",))

        with ExitStack() as p0:
            cs = sb("cs", [128, 8, 2], BF16, p0)
            cs_rep = sb("cs_rep", [128, 8, 128], BF16, p0)
            b_ada_gt_sb = sb("b_ada_gt_sb", [128, 2, 1024], F32, p0)
            wa = [sb("wa%d" % i, [128, 8, 512], BF16, p0) for i in range(3)]
            e_t = sb("e_t", [128, 16], F32, p0)
            t_t = sb("t_t", [128, 16], F32, p0)
            l_t = sb("l_t", [128, 16], F32, p0)
            m_t = sb("m_t", [128, 16], F32, p0)
            pload(b_ada_gt_sb[:], b_ada_gt)
            gt2b = sb("gt2b0", [128, 1024], F32, p0)

            S.op("act", lambda e: e.activation(out=cs[:], in_=cvec_sb[:], func=AF.Silu), reads=("params",), writes=("cs",))
            S.op("dve", lambda e: e.tensor_copy(out=cs_rep[:], in_=cs[:, :, 0:1].to_broadcast([128, 8, 128])),
                 reads=("cs",), writes=("cs_rep",))
            psm = bank(0)[:, 0:96].rearrange("p (j t) -> p j t", t=2)
            for q in range(12):
                s = q % 3
                S.dma("pool", wa[s][:], w_ada[:, 512 * q:512 * q + 512].rearrange("(k p) n -> p k n", p=128),
                      writes=("wa%d" % s,), key="wa%d" % s)
                fns = []
                for jj in range(4):
                    for k in range(8):
                        fns.append(lambda e, jj=jj, k=k, s=s, q=q: e.matmul(
                            psm[:, 4 * q + jj, :], lhsT=wa[s][:, k, 128 * jj:128 * jj + 128], rhs=cs[:, k, :],
                            start=(k == 0), stop=(k == 7)))
                S.group("pe", fns, reads=("wa%d" % s, "cs"), writes=("ps0",))
                if q in (4, 5, 10, 11):
                    bk = 1 + (q % 2)
                    S.group("pe", [lambda e, k=k, s=s, bk=bk: e.matmul(bank(bk), lhsT=cs_rep[:, k, :], rhs=wa[s][:, k, :],
                                                                      start=(k == 0), stop=(k == 7)) for k in range(8)],
                            reads=("wa%d" % s, "cs_rep"), writes=(pk(bk),))
                    dst = gt1h if q < 6 else gt2b
                    gi = 0 if q < 6 else 1
                    cols = slice(512 * (q % 2), 512 * (q % 2) + 512)
                    S.op("dve", lambda e, dst=dst, gi=gi, cols=cols, bk=bk: e.tensor_tensor(
                        out=dst[:, cols], in0=bank(bk), in1=b_ada_gt_sb[:, gi, cols], op=ALU.add),
                        reads=(pk(bk), "params"), writes=("gt",))
            S.op("dve", lambda e: e.tensor_scalar_mul(out=gt1h[:], in0=gt1h[:], scalar1=0.5), reads=("gt",), writes=("gt",))
            S.dma("sp", gt2_scr, gt2b[:], reads=("gt",), writes=("gt2_scr",), key="gt2_scr")
            S.op("dve", lambda e: e.tensor_tensor(out=mods[:], in0=psm, in1=b_ada_fm_sb[:].unsqueeze(2).to_broadcast([128, 48, 2]),
                                                  op=ALU.add), reads=("ps0", "params"), writes=("mods",))
            for (dst, col, j0, gsb) in ((s1, 0, 8, gmix_sb), (s1c, 1, 8, gmix_sb), (s2, 0, 32, gffn_sb)):
                S.op("dve", lambda e, dst=dst, col=col, j0=j0, gsb=gsb: e.scalar_tensor_tensor(
                    out=dst[:], in0=mods[:, j0:j0 + 8, col], scalar=1.0, in1=gsb[:], op0=ALU.add, op1=ALU.mult),
                    reads=("mods", "params"), writes=("sc",))
                S.op("dve", lambda e, dst=dst: e.tensor_scalar_mul(out=dst[:], in0=dst[:], scalar1=32.0),
                     reads=("sc",), writes=("sc",))
            S.op("dve", lambda e: e.tensor_scalar_mul(out=hb_in[:], in0=b_in_sb[:], scalar1=0.5), reads=("params",), writes=("hb_in",))
            S.op("dve", lambda e: e.tensor_scalar_mul(out=hbg[:], in0=bg_sb[:], scalar1=0.5), reads=("params",), writes=("hbg",))
            lamf = lam_sb[:].rearrange("p a b -> p (a b)")
            S.op("act", lambda e: e.activation(out=e_t[:], in_=lamf, func=AF.Exp, scale=-1.0), reads=("params",), writes=("e_t",))
            S.op("dve", lambda e: e.tensor_scalar(out=t_t[:], in0=e_t[:], scalar1=-0.25, scalar2=1.0 / 3.0, op0=ALU.mult, op1=ALU.add),
                 reads=("e_t",), writes=("t_t",))
            S.op("dve", lambda e: e.tensor_tensor(out=t_t[:], in0=t_t[:], in1=e_t[:], op=ALU.mult), reads=("t_t", "e_t"), writes=("t_t",))
            S.op("dve", lambda e: e.tensor_scalar_add(out=t_t[:], in0=t_t[:], scalar1=-0.5), reads=("t_t",), writes=("t_t",))
            S.op("dve", lambda e: e.tensor_tensor(out=t_t[:], in0=t_t[:], in1=e_t[:], op=ALU.mult), reads=("t_t", "e_t"), writes=("t_t",))
            S.op("dve", lambda e: e.tensor_scalar_add(out=t_t[:], in0=t_t[:], scalar1=1.0), reads=("t_t",), writes=("t_t",))
            S.op("dve", lambda e: e.tensor_tensor(out=t_t[:], in0=t_t[:], in1=e_t[:], op=ALU.mult), reads=("t_t", "e_t"), writes=("t_t",))
            S.op("dve", lambda e: e.tensor_scalar_add(out=l_t[:], in0=e_t[:], scalar1=1.0), reads=("e_t",), writes=("l_t",))
            S.op("act", lambda e: e.activation(out=l_t[:], in_=l_t[:], func=AF.Ln), reads=("l_t",), writes=("l_t",))
            S.op("dve", lambda e: e.tensor_single_scalar(out=m_t[:], in_=e_t[:], scalar=0.1, op=ALU.is_lt), reads=("e_t",), writes=("m_t",))
            S.op("dve", lambda e: e.tensor_tensor(out=t_t[:], in0=t_t[:], in1=l_t[:], op=ALU.subtract), reads=("t_t", "l_t"), writes=("t_t",))
            S.op("dve", lambda e: e.tensor_tensor(out=t_t[:], in0=t_t[:], in1=m_t[:], op=ALU.mult), reads=("t_t", "m_t"), writes=("t_t",))
            S.op("dve", lambda e: e.tensor_tensor(out=t_t[:], in0=t_t[:], in1=l_t[:], op=ALU.add), reads=("t_t", "l_t"), writes=("t_t",))
            clf = cl[:].rearrange("p a b -> p (a b)")
            hclf = hcl[:].rearrange("p a b -> p (a b)")
            S.op("dve", lambda e: e.tensor_scalar_mul(out=clf, in0=t_t[:], scalar1=-8.0), reads=("t_t",), writes=("cl",))
            S.op("dve", lambda e: e.tensor_scalar_mul(out=hclf, in0=t_t[:], scalar1=-4.0), reads=("t_t",), writes=("cl",))
            S.barrier()

        mixer = ExitStack()
        wxr = sb("wxr", [128, 8, 1024], BF16, mixer)
        wgb = sb("wgb", [128, 4, 8, 128], BF16, mixer)
        dg5 = sb("dg5", [128, 8, 5, 128], BF16, mixer)
        S.dma("pool", wxr[:], w_in[:, 3072:4096].rearrange("(k p) n -> p k n", p=128), writes=("wxr",), key="wxr")
        S.dma("pool", wgb[:], wg.rearrange("g h p n -> p g h n"), writes=("wgb",), key="wgb")
        for c in range(8):
            S.op("dve", lambda e, c=c: e.tensor_tensor(
                out=dg5[:, c, :, :], in0=identb[:].unsqueeze(1).to_broadcast([128, 5, 128]),
                in1=lw5_sb[:, c, :].unsqueeze(2).to_broadcast([128, 5, 128]), op=ALU.mult),
                reads=("identb", "params"), writes=("dg5",))

        xt = sb("xt", [128, 4, 1024], F32, mixer)
        xh = sb("xh", [4, 1024], F32, mixer)
        xn = sb("xn", [128, 4, 1024], BF16, mixer)
        xnh = sb("xnh", [4, 1024], BF16, mixer)
        hxT = sb("hxT", [128, 8, NB + 4], BF16, mixer)
        xrp = [sb("xrp%d" % i, [128, NB + 4], BF16, mixer) for i in range(2)]
        xcb = [sb("xcb%d" % i, [128, NB], BF16, mixer) for i in range(2)]
        tr = sb("tr", [128, NB], F32, mixer)
        ti = sb("ti", [128, NB], F32, mixer)
        a4 = sb("a4", [128, 4, NB], F32, mixer)
        s4 = sb("s4", [128, 4, NB], F32, mixer)
        t4 = sb("t4", [128, 4, NB], BF16, mixer)
        tmp1 = sb("tmp1", [128, NB], F32, mixer)
        bb_t = sb("bb_t", [128, NB], F32, mixer)
        hf = sb("hf", [128, NB], F32, mixer)
        hsb = sb("hsb", [128, 8, NB], BF16, mixer)

        tph = bank(4).bitcast(BF16)[:, 0:32].rearrange("p (c t) -> p c t", t=4)

        def prep(xsrc, r0, N, sc, bcol, keep_key):
            nt = N // 128
            S.dma("sp", xt[:, 0:nt, :], xsrc[r0:r0 + N, :].rearrange("(j p) d -> p j d", p=128), writes=(keep_key,), key="xt")
            S.dma("sp", xh[0:2, :], xsrc[r0 - 2:r0, :], writes=("xh",), key="xh")
            S.dma("sp", xh[2:4, :], xsrc[r0 + N:r0 + N + 2, :], writes=("xh",), key="xh")
            S.op("pool", lambda e: e.memset(ss[:], 0.0), writes=("ss",))
            for j in range(nt):
                S.op("act", lambda e, j=j: e.activation(out=xn[:, j, :], in_=xt[:, j, :], func=AF.Square, accum_out=ss[:, j:j + 1]),
                     reads=(keep_key,), writes=("xn", "ss"))
            S.op("act", lambda e: e.activation(out=xnh[:], in_=xh[:], func=AF.Square, accum_out=ss[0:4, 4:5]),
                 reads=("xh",), writes=("xnh", "ss"))
            S.op("act", lambda e: e.activation(out=rs[:, 0:5], in_=ss[:, 0:5], func=AF.Sqrt, bias=qtr[:, 1:2]), reads=("ss", "qtr"), writes=("rs",))
            S.op("dve", lambda e: e.reciprocal(out=rs[:, 0:5], in_=rs[:, 0:5]), reads=("rs",), writes=("rs",))
            for j in range(nt):
                S.op("dve", lambda e, j=j: e.tensor_scalar_mul(out=xn[:, j, :], in0=xt[:, j, :], scalar1=rs[:, j:j + 1]),
                     reads=(keep_key, "rs"), writes=("xn",))
            S.op("dve", lambda e: e.tensor_scalar_mul(out=xnh[:], in0=xh[:], scalar1=rs[0:4, 4:5]),
                 reads=("xh", "rs"), writes=("xnh",))
            for j in range(nt):
                S.group("pe", [lambda e, j=j, c=c: e.transpose(out=tpv[:, c, 128 * j:128 * j + 128],
                                                               in_=xn[:, j, 128 * c:128 * c + 128], identity=identb[:])
                               for c in range(8)], reads=("xn", "identb"), writes=TPK)
            S.group("pe", [lambda e, c=c: e.transpose(out=tph[:, c, :], in_=xnh[:, 128 * c:128 * c + 128], identity=identb[0:4, 0:4])
                           for c in range(8)], reads=("xnh", "identb"), writes=("ps4",))
            for c in range(8):
                S.op("act", lambda e, c=c: e.activation(out=hxT[:, c, 0:N], in_=tpv[:, c, 0:N], func=AF.Identity,
                                                        scale=sc[:, c:c + 1], bias=mods[:, c, bcol:bcol + 1]),
                     reads=TPK + ("sc", "mods"), writes=("hxT",))
            S.op("dve", lambda e: e.tensor_tensor(out=hxT[:, :, N:N + 4], in0=tph, in1=sc[:].unsqueeze(2).to_broadcast([128, 8, 4]),
                                                  op=ALU.mult), reads=("ps4", "sc"), writes=("hxTh",))
            S.op("dve", lambda e: e.tensor_tensor(out=hxT[:, :, N:N + 4], in0=hxT[:, :, N:N + 4],
                                                  in1=mods[:, 0:8, bcol:bcol + 1].to_broadcast([128, 8, 4]), op=ALU.add),
                 reads=("hxTh", "mods"), writes=("hxTh",))

        def rglru_block(N, d, reverse, has_lo, has_hi, consumer=None):
            def st1(c):
                par = c % 2
                bxr = bank(par)[:, 0:N]
                bxh = bank(2 + par)[:, 0:4]
                S.group("pe", [lambda e, k=k: e.matmul(bxr, lhsT=wxr[:, k, 128 * c:128 * c + 128], rhs=hxT[:, k, 0:N],
                                                       start=(k == 0), stop=(k == 7)) for k in range(8)],
                        reads=("hxT", "wxr"), writes=(pk(par),))
                S.group("pe", [lambda e, k=k: e.matmul(bxh, lhsT=wxr[:, k, 128 * c:128 * c + 128], rhs=hxT[:, k, N:N + 4],
                                                       start=(k == 0), stop=(k == 7)) for k in range(8)],
                        reads=("hxTh", "wxr"), writes=(pk(2 + par),))
                xk = "xrp%d" % par
                bia = b_in_sb[:, 24 + c:25 + c]
                S.op("dve", lambda e: e.tensor_scalar_add(out=xrp[par][:, 2:2 + N], in0=bxr, scalar1=bia),
                     reads=(pk(par), "params"), writes=(xk,))
                if has_lo:
                    S.op("dve", lambda e: e.tensor_scalar_add(out=xrp[par][:, 0:2], in0=bxh[:, 0:2], scalar1=bia),
                         reads=(pk(2 + par), "params"), writes=(xk,))
                else:
                    S.op("dve", lambda e: e.memset(xrp[par][:, 0:2], 0.0), writes=(xk,))
                if has_hi:
                    S.op("dve", lambda e: e.tensor_scalar_add(out=xrp[par][:, 2 + N:4 + N], in0=bxh[:, 2:4], scalar1=bia),
                         reads=(pk(2 + par), "params"), writes=(xk,))
                else:
                    S.op("dve", lambda e: e.memset(xrp[par][:, 2 + N:4 + N], 0.0), writes=(xk,))

            def st2(c):
                par = c % 2
                xk = "xrp%d" % par
                bcv = bank(4 + par)[:, 0:N]
                S.group("pe", [lambda e, j=j: e.matmul(bcv, lhsT=dg5[:, c, j, :], rhs=xrp[par][:, j:j + N],
                                                       start=(j == 0), stop=(j == 4)) for j in range(5)],
                        reads=(xk, "dg5"), writes=(pk(4 + par),))
                S.op("dve", lambda e: e.tensor_scalar_add(out=xcb[par][:, 0:N], in0=bcv, scalar1=lb_sb[:, c:c + 1]),
                     reads=(pk(4 + par), "params"), writes=("xcb%d" % par,))

            def st3(c):
                par = c % 2
                q = c % 4
                ck = "xcb%d" % par
                br_ = bank(6)[:, 0:N]
                bi_ = bank(7)[:, 0:N]
                S.group("pe", [lambda e: e.matmul(br_, lhsT=wgb[:, 2 * d, c, :], rhs=xcb[par][:, 0:N], start=True, stop=True)],
                        reads=(ck, "wgb"), writes=("ps6",))
                S.group("pe", [lambda e: e.matmul(bi_, lhsT=wgb[:, 2 * d + 1, c, :], rhs=xcb[par][:, 0:N], start=True, stop=True)],
                        reads=(ck, "wgb"), writes=("ps7",))
                S.op("act", lambda e: e.activation(out=tr[:, 0:N], in_=br_, func=AF.Tanh, scale=0.5, bias=hbg[:, 2 * d, c:c + 1]),
                     reads=("ps6", "hbg"), writes=("tr",))
                S.op("act", lambda e: e.activation(out=ti[:, 0:N], in_=bi_, func=AF.Tanh, scale=0.5, bias=hbg[:, 2 * d + 1, c:c + 1]),
                     reads=("ps7", "hbg"), writes=("ti",))
                S.op("act", lambda e: e.activation(out=a4[:, q, 0:N], in_=tr[:, 0:N], func=AF.Exp, scale=hcl[:, d, c:c + 1],
                                                   bias=hcl[:, d, c:c + 1]), reads=("tr", "cl"), writes=("a4_%d" % q,))
                S.op("act", lambda e: e.activation(out=s4[:, q, 0:N], in_=tr[:, 0:N], func=AF.Exp, scale=cl[:, d, c:c + 1],
                                                   bias=cl[:, d, c:c + 1]), reads=("tr", "cl"), writes=("s4_%d" % q,))
                S.op("dve", lambda e: e.scalar_tensor_tensor(out=t4[:, q, 0:N], in0=ti[:, 0:N], scalar=1.0, in1=xcb[par][:, 0:N],
                                                             op0=ALU.add, op1=ALU.mult), reads=("ti", ck), writes=("t4_%d" % q,))

            def st4(c0):
                sk = tuple("s4_%d" % q for q in range(4))
                S.op("act", lambda e: e.activation(out=s4[:, :, 0:N], in_=s4[:, :, 0:N], func=AF.Sqrt, scale=-0.25, bias=qtr[:, 0:1]),
                     reads=sk + ("qtr",), writes=sk)
                for c in range(c0, c0 + 4):
                    q = c % 4
                    S.op("dve", lambda e, q=q: e.tensor_tensor(out=bb_t[:, 0:N], in0=s4[:, q, 0:N], in1=t4[:, q, 0:N], op=ALU.mult),
                         reads=("s4_%d" % q, "t4_%d" % q), writes=("bb_t",))
                    if reverse:
                        S.op("dve", lambda e, q=q, c=c: e.tensor_tensor_scan(
                            out=hf[:, 0:N][:, ::-1], data0=a4[:, q, 0:N][:, ::-1], data1=bb_t[:, 0:N][:, ::-1],
                            initial=state[:, d, c:c + 1], op0=ALU.mult, op1=ALU.add),
                            reads=("a4_%d" % q, "bb_t", "state"), writes=("hf",))
                        S.op("act", lambda e, c=c: e.activation(out=state[:, d, c:c + 1], in_=hf[:, 0:1], func=AF.Copy),
                             reads=("hf",), writes=("state",))
                    else:
                        S.op("dve", lambda e, q=q, c=c: e.tensor_tensor_scan(
                            out=hf[:, 0:N], data0=a4[:, q, 0:N], data1=bb_t[:, 0:N], initial=state[:, d, c:c + 1],
                            op0=ALU.mult, op1=ALU.add), reads=("a4_%d" % q, "bb_t", "state"), writes=("hf",))
                        S.op("act", lambda e, c=c: e.activation(out=state[:, d, c:c + 1], in_=hf[:, N - 1:N], func=AF.Copy),
                             reads=("hf",), writes=("state",))
                    if consumer is not None:
                        consumer(c)

            for s_ in range(10):
                if s_ < 8:
                    st1(s_)
                if 1 <= s_ <= 8:
                    st2(s_ - 1)
                if 2 <= s_ <= 9:
                    st3(s_ - 2)
                    if (s_ - 2) % 4 == 3:
                        st4(s_ - 2 - 3)

        prep(ctxp, 2, 256, s1c, 1, "xt")
        rglru_block(256, 0, False, False, False)
        rglru_block(256, 1, True, False, False)
        for blk in range(15, -1, -1):
            prep(xp, 2 + NB * blk, NB, s1, 0, "xt")
            def cons_a(c):
                S.op("dve", lambda e, c=c: e.tensor_copy(out=hsb[:, c, :], in_=hf[:]), reads=("hf",), writes=("hsb",))
            rglru_block(NB, 1, True, blk != 0, blk != 15, cons_a if blk < 8 else None)
            if blk < 8:
                S.dma("sp", hs_scr[blk], hsb[:], reads=("hsb",), writes=("hs_scr%d" % blk,), key="hs_scr")

        pb_ = ExitStack()
        cwh = sb("cwh", [128, 8, 31], F32, pb_)
        S.op("dve", lambda e: e.tensor_scalar_mul(out=cwh[:], in0=cw_sb[:], scalar1=0.5), reads=("params",), writes=("cwh",))
        wsl = [sb("wsl%d" % i, [128, 8, 512], BF16, pb_) for i in range(3)]
        dgc = [sb("dgc0", [128, 31, 128], BF16, pb_)] * 2
        tv, uu, mean, rstd, mr = tr, ti, a4[:, 0, :], a4[:, 1, :], a4[:, 2, :]
        zb = [sb("zb0", [128, NB], BF16, pb_)] * 2
        zc = sb("zc", [128, 8, NB], BF16, pb_)
        aa = zc
        zsq = [sb("zsq0", [128, NB], BF16, pb_)] * 2
        A_t = sb("A_t", [128, 8, NB], BF16, pb_)
        gy = sb("gy", [128, 8, NB], BF16, pb_)
        mg = gy
        yb = sb("yb", [128, 8, NB], BF16, pb_)
        hs_in = hsb
        x1 = xt

        wctr = [0]

        def wpiece(col0, src=None):
            src = w_in if src is None else src
            i = wctr[0] % 3
            wctr[0] += 1
            S.dma("pool", wsl[i][:], src[:, col0:col0 + 512].rearrange("(k p) n -> p k n", p=128),
                  writes=("wsl%d" % i,), key="wsl%d" % i)
            return wsl[i], "wsl%d" % i

        def inproj(dstbank, wt, wk, j4):
            S.group("pe", [lambda e, k=k: e.matmul(bank(dstbank), lhsT=wt[:, k, 128 * j4:128 * j4 + 128], rhs=hxT[:, k, 0:NB],
                                                   start=(k == 0), stop=(k == 7)) for k in range(8)],
                    reads=("hxT", wk), writes=(pk(dstbank),))

        for blk in range(8):
            prep(xp, 2 + NB * blk, NB, s1, 0, "xt")
            S.dma("sp", hs_in[:], hs_scr[blk], reads=("hs_scr%d" % blk,), writes=("hsb",), key="hs_in")
            for half in range(2):
                wu, wuk = wpiece(512 * half)
                wv, wvk = wpiece(1024 + 512 * half)
                for c4 in range(4):
                    c = 4 * half + c4
                    par = c % 2
                    inproj(par, wu, wuk, c4)
                    inproj(2 + par, wv, wvk, c4)
                    S.op("act", lambda e, c=c, par=par: e.activation(out=tv[:], in_=bank(2 + par), func=AF.Tanh, scale=0.5,
                                                                     bias=hb_in[:, 8 + c:9 + c]),
                         reads=(pk(2 + par), "hb_in"), writes=("tr",))
                    S.op("act", lambda e, c=c, par=par: e.activation(out=uu[:], in_=bank(par), func=AF.Identity,
                                                                     bias=b_in_sb[:, c:c + 1]),
                         reads=(pk(par), "params"), writes=("ti",))
                    zk = "zb0"
                    S.op("dve", lambda e, par=par: e.scalar_tensor_tensor(out=zb[par][:], in0=tv[:], scalar=1.0, in1=uu[:],
                                                                          op0=ALU.add, op1=ALU.mult),
                         reads=("tr", "ti"), writes=(zk,))
                    dk = "dgc0"
                    S.op("dve", lambda e, c=c, par=par: e.tensor_tensor(
                        out=dgc[par][:], in0=identb[:].unsqueeze(1).to_broadcast([128, 31, 128]),
                        in1=cwh[:, c, :].unsqueeze(2).to_broadcast([128, 31, 128]), op=ALU.mult),
                        reads=("identb", "cwh"), writes=(dk,))
                    zv = zb[par][:].rearrange("p (r t) -> p r t", t=64)
                    pcv = bank(4 + par).rearrange("p (r t) -> p r t", t=64)
                    fns = []
                    order = [15] + [k for k in range(31) if k != 15]
                    for idx, k in enumerate(order):
                        o = k - 15
                        t0, t1 = max(0, -o), 64 - max(0, o)
                        fns.append(lambda e, k=k, o=o, t0=t0, t1=t1, idx=idx, par=par, pcv=pcv, zv=zv: e.matmul(
                            pcv[:, :, t0:t1], lhsT=dgc[par][:, k, :], rhs=zv[:, :, t0 + o:t1 + o],
                            start=(idx == 0), stop=(idx == 30)))
                    S.group("pe", fns, reads=(zk, dk), writes=(pk(4 + par),))
                    S.op("act", lambda e, c=c, par=par: e.activation(out=zc[:, c, :], in_=bank(4 + par), func=AF.Identity,
                                                                     bias=cb_sb[:, c:c + 1]),
                         reads=(pk(4 + par), "params"), writes=("zc",))
                    qk = "zsq0"
                    S.op("act", lambda e, c=c, par=par: e.activation(out=zsq[par][:], in_=bank(4 + par), func=AF.Square,
                                                                     bias=cb_sb[:, c:c + 1]),
                         reads=(pk(4 + par), "params"), writes=(qk,))
                    S.group("pe", [lambda e, c=c: e.matmul(bank(6), lhsT=ones_m[:], rhs=zc[:, c, :], start=(c == 0), stop=(c == 7))],
                            reads=("zc", "ones_m"), writes=("ps6",))
                    S.group("pe", [lambda e, c=c, par=par: e.matmul(bank(7), lhsT=ones_m[:], rhs=zsq[par][:], start=(c == 0),
                                                                    stop=(c == 7))], reads=(qk, "ones_m"), writes=("ps7",))
            S.op("act", lambda e: e.activation(out=mean, in_=bank(6), func=AF.Copy), reads=("ps6",), writes=("a4_0",))
            S.op("dve", lambda e: e.tensor_tensor(out=mr, in0=mean, in1=mean, op=ALU.mult), reads=("a4_0",), writes=("a4_2",))
            S.op("dve", lambda e: e.tensor_tensor(out=rstd, in0=bank(7), in1=mr, op=ALU.subtract), reads=("ps7", "a4_2"),
                 writes=("a4_1",))
            S.op("act", lambda e: e.activation(out=rstd, in_=rstd, func=AF.Sqrt, bias=qtr[:, 2:3]), reads=("a4_1", "qtr"), writes=("a4_1",))
            S.op("dve", lambda e: e.reciprocal(out=rstd, in_=rstd), reads=("a4_1",), writes=("a4_1",))
            S.op("dve", lambda e: e.tensor_tensor(out=mr, in0=mean, in1=rstd, op=ALU.mult), reads=("a4_0", "a4_1"),
                 writes=("a4_2",))
            for c in range(8):
                S.op("dve", lambda e, c=c: e.tensor_tensor(out=tv[:], in0=zc[:, c, :], in1=rstd, op=ALU.mult),
                     reads=("zc", "a4_1"), writes=("tr",))
                S.op("dve", lambda e: e.tensor_tensor(out=uu[:], in0=tv[:], in1=mr, op=ALU.subtract), reads=("tr", "a4_2"),
                     writes=("ti",))
                S.op("act", lambda e, c=c: e.activation(out=aa[:, c, :], in_=uu[:], func=AF.Silu, scale=lng_sb[:, c:c + 1],
                                                        bias=lnb_sb[:, c:c + 1]), reads=("ti", "params"), writes=("zc",))
            for half in range(2):
                wga, wgak = wpiece(4096 + 512 * half)
                wpa, wpak = wpiece(512 * half, w_pa)
                for m4 in range(4):
                    m = 4 * half + m4
                    par = m % 2
                    S.group("pe", [lambda e, k=k, m4=m4, par=par, wpa=wpa: e.matmul(bank(par), lhsT=wpa[:, k, 128 * m4:128 * m4 + 128],
                                                                         rhs=aa[:, k, :], start=(k == 0), stop=(k == 7))
                                   for k in range(8)], reads=("zc", wpak), writes=(pk(par),))
                    inproj(2 + par, wga, wgak, m4)
                    S.op("act", lambda e, m=m, par=par: e.activation(out=tv[:], in_=bank(2 + par), func=AF.Tanh, scale=0.5,
                                                                     bias=hb_in[:, 32 + m:33 + m]),
                         reads=(pk(2 + par), "hb_in"), writes=("tr",))
                    S.op("dve", lambda e, m=m, par=par: e.scalar_tensor_tensor(out=A_t[:, m, :], in0=tv[:], scalar=1.0,
                                                                               in1=bank(par), op0=ALU.add, op1=ALU.mult),
                         reads=("tr", pk(par)), writes=("A_t",))
            for half in range(2):
                wy, wyk = wpiece(2048 + 512 * half)
                for c4 in range(4):
                    c = 4 * half + c4
                    par = c % 2
                    inproj(par, wy, wyk, c4)
                    S.op("act", lambda e, c=c, par=par: e.activation(out=gy[:, c, :], in_=bank(par), func=AF.Gelu_apprx_tanh,
                                                                     bias=b_in_sb[:, 16 + c:17 + c]),
                         reads=(pk(par), "params"), writes=("gy",))
            def cons_b(c):
                S.op("dve", lambda e, c=c: e.tensor_tensor(out=tmp1[:], in0=hf[:], in1=hs_in[:, c, :], op=ALU.add),
                     reads=("hf", "hsb"), writes=("tmp1",))
                S.op("dve", lambda e, c=c: e.tensor_tensor(out=yb[:, c, :], in0=tmp1[:], in1=gy[:, c, :], op=ALU.mult),
                     reads=("tmp1", "gy"), writes=("yb",))
            rglru_block(NB, 0, False, blk != 0, True, cons_b)
            for half in range(2):
                wgb_, wgbk = wpiece(5120 + 512 * half)
                wpb, wpbk = wpiece(512 * half, w_pb)
                for m4 in range(4):
                    m = 4 * half + m4
                    par = m % 2
                    S.group("pe", [lambda e, k=k, m4=m4, par=par, wpb=wpb: e.matmul(bank(par), lhsT=wpb[:, k, 128 * m4:128 * m4 + 128],
                                                                         rhs=yb[:, k, :], start=(k == 0), stop=(k == 7))
                                   for k in range(8)], reads=("yb", wpbk), writes=(pk(par),))
                    inproj(2 + par, wgb_, wgbk, m4)
                    S.op("act", lambda e, m=m, par=par: e.activation(out=tv[:], in_=bank(2 + par), func=AF.Tanh, scale=0.5,
                                                                     bias=hb_in[:, 40 + m:41 + m]),
                         reads=(pk(2 + par), "hb_in"), writes=("tr",))
                    S.op("dve", lambda e, par=par: e.scalar_tensor_tensor(out=uu[:], in0=tv[:], scalar=1.0, in1=bank(par),
                                                                          op0=ALU.add, op1=ALU.mult),
                         reads=("tr", pk(par)), writes=("ti",))
                    S.op("dve", lambda e, m=m: e.tensor_tensor(out=mg[:, m, :], in0=uu[:], in1=A_t[:, m, :], op=ALU.add),
                         reads=("ti", "A_t"), writes=("gy",))
            for hh in range(2):
                wo, wok = wpiece(512 * hh, w_o)
                for j in range(4):
                    bk = 4 + j
                    S.group("pe", [lambda e, k=k, j=j, hh=hh, bk=bk, wo=wo: e.matmul(bank(bk), lhsT=mg[:, k, 128 * j:128 * j + 128],
                                                                              rhs=wo[:, k, :],
                                                                              start=(k == 0), stop=(k == 7)) for k in range(8)],
                            reads=("gy", wok), writes=(pk(bk),))
                    S.op("dve", lambda e, hh=hh, bk=bk: e.tensor_tensor(out=tv[:], in0=bank(bk), in1=gt1h[:, 512 * hh:512 * hh + 512],
                                                                        op=ALU.mult), reads=(pk(bk), "gt"), writes=("tr",))
                    S.op("pool", lambda e, j=j, hh=hh: e.tensor_tensor(out=x1[:, j, 512 * hh:512 * hh + 512], in0=tv[:],
                                                                       in1=xt[:, j, 512 * hh:512 * hh + 512], op=ALU.add),
                         reads=("tr", "xt"), writes=("xt",))
            S.dma("sp", x1_scr[NB * blk:NB * blk + NB, :].rearrange("(j p) d -> p j d", p=128), xt[:],
                  reads=("xt",), writes=("x1_scr%d" % blk,), key="x1_scr")
        S.barrier()
        pb_.close()
        mixer.close()

        pc = ExitStack()
        gf32 = sb("gf32", [128, 1024], F32, pc)
        gt2b = sb("gt2b", [128, 1024], F32, pc)
        S.dma("sp", gf32[:], gfin, writes=("gf32",), key="gf32")
        S.dma("sp", gt2b[:], gt2_scr, reads=("gt2_scr",), writes=("gt2b",), key="gt2b")
        S.op("dve", lambda e: e.tensor_scalar_mul(out=gf32[:], in0=gf32[:], scalar1=32.0), reads=("gf32",), writes=("gf32",))
        accs = [sb("acc%d" % i, [128, 4, 1024], F32, pc) for i in range(2)]
        hmTs = [sb("hmT%d" % i, [128, 8, NB], BF16, pc) for i in range(2)]
        xn2 = sb("xn2", [128, 4, 1024], BF16, pc)
        junkF = sb("junkF", [128, 1024], BF16, pc)
        cbc = sb("cbc", [128, 16, NB], BF16, pc)
        dgm = sb("dgm", [128, 16, 128], BF16, pc)
        actb = [sb("actb%d" % i, [128, NB], BF16, pc) for i in range(16)]
        wgu = [sb("wgu%d" % i, [128, 2, 8, 512], BF16, pc) for i in range(2)]
        NWD = 5
        wd = [sb("wd%d" % i, [128, 4, 1024], BF16, pc) for i in range(NWD)]
        sg = [sb("sg%d" % i, [128, NB], F32, pc) for i in range(2)]
        tt = [sb("tt%d" % i, [128, NB], BF16, pc) for i in range(2)]
        evt = [sb("evt%d" % i, [128, NB], F32, pc) for i in range(2)]
        ssA = sb("ssA", [128, 2, 4], F32, pc)
        rsA = sb("rsA", [128, 2, 4], F32, pc)
        ssF = sb("ssF", [128, 2, 4], F32, pc)
        rsF = sb("rsF", [128, 2, 4], F32, pc)
        L = sb("L", [128, 4, 20], F32, pc)
        gmax = sb("gmax", [128, 4, 1], F32, pc)
        oh = sb("oh", [128, 4, 4], F32, pc)
        eg = sb("eg", [128, 4, 4], F32, pc)
        pg = sb("pg", [128, 4, 1], F32, pc)
        tmp16 = sb("tmp16", [128, 4, 16], F32, pc)
        esel = sb("esel", [128, 4, 4], F32, pc)
        m1 = sb("m1", [128, 4, 1], F32, pc)
        m2 = sb("m2", [128, 4, 1], F32, pc)
        k1 = sb("k1", [128, 4, 4], F32, pc)
        k2 = sb("k2", [128, 4, 4], F32, pc)
        e2 = sb("e2", [128, 4, 4], F32, pc)
        w1 = sb("w1", [128, 4, 1], F32, pc)
        w2 = sb("w2", [128, 4, 1], F32, pc)
        wsel = sb("wsel", [128, 4, 4], F32, pc)
        comb = sb("comb", [128, 4, 16], F32, pc)
        ectr = [0]
        evc = [0]

        def bc(ap, shape):
            return ap.to_broadcast(shape)

        for blk in range(8):
            pb2 = blk % 2
            acc, hmT = accs[pb2], hmTs[pb2]
            ak, hk = "acc%d" % pb2, "hmT%d" % pb2
            sak, sfk = "ssA%d" % pb2, "ssF%d" % pb2
            S.dma("sp", acc[:], x1_scr[NB * blk:NB * blk + NB, :].rearrange("(j p) d -> p j d", p=128),
                  reads=("x1_scr%d" % blk,), writes=(ak,), key=ak)
            S.op("pool", lambda e, pb2=pb2: e.memset(ssA[:, pb2, :], 0.0), writes=(sak,))
            for j in range(4):
                S.op("act", lambda e, j=j, acc=acc, pb2=pb2: e.activation(out=xn2[:, j, :], in_=acc[:, j, :], func=AF.Square,
                                                                          accum_out=ssA[:, pb2, j:j + 1]),
                     reads=(ak,), writes=("xn2", sak))
            S.op("act", lambda e, pb2=pb2: e.activation(out=rsA[:, pb2, :], in_=ssA[:, pb2, :], func=AF.Sqrt, bias=qtr[:, 1:2]),
                 reads=(sak, "qtr"), writes=(sak + "r",))
            S.op("dve", lambda e, pb2=pb2: e.reciprocal(out=rsA[:, pb2, :], in_=rsA[:, pb2, :]), reads=(sak + "r",), writes=(sak + "r",))
            for j in range(4):
                S.op("dve", lambda e, j=j, acc=acc, pb2=pb2: e.tensor_scalar_mul(out=xn2[:, j, :], in0=acc[:, j, :],
                                                                                 scalar1=rsA[:, pb2, j:j + 1]),
                     reads=(ak, sak + "r"), writes=("xn2",))
            for j in range(4):
                S.group("pe", [lambda e, j=j, c=c: e.transpose(out=tpv[:, c, 128 * j:128 * j + 128],
                                                               in_=xn2[:, j, 128 * c:128 * c + 128], identity=identb[:])
                               for c in range(8)], reads=("xn2", "identb"), writes=TPK)
            for c in range(8):
                S.op("act", lambda e, c=c, hmT=hmT: e.activation(out=hmT[:, c, :], in_=tpv[:, c, :], func=AF.Identity,
                                                                 scale=s2[:, c:c + 1], bias=mods[:, 24 + c, 0:1]),
                     reads=TPK + ("sc", "mods"), writes=(hk,))
            for j in range(4):
                S.group("pe", [lambda e, k=k, j=j, hmT=hmT: e.matmul(bank(4)[:, 20 * j:20 * j + 20], lhsT=hmT[:, k, 128 * j:128 * j + 128],
                                                                     rhs=w_rt_b[:, k, :], start=(k == 0), stop=(k == 7))
                               for k in range(8)], reads=(hk, "w_rt_b"), writes=("ps4",))
            S.op("dve", lambda e: e.tensor_tensor(out=L[:], in0=bank(4)[:, 0:80].rearrange("p (j n) -> p j n", n=20),
                                                  in1=bc(b_rt_sb[:].unsqueeze(1), [128, 4, 20]), op=ALU.add),
                 reads=("ps4", "params"), writes=("L",))
            R = ("rt",)
            S.op("dve", lambda e: e.tensor_reduce(out=gmax[:], in_=L[:, :, 0:4], axis=AX.X, op=ALU.max), reads=("L",), writes=R)
            S.op("dve", lambda e: e.tensor_tensor(out=oh[:], in0=L[:, :, 0:4], in1=bc(gmax[:], [128, 4, 4]), op=ALU.is_equal),
                 reads=R + ("L",), writes=R)
            S.op("dve", lambda e: e.tensor_tensor(out=eg[:], in0=L[:, :, 0:4], in1=bc(gmax[:], [128, 4, 4]), op=ALU.subtract),
                 reads=R + ("L",), writes=R)
            S.op("act", lambda e: e.activation(out=eg[:], in_=eg[:], func=AF.Exp), reads=R, writes=R)
            S.op("dve", lambda e: e.tensor_reduce(out=pg[:], in_=eg[:], axis=AX.X, op=ALU.add), reads=R, writes=R)
            S.op("dve", lambda e: e.reciprocal(out=pg[:], in_=pg[:]), reads=R, writes=R)
            S.op("dve", lambda e: e.tensor_tensor(out=tmp16[:].rearrange("p j (g x) -> p j g x", x=4),
                                                  in0=L[:, :, 4:20].rearrange("p j (g x) -> p j g x", x=4),
                                                  in1=bc(oh[:].unsqueeze(3), [128, 4, 4, 4]), op=ALU.mult),
                 reads=R + ("L",), writes=R)
            S.op("dve", lambda e: e.tensor_reduce(out=esel[:].unsqueeze(3), in_=tmp16[:].rearrange("p j (g x) -> p j x g", x=4),
                                                  axis=AX.X, op=ALU.add), reads=R, writes=R)
            S.op("dve", lambda e: e.tensor_reduce(out=m1[:], in_=esel[:], axis=AX.X, op=ALU.max), reads=R, writes=R)
            S.op("dve", lambda e: e.tensor_tensor(out=k1[:], in0=esel[:], in1=bc(m1[:], [128, 4, 4]), op=ALU.is_equal),
                 reads=R, writes=R)
            S.op("dve", lambda e: e.scalar_tensor_tensor(out=e2[:], in0=k1[:], scalar=-1e30, in1=esel[:], op0=ALU.mult, op1=ALU.add),
                 reads=R, writes=R)
            S.op("dve", lambda e: e.tensor_reduce(out=m2[:], in_=e2[:], axis=AX.X, op=ALU.max), reads=R, writes=R)
            S.op("dve", lambda e: e.tensor_tensor(out=k2[:], in0=e2[:], in1=bc(m2[:], [128, 4, 4]), op=ALU.is_equal),
                 reads=R, writes=R)
            S.op("dve", lambda e: e.tensor_tensor(out=w2[:], in0=m2[:], in1=m1[:], op=ALU.subtract), reads=R, writes=R)
            S.op("act", lambda e: e.activation(out=w2[:], in_=w2[:], func=AF.Exp), reads=R, writes=R)
            S.op("dve", lambda e: e.tensor_scalar_add(out=w1[:], in0=w2[:], scalar1=1.0), reads=R, writes=R)
            S.op("dve", lambda e: e.reciprocal(out=w1[:], in_=w1[:]), reads=R, writes=R)
            S.op("dve", lambda e: e.tensor_tensor(out=w2[:], in0=w2[:], in1=w1[:], op=ALU.mult), reads=R, writes=R)
            S.op("dve", lambda e: e.tensor_tensor(out=w1[:], in0=w1[:], in1=pg[:], op=ALU.mult), reads=R, writes=R)
            S.op("dve", lambda e: e.tensor_tensor(out=w2[:], in0=w2[:], in1=pg[:], op=ALU.mult), reads=R, writes=R)
            S.op("dve", lambda e: e.tensor_tensor(out=wsel[:], in0=k1[:], in1=bc(w1[:], [128, 4, 4]), op=ALU.mult), reads=R, writes=R)
            S.op("dve", lambda e: e.tensor_tensor(out=k2[:], in0=k2[:], in1=bc(w2[:], [128, 4, 4]), op=ALU.mult), reads=R, writes=R)
            S.op("dve", lambda e: e.tensor_tensor(out=wsel[:], in0=wsel[:], in1=k2[:], op=ALU.add), reads=R, writes=R)
            S.op("dve", lambda e: e.tensor_tensor(out=comb[:].rearrange("p j (g x) -> p j g x", x=4),
                                                  in0=bc(oh[:].unsqueeze(3), [128, 4, 4, 4]),
                                                  in1=bc(wsel[:].unsqueeze(2), [128, 4, 4, 4]), op=ALU.mult),
                 reads=R, writes=("comb",))
            for j in range(4):
                S.op("dve", lambda e, j=j: e.tensor_tensor(out=dgm[:], in0=bc(identb[:].unsqueeze(1), [128, 16, 128]),
                                                           in1=bc(comb[:, j, :].unsqueeze(2), [128, 16, 128]), op=ALU.mult),
                     reads=("identb", "comb"), writes=("dgm",))
                for q in range(4):
                    S.group("pe", [lambda e, q=q: e.matmul(bank(4 + q), lhsT=ones1[:], rhs=dgm[:, 4 * q:4 * q + 4, :],
                                                           start=True, stop=True)], reads=("dgm", "ones1"), writes=(pk(4 + q),))
                    S.op("act", lambda e, q=q, j=j: e.activation(out=cbc[:, 4 * q:4 * q + 4, 128 * j:128 * j + 128],
                                                                 in_=bank(4 + q).rearrange("p (x t) -> p x t", t=128), func=AF.Copy),
                         reads=(pk(4 + q),), writes=("cbc",))
            for g in range(4):
                for el in range(4):
                    ex = 4 * g + el
                    si = ectr[0] % 2
                    di = ectr[0] % NWD
                    ectr[0] += 1
                    gk, dk_ = "wgu%d" % si, "wd%d" % di
                    S.dma("pool", wgu[si][:, 0], w_gate[ex].rearrange("(k p) n -> p k n", p=128), writes=(gk,), key=gk)
                    S.dma("pool", wgu[si][:, 1], w_up[ex].rearrange("(k p) n -> p k n", p=128), writes=(gk,), key=gk)
                    S.dma("pool", wd[di][:], w_down[ex].rearrange("(k p) n -> p k n", p=128), writes=(dk_,), key=dk_)
                    for f in range(4):
                        u = 4 * el + f
                        pp = u % 2
                        S.group("pe", [lambda e, k=k, f=f, si=si, pp=pp, hmT=hmT: e.matmul(
                            bank(2 * pp), lhsT=wgu[si][:, 0, k, 128 * f:128 * f + 128], rhs=hmT[:, k, :],
                            start=(k == 0), stop=(k == 7)) for k in range(8)], reads=(hk, gk), writes=(pk(2 * pp),))
                        S.group("pe", [lambda e, k=k, f=f, si=si, pp=pp, hmT=hmT: e.matmul(
                            bank(2 * pp + 1), lhsT=wgu[si][:, 1, k, 128 * f:128 * f + 128], rhs=hmT[:, k, :],
                            start=(k == 0), stop=(k == 7)) for k in range(8)], reads=(hk, gk), writes=(pk(2 * pp + 1),))
                        S.op("act", lambda e, pp=pp: e.activation(out=sg[pp][:], in_=bank(2 * pp), func=AF.Silu),
                             reads=(pk(2 * pp),), writes=("sg%d" % pp,))
                        S.op("dve", lambda e, pp=pp: e.tensor_tensor(out=tt[pp][:], in0=bank(2 * pp + 1), in1=sg[pp][:], op=ALU.mult),
                             reads=(pk(2 * pp + 1), "sg%d" % pp), writes=("tt%d" % pp,))
                        S.op("dve", lambda e, pp=pp, u=u, ex=ex: e.tensor_tensor(out=actb[u][:], in0=tt[pp][:], in1=cbc[:, ex, :],
                                                                                op=ALU.mult),
                             reads=("tt%d" % pp, "cbc"), writes=("actb%d" % u,))
                dbase = ectr[0] - 4
                for tp_ in range(2):
                    fns = []
                    for u in range(16):
                        el, f = divmod(u, 4)
                        di = (dbase + el) % NWD
                        for jj in range(2):
                            j = 2 * tp_ + jj
                            for hh in range(2):
                                fns.append(lambda e, u=u, f=f, di=di, j=j, jj=jj, hh=hh: e.matmul(
                                    bank(4 + 2 * jj + hh), lhsT=actb[u][:, 128 * j:128 * j + 128],
                                    rhs=wd[di][:, f, 512 * hh:512 * hh + 512], start=(u == 0), stop=(u == 15)))
                    S.group("pe", fns, reads=tuple("actb%d" % u for u in range(16)) + tuple("wd%d" % ((dbase + el) % NWD) for el in range(4)),
                            writes=("ps4", "ps5", "ps6", "ps7"))
                    for jj in range(2):
                        j = 2 * tp_ + jj
                        for hh in range(2):
                            bk = 4 + 2 * jj + hh
                            ei = evc[0] % 2
                            evc[0] += 1
                            dst = acc[:, j, 512 * hh:512 * hh + 512]
                            S.op("dve", lambda e, bk=bk, hh=hh, ei=ei: e.tensor_tensor(out=evt[ei][:], in0=bank(bk),
                                                                                      in1=gt2b[:, 512 * hh:512 * hh + 512], op=ALU.mult),
                                 reads=(pk(bk), "gt2b"), writes=("evt%d" % ei,))
                            S.op("pool", lambda e, dst=dst, ei=ei: e.tensor_tensor(out=dst, in0=dst, in1=evt[ei][:], op=ALU.add),
                                 reads=("evt%d" % ei, ak), writes=(ak,))
            S.op("pool", lambda e, pb2=pb2: e.memset(ssF[:, pb2, :], 0.0), writes=(sfk,))
            for j in range(4):
                S.op("act", lambda e, j=j, acc=acc, pb2=pb2: e.activation(out=junkF[:], in_=acc[:, j, :], func=AF.Square,
                                                                          accum_out=ssF[:, pb2, j:j + 1]),
                     reads=(ak,), writes=("junkF", sfk))
            S.op("act", lambda e, pb2=pb2: e.activation(out=rsF[:, pb2, :], in_=ssF[:, pb2, :], func=AF.Sqrt, bias=qtr[:, 1:2]),
                 reads=(sfk, "qtr"), writes=(sfk + "r",))
            S.op("dve", lambda e, pb2=pb2: e.reciprocal(out=rsF[:, pb2, :], in_=rsF[:, pb2, :]), reads=(sfk + "r",), writes=(sfk + "r",))
            for j in range(4):
                S.op("dve", lambda e, j=j, acc=acc, pb2=pb2: e.scalar_tensor_tensor(out=acc[:, j, :], in0=acc[:, j, :],
                                                                                    scalar=rsF[:, pb2, j:j + 1], in1=gf32[:],
                                                                                    op0=ALU.mult, op1=ALU.mult),
                     reads=(ak, sfk + "r", "gf32"), writes=(ak,))
            S.dma("sp", out[NB * blk:NB * blk + NB, :].rearrange("(j p) d -> p j d", p=128), acc[:],
                  reads=(ak,), writes=("out%d" % blk,), key="out")
        S.barrier()
        pc.close()
    return nc


_NC_CACHE = {}


def _fm(v):
    v = np.asarray(v, np.float32).reshape(-1, 128)
    return np.ascontiguousarray(v.T)


def kernel(x, c, ctx, c_ctx, w_ada, b_ada, g_mix, w_in, b_in, conv_w, conv_b, ln_g, ln_b, w_pa,
           lru_conv_w, lru_conv_b, w_r_f, b_r_f, w_i_f, b_i_f, lam_f, w_r_b, b_r_b, w_i_b, b_i_b, lam_b,
           w_pb, w_o, g_ffn, w_grp, b_grp, w_er, b_er, w_gate, w_up, w_down, g_final):
    f = lambda a: np.ascontiguousarray(np.asarray(a, np.float32))
    x, c, ctx, c_ctx = f(x), f(c), f(ctx), f(c_ctx)
    B = x.shape[0]
    if "nc" not in _NC_CACHE:
        _NC_CACHE["nc"] = build_program()
    nc = _NC_CACHE["nc"]

    common = {
        "w_ada": f(w_ada[0]), "b_ada_fm": _fm(b_ada[0]),
        "b_ada_gt": f(np.broadcast_to(np.stack([b_ada[0][2048:3072], b_ada[0][5120:6144]])[None], (128, 2, 1024))),
        "w_in": f(w_in[0]), "b_in_fm": _fm(b_in[0]),
        "cb": _fm(conv_b[0]), "lng": _fm(ln_g[0]), "lnb": _fm(ln_b[0]),
        "w_pa": f(w_pa[0]), "w_pb": f(w_pb[0]), "w_o": f(w_o[0]),
        "lb": _fm(lru_conv_b[0]),
        "gmix": _fm(g_mix[0]), "gffn": _fm(g_ffn[0]),
        "gfin": f(np.broadcast_to(np.asarray(g_final, np.float32)[None], (128, 1024))),
        "w_rt": f(np.concatenate([w_grp[0], w_er[0]], axis=1)),
        "b_rt": f(np.broadcast_to(np.concatenate([b_grp[0], b_er[0]])[None], (128, 20))),
        "w_gate": f(w_gate[0]), "w_up": f(w_up[0]), "w_down": f(w_down[0]),
        "ident": np.eye(128, dtype=np.float32),
    }
    cwn = np.asarray(conv_w[0], np.float32)
    lwn = np.asarray(lru_conv_w[0], np.float32)
    zero = np.zeros((1, 1024), np.float32)
    lw5_nat = np.concatenate([lwn, zero], axis=0)
    lw5_rev = lw5_nat[::-1]

    def fm3(a):
        T = a.shape[0]
        return np.ascontiguousarray(a.reshape(T, 8, 128).transpose(2, 1, 0))

    pf = (w_r_f[0], b_r_f[0], w_i_f[0], b_i_f[0], lam_f[0])
    pbk = (w_r_b[0], b_r_b[0], w_i_b[0], b_i_b[0], lam_b[0])

    def gates(P, Sd):
        wgs = np.stack([P[0], P[2], Sd[0], Sd[2]]).astype(np.float32)
        bgs = np.stack([np.asarray(t, np.float32) for t in (P[1], P[3], Sd[1], Sd[3])])
        bgs = np.ascontiguousarray(bgs.transpose(2, 0, 1))
        lams = np.stack([np.asarray(P[4], np.float32).reshape(8, 128), np.asarray(Sd[4], np.float32).reshape(8, 128)])
        lams = np.ascontiguousarray(lams.transpose(2, 0, 1))
        return f(wgs), bgs, lams

    per_half = []
    for half in range(2):
        if half == 0:
            wgs, bgs, lams = gates(pf, pbk)
            d = {"cw": fm3(cwn), "lw5": fm3(lw5_nat), "wg": wgs, "bg": bgs, "lam": lams}
        else:
            wgs, bgs, lams = gates(pbk, pf)
            d = {"cw": fm3(cwn[::-1]), "lw5": fm3(lw5_rev), "wg": wgs, "bg": bgs, "lam": lams}
        per_half.append(d)

    in_maps = []
    pad2 = np.zeros((2, 1024), np.float32)
    for b in range(B):
        for half in range(2):
            xs = x[b] if half == 0 else x[b, ::-1]
            cs_ = ctx[b] if half == 0 else ctx[b, ::-1]
            m = dict(common)
            m.update(per_half[half])
            m["xp"] = np.ascontiguousarray(np.concatenate([pad2, xs, pad2], axis=0))
            m["ctxp"] = np.ascontiguousarray(np.concatenate([pad2, cs_, pad2], axis=0))
            m["cvec"] = np.ascontiguousarray(np.stack([_fm(c[b]), _fm(c_ctx)], axis=-1))
            in_maps.append(m)
    res = run_bass_kernel_spmd(nc, in_maps, core_ids=list(range(2 * B)))
    outp = np.empty((B, 2 * NOWN, 1024), np.float32)
    for b in range(B):
        outp[b, :NOWN] = res.results[2 * b]["out"]
        outp[b, NOWN:] = res.results[2 * b + 1]["out"][::-1]
    if DEBUG:
        kernel.last = res
    return outp
```

```python
from contextlib import ExitStack
import os
import numpy as np
import concourse.bass as bass
import concourse.mybir as mybir
from concourse.bass_utils import run_bass_kernel_spmd

F32 = mybir.dt.float32
BF16 = mybir.dt.bfloat16
AF = mybir.ActivationFunctionType
ALU = mybir.AluOpType
AX = mybir.AxisListType
EPS = 1e-6
NB = 512
NOWN = 4096
DEBUG = bool(int(os.environ.get("MK_DEBUG", "0")))


class _Rec:
    def __init__(self):
        self.calls = []

    def __getattr__(self, name):
        def f(*args, **kw):
            self.calls.append((name, args, kw))
            return self
        return f


_TBL = {"Exp": "exp", "Tanh": None, "Identity": None, "Copy": None, "Square": None, "Sqrt": "sqrt", "Silu": "silu",
        "Gelu_apprx_tanh": "gelu", "Ln": "ln"}


def _fsize(ap):
    n = 1
    for d in ap.shape[1:]:
        n *= int(d)
    return n


class Sched:
    REORDER = True
    WINDOW = 600

    def __init__(self, nc, es):
        self.nc = nc
        self.es = es
        self.E = dict(pe=nc.tensor, act=nc.scalar, dve=nc.vector, pool=nc.gpsimd, sp=nc.sync)
        self.sem = {e: es.enter_context(nc.semaphore("c_" + e)) for e in self.E}
        self.cnt = {e: 0 for e in self.E}
        self.seen = {e: {} for e in self.E}
        self.lastw = {}
        self.readers = {}
        self.dsem = {}
        self.dcnt = {}
        self.ops = []

    def op(self, e, fn, reads=(), writes=()):
        r = _Rec()
        fn(r)
        self._add(e, "op", r.calls, tuple(reads), tuple(writes), None)

    def group(self, e, fns, reads=(), writes=()):
        r = _Rec()
        for f in fns:
            f(r)
        self._add(e, "op", r.calls, tuple(reads), tuple(writes), None)

    def dma(self, q, out, in_, reads=(), writes=(), key=None):
        self._add(q, "dma", [("dma_start", (), dict(out=out, in_=in_))], tuple(reads), tuple(writes), key)

    def _add(self, e, kind, calls, reads, writes, key):
        dur = 0.0
        tbl = None
        if kind == "dma":
            nb = 128 * _fsize(calls[0][2]["out"]) * 4
            dur = 1000.0 if e == "pool" else 150.0
            lat = 2000.0 + nb / 300.0
        else:
            lat = 0.0
            for (name, args, kw) in calls:
                if e == "pe":
                    src = kw.get("rhs", kw.get("in_"))
                    dur += 25.0 + 0.5 * max(_fsize(src), 64)
                else:
                    oap = kw.get("out", kw.get("ap", args[0] if args else None))
                    n = _fsize(oap)
                    if e == "act":
                        dur += 250.0 + 0.73 * n
                        fnm = kw.get("func")
                        tbl = _TBL.get(getattr(fnm, "name", str(fnm)), None) if fnm is not None else None
                    elif e == "dve":
                        dur += 160.0 + 1.04 * n
                    else:
                        dur += 300.0 + 3.1 * n
        self.ops.append(dict(e=e, kind=kind, calls=calls, reads=reads, writes=writes, key=key, dur=dur, lat=lat, tbl=tbl))

    def flush(self):
        ops = self.ops
        self.ops = []
        n = len(ops)
        if n == 0:
            return
        lastw, readers = {}, {}
        preds = [None] * n
        succs = [[] for _ in range(n)]
        for i, o in enumerate(ops):
            p = set()
            for k in o["reads"]:
                if k in lastw:
                    p.add(lastw[k])
            for k in o["writes"]:
                if k in lastw:
                    p.add(lastw[k])
                p.update(readers.get(k, ()))
            p.discard(i)
            preds[i] = p
            for j in p:
                succs[j].append(i)
            for k in o["reads"]:
                readers.setdefault(k, []).append(i)
            for k in o["writes"]:
                lastw[k] = i
                readers[k] = []
        if not self.REORDER:
            order = range(n)
        else:
            indeg = [len(p) for p in preds]
            ready = [i for i in range(n) if indeg[i] == 0]
            finish = [0.0] * n
            efree = {e: 0.0 for e in self.E}
            etbl = [None]
            done = [False] * n
            lo = 0
            order = []
            while len(order) < n:
                while lo < n and done[lo]:
                    lo += 1
                best, bkey = None, None
                for i in ready:
                    if i > lo + self.WINDOW:
                        continue
                    o = ops[i]
                    st = efree[o["e"]]
                    for j in preds[i]:
                        f = finish[j] + (0.0 if ops[j]["e"] == o["e"] else 120.0)
                        if f > st:
                            st = f
                    if o["e"] == "act" and o["tbl"] is not None and o["tbl"] != etbl[0]:
                        st += 1300.0
                    kk = (st, i)
                    if bkey is None or kk < bkey:
                        best, bkey = i, kk
                i = best
                o = ops[i]
                st = bkey[0]
                if os.environ.get("MK_TL") and n > 3000 and len(ops) == int(os.environ.get("MK_TL")):
                    lim = None
                    for j in preds[i]:
                        f = finish[j]
                        if lim is None or f > lim[0]:
                            lim = (f, j)
                    gap = st - efree[o["e"]]
                    if o["e"] == "pe" and gap > 300:
                        print("PE gap %.1fus at t=%.1fus op#%d writes=%s waits for %s op#%d writes=%s" % (
                            gap / 1e3, st / 1e3, i, o["writes"][:2], ops[lim[1]]["e"], lim[1], ops[lim[1]]["writes"][:2]))
                if o["e"] == "act" and o["tbl"] is not None:
                    etbl[0] = o["tbl"]
                efree[o["e"]] = st + o["dur"]
                finish[i] = st + o["dur"] + o["lat"]
                done[i] = True
                ready.remove(i)
                order.append(i)
                for j in succs[i]:
                    indeg[j] -= 1
                    if indeg[j] == 0:
                        ready.append(j)
        if self.REORDER and os.environ.get("MK_STATS"):
            busy = {e: 0.0 for e in self.E}
            for o in ops:
                busy[o["e"]] += o["dur"]
            print("phase: n=%d est_makespan=%.0fus busy(us): %s" % (n, max(finish) / 1e3, {e: int(v / 1e3) for e, v in busy.items()}))
        for i in order:
            self._emit(ops[i])

    def _wait(self, e, tok, same_ok=False):
        if tok is None:
            return
        name, sem, val, src = tok
        if same_ok and src == e:
            return
        d = self.seen[e]
        if d.get(name, 0) >= val:
            return
        self.E[e].wait_ge(sem, val)
        d[name] = val

    def _emit(self, o):
        e, reads, writes = o["e"], o["reads"], o["writes"]
        for k in reads:
            self._wait(e, self.lastw.get(k))
        for k in writes:
            self._wait(e, self.lastw.get(k), same_ok=True)
            for t in self.readers.get(k, {}).values():
                self._wait(e, t, same_ok=True)
        ins = None
        for (name, args, kw) in o["calls"]:
            ins = getattr(self.E[e], name)(*args, **kw)
        if o["kind"] == "dma":
            key = o["key"]
            if key not in self.dsem:
                self.dsem[key] = self.es.enter_context(self.nc.semaphore("d_" + key))
                self.dcnt[key] = 0
            self.dcnt[key] += 16
            ins.then_inc(self.dsem[key], 16)
            tok = ("d_" + key, self.dsem[key], self.dcnt[key], "dma")
        else:
            self.cnt[e] += 1
            ins.then_inc(self.sem[e], 1)
            tok = ("c_" + e, self.sem[e], self.cnt[e], e)
        for k in reads:
            self.readers.setdefault(k, {})[tok[0]] = tok
        for k in writes:
            self.lastw[k] = tok
            self.readers[k] = {}

    def barrier(self):
        self.flush()
        for e in self.E:
            for e2 in self.E:
                if self.cnt[e2] > 0:
                    self._wait(e, ("c_" + e2, self.sem[e2], self.cnt[e2], e2))
            for k, sem in self.dsem.items():
                self._wait(e, ("d_" + k, sem, self.dcnt[k], "dma"))


def build_program():
    nc = bass.Bass("TRN2", target_bir_lowering=False)

    def din(name, shape):
        return nc.dram_tensor(name, list(shape), F32, kind="ExternalInput").ap()

    xp = din("xp", [8196, 1024])
    ctxp = din("ctxp", [260, 1024])
    cvec = din("cvec", [128, 8, 2])
    w_ada = din("w_ada", [1024, 6144])
    b_ada_fm = din("b_ada_fm", [128, 48])
    b_ada_gt = din("b_ada_gt", [128, 2, 1024])
    w_in = din("w_in", [1024, 6144])
    b_in_fm = din("b_in_fm", [128, 48])
    cw = din("cw", [128, 8, 31])
    cb = din("cb", [128, 8])
    lng = din("lng", [128, 8])
    lnb = din("lnb", [128, 8])
    w_pa = din("w_pa", [1024, 1024])
    w_pb = din("w_pb", [1024, 1024])
    w_o = din("w_o", [1024, 1024])
    lw5 = din("lw5", [128, 8, 5])
    lb = din("lb", [128, 8])
    wg = din("wg", [4, 8, 128, 128])
    bg = din("bg", [128, 4, 8])
    lam = din("lam", [128, 2, 8])
    gmix = din("gmix", [128, 8])
    gffn = din("gffn", [128, 8])
    gfin = din("gfin", [128, 1024])
    w_rt = din("w_rt", [1024, 20])
    b_rt = din("b_rt", [128, 20])
    w_gate = din("w_gate", [16, 1024, 512])
    w_up = din("w_up", [16, 1024, 512])
    w_down = din("w_down", [16, 512, 1024])
    ident = din("ident", [128, 128])
    out = nc.dram_tensor("out", [NOWN, 1024], F32, kind="ExternalOutput").ap()
    if DEBUG:
        hs_scr = nc.dram_tensor("hs_scr", [8, 128, 8, NB], BF16, kind="ExternalOutput").ap()
        x1_scr = nc.dram_tensor("x1_scr", [NOWN, 1024], F32, kind="ExternalOutput").ap()
    else:
        hs_scr = nc.dram_tensor("hs_scr", [8, 128, 8, NB], BF16, kind="Internal").ap()
        x1_scr = nc.dram_tensor("x1_scr", [NOWN, 1024], F32, kind="Internal").ap()
    gt2_scr = nc.dram_tensor("gt2_scr", [128, 1024], F32, kind="Internal").ap()

    with ExitStack() as es:
        S = Sched(nc, es)

        def sb(name, shape, dt=F32, stack=es):
            return stack.enter_context(nc.sbuf_tensor(name, list(shape), dt))

        psA = es.enter_context(nc.psum_tensor("psA", [128, 2048], F32))
        psB = es.enter_context(nc.psum_tensor("psB", [128, 2048], F32))

        def bank(i):
            t = psA if i < 4 else psB
            return t[:, 512 * (i % 4):512 * (i % 4) + 512]

        def pk(i):
            return "ps%d" % i

        tpv = psA[:, :].bitcast(BF16).rearrange("p (c t) -> p c t", t=512)
        TPK = ("ps0", "ps1", "ps2", "ps3")

        identb = sb("identb", [128, 128], BF16)
        ones_m = sb("ones_m", [128, 128], BF16)
        ones1 = sb("ones1", [128, 128], BF16)
        b_in_sb = sb("b_in_sb", [128, 48])
        hb_in = sb("hb_in", [128, 48])
        cw_sb = sb("cw_sb", [128, 8, 31])
        cb_sb = sb("cb_sb", [128, 8])
        lng_sb = sb("lng_sb", [128, 8])
        lnb_sb = sb("lnb_sb", [128, 8])
        lw5_sb = sb("lw5_sb", [128, 8, 5])
        lb_sb = sb("lb_sb", [128, 8])
        bg_sb = sb("bg_sb", [128, 4, 8])
        hbg = sb("hbg", [128, 4, 8])
        lam_sb = sb("lam_sb", [128, 2, 8])
        gmix_sb = sb("gmix_sb", [128, 8])
        gffn_sb = sb("gffn_sb", [128, 8])
        b_rt_sb = sb("b_rt_sb", [128, 20])
        b_ada_fm_sb = sb("b_ada_fm_sb", [128, 48])
        cvec_sb = sb("cvec_sb", [128, 8, 2])
        mods = sb("mods", [128, 48, 2])
        s1 = sb("s1", [128, 8])
        s1c = sb("s1c", [128, 8])
        s2 = sb("s2", [128, 8])
        gt1h = sb("gt1h", [128, 1024])
        cl = sb("cl", [128, 2, 8])
        hcl = sb("hcl", [128, 2, 8])
        state = sb("state", [128, 2, 8])
        ss = sb("ss", [128, 8])
        rs = sb("rs", [128, 8])
        w_rt_b = sb("w_rt_b", [128, 8, 20], BF16)
        qtr = sb("qtr", [128, 4], F32)

        def pload(t, src):
            S.dma("sp", t, src, writes=("params",), key="params")

        pload(b_in_sb[:], b_in_fm)
        pload(cw_sb[:], cw)
        pload(cb_sb[:], cb)
        pload(lng_sb[:], lng)
        pload(lnb_sb[:], lnb)
        pload(lw5_sb[:], lw5)
        pload(lb_sb[:], lb)
        pload(bg_sb[:], bg)
        pload(lam_sb[:], lam)
        pload(gmix_sb[:], gmix)
        pload(gffn_sb[:], gffn)
        pload(b_rt_sb[:], b_rt)
        pload(b_ada_fm_sb[:], b_ada_fm)
        pload(cvec_sb[:], cvec)
        S.dma("pool", identb[:], ident, writes=("identb",), key="identb")
        S.dma("pool", w_rt_b[:], w_rt.rearrange("(k p) n -> p k n", p=128), writes=("w_rt_b",), key="w_rt_b")
        S.op("pool", lambda e: e.memset(ones_m[:], 1.0 / 1024.0), writes=("ones_m",))
        S.op("pool", lambda e: e.memset(ones1[:], 1.0), writes=("ones1",))
        S.op("pool", lambda e: e.memset(qtr[:, 0:1], 0.25), writes=("qtr",))
        S.op("pool", lambda e: e.memset(qtr[:, 1:2], 1024.0 * EPS), writes=("qtr",))
        S.op("pool", lambda e: e.memset(qtr[:, 2:3], EPS), writes=("qtr",))
        S.op("pool", lambda e: e.memset(state[:], 0.0), writes=("state",))
        S.op("pool", lambda e: e.memset(ss[:], 0.0), writes=("ss",))

        with ExitStack() as p0:
            cs = sb("cs", [128, 8, 2], BF16, p0)
            cs_rep = sb("cs_rep", [128, 8, 128], BF16, p0)
            b_ada_gt_sb = sb("b_ada_gt_sb", [128, 2, 1024], F32, p0)
            wa = [sb("wa%d" % i, [128, 8, 512], BF16, p0) for i in range(3)]
            e_t = sb("e_t", [128, 16], F32, p0)
            t_t = sb("t_t", [128, 16], F32, p0)
            l_t = sb("l_t", [128, 16], F32, p0)
            m_t = sb("m_t", [128, 16], F32, p0)
            pload(b_ada_gt_sb[:], b_ada_gt)
            gt2b = sb("gt2b0", [128, 1024], F32, p0)

            S.op("act", lambda e: e.activation(out=cs[:], in_=cvec_sb[:], func=AF.Silu), reads=("params",), writes=("cs",))
            S.op("dve", lambda e: e.tensor_copy(out=cs_rep[:], in_=cs[:, :, 0:1].to_broadcast([128, 8, 128])),
                 reads=("cs",), writes=("cs_rep",))
            psm = bank(0)[:, 0:96].rearrange("p (j t) -> p j t", t=2)
            for q in range(12):
                s = q % 3
                S.dma("pool", wa[s][:], w_ada[:, 512 * q:512 * q + 512].rearrange("(k p) n -> p k n", p=128),
                      writes=("wa%d" % s,), key="wa%d" % s)
                fns = []
                for jj in range(4):
                    for k in range(8):
                        fns.append(lambda e, jj=jj, k=k, s=s, q=q: e.matmul(
                            psm[:, 4 * q + jj, :], lhsT=wa[s][:, k, 128 * jj:128 * jj + 128], rhs=cs[:, k, :],
                            start=(k == 0), stop=(k == 7)))
                S.group("pe", fns, reads=("wa%d" % s, "cs"), writes=("ps0",))
                if q in (4, 5, 10, 11):
                    bk = 1 + (q % 2)
                    S.group("pe", [lambda e, k=k, s=s, bk=bk: e.matmul(bank(bk), lhsT=cs_rep[:, k, :], rhs=wa[s][:, k, :],
                                                                      start=(k == 0), stop=(k == 7)) for k in range(8)],
                            reads=("wa%d" % s, "cs_rep"), writes=(pk(bk),))
                    dst = gt1h if q < 6 else gt2b
                    gi = 0 if q < 6 else 1
                    cols = slice(512 * (q % 2), 512 * (q % 2) + 512)
                    S.op("dve", lambda e, dst=dst, gi=gi, cols=cols, bk=bk: e.tensor_tensor(
                        out=dst[:, cols], in0=bank(bk), in1=b_ada_gt_sb[:, gi, cols], op=ALU.add),
                        reads=(pk(bk), "params"), writes=("gt",))
            S.op("dve", lambda e: e.tensor_scalar_mul(out=gt1h[:], in0=gt1h[:], scalar1=0.5), reads=("gt",), writes=("gt",))
            S.dma("sp", gt2_scr, gt2b[:], reads=("gt",), writes=("gt2_scr",), key="gt2_scr")
            S.op("dve", lambda e: e.tensor_tensor(out=mods[:], in0=psm, in1=b_ada_fm_sb[:].unsqueeze(2).to_broadcast([128, 48, 2]),
                                                  op=ALU.add), reads=("ps0", "params"), writes=("mods",))
            for (dst, col, j0, gsb) in ((s1, 0, 8, gmix_sb), (s1c, 1, 8, gmix_sb), (s2, 0, 32, gffn_sb)):
                S.op("dve", lambda e, dst=dst, col=col, j0=j0, gsb=gsb: e.scalar_tensor_tensor(
                    out=dst[:], in0=mods[:, j0:j0 + 8, col], scalar=1.0, in1=gsb[:], op0=ALU.add, op1=ALU.mult),
                    reads=("mods", "params"), writes=("sc",))
                S.op("dve", lambda e, dst=dst: e.tensor_scalar_mul(out=dst[:], in0=dst[:], scalar1=32.0),
                     reads=("sc",), writes=("sc",))
            S.op("dve", lambda e: e.tensor_scalar_mul(out=hb_in[:], in0=b_in_sb[:], scalar1=0.5), reads=("params",), writes=("hb_in",))
            S.op("dve", lambda e: e.tensor_scalar_mul(out=hbg[:], in0=bg_sb[:], scalar1=0.5), reads=("params",), writes=("hbg",))
            lamf = lam_sb[:].rearrange("p a b -> p (a b)")
            S.op("act", lambda e: e.activation(out=e_t[:], in_=lamf, func=AF.Exp, scale=-1.0), reads=("params",), writes=("e_t",))
            S.op("dve", lambda e: e.tensor_scalar(out=t_t[:], in0=e_t[:], scalar1=-0.25, scalar2=1.0 / 3.0, op0=ALU.mult, op1=ALU.add),
                 reads=("e_t",), writes=("t_t",))
            S.op("dve", lambda e: e.tensor_tensor(out=t_t[:], in0=t_t[:], in1=e_t[:], op=ALU.mult), reads=("t_t", "e_t"), writes=("t_t",))
            S.op("dve", lambda e: e.tensor_scalar_add(out=t_t[:], in0=t_t[:], scalar1=-0.5), reads=("t_t",), writes=("t_t",))
            S.op("dve", lambda e: e.tensor_tensor(out=t_t[:], in0=t_t[:], in1=e_t[:], op=ALU.mult), reads=("t_t", "e_t"), writes=("t_t",))
            S.op("dve", lambda e: e.tensor_scalar_add(out=t_t[:], in0=t_t[:], scalar1=1.0), reads=("t_t",), writes=("t_t",))
            S.op("dve", lambda e: e.tensor_tensor(out=t_t[:], in0=t_t[:], in1=e_t[:], op=ALU.mult), reads=("t_t", "e_t"), writes=("t_t",))
            S.op("dve", lambda e: e.tensor_scalar_add(out=l_t[:], in0=e_t[:], scalar1=1.0), reads=("e_t",), writes=("l_t",))
            S.op("act", lambda e: e.activation(out=l_t[:], in_=l_t[:], func=AF.Ln), reads=("l_t",), writes=("l_t",))
            S.op("dve", lambda e: e.tensor_single_scalar(out=m_t[:], in_=e_t[:], scalar=0.1, op=ALU.is_lt), reads=("e_t",), writes=("m_t",))
            S.op("dve", lambda e: e.tensor_tensor(out=t_t[:], in0=t_t[:], in1=l_t[:], op=ALU.subtract), reads=("t_t", "l_t"), writes=("t_t",))
            S.op("dve", lambda e: e.tensor_tensor(out=t_t[:], in0=t_t[:], in1=m_t[:], op=ALU.mult), reads=("t_t", "m_t"), writes=("t_t",))
            S.op("dve", lambda e: e.tensor_tensor(out=t_t[:], in0=t_t[:], in1=l_t[:], op=ALU.add), reads=("t_t", "l_t"), writes=("t_t",))
            clf = cl[:].rearrange("p a b -> p (a b)")
            hclf = hcl[:].rearrange("p a b -> p (a b)")
            S.op("dve", lambda e: e.tensor_scalar_mul(out=clf, in0=t_t[:], scalar1=-8.0), reads=("t_t",), writes=("cl",))
            S.op("dve", lambda e: e.tensor_scalar_mul(out=hclf, in0=t_t[:], scalar1=-4.0), reads=("t_t",), writes=("cl",))
            S.barrier()

        mixer = ExitStack()
        wxr = sb("wxr", [128, 8, 1024], BF16, mixer)
        wgb = sb("wgb", [128, 4, 8, 128], BF16, mixer)
        dg5 = sb("dg5", [128, 8, 5, 128], BF16, mixer)
        S.dma("pool", wxr[:], w_in[:, 3072:4096].rearrange("(k p) n -> p k n", p=128), writes=("wxr",), key="wxr")
        S.dma("pool", wgb[:], wg.rearrange("g h p n -> p g h n"), writes=("wgb",), key="wgb")
        for c in range(8):
            S.op("dve", lambda e, c=c: e.tensor_tensor(
                out=dg5[:, c, :, :], in0=identb[:].unsqueeze(1).to_broadcast([128, 5, 128]),
                in1=lw5_sb[:, c, :].unsqueeze(2).to_broadcast([128, 5, 128]), op=ALU.mult),
                reads=("identb", "params"), writes=("dg5",))

        xt = sb("xt", [128, 4, 1024], F32, mixer)
        xh = sb("xh", [4, 1024], F32, mixer)
        xn = sb("xn", [128, 4, 1024], BF16, mixer)
        xnh = sb("xnh", [4, 1024], BF16, mixer)
        hxTs = [sb("hxT%d" % i, [128, 8, NB + 4], BF16, mixer) for i in range(2)]
        hxT, hxk, hxhk = hxTs[0], "hxT0", "hxTh0"
        hbc = [0]
        xrp = [sb("xrp%d" % i, [128, NB + 4], BF16, mixer) for i in range(2)]
        xcb = [sb("xcb%d" % i, [128, NB], BF16, mixer) for i in range(2)]
        tr = sb("tr", [128, NB], F32, mixer)
        ti = sb("ti", [128, NB], F32, mixer)
        a4 = sb("a4", [128, 4, NB], F32, mixer)
        s4 = sb("s4", [128, 4, NB], F32, mixer)
        t4 = sb("t4", [128, 4, NB], BF16, mixer)
        tmp1 = sb("tmp1", [128, NB], F32, mixer)
        bb_t = sb("bb_t", [128, NB], F32, mixer)
        hf = sb("hf", [128, NB], F32, mixer)
        hsb = sb("hsb", [128, 8, NB], BF16, mixer)

        tph = bank(4).bitcast(BF16)[:, 0:32].rearrange("p (c t) -> p c t", t=4)

        def prep(xsrc, r0, N, sc, bcol, keep_key):
            nt = N // 128
            S.dma("sp", xt[:, 0:nt, :], xsrc[r0:r0 + N, :].rearrange("(j p) d -> p j d", p=128), writes=(keep_key,), key="xt")
            S.dma("sp", xh[0:2, :], xsrc[r0 - 2:r0, :], writes=("xh",), key="xh")
            S.dma("sp", xh[2:4, :], xsrc[r0 + N:r0 + N + 2, :], writes=("xh",), key="xh")
            S.op("pool", lambda e: e.memset(ss[:], 0.0), writes=("ss",))
            for j in range(nt):
                S.op("act", lambda e, j=j: e.activation(out=xn[:, j, :], in_=xt[:, j, :], func=AF.Square, accum_out=ss[:, j:j + 1]),
                     reads=(keep_key,), writes=("xn", "ss"))
            S.op("act", lambda e: e.activation(out=xnh[:], in_=xh[:], func=AF.Square, accum_out=ss[0:4, 4:5]),
                 reads=("xh",), writes=("xnh", "ss"))
            S.op("act", lambda e: e.activation(out=rs[:, 0:5], in_=ss[:, 0:5], func=AF.Sqrt, bias=qtr[:, 1:2]), reads=("ss", "qtr"), writes=("rs",))
            S.op("dve", lambda e: e.reciprocal(out=rs[:, 0:5], in_=rs[:, 0:5]), reads=("rs",), writes=("rs",))
            for j in range(nt):
                S.op("dve", lambda e, j=j: e.tensor_scalar_mul(out=xn[:, j, :], in0=xt[:, j, :], scalar1=rs[:, j:j + 1]),
                     reads=(keep_key, "rs"), writes=("xn",))
            S.op("dve", lambda e: e.tensor_scalar_mul(out=xnh[:], in0=xh[:], scalar1=rs[0:4, 4:5]),
                 reads=("xh", "rs"), writes=("xnh",))
            for j in range(nt):
                S.group("pe", [lambda e, j=j, c=c: e.transpose(out=tpv[:, c, 128 * j:128 * j + 128],
                                                               in_=xn[:, j, 128 * c:128 * c + 128], identity=identb[:])
                               for c in range(8)], reads=("xn", "identb"), writes=TPK)
            S.group("pe", [lambda e, c=c: e.transpose(out=tph[:, c, :], in_=xnh[:, 128 * c:128 * c + 128], identity=identb[0:4, 0:4])
                           for c in range(8)], reads=("xnh", "identb"), writes=("ps4",))
            for c in range(8):
                S.op("act", lambda e, c=c: e.activation(out=hxT[:, c, 0:N], in_=tpv[:, c, 0:N], func=AF.Identity,
                                                        scale=sc[:, c:c + 1], bias=mods[:, c, bcol:bcol + 1]),
                     reads=TPK + ("sc", "mods"), writes=(hxk,))
            S.op("dve", lambda e: e.tensor_tensor(out=hxT[:, :, N:N + 4], in0=tph, in1=sc[:].unsqueeze(2).to_broadcast([128, 8, 4]),
                                                  op=ALU.mult), reads=("ps4", "sc"), writes=(hxhk,))
            S.op("dve", lambda e: e.tensor_tensor(out=hxT[:, :, N:N + 4], in0=hxT[:, :, N:N + 4],
                                                  in1=mods[:, 0:8, bcol:bcol + 1].to_broadcast([128, 8, 4]), op=ALU.add),
                 reads=(hxhk, "mods"), writes=(hxhk,))

        def rglru_block(N, d, reverse, has_lo, has_hi, consumer=None):
            def st1(c):
                par = c % 2
                bxr = bank(par)[:, 0:N]
                bxh = bank(2 + par)[:, 0:4]
                S.group("pe", [lambda e, k=k: e.matmul(bxr, lhsT=wxr[:, k, 128 * c:128 * c + 128], rhs=hxT[:, k, 0:N],
                                                       start=(k == 0), stop=(k == 7)) for k in range(8)],
                        reads=(hxk, "wxr"), writes=(pk(par),))
                S.group("pe", [lambda e, k=k: e.matmul(bxh, lhsT=wxr[:, k, 128 * c:128 * c + 128], rhs=hxT[:, k, N:N + 4],
                                                       start=(k == 0), stop=(k == 7)) for k in range(8)],
                        reads=(hxhk, "wxr"), writes=(pk(2 + par),))
                xk = "xrp%d" % par
                bia = b_in_sb[:, 24 + c:25 + c]
                S.op("act", lambda e: e.activation(out=xrp[par][:, 2:2 + N], in_=bxr, func=AF.Identity, bias=bia),
                     reads=(pk(par), "params"), writes=(xk,))
                if has_lo:
                    S.op("dve", lambda e: e.tensor_scalar_add(out=xrp[par][:, 0:2], in0=bxh[:, 0:2], scalar1=bia),
                         reads=(pk(2 + par), "params"), writes=(xk,))
                else:
                    S.op("dve", lambda e: e.memset(xrp[par][:, 0:2], 0.0), writes=(xk,))
                if has_hi:
                    S.op("dve", lambda e: e.tensor_scalar_add(out=xrp[par][:, 2 + N:4 + N], in0=bxh[:, 2:4], scalar1=bia),
                         reads=(pk(2 + par), "params"), writes=(xk,))
                else:
                    S.op("dve", lambda e: e.memset(xrp[par][:, 2 + N:4 + N], 0.0), writes=(xk,))

            def st2(c):
                par = c % 2
                xk = "xrp%d" % par
                bcv = bank(4 + par)[:, 0:N]
                S.group("pe", [lambda e, j=j: e.matmul(bcv, lhsT=dg5[:, c, j, :], rhs=xrp[par][:, j:j + N],
                                                       start=(j == 0), stop=(j == 4)) for j in range(5)],
                        reads=(xk, "dg5"), writes=(pk(4 + par),))
                S.op("dve", lambda e: e.tensor_scalar_add(out=xcb[par][:, 0:N], in0=bcv, scalar1=lb_sb[:, c:c + 1]),
                     reads=(pk(4 + par), "params"), writes=("xcb%d" % par,))

            def st3(c):
                par = c % 2
                q = c % 4
                ck = "xcb%d" % par
                br_ = bank(6)[:, 0:N]
                bi_ = bank(7)[:, 0:N]
                S.group("pe", [lambda e: e.matmul(br_, lhsT=wgb[:, 2 * d, c, :], rhs=xcb[par][:, 0:N], start=True, stop=True)],
                        reads=(ck, "wgb"), writes=("ps6",))
                S.group("pe", [lambda e: e.matmul(bi_, lhsT=wgb[:, 2 * d + 1, c, :], rhs=xcb[par][:, 0:N], start=True, stop=True)],
                        reads=(ck, "wgb"), writes=("ps7",))
                S.op("act", lambda e: e.activation(out=tr[:, 0:N], in_=br_, func=AF.Tanh, scale=0.5, bias=hbg[:, 2 * d, c:c + 1]),
                     reads=("ps6", "hbg"), writes=("tr",))
                S.op("act", lambda e: e.activation(out=ti[:, 0:N], in_=bi_, func=AF.Tanh, scale=0.5, bias=hbg[:, 2 * d + 1, c:c + 1]),
                     reads=("ps7", "hbg"), writes=("ti",))
                S.op("act", lambda e: e.activation(out=a4[:, q, 0:N], in_=tr[:, 0:N], func=AF.Exp, scale=hcl[:, d, c:c + 1],
                                                   bias=hcl[:, d, c:c + 1]), reads=("tr", "cl"), writes=("a4_%d" % q,))
                S.op("pool", lambda e: e.tensor_tensor(out=s4[:, q, 0:N], in0=a4[:, q, 0:N], in1=a4[:, q, 0:N], op=ALU.mult),
                     reads=("a4_%d" % q,), writes=("s4_%d" % q,))
                S.op("dve", lambda e: e.scalar_tensor_tensor(out=t4[:, q, 0:N], in0=ti[:, 0:N], scalar=1.0, in1=xcb[par][:, 0:N],
                                                             op0=ALU.add, op1=ALU.mult), reads=("ti", ck), writes=("t4_%d" % q,))

            def st4(c0):
                sk = tuple("s4_%d" % q for q in range(4))
                S.op("act", lambda e: e.activation(out=s4[:, :, 0:N], in_=s4[:, :, 0:N], func=AF.Sqrt, scale=-0.25, bias=qtr[:, 0:1]),
                     reads=sk + ("qtr",), writes=sk)
                for c in range(c0, c0 + 4):
                    q = c % 4
                    S.op("dve", lambda e, q=q: e.tensor_tensor(out=bb_t[:, 0:N], in0=s4[:, q, 0:N], in1=t4[:, q, 0:N], op=ALU.mult),
                         reads=("s4_%d" % q, "t4_%d" % q), writes=("bb_t",))
                    if reverse:
                        S.op("dve", lambda e, q=q, c=c: e.tensor_tensor_scan(
                            out=hf[:, 0:N][:, ::-1], data0=a4[:, q, 0:N][:, ::-1], data1=bb_t[:, 0:N][:, ::-1],
                            initial=state[:, d, c:c + 1], op0=ALU.mult, op1=ALU.add),
                            reads=("a4_%d" % q, "bb_t", "state"), writes=("hf",))
                        S.op("pool", lambda e, c=c: e.tensor_copy(out=state[:, d, c:c + 1], in_=hf[:, 0:1]),
                             reads=("hf",), writes=("state",))
                    else:
                        S.op("dve", lambda e, q=q, c=c: e.tensor_tensor_scan(
                            out=hf[:, 0:N], data0=a4[:, q, 0:N], data1=bb_t[:, 0:N], initial=state[:, d, c:c + 1],
                            op0=ALU.mult, op1=ALU.add), reads=("a4_%d" % q, "bb_t", "state"), writes=("hf",))
                        S.op("pool", lambda e, c=c: e.tensor_copy(out=state[:, d, c:c + 1], in_=hf[:, N - 1:N]),
                             reads=("hf",), writes=("state",))
                    if consumer is not None:
                        consumer(c)

            for s_ in range(10):
                if s_ < 8:
                    st1(s_)
                if 1 <= s_ <= 8:
                    st2(s_ - 1)
                if 2 <= s_ <= 9:
                    st3(s_ - 2)
                    if (s_ - 2) % 4 == 3:
                        st4(s_ - 2 - 3)

        hxT, hxk, hxhk = hxTs[0], "hxT0", "hxTh0"
        prep(ctxp, 2, 256, s1c, 1, "xt")
        hbc[0] = 1
        rglru_block(256, 0, False, False, False)
        rglru_block(256, 1, True, False, False)
        for blk in range(15, -1, -1):
            _i = hbc[0] % 2
            hbc[0] += 1
            hxT, hxk, hxhk = hxTs[_i], "hxT%d" % _i, "hxTh%d" % _i
            prep(xp, 2 + NB * blk, NB, s1, 0, "xt")
            def cons_a(c):
                S.op("dve", lambda e, c=c: e.tensor_copy(out=hsb[:, c, :], in_=hf[:]), reads=("hf",), writes=("hsb",))
            rglru_block(NB, 1, True, blk != 0, blk != 15, cons_a if blk < 8 else None)
            if blk < 8:
                S.dma("sp", hs_scr[blk], hsb[:], reads=("hsb",), writes=("hs_scr%d" % blk,), key="hs_scr")

        pb_ = ExitStack()
        cwh = cw_sb
        S.op("dve", lambda e: e.tensor_scalar_mul(out=cwh[:], in0=cw_sb[:], scalar1=0.5), reads=("params",), writes=("cwh",))
        lnr = sb("lnr", [128, NB], F32, pb_)
        lmr = sb("lmr", [128, NB], F32, pb_)
        wsl = [sb("wsl%d" % i, [128, 8, 512], BF16, pb_) for i in range(3)]
        dgc = [sb("dgc0", [128, 31, 128], BF16, pb_)] * 2
        tv, uu = tr, ti
        zb = [sb("zb0", [128, NB], BF16, pb_)] * 2
        zc = sb("zc", [128, 8, NB], BF16, pb_)
        aa = zc
        zsq = [sb("zsq0", [128, NB], BF16, pb_)] * 2
        A_t = sb("A_t", [128, 8, NB], BF16, pb_)
        gy = sb("gy", [128, 8, NB], BF16, pb_)
        mg = gy
        yb = sb("yb", [128, 8, NB], BF16, pb_)
        hs_in = hsb
        x1 = xt

        xres = [sb("xres%d" % i, [128, 512], F32, pb_) for i in range(3)]
        xrc = [0]
        wctr = [0]

        def wpiece(col0, src=None):
            src = w_in if src is None else src
            i = wctr[0] % 3
            wctr[0] += 1
            S.dma("pool", wsl[i][:], src[:, col0:col0 + 512].rearrange("(k p) n -> p k n", p=128),
                  writes=("wsl%d" % i,), key="wsl%d" % i)
            return wsl[i], "wsl%d" % i

        def inproj(dstbank, wt, wk, j4):
            S.group("pe", [lambda e, k=k: e.matmul(bank(dstbank), lhsT=wt[:, k, 128 * j4:128 * j4 + 128], rhs=hxT[:, k, 0:NB],
                                                   start=(k == 0), stop=(k == 7)) for k in range(8)],
                    reads=(hxk, wk), writes=(pk(dstbank),))

        for blk in range(8):
            _i = hbc[0] % 2
            hbc[0] += 1
            hxT, hxk, hxhk = hxTs[_i], "hxT%d" % _i, "hxTh%d" % _i
            prep(xp, 2 + NB * blk, NB, s1, 0, "xt")
            S.dma("sp", hs_in[:], hs_scr[blk], reads=("hs_scr%d" % blk,), writes=("hsb",), key="hs_in")
            for half in range(2):
                wu, wuk = wpiece(512 * half)
                wv, wvk = wpiece(1024 + 512 * half)
                for c4 in range(4):
                    c = 4 * half + c4
                    par = c % 2
                    inproj(par, wu, wuk, c4)
                    inproj(2 + par, wv, wvk, c4)
                    S.op("act", lambda e, c=c, par=par: e.activation(out=tv[:], in_=bank(2 + par), func=AF.Tanh, scale=0.5,
                                                                     bias=hb_in[:, 8 + c:9 + c]),
                         reads=(pk(2 + par), "hb_in"), writes=("tr",))
                    S.op("act", lambda e, c=c, par=par: e.activation(out=uu[:], in_=bank(par), func=AF.Identity,
                                                                     bias=b_in_sb[:, c:c + 1]),
                         reads=(pk(par), "params"), writes=("ti",))
                    zk = "zb0"
                    S.op("dve", lambda e, par=par: e.scalar_tensor_tensor(out=zb[par][:], in0=tv[:], scalar=1.0, in1=uu[:],
                                                                          op0=ALU.add, op1=ALU.mult),
                         reads=("tr", "ti"), writes=(zk,))
                    dk = "dgc0"
                    S.op("dve", lambda e, c=c, par=par: e.tensor_tensor(
                        out=dgc[par][:], in0=identb[:].unsqueeze(1).to_broadcast([128, 31, 128]),
                        in1=cwh[:, c, :].unsqueeze(2).to_broadcast([128, 31, 128]), op=ALU.mult),
                        reads=("identb", "cwh"), writes=(dk,))
                    zv = zb[par][:].rearrange("p (r t) -> p r t", t=64)
                    pcv = bank(4 + par).rearrange("p (r t) -> p r t", t=64)
                    fns = []
                    order = [15] + [k for k in range(31) if k != 15]
                    for idx, k in enumerate(order):
                        o = k - 15
                        t0, t1 = max(0, -o), 64 - max(0, o)
                        fns.append(lambda e, k=k, o=o, t0=t0, t1=t1, idx=idx, par=par, pcv=pcv, zv=zv: e.matmul(
                            pcv[:, :, t0:t1], lhsT=dgc[par][:, k, :], rhs=zv[:, :, t0 + o:t1 + o],
                            start=(idx == 0), stop=(idx == 30)))
                    S.group("pe", fns, reads=(zk, dk), writes=(pk(4 + par),))
                    S.op("act", lambda e, c=c, par=par: e.activation(out=zc[:, c, :], in_=bank(4 + par), func=AF.Identity,
                                                                     bias=cb_sb[:, c:c + 1]),
                         reads=(pk(4 + par), "params"), writes=("zc",))
                    qk = "zsq0"
                    S.op("act", lambda e, c=c, par=par: e.activation(out=zsq[par][:], in_=bank(4 + par), func=AF.Square,
                                                                     bias=cb_sb[:, c:c + 1]),
                         reads=(pk(4 + par), "params"), writes=(qk,))
                    S.group("pe", [lambda e, c=c: e.matmul(bank(6), lhsT=ones_m[:], rhs=zc[:, c, :], start=(c == 0), stop=(c == 7))],
                            reads=("zc", "ones_m"), writes=("ps6",))
                    S.group("pe", [lambda e, c=c, par=par: e.matmul(bank(7), lhsT=ones_m[:], rhs=zsq[par][:], start=(c == 0),
                                                                    stop=(c == 7))], reads=(qk, "ones_m"), writes=("ps7",))
            S.op("act", lambda e: e.activation(out=tv[:], in_=bank(6), func=AF.Copy), reads=("ps6",), writes=("tr",))
            S.op("dve", lambda e: e.tensor_tensor(out=uu[:], in0=tv[:], in1=tv[:], op=ALU.mult), reads=("tr",), writes=("ti",))
            S.op("dve", lambda e: e.tensor_tensor(out=lnr[:], in0=bank(7), in1=uu[:], op=ALU.subtract), reads=("ps7", "ti"),
                 writes=("lnr",))
            S.op("act", lambda e: e.activation(out=lnr[:], in_=lnr[:], func=AF.Sqrt, bias=qtr[:, 2:3]), reads=("lnr", "qtr"), writes=("lnr",))
            S.op("dve", lambda e: e.reciprocal(out=lnr[:], in_=lnr[:]), reads=("lnr",), writes=("lnr",))
            S.op("dve", lambda e: e.tensor_tensor(out=lmr[:], in0=tv[:], in1=lnr[:], op=ALU.mult), reads=("tr", "lnr"),
                 writes=("lmr",))
            for half in range(2):
                wy, wyk = wpiece(2048 + 512 * half)
                for c4 in range(4):
                    c = 4 * half + c4
                    par = c % 2
                    inproj(par, wy, wyk, c4)
                    S.op("act", lambda e, c=c, par=par: e.activation(out=gy[:, c, :], in_=bank(par), func=AF.Gelu_apprx_tanh,
                                                                     bias=b_in_sb[:, 16 + c:17 + c]),
                         reads=(pk(par), "params"), writes=("gy",))
            def cons_b(c):
                S.op("dve", lambda e, c=c: e.tensor_tensor(out=tmp1[:], in0=hf[:], in1=hs_in[:, c, :], op=ALU.add),
                     reads=("hf", "hsb"), writes=("tmp1",))
                S.op("dve", lambda e, c=c: e.tensor_tensor(out=yb[:, c, :], in0=tmp1[:], in1=gy[:, c, :], op=ALU.mult),
                     reads=("tmp1", "gy"), writes=("yb",))
            rglru_block(NB, 0, False, blk != 0, True, cons_b)
            for c in range(8):
                S.op("dve", lambda e, c=c: e.tensor_tensor(out=tv[:], in0=zc[:, c, :], in1=lnr[:], op=ALU.mult),
                     reads=("zc", "lnr"), writes=("tr",))
                S.op("dve", lambda e: e.tensor_tensor(out=uu[:], in0=tv[:], in1=lmr[:], op=ALU.subtract), reads=("tr", "lmr"),
                     writes=("ti",))
                S.op("act", lambda e, c=c: e.activation(out=aa[:, c, :], in_=uu[:], func=AF.Silu, scale=lng_sb[:, c:c + 1],
                                                        bias=lnb_sb[:, c:c + 1]), reads=("ti", "params"), writes=("zc",))
            for half in range(2):
                wga, wgak = wpiece(4096 + 512 * half)
                wpa, wpak = wpiece(512 * half, w_pa)
                for m4 in range(4):
                    m = 4 * half + m4
                    par = m % 2
                    S.group("pe", [lambda e, k=k, m4=m4, par=par, wpa=wpa: e.matmul(bank(par), lhsT=wpa[:, k, 128 * m4:128 * m4 + 128],
                                                                         rhs=aa[:, k, :], start=(k == 0), stop=(k == 7))
                                   for k in range(8)], reads=("zc", wpak), writes=(pk(par),))
                    inproj(2 + par, wga, wgak, m4)
                    S.op("act", lambda e, m=m, par=par: e.activation(out=tv[:], in_=bank(2 + par), func=AF.Tanh, scale=0.5,
                                                                     bias=hb_in[:, 32 + m:33 + m]),
                         reads=(pk(2 + par), "hb_in"), writes=("tr",))
                    S.op("dve", lambda e, m=m, par=par: e.scalar_tensor_tensor(out=A_t[:, m, :], in0=tv[:], scalar=1.0,
                                                                               in1=bank(par), op0=ALU.add, op1=ALU.mult),
                         reads=("tr", pk(par)), writes=("A_t",))
            for half in range(2):
                wgb_, wgbk = wpiece(5120 + 512 * half)
                wpb, wpbk = wpiece(512 * half, w_pb)
                for m4 in range(4):
                    m = 4 * half + m4
                    par = m % 2
                    S.group("pe", [lambda e, k=k, m4=m4, par=par, wpb=wpb: e.matmul(bank(par), lhsT=wpb[:, k, 128 * m4:128 * m4 + 128],
                                                                         rhs=yb[:, k, :], start=(k == 0), stop=(k == 7))
                                   for k in range(8)], reads=("yb", wpbk), writes=(pk(par),))
                    inproj(2 + par, wgb_, wgbk, m4)
                    S.op("act", lambda e, m=m, par=par: e.activation(out=tv[:], in_=bank(2 + par), func=AF.Tanh, scale=0.5,
                                                                     bias=hb_in[:, 40 + m:41 + m]),
                         reads=(pk(2 + par), "hb_in"), writes=("tr",))
                    S.op("dve", lambda e, par=par: e.scalar_tensor_tensor(out=uu[:], in0=tv[:], scalar=1.0, in1=bank(par),
                                                                          op0=ALU.add, op1=ALU.mult),
                         reads=("tr", pk(par)), writes=("ti",))
                    S.op("dve", lambda e, m=m: e.tensor_tensor(out=mg[:, m, :], in0=uu[:], in1=A_t[:, m, :], op=ALU.add),
                         reads=("ti", "A_t"), writes=("gy",))
            for hh in range(2):
                wo, wok = wpiece(512 * hh, w_o)
                for j in range(4):
                    bk = 4 + j
                    xi = xrc[0] % 3
                    xrc[0] += 1
                    xk_ = "xres%d" % xi
                    r0_ = 2 + NB * blk + 128 * j
                    S.dma("sp", xres[xi][:], xp[r0_:r0_ + 128, 512 * hh:512 * hh + 512], writes=(xk_,), key=xk_)
                    S.group("pe", [lambda e, k=k, j=j, bk=bk, wo=wo: e.matmul(bank(bk), lhsT=mg[:, k, 128 * j:128 * j + 128],
                                                                             rhs=wo[:, k, :], start=(k == 0), stop=(k == 7))
                                   for k in range(8)], reads=("gy", wok), writes=(pk(bk),))
                    S.op("dve", lambda e, hh=hh, bk=bk: e.tensor_tensor(out=tmp1[:], in0=bank(bk), in1=gt1h[:, 512 * hh:512 * hh + 512],
                                                                        op=ALU.mult), reads=(pk(bk), "gt"), writes=("tmp1",))
                    S.op("pool", lambda e, xi=xi: e.tensor_tensor(out=xres[xi][:], in0=xres[xi][:], in1=tmp1[:], op=ALU.add),
                         reads=("tmp1", xk_), writes=(xk_,))
                    S.dma("sp", x1_scr[NB * blk + 128 * j:NB * blk + 128 * j + 128, 512 * hh:512 * hh + 512], xres[xi][:],
                          reads=(xk_,), writes=("x1_scr%d" % blk,), key="x1_scr")
        S.barrier()
        pb_.close()
        mixer.close()

        pc = ExitStack()
        gf32 = sb("gf32", [128, 1024], F32, pc)
        gt2b = sb("gt2b", [128, 1024], F32, pc)
        S.dma("sp", gf32[:], gfin, writes=("gf32",), key="gf32")
        S.dma("sp", gt2b[:], gt2_scr, reads=("gt2_scr",), writes=("gt2b",), key="gt2b")
        S.op("dve", lambda e: e.tensor_scalar_mul(out=gf32[:], in0=gf32[:], scalar1=32.0), reads=("gf32",), writes=("gf32",))
        accs = [sb("acc%d" % i, [128, 4, 1024], F32, pc) for i in range(2)]
        hmTs = [sb("hmT%d" % i, [128, 8, NB], BF16, pc) for i in range(2)]
        xn2 = sb("xn2", [128, 4, 1024], BF16, pc)
        junkF = sb("junkF", [128, 1024], BF16, pc)
        cbc = sb("cbc", [128, 16, NB], BF16, pc)
        dgm = sb("dgm", [128, 16, 128], BF16, pc)
        actb = [sb("actb%d" % i, [128, NB], BF16, pc) for i in range(16)]
        wgu = [sb("wgu%d" % i, [128, 2, 8, 512], BF16, pc) for i in range(2)]
        NWD = 5
        wd = [sb("wd%d" % i, [128, 4, 1024], BF16, pc) for i in range(NWD)]
        sg = [sb("sg%d" % i, [128, NB], F32, pc) for i in range(2)]
        tt = [sb("tt%d" % i, [128, NB], BF16, pc) for i in range(2)]
        evt = [sb("evt%d" % i, [128, NB], F32, pc) for i in range(2)]
        ssA = sb("ssA", [128, 2, 4], F32, pc)
        rsA = sb("rsA", [128, 2, 4], F32, pc)
        ssF = sb("ssF", [128, 2, 4], F32, pc)
        rsF = sb("rsF", [128, 2, 4], F32, pc)
        L = sb("L", [128, 4, 20], F32, pc)
        gmax = sb("gmax", [128, 4, 1], F32, pc)
        oh = sb("oh", [128, 4, 4], F32, pc)
        eg = sb("eg", [128, 4, 4], F32, pc)
        pg = sb("pg", [128, 4, 1], F32, pc)
        tmp16 = sb("tmp16", [128, 4, 16], F32, pc)
        esel = sb("esel", [128, 4, 4], F32, pc)
        m1 = sb("m1", [128, 4, 1], F32, pc)
        m2 = sb("m2", [128, 4, 1], F32, pc)
        k1 = sb("k1", [128, 4, 4], F32, pc)
        k2 = sb("k2", [128, 4, 4], F32, pc)
        e2 = sb("e2", [128, 4, 4], F32, pc)
        w1 = sb("w1", [128, 4, 1], F32, pc)
        w2 = sb("w2", [128, 4, 1], F32, pc)
        wsel = sb("wsel", [128, 4, 4], F32, pc)
        comb = sb("comb", [128, 4, 16], F32, pc)
        ectr = [0]
        evc = [0]

        def bc(ap, shape):
            return ap.to_broadcast(shape)

        for blk in range(8):
            pb2 = blk % 2
            acc, hmT = accs[pb2], hmTs[pb2]
            ak, hk = "acc%d" % pb2, "hmT%d" % pb2
            sak, sfk = "ssA%d" % pb2, "ssF%d" % pb2
            S.dma("sp", acc[:], x1_scr[NB * blk:NB * blk + NB, :].rearrange("(j p) d -> p j d", p=128),
                  reads=("x1_scr%d" % blk,), writes=(ak,), key=ak)
            S.op("pool", lambda e, pb2=pb2: e.memset(ssA[:, pb2, :], 0.0), writes=(sak,))
            for j in range(4):
                S.op("act", lambda e, j=j, acc=acc, pb2=pb2: e.activation(out=xn2[:, j, :], in_=acc[:, j, :], func=AF.Square,
                                                                          accum_out=ssA[:, pb2, j:j + 1]),
                     reads=(ak,), writes=("xn2", sak))
            S.op("act", lambda e, pb2=pb2: e.activation(out=rsA[:, pb2, :], in_=ssA[:, pb2, :], func=AF.Sqrt, bias=qtr[:, 1:2]),
                 reads=(sak, "qtr"), writes=(sak + "r",))
            S.op("dve", lambda e, pb2=pb2: e.reciprocal(out=rsA[:, pb2, :], in_=rsA[:, pb2, :]), reads=(sak + "r",), writes=(sak + "r",))
            for j in range(4):
                S.op("dve", lambda e, j=j, acc=acc, pb2=pb2: e.tensor_scalar_mul(out=xn2[:, j, :], in0=acc[:, j, :],
                                                                                 scalar1=rsA[:, pb2, j:j + 1]),
                     reads=(ak, sak + "r"), writes=("xn2",))
            for j in range(4):
                S.group("pe", [lambda e, j=j, c=c: e.transpose(out=tpv[:, c, 128 * j:128 * j + 128],
                                                               in_=xn2[:, j, 128 * c:128 * c + 128], identity=identb[:])
                               for c in range(8)], reads=("xn2", "identb"), writes=TPK)
            for c in range(8):
                S.op("act", lambda e, c=c, hmT=hmT: e.activation(out=hmT[:, c, :], in_=tpv[:, c, :], func=AF.Identity,
                                                                 scale=s2[:, c:c + 1], bias=mods[:, 24 + c, 0:1]),
                     reads=TPK + ("sc", "mods"), writes=(hk,))
            for j in range(4):
                S.group("pe", [lambda e, k=k, j=j, hmT=hmT: e.matmul(bank(4)[:, 20 * j:20 * j + 20], lhsT=hmT[:, k, 128 * j:128 * j + 128],
                                                                     rhs=w_rt_b[:, k, :], start=(k == 0), stop=(k == 7))
                               for k in range(8)], reads=(hk, "w_rt_b"), writes=("ps4",))
            S.op("dve", lambda e: e.tensor_tensor(out=L[:], in0=bank(4)[:, 0:80].rearrange("p (j n) -> p j n", n=20),
                                                  in1=bc(b_rt_sb[:].unsqueeze(1), [128, 4, 20]), op=ALU.add),
                 reads=("ps4", "params"), writes=("L",))
            R = ("rt",)
            S.op("dve", lambda e: e.tensor_reduce(out=gmax[:], in_=L[:, :, 0:4], axis=AX.X, op=ALU.max), reads=("L",), writes=R)
            S.op("dve", lambda e: e.tensor_tensor(out=oh[:], in0=L[:, :, 0:4], in1=bc(gmax[:], [128, 4, 4]), op=ALU.is_equal),
                 reads=R + ("L",), writes=R)
            S.op("dve", lambda e: e.tensor_tensor(out=eg[:], in0=L[:, :, 0:4], in1=bc(gmax[:], [128, 4, 4]), op=ALU.subtract),
                 reads=R + ("L",), writes=R)
            S.op("act", lambda e: e.activation(out=eg[:], in_=eg[:], func=AF.Exp), reads=R, writes=R)
            S.op("dve", lambda e: e.tensor_reduce(out=pg[:], in_=eg[:], axis=AX.X, op=ALU.add), reads=R, writes=R)
            S.op("dve", lambda e: e.reciprocal(out=pg[:], in_=pg[:]), reads=R, writes=R)
            S.op("dve", lambda e: e.tensor_tensor(out=tmp16[:].rearrange("p j (g x) -> p j g x", x=4),
                                                  in0=L[:, :, 4:20].rearrange("p j (g x) -> p j g x", x=4),
                                                  in1=bc(oh[:].unsqueeze(3), [128, 4, 4, 4]), op=ALU.mult),
                 reads=R + ("L",), writes=R)
            S.op("dve", lambda e: e.tensor_reduce(out=esel[:].unsqueeze(3), in_=tmp16[:].rearrange("p j (g x) -> p j x g", x=4),
                                                  axis=AX.X, op=ALU.add), reads=R, writes=R)
            S.op("dve", lambda e: e.tensor_reduce(out=m1[:], in_=esel[:], axis=AX.X, op=ALU.max), reads=R, writes=R)
            S.op("dve", lambda e: e.tensor_tensor(out=k1[:], in0=esel[:], in1=bc(m1[:], [128, 4, 4]), op=ALU.is_equal),
                 reads=R, writes=R)
            S.op("dve", lambda e: e.scalar_tensor_tensor(out=e2[:], in0=k1[:], scalar=-1e30, in1=esel[:], op0=ALU.mult, op1=ALU.add),
                 reads=R, writes=R)
            S.op("dve", lambda e: e.tensor_reduce(out=m2[:], in_=e2[:], axis=AX.X, op=ALU.max), reads=R, writes=R)
            S.op("dve", lambda e: e.tensor_tensor(out=k2[:], in0=e2[:], in1=bc(m2[:], [128, 4, 4]), op=ALU.is_equal),
                 reads=R, writes=R)
            S.op("dve", lambda e: e.tensor_tensor(out=w2[:], in0=m2[:], in1=m1[:], op=ALU.subtract), reads=R, writes=R)
            S.op("act", lambda e: e.activation(out=w2[:], in_=w2[:], func=AF.Exp), reads=R, writes=R)
            S.op("dve", lambda e: e.tensor_scalar_add(out=w1[:], in0=w2[:], scalar1=1.0), reads=R, writes=R)
            S.op("dve", lambda e: e.reciprocal(out=w1[:], in_=w1[:]), reads=R, writes=R)
            S.op("dve", lambda e: e.tensor_tensor(out=w2[:], in0=w2[:], in1=w1[:], op=ALU.mult), reads=R, writes=R)
            S.op("dve", lambda e: e.tensor_tensor(out=w1[:], in0=w1[:], in1=pg[:], op=ALU.mult), reads=R, writes=R)
            S.op("dve", lambda e: e.tensor_tensor(out=w2[:], in0=w2[:], in1=pg[:], op=ALU.mult), reads=R, writes=R)
            S.op("dve", lambda e: e.tensor_tensor(out=wsel[:], in0=k1[:], in1=bc(w1[:], [128, 4, 4]), op=ALU.mult), reads=R, writes=R)
            S.op("dve", lambda e: e.tensor_tensor(out=k2[:], in0=k2[:], in1=bc(w2[:], [128, 4, 4]), op=ALU.mult), reads=R, writes=R)
            S.op("dve", lambda e: e.tensor_tensor(out=wsel[:], in0=wsel[:], in1=k2[:], op=ALU.add), reads=R, writes=R)
            S.op("dve", lambda e: e.tensor_tensor(out=comb[:].rearrange("p j (g x) -> p j g x", x=4),
                                                  in0=bc(oh[:].unsqueeze(3), [128, 4, 4, 4]),
                                                  in1=bc(wsel[:].unsqueeze(2), [128, 4, 4, 4]), op=ALU.mult),
                 reads=R, writes=("comb",))
            for j in range(4):
                S.op("dve", lambda e, j=j: e.tensor_tensor(out=dgm[:], in0=bc(identb[:].unsqueeze(1), [128, 16, 128]),
                                                           in1=bc(comb[:, j, :].unsqueeze(2), [128, 16, 128]), op=ALU.mult),
                     reads=("identb", "comb"), writes=("dgm",))
                for q in range(4):
                    S.group("pe", [lambda e, q=q: e.matmul(bank(4 + q), lhsT=ones1[:], rhs=dgm[:, 4 * q:4 * q + 4, :],
                                                           start=True, stop=True)], reads=("dgm", "ones1"), writes=(pk(4 + q),))
                    S.op("act", lambda e, q=q, j=j: e.activation(out=cbc[:, 4 * q:4 * q + 4, 128 * j:128 * j + 128],
                                                                 in_=bank(4 + q).rearrange("p (x t) -> p x t", t=128), func=AF.Copy),
                         reads=(pk(4 + q),), writes=("cbc",))
            for g in range(4):
                for el in range(4):
                    ex = 4 * g + el
                    si = ectr[0] % 2
                    di = ectr[0] % NWD
                    ectr[0] += 1
                    gk, dk_ = "wgu%d" % si, "wd%d" % di
                    S.dma("pool", wgu[si][:, 0], w_gate[ex].rearrange("(k p) n -> p k n", p=128), writes=(gk,), key=gk)
                    S.dma("pool", wgu[si][:, 1], w_up[ex].rearrange("(k p) n -> p k n", p=128), writes=(gk,), key=gk)
                    S.dma("pool", wd[di][:], w_down[ex].rearrange("(k p) n -> p k n", p=128), writes=(dk_,), key=dk_)
                    for f in range(4):
                        u = 4 * el + f
                        pp = u % 2
                        S.group("pe", [lambda e, k=k, f=f, si=si, pp=pp, hmT=hmT: e.matmul(
                            bank(2 * pp), lhsT=wgu[si][:, 0, k, 128 * f:128 * f + 128], rhs=hmT[:, k, :],
                            start=(k == 0), stop=(k == 7)) for k in range(8)], reads=(hk, gk), writes=(pk(2 * pp),))
                        S.group("pe", [lambda e, k=k, f=f, si=si, pp=pp, hmT=hmT: e.matmul(
                            bank(2 * pp + 1), lhsT=wgu[si][:, 1, k, 128 * f:128 * f + 128], rhs=hmT[:, k, :],
                            start=(k == 0), stop=(k == 7)) for k in range(8)], reads=(hk, gk), writes=(pk(2 * pp + 1),))
                        S.op("act", lambda e, pp=pp: e.activation(out=sg[pp][:], in_=bank(2 * pp), func=AF.Silu),
                             reads=(pk(2 * pp),), writes=("sg%d" % pp,))
                        S.op("dve", lambda e, pp=pp: e.tensor_tensor(out=tt[pp][:], in0=bank(2 * pp + 1), in1=sg[pp][:], op=ALU.mult),
                             reads=(pk(2 * pp + 1), "sg%d" % pp), writes=("tt%d" % pp,))
                        S.op("dve", lambda e, pp=pp, u=u, ex=ex: e.tensor_tensor(out=actb[u][:], in0=tt[pp][:], in1=cbc[:, ex, :],
                                                                                op=ALU.mult),
                             reads=("tt%d" % pp, "cbc"), writes=("actb%d" % u,))
                dbase = ectr[0] - 4
                for tp_ in range(2):
                    fns = []
                    for u in range(16):
                        el, f = divmod(u, 4)
                        di = (dbase + el) % NWD
                        for jj in range(2):
                            j = 2 * tp_ + jj
                            for hh in range(2):
                                fns.append(lambda e, u=u, f=f, di=di, j=j, jj=jj, hh=hh: e.matmul(
                                    bank(4 + 2 * jj + hh), lhsT=actb[u][:, 128 * j:128 * j + 128],
                                    rhs=wd[di][:, f, 512 * hh:512 * hh + 512], start=(u == 0), stop=(u == 15)))
                    S.group("pe", fns, reads=tuple("actb%d" % u for u in range(16)) + tuple("wd%d" % ((dbase + el) % NWD) for el in range(4)),
                            writes=("ps4", "ps5", "ps6", "ps7"))
                    for jj in range(2):
                        j = 2 * tp_ + jj
                        for hh in range(2):
                            bk = 4 + 2 * jj + hh
                            ei = evc[0] % 2
                            evc[0] += 1
                            dst = acc[:, j, 512 * hh:512 * hh + 512]
                            S.op("dve", lambda e, bk=bk, hh=hh, ei=ei: e.tensor_tensor(out=evt[ei][:], in0=bank(bk),
                                                                                      in1=gt2b[:, 512 * hh:512 * hh + 512], op=ALU.mult),
                                 reads=(pk(bk), "gt2b"), writes=("evt%d" % ei,))
                            S.op("pool", lambda e, dst=dst, ei=ei: e.tensor_tensor(out=dst, in0=dst, in1=evt[ei][:], op=ALU.add),
                                 reads=("evt%d" % ei, ak), writes=(ak,))
            S.op("pool", lambda e, pb2=pb2: e.memset(ssF[:, pb2, :], 0.0), writes=(sfk,))
            for j in range(4):
                S.op("act", lambda e, j=j, acc=acc, pb2=pb2: e.activation(out=junkF[:], in_=acc[:, j, :], func=AF.Square,
                                                                          accum_out=ssF[:, pb2, j:j + 1]),
                     reads=(ak,), writes=("junkF", sfk))
            S.op("act", lambda e, pb2=pb2: e.activation(out=rsF[:, pb2, :], in_=ssF[:, pb2, :], func=AF.Sqrt, bias=qtr[:, 1:2]),
                 reads=(sfk, "qtr"), writes=(sfk + "r",))
            S.op("dve", lambda e, pb2=pb2: e.reciprocal(out=rsF[:, pb2, :], in_=rsF[:, pb2, :]), reads=(sfk + "r",), writes=(sfk + "r",))
            for j in range(4):
                S.op("dve", lambda e, j=j, acc=acc, pb2=pb2: e.scalar_tensor_tensor(out=acc[:, j, :], in0=acc[:, j, :],
                                                                                    scalar=rsF[:, pb2, j:j + 1], in1=gf32[:],
                                                                                    op0=ALU.mult, op1=ALU.mult),
                     reads=(ak, sfk + "r", "gf32"), writes=(ak,))
            S.dma("sp", out[NB * blk:NB * blk + NB, :].rearrange("(j p) d -> p j d", p=128), acc[:],
                  reads=(ak,), writes=("out%d" % blk,), key="out")
        S.barrier()
        pc.close()
    return nc


_NC_CACHE = {}


def _fm(v):
    v = np.asarray(v, np.float32).reshape(-1, 128)
    return np.ascontiguousarray(v.T)


def kernel(x, c, ctx, c_ctx, w_ada, b_ada, g_mix, w_in, b_in, conv_w, conv_b, ln_g, ln_b, w_pa,
           lru_conv_w, lru_conv_b, w_r_f, b_r_f, w_i_f, b_i_f, lam_f, w_r_b, b_r_b, w_i_b, b_i_b, lam_b,
           w_pb, w_o, g_ffn, w_grp, b_grp, w_er, b_er, w_gate, w_up, w_down, g_final):
    f = lambda a: np.ascontiguousarray(np.asarray(a, np.float32))
    x, c, ctx, c_ctx = f(x), f(c), f(ctx), f(c_ctx)
    B = x.shape[0]
    if "nc" not in _NC_CACHE:
        _NC_CACHE["nc"] = build_program()
    nc = _NC_CACHE["nc"]

    common = {
        "w_ada": f(w_ada[0]), "b_ada_fm": _fm(b_ada[0]),
        "b_ada_gt": f(np.broadcast_to(np.stack([b_ada[0][2048:3072], b_ada[0][5120:6144]])[None], (128, 2, 1024))),
        "w_in": f(w_in[0]), "b_in_fm": _fm(b_in[0]),
        "cb": _fm(conv_b[0]), "lng": _fm(ln_g[0]), "lnb": _fm(ln_b[0]),
        "w_pa": f(w_pa[0]), "w_pb": f(w_pb[0]), "w_o": f(w_o[0]),
        "lb": _fm(lru_conv_b[0]),
        "gmix": _fm(g_mix[0]), "gffn": _fm(g_ffn[0]),
        "gfin": f(np.broadcast_to(np.asarray(g_final, np.float32)[None], (128, 1024))),
        "w_rt": f(np.concatenate([w_grp[0], w_er[0]], axis=1)),
        "b_rt": f(np.broadcast_to(np.concatenate([b_grp[0], b_er[0]])[None], (128, 20))),
        "w_gate": f(w_gate[0]), "w_up": f(w_up[0]), "w_down": f(w_down[0]),
        "ident": np.eye(128, dtype=np.float32),
    }
    cwn = np.asarray(conv_w[0], np.float32)
    lwn = np.asarray(lru_conv_w[0], np.float32)
    zero = np.zeros((1, 1024), np.float32)
    lw5_nat = np.concatenate([lwn, zero], axis=0)
    lw5_rev = lw5_nat[::-1]

    def fm3(a):
        T = a.shape[0]
        return np.ascontiguousarray(a.reshape(T, 8, 128).transpose(2, 1, 0))

    pf = (w_r_f[0], b_r_f[0], w_i_f[0], b_i_f[0], lam_f[0])
    pbk = (w_r_b[0], b_r_b[0], w_i_b[0], b_i_b[0], lam_b[0])

    def gates(P, Sd):
        wgs = np.stack([P[0], P[2], Sd[0], Sd[2]]).astype(np.float32)
        bgs = np.stack([np.asarray(t, np.float32) for t in (P[1], P[3], Sd[1], Sd[3])])
        bgs = np.ascontiguousarray(bgs.transpose(2, 0, 1))
        lams = np.stack([np.asarray(P[4], np.float32).reshape(8, 128), np.asarray(Sd[4], np.float32).reshape(8, 128)])
        lams = np.ascontiguousarray(lams.transpose(2, 0, 1))
        return f(wgs), bgs, lams

    per_half = []
    for half in range(2):
        if half == 0:
            wgs, bgs, lams = gates(pf, pbk)
            d = {"cw": fm3(cwn), "lw5": fm3(lw5_nat), "wg": wgs, "bg": bgs, "lam": lams}
        else:
            wgs, bgs, lams = gates(pbk, pf)
            d = {"cw": fm3(cwn[::-1]), "lw5": fm3(lw5_rev), "wg": wgs, "bg": bgs, "lam": lams}
        per_half.append(d)

    in_maps = []
    pad2 = np.zeros((2, 1024), np.float32)
    for b in range(B):
        for half in range(2):
            xs = x[b] if half == 0 else x[b, ::-1]
            cs_ = ctx[b] if half == 0 else ctx[b, ::-1]
            m = dict(common)
            m.update(per_half[half])
            m["xp"] = np.ascontiguousarray(np.concatenate([pad2, xs, pad2], axis=0))
            m["ctxp"] = np.ascontiguousarray(np.concatenate([pad2, cs_, pad2], axis=0))
            m["cvec"] = np.ascontiguousarray(np.stack([_fm(c[b]), _fm(c_ctx)], axis=-1))
            in_maps.append(m)
    res = run_bass_kernel_spmd(nc, in_maps, core_ids=list(range(2 * B)))
    outp = np.empty((B, 2 * NOWN, 1024), np.float32)
    for b in range(B):
        outp[b, :NOWN] = res.results[2 * b]["out"]
        outp[b, NOWN:] = res.results[2 * b + 1]["out"][::-1]
    if DEBUG:
        kernel.last = res
    return outp
```

```python
from contextlib import ExitStack
import os
import numpy as np
import concourse.bass as bass
import concourse.mybir as mybir
from concourse.bass_utils import run_bass_kernel_spmd

F32 = mybir.dt.float32
BF16 = mybir.dt.bfloat16
AF = mybir.ActivationFunctionType
ALU = mybir.AluOpType
AX = mybir.AxisListType
EPS = 1e-6
NB = 512
NOWN = 4096
DEBUG = bool(int(os.environ.get("MK_DEBUG", "0")))


class _Rec:
    def __init__(self):
        self.calls = []

    def __getattr__(self, name):
        def f(*args, **kw):
            self.calls.append((name, args, kw))
            return self
        return f


_TBL = {"Exp": "exp", "Tanh": None, "Identity": None, "Copy": None, "Square": None, "Sqrt": "sqrt", "Silu": "silu",
        "Gelu_apprx_tanh": "gelu", "Ln": "ln"}


def _fsize(ap):
    n = 1
    for d in ap.shape[1:]:
        n *= int(d)
    return n


class Sched:
    REORDER = True
    WINDOW = 600

    def __init__(self, nc, es):
        self.nc = nc
        self.es = es
        self.E = dict(pe=nc.tensor, act=nc.scalar, dve=nc.vector, pool=nc.gpsimd, sp=nc.sync)
        self.sem = {e: es.enter_context(nc.semaphore("c_" + e)) for e in self.E}
        self.cnt = {e: 0 for e in self.E}
        self.seen = {e: {} for e in self.E}
        self.lastw = {}
        self.readers = {}
        self.dsem = {}
        self.dcnt = {}
        self.ops = []

    def op(self, e, fn, reads=(), writes=()):
        r = _Rec()
        fn(r)
        self._add(e, "op", r.calls, tuple(reads), tuple(writes), None)

    def group(self, e, fns, reads=(), writes=()):
        r = _Rec()
        for f in fns:
            f(r)
        self._add(e, "op", r.calls, tuple(reads), tuple(writes), None)

    def dma(self, q, out, in_, reads=(), writes=(), key=None):
        self._add(q, "dma", [("dma_start", (), dict(out=out, in_=in_))], tuple(reads), tuple(writes), key)

    def idma(self, q, out, out_offset, in_, in_offset, reads=(), writes=(), key=None):
        self._add(q, "dma", [("indirect_dma_start", (), dict(out=out, out_offset=out_offset, in_=in_, in_offset=in_offset))],
                  tuple(reads), tuple(writes), key)

    def _add(self, e, kind, calls, reads, writes, key):
        dur = 0.0
        tbl = None
        if kind == "dma":
            kw0 = calls[0][2]
            side = kw0["in_"] if kw0.get("out_offset") is not None else kw0["out"]
            nb = 128 * _fsize(side) * 4
            dur = 1000.0 if e == "pool" else 150.0
            lat = 2000.0 + nb / 300.0
        else:
            lat = 0.0
            for (name, args, kw) in calls:
                if e == "pe":
                    src = kw.get("rhs", kw.get("in_"))
                    dur += 25.0 + 0.5 * max(_fsize(src), 64)
                else:
                    oap = kw.get("out", kw.get("ap", args[0] if args else None))
                    n = _fsize(oap)
                    if e == "act":
                        dur += 250.0 + 0.73 * n
                        fnm = kw.get("func")
                        tbl = _TBL.get(getattr(fnm, "name", str(fnm)), None) if fnm is not None else None
                    elif e == "dve":
                        dur += 160.0 + 1.04 * n
                    else:
                        dur += 300.0 + 3.1 * n
        self.ops.append(dict(e=e, kind=kind, calls=calls, reads=reads, writes=writes, key=key, dur=dur, lat=lat, tbl=tbl))

    def flush(self):
        ops = self.ops
        self.ops = []
        n = len(ops)
        if n == 0:
            return
        lastw, readers = {}, {}
        preds = [None] * n
        succs = [[] for _ in range(n)]
        for i, o in enumerate(ops):
            p = set()
            for k in o["reads"]:
                if k in lastw:
                    p.add(lastw[k])
            for k in o["writes"]:
                if k in lastw:
                    p.add(lastw[k])
                p.update(readers.get(k, ()))
            p.discard(i)
            preds[i] = p
            for j in p:
                succs[j].append(i)
            for k in o["reads"]:
                readers.setdefault(k, []).append(i)
            for k in o["writes"]:
                lastw[k] = i
                readers[k] = []
        if not self.REORDER:
            order = range(n)
        else:
            indeg = [len(p) for p in preds]
            ready = [i for i in range(n) if indeg[i] == 0]
            finish = [0.0] * n
            efree = {e: 0.0 for e in self.E}
            etbl = [None]
            done = [False] * n
            lo = 0
            order = []
            while len(order) < n:
                while lo < n and done[lo]:
                    lo += 1
                best, bkey = None, None
                for i in ready:
                    if i > lo + self.WINDOW:
                        continue
                    o = ops[i]
                    st = efree[o["e"]]
                    for j in preds[i]:
                        f = finish[j] + (0.0 if ops[j]["e"] == o["e"] else 120.0)
                        if f > st:
                            st = f
                    if o["e"] == "act" and o["tbl"] is not None and o["tbl"] != etbl[0]:
                        st += 1300.0
                    kk = (st, i)
                    if bkey is None or kk < bkey:
                        best, bkey = i, kk
                i = best
                o = ops[i]
                st = bkey[0]
                if os.environ.get("MK_TL") and n > 3000 and len(ops) == int(os.environ.get("MK_TL")):
                    lim = None
                    for j in preds[i]:
                        f = finish[j]
                        if lim is None or f > lim[0]:
                            lim = (f, j)
                    gap = st - efree[o["e"]]
                    if o["e"] == "pe" and gap > 300:
                        print("PE gap %.1fus at t=%.1fus op#%d writes=%s waits for %s op#%d writes=%s" % (
                            gap / 1e3, st / 1e3, i, o["writes"][:2], ops[lim[1]]["e"], lim[1], ops[lim[1]]["writes"][:2]))
                if o["e"] == "act" and o["tbl"] is not None:
                    etbl[0] = o["tbl"]
                efree[o["e"]] = st + o["dur"]
                finish[i] = st + o["dur"] + o["lat"]
                done[i] = True
                ready.remove(i)
                order.append(i)
                for j in succs[i]:
                    indeg[j] -= 1
                    if indeg[j] == 0:
                        ready.append(j)
        if self.REORDER and os.environ.get("MK_STATS"):
            busy = {e: 0.0 for e in self.E}
            for o in ops:
                busy[o["e"]] += o["dur"]
            print("phase: n=%d est_makespan=%.0fus busy(us): %s" % (n, max(finish) / 1e3, {e: int(v / 1e3) for e, v in busy.items()}))
        for i in order:
            self._emit(ops[i])

    def _wait(self, e, tok, same_ok=False):
        if tok is None:
            return
        name, sem, val, src = tok
        if same_ok and src == e:
            return
        d = self.seen[e]
        if d.get(name, 0) >= val:
            return
        self.E[e].wait_ge(sem, val)
        d[name] = val

    def _emit(self, o):
        e, reads, writes = o["e"], o["reads"], o["writes"]
        for k in reads:
            self._wait(e, self.lastw.get(k))
        for k in writes:
            self._wait(e, self.lastw.get(k), same_ok=True)
            for t in self.readers.get(k, {}).values():
                self._wait(e, t, same_ok=True)
        ins = None
        for (name, args, kw) in o["calls"]:
            ins = getattr(self.E[e], name)(*args, **kw)
        if o["kind"] == "dma":
            key = o["key"]
            if key not in self.dsem:
                self.dsem[key] = self.es.enter_context(self.nc.semaphore("d_" + key))
                self.dcnt[key] = 0
            self.dcnt[key] += 16
            ins.then_inc(self.dsem[key], 16)
            tok = ("d_" + key, self.dsem[key], self.dcnt[key], "dma")
        else:
            self.cnt[e] += 1
            ins.then_inc(self.sem[e], 1)
            tok = ("c_" + e, self.sem[e], self.cnt[e], e)
        for k in reads:
            self.readers.setdefault(k, {})[tok[0]] = tok
        for k in writes:
            self.lastw[k] = tok
            self.readers[k] = {}

    def barrier(self):
        self.flush()
        for e in self.E:
            for e2 in self.E:
                if self.cnt[e2] > 0:
                    self._wait(e, ("c_" + e2, self.sem[e2], self.cnt[e2], e2))
            for k, sem in self.dsem.items():
                self._wait(e, ("d_" + k, sem, self.dcnt[k], "dma"))


def build_program():
    nc = bass.Bass("TRN2", target_bir_lowering=False)

    def din(name, shape):
        return nc.dram_tensor(name, list(shape), F32, kind="ExternalInput").ap()

    xp = din("xp", [8196, 1024])
    ctxp = din("ctxp", [260, 1024])
    cvec = din("cvec", [128, 8, 2])
    w_ada = din("w_ada", [1024, 6144])
    b_ada_fm = din("b_ada_fm", [128, 48])
    b_ada_gt = din("b_ada_gt", [128, 2, 1024])
    w_in = din("w_in", [1024, 6144])
    b_in_fm = din("b_in_fm", [128, 48])
    cw = din("cw", [128, 8, 31])
    cb = din("cb", [128, 8])
    lng = din("lng", [128, 8])
    lnb = din("lnb", [128, 8])
    w_pa = din("w_pa", [1024, 1024])
    w_pb = din("w_pb", [1024, 1024])
    w_o = din("w_o", [1024, 1024])
    lw5 = din("lw5", [128, 8, 5])
    lb = din("lb", [128, 8])
    wg = din("wg", [4, 8, 128, 128])
    bg = din("bg", [128, 4, 8])
    lam = din("lam", [128, 2, 8])
    gmix = din("gmix", [128, 8])
    gffn = din("gffn", [128, 8])
    gfin = din("gfin", [128, 1024])
    w_rt = din("w_rt", [1024, 20])
    b_rt = din("b_rt", [128, 20])
    w_gate = din("w_gate", [16, 1024, 512])
    w_up = din("w_up", [16, 1024, 512])
    w_down = din("w_down", [16, 512, 1024])
    ident = din("ident", [128, 128])
    tri = din("tri", [128, 128])
    cGU = din("cGU", [128, 32])
    cD = din("cD", [128, 16])
    sidx = din("sidx", [128, 12])
    out = nc.dram_tensor("out", [NOWN, 1024], F32, kind="ExternalOutput").ap()
    if DEBUG:
        hs_scr = nc.dram_tensor("hs_scr", [8, 128, 8, NB], BF16, kind="ExternalOutput").ap()
        x1_scr = nc.dram_tensor("x1_scr", [NOWN, 1024], F32, kind="ExternalOutput").ap()
    else:
        hs_scr = nc.dram_tensor("hs_scr", [8, 128, 8, NB], BF16, kind="Internal").ap()
        x1_scr = nc.dram_tensor("x1_scr", [NOWN, 1024], F32, kind="Internal").ap()
    gt2_scr = nc.dram_tensor("gt2_scr", [128, 1024], F32, kind="Internal").ap()
    NSEG = 12
    xs_sorted = nc.dram_tensor("xs_sorted", [NSEG * NB, 1024], BF16, kind="Internal").ap()
    ws_sorted = nc.dram_tensor("ws_sorted", [NSEG * NB, 4], F32, kind="Internal").ap()
    y_sorted = nc.dram_tensor("y_sorted", [NSEG * NB, 1024], F32, kind="Internal").ap()
    I32 = mybir.dt.int32

    with ExitStack() as es:
        S = Sched(nc, es)

        def sb(name, shape, dt=F32, stack=es):
            return stack.enter_context(nc.sbuf_tensor(name, list(shape), dt))

        psA = es.enter_context(nc.psum_tensor("psA", [128, 2048], F32))
        psB = es.enter_context(nc.psum_tensor("psB", [128, 2048], F32))

        def bank(i):
            t = psA if i < 4 else psB
            return t[:, 512 * (i % 4):512 * (i % 4) + 512]

        def pk(i):
            return "ps%d" % i

        tpv = psA[:, :].bitcast(BF16).rearrange("p (c t) -> p c t", t=512)
        TPK = ("ps0", "ps1", "ps2", "ps3")

        identb = sb("identb", [128, 128], BF16)
        ones_m = sb("ones_m", [128, 128], BF16)
        ones1 = sb("ones1", [128, 128], BF16)
        b_in_sb = sb("b_in_sb", [128, 48])
        hb_in = sb("hb_in", [128, 48])
        cw_sb = sb("cw_sb", [128, 8, 31])
        cb_sb = sb("cb_sb", [128, 8])
        lng_sb = sb("lng_sb", [128, 8])
        lnb_sb = sb("lnb_sb", [128, 8])
        lw5_sb = sb("lw5_sb", [128, 8, 5])
        lb_sb = sb("lb_sb", [128, 8])
        bg_sb = sb("bg_sb", [128, 4, 8])
        hbg = sb("hbg", [128, 4, 8])
        lam_sb = sb("lam_sb", [128, 2, 8])
        gmix_sb = sb("gmix_sb", [128, 8])
        gffn_sb = sb("gffn_sb", [128, 8])
        b_rt_sb = sb("b_rt_sb", [128, 20])
        b_ada_fm_sb = sb("b_ada_fm_sb", [128, 48])
        cvec_sb = sb("cvec_sb", [128, 8, 2])
        mods = sb("mods", [128, 48, 2])
        s1 = sb("s1", [128, 8])
        s1c = sb("s1c", [128, 8])
        s2 = sb("s2", [128, 8])
        gt1h = sb("gt1h", [128, 1024])
        cl = sb("cl", [128, 2, 8])
        hcl = sb("hcl", [128, 2, 8])
        state = sb("state", [128, 2, 8])
        ss = sb("ss", [128, 8])
        rs = sb("rs", [128, 8])
        w_rt_b = sb("w_rt_b", [128, 8, 20], BF16)
        qtr = sb("qtr", [128, 4], F32)

        def pload(t, src):
            S.dma("sp", t, src, writes=("params",), key="params")

        pload(b_in_sb[:], b_in_fm)
        pload(cw_sb[:], cw)
        pload(cb_sb[:], cb)
        pload(lng_sb[:], lng)
        pload(lnb_sb[:], lnb)
        pload(lw5_sb[:], lw5)
        pload(lb_sb[:], lb)
        pload(bg_sb[:], bg)
        pload(lam_sb[:], lam)
        pload(gmix_sb[:], gmix)
        pload(gffn_sb[:], gffn)
        pload(b_rt_sb[:], b_rt)
        pload(b_ada_fm_sb[:], b_ada_fm)
        pload(cvec_sb[:], cvec)
        S.dma("pool", identb[:], ident, writes=("identb",), key="identb")
        S.dma("pool", w_rt_b[:], w_rt.rearrange("(k p) n -> p k n", p=128), writes=("w_rt_b",), key="w_rt_b")
        S.op("pool", lambda e: e.memset(ones_m[:], 1.0 / 1024.0), writes=("ones_m",))
        S.op("pool", lambda e: e.memset(ones1[:], 1.0), writes=("ones1",))
        S.op("pool", lambda e: e.memset(qtr[:, 0:1], 0.25), writes=("qtr",))
        S.op("pool", lambda e: e.memset(qtr[:, 1:2], 1024.0 * EPS), writes=("qtr",))
        S.op("pool", lambda e: e.memset(qtr[:, 2:3], EPS), writes=("qtr",))
        S.op("pool", lambda e: e.memset(state[:], 0.0), writes=("state",))
        S.op("pool", lambda e: e.memset(ss[:], 0.0), writes=("ss",))

        with ExitStack() as p0:
            cs = sb("cs", [128, 8, 2], BF16, p0)
            cs_rep = sb("cs_rep", [128, 8, 128], BF16, p0)
            b_ada_gt_sb = sb("b_ada_gt_sb", [128, 2, 1024], F32, p0)
            wa = [sb("wa%d" % i, [128, 8, 512], BF16, p0) for i in range(3)]
            e_t = sb("e_t", [128, 16], F32, p0)
            t_t = sb("t_t", [128, 16], F32, p0)
            l_t = sb("l_t", [128, 16], F32, p0)
            m_t = sb("m_t", [128, 16], F32, p0)
            pload(b_ada_gt_sb[:], b_ada_gt)
            gt2b = sb("gt2b0", [128, 1024], F32, p0)

            S.op("act", lambda e: e.activation(out=cs[:], in_=cvec_sb[:], func=AF.Silu), reads=("params",), writes=("cs",))
            S.op("dve", lambda e: e.tensor_copy(out=cs_rep[:], in_=cs[:, :, 0:1].to_broadcast([128, 8, 128])),
                 reads=("cs",), writes=("cs_rep",))
            psm = bank(0)[:, 0:96].rearrange("p (j t) -> p j t", t=2)
            for q in range(12):
                s = q % 3
                S.dma("pool", wa[s][:], w_ada[:, 512 * q:512 * q + 512].rearrange("(k p) n -> p k n", p=128),
                      writes=("wa%d" % s,), key="wa%d" % s)
                fns = []
                for jj in range(4):
                    for k in range(8):
                        fns.append(lambda e, jj=jj, k=k, s=s, q=q: e.matmul(
                            psm[:, 4 * q + jj, :], lhsT=wa[s][:, k, 128 * jj:128 * jj + 128], rhs=cs[:, k, :],
                            start=(k == 0), stop=(k == 7)))
                S.group("pe", fns, reads=("wa%d" % s, "cs"), writes=("ps0",))
                if q in (4, 5, 10, 11):
                    bk = 1 + (q % 2)
                    S.group("pe", [lambda e, k=k, s=s, bk=bk: e.matmul(bank(bk), lhsT=cs_rep[:, k, :], rhs=wa[s][:, k, :],
                                                                      start=(k == 0), stop=(k == 7)) for k in range(8)],
                            reads=("wa%d" % s, "cs_rep"), writes=(pk(bk),))
                    dst = gt1h if q < 6 else gt2b
                    gi = 0 if q < 6 else 1
                    cols = slice(512 * (q % 2), 512 * (q % 2) + 512)
                    S.op("dve", lambda e, dst=dst, gi=gi, cols=cols, bk=bk: e.tensor_tensor(
                        out=dst[:, cols], in0=bank(bk), in1=b_ada_gt_sb[:, gi, cols], op=ALU.add),
                        reads=(pk(bk), "params"), writes=("gt",))
            S.op("dve", lambda e: e.tensor_scalar_mul(out=gt1h[:], in0=gt1h[:], scalar1=0.5), reads=("gt",), writes=("gt",))
            S.dma("sp", gt2_scr, gt2b[:], reads=("gt",), writes=("gt2_scr",), key="gt2_scr")
            S.op("dve", lambda e: e.tensor_tensor(out=mods[:], in0=psm, in1=b_ada_fm_sb[:].unsqueeze(2).to_broadcast([128, 48, 2]),
                                                  op=ALU.add), reads=("ps0", "params"), writes=("mods",))
            for (dst, col, j0, gsb) in ((s1, 0, 8, gmix_sb), (s1c, 1, 8, gmix_sb), (s2, 0, 32, gffn_sb)):
                S.op("dve", lambda e, dst=dst, col=col, j0=j0, gsb=gsb: e.scalar_tensor_tensor(
                    out=dst[:], in0=mods[:, j0:j0 + 8, col], scalar=1.0, in1=gsb[:], op0=ALU.add, op1=ALU.mult),
                    reads=("mods", "params"), writes=("sc",))
                S.op("dve", lambda e, dst=dst: e.tensor_scalar_mul(out=dst[:], in0=dst[:], scalar1=32.0),
                     reads=("sc",), writes=("sc",))
            S.op("dve", lambda e: e.tensor_scalar_mul(out=hb_in[:], in0=b_in_sb[:], scalar1=0.5), reads=("params",), writes=("hb_in",))
            S.op("dve", lambda e: e.tensor_scalar_mul(out=hbg[:], in0=bg_sb[:], scalar1=0.5), reads=("params",), writes=("hbg",))
            lamf = lam_sb[:].rearrange("p a b -> p (a b)")
            S.op("act", lambda e: e.activation(out=e_t[:], in_=lamf, func=AF.Exp, scale=-1.0), reads=("params",), writes=("e_t",))
            S.op("dve", lambda e: e.tensor_scalar(out=t_t[:], in0=e_t[:], scalar1=-0.25, scalar2=1.0 / 3.0, op0=ALU.mult, op1=ALU.add),
                 reads=("e_t",), writes=("t_t",))
            S.op("dve", lambda e: e.tensor_tensor(out=t_t[:], in0=t_t[:], in1=e_t[:], op=ALU.mult), reads=("t_t", "e_t"), writes=("t_t",))
            S.op("dve", lambda e: e.tensor_scalar_add(out=t_t[:], in0=t_t[:], scalar1=-0.5), reads=("t_t",), writes=("t_t",))
            S.op("dve", lambda e: e.tensor_tensor(out=t_t[:], in0=t_t[:], in1=e_t[:], op=ALU.mult), reads=("t_t", "e_t"), writes=("t_t",))
            S.op("dve", lambda e: e.tensor_scalar_add(out=t_t[:], in0=t_t[:], scalar1=1.0), reads=("t_t",), writes=("t_t",))
            S.op("dve", lambda e: e.tensor_tensor(out=t_t[:], in0=t_t[:], in1=e_t[:], op=ALU.mult), reads=("t_t", "e_t"), writes=("t_t",))
            S.op("dve", lambda e: e.tensor_scalar_add(out=l_t[:], in0=e_t[:], scalar1=1.0), reads=("e_t",), writes=("l_t",))
            S.op("act", lambda e: e.activation(out=l_t[:], in_=l_t[:], func=AF.Ln), reads=("l_t",), writes=("l_t",))
            S.op("dve", lambda e: e.tensor_single_scalar(out=m_t[:], in_=e_t[:], scalar=0.1, op=ALU.is_lt), reads=("e_t",), writes=("m_t",))
            S.op("dve", lambda e: e.tensor_tensor(out=t_t[:], in0=t_t[:], in1=l_t[:], op=ALU.subtract), reads=("t_t", "l_t"), writes=("t_t",))
            S.op("dve", lambda e: e.tensor_tensor(out=t_t[:], in0=t_t[:], in1=m_t[:], op=ALU.mult), reads=("t_t", "m_t"), writes=("t_t",))
            S.op("dve", lambda e: e.tensor_tensor(out=t_t[:], in0=t_t[:], in1=l_t[:], op=ALU.add), reads=("t_t", "l_t"), writes=("t_t",))
            clf = cl[:].rearrange("p a b -> p (a b)")
            hclf = hcl[:].rearrange("p a b -> p (a b)")
            S.op("dve", lambda e: e.tensor_scalar_mul(out=clf, in0=t_t[:], scalar1=-8.0), reads=("t_t",), writes=("cl",))
            S.op("dve", lambda e: e.tensor_scalar_mul(out=hclf, in0=t_t[:], scalar1=-4.0), reads=("t_t",), writes=("cl",))
            S.barrier()

        mixer = ExitStack()
        wxr = sb("wxr", [128, 8, 1024], BF16, mixer)
        wgb = sb("wgb", [128, 4, 8, 128], BF16, mixer)
        dg5 = sb("dg5", [128, 8, 5, 128], BF16, mixer)
        S.dma("pool", wxr[:], w_in[:, 3072:4096].rearrange("(k p) n -> p k n", p=128), writes=("wxr",), key="wxr")
        S.dma("pool", wgb[:], wg.rearrange("g h p n -> p g h n"), writes=("wgb",), key="wgb")
        for c in range(8):
            S.op("dve", lambda e, c=c: e.tensor_tensor(
                out=dg5[:, c, :, :], in0=identb[:].unsqueeze(1).to_broadcast([128, 5, 128]),
                in1=lw5_sb[:, c, :].unsqueeze(2).to_broadcast([128, 5, 128]), op=ALU.mult),
                reads=("identb", "params"), writes=("dg5",))

        xt = sb("xt", [128, 4, 1024], F32, mixer)
        xh = sb("xh", [4, 1024], F32, mixer)
        xn = sb("xn", [128, 4, 1024], BF16, mixer)
        xnh = sb("xnh", [4, 1024], BF16, mixer)
        hxTs = [sb("hxT%d" % i, [128, 8, NB + 4], BF16, mixer) for i in range(2)]
        hxT, hxk, hxhk = hxTs[0], "hxT0", "hxTh0"
        hbc = [0]
        xrp = [sb("xrp%d" % i, [128, NB + 4], BF16, mixer) for i in range(2)]
        xcb = [sb("xcb%d" % i, [128, NB], BF16, mixer) for i in range(2)]
        tr = sb("tr", [128, NB], F32, mixer)
        ti = sb("ti", [128, NB], F32, mixer)
        a4 = sb("a4", [128, 4, NB], F32, mixer)
        s4 = sb("s4", [128, 4, NB], F32, mixer)
        t4 = sb("t4", [128, 4, NB], BF16, mixer)
        tmp1 = sb("tmp1", [128, NB], F32, mixer)
        bb_t = sb("bb_t", [128, NB], F32, mixer)
        hf = sb("hf", [128, NB], F32, mixer)
        hsb = sb("hsb", [128, 8, NB], BF16, mixer)

        tph = bank(4).bitcast(BF16)[:, 0:32].rearrange("p (c t) -> p c t", t=4)

        def prep(xsrc, r0, N, sc, bcol, keep_key):
            nt = N // 128
            S.dma("sp", xt[:, 0:nt, :], xsrc[r0:r0 + N, :].rearrange("(j p) d -> p j d", p=128), writes=(keep_key,), key="xt")
            S.dma("sp", xh[0:2, :], xsrc[r0 - 2:r0, :], writes=("xh",), key="xh")
            S.dma("sp", xh[2:4, :], xsrc[r0 + N:r0 + N + 2, :], writes=("xh",), key="xh")
            S.op("pool", lambda e: e.memset(ss[:], 0.0), writes=("ss",))
            for j in range(nt):
                S.op("act", lambda e, j=j: e.activation(out=xn[:, j, :], in_=xt[:, j, :], func=AF.Square, accum_out=ss[:, j:j + 1]),
                     reads=(keep_key,), writes=("xn", "ss"))
            S.op("act", lambda e: e.activation(out=xnh[:], in_=xh[:], func=AF.Square, accum_out=ss[0:4, 4:5]),
                 reads=("xh",), writes=("xnh", "ss"))
            S.op("act", lambda e: e.activation(out=rs[:, 0:5], in_=ss[:, 0:5], func=AF.Sqrt, bias=qtr[:, 1:2]), reads=("ss", "qtr"), writes=("rs",))
            S.op("dve", lambda e: e.reciprocal(out=rs[:, 0:5], in_=rs[:, 0:5]), reads=("rs",), writes=("rs",))
            for j in range(nt):
                S.op("dve", lambda e, j=j: e.tensor_scalar_mul(out=xn[:, j, :], in0=xt[:, j, :], scalar1=rs[:, j:j + 1]),
                     reads=(keep_key, "rs"), writes=("xn",))
            S.op("dve", lambda e: e.tensor_scalar_mul(out=xnh[:], in0=xh[:], scalar1=rs[0:4, 4:5]),
                 reads=("xh", "rs"), writes=("xnh",))
            for j in range(nt):
                S.group("pe", [lambda e, j=j, c=c: e.transpose(out=tpv[:, c, 128 * j:128 * j + 128],
                                                               in_=xn[:, j, 128 * c:128 * c + 128], identity=identb[:])
                               for c in range(8)], reads=("xn", "identb"), writes=TPK)
            S.group("pe", [lambda e, c=c: e.transpose(out=tph[:, c, :], in_=xnh[:, 128 * c:128 * c + 128], identity=identb[0:4, 0:4])
                           for c in range(8)], reads=("xnh", "identb"), writes=("ps4",))
            for c in range(8):
                S.op("act", lambda e, c=c: e.activation(out=hxT[:, c, 0:N], in_=tpv[:, c, 0:N], func=AF.Identity,
                                                        scale=sc[:, c:c + 1], bias=mods[:, c, bcol:bcol + 1]),
                     reads=TPK + ("sc", "mods"), writes=(hxk,))
            S.op("dve", lambda e: e.tensor_tensor(out=hxT[:, :, N:N + 4], in0=tph, in1=sc[:].unsqueeze(2).to_broadcast([128, 8, 4]),
                                                  op=ALU.mult), reads=("ps4", "sc"), writes=(hxhk,))
            S.op("dve", lambda e: e.tensor_tensor(out=hxT[:, :, N:N + 4], in0=hxT[:, :, N:N + 4],
                                                  in1=mods[:, 0:8, bcol:bcol + 1].to_broadcast([128, 8, 4]), op=ALU.add),
                 reads=(hxhk, "mods"), writes=(hxhk,))

        def rglru_block(N, d, reverse, has_lo, has_hi, consumer=None):
            def st1(c):
                par = c % 2
                bxr = bank(par)[:, 0:N]
                bxh = bank(2 + par)[:, 0:4]
                S.group("pe", [lambda e, k=k: e.matmul(bxr, lhsT=wxr[:, k, 128 * c:128 * c + 128], rhs=hxT[:, k, 0:N],
                                                       start=(k == 0), stop=(k == 7)) for k in range(8)],
                        reads=(hxk, "wxr"), writes=(pk(par),))
                S.group("pe", [lambda e, k=k: e.matmul(bxh, lhsT=wxr[:, k, 128 * c:128 * c + 128], rhs=hxT[:, k, N:N + 4],
                                                       start=(k == 0), stop=(k == 7)) for k in range(8)],
                        reads=(hxhk, "wxr"), writes=(pk(2 + par),))
                xk = "xrp%d" % par
                bia = b_in_sb[:, 24 + c:25 + c]
                S.op("act", lambda e: e.activation(out=xrp[par][:, 2:2 + N], in_=bxr, func=AF.Identity, bias=bia),
                     reads=(pk(par), "params"), writes=(xk,))
                if has_lo:
                    S.op("dve", lambda e: e.tensor_scalar_add(out=xrp[par][:, 0:2], in0=bxh[:, 0:2], scalar1=bia),
                         reads=(pk(2 + par), "params"), writes=(xk,))
                else:
                    S.op("dve", lambda e: e.memset(xrp[par][:, 0:2], 0.0), writes=(xk,))
                if has_hi:
                    S.op("dve", lambda e: e.tensor_scalar_add(out=xrp[par][:, 2 + N:4 + N], in0=bxh[:, 2:4], scalar1=bia),
                         reads=(pk(2 + par), "params"), writes=(xk,))
                else:
                    S.op("dve", lambda e: e.memset(xrp[par][:, 2 + N:4 + N], 0.0), writes=(xk,))

            def st2(c):
                par = c % 2
                xk = "xrp%d" % par
                bcv = bank(4 + par)[:, 0:N]
                S.group("pe", [lambda e, j=j: e.matmul(bcv, lhsT=dg5[:, c, j, :], rhs=xrp[par][:, j:j + N],
                                                       start=(j == 0), stop=(j == 4)) for j in range(5)],
                        reads=(xk, "dg5"), writes=(pk(4 + par),))
                S.op("dve", lambda e: e.tensor_scalar_add(out=xcb[par][:, 0:N], in0=bcv, scalar1=lb_sb[:, c:c + 1]),
                     reads=(pk(4 + par), "params"), writes=("xcb%d" % par,))

            def st3(c):
                par = c % 2
                q = c % 4
                ck = "xcb%d" % par
                br_ = bank(6)[:, 0:N]
                bi_ = bank(7)[:, 0:N]
                S.group("pe", [lambda e: e.matmul(br_, lhsT=wgb[:, 2 * d, c, :], rhs=xcb[par][:, 0:N], start=True, stop=True)],
                        reads=(ck, "wgb"), writes=("ps6",))
                S.group("pe", [lambda e: e.matmul(bi_, lhsT=wgb[:, 2 * d + 1, c, :], rhs=xcb[par][:, 0:N], start=True, stop=True)],
                        reads=(ck, "wgb"), writes=("ps7",))
                S.op("act", lambda e: e.activation(out=tr[:, 0:N], in_=br_, func=AF.Tanh, scale=0.5, bias=hbg[:, 2 * d, c:c + 1]),
                     reads=("ps6", "hbg"), writes=("tr",))
                S.op("act", lambda e: e.activation(out=ti[:, 0:N], in_=bi_, func=AF.Tanh, scale=0.5, bias=hbg[:, 2 * d + 1, c:c + 1]),
                     reads=("ps7", "hbg"), writes=("ti",))
                S.op("act", lambda e: e.activation(out=a4[:, q, 0:N], in_=tr[:, 0:N], func=AF.Exp, scale=hcl[:, d, c:c + 1],
                                                   bias=hcl[:, d, c:c + 1]), reads=("tr", "cl"), writes=("a4_%d" % q,))
                S.op("pool", lambda e: e.tensor_tensor(out=s4[:, q, 0:N], in0=a4[:, q, 0:N], in1=a4[:, q, 0:N], op=ALU.mult),
                     reads=("a4_%d" % q,), writes=("s4_%d" % q,))
                S.op("dve", lambda e: e.scalar_tensor_tensor(out=t4[:, q, 0:N], in0=ti[:, 0:N], scalar=1.0, in1=xcb[par][:, 0:N],
                                                             op0=ALU.add, op1=ALU.mult), reads=("ti", ck), writes=("t4_%d" % q,))

            def st4(c0):
                sk = tuple("s4_%d" % q for q in range(4))
                S.op("act", lambda e: e.activation(out=s4[:, :, 0:N], in_=s4[:, :, 0:N], func=AF.Sqrt, scale=-0.25, bias=qtr[:, 0:1]),
                     reads=sk + ("qtr",), writes=sk)
                for c in range(c0, c0 + 4):
                    q = c % 4
                    S.op("dve", lambda e, q=q: e.tensor_tensor(out=bb_t[:, 0:N], in0=s4[:, q, 0:N], in1=t4[:, q, 0:N], op=ALU.mult),
                         reads=("s4_%d" % q, "t4_%d" % q), writes=("bb_t",))
                    if reverse:
                        S.op("dve", lambda e, q=q, c=c: e.tensor_tensor_scan(
                            out=hf[:, 0:N][:, ::-1], data0=a4[:, q, 0:N][:, ::-1], data1=bb_t[:, 0:N][:, ::-1],
                            initial=state[:, d, c:c + 1], op0=ALU.mult, op1=ALU.add),
                            reads=("a4_%d" % q, "bb_t", "state"), writes=("hf",))
                        S.op("pool", lambda e, c=c: e.tensor_copy(out=state[:, d, c:c + 1], in_=hf[:, 0:1]),
                             reads=("hf",), writes=("state",))
                    else:
                        S.op("dve", lambda e, q=q, c=c: e.tensor_tensor_scan(
                            out=hf[:, 0:N], data0=a4[:, q, 0:N], data1=bb_t[:, 0:N], initial=state[:, d, c:c + 1],
                            op0=ALU.mult, op1=ALU.add), reads=("a4_%d" % q, "bb_t", "state"), writes=("hf",))
                        S.op("pool", lambda e, c=c: e.tensor_copy(out=state[:, d, c:c + 1], in_=hf[:, N - 1:N]),
                             reads=("hf",), writes=("state",))
                    if consumer is not None:
                        consumer(c)

            for s_ in range(10):
                if s_ < 8:
                    st1(s_)
                if 1 <= s_ <= 8:
                    st2(s_ - 1)
                if 2 <= s_ <= 9:
                    st3(s_ - 2)
                    if (s_ - 2) % 4 == 3:
                        st4(s_ - 2 - 3)

        hxT, hxk, hxhk = hxTs[0], "hxT0", "hxTh0"
        prep(ctxp, 2, 256, s1c, 1, "xt")
        hbc[0] = 1
        rglru_block(256, 0, False, False, False)
        rglru_block(256, 1, True, False, False)
        for blk in range(15, -1, -1):
            _i = hbc[0] % 2
            hbc[0] += 1
            hxT, hxk, hxhk = hxTs[_i], "hxT%d" % _i, "hxTh%d" % _i
            prep(xp, 2 + NB * blk, NB, s1, 0, "xt")
            def cons_a(c):
                S.op("dve", lambda e, c=c: e.tensor_copy(out=hsb[:, c, :], in_=hf[:]), reads=("hf",), writes=("hsb",))
            rglru_block(NB, 1, True, blk != 0, blk != 15, cons_a if blk < 8 else None)
            if blk < 8:
                S.dma("sp", hs_scr[blk], hsb[:], reads=("hsb",), writes=("hs_scr%d" % blk,), key="hs_scr")

        pb_ = ExitStack()
        cwh = cw_sb
        S.op("dve", lambda e: e.tensor_scalar_mul(out=cwh[:], in0=cw_sb[:], scalar1=0.5), reads=("params",), writes=("cwh",))
        lnr = sb("lnr", [128, NB], F32, pb_)
        lmr = sb("lmr", [128, NB], F32, pb_)
        wsl = [sb("wsl%d" % i, [128, 8, 512], BF16, pb_) for i in range(3)]
        dgc = [sb("dgc0", [128, 31, 128], BF16, pb_)] * 2
        tv, uu = tr, ti
        zb = [sb("zb0", [128, NB], BF16, pb_)] * 2
        zc = sb("zc", [128, 8, NB], BF16, pb_)
        aa = zc
        zsq = [sb("zsq0", [128, NB], BF16, pb_)] * 2
        A_t = sb("A_t", [128, 8, NB], BF16, pb_)
        gy = sb("gy", [128, 8, NB], BF16, pb_)
        mg = gy
        yb = sb("yb", [128, 8, NB], BF16, pb_)
        hs_in = hsb
        x1 = xt

        xres = [sb("xres%d" % i, [128, 512], F32, pb_) for i in range(3)]
        xrc = [0]
        wctr = [0]

        def wpiece(col0, src=None):
            src = w_in if src is None else src
            i = wctr[0] % 3
            wctr[0] += 1
            S.dma("pool", wsl[i][:], src[:, col0:col0 + 512].rearrange("(k p) n -> p k n", p=128),
                  writes=("wsl%d" % i,), key="wsl%d" % i)
            return wsl[i], "wsl%d" % i

        def inproj(dstbank, wt, wk, j4):
            S.group("pe", [lambda e, k=k: e.matmul(bank(dstbank), lhsT=wt[:, k, 128 * j4:128 * j4 + 128], rhs=hxT[:, k, 0:NB],
                                                   start=(k == 0), stop=(k == 7)) for k in range(8)],
                    reads=(hxk, wk), writes=(pk(dstbank),))

        for blk in range(8):
            _i = hbc[0] % 2
            hbc[0] += 1
            hxT, hxk, hxhk = hxTs[_i], "hxT%d" % _i, "hxTh%d" % _i
            prep(xp, 2 + NB * blk, NB, s1, 0, "xt")
            S.dma("sp", hs_in[:], hs_scr[blk], reads=("hs_scr%d" % blk,), writes=("hsb",), key="hs_in")
            for half in range(2):
                wu, wuk = wpiece(512 * half)
                wv, wvk = wpiece(1024 + 512 * half)
                for c4 in range(4):
                    c = 4 * half + c4
                    par = c % 2
                    inproj(par, wu, wuk, c4)
                    inproj(2 + par, wv, wvk, c4)
                    S.op("act", lambda e, c=c, par=par: e.activation(out=tv[:], in_=bank(2 + par), func=AF.Tanh, scale=0.5,
                                                                     bias=hb_in[:, 8 + c:9 + c]),
                         reads=(pk(2 + par), "hb_in"), writes=("tr",))
                    S.op("act", lambda e, c=c, par=par: e.activation(out=uu[:], in_=bank(par), func=AF.Identity,
                                                                     bias=b_in_sb[:, c:c + 1]),
                         reads=(pk(par), "params"), writes=("ti",))
                    zk = "zb0"
                    S.op("dve", lambda e, par=par: e.scalar_tensor_tensor(out=zb[par][:], in0=tv[:], scalar=1.0, in1=uu[:],
                                                                          op0=ALU.add, op1=ALU.mult),
                         reads=("tr", "ti"), writes=(zk,))
                    dk = "dgc0"
                    S.op("dve", lambda e, c=c, par=par: e.tensor_tensor(
                        out=dgc[par][:], in0=identb[:].unsqueeze(1).to_broadcast([128, 31, 128]),
                        in1=cwh[:, c, :].unsqueeze(2).to_broadcast([128, 31, 128]), op=ALU.mult),
                        reads=("identb", "cwh"), writes=(dk,))
                    zv = zb[par][:].rearrange("p (r t) -> p r t", t=64)
                    pcv = bank(4 + par).rearrange("p (r t) -> p r t", t=64)
                    fns = []
                    order = [15] + [k for k in range(31) if k != 15]
                    for idx, k in enumerate(order):
                        o = k - 15
                        t0, t1 = max(0, -o), 64 - max(0, o)
                        fns.append(lambda e, k=k, o=o, t0=t0, t1=t1, idx=idx, par=par, pcv=pcv, zv=zv: e.matmul(
                            pcv[:, :, t0:t1], lhsT=dgc[par][:, k, :], rhs=zv[:, :, t0 + o:t1 + o],
                            start=(idx == 0), stop=(idx == 30)))
                    S.group("pe", fns, reads=(zk, dk), writes=(pk(4 + par),))
                    S.op("act", lambda e, c=c, par=par: e.activation(out=zc[:, c, :], in_=bank(4 + par), func=AF.Identity,
                                                                     bias=cb_sb[:, c:c + 1]),
                         reads=(pk(4 + par), "params"), writes=("zc",))
                    qk = "zsq0"
                    S.op("act", lambda e, c=c, par=par: e.activation(out=zsq[par][:], in_=bank(4 + par), func=AF.Square,
                                                                     bias=cb_sb[:, c:c + 1]),
                         reads=(pk(4 + par), "params"), writes=(qk,))
                    S.group("pe", [lambda e, c=c: e.matmul(bank(6), lhsT=ones_m[:], rhs=zc[:, c, :], start=(c == 0), stop=(c == 7))],
                            reads=("zc", "ones_m"), writes=("ps6",))
                    S.group("pe", [lambda e, c=c, par=par: e.matmul(bank(7), lhsT=ones_m[:], rhs=zsq[par][:], start=(c == 0),
                                                                    stop=(c == 7))], reads=(qk, "ones_m"), writes=("ps7",))
            S.op("act", lambda e: e.activation(out=tv[:], in_=bank(6), func=AF.Copy), reads=("ps6",), writes=("tr",))
            S.op("dve", lambda e: e.tensor_tensor(out=uu[:], in0=tv[:], in1=tv[:], op=ALU.mult), reads=("tr",), writes=("ti",))
            S.op("dve", lambda e: e.tensor_tensor(out=lnr[:], in0=bank(7), in1=uu[:], op=ALU.subtract), reads=("ps7", "ti"),
                 writes=("lnr",))
            S.op("act", lambda e: e.activation(out=lnr[:], in_=lnr[:], func=AF.Sqrt, bias=qtr[:, 2:3]), reads=("lnr", "qtr"), writes=("lnr",))
            S.op("dve", lambda e: e.reciprocal(out=lnr[:], in_=lnr[:]), reads=("lnr",), writes=("lnr",))
            S.op("dve", lambda e: e.tensor_tensor(out=lmr[:], in0=tv[:], in1=lnr[:], op=ALU.mult), reads=("tr", "lnr"),
                 writes=("lmr",))
            for half in range(2):
                wy, wyk = wpiece(2048 + 512 * half)
                for c4 in range(4):
                    c = 4 * half + c4
                    par = c % 2
                    inproj(par, wy, wyk, c4)
                    S.op("act", lambda e, c=c, par=par: e.activation(out=gy[:, c, :], in_=bank(par), func=AF.Gelu_apprx_tanh,
                                                                     bias=b_in_sb[:, 16 + c:17 + c]),
                         reads=(pk(par), "params"), writes=("gy",))
            def cons_b(c):
                S.op("dve", lambda e, c=c: e.tensor_tensor(out=tmp1[:], in0=hf[:], in1=hs_in[:, c, :], op=ALU.add),
                     reads=("hf", "hsb"), writes=("tmp1",))
                S.op("dve", lambda e, c=c: e.tensor_tensor(out=yb[:, c, :], in0=tmp1[:], in1=gy[:, c, :], op=ALU.mult),
                     reads=("tmp1", "gy"), writes=("yb",))
            rglru_block(NB, 0, False, blk != 0, True, cons_b)
            for c in range(8):
                S.op("dve", lambda e, c=c: e.tensor_tensor(out=tv[:], in0=zc[:, c, :], in1=lnr[:], op=ALU.mult),
                     reads=("zc", "lnr"), writes=("tr",))
                S.op("dve", lambda e: e.tensor_tensor(out=uu[:], in0=tv[:], in1=lmr[:], op=ALU.subtract), reads=("tr", "lmr"),
                     writes=("ti",))
                S.op("act", lambda e, c=c: e.activation(out=aa[:, c, :], in_=uu[:], func=AF.Silu, scale=lng_sb[:, c:c + 1],
                                                        bias=lnb_sb[:, c:c + 1]), reads=("ti", "params"), writes=("zc",))
            for half in range(2):
                wga, wgak = wpiece(4096 + 512 * half)
                wpa, wpak = wpiece(512 * half, w_pa)
                for m4 in range(4):
                    m = 4 * half + m4
                    par = m % 2
                    S.group("pe", [lambda e, k=k, m4=m4, par=par, wpa=wpa: e.matmul(bank(par), lhsT=wpa[:, k, 128 * m4:128 * m4 + 128],
                                                                         rhs=aa[:, k, :], start=(k == 0), stop=(k == 7))
                                   for k in range(8)], reads=("zc", wpak), writes=(pk(par),))
                    inproj(2 + par, wga, wgak, m4)
                    S.op("act", lambda e, m=m, par=par: e.activation(out=tv[:], in_=bank(2 + par), func=AF.Tanh, scale=0.5,
                                                                     bias=hb_in[:, 32 + m:33 + m]),
                         reads=(pk(2 + par), "hb_in"), writes=("tr",))
                    S.op("dve", lambda e, m=m, par=par: e.scalar_tensor_tensor(out=A_t[:, m, :], in0=tv[:], scalar=1.0,
                                                                               in1=bank(par), op0=ALU.add, op1=ALU.mult),
                         reads=("tr", pk(par)), writes=("A_t",))
            for half in range(2):
                wgb_, wgbk = wpiece(5120 + 512 * half)
                wpb, wpbk = wpiece(512 * half, w_pb)
                for m4 in range(4):
                    m = 4 * half + m4
                    par = m % 2
                    S.group("pe", [lambda e, k=k, m4=m4, par=par, wpb=wpb: e.matmul(bank(par), lhsT=wpb[:, k, 128 * m4:128 * m4 + 128],
                                                                         rhs=yb[:, k, :], start=(k == 0), stop=(k == 7))
                                   for k in range(8)], reads=("yb", wpbk), writes=(pk(par),))
                    inproj(2 + par, wgb_, wgbk, m4)
                    S.op("act", lambda e, m=m, par=par: e.activation(out=tv[:], in_=bank(2 + par), func=AF.Tanh, scale=0.5,
                                                                     bias=hb_in[:, 40 + m:41 + m]),
                         reads=(pk(2 + par), "hb_in"), writes=("tr",))
                    S.op("dve", lambda e, par=par: e.scalar_tensor_tensor(out=uu[:], in0=tv[:], scalar=1.0, in1=bank(par),
                                                                          op0=ALU.add, op1=ALU.mult),
                         reads=("tr", pk(par)), writes=("ti",))
                    S.op("dve", lambda e, m=m: e.tensor_tensor(out=mg[:, m, :], in0=uu[:], in1=A_t[:, m, :], op=ALU.add),
                         reads=("ti", "A_t"), writes=("gy",))
            for hh in range(2):
                wo, wok = wpiece(512 * hh, w_o)
                for j in range(4):
                    bk = 4 + j
                    xi = xrc[0] % 3
                    xrc[0] += 1
                    xk_ = "xres%d" % xi
                    r0_ = 2 + NB * blk + 128 * j
                    S.dma("sp", xres[xi][:], xp[r0_:r0_ + 128, 512 * hh:512 * hh + 512], writes=(xk_,), key=xk_)
                    S.group("pe", [lambda e, k=k, j=j, bk=bk, wo=wo: e.matmul(bank(bk), lhsT=mg[:, k, 128 * j:128 * j + 128],
                                                                             rhs=wo[:, k, :], start=(k == 0), stop=(k == 7))
                                   for k in range(8)], reads=("gy", wok), writes=(pk(bk),))
                    S.op("dve", lambda e, hh=hh, bk=bk: e.tensor_tensor(out=tmp1[:], in0=bank(bk), in1=gt1h[:, 512 * hh:512 * hh + 512],
                                                                        op=ALU.mult), reads=(pk(bk), "gt"), writes=("tmp1",))
                    S.op("pool", lambda e, xi=xi: e.tensor_tensor(out=xres[xi][:], in0=xres[xi][:], in1=tmp1[:], op=ALU.add),
                         reads=("tmp1", xk_), writes=(xk_,))
                    S.dma("sp", x1_scr[NB * blk + 128 * j:NB * blk + 128 * j + 128, 512 * hh:512 * hh + 512], xres[xi][:],
                          reads=(xk_,), writes=("x1_scr%d" % blk,), key="x1_scr")
        S.barrier()
        pb_.close()
        mixer.close()

        def bc(ap, shape):
            return ap.to_broadcast(shape)

        pcg = ExitStack()
        gf32 = sb("gf32", [128, 1024], F32, pcg)
        gt2b = sb("gt2b", [128, 1024], F32, pcg)
        S.dma("sp", gf32[:], gfin, writes=("gf32",), key="gf32")
        S.dma("sp", gt2b[:], gt2_scr, reads=("gt2_scr",), writes=("gt2b",), key="gt2b")
        S.op("dve", lambda e: e.tensor_scalar_mul(out=gf32[:], in0=gf32[:], scalar1=32.0), reads=("gf32",), writes=("gf32",))
        slot_i = sb("slot_i", [128, 32], I32, pcg)
        offGU_i = sb("offGU_i", [128, NSEG, 32], I32, pcg)
        offD_i = sb("offD_i", [128, NSEG, 16], I32, pcg)
        trib = sb("trib", [128, 128], BF16, pcg)
        S.dma("pool", trib[:], tri, writes=("trib",), key="trib")

        c1 = ExitStack()
        x1l = [sb("x1l%d" % i, [128, 4, 1024], F32, c1) for i in range(2)]
        xn2_all = sb("xn2_all", [128, 32, 1024], BF16, c1)
        hmT1 = sb("hmTr", [128, 8, NB], BF16, c1)
        zt = sb("zt", [128, 4096], BF16, c1)
        ztf = sb("ztf", [128, 192], F32, c1)
        ssA = sb("ssA", [128, 8, 4], F32, c1)
        rsA = sb("rsA", [128, 8, 4], F32, c1)
        oh_all = sb("oh_all", [128, 32, 4], F32, c1)
        wsel_all = sb("wsel_all", [128, 32, 4], F32, c1)
        oh_bf = sb("oh_bf", [128, 32, 4], BF16, c1)
        R1s = sb("R1s", [128, 32, 4], F32, c1)
        Cs = sb("Cs", [128, 32, 4], F32, c1)
        incl = sb("incl", [128, 4, 32], F32, c1)
        onesf = sb("onesf", [128, 32], F32, c1)
        ng = sb("ng", [128, 4], F32, c1)
        nseg = sb("nseg", [128, 4], F32, c1)
        sst = sb("sst", [128, 4], F32, c1)
        sen = sb("sen", [128, 4], F32, c1)
        slot_f = sb("slot_f", [128, 32], F32, c1)
        Gs = sb("Gs", [128, NSEG], F32, c1)
        sidx_sb = sb("sidx_sb", [128, NSEG], F32, c1)
        cGU_sb = sb("cGU_sb", [128, 32], F32, c1)
        cD_sb = sb("cD_sb", [128, 16], F32, c1)
        offGU_f = sb("offGU_f", [128, NSEG, 32], F32, c1)
        offD_f = sb("offD_f", [128, NSEG, 16], F32, c1)
        L = sb("L", [128, 4, 20], F32, c1)
        gmax = sb("gmax", [128, 4, 1], F32, c1)
        eg = sb("eg", [128, 4, 4], F32, c1)
        pg = sb("pg", [128, 4, 1], F32, c1)
        tmp16 = sb("tmp16", [128, 4, 16], F32, c1)
        esel = sb("esel", [128, 4, 4], F32, c1)
        m1 = sb("m1", [128, 4, 1], F32, c1)
        m2 = sb("m2", [128, 4, 1], F32, c1)
        k1 = sb("k1", [128, 4, 4], F32, c1)
        k2 = sb("k2", [128, 4, 4], F32, c1)
        e2 = sb("e2", [128, 4, 4], F32, c1)
        w1 = sb("w1", [128, 4, 1], F32, c1)
        w2 = sb("w2", [128, 4, 1], F32, c1)
        S.dma("sp", sidx_sb[:], sidx, writes=("cidx",), key="cidx")
        S.dma("sp", cGU_sb[:], cGU, writes=("cidx",), key="cidx")
        S.dma("sp", cD_sb[:], cD, writes=("cidx",), key="cidx")
        S.op("pool", lambda e: e.memset(zt[:], 0.0), writes=("zt",))
        S.op("pool", lambda e: e.memset(ztf[:], 0.0), writes=("zt",))
        S.op("pool", lambda e: e.memset(onesf[:], 1.0), writes=("onesf",))
        for sg_ in range(NSEG):
            S.dma("sp", xs_sorted[NB * sg_:NB * sg_ + NB, :].rearrange("(p r) d -> p (r d)", r=4), zt[:],
                  reads=("zt",), writes=("xs_sorted",), key="xs_z")
        S.dma("sp", ws_sorted.rearrange("(p r) c -> p (r c)", r=48), ztf[:], reads=("zt",), writes=("ws_sorted",), key="xs_z")

        for blk in range(8):
            pb2 = blk % 2
            x1t = x1l[pb2]
            ak = "x1l%d" % pb2
            sak = "ssA%d" % blk
            oh = oh_all[:, 4 * blk:4 * blk + 4, :]
            wsel = wsel_all[:, 4 * blk:4 * blk + 4, :]
            S.dma("sp", x1t[:], x1_scr[NB * blk:NB * blk + NB, :].rearrange("(j p) d -> p j d", p=128),
                  reads=("x1_scr%d" % blk,), writes=(ak,), key=ak)
            S.op("pool", lambda e, blk=blk: e.memset(ssA[:, blk, :], 0.0), writes=(sak,))
            for j in range(4):
                S.op("act", lambda e, j=j, x1t=x1t, blk=blk: e.activation(out=xn2_all[:, 4 * blk + j, :], in_=x1t[:, j, :], func=AF.Square,
                                                                          accum_out=ssA[:, blk, j:j + 1]),
                     reads=(ak,), writes=("xn2_%d" % blk, sak))
            S.op("act", lambda e, blk=blk: e.activation(out=rsA[:, blk, :], in_=ssA[:, blk, :], func=AF.Sqrt, bias=qtr[:, 1:2]),
                 reads=(sak, "qtr"), writes=(sak + "r",))
            S.op("dve", lambda e, blk=blk: e.reciprocal(out=rsA[:, blk, :], in_=rsA[:, blk, :]), reads=(sak + "r",), writes=(sak + "r",))
            for j in range(4):
                S.op("dve", lambda e, j=j, x1t=x1t, blk=blk: e.tensor_scalar_mul(out=xn2_all[:, 4 * blk + j, :], in0=x1t[:, j, :],
                                                                                 scalar1=rsA[:, blk, j:j + 1]),
                     reads=(ak, sak + "r"), writes=("xn2_%d" % blk,))
            for j in range(4):
                S.group("pe", [lambda e, j=j, c=c, blk=blk: e.transpose(out=tpv[:, c, 128 * j:128 * j + 128],
                                                                        in_=xn2_all[:, 4 * blk + j, 128 * c:128 * c + 128], identity=identb[:])
                               for c in range(8)], reads=("xn2_%d" % blk, "identb"), writes=TPK)
            for c in range(8):
                S.op("act", lambda e, c=c: e.activation(out=hmT1[:, c, :], in_=tpv[:, c, :], func=AF.Identity,
                                                        scale=s2[:, c:c + 1], bias=mods[:, 24 + c, 0:1]),
                     reads=TPK + ("sc", "mods"), writes=("hmT1",))
            for j in range(4):
                S.group("pe", [lambda e, k=k, j=j: e.matmul(bank(4)[:, 20 * j:20 * j + 20], lhsT=hmT1[:, k, 128 * j:128 * j + 128],
                                                            rhs=w_rt_b[:, k, :], start=(k == 0), stop=(k == 7))
                               for k in range(8)], reads=("hmT1", "w_rt_b"), writes=("ps4",))
            S.op("dve", lambda e: e.tensor_tensor(out=L[:], in0=bank(4)[:, 0:80].rearrange("p (j n) -> p j n", n=20),
                                                  in1=bc(b_rt_sb[:].unsqueeze(1), [128, 4, 20]), op=ALU.add),
                 reads=("ps4", "params"), writes=("L",))
            R = ("rt",)
            OK_ = ("oh_all",)
            S.op("dve", lambda e: e.tensor_reduce(out=gmax[:], in_=L[:, :, 0:4], axis=AX.X, op=ALU.max), reads=("L",), writes=R)
            S.op("dve", lambda e, oh=oh: e.tensor_tensor(out=oh, in0=L[:, :, 0:4], in1=bc(gmax[:], [128, 4, 4]), op=ALU.is_equal),
                 reads=R + ("L",), writes=R + OK_)
            S.op("dve", lambda e: e.tensor_tensor(out=eg[:], in0=L[:, :, 0:4], in1=bc(gmax[:], [128, 4, 4]), op=ALU.subtract),
                 reads=R + ("L",), writes=R)
            S.op("act", lambda e: e.activation(out=eg[:], in_=eg[:], func=AF.Exp), reads=R, writes=R)
            S.op("dve", lambda e: e.tensor_reduce(out=pg[:], in_=eg[:], axis=AX.X, op=ALU.add), reads=R, writes=R)
            S.op("dve", lambda e: e.reciprocal(out=pg[:], in_=pg[:]), reads=R, writes=R)
            S.op("dve", lambda e, oh=oh: e.tensor_tensor(out=tmp16[:].rearrange("p j (g x) -> p j g x", x=4),
                                                         in0=L[:, :, 4:20].rearrange("p j (g x) -> p j g x", x=4),
                                                         in1=bc(oh.unsqueeze(3), [128, 4, 4, 4]), op=ALU.mult),
                 reads=R + ("L",), writes=R)
            S.op("dve", lambda e: e.tensor_reduce(out=esel[:].unsqueeze(3), in_=tmp16[:].rearrange("p j (g x) -> p j x g", x=4),
                                                  axis=AX.X, op=ALU.add), reads=R, writes=R)
            S.op("dve", lambda e: e.tensor_reduce(out=m1[:], in_=esel[:], axis=AX.X, op=ALU.max), reads=R, writes=R)
            S.op("dve", lambda e: e.tensor_tensor(out=k1[:], in0=esel[:], in1=bc(m1[:], [128, 4, 4]), op=ALU.is_equal),
                 reads=R, writes=R)
            S.op("dve", lambda e: e.scalar_tensor_tensor(out=e2[:], in0=k1[:], scalar=-1e30, in1=esel[:], op0=ALU.mult, op1=ALU.add),
                 reads=R, writes=R)
            S.op("dve", lambda e: e.tensor_reduce(out=m2[:], in_=e2[:], axis=AX.X, op=ALU.max), reads=R, writes=R)
            S.op("dve", lambda e: e.tensor_tensor(out=k2[:], in0=e2[:], in1=bc(m2[:], [128, 4, 4]), op=ALU.is_equal),
                 reads=R, writes=R)
            S.op("dve", lambda e: e.tensor_tensor(out=w2[:], in0=m2[:], in1=m1[:], op=ALU.subtract), reads=R, writes=R)
            S.op("act", lambda e: e.activation(out=w2[:], in_=w2[:], func=AF.Exp), reads=R, writes=R)
            S.op("dve", lambda e: e.tensor_scalar_add(out=w1[:], in0=w2[:], scalar1=1.0), reads=R, writes=R)
            S.op("dve", lambda e: e.reciprocal(out=w1[:], in_=w1[:]), reads=R, writes=R)
            S.op("dve", lambda e: e.tensor_tensor(out=w2[:], in0=w2[:], in1=w1[:], op=ALU.mult), reads=R, writes=R)
            S.op("dve", lambda e: e.tensor_tensor(out=w1[:], in0=w1[:], in1=pg[:], op=ALU.mult), reads=R, writes=R)
            S.op("dve", lambda e: e.tensor_tensor(out=w2[:], in0=w2[:], in1=pg[:], op=ALU.mult), reads=R, writes=R)
            S.op("dve", lambda e, wsel=wsel: e.tensor_tensor(out=wsel, in0=k1[:], in1=bc(w1[:], [128, 4, 4]), op=ALU.mult),
                 reads=R, writes=R + ("wsel_all",))
            S.op("dve", lambda e: e.tensor_tensor(out=k2[:], in0=k2[:], in1=bc(w2[:], [128, 4, 4]), op=ALU.mult), reads=R, writes=R)
            S.op("dve", lambda e, wsel=wsel: e.tensor_tensor(out=wsel, in0=wsel, in1=k2[:], op=ALU.add), reads=R + ("wsel_all",),
                 writes=R + ("wsel_all",))

        ohf = oh_all[:].rearrange("p t g -> p (t g)")
        S.op("dve", lambda e: e.tensor_copy(out=oh_bf[:], in_=oh_all[:]), reads=("oh_all",), writes=("oh_bf",))
        S.group("pe", [lambda e: e.matmul(bank(0)[:, 0:128], lhsT=trib[:], rhs=oh_bf[:].rearrange("p t g -> p (t g)"), start=True, stop=True)],
                reads=("oh_bf", "trib"), writes=("ps0",))
        S.group("pe", [lambda e: e.matmul(bank(1)[:, 0:128], lhsT=ones1[:], rhs=oh_bf[:].rearrange("p t g -> p (t g)"), start=True, stop=True)],
                reads=("oh_bf", "ones1"), writes=("ps1",))
        S.op("act", lambda e: e.activation(out=R1s[:].rearrange("p t g -> p (t g)"), in_=bank(0)[:, 0:128], func=AF.Copy),
             reads=("ps0",), writes=("R1s",))
        S.op("act", lambda e: e.activation(out=Cs[:].rearrange("p t g -> p (t g)"), in_=bank(1)[:, 0:128], func=AF.Copy),
             reads=("ps1",), writes=("Cs",))
        for g in range(4):
            S.op("dve", lambda e, g=g: e.tensor_tensor_scan(out=incl[:, g, :], data0=onesf[:], data1=Cs[:, :, g], initial=0.0,
                                                            op0=ALU.mult, op1=ALU.add), reads=("Cs", "onesf"), writes=("incl",))
        S.op("dve", lambda e: e.tensor_copy(out=ng[:], in_=incl[:, :, 31]), reads=("incl",), writes=("ng",))
        S.op("dve", lambda e: e.tensor_tensor(out=incl[:], in0=incl[:], in1=Cs[:].rearrange("p t g -> p g t"), op=ALU.subtract),
             reads=("incl", "Cs"), writes=("incl",))
        S.op("dve", lambda e: e.memset(nseg[:], 0.0), writes=("nseg",))
        for k in range(8):
            S.op("dve", lambda e, k=k: e.scalar_tensor_tensor(out=nseg[:], in0=ng[:], scalar=float(NB * k), in1=nseg[:],
                                                              op0=ALU.is_gt, op1=ALU.add), reads=("ng", "nseg"), writes=("nseg",))
        S.op("dve", lambda e: e.memset(sst[:], 0.0), writes=("sst",))
        for g in range(1, 4):
            S.op("dve", lambda e, g=g: e.tensor_tensor(out=sst[:, g:g + 1], in0=sst[:, g - 1:g], in1=nseg[:, g - 1:g], op=ALU.add),
                 reads=("sst", "nseg"), writes=("sst",))
        S.op("dve", lambda e: e.tensor_tensor(out=sen[:], in0=sst[:], in1=nseg[:], op=ALU.add), reads=("sst", "nseg"), writes=("sen",))
        S.op("dve", lambda e: e.tensor_scalar_mul(out=sst[:], in0=sst[:], scalar1=float(NB)), reads=("sst", "sen"), writes=("sst",))
        S.op("dve", lambda e: e.tensor_tensor(out=R1s[:], in0=R1s[:], in1=incl[:].rearrange("p g t -> p t g"), op=ALU.add),
             reads=("R1s", "incl"), writes=("R1s",))
        S.op("dve", lambda e: e.tensor_tensor(out=R1s[:], in0=R1s[:], in1=bc(sst[:].unsqueeze(1), [128, 32, 4]), op=ALU.add),
             reads=("R1s", "sst"), writes=("R1s",))
        S.op("dve", lambda e: e.tensor_tensor(out=R1s[:], in0=R1s[:], in1=oh_all[:], op=ALU.mult), reads=("R1s", "oh_all"), writes=("R1s",))
        S.op("dve", lambda e: e.tensor_reduce(out=slot_f[:].unsqueeze(2), in_=R1s[:], axis=AX.X, op=ALU.add), reads=("R1s",), writes=("slot_f",))
        S.op("dve", lambda e: e.tensor_copy(out=slot_i[:], in_=slot_f[:]), reads=("slot_f",), writes=("slot_i",))
        S.op("dve", lambda e: e.memset(Gs[:], 0.0), writes=("Gs",))
        for g in range(3):
            S.op("dve", lambda e, g=g: e.scalar_tensor_tensor(out=Gs[:], in0=sidx_sb[:], scalar=sen[:, g:g + 1], in1=Gs[:],
                                                              op0=ALU.is_ge, op1=ALU.add), reads=("cidx", "sen", "Gs"), writes=("Gs",))
        S.op("dve", lambda e: e.tensor_scalar_mul(out=offGU_f[:], in0=bc(Gs[:].unsqueeze(2), [128, NSEG, 32]), scalar1=4096.0),
             reads=("Gs",), writes=("offGU_f",))
        S.op("dve", lambda e: e.tensor_tensor(out=offGU_f[:], in0=offGU_f[:], in1=bc(cGU_sb[:].unsqueeze(1), [128, NSEG, 32]), op=ALU.add),
             reads=("offGU_f", "cidx"), writes=("offGU_f",))
        S.op("dve", lambda e: e.tensor_copy(out=offGU_i[:], in_=offGU_f[:]), reads=("offGU_f",), writes=("offGU_i",))
        S.op("dve", lambda e: e.tensor_scalar_mul(out=offD_f[:], in0=bc(Gs[:].unsqueeze(2), [128, NSEG, 16]), scalar1=2048.0),
             reads=("Gs",), writes=("offD_f",))
        S.op("dve", lambda e: e.tensor_tensor(out=offD_f[:], in0=offD_f[:], in1=bc(cD_sb[:].unsqueeze(1), [128, NSEG, 16]), op=ALU.add),
             reads=("offD_f", "cidx"), writes=("offD_f",))
        S.op("dve", lambda e: e.tensor_copy(out=offD_i[:], in_=offD_f[:]), reads=("offD_f",), writes=("offD_i",))
        for t in range(32):
            S.idma("pool", xs_sorted[:, :], bass.IndirectOffsetOnAxis(ap=slot_i[:, t:t + 1], axis=0), xn2_all[:, t, :], None,
                   reads=("slot_i", "xn2_%d" % (t // 4), "xs_sorted"), writes=("xs_sorted_s",), key="scat")
            S.idma("pool", ws_sorted[:, :], bass.IndirectOffsetOnAxis(ap=slot_i[:, t:t + 1], axis=0), wsel_all[:, t, :], None,
                   reads=("slot_i", "wsel_all", "ws_sorted"), writes=("ws_sorted_s",), key="scat")
        S.barrier()
        c1.close()

        c2 = ExitStack()
        xst = [sb("xst%d" % i, [128, 4, 1024], BF16, c2) for i in range(2)]
        wst = [sb("wst%d" % i, [128, 4, 4], F32, c2) for i in range(2)]
        hmTs = [sb("hmT%d" % i, [128, 8, NB], BF16, c2) for i in range(2)]
        cbc = [sb("cbc%d" % i, [128, 4, NB], BF16, c2) for i in range(2)]
        dgm = sb("dgm", [128, 4, 128], BF16, c2)
        actb = [sb("actb%d" % i, [128, NB], BF16, c2) for i in range(16)]
        wgu = [sb("wgu%d" % i, [128, 2, 8, 512], BF16, c2) for i in range(2)]
        NWD = 5
        wd = [sb("wd%d" % i, [128, 4, 1024], BF16, c2) for i in range(NWD)]
        sg = [sb("sg%d" % i, [128, NB], F32, c2) for i in range(2)]
        tt = [sb("tt%d" % i, [128, NB], BF16, c2) for i in range(2)]
        ysb = [sb("ysb%d" % i, [128, 4, 1024], F32, c2) for i in range(2)]
        w2d_g = w_gate.rearrange("e r n -> (e r) n")
        w2d_u = w_up.rearrange("e r n -> (e r) n")
        w2d_d = w_down.rearrange("e r n -> (e r) n")
        ectr = [0]
        for sgi in range(NSEG):
            pb2 = sgi % 2
            hmT, hk = hmTs[pb2], "hmT%d" % pb2
            xk2, wk2, ck2, yk2 = "xst%d" % pb2, "wst%d" % pb2, "cbc%d" % pb2, "ysb%d" % pb2
            S.dma("sp", xst[pb2][:], xs_sorted[NB * sgi:NB * sgi + NB, :].rearrange("(j p) d -> p j d", p=128), writes=(xk2,), key=xk2)
            S.dma("sp", wst[pb2][:], ws_sorted[NB * sgi:NB * sgi + NB, :].rearrange("(j p) c -> p j c", p=128), writes=(wk2,), key=wk2)
            for j in range(4):
                S.group("pe", [lambda e, j=j, c=c, pb2=pb2: e.transpose(out=tpv[:, c, 128 * j:128 * j + 128],
                                                                        in_=xst[pb2][:, j, 128 * c:128 * c + 128], identity=identb[:])
                               for c in range(8)], reads=(xk2, "identb"), writes=TPK)
            for c in range(8):
                S.op("act", lambda e, c=c, hmT=hmT: e.activation(out=hmT[:, c, :], in_=tpv[:, c, :], func=AF.Identity,
                                                                 scale=s2[:, c:c + 1], bias=mods[:, 24 + c, 0:1]),
                     reads=TPK + ("sc", "mods"), writes=(hk,))
            for j in range(4):
                S.op("dve", lambda e, j=j, pb2=pb2: e.tensor_tensor(out=dgm[:], in0=bc(identb[:].unsqueeze(1), [128, 4, 128]),
                                                                    in1=bc(wst[pb2][:, j, :].unsqueeze(2), [128, 4, 128]), op=ALU.mult),
                     reads=("identb", wk2), writes=("dgm",))
                S.group("pe", [lambda e: e.matmul(bank(4 + (j % 2)), lhsT=ones1[:], rhs=dgm[:], start=True, stop=True)],
                        reads=("dgm", "ones1"), writes=(pk(4 + (j % 2)),))
                S.op("act", lambda e, j=j, pb2=pb2: e.activation(out=cbc[pb2][:, :, 128 * j:128 * j + 128],
                                                                 in_=bank(4 + (j % 2)).rearrange("p (x t) -> p x t", t=128), func=AF.Copy),
                     reads=(pk(4 + (j % 2)),), writes=(ck2,))
            for el in range(4):
                si = ectr[0] % 2
                di = ectr[0] % NWD
                ectr[0] += 1
                gk, dk_ = "wgu%d" % si, "wd%d" % di
                for k in range(8):
                    S.idma("pool", wgu[si][:, 0, k, :], None, w2d_g[:, :],
                           bass.IndirectOffsetOnAxis(ap=offGU_i[:, sgi, 8 * el + k:8 * el + k + 1], axis=0), reads=("offGU_i",), writes=(gk,), key=gk)
                    S.idma("pool", wgu[si][:, 1, k, :], None, w2d_u[:, :],
                           bass.IndirectOffsetOnAxis(ap=offGU_i[:, sgi, 8 * el + k:8 * el + k + 1], axis=0), reads=("offGU_i",), writes=(gk,), key=gk)
                for f in range(4):
                    S.idma("pool", wd[di][:, f, :], None, w2d_d[:, :],
                           bass.IndirectOffsetOnAxis(ap=offD_i[:, sgi, 4 * el + f:4 * el + f + 1], axis=0), reads=("offD_i",), writes=(dk_,), key=dk_)
                for f in range(4):
                    u = 4 * el + f
                    pp = u % 2
                    S.group("pe", [lambda e, k=k, f=f, si=si, pp=pp, hmT=hmT: e.matmul(
                        bank(2 * pp), lhsT=wgu[si][:, 0, k, 128 * f:128 * f + 128], rhs=hmT[:, k, :],
                        start=(k == 0), stop=(k == 7)) for k in range(8)], reads=(hk, gk), writes=(pk(2 * pp),))
                    S.group("pe", [lambda e, k=k, f=f, si=si, pp=pp, hmT=hmT: e.matmul(
                        bank(2 * pp + 1), lhsT=wgu[si][:, 1, k, 128 * f:128 * f + 128], rhs=hmT[:, k, :],
                        start=(k == 0), stop=(k == 7)) for k in range(8)], reads=(hk, gk), writes=(pk(2 * pp + 1),))
                    S.op("act", lambda e, pp=pp: e.activation(out=sg[pp][:], in_=bank(2 * pp), func=AF.Silu),
                         reads=(pk(2 * pp),), writes=("sg%d" % pp,))
                    S.op("dve", lambda e, pp=pp: e.tensor_tensor(out=tt[pp][:], in0=bank(2 * pp + 1), in1=sg[pp][:], op=ALU.mult),
                         reads=(pk(2 * pp + 1), "sg%d" % pp), writes=("tt%d" % pp,))
                    S.op("dve", lambda e, pp=pp, u=u, el=el, pb2=pb2: e.tensor_tensor(out=actb[u][:], in0=tt[pp][:], in1=cbc[pb2][:, el, :],
                                                                                      op=ALU.mult),
                         reads=("tt%d" % pp, ck2), writes=("actb%d" % u,))
            dbase = ectr[0] - 4
            for tp_ in range(2):
                fns = []
                for u in range(16):
                    el, f = divmod(u, 4)
                    di = (dbase + el) % NWD
                    for jj in range(2):
                        j = 2 * tp_ + jj
                        for hh in range(2):
                            fns.append(lambda e, u=u, f=f, di=di, j=j, jj=jj, hh=hh: e.matmul(
                                bank(4 + 2 * jj + hh), lhsT=actb[u][:, 128 * j:128 * j + 128],
                                rhs=wd[di][:, f, 512 * hh:512 * hh + 512], start=(u == 0), stop=(u == 15)))
                S.group("pe", fns, reads=tuple("actb%d" % u for u in range(16)) + tuple("wd%d" % ((dbase + el) % NWD) for el in range(4)),
                        writes=("ps4", "ps5", "ps6", "ps7"))
                for jj in range(2):
                    j = 2 * tp_ + jj
                    for hh in range(2):
                        bk = 4 + 2 * jj + hh
                        S.op("dve", lambda e, bk=bk, hh=hh, j=j, pb2=pb2: e.tensor_tensor(
                            out=ysb[pb2][:, j, 512 * hh:512 * hh + 512], in0=bank(bk), in1=gt2b[:, 512 * hh:512 * hh + 512], op=ALU.mult),
                            reads=(pk(bk), "gt2b"), writes=(yk2,))
            S.dma("sp", y_sorted[NB * sgi:NB * sgi + NB, :].rearrange("(j p) d -> p j d", p=128), ysb[pb2][:],
                  reads=(yk2,), writes=("y_sorted",), key="y_sorted")
        S.barrier()
        c2.close()

        c3 = ExitStack()
        yg = [sb("yg%d" % i, [128, 4, 1024], F32, c3) for i in range(2)]
        x1b = [sb("x1b%d" % i, [128, 4, 1024], F32, c3) for i in range(2)]
        junkF = sb("junkF", [128, 1024], BF16, c3)
        ssF = sb("ssF", [128, 8, 4], F32, c3)
        rsF = sb("rsF", [128, 8, 4], F32, c3)
        for blk in range(8):
            pb2 = blk % 2
            yk3, xk3, sfk = "yg%d" % pb2, "x1b%d" % pb2, "ssF%d" % blk
            S.dma("sp", x1b[pb2][:], x1_scr[NB * blk:NB * blk + NB, :].rearrange("(j p) d -> p j d", p=128), writes=(xk3,), key=xk3)
            for j in range(4):
                S.idma("pool", yg[pb2][:, j, :], None, y_sorted[:, :], bass.IndirectOffsetOnAxis(ap=slot_i[:, 4 * blk + j:4 * blk + j + 1], axis=0),
                       reads=("slot_i",), writes=(yk3,), key=yk3)
            S.op("pool", lambda e, blk=blk: e.memset(ssF[:, blk, :], 0.0), writes=(sfk,))
            for j in range(4):
                S.op("dve", lambda e, j=j, pb2=pb2: e.tensor_tensor(out=x1b[pb2][:, j, :], in0=x1b[pb2][:, j, :], in1=yg[pb2][:, j, :], op=ALU.add),
                     reads=(xk3, yk3), writes=(xk3,))
                S.op("act", lambda e, j=j, pb2=pb2, blk=blk: e.activation(out=junkF[:], in_=x1b[pb2][:, j, :], func=AF.Square,
                                                                          accum_out=ssF[:, blk, j:j + 1]),
                     reads=(xk3,), writes=("junkF", sfk))
            S.op("act", lambda e, blk=blk: e.activation(out=rsF[:, blk, :], in_=ssF[:, blk, :], func=AF.Sqrt, bias=qtr[:, 1:2]),
                 reads=(sfk, "qtr"), writes=(sfk + "r",))
            S.op("dve", lambda e, blk=blk: e.reciprocal(out=rsF[:, blk, :], in_=rsF[:, blk, :]), reads=(sfk + "r",), writes=(sfk + "r",))
            for j in range(4):
                S.op("dve", lambda e, j=j, pb2=pb2, blk=blk: e.scalar_tensor_tensor(out=x1b[pb2][:, j, :], in0=x1b[pb2][:, j, :],
                                                                                    scalar=rsF[:, blk, j:j + 1], in1=gf32[:],
                                                                                    op0=ALU.mult, op1=ALU.mult),
                     reads=(xk3, sfk + "r", "gf32"), writes=(xk3,))
            S.dma("sp", out[NB * blk:NB * blk + NB, :].rearrange("(j p) d -> p j d", p=128), x1b[pb2][:],
                  reads=(xk3,), writes=("out%d" % blk,), key="out")
        S.barrier()
        c3.close()
        pcg.close()
    return nc


_NC_CACHE = {}


def _fm(v):
    v = np.asarray(v, np.float32).reshape(-1, 128)
    return np.ascontiguousarray(v.T)


def kernel(x, c, ctx, c_ctx, w_ada, b_ada, g_mix, w_in, b_in, conv_w, conv_b, ln_g, ln_b, w_pa,
           lru_conv_w, lru_conv_b, w_r_f, b_r_f, w_i_f, b_i_f, lam_f, w_r_b, b_r_b, w_i_b, b_i_b, lam_b,
           w_pb, w_o, g_ffn, w_grp, b_grp, w_er, b_er, w_gate, w_up, w_down, g_final):
    f = lambda a: np.ascontiguousarray(np.asarray(a, np.float32))
    x, c, ctx, c_ctx = f(x), f(c), f(ctx), f(c_ctx)
    B = x.shape[0]
    if "nc" not in _NC_CACHE:
        _NC_CACHE["nc"] = build_program()
    nc = _NC_CACHE["nc"]

    common = {
        "w_ada": f(w_ada[0]), "b_ada_fm": _fm(b_ada[0]),
        "b_ada_gt": f(np.broadcast_to(np.stack([b_ada[0][2048:3072], b_ada[0][5120:6144]])[None], (128, 2, 1024))),
        "w_in": f(w_in[0]), "b_in_fm": _fm(b_in[0]),
        "cb": _fm(conv_b[0]), "lng": _fm(ln_g[0]), "lnb": _fm(ln_b[0]),
        "w_pa": f(w_pa[0]), "w_pb": f(w_pb[0]), "w_o": f(w_o[0]),
        "lb": _fm(lru_conv_b[0]),
        "gmix": _fm(g_mix[0]), "gffn": _fm(g_ffn[0]),
        "gfin": f(np.broadcast_to(np.asarray(g_final, np.float32)[None], (128, 1024))),
        "w_rt": f(np.concatenate([w_grp[0], w_er[0]], axis=1)),
        "b_rt": f(np.broadcast_to(np.concatenate([b_grp[0], b_er[0]])[None], (128, 20))),
        "w_gate": f(w_gate[0]), "w_up": f(w_up[0]), "w_down": f(w_down[0]),
        "ident": np.eye(128, dtype=np.float32),
        "tri": np.triu(np.ones((128, 128), np.float32), 1),
        "cGU": (np.arange(4)[None, :, None] * 1024 + np.arange(8)[None, None, :] * 128 + np.arange(128)[:, None, None]).reshape(128, 32).astype(np.float32),
        "cD": (np.arange(4)[None, :, None] * 512 + np.arange(4)[None, None, :] * 128 + np.arange(128)[:, None, None]).reshape(128, 16).astype(np.float32),
        "sidx": np.broadcast_to(np.arange(12, dtype=np.float32)[None], (128, 12)).copy(),
    }
    cwn = np.asarray(conv_w[0], np.float32)
    lwn = np.asarray(lru_conv_w[0], np.float32)
    zero = np.zeros((1, 1024), np.float32)
    lw5_nat = np.concatenate([lwn, zero], axis=0)
    lw5_rev = lw5_nat[::-1]

    def fm3(a):
        T = a.shape[0]
        return np.ascontiguousarray(a.reshape(T, 8, 128).transpose(2, 1, 0))

    pf = (w_r_f[0], b_r_f[0], w_i_f[0], b_i_f[0], lam_f[0])
    pbk = (w_r_b[0], b_r_b[0], w_i_b[0], b_i_b[0], lam_b[0])

    def gates(P, Sd):
        wgs = np.stack([P[0], P[2], Sd[0], Sd[2]]).astype(np.float32)
        bgs = np.stack([np.asarray(t, np.float32) for t in (P[1], P[3], Sd[1], Sd[3])])
        bgs = np.ascontiguousarray(bgs.transpose(2, 0, 1))
        lams = np.stack([np.asarray(P[4], np.float32).reshape(8, 128), np.asarray(Sd[4], np.float32).reshape(8, 128)])
        lams = np.ascontiguousarray(lams.transpose(2, 0, 1))
        return f(wgs), bgs, lams

    per_half = []
    for half in range(2):
        if half == 0:
            wgs, bgs, lams = gates(pf, pbk)
            d = {"cw": fm3(cwn), "lw5": fm3(lw5_nat), "wg": wgs, "bg": bgs, "lam": lams}
        else:
            wgs, bgs, lams = gates(pbk, pf)
            d = {"cw": fm3(cwn[::-1]), "lw5": fm3(lw5_rev), "wg": wgs, "bg": bgs, "lam": lams}
        per_half.append(d)

    in_maps = []
    pad2 = np.zeros((2, 1024), np.float32)
    for b in range(B):
        for half in range(2):
            xs = x[b] if half == 0 else x[b, ::-1]
            cs_ = ctx[b] if half == 0 else ctx[b, ::-1]
            m = dict(common)
            m.update(per_half[half])
            m["xp"] = np.ascontiguousarray(np.concatenate([pad2, xs, pad2], axis=0))
            m["ctxp"] = np.ascontiguousarray(np.concatenate([pad2, cs_, pad2], axis=0))
            m["cvec"] = np.ascontiguousarray(np.stack([_fm(c[b]), _fm(c_ctx)], axis=-1))
            in_maps.append(m)
    res = run_bass_kernel_spmd(nc, in_maps, core_ids=list(range(2 * B)))
    outp = np.empty((B, 2 * NOWN, 1024), np.float32)
    for b in range(B):
        outp[b, :NOWN] = res.results[2 * b]["out"]
        outp[b, NOWN:] = res.results[2 * b + 1]["out"][::-1]
    if DEBUG:
        kernel.last = res
    return outp
```

```python
from contextlib import ExitStack
import os
import numpy as np
import concourse.bass as bass
import concourse.mybir as mybir
from concourse.bass_utils import run_bass_kernel_spmd

F32 = mybir.dt.float32
BF16 = mybir.dt.bfloat16
AF = mybir.ActivationFunctionType
ALU = mybir.AluOpType
AX = mybir.AxisListType
EPS = 1e-6
NB = 512
NOWN = 4096
DEBUG = bool(int(os.environ.get("MK_DEBUG", "0")))


class _Rec:
    def __init__(self):
        self.calls = []

    def __getattr__(self, name):
        def f(*args, **kw):
            self.calls.append((name, args, kw))
            return self
        return f


_TBL = {"Exp": "exp", "Tanh": None, "Identity": None, "Copy": None, "Square": None, "Sqrt": "sqrt", "Silu": "silu",
        "Gelu_apprx_tanh": "gelu", "Ln": "ln"}


def _fsize(ap):
    n = 1
    for d in ap.shape[1:]:
        n *= int(d)
    return n


class Sched:
    REORDER = True
    WINDOW = 600

    def __init__(self, nc, es):
        self.nc = nc
        self.es = es
        self.E = dict(pe=nc.tensor, act=nc.scalar, dve=nc.vector, pool=nc.gpsimd, sp=nc.sync)
        self.sem = {e: es.enter_context(nc.semaphore("c_" + e)) for e in self.E}
        self.cnt = {e: 0 for e in self.E}
        self.seen = {e: {} for e in self.E}
        self.lastw = {}
        self.readers = {}
        self.dsem = {}
        self.dcnt = {}
        self.ops = []

    def op(self, e, fn, reads=(), writes=()):
        r = _Rec()
        fn(r)
        self._add(e, "op", r.calls, tuple(reads), tuple(writes), None)

    def group(self, e, fns, reads=(), writes=()):
        r = _Rec()
        for f in fns:
            f(r)
        self._add(e, "op", r.calls, tuple(reads), tuple(writes), None)

    def dma(self, q, out, in_, reads=(), writes=(), key=None):
        self._add(q, "dma", [("dma_start", (), dict(out=out, in_=in_))], tuple(reads), tuple(writes), key)

    def idma(self, q, out, out_offset, in_, in_offset, reads=(), writes=(), key=None):
        self._add(q, "dma", [("indirect_dma_start", (), dict(out=out, out_offset=out_offset, in_=in_, in_offset=in_offset))],
                  tuple(reads), tuple(writes), key)

    def _add(self, e, kind, calls, reads, writes, key):
        dur = 0.0
        tbl = None
        if kind == "dma":
            kw0 = calls[0][2]
            side = kw0["in_"] if kw0.get("out_offset") is not None else kw0["out"]
            nb = 128 * _fsize(side) * 4
            dur = 1000.0 if e == "pool" else 150.0
            lat = 2000.0 + nb / 300.0
        else:
            lat = 0.0
            for (name, args, kw) in calls:
                if e == "pe":
                    src = kw.get("rhs", kw.get("in_"))
                    dur += 25.0 + 0.5 * max(_fsize(src), 64)
                else:
                    oap = kw.get("out", kw.get("ap", args[0] if args else None))
                    n = _fsize(oap)
                    if e == "act":
                        dur += 250.0 + 0.73 * n
                        fnm = kw.get("func")
                        tbl = _TBL.get(getattr(fnm, "name", str(fnm)), None) if fnm is not None else None
                    elif e == "dve":
                        dur += 160.0 + 1.04 * n
                    else:
                        dur += 300.0 + 3.1 * n
        self.ops.append(dict(e=e, kind=kind, calls=calls, reads=reads, writes=writes, key=key, dur=dur, lat=lat, tbl=tbl))

    def flush(self):
        ops = self.ops
        self.ops = []
        n = len(ops)
        if n == 0:
            return
        lastw, readers = {}, {}
        preds = [None] * n
        succs = [[] for _ in range(n)]
        for i, o in enumerate(ops):
            p = set()
            for k in o["reads"]:
                if k in lastw:
                    p.add(lastw[k])
            for k in o["writes"]:
                if k in lastw:
                    p.add(lastw[k])
                p.update(readers.get(k, ()))
            p.discard(i)
            preds[i] = p
            for j in p:
                succs[j].append(i)
            for k in o["reads"]:
                readers.setdefault(k, []).append(i)
            for k in o["writes"]:
                lastw[k] = i
                readers[k] = []
        if not self.REORDER:
            order = range(n)
        else:
            indeg = [len(p) for p in preds]
            ready = [i for i in range(n) if indeg[i] == 0]
            finish = [0.0] * n
            efree = {e: 0.0 for e in self.E}
            etbl = [None]
            done = [False] * n
            lo = 0
            order = []
            while len(order) < n:
                while lo < n and done[lo]:
                    lo += 1
                best, bkey = None, None
                for i in ready:
                    if i > lo + self.WINDOW:
                        continue
                    o = ops[i]
                    st = efree[o["e"]]
                    for j in preds[i]:
                        f = finish[j] + (0.0 if ops[j]["e"] == o["e"] else 120.0)
                        if f > st:
                            st = f
                    if o["e"] == "act" and o["tbl"] is not None and o["tbl"] != etbl[0]:
                        st += 1300.0
                    kk = (st, i)
                    if bkey is None or kk < bkey:
                        best, bkey = i, kk
                i = best
                o = ops[i]
                st = bkey[0]
                if os.environ.get("MK_TL") and n > 3000 and len(ops) == int(os.environ.get("MK_TL")):
                    lim = None
                    for j in preds[i]:
                        f = finish[j]
                        if lim is None or f > lim[0]:
                            lim = (f, j)
                    gap = st - efree[o["e"]]
                    if o["e"] == "pe" and gap > 300:
                        print("PE gap %.1fus at t=%.1fus op#%d writes=%s waits for %s op#%d writes=%s" % (
                            gap / 1e3, st / 1e3, i, o["writes"][:2], ops[lim[1]]["e"], lim[1], ops[lim[1]]["writes"][:2]))
                if o["e"] == "act" and o["tbl"] is not None:
                    etbl[0] = o["tbl"]
                efree[o["e"]] = st + o["dur"]
                finish[i] = st + o["dur"] + o["lat"]
                done[i] = True
                ready.remove(i)
                order.append(i)
                for j in succs[i]:
                    indeg[j] -= 1
                    if indeg[j] == 0:
                        ready.append(j)
        if self.REORDER and os.environ.get("MK_STATS"):
            busy = {e: 0.0 for e in self.E}
            for o in ops:
                busy[o["e"]] += o["dur"]
            print("phase: n=%d est_makespan=%.0fus busy(us): %s" % (n, max(finish) / 1e3, {e: int(v / 1e3) for e, v in busy.items()}))
        for i in order:
            self._emit(ops[i])

    def _wait(self, e, tok, same_ok=False):
        if tok is None:
            return
        name, sem, val, src = tok
        if same_ok and src == e:
            return
        d = self.seen[e]
        if d.get(name, 0) >= val:
            return
        self.E[e].wait_ge(sem, val)
        d[name] = val

    def _emit(self, o):
        e, reads, writes = o["e"], o["reads"], o["writes"]
        for k in reads:
            self._wait(e, self.lastw.get(k))
        for k in writes:
            self._wait(e, self.lastw.get(k), same_ok=True)
            for t in self.readers.get(k, {}).values():
                self._wait(e, t, same_ok=True)
        ins = None
        for (name, args, kw) in o["calls"]:
            ins = getattr(self.E[e], name)(*args, **kw)
        if o["kind"] == "dma":
            key = o["key"]
            if key not in self.dsem:
                self.dsem[key] = self.es.enter_context(self.nc.semaphore("d_" + key))
                self.dcnt[key] = 0
            self.dcnt[key] += 16
            ins.then_inc(self.dsem[key], 16)
            tok = ("d_" + key, self.dsem[key], self.dcnt[key], "dma")
        else:
            self.cnt[e] += 1
            ins.then_inc(self.sem[e], 1)
            tok = ("c_" + e, self.sem[e], self.cnt[e], e)
        for k in reads:
            self.readers.setdefault(k, {})[tok[0]] = tok
        for k in writes:
            self.lastw[k] = tok
            self.readers[k] = {}

    def barrier(self):
        self.flush()
        for e in self.E:
            for e2 in self.E:
                if self.cnt[e2] > 0:
                    self._wait(e, ("c_" + e2, self.sem[e2], self.cnt[e2], e2))
            for k, sem in self.dsem.items():
                self._wait(e, ("d_" + k, sem, self.dcnt[k], "dma"))


def build_program():
    nc = bass.Bass("TRN2", target_bir_lowering=False)

    def din(name, shape):
        return nc.dram_tensor(name, list(shape), F32, kind="ExternalInput").ap()

    xp = din("xp", [8196, 1024])
    ctxp = din("ctxp", [260, 1024])
    cvec = din("cvec", [128, 8, 2])
    w_ada = din("w_ada", [1024, 6144])
    b_ada_fm = din("b_ada_fm", [128, 48])
    b_ada_gt = din("b_ada_gt", [128, 2, 1024])
    w_in = din("w_in", [1024, 6144])
    b_in_fm = din("b_in_fm", [128, 48])
    cw = din("cw", [128, 8, 31])
    cb = din("cb", [128, 8])
    lng = din("lng", [128, 8])
    lnb = din("lnb", [128, 8])
    w_pa = din("w_pa", [1024, 1024])
    w_pb = din("w_pb", [1024, 1024])
    w_o = din("w_o", [1024, 1024])
    lw5 = din("lw5", [128, 8, 5])
    lb = din("lb", [128, 8])
    wg = din("wg", [4, 8, 128, 128])
    bg = din("bg", [128, 4, 8])
    lam = din("lam", [128, 2, 8])
    gmix = din("gmix", [128, 8])
    gffn = din("gffn", [128, 8])
    gfin = din("gfin", [128, 1024])
    w_rt = din("w_rt", [1024, 20])
    b_rt = din("b_rt", [128, 20])
    w_gate = din("w_gate", [2048, 4096])
    w_up = din("w_up", [2048, 4096])
    w_down = din("w_down", [2048, 4096])
    ident = din("ident", [128, 128])
    tri = din("tri", [128, 128])
    cE = din("cE", [128, 4])
    sidx = din("sidx", [128, 12])
    out = nc.dram_tensor("out", [NOWN, 1024], F32, kind="ExternalOutput").ap()
    if DEBUG:
        hs_scr = nc.dram_tensor("hs_scr", [8, 128, 8, NB], BF16, kind="ExternalOutput").ap()
        x1_scr = nc.dram_tensor("x1_scr", [NOWN, 1024], F32, kind="ExternalOutput").ap()
    else:
        hs_scr = nc.dram_tensor("hs_scr", [8, 128, 8, NB], BF16, kind="Internal").ap()
        x1_scr = nc.dram_tensor("x1_scr", [NOWN, 1024], F32, kind="Internal").ap()
    gt2_scr = nc.dram_tensor("gt2_scr", [128, 1024], F32, kind="Internal").ap()
    NSEG = 12
    xs_sorted = nc.dram_tensor("xs_sorted", [NSEG * NB, 1024], BF16, kind="Internal").ap()
    ws_sorted = nc.dram_tensor("ws_sorted", [NSEG * NB, 4], F32, kind="Internal").ap()
    y_sorted = nc.dram_tensor("y_sorted", [NSEG * NB, 1024], F32, kind="Internal").ap()
    I32 = mybir.dt.int32

    with ExitStack() as es:
        S = Sched(nc, es)

        def sb(name, shape, dt=F32, stack=es):
            return stack.enter_context(nc.sbuf_tensor(name, list(shape), dt))

        psA = es.enter_context(nc.psum_tensor("psA", [128, 2048], F32))
        psB = es.enter_context(nc.psum_tensor("psB", [128, 2048], F32))

        def bank(i):
            t = psA if i < 4 else psB
            return t[:, 512 * (i % 4):512 * (i % 4) + 512]

        def pk(i):
            return "ps%d" % i

        tpv = psA[:, :].bitcast(BF16).rearrange("p (c t) -> p c t", t=512)
        TPK = ("ps0", "ps1", "ps2", "ps3")

        identb = sb("identb", [128, 128], BF16)
        ones_m = sb("ones_m", [128, 128], BF16)
        ones1 = sb("ones1", [128, 128], BF16)
        b_in_sb = sb("b_in_sb", [128, 48])
        hb_in = sb("hb_in", [128, 48])
        cw_sb = sb("cw_sb", [128, 8, 31])
        cb_sb = sb("cb_sb", [128, 8])
        lng_sb = sb("lng_sb", [128, 8])
        lnb_sb = sb("lnb_sb", [128, 8])
        lw5_sb = sb("lw5_sb", [128, 8, 5])
        lb_sb = sb("lb_sb", [128, 8])
        bg_sb = sb("bg_sb", [128, 4, 8])
        hbg = sb("hbg", [128, 4, 8])
        lam_sb = sb("lam_sb", [128, 2, 8])
        gmix_sb = sb("gmix_sb", [128, 8])
        gffn_sb = sb("gffn_sb", [128, 8])
        b_rt_sb = sb("b_rt_sb", [128, 20])
        b_ada_fm_sb = sb("b_ada_fm_sb", [128, 48])
        cvec_sb = sb("cvec_sb", [128, 8, 2])
        mods = sb("mods", [128, 48, 2])
        s1 = sb("s1", [128, 8])
        s1c = sb("s1c", [128, 8])
        s2 = sb("s2", [128, 8])
        gt1h = sb("gt1h", [128, 1024])
        cl = sb("cl", [128, 2, 8])
        hcl = sb("hcl", [128, 2, 8])
        state = sb("state", [128, 2, 8])
        ss = sb("ss", [128, 8])
        rs = sb("rs", [128, 8])
        w_rt_b = sb("w_rt_b", [128, 8, 20], BF16)
        qtr = sb("qtr", [128, 4], F32)

        def pload(t, src):
            S.dma("sp", t, src, writes=("params",), key="params")

        pload(b_in_sb[:], b_in_fm)
        pload(cw_sb[:], cw)
        pload(cb_sb[:], cb)
        pload(lng_sb[:], lng)
        pload(lnb_sb[:], lnb)
        pload(lw5_sb[:], lw5)
        pload(lb_sb[:], lb)
        pload(bg_sb[:], bg)
        pload(lam_sb[:], lam)
        pload(gmix_sb[:], gmix)
        pload(gffn_sb[:], gffn)
        pload(b_rt_sb[:], b_rt)
        pload(b_ada_fm_sb[:], b_ada_fm)
        pload(cvec_sb[:], cvec)
        S.dma("pool", identb[:], ident, writes=("identb",), key="identb")
        S.dma("pool", w_rt_b[:], w_rt.rearrange("(k p) n -> p k n", p=128), writes=("w_rt_b",), key="w_rt_b")
        S.op("pool", lambda e: e.memset(ones_m[:], 1.0 / 1024.0), writes=("ones_m",))
        S.op("pool", lambda e: e.memset(ones1[:], 1.0), writes=("ones1",))
        S.op("pool", lambda e: e.memset(qtr[:, 0:1], 0.25), writes=("qtr",))
        S.op("pool", lambda e: e.memset(qtr[:, 1:2], 1024.0 * EPS), writes=("qtr",))
        S.op("pool", lambda e: e.memset(qtr[:, 2:3], EPS), writes=("qtr",))
        S.op("pool", lambda e: e.memset(state[:], 0.0), writes=("state",))
        S.op("pool", lambda e: e.memset(ss[:], 0.0), writes=("ss",))

        with ExitStack() as p0:
            cs = sb("cs", [128, 8, 2], BF16, p0)
            cs_rep = sb("cs_rep", [128, 8, 128], BF16, p0)
            b_ada_gt_sb = sb("b_ada_gt_sb", [128, 2, 1024], F32, p0)
            wa = [sb("wa%d" % i, [128, 8, 512], BF16, p0) for i in range(3)]
            e_t = sb("e_t", [128, 16], F32, p0)
            t_t = sb("t_t", [128, 16], F32, p0)
            l_t = sb("l_t", [128, 16], F32, p0)
            m_t = sb("m_t", [128, 16], F32, p0)
            pload(b_ada_gt_sb[:], b_ada_gt)
            gt2b = sb("gt2b0", [128, 1024], F32, p0)

            S.op("act", lambda e: e.activation(out=cs[:], in_=cvec_sb[:], func=AF.Silu), reads=("params",), writes=("cs",))
            S.op("dve", lambda e: e.tensor_copy(out=cs_rep[:], in_=cs[:, :, 0:1].to_broadcast([128, 8, 128])),
                 reads=("cs",), writes=("cs_rep",))
            psm = bank(0)[:, 0:96].rearrange("p (j t) -> p j t", t=2)
            for q in range(12):
                s = q % 3
                S.dma("pool", wa[s][:], w_ada[:, 512 * q:512 * q + 512].rearrange("(k p) n -> p k n", p=128),
                      writes=("wa%d" % s,), key="wa%d" % s)
                fns = []
                for jj in range(4):
                    for k in range(8):
                        fns.append(lambda e, jj=jj, k=k, s=s, q=q: e.matmul(
                            psm[:, 4 * q + jj, :], lhsT=wa[s][:, k, 128 * jj:128 * jj + 128], rhs=cs[:, k, :],
                            start=(k == 0), stop=(k == 7)))
                S.group("pe", fns, reads=("wa%d" % s, "cs"), writes=("ps0",))
                if q in (4, 5, 10, 11):
                    bk = 1 + (q % 2)
                    S.group("pe", [lambda e, k=k, s=s, bk=bk: e.matmul(bank(bk), lhsT=cs_rep[:, k, :], rhs=wa[s][:, k, :],
                                                                      start=(k == 0), stop=(k == 7)) for k in range(8)],
                            reads=("wa%d" % s, "cs_rep"), writes=(pk(bk),))
                    dst = gt1h if q < 6 else gt2b
                    gi = 0 if q < 6 else 1
                    cols = slice(512 * (q % 2), 512 * (q % 2) + 512)
                    S.op("dve", lambda e, dst=dst, gi=gi, cols=cols, bk=bk: e.tensor_tensor(
                        out=dst[:, cols], in0=bank(bk), in1=b_ada_gt_sb[:, gi, cols], op=ALU.add),
                        reads=(pk(bk), "params"), writes=("gt",))
            S.op("dve", lambda e: e.tensor_scalar_mul(out=gt1h[:], in0=gt1h[:], scalar1=0.5), reads=("gt",), writes=("gt",))
            S.dma("sp", gt2_scr, gt2b[:], reads=("gt",), writes=("gt2_scr",), key="gt2_scr")
            S.op("dve", lambda e: e.tensor_tensor(out=mods[:], in0=psm, in1=b_ada_fm_sb[:].unsqueeze(2).to_broadcast([128, 48, 2]),
                                                  op=ALU.add), reads=("ps0", "params"), writes=("mods",))
            for (dst, col, j0, gsb) in ((s1, 0, 8, gmix_sb), (s1c, 1, 8, gmix_sb), (s2, 0, 32, gffn_sb)):
                S.op("dve", lambda e, dst=dst, col=col, j0=j0, gsb=gsb: e.scalar_tensor_tensor(
                    out=dst[:], in0=mods[:, j0:j0 + 8, col], scalar=1.0, in1=gsb[:], op0=ALU.add, op1=ALU.mult),
                    reads=("mods", "params"), writes=("sc",))
                S.op("dve", lambda e, dst=dst: e.tensor_scalar_mul(out=dst[:], in0=dst[:], scalar1=32.0),
                     reads=("sc",), writes=("sc",))
            S.op("dve", lambda e: e.tensor_scalar_mul(out=hb_in[:], in0=b_in_sb[:], scalar1=0.5), reads=("params",), writes=("hb_in",))
            S.op("dve", lambda e: e.tensor_scalar_mul(out=hbg[:], in0=bg_sb[:], scalar1=0.5), reads=("params",), writes=("hbg",))
            lamf = lam_sb[:].rearrange("p a b -> p (a b)")
            S.op("act", lambda e: e.activation(out=e_t[:], in_=lamf, func=AF.Exp, scale=-1.0), reads=("params",), writes=("e_t",))
            S.op("dve", lambda e: e.tensor_scalar(out=t_t[:], in0=e_t[:], scalar1=-0.25, scalar2=1.0 / 3.0, op0=ALU.mult, op1=ALU.add),
                 reads=("e_t",), writes=("t_t",))
            S.op("dve", lambda e: e.tensor_tensor(out=t_t[:], in0=t_t[:], in1=e_t[:], op=ALU.mult), reads=("t_t", "e_t"), writes=("t_t",))
            S.op("dve", lambda e: e.tensor_scalar_add(out=t_t[:], in0=t_t[:], scalar1=-0.5), reads=("t_t",), writes=("t_t",))
            S.op("dve", lambda e: e.tensor_tensor(out=t_t[:], in0=t_t[:], in1=e_t[:], op=ALU.mult), reads=("t_t", "e_t"), writes=("t_t",))
            S.op("dve", lambda e: e.tensor_scalar_add(out=t_t[:], in0=t_t[:], scalar1=1.0), reads=("t_t",), writes=("t_t",))
            S.op("dve", lambda e: e.tensor_tensor(out=t_t[:], in0=t_t[:], in1=e_t[:], op=ALU.mult), reads=("t_t", "e_t"), writes=("t_t",))
            S.op("dve", lambda e: e.tensor_scalar_add(out=l_t[:], in0=e_t[:], scalar1=1.0), reads=("e_t",), writes=("l_t",))
            S.op("act", lambda e: e.activation(out=l_t[:], in_=l_t[:], func=AF.Ln), reads=("l_t",), writes=("l_t",))
            S.op("dve", lambda e: e.tensor_single_scalar(out=m_t[:], in_=e_t[:], scalar=0.1, op=ALU.is_lt), reads=("e_t",), writes=("m_t",))
            S.op("dve", lambda e: e.tensor_tensor(out=t_t[:], in0=t_t[:], in1=l_t[:], op=ALU.subtract), reads=("t_t", "l_t"), writes=("t_t",))
            S.op("dve", lambda e: e.tensor_tensor(out=t_t[:], in0=t_t[:], in1=m_t[:], op=ALU.mult), reads=("t_t", "m_t"), writes=("t_t",))
            S.op("dve", lambda e: e.tensor_tensor(out=t_t[:], in0=t_t[:], in1=l_t[:], op=ALU.add), reads=("t_t", "l_t"), writes=("t_t",))
            clf = cl[:].rearrange("p a b -> p (a b)")
            hclf = hcl[:].rearrange("p a b -> p (a b)")
            S.op("dve", lambda e: e.tensor_scalar_mul(out=clf, in0=t_t[:], scalar1=-8.0), reads=("t_t",), writes=("cl",))
            S.op("dve", lambda e: e.tensor_scalar_mul(out=hclf, in0=t_t[:], scalar1=-4.0), reads=("t_t",), writes=("cl",))
            S.barrier()

        mixer = ExitStack()
        wxr = sb("wxr", [128, 8, 1024], BF16, mixer)
        wgb = sb("wgb", [128, 4, 8, 128], BF16, mixer)
        dg5 = sb("dg5", [128, 8, 5, 128], BF16, mixer)
        S.dma("pool", wxr[:], w_in[:, 3072:4096].rearrange("(k p) n -> p k n", p=128), writes=("wxr",), key="wxr")
        S.dma("pool", wgb[:], wg.rearrange("g h p n -> p g h n"), writes=("wgb",), key="wgb")
        for c in range(8):
            S.op("dve", lambda e, c=c: e.tensor_tensor(
                out=dg5[:, c, :, :], in0=identb[:].unsqueeze(1).to_broadcast([128, 5, 128]),
                in1=lw5_sb[:, c, :].unsqueeze(2).to_broadcast([128, 5, 128]), op=ALU.mult),
                reads=("identb", "params"), writes=("dg5",))

        xt = sb("xt", [128, 4, 1024], F32, mixer)
        xh = sb("xh", [4, 1024], F32, mixer)
        xn = sb("xn", [128, 4, 1024], BF16, mixer)
        xnh = sb("xnh", [4, 1024], BF16, mixer)
        hxTs = [sb("hxT%d" % i, [128, 8, NB + 4], BF16, mixer) for i in range(2)]
        hxT, hxk, hxhk = hxTs[0], "hxT0", "hxTh0"
        hbc = [0]
        xrp = [sb("xrp%d" % i, [128, NB + 4], BF16, mixer) for i in range(2)]
        xcb = [sb("xcb%d" % i, [128, NB], BF16, mixer) for i in range(2)]
        tr = sb("tr", [128, NB], F32, mixer)
        ti = sb("ti", [128, NB], F32, mixer)
        a4 = sb("a4", [128, 4, NB], F32, mixer)
        s4 = sb("s4", [128, 4, NB], F32, mixer)
        t4 = sb("t4", [128, 4, NB], BF16, mixer)
        tmp1 = sb("tmp1", [128, NB], F32, mixer)
        bb_t = sb("bb_t", [128, NB], F32, mixer)
        hf = sb("hf", [128, NB], F32, mixer)
        hsb = sb("hsb", [128, 8, NB], BF16, mixer)

        tph = bank(4).bitcast(BF16)[:, 0:32].rearrange("p (c t) -> p c t", t=4)

        def prep(xsrc, r0, N, sc, bcol, keep_key):
            nt = N // 128
            S.dma("sp", xt[:, 0:nt, :], xsrc[r0:r0 + N, :].rearrange("(j p) d -> p j d", p=128), writes=(keep_key,), key="xt")
            S.dma("sp", xh[0:2, :], xsrc[r0 - 2:r0, :], writes=("xh",), key="xh")
            S.dma("sp", xh[2:4, :], xsrc[r0 + N:r0 + N + 2, :], writes=("xh",), key="xh")
            S.op("pool", lambda e: e.memset(ss[:], 0.0), writes=("ss",))
            for j in range(nt):
                S.op("act", lambda e, j=j: e.activation(out=xn[:, j, :], in_=xt[:, j, :], func=AF.Square, accum_out=ss[:, j:j + 1]),
                     reads=(keep_key,), writes=("xn", "ss"))
            S.op("act", lambda e: e.activation(out=xnh[:], in_=xh[:], func=AF.Square, accum_out=ss[0:4, 4:5]),
                 reads=("xh",), writes=("xnh", "ss"))
            S.op("act", lambda e: e.activation(out=rs[:, 0:5], in_=ss[:, 0:5], func=AF.Sqrt, bias=qtr[:, 1:2]), reads=("ss", "qtr"), writes=("rs",))
            S.op("dve", lambda e: e.reciprocal(out=rs[:, 0:5], in_=rs[:, 0:5]), reads=("rs",), writes=("rs",))
            for j in range(nt):
                S.op("dve", lambda e, j=j: e.tensor_scalar_mul(out=xn[:, j, :], in0=xt[:, j, :], scalar1=rs[:, j:j + 1]),
                     reads=(keep_key, "rs"), writes=("xn",))
            S.op("dve", lambda e: e.tensor_scalar_mul(out=xnh[:], in0=xh[:], scalar1=rs[0:4, 4:5]),
                 reads=("xh", "rs"), writes=("xnh",))
            for j in range(nt):
                S.group("pe", [lambda e, j=j, c=c: e.transpose(out=tpv[:, c, 128 * j:128 * j + 128],
                                                               in_=xn[:, j, 128 * c:128 * c + 128], identity=identb[:])
                               for c in range(8)], reads=("xn", "identb"), writes=TPK)
            S.group("pe", [lambda e, c=c: e.transpose(out=tph[:, c, :], in_=xnh[:, 128 * c:128 * c + 128], identity=identb[0:4, 0:4])
                           for c in range(8)], reads=("xnh", "identb"), writes=("ps4",))
            for c in range(8):
                S.op("act", lambda e, c=c: e.activation(out=hxT[:, c, 0:N], in_=tpv[:, c, 0:N], func=AF.Identity,
                                                        scale=sc[:, c:c + 1], bias=mods[:, c, bcol:bcol + 1]),
                     reads=TPK + ("sc", "mods"), writes=(hxk,))
            S.op("dve", lambda e: e.tensor_tensor(out=hxT[:, :, N:N + 4], in0=tph, in1=sc[:].unsqueeze(2).to_broadcast([128, 8, 4]),
                                                  op=ALU.mult), reads=("ps4", "sc"), writes=(hxhk,))
            S.op("dve", lambda e: e.tensor_tensor(out=hxT[:, :, N:N + 4], in0=hxT[:, :, N:N + 4],
                                                  in1=mods[:, 0:8, bcol:bcol + 1].to_broadcast([128, 8, 4]), op=ALU.add),
                 reads=(hxhk, "mods"), writes=(hxhk,))

        def rglru_block(N, d, reverse, has_lo, has_hi, consumer=None):
            def st1(c):
                par = c % 2
                bxr = bank(par)[:, 0:N]
                bxh = bank(2 + par)[:, 0:4]
                S.group("pe", [lambda e, k=k: e.matmul(bxr, lhsT=wxr[:, k, 128 * c:128 * c + 128], rhs=hxT[:, k, 0:N],
                                                       start=(k == 0), stop=(k == 7)) for k in range(8)],
                        reads=(hxk, "wxr"), writes=(pk(par),))
                S.group("pe", [lambda e, k=k: e.matmul(bxh, lhsT=wxr[:, k, 128 * c:128 * c + 128], rhs=hxT[:, k, N:N + 4],
                                                       start=(k == 0), stop=(k == 7)) for k in range(8)],
                        reads=(hxhk, "wxr"), writes=(pk(2 + par),))
                xk = "xrp%d" % par
                bia = b_in_sb[:, 24 + c:25 + c]
                S.op("act", lambda e: e.activation(out=xrp[par][:, 2:2 + N], in_=bxr, func=AF.Identity, bias=bia),
                     reads=(pk(par), "params"), writes=(xk,))
                if has_lo:
                    S.op("dve", lambda e: e.tensor_scalar_add(out=xrp[par][:, 0:2], in0=bxh[:, 0:2], scalar1=bia),
                         reads=(pk(2 + par), "params"), writes=(xk,))
                else:
                    S.op("dve", lambda e: e.memset(xrp[par][:, 0:2], 0.0), writes=(xk,))
                if has_hi:
                    S.op("dve", lambda e: e.tensor_scalar_add(out=xrp[par][:, 2 + N:4 + N], in0=bxh[:, 2:4], scalar1=bia),
                         reads=(pk(2 + par), "params"), writes=(xk,))
                else:
                    S.op("dve", lambda e: e.memset(xrp[par][:, 2 + N:4 + N], 0.0), writes=(xk,))

            def st2(c):
                par = c % 2
                xk = "xrp%d" % par
                bcv = bank(4 + par)[:, 0:N]
                S.group("pe", [lambda e, j=j: e.matmul(bcv, lhsT=dg5[:, c, j, :], rhs=xrp[par][:, j:j + N],
                                                       start=(j == 0), stop=(j == 4)) for j in range(5)],
                        reads=(xk, "dg5"), writes=(pk(4 + par),))
                S.op("dve", lambda e: e.tensor_scalar_add(out=xcb[par][:, 0:N], in0=bcv, scalar1=lb_sb[:, c:c + 1]),
                     reads=(pk(4 + par), "params"), writes=("xcb%d" % par,))

            def st3(c):
                par = c % 2
                q = c % 4
                ck = "xcb%d" % par
                br_ = bank(6)[:, 0:N]
                bi_ = bank(7)[:, 0:N]
                S.group("pe", [lambda e: e.matmul(br_, lhsT=wgb[:, 2 * d, c, :], rhs=xcb[par][:, 0:N], start=True, stop=True)],
                        reads=(ck, "wgb"), writes=("ps6",))
                S.group("pe", [lambda e: e.matmul(bi_, lhsT=wgb[:, 2 * d + 1, c, :], rhs=xcb[par][:, 0:N], start=True, stop=True)],
                        reads=(ck, "wgb"), writes=("ps7",))
                S.op("act", lambda e: e.activation(out=tr[:, 0:N], in_=br_, func=AF.Tanh, scale=0.5, bias=hbg[:, 2 * d, c:c + 1]),
                     reads=("ps6", "hbg"), writes=("tr",))
                S.op("act", lambda e: e.activation(out=ti[:, 0:N], in_=bi_, func=AF.Tanh, scale=0.5, bias=hbg[:, 2 * d + 1, c:c + 1]),
                     reads=("ps7", "hbg"), writes=("ti",))
                S.op("act", lambda e: e.activation(out=a4[:, q, 0:N], in_=tr[:, 0:N], func=AF.Exp, scale=hcl[:, d, c:c + 1],
                                                   bias=hcl[:, d, c:c + 1]), reads=("tr", "cl"), writes=("a4_%d" % q,))
                S.op("pool", lambda e: e.tensor_tensor(out=s4[:, q, 0:N], in0=a4[:, q, 0:N], in1=a4[:, q, 0:N], op=ALU.mult),
                     reads=("a4_%d" % q,), writes=("s4_%d" % q,))
                S.op("dve", lambda e: e.scalar_tensor_tensor(out=t4[:, q, 0:N], in0=ti[:, 0:N], scalar=1.0, in1=xcb[par][:, 0:N],
                                                             op0=ALU.add, op1=ALU.mult), reads=("ti", ck), writes=("t4_%d" % q,))

            def st4(c0):
                sk = tuple("s4_%d" % q for q in range(4))
                S.op("act", lambda e: e.activation(out=s4[:, :, 0:N], in_=s4[:, :, 0:N], func=AF.Sqrt, scale=-0.25, bias=qtr[:, 0:1]),
                     reads=sk + ("qtr",), writes=sk)
                for c in range(c0, c0 + 4):
                    q = c % 4
                    S.op("dve", lambda e, q=q: e.tensor_tensor(out=bb_t[:, 0:N], in0=s4[:, q, 0:N], in1=t4[:, q, 0:N], op=ALU.mult),
                         reads=("s4_%d" % q, "t4_%d" % q), writes=("bb_t",))
                    if reverse:
                        S.op("dve", lambda e, q=q, c=c: e.tensor_tensor_scan(
                            out=hf[:, 0:N][:, ::-1], data0=a4[:, q, 0:N][:, ::-1], data1=bb_t[:, 0:N][:, ::-1],
                            initial=state[:, d, c:c + 1], op0=ALU.mult, op1=ALU.add),
                            reads=("a4_%d" % q, "bb_t", "state"), writes=("hf",))
                        S.op("pool", lambda e, c=c: e.tensor_copy(out=state[:, d, c:c + 1], in_=hf[:, 0:1]),
                             reads=("hf",), writes=("state",))
                    else:
                        S.op("dve", lambda e, q=q, c=c: e.tensor_tensor_scan(
                            out=hf[:, 0:N], data0=a4[:, q, 0:N], data1=bb_t[:, 0:N], initial=state[:, d, c:c + 1],
                            op0=ALU.mult, op1=ALU.add), reads=("a4_%d" % q, "bb_t", "state"), writes=("hf",))
                        S.op("pool", lambda e, c=c: e.tensor_copy(out=state[:, d, c:c + 1], in_=hf[:, N - 1:N]),
                             reads=("hf",), writes=("state",))
                    if consumer is not None:
                        consumer(c)

            for s_ in range(10):
                if s_ < 8:
                    st1(s_)
                if 1 <= s_ <= 8:
                    st2(s_ - 1)
                if 2 <= s_ <= 9:
                    st3(s_ - 2)
                    if (s_ - 2) % 4 == 3:
                        st4(s_ - 2 - 3)

        hxT, hxk, hxhk = hxTs[0], "hxT0", "hxTh0"
        prep(ctxp, 2, 256, s1c, 1, "xt")
        hbc[0] = 1
        rglru_block(256, 0, False, False, False)
        rglru_block(256, 1, True, False, False)
        for blk in range(15, -1, -1):
            _i = hbc[0] % 2
            hbc[0] += 1
            hxT, hxk, hxhk = hxTs[_i], "hxT%d" % _i, "hxTh%d" % _i
            prep(xp, 2 + NB * blk, NB, s1, 0, "xt")
            def cons_a(c):
                S.op("dve", lambda e, c=c: e.tensor_copy(out=hsb[:, c, :], in_=hf[:]), reads=("hf",), writes=("hsb",))
            rglru_block(NB, 1, True, blk != 0, blk != 15, cons_a if blk < 8 else None)
            if blk < 8:
                S.dma("sp", hs_scr[blk], hsb[:], reads=("hsb",), writes=("hs_scr%d" % blk,), key="hs_scr")

        pb_ = ExitStack()
        cwh = cw_sb
        S.op("dve", lambda e: e.tensor_scalar_mul(out=cwh[:], in0=cw_sb[:], scalar1=0.5), reads=("params",), writes=("cwh",))
        lnr = sb("lnr", [128, NB], F32, pb_)
        lmr = sb("lmr", [128, NB], F32, pb_)
        wsl = [sb("wsl%d" % i, [128, 8, 512], BF16, pb_) for i in range(3)]
        dgc = [sb("dgc0", [128, 31, 128], BF16, pb_)] * 2
        tv, uu = tr, ti
        zb = [sb("zb0", [128, NB], BF16, pb_)] * 2
        zc = sb("zc", [128, 8, NB], BF16, pb_)
        aa = zc
        zsq = [sb("zsq0", [128, NB], BF16, pb_)] * 2
        A_t = sb("A_t", [128, 8, NB], BF16, pb_)
        gy = sb("gy", [128, 8, NB], BF16, pb_)
        mg = gy
        yb = sb("yb", [128, 8, NB], BF16, pb_)
        hs_in = hsb
        x1 = xt

        xres = [sb("xres%d" % i, [128, 512], F32, pb_) for i in range(3)]
        xrc = [0]
        wctr = [0]

        def wpiece(col0, src=None):
            src = w_in if src is None else src
            i = wctr[0] % 3
            wctr[0] += 1
            S.dma("pool", wsl[i][:], src[:, col0:col0 + 512].rearrange("(k p) n -> p k n", p=128),
                  writes=("wsl%d" % i,), key="wsl%d" % i)
            return wsl[i], "wsl%d" % i

        def inproj(dstbank, wt, wk, j4):
            S.group("pe", [lambda e, k=k: e.matmul(bank(dstbank), lhsT=wt[:, k, 128 * j4:128 * j4 + 128], rhs=hxT[:, k, 0:NB],
                                                   start=(k == 0), stop=(k == 7)) for k in range(8)],
                    reads=(hxk, wk), writes=(pk(dstbank),))

        for blk in range(8):
            _i = hbc[0] % 2
            hbc[0] += 1
            hxT, hxk, hxhk = hxTs[_i], "hxT%d" % _i, "hxTh%d" % _i
            prep(xp, 2 + NB * blk, NB, s1, 0, "xt")
            S.dma("sp", hs_in[:], hs_scr[blk], reads=("hs_scr%d" % blk,), writes=("hsb",), key="hs_in")
            for half in range(2):
                wu, wuk = wpiece(512 * half)
                wv, wvk = wpiece(1024 + 512 * half)
                for c4 in range(4):
                    c = 4 * half + c4
                    par = c % 2
                    inproj(par, wu, wuk, c4)
                    inproj(2 + par, wv, wvk, c4)
                    S.op("act", lambda e, c=c, par=par: e.activation(out=tv[:], in_=bank(2 + par), func=AF.Tanh, scale=0.5,
                                                                     bias=hb_in[:, 8 + c:9 + c]),
                         reads=(pk(2 + par), "hb_in"), writes=("tr",))
                    S.op("act", lambda e, c=c, par=par: e.activation(out=uu[:], in_=bank(par), func=AF.Identity,
                                                                     bias=b_in_sb[:, c:c + 1]),
                         reads=(pk(par), "params"), writes=("ti",))
                    zk = "zb0"
                    S.op("dve", lambda e, par=par: e.scalar_tensor_tensor(out=zb[par][:], in0=tv[:], scalar=1.0, in1=uu[:],
                                                                          op0=ALU.add, op1=ALU.mult),
                         reads=("tr", "ti"), writes=(zk,))
                    dk = "dgc0"
                    S.op("dve", lambda e, c=c, par=par: e.tensor_tensor(
                        out=dgc[par][:], in0=identb[:].unsqueeze(1).to_broadcast([128, 31, 128]),
                        in1=cwh[:, c, :].unsqueeze(2).to_broadcast([128, 31, 128]), op=ALU.mult),
                        reads=("identb", "cwh"), writes=(dk,))
                    zv = zb[par][:].rearrange("p (r t) -> p r t", t=64)
                    pcv = bank(4 + par).rearrange("p (r t) -> p r t", t=64)
                    fns = []
                    order = [15] + [k for k in range(31) if k != 15]
                    for idx, k in enumerate(order):
                        o = k - 15
                        t0, t1 = max(0, -o), 64 - max(0, o)
                        fns.append(lambda e, k=k, o=o, t0=t0, t1=t1, idx=idx, par=par, pcv=pcv, zv=zv: e.matmul(
                            pcv[:, :, t0:t1], lhsT=dgc[par][:, k, :], rhs=zv[:, :, t0 + o:t1 + o],
                            start=(idx == 0), stop=(idx == 30)))
                    S.group("pe", fns, reads=(zk, dk), writes=(pk(4 + par),))
                    S.op("act", lambda e, c=c, par=par: e.activation(out=zc[:, c, :], in_=bank(4 + par), func=AF.Identity,
                                                                     bias=cb_sb[:, c:c + 1]),
                         reads=(pk(4 + par), "params"), writes=("zc",))
                    qk = "zsq0"
                    S.op("act", lambda e, c=c, par=par: e.activation(out=zsq[par][:], in_=bank(4 + par), func=AF.Square,
                                                                     bias=cb_sb[:, c:c + 1]),
                         reads=(pk(4 + par), "params"), writes=(qk,))
                    S.group("pe", [lambda e, c=c: e.matmul(bank(6), lhsT=ones_m[:], rhs=zc[:, c, :], start=(c == 0), stop=(c == 7))],
                            reads=("zc", "ones_m"), writes=("ps6",))
                    S.group("pe", [lambda e, c=c, par=par: e.matmul(bank(7), lhsT=ones_m[:], rhs=zsq[par][:], start=(c == 0),
                                                                    stop=(c == 7))], reads=(qk, "ones_m"), writes=("ps7",))
            S.op("act", lambda e: e.activation(out=tv[:], in_=bank(6), func=AF.Copy), reads=("ps6",), writes=("tr",))
            S.op("dve", lambda e: e.tensor_tensor(out=uu[:], in0=tv[:], in1=tv[:], op=ALU.mult), reads=("tr",), writes=("ti",))
            S.op("dve", lambda e: e.tensor_tensor(out=lnr[:], in0=bank(7), in1=uu[:], op=ALU.subtract), reads=("ps7", "ti"),
                 writes=("lnr",))
            S.op("act", lambda e: e.activation(out=lnr[:], in_=lnr[:], func=AF.Sqrt, bias=qtr[:, 2:3]), reads=("lnr", "qtr"), writes=("lnr",))
            S.op("dve", lambda e: e.reciprocal(out=lnr[:], in_=lnr[:]), reads=("lnr",), writes=("lnr",))
            S.op("dve", lambda e: e.tensor_tensor(out=lmr[:], in0=tv[:], in1=lnr[:], op=ALU.mult), reads=("tr", "lnr"),
                 writes=("lmr",))
            for half in range(2):
                wy, wyk = wpiece(2048 + 512 * half)
                for c4 in range(4):
                    c = 4 * half + c4
                    par = c % 2
                    inproj(par, wy, wyk, c4)
                    S.op("act", lambda e, c=c, par=par: e.activation(out=gy[:, c, :], in_=bank(par), func=AF.Gelu_apprx_tanh,
                                                                     bias=b_in_sb[:, 16 + c:17 + c]),
                         reads=(pk(par), "params"), writes=("gy",))
            def cons_b(c):
                S.op("dve", lambda e, c=c: e.tensor_tensor(out=tmp1[:], in0=hf[:], in1=hs_in[:, c, :], op=ALU.add),
                     reads=("hf", "hsb"), writes=("tmp1",))
                S.op("dve", lambda e, c=c: e.tensor_tensor(out=yb[:, c, :], in0=tmp1[:], in1=gy[:, c, :], op=ALU.mult),
                     reads=("tmp1", "gy"), writes=("yb",))
            rglru_block(NB, 0, False, blk != 0, True, cons_b)
            for c in range(8):
                S.op("dve", lambda e, c=c: e.tensor_tensor(out=tv[:], in0=zc[:, c, :], in1=lnr[:], op=ALU.mult),
                     reads=("zc", "lnr"), writes=("tr",))
                S.op("dve", lambda e: e.tensor_tensor(out=uu[:], in0=tv[:], in1=lmr[:], op=ALU.subtract), reads=("tr", "lmr"),
                     writes=("ti",))
                S.op("act", lambda e, c=c: e.activation(out=aa[:, c, :], in_=uu[:], func=AF.Silu, scale=lng_sb[:, c:c + 1],
                                                        bias=lnb_sb[:, c:c + 1]), reads=("ti", "params"), writes=("zc",))
            for half in range(2):
                wga, wgak = wpiece(4096 + 512 * half)
                wpa, wpak = wpiece(512 * half, w_pa)
                for m4 in range(4):
                    m = 4 * half + m4
                    par = m % 2
                    S.group("pe", [lambda e, k=k, m4=m4, par=par, wpa=wpa: e.matmul(bank(par), lhsT=wpa[:, k, 128 * m4:128 * m4 + 128],
                                                                         rhs=aa[:, k, :], start=(k == 0), stop=(k == 7))
                                   for k in range(8)], reads=("zc", wpak), writes=(pk(par),))
                    inproj(2 + par, wga, wgak, m4)
                    S.op("act", lambda e, m=m, par=par: e.activation(out=tv[:], in_=bank(2 + par), func=AF.Tanh, scale=0.5,
                                                                     bias=hb_in[:, 32 + m:33 + m]),
                         reads=(pk(2 + par), "hb_in"), writes=("tr",))
                    S.op("dve", lambda e, m=m, par=par: e.scalar_tensor_tensor(out=A_t[:, m, :], in0=tv[:], scalar=1.0,
                                                                               in1=bank(par), op0=ALU.add, op1=ALU.mult),
                         reads=("tr", pk(par)), writes=("A_t",))
            for half in range(2):
                wgb_, wgbk = wpiece(5120 + 512 * half)
                wpb, wpbk = wpiece(512 * half, w_pb)
                for m4 in range(4):
                    m = 4 * half + m4
                    par = m % 2
                    S.group("pe", [lambda e, k=k, m4=m4, par=par, wpb=wpb: e.matmul(bank(par), lhsT=wpb[:, k, 128 * m4:128 * m4 + 128],
                                                                         rhs=yb[:, k, :], start=(k == 0), stop=(k == 7))
                                   for k in range(8)], reads=("yb", wpbk), writes=(pk(par),))
                    inproj(2 + par, wgb_, wgbk, m4)
                    S.op("act", lambda e, m=m, par=par: e.activation(out=tv[:], in_=bank(2 + par), func=AF.Tanh, scale=0.5,
                                                                     bias=hb_in[:, 40 + m:41 + m]),
                         reads=(pk(2 + par), "hb_in"), writes=("tr",))
                    S.op("dve", lambda e, par=par: e.scalar_tensor_tensor(out=uu[:], in0=tv[:], scalar=1.0, in1=bank(par),
                                                                          op0=ALU.add, op1=ALU.mult),
                         reads=("tr", pk(par)), writes=("ti",))
                    S.op("dve", lambda e, m=m: e.tensor_tensor(out=mg[:, m, :], in0=uu[:], in1=A_t[:, m, :], op=ALU.add),
                         reads=("ti", "A_t"), writes=("gy",))
            for hh in range(2):
                wo, wok = wpiece(512 * hh, w_o)
                for j in range(4):
                    bk = 4 + j
                    xi = xrc[0] % 3
                    xrc[0] += 1
                    xk_ = "xres%d" % xi
                    r0_ = 2 + NB * blk + 128 * j
                    S.dma("sp", xres[xi][:], xp[r0_:r0_ + 128, 512 * hh:512 * hh + 512], writes=(xk_,), key=xk_)
                    S.group("pe", [lambda e, k=k, j=j, bk=bk, wo=wo: e.matmul(bank(bk), lhsT=mg[:, k, 128 * j:128 * j + 128],
                                                                             rhs=wo[:, k, :], start=(k == 0), stop=(k == 7))
                                   for k in range(8)], reads=("gy", wok), writes=(pk(bk),))
                    S.op("dve", lambda e, hh=hh, bk=bk: e.tensor_tensor(out=tmp1[:], in0=bank(bk), in1=gt1h[:, 512 * hh:512 * hh + 512],
                                                                        op=ALU.mult), reads=(pk(bk), "gt"), writes=("tmp1",))
                    S.op("pool", lambda e, xi=xi: e.tensor_tensor(out=xres[xi][:], in0=xres[xi][:], in1=tmp1[:], op=ALU.add),
                         reads=("tmp1", xk_), writes=(xk_,))
                    S.dma("sp", x1_scr[NB * blk + 128 * j:NB * blk + 128 * j + 128, 512 * hh:512 * hh + 512], xres[xi][:],
                          reads=(xk_,), writes=("x1_scr%d" % blk,), key="x1_scr")
        S.barrier()
        pb_.close()
        mixer.close()

        def bc(ap, shape):
            return ap.to_broadcast(shape)

        pcg = ExitStack()
        gf32 = sb("gf32", [128, 1024], F32, pcg)
        gt2b = sb("gt2b", [128, 1024], F32, pcg)
        S.dma("sp", gf32[:], gfin, writes=("gf32",), key="gf32")
        S.dma("sp", gt2b[:], gt2_scr, reads=("gt2_scr",), writes=("gt2b",), key="gt2b")
        S.op("dve", lambda e: e.tensor_scalar_mul(out=gf32[:], in0=gf32[:], scalar1=32.0), reads=("gf32",), writes=("gf32",))
        slot_i = sb("slot_i", [128, 32], I32, pcg)
        offE_i = sb("offE_i", [128, NSEG, 4], I32, pcg)
        trib = sb("trib", [128, 128], BF16, pcg)
        S.dma("pool", trib[:], tri, writes=("trib",), key="trib")

        c1 = ExitStack()
        x1l = [sb("x1l%d" % i, [128, 4, 1024], F32, c1) for i in range(2)]
        xn2_all = sb("xn2_all", [128, 32, 1024], BF16, c1)
        hmT1 = sb("hmTr", [128, 8, NB], BF16, c1)
        zt = sb("zt", [128, 4096], BF16, c1)
        ztf = sb("ztf", [128, 192], F32, c1)
        ssA = sb("ssA", [128, 8, 4], F32, c1)
        rsA = sb("rsA", [128, 8, 4], F32, c1)
        oh_all = sb("oh_all", [128, 32, 4], F32, c1)
        wsel_all = sb("wsel_all", [128, 32, 4], F32, c1)
        oh_bf = sb("oh_bf", [128, 32, 4], BF16, c1)
        R1s = sb("R1s", [128, 32, 4], F32, c1)
        Cs = sb("Cs", [128, 32, 4], F32, c1)
        incl = sb("incl", [128, 4, 32], F32, c1)
        onesf = sb("onesf", [128, 32], F32, c1)
        ng = sb("ng", [128, 4], F32, c1)
        nseg = sb("nseg", [128, 4], F32, c1)
        sst = sb("sst", [128, 4], F32, c1)
        sen = sb("sen", [128, 4], F32, c1)
        slot_f = sb("slot_f", [128, 32], F32, c1)
        Gs = sb("Gs", [128, NSEG], F32, c1)
        sidx_sb = sb("sidx_sb", [128, NSEG], F32, c1)
        cE_sb = sb("cE_sb", [128, 4], F32, c1)
        offE_f = sb("offE_f", [128, NSEG, 4], F32, c1)
        L = sb("L", [128, 4, 20], F32, c1)
        gmax = sb("gmax", [128, 4, 1], F32, c1)
        eg = sb("eg", [128, 4, 4], F32, c1)
        pg = sb("pg", [128, 4, 1], F32, c1)
        tmp16 = sb("tmp16", [128, 4, 16], F32, c1)
        esel = sb("esel", [128, 4, 4], F32, c1)
        m1 = sb("m1", [128, 4, 1], F32, c1)
        m2 = sb("m2", [128, 4, 1], F32, c1)
        k1 = sb("k1", [128, 4, 4], F32, c1)
        k2 = sb("k2", [128, 4, 4], F32, c1)
        e2 = sb("e2", [128, 4, 4], F32, c1)
        w1 = sb("w1", [128, 4, 1], F32, c1)
        w2 = sb("w2", [128, 4, 1], F32, c1)
        S.dma("sp", sidx_sb[:], sidx, writes=("cidx",), key="cidx")
        S.dma("sp", cE_sb[:], cE, writes=("cidx",), key="cidx")
        S.op("pool", lambda e: e.memset(zt[:], 0.0), writes=("zt",))
        S.op("pool", lambda e: e.memset(ztf[:], 0.0), writes=("zt",))
        S.op("pool", lambda e: e.memset(onesf[:], 1.0), writes=("onesf",))
        for sg_ in range(NSEG):
            S.dma("sp", xs_sorted[NB * sg_:NB * sg_ + NB, :].rearrange("(p r) d -> p (r d)", r=4), zt[:],
                  reads=("zt",), writes=("xs_sorted",), key="xs_z")
        S.dma("sp", ws_sorted.rearrange("(p r) c -> p (r c)", r=48), ztf[:], reads=("zt",), writes=("ws_sorted",), key="xs_z")

        for blk in range(8):
            pb2 = blk % 2
            x1t = x1l[pb2]
            ak = "x1l%d" % pb2
            sak = "ssA%d" % blk
            oh = oh_all[:, 4 * blk:4 * blk + 4, :]
            wsel = wsel_all[:, 4 * blk:4 * blk + 4, :]
            S.dma("sp", x1t[:], x1_scr[NB * blk:NB * blk + NB, :].rearrange("(j p) d -> p j d", p=128),
                  reads=("x1_scr%d" % blk,), writes=(ak,), key=ak)
            S.op("pool", lambda e, blk=blk: e.memset(ssA[:, blk, :], 0.0), writes=(sak,))
            for j in range(4):
                S.op("act", lambda e, j=j, x1t=x1t, blk=blk: e.activation(out=xn2_all[:, 4 * blk + j, :], in_=x1t[:, j, :], func=AF.Square,
                                                                          accum_out=ssA[:, blk, j:j + 1]),
                     reads=(ak,), writes=("xn2_%d" % blk, sak))
            S.op("act", lambda e, blk=blk: e.activation(out=rsA[:, blk, :], in_=ssA[:, blk, :], func=AF.Sqrt, bias=qtr[:, 1:2]),
                 reads=(sak, "qtr"), writes=(sak + "r",))
            S.op("dve", lambda e, blk=blk: e.reciprocal(out=rsA[:, blk, :], in_=rsA[:, blk, :]), reads=(sak + "r",), writes=(sak + "r",))
            for j in range(4):
                S.op("dve", lambda e, j=j, x1t=x1t, blk=blk: e.tensor_scalar_mul(out=xn2_all[:, 4 * blk + j, :], in0=x1t[:, j, :],
                                                                                 scalar1=rsA[:, blk, j:j + 1]),
                     reads=(ak, sak + "r"), writes=("xn2_%d" % blk,))
            for j in range(4):
                S.group("pe", [lambda e, j=j, c=c, blk=blk: e.transpose(out=tpv[:, c, 128 * j:128 * j + 128],
                                                                        in_=xn2_all[:, 4 * blk + j, 128 * c:128 * c + 128], identity=identb[:])
                               for c in range(8)], reads=("xn2_%d" % blk, "identb"), writes=TPK)
            for c in range(8):
                S.op("act", lambda e, c=c: e.activation(out=hmT1[:, c, :], in_=tpv[:, c, :], func=AF.Identity,
                                                        scale=s2[:, c:c + 1], bias=mods[:, 24 + c, 0:1]),
                     reads=TPK + ("sc", "mods"), writes=("hmT1",))
            for j in range(4):
                S.group("pe", [lambda e, k=k, j=j: e.matmul(bank(4)[:, 20 * j:20 * j + 20], lhsT=hmT1[:, k, 128 * j:128 * j + 128],
                                                            rhs=w_rt_b[:, k, :], start=(k == 0), stop=(k == 7))
                               for k in range(8)], reads=("hmT1", "w_rt_b"), writes=("ps4",))
            S.op("dve", lambda e: e.tensor_tensor(out=L[:], in0=bank(4)[:, 0:80].rearrange("p (j n) -> p j n", n=20),
                                                  in1=bc(b_rt_sb[:].unsqueeze(1), [128, 4, 20]), op=ALU.add),
                 reads=("ps4", "params"), writes=("L",))
            R = ("rt",)
            OK_ = ("oh_all",)
            S.op("dve", lambda e: e.tensor_reduce(out=gmax[:], in_=L[:, :, 0:4], axis=AX.X, op=ALU.max), reads=("L",), writes=R)
            S.op("dve", lambda e, oh=oh: e.tensor_tensor(out=oh, in0=L[:, :, 0:4], in1=bc(gmax[:], [128, 4, 4]), op=ALU.is_equal),
                 reads=R + ("L",), writes=R + OK_)
            S.op("dve", lambda e: e.tensor_tensor(out=eg[:], in0=L[:, :, 0:4], in1=bc(gmax[:], [128, 4, 4]), op=ALU.subtract),
                 reads=R + ("L",), writes=R)
            S.op("act", lambda e: e.activation(out=eg[:], in_=eg[:], func=AF.Exp), reads=R, writes=R)
            S.op("dve", lambda e: e.tensor_reduce(out=pg[:], in_=eg[:], axis=AX.X, op=ALU.add), reads=R, writes=R)
            S.op("dve", lambda e: e.reciprocal(out=pg[:], in_=pg[:]), reads=R, writes=R)
            S.op("dve", lambda e, oh=oh: e.tensor_tensor(out=tmp16[:].rearrange("p j (g x) -> p j g x", x=4),
                                                         in0=L[:, :, 4:20].rearrange("p j (g x) -> p j g x", x=4),
                                                         in1=bc(oh.unsqueeze(3), [128, 4, 4, 4]), op=ALU.mult),
                 reads=R + ("L",), writes=R)
            S.op("dve", lambda e: e.tensor_reduce(out=esel[:].unsqueeze(3), in_=tmp16[:].rearrange("p j (g x) -> p j x g", x=4),
                                                  axis=AX.X, op=ALU.add), reads=R, writes=R)
            S.op("dve", lambda e: e.tensor_reduce(out=m1[:], in_=esel[:], axis=AX.X, op=ALU.max), reads=R, writes=R)
            S.op("dve", lambda e: e.tensor_tensor(out=k1[:], in0=esel[:], in1=bc(m1[:], [128, 4, 4]), op=ALU.is_equal),
                 reads=R, writes=R)
            S.op("dve", lambda e: e.scalar_tensor_tensor(out=e2[:], in0=k1[:], scalar=-1e30, in1=esel[:], op0=ALU.mult, op1=ALU.add),
                 reads=R, writes=R)
            S.op("dve", lambda e: e.tensor_reduce(out=m2[:], in_=e2[:], axis=AX.X, op=ALU.max), reads=R, writes=R)
            S.op("dve", lambda e: e.tensor_tensor(out=k2[:], in0=e2[:], in1=bc(m2[:], [128, 4, 4]), op=ALU.is_equal),
                 reads=R, writes=R)
            S.op("dve", lambda e: e.tensor_tensor(out=w2[:], in0=m2[:], in1=m1[:], op=ALU.subtract), reads=R, writes=R)
            S.op("act", lambda e: e.activation(out=w2[:], in_=w2[:], func=AF.Exp), reads=R, writes=R)
            S.op("dve", lambda e: e.tensor_scalar_add(out=w1[:], in0=w2[:], scalar1=1.0), reads=R, writes=R)
            S.op("dve", lambda e: e.reciprocal(out=w1[:], in_=w1[:]), reads=R, writes=R)
            S.op("dve", lambda e: e.tensor_tensor(out=w2[:], in0=w2[:], in1=w1[:], op=ALU.mult), reads=R, writes=R)
            S.op("dve", lambda e: e.tensor_tensor(out=w1[:], in0=w1[:], in1=pg[:], op=ALU.mult), reads=R, writes=R)
            S.op("dve", lambda e: e.tensor_tensor(out=w2[:], in0=w2[:], in1=pg[:], op=ALU.mult), reads=R, writes=R)
            S.op("dve", lambda e, wsel=wsel: e.tensor_tensor(out=wsel, in0=k1[:], in1=bc(w1[:], [128, 4, 4]), op=ALU.mult),
                 reads=R, writes=R + ("wsel_all",))
            S.op("dve", lambda e: e.tensor_tensor(out=k2[:], in0=k2[:], in1=bc(w2[:], [128, 4, 4]), op=ALU.mult), reads=R, writes=R)
            S.op("dve", lambda e, wsel=wsel: e.tensor_tensor(out=wsel, in0=wsel, in1=k2[:], op=ALU.add), reads=R + ("wsel_all",),
                 writes=R + ("wsel_all",))

        ohf = oh_all[:].rearrange("p t g -> p (t g)")
        S.op("dve", lambda e: e.tensor_copy(out=oh_bf[:], in_=oh_all[:]), reads=("oh_all",), writes=("oh_bf",))
        S.group("pe", [lambda e: e.matmul(bank(0)[:, 0:128], lhsT=trib[:], rhs=oh_bf[:].rearrange("p t g -> p (t g)"), start=True, stop=True)],
                reads=("oh_bf", "trib"), writes=("ps0",))
        S.group("pe", [lambda e: e.matmul(bank(1)[:, 0:128], lhsT=ones1[:], rhs=oh_bf[:].rearrange("p t g -> p (t g)"), start=True, stop=True)],
                reads=("oh_bf", "ones1"), writes=("ps1",))
        S.op("act", lambda e: e.activation(out=R1s[:].rearrange("p t g -> p (t g)"), in_=bank(0)[:, 0:128], func=AF.Copy),
             reads=("ps0",), writes=("R1s",))
        S.op("act", lambda e: e.activation(out=Cs[:].rearrange("p t g -> p (t g)"), in_=bank(1)[:, 0:128], func=AF.Copy),
             reads=("ps1",), writes=("Cs",))
        for g in range(4):
            S.op("dve", lambda e, g=g: e.tensor_tensor_scan(out=incl[:, g, :], data0=onesf[:], data1=Cs[:, :, g], initial=0.0,
                                                            op0=ALU.mult, op1=ALU.add), reads=("Cs", "onesf"), writes=("incl",))
        S.op("dve", lambda e: e.tensor_copy(out=ng[:], in_=incl[:, :, 31]), reads=("incl",), writes=("ng",))
        S.op("dve", lambda e: e.tensor_tensor(out=incl[:], in0=incl[:], in1=Cs[:].rearrange("p t g -> p g t"), op=ALU.subtract),
             reads=("incl", "Cs"), writes=("incl",))
        S.op("dve", lambda e: e.memset(nseg[:], 0.0), writes=("nseg",))
        for k in range(8):
            S.op("dve", lambda e, k=k: e.scalar_tensor_tensor(out=nseg[:], in0=ng[:], scalar=float(NB * k), in1=nseg[:],
                                                              op0=ALU.is_gt, op1=ALU.add), reads=("ng", "nseg"), writes=("nseg",))
        S.op("dve", lambda e: e.memset(sst[:], 0.0), writes=("sst",))
        for g in range(1, 4):
            S.op("dve", lambda e, g=g: e.tensor_tensor(out=sst[:, g:g + 1], in0=sst[:, g - 1:g], in1=nseg[:, g - 1:g], op=ALU.add),
                 reads=("sst", "nseg"), writes=("sst",))
        S.op("dve", lambda e: e.tensor_tensor(out=sen[:], in0=sst[:], in1=nseg[:], op=ALU.add), reads=("sst", "nseg"), writes=("sen",))
        S.op("dve", lambda e: e.tensor_scalar_mul(out=sst[:], in0=sst[:], scalar1=float(NB)), reads=("sst", "sen"), writes=("sst",))
        S.op("dve", lambda e: e.tensor_tensor(out=R1s[:], in0=R1s[:], in1=incl[:].rearrange("p g t -> p t g"), op=ALU.add),
             reads=("R1s", "incl"), writes=("R1s",))
        S.op("dve", lambda e: e.tensor_tensor(out=R1s[:], in0=R1s[:], in1=bc(sst[:].unsqueeze(1), [128, 32, 4]), op=ALU.add),
             reads=("R1s", "sst"), writes=("R1s",))
        S.op("dve", lambda e: e.tensor_tensor(out=R1s[:], in0=R1s[:], in1=oh_all[:], op=ALU.mult), reads=("R1s", "oh_all"), writes=("R1s",))
        S.op("dve", lambda e: e.tensor_reduce(out=slot_f[:].unsqueeze(2), in_=R1s[:], axis=AX.X, op=ALU.add), reads=("R1s",), writes=("slot_f",))
        S.op("dve", lambda e: e.tensor_copy(out=slot_i[:], in_=slot_f[:]), reads=("slot_f",), writes=("slot_i",))
        S.op("dve", lambda e: e.memset(Gs[:], 0.0), writes=("Gs",))
        for g in range(3):
            S.op("dve", lambda e, g=g: e.scalar_tensor_tensor(out=Gs[:], in0=sidx_sb[:], scalar=sen[:, g:g + 1], in1=Gs[:],
                                                              op0=ALU.is_ge, op1=ALU.add), reads=("cidx", "sen", "Gs"), writes=("Gs",))
        S.op("dve", lambda e: e.tensor_scalar_mul(out=offE_f[:], in0=bc(Gs[:].unsqueeze(2), [128, NSEG, 4]), scalar1=512.0),
             reads=("Gs",), writes=("offE_f",))
        S.op("dve", lambda e: e.tensor_tensor(out=offE_f[:], in0=offE_f[:], in1=bc(cE_sb[:].unsqueeze(1), [128, NSEG, 4]), op=ALU.add),
             reads=("offE_f", "cidx"), writes=("offE_f",))
        S.op("dve", lambda e: e.tensor_copy(out=offE_i[:], in_=offE_f[:]), reads=("offE_f",), writes=("offE_i",))
        for t in range(32):
            S.idma("pool", xs_sorted[:, :], bass.IndirectOffsetOnAxis(ap=slot_i[:, t:t + 1], axis=0), xn2_all[:, t, :], None,
                   reads=("slot_i", "xn2_%d" % (t // 4), "xs_sorted"), writes=("xs_sorted_s",), key="scat")
            S.idma("pool", ws_sorted[:, :], bass.IndirectOffsetOnAxis(ap=slot_i[:, t:t + 1], axis=0), wsel_all[:, t, :], None,
                   reads=("slot_i", "wsel_all", "ws_sorted"), writes=("ws_sorted_s",), key="scat")
        S.barrier()
        c1.close()

        c2 = ExitStack()
        xst = [sb("xst%d" % i, [128, 4, 1024], BF16, c2) for i in range(2)]
        wst = [sb("wst%d" % i, [128, 4, 4], F32, c2) for i in range(2)]
        hmTs = [sb("hmT%d" % i, [128, 8, NB], BF16, c2) for i in range(2)]
        cbc = [sb("cbc%d" % i, [128, 4, NB], BF16, c2) for i in range(2)]
        dgm = sb("dgm", [128, 4, 128], BF16, c2)
        actb = [sb("actb%d" % i, [128, NB], BF16, c2) for i in range(16)]
        wgu = [sb("wgu%d" % i, [128, 2, 8, 512], BF16, c2) for i in range(3)]
        NWD = 5
        wd = [sb("wd%d" % i, [128, 4, 1024], BF16, c2) for i in range(NWD)]
        sg = [sb("sg%d" % i, [128, NB], F32, c2) for i in range(2)]
        tt = [sb("tt%d" % i, [128, NB], BF16, c2) for i in range(2)]
        ysb = [sb("ysb%d" % i, [128, 4, 1024], F32, c2) for i in range(2)]
        ectr = [0]
        for sgi in range(NSEG):
            pb2 = sgi % 2
            hmT, hk = hmTs[pb2], "hmT%d" % pb2
            xk2, wk2, ck2, yk2 = "xst%d" % pb2, "wst%d" % pb2, "cbc%d" % pb2, "ysb%d" % pb2
            S.dma("sp", xst[pb2][:], xs_sorted[NB * sgi:NB * sgi + NB, :].rearrange("(j p) d -> p j d", p=128), writes=(xk2,), key=xk2)
            S.dma("sp", wst[pb2][:], ws_sorted[NB * sgi:NB * sgi + NB, :].rearrange("(j p) c -> p j c", p=128), writes=(wk2,), key=wk2)
            for j in range(4):
                S.group("pe", [lambda e, j=j, c=c, pb2=pb2: e.transpose(out=tpv[:, c, 128 * j:128 * j + 128],
                                                                        in_=xst[pb2][:, j, 128 * c:128 * c + 128], identity=identb[:])
                               for c in range(8)], reads=(xk2, "identb"), writes=TPK)
            for c in range(8):
                S.op("act", lambda e, c=c, hmT=hmT: e.activation(out=hmT[:, c, :], in_=tpv[:, c, :], func=AF.Identity,
                                                                 scale=s2[:, c:c + 1], bias=mods[:, 24 + c, 0:1]),
                     reads=TPK + ("sc", "mods"), writes=(hk,))
            for j in range(4):
                S.op("dve", lambda e, j=j, pb2=pb2: e.tensor_tensor(out=dgm[:], in0=bc(identb[:].unsqueeze(1), [128, 4, 128]),
                                                                    in1=bc(wst[pb2][:, j, :].unsqueeze(2), [128, 4, 128]), op=ALU.mult),
                     reads=("identb", wk2), writes=("dgm",))
                S.group("pe", [lambda e: e.matmul(bank(4 + (j % 2)), lhsT=ones1[:], rhs=dgm[:], start=True, stop=True)],
                        reads=("dgm", "ones1"), writes=(pk(4 + (j % 2)),))
                S.op("act", lambda e, j=j, pb2=pb2: e.activation(out=cbc[pb2][:, :, 128 * j:128 * j + 128],
                                                                 in_=bank(4 + (j % 2)).rearrange("p (x t) -> p x t", t=128), func=AF.Copy),
                     reads=(pk(4 + (j % 2)),), writes=(ck2,))
            for el in range(4):
                si = ectr[0] % 3
                di = ectr[0] % NWD
                ectr[0] += 1
                gk, dk_ = "wgu%d" % si, "wd%d" % di
                ofs = bass.IndirectOffsetOnAxis(ap=offE_i[:, sgi, el:el + 1], axis=0)
                S.idma("pool", wgu[si][:, 0].rearrange("p k n -> p (k n)"), None, w_gate[:, :], ofs, reads=("offE_i",), writes=(gk,), key=gk)
                S.idma("pool", wgu[si][:, 1].rearrange("p k n -> p (k n)"), None, w_up[:, :], ofs, reads=("offE_i",), writes=(gk,), key=gk)
                S.idma("pool", wd[di][:].rearrange("p k n -> p (k n)"), None, w_down[:, :], ofs, reads=("offE_i",), writes=(dk_,), key=dk_)
                for f in range(4):
                    u = 4 * el + f
                    pp = u % 2
                    S.group("pe", [lambda e, k=k, f=f, si=si, pp=pp, hmT=hmT: e.matmul(
                        bank(2 * pp), lhsT=wgu[si][:, 0, k, 128 * f:128 * f + 128], rhs=hmT[:, k, :],
                        start=(k == 0), stop=(k == 7)) for k in range(8)], reads=(hk, gk), writes=(pk(2 * pp),))
                    S.group("pe", [lambda e, k=k, f=f, si=si, pp=pp, hmT=hmT: e.matmul(
                        bank(2 * pp + 1), lhsT=wgu[si][:, 1, k, 128 * f:128 * f + 128], rhs=hmT[:, k, :],
                        start=(k == 0), stop=(k == 7)) for k in range(8)], reads=(hk, gk), writes=(pk(2 * pp + 1),))
                    S.op("act", lambda e, pp=pp: e.activation(out=sg[pp][:], in_=bank(2 * pp), func=AF.Silu),
                         reads=(pk(2 * pp),), writes=("sg%d" % pp,))
                    S.op("dve", lambda e, pp=pp: e.tensor_tensor(out=tt[pp][:], in0=bank(2 * pp + 1), in1=sg[pp][:], op=ALU.mult),
                         reads=(pk(2 * pp + 1), "sg%d" % pp), writes=("tt%d" % pp,))
                    S.op("dve", lambda e, pp=pp, u=u, el=el, pb2=pb2: e.tensor_tensor(out=actb[u][:], in0=tt[pp][:], in1=cbc[pb2][:, el, :],
                                                                                      op=ALU.mult),
                         reads=("tt%d" % pp, ck2), writes=("actb%d" % u,))
            dbase = ectr[0] - 4
            for tp_ in range(2):
                fns = []
                for u in range(16):
                    el, f = divmod(u, 4)
                    di = (dbase + el) % NWD
                    for jj in range(2):
                        j = 2 * tp_ + jj
                        for hh in range(2):
                            fns.append(lambda e, u=u, f=f, di=di, j=j, jj=jj, hh=hh: e.matmul(
                                bank(4 + 2 * jj + hh), lhsT=actb[u][:, 128 * j:128 * j + 128],
                                rhs=wd[di][:, f, 512 * hh:512 * hh + 512], start=(u == 0), stop=(u == 15)))
                S.group("pe", fns, reads=tuple("actb%d" % u for u in range(16)) + tuple("wd%d" % ((dbase + el) % NWD) for el in range(4)),
                        writes=("ps4", "ps5", "ps6", "ps7"))
                for jj in range(2):
                    j = 2 * tp_ + jj
                    for hh in range(2):
                        bk = 4 + 2 * jj + hh
                        S.op("dve", lambda e, bk=bk, hh=hh, j=j, pb2=pb2: e.tensor_tensor(
                            out=ysb[pb2][:, j, 512 * hh:512 * hh + 512], in0=bank(bk), in1=gt2b[:, 512 * hh:512 * hh + 512], op=ALU.mult),
                            reads=(pk(bk), "gt2b"), writes=(yk2,))
            S.dma("sp", y_sorted[NB * sgi:NB * sgi + NB, :].rearrange("(j p) d -> p j d", p=128), ysb[pb2][:],
                  reads=(yk2,), writes=("y_sorted",), key="y_sorted")
        S.barrier()
        c2.close()

        c3 = ExitStack()
        yg = [sb("yg%d" % i, [128, 4, 1024], F32, c3) for i in range(2)]
        x1b = [sb("x1b%d" % i, [128, 4, 1024], F32, c3) for i in range(2)]
        junkF = sb("junkF", [128, 1024], BF16, c3)
        ssF = sb("ssF", [128, 8, 4], F32, c3)
        rsF = sb("rsF", [128, 8, 4], F32, c3)
        for blk in range(8):
            pb2 = blk % 2
            yk3, xk3, sfk = "yg%d" % pb2, "x1b%d" % pb2, "ssF%d" % blk
            S.dma("sp", x1b[pb2][:], x1_scr[NB * blk:NB * blk + NB, :].rearrange("(j p) d -> p j d", p=128), writes=(xk3,), key=xk3)
            for j in range(4):
                S.idma("pool", yg[pb2][:, j, :], None, y_sorted[:, :], bass.IndirectOffsetOnAxis(ap=slot_i[:, 4 * blk + j:4 * blk + j + 1], axis=0),
                       reads=("slot_i",), writes=(yk3,), key=yk3)
            S.op("pool", lambda e, blk=blk: e.memset(ssF[:, blk, :], 0.0), writes=(sfk,))
            for j in range(4):
                S.op("dve", lambda e, j=j, pb2=pb2: e.tensor_tensor(out=x1b[pb2][:, j, :], in0=x1b[pb2][:, j, :], in1=yg[pb2][:, j, :], op=ALU.add),
                     reads=(xk3, yk3), writes=(xk3,))
                S.op("act", lambda e, j=j, pb2=pb2, blk=blk: e.activation(out=junkF[:], in_=x1b[pb2][:, j, :], func=AF.Square,
                                                                          accum_out=ssF[:, blk, j:j + 1]),
                     reads=(xk3,), writes=("junkF", sfk))
            S.op("act", lambda e, blk=blk: e.activation(out=rsF[:, blk, :], in_=ssF[:, blk, :], func=AF.Sqrt, bias=qtr[:, 1:2]),
                 reads=(sfk, "qtr"), writes=(sfk + "r",))
            S.op("dve", lambda e, blk=blk: e.reciprocal(out=rsF[:, blk, :], in_=rsF[:, blk, :]), reads=(sfk + "r",), writes=(sfk + "r",))
            for j in range(4):
                S.op("dve", lambda e, j=j, pb2=pb2, blk=blk: e.scalar_tensor_tensor(out=x1b[pb2][:, j, :], in0=x1b[pb2][:, j, :],
                                                                                    scalar=rsF[:, blk, j:j + 1], in1=gf32[:],
                                                                                    op0=ALU.mult, op1=ALU.mult),
                     reads=(xk3, sfk + "r", "gf32"), writes=(xk3,))
            S.dma("sp", out[NB * blk:NB * blk + NB, :].rearrange("(j p) d -> p j d", p=128), x1b[pb2][:],
                  reads=(xk3,), writes=("out%d" % blk,), key="out")
        S.barrier()
        c3.close()
        pcg.close()
    return nc


_NC_CACHE = {}


def _fm(v):
    v = np.asarray(v, np.float32).reshape(-1, 128)
    return np.ascontiguousarray(v.T)


def kernel(x, c, ctx, c_ctx, w_ada, b_ada, g_mix, w_in, b_in, conv_w, conv_b, ln_g, ln_b, w_pa,
           lru_conv_w, lru_conv_b, w_r_f, b_r_f, w_i_f, b_i_f, lam_f, w_r_b, b_r_b, w_i_b, b_i_b, lam_b,
           w_pb, w_o, g_ffn, w_grp, b_grp, w_er, b_er, w_gate, w_up, w_down, g_final):
    f = lambda a: np.ascontiguousarray(np.asarray(a, np.float32))
    x, c, ctx, c_ctx = f(x), f(c), f(ctx), f(c_ctx)
    B = x.shape[0]
    if "nc" not in _NC_CACHE:
        _NC_CACHE["nc"] = build_program()
    nc = _NC_CACHE["nc"]

    common = {
        "w_ada": f(w_ada[0]), "b_ada_fm": _fm(b_ada[0]),
        "b_ada_gt": f(np.broadcast_to(np.stack([b_ada[0][2048:3072], b_ada[0][5120:6144]])[None], (128, 2, 1024))),
        "w_in": f(w_in[0]), "b_in_fm": _fm(b_in[0]),
        "cb": _fm(conv_b[0]), "lng": _fm(ln_g[0]), "lnb": _fm(ln_b[0]),
        "w_pa": f(w_pa[0]), "w_pb": f(w_pb[0]), "w_o": f(w_o[0]),
        "lb": _fm(lru_conv_b[0]),
        "gmix": _fm(g_mix[0]), "gffn": _fm(g_ffn[0]),
        "gfin": f(np.broadcast_to(np.asarray(g_final, np.float32)[None], (128, 1024))),
        "w_rt": f(np.concatenate([w_grp[0], w_er[0]], axis=1)),
        "b_rt": f(np.broadcast_to(np.concatenate([b_grp[0], b_er[0]])[None], (128, 20))),
        "w_gate": f(np.asarray(w_gate[0], np.float32).reshape(16, 8, 128, 512).transpose(0, 2, 1, 3).reshape(2048, 4096)),
        "w_up": f(np.asarray(w_up[0], np.float32).reshape(16, 8, 128, 512).transpose(0, 2, 1, 3).reshape(2048, 4096)),
        "w_down": f(np.asarray(w_down[0], np.float32).reshape(16, 4, 128, 1024).transpose(0, 2, 1, 3).reshape(2048, 4096)),
        "ident": np.eye(128, dtype=np.float32),
        "tri": np.triu(np.ones((128, 128), np.float32), 1),
        "cE": (np.arange(4)[None, :] * 128 + np.arange(128)[:, None]).astype(np.float32),
        "sidx": np.broadcast_to(np.arange(12, dtype=np.float32)[None], (128, 12)).copy(),
    }
    cwn = np.asarray(conv_w[0], np.float32)
    lwn = np.asarray(lru_conv_w[0], np.float32)
    zero = np.zeros((1, 1024), np.float32)
    lw5_nat = np.concatenate([lwn, zero], axis=0)
    lw5_rev = lw5_nat[::-1]

    def fm3(a):
        T = a.shape[0]
        return np.ascontiguousarray(a.reshape(T, 8, 128).transpose(2, 1, 0))

    pf = (w_r_f[0], b_r_f[0], w_i_f[0], b_i_f[0], lam_f[0])
    pbk = (w_r_b[0], b_r_b[0], w_i_b[0], b_i_b[0], lam_b[0])

    def gates(P, Sd):
        wgs = np.stack([P[0], P[2], Sd[0], Sd[2]]).astype(np.float32)
        bgs = np.stack([np.asarray(t, np.float32) for t in (P[1], P[3], Sd[1], Sd[3])])
        bgs = np.ascontiguousarray(bgs.transpose(2, 0, 1))
        lams = np.stack([np.asarray(P[4], np.float32).reshape(8, 128), np.asarray(Sd[4], np.float32).reshape(8, 128)])
        lams = np.ascontiguousarray(lams.transpose(2, 0, 1))
        return f(wgs), bgs, lams

    per_half = []
    for half in range(2):
        if half == 0:
            wgs, bgs, lams = gates(pf, pbk)
            d = {"cw": fm3(cwn), "lw5": fm3(lw5_nat), "wg": wgs, "bg": bgs, "lam": lams}
        else:
            wgs, bgs, lams = gates(pbk, pf)
            d = {"cw": fm3(cwn[::-1]), "lw5": fm3(lw5_rev), "wg": wgs, "bg": bgs, "lam": lams}
        per_half.append(d)

    in_maps = []
    pad2 = np.zeros((2, 1024), np.float32)
    for b in range(B):
        for half in range(2):
            xs = x[b] if half == 0 else x[b, ::-1]
            cs_ = ctx[b] if half == 0 else ctx[b, ::-1]
            m = dict(common)
            m.update(per_half[half])
            m["xp"] = np.ascontiguousarray(np.concatenate([pad2, xs, pad2], axis=0))
            m["ctxp"] = np.ascontiguousarray(np.concatenate([pad2, cs_, pad2], axis=0))
            m["cvec"] = np.ascontiguousarray(np.stack([_fm(c[b]), _fm(c_ctx)], axis=-1))
            in_maps.append(m)
    res = run_bass_kernel_spmd(nc, in_maps, core_ids=list(range(2 * B)))
    outp = np.empty((B, 2 * NOWN, 1024), np.float32)
    for b in range(B):
        outp[b, :NOWN] = res.results[2 * b]["out"]
        outp[b, NOWN:] = res.results[2 * b + 1]["out"][::-1]
    if DEBUG:
        kernel.last = res
    return outp
```

```python
from contextlib import ExitStack
import os
import numpy as np
import concourse.bass as bass
import concourse.mybir as mybir
from concourse.bass_utils import run_bass_kernel_spmd

F32 = mybir.dt.float32
BF16 = mybir.dt.bfloat16
AF = mybir.ActivationFunctionType
ALU = mybir.AluOpType
AX = mybir.AxisListType
EPS = 1e-6
NB = 512
NOWN = 4096
DEBUG = bool(int(os.environ.get("MK_DEBUG", "0")))


class _Rec:
    def __init__(self):
        self.calls = []

    def __getattr__(self, name):
        def f(*args, **kw):
            self.calls.append((name, args, kw))
            return self
        return f


_TBL = {"Exp": "exp", "Tanh": None, "Identity": None, "Copy": None, "Square": None, "Sqrt": "sqrt", "Silu": "silu",
        "Gelu_apprx_tanh": "gelu", "Ln": "ln"}


def _fsize(ap):
    n = 1
    for d in ap.shape[1:]:
        n *= int(d)
    return n


class Sched:
    REORDER = True
    WINDOW = 600

    def __init__(self, nc, es):
        self.nc = nc
        self.es = es
        self.E = dict(pe=nc.tensor, act=nc.scalar, dve=nc.vector, pool=nc.gpsimd, sp=nc.sync)
        self.sem = {e: es.enter_context(nc.semaphore("c_" + e)) for e in self.E}
        self.cnt = {e: 0 for e in self.E}
        self.seen = {e: {} for e in self.E}
        self.lastw = {}
        self.readers = {}
        self.dsem = {}
        self.dcnt = {}
        self.ops = []

    def op(self, e, fn, reads=(), writes=()):
        r = _Rec()
        fn(r)
        self._add(e, "op", r.calls, tuple(reads), tuple(writes), None)

    def group(self, e, fns, reads=(), writes=()):
        r = _Rec()
        for f in fns:
            f(r)
        self._add(e, "op", r.calls, tuple(reads), tuple(writes), None)

    def dma(self, q, out, in_, reads=(), writes=(), key=None):
        self._add(q, "dma", [("dma_start", (), dict(out=out, in_=in_))], tuple(reads), tuple(writes), key)

    def idma(self, q, out, out_offset, in_, in_offset, reads=(), writes=(), key=None):
        self._add(q, "dma", [("indirect_dma_start", (), dict(out=out, out_offset=out_offset, in_=in_, in_offset=in_offset))],
                  tuple(reads), tuple(writes), key)

    def _add(self, e, kind, calls, reads, writes, key):
        dur = 0.0
        tbl = None
        if kind == "dma":
            kw0 = calls[0][2]
            side = kw0["in_"] if kw0.get("out_offset") is not None else kw0["out"]
            nb = 128 * _fsize(side) * 4
            dur = 1000.0 if e == "pool" else 150.0
            lat = 2000.0 + nb / 300.0
        else:
            lat = 0.0
            for (name, args, kw) in calls:
                if e == "pe":
                    src = kw.get("rhs", kw.get("in_"))
                    dur += 25.0 + 0.5 * max(_fsize(src), 64)
                else:
                    oap = kw.get("out", kw.get("ap", args[0] if args else None))
                    n = _fsize(oap)
                    if e == "act":
                        dur += 250.0 + 0.73 * n
                        fnm = kw.get("func")
                        tbl = _TBL.get(getattr(fnm, "name", str(fnm)), None) if fnm is not None else None
                    elif e == "dve":
                        dur += 160.0 + 1.04 * n
                    else:
                        dur += 300.0 + 3.1 * n
        self.ops.append(dict(e=e, kind=kind, calls=calls, reads=reads, writes=writes, key=key, dur=dur, lat=lat, tbl=tbl))

    def flush(self):
        ops = self.ops
        self.ops = []
        n = len(ops)
        if n == 0:
            return
        lastw, readers = {}, {}
        preds = [None] * n
        succs = [[] for _ in range(n)]
        for i, o in enumerate(ops):
            p = set()
            for k in o["reads"]:
                if k in lastw:
                    p.add(lastw[k])
            for k in o["writes"]:
                if k in lastw:
                    p.add(lastw[k])
                p.update(readers.get(k, ()))
            p.discard(i)
            preds[i] = p
            for j in p:
                succs[j].append(i)
            for k in o["reads"]:
                readers.setdefault(k, []).append(i)
            for k in o["writes"]:
                lastw[k] = i
                readers[k] = []
        if not self.REORDER:
            order = range(n)
        else:
            indeg = [len(p) for p in preds]
            ready = [i for i in range(n) if indeg[i] == 0]
            finish = [0.0] * n
            efree = {e: 0.0 for e in self.E}
            etbl = [None]
            done = [False] * n
            lo = 0
            order = []
            while len(order) < n:
                while lo < n and done[lo]:
                    lo += 1
                best, bkey = None, None
                for i in ready:
                    if i > lo + self.WINDOW:
                        continue
                    o = ops[i]
                    st = efree[o["e"]]
                    for j in preds[i]:
                        f = finish[j] + (0.0 if ops[j]["e"] == o["e"] else 120.0)
                        if f > st:
                            st = f
                    if o["e"] == "act" and o["tbl"] is not None and o["tbl"] != etbl[0]:
                        st += 1300.0
                    kk = (st, i)
                    if bkey is None or kk < bkey:
                        best, bkey = i, kk
                i = best
                o = ops[i]
                st = bkey[0]
                if os.environ.get("MK_TL") and n > 3000 and len(ops) == int(os.environ.get("MK_TL")):
                    lim = None
                    for j in preds[i]:
                        f = finish[j]
                        if lim is None or f > lim[0]:
                            lim = (f, j)
                    gap = st - efree[o["e"]]
                    if o["e"] == "pe" and gap > 300:
                        print("PE gap %.1fus at t=%.1fus op#%d writes=%s waits for %s op#%d writes=%s" % (
                            gap / 1e3, st / 1e3, i, o["writes"][:2], ops[lim[1]]["e"], lim[1], ops[lim[1]]["writes"][:2]))
                if o["e"] == "act" and o["tbl"] is not None:
                    etbl[0] = o["tbl"]
                efree[o["e"]] = st + o["dur"]
                finish[i] = st + o["dur"] + o["lat"]
                done[i] = True
                ready.remove(i)
                order.append(i)
                for j in succs[i]:
                    indeg[j] -= 1
                    if indeg[j] == 0:
                        ready.append(j)
        if self.REORDER and os.environ.get("MK_STATS"):
            busy = {e: 0.0 for e in self.E}
            for o in ops:
                busy[o["e"]] += o["dur"]
            print("phase: n=%d est_makespan=%.0fus busy(us): %s" % (n, max(finish) / 1e3, {e: int(v / 1e3) for e, v in busy.items()}))
        for i in order:
            self._emit(ops[i])

    def _wait(self, e, tok, same_ok=False):
        if tok is None:
            return
        name, sem, val, src = tok
        if same_ok and src == e:
            return
        d = self.seen[e]
        if d.get(name, 0) >= val:
            return
        self.E[e].wait_ge(sem, val)
        d[name] = val

    def _emit(self, o):
        e, reads, writes = o["e"], o["reads"], o["writes"]
        for k in reads:
            self._wait(e, self.lastw.get(k))
        for k in writes:
            self._wait(e, self.lastw.get(k), same_ok=True)
            for t in self.readers.get(k, {}).values():
                self._wait(e, t, same_ok=True)
        ins = None
        for (name, args, kw) in o["calls"]:
            ins = getattr(self.E[e], name)(*args, **kw)
        if o["kind"] == "dma":
            key = o["key"]
            if key not in self.dsem:
                self.dsem[key] = self.es.enter_context(self.nc.semaphore("d_" + key))
                self.dcnt[key] = 0
            self.dcnt[key] += 16
            ins.then_inc(self.dsem[key], 16)
            tok = ("d_" + key, self.dsem[key], self.dcnt[key], "dma")
        else:
            self.cnt[e] += 1
            ins.then_inc(self.sem[e], 1)
            tok = ("c_" + e, self.sem[e], self.cnt[e], e)
        for k in reads:
            self.readers.setdefault(k, {})[tok[0]] = tok
        for k in writes:
            self.lastw[k] = tok
            self.readers[k] = {}

    def barrier(self):
        self.flush()
        for e in self.E:
            for e2 in self.E:
                if self.cnt[e2] > 0:
                    self._wait(e, ("c_" + e2, self.sem[e2], self.cnt[e2], e2))
            for k, sem in self.dsem.items():
                self._wait(e, ("d_" + k, sem, self.dcnt[k], "dma"))


def build_program():
    nc = bass.Bass("TRN2", target_bir_lowering=False)

    def din(name, shape):
        return nc.dram_tensor(name, list(shape), F32, kind="ExternalInput").ap()

    xp = din("xp", [8196, 1024])
    ctxp = din("ctxp", [260, 1024])
    cvec = din("cvec", [128, 8, 2])
    w_ada = din("w_ada", [1024, 6144])
    b_ada_fm = din("b_ada_fm", [128, 48])
    b_ada_gt = din("b_ada_gt", [128, 2, 1024])
    w_in = din("w_in", [1024, 6144])
    b_in_fm = din("b_in_fm", [128, 48])
    cw = din("cw", [128, 8, 31])
    cb = din("cb", [128, 8])
    lng = din("lng", [128, 8])
    lnb = din("lnb", [128, 8])
    w_pa = din("w_pa", [1024, 1024])
    w_pb = din("w_pb", [1024, 1024])
    w_o = din("w_o", [1024, 1024])
    lw5 = din("lw5", [128, 8, 5])
    lb = din("lb", [128, 8])
    wg = din("wg", [4, 8, 128, 128])
    bg = din("bg", [128, 4, 8])
    lam = din("lam", [128, 2, 8])
    gmix = din("gmix", [128, 8])
    gffn = din("gffn", [128, 8])
    gfin = din("gfin", [128, 1024])
    w_rt = din("w_rt", [1024, 20])
    b_rt = din("b_rt", [128, 20])
    w_gate = din("w_gate", [2048, 4096])
    w_up = din("w_up", [2048, 4096])
    w_down = din("w_down", [2048, 4096])
    ident = din("ident", [128, 128])
    tri = din("tri", [128, 128])
    cE = din("cE", [128, 4])
    sidx = din("sidx", [128, 12])
    out = nc.dram_tensor("out", [NOWN, 1024], F32, kind="ExternalOutput").ap()
    if DEBUG:
        hs_scr = nc.dram_tensor("hs_scr", [8, 128, 8, NB], BF16, kind="ExternalOutput").ap()
        x1_scr = nc.dram_tensor("x1_scr", [NOWN, 1024], F32, kind="ExternalOutput").ap()
    else:
        hs_scr = nc.dram_tensor("hs_scr", [8, 128, 8, NB], BF16, kind="Internal").ap()
        x1_scr = nc.dram_tensor("x1_scr", [NOWN, 1024], F32, kind="Internal").ap()
    gt2_scr = nc.dram_tensor("gt2_scr", [128, 1024], F32, kind="Internal").ap()
    NSEG = 12
    xs_sorted = nc.dram_tensor("xs_sorted", [NSEG * NB, 1024], BF16, kind="Internal").ap()
    ws_sorted = nc.dram_tensor("ws_sorted", [NSEG * NB, 4], F32, kind="Internal").ap()
    y_sorted = nc.dram_tensor("y_sorted", [NSEG * NB, 1024], F32, kind="Internal").ap()
    I32 = mybir.dt.int32

    with ExitStack() as es:
        S = Sched(nc, es)

        def sb(name, shape, dt=F32, stack=es):
            return stack.enter_context(nc.sbuf_tensor(name, list(shape), dt))

        psA = es.enter_context(nc.psum_tensor("psA", [128, 2048], F32))
        psB = es.enter_context(nc.psum_tensor("psB", [128, 2048], F32))

        def bank(i):
            t = psA if i < 4 else psB
            return t[:, 512 * (i % 4):512 * (i % 4) + 512]

        def pk(i):
            return "ps%d" % i

        tpv = psA[:, :].bitcast(BF16).rearrange("p (c t) -> p c t", t=512)
        TPK = ("ps0", "ps1", "ps2", "ps3")

        identb = sb("identb", [128, 128], BF16)
        ones_m = sb("ones_m", [128, 128], BF16)
        ones1 = sb("ones1", [128, 128], BF16)
        b_in_sb = sb("b_in_sb", [128, 48])
        hb_in = sb("hb_in", [128, 48])
        cw_sb = sb("cw_sb", [128, 8, 31])
        cb_sb = sb("cb_sb", [128, 8])
        lng_sb = sb("lng_sb", [128, 8])
        lnb_sb = sb("lnb_sb", [128, 8])
        lw5_sb = sb("lw5_sb", [128, 8, 5])
        lb_sb = sb("lb_sb", [128, 8])
        bg_sb = sb("bg_sb", [128, 4, 8])
        hbg = sb("hbg", [128, 4, 8])
        lam_sb = sb("lam_sb", [128, 2, 8])
        gmix_sb = sb("gmix_sb", [128, 8])
        gffn_sb = sb("gffn_sb", [128, 8])
        b_rt_sb = sb("b_rt_sb", [128, 20])
        b_ada_fm_sb = sb("b_ada_fm_sb", [128, 48])
        cvec_sb = sb("cvec_sb", [128, 8, 2])
        mods = sb("mods", [128, 48, 2])
        s1 = sb("s1", [128, 8])
        s1c = sb("s1c", [128, 8])
        s2 = sb("s2", [128, 8])
        gt1h = sb("gt1h", [128, 1024])
        cl = sb("cl", [128, 2, 8])
        hcl = sb("hcl", [128, 2, 8])
        state = sb("state", [128, 2, 8])
        ss = sb("ss", [128, 8])
        rs = sb("rs", [128, 8])
        w_rt_b = sb("w_rt_b", [128, 8, 20], BF16)
        qtr = sb("qtr", [128, 4], F32)

        def pload(t, src):
            S.dma("sp", t, src, writes=("params",), key="params")

        pload(b_in_sb[:], b_in_fm)
        pload(cw_sb[:], cw)
        pload(cb_sb[:], cb)
        pload(lng_sb[:], lng)
        pload(lnb_sb[:], lnb)
        pload(lw5_sb[:], lw5)
        pload(lb_sb[:], lb)
        pload(bg_sb[:], bg)
        pload(lam_sb[:], lam)
        pload(gmix_sb[:], gmix)
        pload(gffn_sb[:], gffn)
        pload(b_rt_sb[:], b_rt)
        pload(b_ada_fm_sb[:], b_ada_fm)
        pload(cvec_sb[:], cvec)
        S.dma("pool", identb[:], ident, writes=("identb",), key="identb")
        S.dma("pool", w_rt_b[:], w_rt.rearrange("(k p) n -> p k n", p=128), writes=("w_rt_b",), key="w_rt_b")
        S.op("pool", lambda e: e.memset(ones_m[:], 1.0 / 1024.0), writes=("ones_m",))
        S.op("pool", lambda e: e.memset(ones1[:], 1.0), writes=("ones1",))
        S.op("pool", lambda e: e.memset(qtr[:, 0:1], 0.25), writes=("qtr",))
        S.op("pool", lambda e: e.memset(qtr[:, 1:2], 1024.0 * EPS), writes=("qtr",))
        S.op("pool", lambda e: e.memset(qtr[:, 2:3], EPS), writes=("qtr",))
        S.op("pool", lambda e: e.memset(state[:], 0.0), writes=("state",))
        S.op("pool", lambda e: e.memset(ss[:], 0.0), writes=("ss",))

        with ExitStack() as p0:
            cs = sb("cs", [128, 8, 2], BF16, p0)
            cs_rep = sb("cs_rep", [128, 8, 128], BF16, p0)
            b_ada_gt_sb = sb("b_ada_gt_sb", [128, 2, 1024], F32, p0)
            wa = [sb("wa%d" % i, [128, 8, 512], BF16, p0) for i in range(3)]
            e_t = sb("e_t", [128, 16], F32, p0)
            t_t = sb("t_t", [128, 16], F32, p0)
            l_t = sb("l_t", [128, 16], F32, p0)
            m_t = sb("m_t", [128, 16], F32, p0)
            pload(b_ada_gt_sb[:], b_ada_gt)
            gt2b = sb("gt2b0", [128, 1024], F32, p0)

            S.op("act", lambda e: e.activation(out=cs[:], in_=cvec_sb[:], func=AF.Silu), reads=("params",), writes=("cs",))
            S.op("dve", lambda e: e.tensor_copy(out=cs_rep[:], in_=cs[:, :, 0:1].to_broadcast([128, 8, 128])),
                 reads=("cs",), writes=("cs_rep",))
            psm = bank(0)[:, 0:96].rearrange("p (j t) -> p j t", t=2)
            for q in range(12):
                s = q % 3
                S.dma("pool", wa[s][:], w_ada[:, 512 * q:512 * q + 512].rearrange("(k p) n -> p k n", p=128),
                      writes=("wa%d" % s,), key="wa%d" % s)
                fns = []
                for jj in range(4):
                    for k in range(8):
                        fns.append(lambda e, jj=jj, k=k, s=s, q=q: e.matmul(
                            psm[:, 4 * q + jj, :], lhsT=wa[s][:, k, 128 * jj:128 * jj + 128], rhs=cs[:, k, :],
                            start=(k == 0), stop=(k == 7)))
                S.group("pe", fns, reads=("wa%d" % s, "cs"), writes=("ps0",))
                if q in (4, 5, 10, 11):
                    bk = 1 + (q % 2)
                    S.group("pe", [lambda e, k=k, s=s, bk=bk: e.matmul(bank(bk), lhsT=cs_rep[:, k, :], rhs=wa[s][:, k, :],
                                                                      start=(k == 0), stop=(k == 7)) for k in range(8)],
                            reads=("wa%d" % s, "cs_rep"), writes=(pk(bk),))
                    dst = gt1h if q < 6 else gt2b
                    gi = 0 if q < 6 else 1
                    cols = slice(512 * (q % 2), 512 * (q % 2) + 512)
                    S.op("dve", lambda e, dst=dst, gi=gi, cols=cols, bk=bk: e.tensor_tensor(
                        out=dst[:, cols], in0=bank(bk), in1=b_ada_gt_sb[:, gi, cols], op=ALU.add),
                        reads=(pk(bk), "params"), writes=("gt",))
            S.op("dve", lambda e: e.tensor_scalar_mul(out=gt1h[:], in0=gt1h[:], scalar1=0.5), reads=("gt",), writes=("gt",))
            S.dma("sp", gt2_scr, gt2b[:], reads=("gt",), writes=("gt2_scr",), key="gt2_scr")
            S.op("dve", lambda e: e.tensor_tensor(out=mods[:], in0=psm, in1=b_ada_fm_sb[:].unsqueeze(2).to_broadcast([128, 48, 2]),
                                                  op=ALU.add), reads=("ps0", "params"), writes=("mods",))
            for (dst, col, j0, gsb) in ((s1, 0, 8, gmix_sb), (s1c, 1, 8, gmix_sb), (s2, 0, 32, gffn_sb)):
                S.op("dve", lambda e, dst=dst, col=col, j0=j0, gsb=gsb: e.scalar_tensor_tensor(
                    out=dst[:], in0=mods[:, j0:j0 + 8, col], scalar=1.0, in1=gsb[:], op0=ALU.add, op1=ALU.mult),
                    reads=("mods", "params"), writes=("sc",))
                S.op("dve", lambda e, dst=dst: e.tensor_scalar_mul(out=dst[:], in0=dst[:], scalar1=32.0),
                     reads=("sc",), writes=("sc",))
            S.op("dve", lambda e: e.tensor_scalar_mul(out=hb_in[:], in0=b_in_sb[:], scalar1=0.5), reads=("params",), writes=("hb_in",))
            S.op("dve", lambda e: e.tensor_scalar_mul(out=hbg[:], in0=bg_sb[:], scalar1=0.5), reads=("params",), writes=("hbg",))
            lamf = lam_sb[:].rearrange("p a b -> p (a b)")
            S.op("act", lambda e: e.activation(out=e_t[:], in_=lamf, func=AF.Exp, scale=-1.0), reads=("params",), writes=("e_t",))
            S.op("dve", lambda e: e.tensor_scalar(out=t_t[:], in0=e_t[:], scalar1=-0.25, scalar2=1.0 / 3.0, op0=ALU.mult, op1=ALU.add),
                 reads=("e_t",), writes=("t_t",))
            S.op("dve", lambda e: e.tensor_tensor(out=t_t[:], in0=t_t[:], in1=e_t[:], op=ALU.mult), reads=("t_t", "e_t"), writes=("t_t",))
            S.op("dve", lambda e: e.tensor_scalar_add(out=t_t[:], in0=t_t[:], scalar1=-0.5), reads=("t_t",), writes=("t_t",))
            S.op("dve", lambda e: e.tensor_tensor(out=t_t[:], in0=t_t[:], in1=e_t[:], op=ALU.mult), reads=("t_t", "e_t"), writes=("t_t",))
            S.op("dve", lambda e: e.tensor_scalar_add(out=t_t[:], in0=t_t[:], scalar1=1.0), reads=("t_t",), writes=("t_t",))
            S.op("dve", lambda e: e.tensor_tensor(out=t_t[:], in0=t_t[:], in1=e_t[:], op=ALU.mult), reads=("t_t", "e_t"), writes=("t_t",))
            S.op("dve", lambda e: e.tensor_scalar_add(out=l_t[:], in0=e_t[:], scalar1=1.0), reads=("e_t",), writes=("l_t",))
            S.op("act", lambda e: e.activation(out=l_t[:], in_=l_t[:], func=AF.Ln), reads=("l_t",), writes=("l_t",))
            S.op("dve", lambda e: e.tensor_single_scalar(out=m_t[:], in_=e_t[:], scalar=0.1, op=ALU.is_lt), reads=("e_t",), writes=("m_t",))
            S.op("dve", lambda e: e.tensor_tensor(out=t_t[:], in0=t_t[:], in1=l_t[:], op=ALU.subtract), reads=("t_t", "l_t"), writes=("t_t",))
            S.op("dve", lambda e: e.tensor_tensor(out=t_t[:], in0=t_t[:], in1=m_t[:], op=ALU.mult), reads=("t_t", "m_t"), writes=("t_t",))
            S.op("dve", lambda e: e.tensor_tensor(out=t_t[:], in0=t_t[:], in1=l_t[:], op=ALU.add), reads=("t_t", "l_t"), writes=("t_t",))
            clf = cl[:].rearrange("p a b -> p (a b)")
            hclf = hcl[:].rearrange("p a b -> p (a b)")
            S.op("dve", lambda e: e.tensor_scalar_mul(out=clf, in0=t_t[:], scalar1=-8.0), reads=("t_t",), writes=("cl",))
            S.op("dve", lambda e: e.tensor_scalar_mul(out=hclf, in0=t_t[:], scalar1=-4.0), reads=("t_t",), writes=("cl",))
            S.barrier()

        mixer = ExitStack()
        wxr = sb("wxr", [128, 8, 1024], BF16, mixer)
        wgb = sb("wgb", [128, 4, 8, 128], BF16, mixer)
        dg5 = sb("dg5", [128, 8, 5, 128], BF16, mixer)
        S.dma("pool", wxr[:], w_in[:, 3072:4096].rearrange("(k p) n -> p k n", p=128), writes=("wxr",), key="wxr")
        S.dma("pool", wgb[:], wg.rearrange("g h p n -> p g h n"), writes=("wgb",), key="wgb")
        for c in range(8):
            S.op("dve", lambda e, c=c: e.tensor_tensor(
                out=dg5[:, c, :, :], in0=identb[:].unsqueeze(1).to_broadcast([128, 5, 128]),
                in1=lw5_sb[:, c, :].unsqueeze(2).to_broadcast([128, 5, 128]), op=ALU.mult),
                reads=("identb", "params"), writes=("dg5",))

        xt = sb("xt", [128, 4, 1024], F32, mixer)
        xh = sb("xh", [4, 1024], F32, mixer)
        xn = sb("xn", [128, 4, 1024], BF16, mixer)
        xnh = sb("xnh", [4, 1024], BF16, mixer)
        hxTs = [sb("hxT%d" % i, [128, 8, NB + 4], BF16, mixer) for i in range(2)]
        hxT, hxk, hxhk = hxTs[0], "hxT0", "hxTh0"
        hbc = [0]
        xrp = [sb("xrp%d" % i, [128, NB + 4], BF16, mixer) for i in range(2)]
        xcb = [sb("xcb%d" % i, [128, NB], BF16, mixer) for i in range(2)]
        tr = sb("tr", [128, NB], F32, mixer)
        ti = sb("ti", [128, NB], F32, mixer)
        a4 = sb("a4", [128, 4, NB], F32, mixer)
        s4 = sb("s4", [128, 4, NB], F32, mixer)
        t4 = sb("t4", [128, 4, NB], BF16, mixer)
        tmp1 = sb("tmp1", [128, NB], F32, mixer)
        bb_t = sb("bb_t", [128, NB], F32, mixer)
        hf = sb("hf", [128, NB], F32, mixer)
        hsb = sb("hsb", [128, 8, NB], BF16, mixer)

        tph = bank(4).bitcast(BF16)[:, 0:32].rearrange("p (c t) -> p c t", t=4)

        def prep(xsrc, r0, N, sc, bcol, keep_key):
            nt = N // 128
            S.dma("sp", xt[:, 0:nt, :], xsrc[r0:r0 + N, :].rearrange("(j p) d -> p j d", p=128), writes=(keep_key,), key="xt")
            S.dma("sp", xh[0:2, :], xsrc[r0 - 2:r0, :], writes=("xh",), key="xh")
            S.dma("sp", xh[2:4, :], xsrc[r0 + N:r0 + N + 2, :], writes=("xh",), key="xh")
            S.op("pool", lambda e: e.memset(ss[:], 0.0), writes=("ss",))
            for j in range(nt):
                S.op("act", lambda e, j=j: e.activation(out=xn[:, j, :], in_=xt[:, j, :], func=AF.Square, accum_out=ss[:, j:j + 1]),
                     reads=(keep_key,), writes=("xn", "ss"))
            S.op("act", lambda e: e.activation(out=xnh[:], in_=xh[:], func=AF.Square, accum_out=ss[0:4, 4:5]),
                 reads=("xh",), writes=("xnh", "ss"))
            S.op("act", lambda e: e.activation(out=rs[:, 0:5], in_=ss[:, 0:5], func=AF.Sqrt, bias=qtr[:, 1:2]), reads=("ss", "qtr"), writes=("rs",))
            S.op("dve", lambda e: e.reciprocal(out=rs[:, 0:5], in_=rs[:, 0:5]), reads=("rs",), writes=("rs",))
            for j in range(nt):
                S.op("dve", lambda e, j=j: e.tensor_scalar_mul(out=xn[:, j, :], in0=xt[:, j, :], scalar1=rs[:, j:j + 1]),
                     reads=(keep_key, "rs"), writes=("xn",))
            S.op("dve", lambda e: e.tensor_scalar_mul(out=xnh[:], in0=xh[:], scalar1=rs[0:4, 4:5]),
                 reads=("xh", "rs"), writes=("xnh",))
            for j in range(nt):
                S.group("pe", [lambda e, j=j, c=c: e.transpose(out=tpv[:, c, 128 * j:128 * j + 128],
                                                               in_=xn[:, j, 128 * c:128 * c + 128], identity=identb[:])
                               for c in range(8)], reads=("xn", "identb"), writes=TPK)
            S.group("pe", [lambda e, c=c: e.transpose(out=tph[:, c, :], in_=xnh[:, 128 * c:128 * c + 128], identity=identb[0:4, 0:4])
                           for c in range(8)], reads=("xnh", "identb"), writes=("ps4",))
            for c in range(8):
                S.op("act", lambda e, c=c: e.activation(out=hxT[:, c, 0:N], in_=tpv[:, c, 0:N], func=AF.Identity,
                                                        scale=sc[:, c:c + 1], bias=mods[:, c, bcol:bcol + 1]),
                     reads=TPK + ("sc", "mods"), writes=(hxk,))
            S.op("dve", lambda e: e.tensor_tensor(out=hxT[:, :, N:N + 4], in0=tph, in1=sc[:].unsqueeze(2).to_broadcast([128, 8, 4]),
                                                  op=ALU.mult), reads=("ps4", "sc"), writes=(hxhk,))
            S.op("dve", lambda e: e.tensor_tensor(out=hxT[:, :, N:N + 4], in0=hxT[:, :, N:N + 4],
                                                  in1=mods[:, 0:8, bcol:bcol + 1].to_broadcast([128, 8, 4]), op=ALU.add),
                 reads=(hxhk, "mods"), writes=(hxhk,))

        xr_evac_eng = ["dve"]

        def rglru_block(N, d, reverse, has_lo, has_hi, consumer=None):
            def st1(c):
                par = c % 2
                bxr = bank(par)[:, 0:N]
                bxh = bank(2 + par)[:, 0:4]
                S.group("pe", [lambda e, k=k: e.matmul(bxr, lhsT=wxr[:, k, 128 * c:128 * c + 128], rhs=hxT[:, k, 0:N],
                                                       start=(k == 0), stop=(k == 7)) for k in range(8)],
                        reads=(hxk, "wxr"), writes=(pk(par),))
                S.group("pe", [lambda e, k=k: e.matmul(bxh, lhsT=wxr[:, k, 128 * c:128 * c + 128], rhs=hxT[:, k, N:N + 4],
                                                       start=(k == 0), stop=(k == 7)) for k in range(8)],
                        reads=(hxhk, "wxr"), writes=(pk(2 + par),))
                xk = "xrp%d" % par
                bia = b_in_sb[:, 24 + c:25 + c]
                if xr_evac_eng[0] == "act":
                    S.op("act", lambda e: e.activation(out=xrp[par][:, 2:2 + N], in_=bxr, func=AF.Identity, bias=bia),
                         reads=(pk(par), "params"), writes=(xk,))
                else:
                    S.op("dve", lambda e: e.tensor_scalar_add(out=xrp[par][:, 2:2 + N], in0=bxr, scalar1=bia),
                         reads=(pk(par), "params"), writes=(xk,))
                if has_lo:
                    S.op("dve", lambda e: e.tensor_scalar_add(out=xrp[par][:, 0:2], in0=bxh[:, 0:2], scalar1=bia),
                         reads=(pk(2 + par), "params"), writes=(xk,))
                else:
                    S.op("dve", lambda e: e.memset(xrp[par][:, 0:2], 0.0), writes=(xk,))
                if has_hi:
                    S.op("dve", lambda e: e.tensor_scalar_add(out=xrp[par][:, 2 + N:4 + N], in0=bxh[:, 2:4], scalar1=bia),
                         reads=(pk(2 + par), "params"), writes=(xk,))
                else:
                    S.op("dve", lambda e: e.memset(xrp[par][:, 2 + N:4 + N], 0.0), writes=(xk,))

            def st2(c):
                par = c % 2
                xk = "xrp%d" % par
                bcv = bank(4 + par)[:, 0:N]
                S.group("pe", [lambda e, j=j: e.matmul(bcv, lhsT=dg5[:, c, j, :], rhs=xrp[par][:, j:j + N],
                                                       start=(j == 0), stop=(j == 4)) for j in range(5)],
                        reads=(xk, "dg5"), writes=(pk(4 + par),))
                S.op("dve", lambda e: e.tensor_scalar_add(out=xcb[par][:, 0:N], in0=bcv, scalar1=lb_sb[:, c:c + 1]),
                     reads=(pk(4 + par), "params"), writes=("xcb%d" % par,))

            def st3(c):
                par = c % 2
                q = c % 4
                ck = "xcb%d" % par
                br_ = bank(6)[:, 0:N]
                bi_ = bank(7)[:, 0:N]
                S.group("pe", [lambda e: e.matmul(br_, lhsT=wgb[:, 2 * d, c, :], rhs=xcb[par][:, 0:N], start=True, stop=True)],
                        reads=(ck, "wgb"), writes=("ps6",))
                S.group("pe", [lambda e: e.matmul(bi_, lhsT=wgb[:, 2 * d + 1, c, :], rhs=xcb[par][:, 0:N], start=True, stop=True)],
                        reads=(ck, "wgb"), writes=("ps7",))
                S.op("act", lambda e: e.activation(out=tr[:, 0:N], in_=br_, func=AF.Tanh, scale=0.5, bias=hbg[:, 2 * d, c:c + 1]),
                     reads=("ps6", "hbg"), writes=("tr",))
                S.op("act", lambda e: e.activation(out=ti[:, 0:N], in_=bi_, func=AF.Tanh, scale=0.5, bias=hbg[:, 2 * d + 1, c:c + 1]),
                     reads=("ps7", "hbg"), writes=("ti",))
                S.op("act", lambda e: e.activation(out=a4[:, q, 0:N], in_=tr[:, 0:N], func=AF.Exp, scale=hcl[:, d, c:c + 1],
                                                   bias=hcl[:, d, c:c + 1]), reads=("tr", "cl"), writes=("a4_%d" % q,))
                S.op("pool", lambda e: e.tensor_tensor(out=s4[:, q, 0:N], in0=a4[:, q, 0:N], in1=a4[:, q, 0:N], op=ALU.mult),
                     reads=("a4_%d" % q,), writes=("s4_%d" % q,))
                S.op("dve", lambda e: e.scalar_tensor_tensor(out=t4[:, q, 0:N], in0=ti[:, 0:N], scalar=1.0, in1=xcb[par][:, 0:N],
                                                             op0=ALU.add, op1=ALU.mult), reads=("ti", ck), writes=("t4_%d" % q,))

            def st4(c0):
                sk = tuple("s4_%d" % q for q in range(4))
                S.op("act", lambda e: e.activation(out=s4[:, :, 0:N], in_=s4[:, :, 0:N], func=AF.Sqrt, scale=-0.25, bias=qtr[:, 0:1]),
                     reads=sk + ("qtr",), writes=sk)
                for c in range(c0, c0 + 4):
                    q = c % 4
                    S.op("pool", lambda e, q=q: e.tensor_tensor(out=bb_t[:, 0:N], in0=s4[:, q, 0:N], in1=t4[:, q, 0:N], op=ALU.mult),
                         reads=("s4_%d" % q, "t4_%d" % q), writes=("bb_t",))
                    if reverse:
                        S.op("dve", lambda e, q=q, c=c: e.tensor_tensor_scan(
                            out=hf[:, 0:N][:, ::-1], data0=a4[:, q, 0:N][:, ::-1], data1=bb_t[:, 0:N][:, ::-1],
                            initial=state[:, d, c:c + 1], op0=ALU.mult, op1=ALU.add),
                            reads=("a4_%d" % q, "bb_t", "state"), writes=("hf",))
                        S.op("pool", lambda e, c=c: e.tensor_copy(out=state[:, d, c:c + 1], in_=hf[:, 0:1]),
                             reads=("hf",), writes=("state",))
                    else:
                        S.op("dve", lambda e, q=q, c=c: e.tensor_tensor_scan(
                            out=hf[:, 0:N], data0=a4[:, q, 0:N], data1=bb_t[:, 0:N], initial=state[:, d, c:c + 1],
                            op0=ALU.mult, op1=ALU.add), reads=("a4_%d" % q, "bb_t", "state"), writes=("hf",))
                        S.op("pool", lambda e, c=c: e.tensor_copy(out=state[:, d, c:c + 1], in_=hf[:, N - 1:N]),
                             reads=("hf",), writes=("state",))
                    if consumer is not None:
                        consumer(c)

            for s_ in range(10):
                if s_ < 8:
                    st1(s_)
                if 1 <= s_ <= 8:
                    st2(s_ - 1)
                if 2 <= s_ <= 9:
                    st3(s_ - 2)
                    if (s_ - 2) % 4 == 3:
                        st4(s_ - 2 - 3)

        hxT, hxk, hxhk = hxTs[0], "hxT0", "hxTh0"
        prep(ctxp, 2, 256, s1c, 1, "xt")
        hbc[0] = 1
        rglru_block(256, 0, False, False, False)
        rglru_block(256, 1, True, False, False)
        for blk in range(15, -1, -1):
            _i = hbc[0] % 2
            hbc[0] += 1
            hxT, hxk, hxhk = hxTs[_i], "hxT%d" % _i, "hxTh%d" % _i
            prep(xp, 2 + NB * blk, NB, s1, 0, "xt")
            def cons_a(c):
                S.op("dve", lambda e, c=c: e.tensor_copy(out=hsb[:, c, :], in_=hf[:]), reads=("hf",), writes=("hsb",))
            rglru_block(NB, 1, True, blk != 0, blk != 15, cons_a if blk < 8 else None)
            if blk < 8:
                S.dma("sp", hs_scr[blk], hsb[:], reads=("hsb",), writes=("hs_scr%d" % blk,), key="hs_scr")

        pb_ = ExitStack()
        cwh = cw_sb
        S.op("dve", lambda e: e.tensor_scalar_mul(out=cwh[:], in0=cw_sb[:], scalar1=0.5), reads=("params",), writes=("cwh",))
        lnr = sb("lnr", [128, NB], F32, pb_)
        lmr = sb("lmr", [128, NB], F32, pb_)
        wsl = [sb("wsl%d" % i, [128, 8, 512], BF16, pb_) for i in range(3)]
        dgc = [sb("dgc0", [128, 31, 128], BF16, pb_)] * 2
        tv, uu = tr, ti
        zb = [sb("zb0", [128, NB], BF16, pb_)] * 2
        zc = sb("zc", [128, 8, NB], BF16, pb_)
        aa = zc
        zsq = [sb("zsq0", [128, NB], BF16, pb_)] * 2
        A_t = sb("A_t", [128, 8, NB], BF16, pb_)
        gy = sb("gy", [128, 8, NB], BF16, pb_)
        mg = gy
        yb = sb("yb", [128, 8, NB], BF16, pb_)
        hs_in = hsb
        x1 = xt

        xres = [sb("xres%d" % i, [128, 512], F32, pb_) for i in range(3)]
        xrc = [0]
        wctr = [0]

        def wpiece(col0, src=None):
            src = w_in if src is None else src
            i = wctr[0] % 3
            wctr[0] += 1
            S.dma("pool", wsl[i][:], src[:, col0:col0 + 512].rearrange("(k p) n -> p k n", p=128),
                  writes=("wsl%d" % i,), key="wsl%d" % i)
            return wsl[i], "wsl%d" % i

        def inproj(dstbank, wt, wk, j4):
            S.group("pe", [lambda e, k=k: e.matmul(bank(dstbank), lhsT=wt[:, k, 128 * j4:128 * j4 + 128], rhs=hxT[:, k, 0:NB],
                                                   start=(k == 0), stop=(k == 7)) for k in range(8)],
                    reads=(hxk, wk), writes=(pk(dstbank),))

        xr_evac_eng[0] = "act"
        for blk in range(8):
            _i = hbc[0] % 2
            hbc[0] += 1
            hxT, hxk, hxhk = hxTs[_i], "hxT%d" % _i, "hxTh%d" % _i
            prep(xp, 2 + NB * blk, NB, s1, 0, "xt")
            S.dma("sp", hs_in[:], hs_scr[blk], reads=("hs_scr%d" % blk,), writes=("hsb",), key="hs_in")
            for half in range(2):
                wu, wuk = wpiece(512 * half)
                wv, wvk = wpiece(1024 + 512 * half)
                for c4 in range(4):
                    c = 4 * half + c4
                    par = c % 2
                    inproj(par, wu, wuk, c4)
                    inproj(2 + par, wv, wvk, c4)
                    S.op("act", lambda e, c=c, par=par: e.activation(out=tv[:], in_=bank(2 + par), func=AF.Tanh, scale=0.5,
                                                                     bias=hb_in[:, 8 + c:9 + c]),
                         reads=(pk(2 + par), "hb_in"), writes=("tr",))
                    S.op("act", lambda e, c=c, par=par: e.activation(out=uu[:], in_=bank(par), func=AF.Identity,
                                                                     bias=b_in_sb[:, c:c + 1]),
                         reads=(pk(par), "params"), writes=("ti",))
                    zk = "zb0"
                    S.op("dve", lambda e, par=par: e.scalar_tensor_tensor(out=zb[par][:], in0=tv[:], scalar=1.0, in1=uu[:],
                                                                          op0=ALU.add, op1=ALU.mult),
                         reads=("tr", "ti"), writes=(zk,))
                    dk = "dgc0"
                    S.op("dve", lambda e, c=c, par=par: e.tensor_tensor(
                        out=dgc[par][:], in0=identb[:].unsqueeze(1).to_broadcast([128, 31, 128]),
                        in1=cwh[:, c, :].unsqueeze(2).to_broadcast([128, 31, 128]), op=ALU.mult),
                        reads=("identb", "cwh"), writes=(dk,))
                    zv = zb[par][:].rearrange("p (r t) -> p r t", t=64)
                    pcv = bank(4 + par).rearrange("p (r t) -> p r t", t=64)
                    fns = []
                    order = [15] + [k for k in range(31) if k != 15]
                    for idx, k in enumerate(order):
                        o = k - 15
                        t0, t1 = max(0, -o), 64 - max(0, o)
                        fns.append(lambda e, k=k, o=o, t0=t0, t1=t1, idx=idx, par=par, pcv=pcv, zv=zv: e.matmul(
                            pcv[:, :, t0:t1], lhsT=dgc[par][:, k, :], rhs=zv[:, :, t0 + o:t1 + o],
                            start=(idx == 0), stop=(idx == 30)))
                    S.group("pe", fns, reads=(zk, dk), writes=(pk(4 + par),))
                    S.op("act", lambda e, c=c, par=par: e.activation(out=zc[:, c, :], in_=bank(4 + par), func=AF.Identity,
                                                                     bias=cb_sb[:, c:c + 1]),
                         reads=(pk(4 + par), "params"), writes=("zc",))
                    qk = "zsq0"
                    S.op("act", lambda e, c=c, par=par: e.activation(out=zsq[par][:], in_=bank(4 + par), func=AF.Square,
                                                                     bias=cb_sb[:, c:c + 1]),
                         reads=(pk(4 + par), "params"), writes=(qk,))
                    S.group("pe", [lambda e, c=c: e.matmul(bank(6), lhsT=ones_m[:], rhs=zc[:, c, :], start=(c == 0), stop=(c == 7))],
                            reads=("zc", "ones_m"), writes=("ps6",))
                    S.group("pe", [lambda e, c=c, par=par: e.matmul(bank(7), lhsT=ones_m[:], rhs=zsq[par][:], start=(c == 0),
                                                                    stop=(c == 7))], reads=(qk, "ones_m"), writes=("ps7",))
            S.op("act", lambda e: e.activation(out=tv[:], in_=bank(6), func=AF.Copy), reads=("ps6",), writes=("tr",))
            S.op("dve", lambda e: e.tensor_tensor(out=uu[:], in0=tv[:], in1=tv[:], op=ALU.mult), reads=("tr",), writes=("ti",))
            S.op("dve", lambda e: e.tensor_tensor(out=lnr[:], in0=bank(7), in1=uu[:], op=ALU.subtract), reads=("ps7", "ti"),
                 writes=("lnr",))
            S.op("act", lambda e: e.activation(out=lnr[:], in_=lnr[:], func=AF.Sqrt, bias=qtr[:, 2:3]), reads=("lnr", "qtr"), writes=("lnr",))
            S.op("dve", lambda e: e.reciprocal(out=lnr[:], in_=lnr[:]), reads=("lnr",), writes=("lnr",))
            S.op("dve", lambda e: e.tensor_tensor(out=lmr[:], in0=tv[:], in1=lnr[:], op=ALU.mult), reads=("tr", "lnr"),
                 writes=("lmr",))
            for half in range(2):
                wy, wyk = wpiece(2048 + 512 * half)
                for c4 in range(4):
                    c = 4 * half + c4
                    par = c % 2
                    inproj(par, wy, wyk, c4)
                    S.op("act", lambda e, c=c, par=par: e.activation(out=gy[:, c, :], in_=bank(par), func=AF.Gelu_apprx_tanh,
                                                                     bias=b_in_sb[:, 16 + c:17 + c]),
                         reads=(pk(par), "params"), writes=("gy",))
            def cons_b(c):
                S.op("dve", lambda e, c=c: e.tensor_tensor(out=tmp1[:], in0=hf[:], in1=hs_in[:, c, :], op=ALU.add),
                     reads=("hf", "hsb"), writes=("tmp1",))
                S.op("dve", lambda e, c=c: e.tensor_tensor(out=yb[:, c, :], in0=tmp1[:], in1=gy[:, c, :], op=ALU.mult),
                     reads=("tmp1", "gy"), writes=("yb",))
            rglru_block(NB, 0, False, blk != 0, True, cons_b)
            for c in range(8):
                S.op("dve", lambda e, c=c: e.tensor_tensor(out=tv[:], in0=zc[:, c, :], in1=lnr[:], op=ALU.mult),
                     reads=("zc", "lnr"), writes=("tr",))
                S.op("dve", lambda e: e.tensor_tensor(out=uu[:], in0=tv[:], in1=lmr[:], op=ALU.subtract), reads=("tr", "lmr"),
                     writes=("ti",))
                S.op("act", lambda e, c=c: e.activation(out=aa[:, c, :], in_=uu[:], func=AF.Silu, scale=lng_sb[:, c:c + 1],
                                                        bias=lnb_sb[:, c:c + 1]), reads=("ti", "params"), writes=("zc",))
            for half in range(2):
                wga, wgak = wpiece(4096 + 512 * half)
                wpa, wpak = wpiece(512 * half, w_pa)
                for m4 in range(4):
                    m = 4 * half + m4
                    par = m % 2
                    S.group("pe", [lambda e, k=k, m4=m4, par=par, wpa=wpa: e.matmul(bank(par), lhsT=wpa[:, k, 128 * m4:128 * m4 + 128],
                                                                         rhs=aa[:, k, :], start=(k == 0), stop=(k == 7))
                                   for k in range(8)], reads=("zc", wpak), writes=(pk(par),))
                    inproj(2 + par, wga, wgak, m4)
                    S.op("act", lambda e, m=m, par=par: e.activation(out=tv[:], in_=bank(2 + par), func=AF.Tanh, scale=0.5,
                                                                     bias=hb_in[:, 32 + m:33 + m]),
                         reads=(pk(2 + par), "hb_in"), writes=("tr",))
                    S.op("dve", lambda e, m=m, par=par: e.scalar_tensor_tensor(out=A_t[:, m, :], in0=tv[:], scalar=1.0,
                                                                               in1=bank(par), op0=ALU.add, op1=ALU.mult),
                         reads=("tr", pk(par)), writes=("A_t",))
            for half in range(2):
                wgb_, wgbk = wpiece(5120 + 512 * half)
                wpb, wpbk = wpiece(512 * half, w_pb)
                for m4 in range(4):
                    m = 4 * half + m4
                    par = m % 2
                    S.group("pe", [lambda e, k=k, m4=m4, par=par, wpb=wpb: e.matmul(bank(par), lhsT=wpb[:, k, 128 * m4:128 * m4 + 128],
                                                                         rhs=yb[:, k, :], start=(k == 0), stop=(k == 7))
                                   for k in range(8)], reads=("yb", wpbk), writes=(pk(par),))
                    inproj(2 + par, wgb_, wgbk, m4)
                    S.op("act", lambda e, m=m, par=par: e.activation(out=tv[:], in_=bank(2 + par), func=AF.Tanh, scale=0.5,
                                                                     bias=hb_in[:, 40 + m:41 + m]),
                         reads=(pk(2 + par), "hb_in"), writes=("tr",))
                    S.op("dve", lambda e, par=par: e.scalar_tensor_tensor(out=uu[:], in0=tv[:], scalar=1.0, in1=bank(par),
                                                                          op0=ALU.add, op1=ALU.mult),
                         reads=("tr", pk(par)), writes=("ti",))
                    S.op("dve", lambda e, m=m: e.tensor_tensor(out=mg[:, m, :], in0=uu[:], in1=A_t[:, m, :], op=ALU.add),
                         reads=("ti", "A_t"), writes=("gy",))
            for hh in range(2):
                wo, wok = wpiece(512 * hh, w_o)
                for j in range(4):
                    bk = 4 + j
                    xi = xrc[0] % 3
                    xrc[0] += 1
                    xk_ = "xres%d" % xi
                    r0_ = 2 + NB * blk + 128 * j
                    S.dma("sp", xres[xi][:], xp[r0_:r0_ + 128, 512 * hh:512 * hh + 512], writes=(xk_,), key=xk_)
                    S.group("pe", [lambda e, k=k, j=j, bk=bk, wo=wo: e.matmul(bank(bk), lhsT=mg[:, k, 128 * j:128 * j + 128],
                                                                             rhs=wo[:, k, :], start=(k == 0), stop=(k == 7))
                                   for k in range(8)], reads=("gy", wok), writes=(pk(bk),))
                    S.op("dve", lambda e, hh=hh, bk=bk: e.tensor_tensor(out=tmp1[:], in0=bank(bk), in1=gt1h[:, 512 * hh:512 * hh + 512],
                                                                        op=ALU.mult), reads=(pk(bk), "gt"), writes=("tmp1",))
                    S.op("pool", lambda e, xi=xi: e.tensor_tensor(out=xres[xi][:], in0=xres[xi][:], in1=tmp1[:], op=ALU.add),
                         reads=("tmp1", xk_), writes=(xk_,))
                    S.dma("sp", x1_scr[NB * blk + 128 * j:NB * blk + 128 * j + 128, 512 * hh:512 * hh + 512], xres[xi][:],
                          reads=(xk_,), writes=("x1_scr%d" % blk,), key="x1_scr")
        S.barrier()
        pb_.close()
        mixer.close()

        def bc(ap, shape):
            return ap.to_broadcast(shape)

        pcg = ExitStack()
        gf32 = sb("gf32", [128, 1024], F32, pcg)
        gt2b = sb("gt2b", [128, 1024], F32, pcg)
        S.dma("sp", gf32[:], gfin, writes=("gf32",), key="gf32")
        S.dma("sp", gt2b[:], gt2_scr, reads=("gt2_scr",), writes=("gt2b",), key="gt2b")
        S.op("dve", lambda e: e.tensor_scalar_mul(out=gf32[:], in0=gf32[:], scalar1=32.0), reads=("gf32",), writes=("gf32",))
        slot_i = sb("slot_i", [128, 32], I32, pcg)
        offE_i = sb("offE_i", [128, NSEG, 4], I32, pcg)
        trib = sb("trib", [128, 128], BF16, pcg)
        S.dma("pool", trib[:], tri, writes=("trib",), key="trib")

        c1 = ExitStack()
        x1l = [sb("x1l%d" % i, [128, 4, 1024], F32, c1) for i in range(2)]
        xn2_all = sb("xn2_all", [128, 32, 1024], BF16, c1)
        hmT1 = sb("hmTr", [128, 8, NB], BF16, c1)
        zt = sb("zt", [128, 4096], BF16, c1)
        ztf = sb("ztf", [128, 192], F32, c1)
        ssA = sb("ssA", [128, 8, 4], F32, c1)
        rsA = sb("rsA", [128, 8, 4], F32, c1)
        oh_all = sb("oh_all", [128, 32, 4], F32, c1)
        wsel_all = sb("wsel_all", [128, 32, 4], F32, c1)
        oh_bf = sb("oh_bf", [128, 32, 4], BF16, c1)
        R1s = sb("R1s", [128, 32, 4], F32, c1)
        Cs = sb("Cs", [128, 32, 4], F32, c1)
        incl = sb("incl", [128, 4, 32], F32, c1)
        onesf = sb("onesf", [128, 32], F32, c1)
        ng = sb("ng", [128, 4], F32, c1)
        nseg = sb("nseg", [128, 4], F32, c1)
        sst = sb("sst", [128, 4], F32, c1)
        sen = sb("sen", [128, 4], F32, c1)
        slot_f = sb("slot_f", [128, 32], F32, c1)
        Gs = sb("Gs", [128, NSEG], F32, c1)
        sidx_sb = sb("sidx_sb", [128, NSEG], F32, c1)
        cE_sb = sb("cE_sb", [128, 4], F32, c1)
        offE_f = sb("offE_f", [128, NSEG, 4], F32, c1)
        L = sb("L", [128, 4, 20], F32, c1)
        gmax = sb("gmax", [128, 4, 1], F32, c1)
        eg = sb("eg", [128, 4, 4], F32, c1)
        pg = sb("pg", [128, 4, 1], F32, c1)
        tmp16 = sb("tmp16", [128, 4, 16], F32, c1)
        esel = sb("esel", [128, 4, 4], F32, c1)
        m1 = sb("m1", [128, 4, 1], F32, c1)
        m2 = sb("m2", [128, 4, 1], F32, c1)
        k1 = sb("k1", [128, 4, 4], F32, c1)
        k2 = sb("k2", [128, 4, 4], F32, c1)
        e2 = sb("e2", [128, 4, 4], F32, c1)
        w1 = sb("w1", [128, 4, 1], F32, c1)
        w2 = sb("w2", [128, 4, 1], F32, c1)
        S.dma("sp", sidx_sb[:], sidx, writes=("cidx",), key="cidx")
        S.dma("sp", cE_sb[:], cE, writes=("cidx",), key="cidx")
        S.op("pool", lambda e: e.memset(zt[:], 0.0), writes=("zt",))
        S.op("pool", lambda e: e.memset(ztf[:], 0.0), writes=("zt",))
        S.op("pool", lambda e: e.memset(onesf[:], 1.0), writes=("onesf",))
        for sg_ in range(NSEG):
            S.dma("sp", xs_sorted[NB * sg_:NB * sg_ + NB, :].rearrange("(p r) d -> p (r d)", r=4), zt[:],
                  reads=("zt",), writes=("xs_sorted",), key="xs_z")
        S.dma("sp", ws_sorted.rearrange("(p r) c -> p (r c)", r=48), ztf[:], reads=("zt",), writes=("ws_sorted",), key="xs_z")

        for blk in range(8):
            pb2 = blk % 2
            x1t = x1l[pb2]
            ak = "x1l%d" % pb2
            sak = "ssA%d" % blk
            oh = oh_all[:, 4 * blk:4 * blk + 4, :]
            wsel = wsel_all[:, 4 * blk:4 * blk + 4, :]
            S.dma("sp", x1t[:], x1_scr[NB * blk:NB * blk + NB, :].rearrange("(j p) d -> p j d", p=128),
                  reads=("x1_scr%d" % blk,), writes=(ak,), key=ak)
            S.op("pool", lambda e, blk=blk: e.memset(ssA[:, blk, :], 0.0), writes=(sak,))
            for j in range(4):
                S.op("act", lambda e, j=j, x1t=x1t, blk=blk: e.activation(out=xn2_all[:, 4 * blk + j, :], in_=x1t[:, j, :], func=AF.Square,
                                                                          accum_out=ssA[:, blk, j:j + 1]),
                     reads=(ak,), writes=("xn2_%d" % blk, sak))
            S.op("act", lambda e, blk=blk: e.activation(out=rsA[:, blk, :], in_=ssA[:, blk, :], func=AF.Sqrt, bias=qtr[:, 1:2]),
                 reads=(sak, "qtr"), writes=(sak + "r",))
            S.op("dve", lambda e, blk=blk: e.reciprocal(out=rsA[:, blk, :], in_=rsA[:, blk, :]), reads=(sak + "r",), writes=(sak + "r",))
            for j in range(4):
                S.op("dve", lambda e, j=j, x1t=x1t, blk=blk: e.tensor_scalar_mul(out=xn2_all[:, 4 * blk + j, :], in0=x1t[:, j, :],
                                                                                 scalar1=rsA[:, blk, j:j + 1]),
                     reads=(ak, sak + "r"), writes=("xn2_%d" % blk,))
            for j in range(4):
                S.group("pe", [lambda e, j=j, c=c, blk=blk: e.transpose(out=tpv[:, c, 128 * j:128 * j + 128],
                                                                        in_=xn2_all[:, 4 * blk + j, 128 * c:128 * c + 128], identity=identb[:])
                               for c in range(8)], reads=("xn2_%d" % blk, "identb"), writes=TPK)
            for c in range(8):
                S.op("act", lambda e, c=c: e.activation(out=hmT1[:, c, :], in_=tpv[:, c, :], func=AF.Identity,
                                                        scale=s2[:, c:c + 1], bias=mods[:, 24 + c, 0:1]),
                     reads=TPK + ("sc", "mods"), writes=("hmT1",))
            for j in range(4):
                S.group("pe", [lambda e, k=k, j=j: e.matmul(bank(4)[:, 20 * j:20 * j + 20], lhsT=hmT1[:, k, 128 * j:128 * j + 128],
                                                            rhs=w_rt_b[:, k, :], start=(k == 0), stop=(k == 7))
                               for k in range(8)], reads=("hmT1", "w_rt_b"), writes=("ps4",))
            S.op("dve", lambda e: e.tensor_tensor(out=L[:], in0=bank(4)[:, 0:80].rearrange("p (j n) -> p j n", n=20),
                                                  in1=bc(b_rt_sb[:].unsqueeze(1), [128, 4, 20]), op=ALU.add),
                 reads=("ps4", "params"), writes=("L",))
            R = ("rt",)
            OK_ = ("oh_all",)
            S.op("dve", lambda e: e.tensor_reduce(out=gmax[:], in_=L[:, :, 0:4], axis=AX.X, op=ALU.max), reads=("L",), writes=R)
            S.op("dve", lambda e, oh=oh: e.tensor_tensor(out=oh, in0=L[:, :, 0:4], in1=bc(gmax[:], [128, 4, 4]), op=ALU.is_equal),
                 reads=R + ("L",), writes=R + OK_)
            S.op("dve", lambda e: e.tensor_tensor(out=eg[:], in0=L[:, :, 0:4], in1=bc(gmax[:], [128, 4, 4]), op=ALU.subtract),
                 reads=R + ("L",), writes=R)
            S.op("act", lambda e: e.activation(out=eg[:], in_=eg[:], func=AF.Exp), reads=R, writes=R)
            S.op("dve", lambda e: e.tensor_reduce(out=pg[:], in_=eg[:], axis=AX.X, op=ALU.add), reads=R, writes=R)
            S.op("dve", lambda e: e.reciprocal(out=pg[:], in_=pg[:]), reads=R, writes=R)
            S.op("dve", lambda e, oh=oh: e.tensor_tensor(out=tmp16[:].rearrange("p j (g x) -> p j g x", x=4),
                                                         in0=L[:, :, 4:20].rearrange("p j (g x) -> p j g x", x=4),
                                                         in1=bc(oh.unsqueeze(3), [128, 4, 4, 4]), op=ALU.mult),
                 reads=R + ("L",), writes=R)
            S.op("dve", lambda e: e.tensor_reduce(out=esel[:].unsqueeze(3), in_=tmp16[:].rearrange("p j (g x) -> p j x g", x=4),
                                                  axis=AX.X, op=ALU.add), reads=R, writes=R)
            S.op("dve", lambda e: e.tensor_reduce(out=m1[:], in_=esel[:], axis=AX.X, op=ALU.max), reads=R, writes=R)
            S.op("dve", lambda e: e.tensor_tensor(out=k1[:], in0=esel[:], in1=bc(m1[:], [128, 4, 4]), op=ALU.is_equal),
                 reads=R, writes=R)
            S.op("dve", lambda e: e.scalar_tensor_tensor(out=e2[:], in0=k1[:], scalar=-1e30, in1=esel[:], op0=ALU.mult, op1=ALU.add),
                 reads=R, writes=R)
            S.op("dve", lambda e: e.tensor_reduce(out=m2[:], in_=e2[:], axis=AX.X, op=ALU.max), reads=R, writes=R)
            S.op("dve", lambda e: e.tensor_tensor(out=k2[:], in0=e2[:], in1=bc(m2[:], [128, 4, 4]), op=ALU.is_equal),
                 reads=R, writes=R)
            S.op("dve", lambda e: e.tensor_tensor(out=w2[:], in0=m2[:], in1=m1[:], op=ALU.subtract), reads=R, writes=R)
            S.op("act", lambda e: e.activation(out=w2[:], in_=w2[:], func=AF.Exp), reads=R, writes=R)
            S.op("dve", lambda e: e.tensor_scalar_add(out=w1[:], in0=w2[:], scalar1=1.0), reads=R, writes=R)
            S.op("dve", lambda e: e.reciprocal(out=w1[:], in_=w1[:]), reads=R, writes=R)
            S.op("dve", lambda e: e.tensor_tensor(out=w2[:], in0=w2[:], in1=w1[:], op=ALU.mult), reads=R, writes=R)
            S.op("dve", lambda e: e.tensor_tensor(out=w1[:], in0=w1[:], in1=pg[:], op=ALU.mult), reads=R, writes=R)
            S.op("dve", lambda e: e.tensor_tensor(out=w2[:], in0=w2[:], in1=pg[:], op=ALU.mult), reads=R, writes=R)
            S.op("dve", lambda e, wsel=wsel: e.tensor_tensor(out=wsel, in0=k1[:], in1=bc(w1[:], [128, 4, 4]), op=ALU.mult),
                 reads=R, writes=R + ("wsel_all",))
            S.op("dve", lambda e: e.tensor_tensor(out=k2[:], in0=k2[:], in1=bc(w2[:], [128, 4, 4]), op=ALU.mult), reads=R, writes=R)
            S.op("dve", lambda e, wsel=wsel: e.tensor_tensor(out=wsel, in0=wsel, in1=k2[:], op=ALU.add), reads=R + ("wsel_all",),
                 writes=R + ("wsel_all",))

        ohf = oh_all[:].rearrange("p t g -> p (t g)")
        S.op("dve", lambda e: e.tensor_copy(out=oh_bf[:], in_=oh_all[:]), reads=("oh_all",), writes=("oh_bf",))
        S.group("pe", [lambda e: e.matmul(bank(0)[:, 0:128], lhsT=trib[:], rhs=oh_bf[:].rearrange("p t g -> p (t g)"), start=True, stop=True)],
                reads=("oh_bf", "trib"), writes=("ps0",))
        S.group("pe", [lambda e: e.matmul(bank(1)[:, 0:128], lhsT=ones1[:], rhs=oh_bf[:].rearrange("p t g -> p (t g)"), start=True, stop=True)],
                reads=("oh_bf", "ones1"), writes=("ps1",))
        S.op("act", lambda e: e.activation(out=R1s[:].rearrange("p t g -> p (t g)"), in_=bank(0)[:, 0:128], func=AF.Copy),
             reads=("ps0",), writes=("R1s",))
        S.op("act", lambda e: e.activation(out=Cs[:].rearrange("p t g -> p (t g)"), in_=bank(1)[:, 0:128], func=AF.Copy),
             reads=("ps1",), writes=("Cs",))
        for g in range(4):
            S.op("dve", lambda e, g=g: e.tensor_tensor_scan(out=incl[:, g, :], data0=onesf[:], data1=Cs[:, :, g], initial=0.0,
                                                            op0=ALU.mult, op1=ALU.add), reads=("Cs", "onesf"), writes=("incl",))
        S.op("dve", lambda e: e.tensor_copy(out=ng[:], in_=incl[:, :, 31]), reads=("incl",), writes=("ng",))
        S.op("dve", lambda e: e.tensor_tensor(out=incl[:], in0=incl[:], in1=Cs[:].rearrange("p t g -> p g t"), op=ALU.subtract),
             reads=("incl", "Cs"), writes=("incl",))
        S.op("dve", lambda e: e.memset(nseg[:], 0.0), writes=("nseg",))
        for k in range(8):
            S.op("dve", lambda e, k=k: e.scalar_tensor_tensor(out=nseg[:], in0=ng[:], scalar=float(NB * k), in1=nseg[:],
                                                              op0=ALU.is_gt, op1=ALU.add), reads=("ng", "nseg"), writes=("nseg",))
        S.op("dve", lambda e: e.memset(sst[:], 0.0), writes=("sst",))
        for g in range(1, 4):
            S.op("dve", lambda e, g=g: e.tensor_tensor(out=sst[:, g:g + 1], in0=sst[:, g - 1:g], in1=nseg[:, g - 1:g], op=ALU.add),
                 reads=("sst", "nseg"), writes=("sst",))
        S.op("dve", lambda e: e.tensor_tensor(out=sen[:], in0=sst[:], in1=nseg[:], op=ALU.add), reads=("sst", "nseg"), writes=("sen",))
        S.op("dve", lambda e: e.tensor_scalar_mul(out=sst[:], in0=sst[:], scalar1=float(NB)), reads=("sst", "sen"), writes=("sst",))
        S.op("dve", lambda e: e.tensor_tensor(out=R1s[:], in0=R1s[:], in1=incl[:].rearrange("p g t -> p t g"), op=ALU.add),
             reads=("R1s", "incl"), writes=("R1s",))
        S.op("dve", lambda e: e.tensor_tensor(out=R1s[:], in0=R1s[:], in1=bc(sst[:].unsqueeze(1), [128, 32, 4]), op=ALU.add),
             reads=("R1s", "sst"), writes=("R1s",))
        S.op("dve", lambda e: e.tensor_tensor(out=R1s[:], in0=R1s[:], in1=oh_all[:], op=ALU.mult), reads=("R1s", "oh_all"), writes=("R1s",))
        S.op("dve", lambda e: e.tensor_reduce(out=slot_f[:].unsqueeze(2), in_=R1s[:], axis=AX.X, op=ALU.add), reads=("R1s",), writes=("slot_f",))
        S.op("dve", lambda e: e.tensor_copy(out=slot_i[:], in_=slot_f[:]), reads=("slot_f",), writes=("slot_i",))
        S.op("dve", lambda e: e.memset(Gs[:], 0.0), writes=("Gs",))
        for g in range(3):
            S.op("dve", lambda e, g=g: e.scalar_tensor_tensor(out=Gs[:], in0=sidx_sb[:], scalar=sen[:, g:g + 1], in1=Gs[:],
                                                              op0=ALU.is_ge, op1=ALU.add), reads=("cidx", "sen", "Gs"), writes=("Gs",))
        S.op("dve", lambda e: e.tensor_scalar_mul(out=offE_f[:], in0=bc(Gs[:].unsqueeze(2), [128, NSEG, 4]), scalar1=512.0),
             reads=("Gs",), writes=("offE_f",))
        S.op("dve", lambda e: e.tensor_tensor(out=offE_f[:], in0=offE_f[:], in1=bc(cE_sb[:].unsqueeze(1), [128, NSEG, 4]), op=ALU.add),
             reads=("offE_f", "cidx"), writes=("offE_f",))
        S.op("dve", lambda e: e.tensor_copy(out=offE_i[:], in_=offE_f[:]), reads=("offE_f",), writes=("offE_i",))
        for t in range(32):
            S.idma("pool", xs_sorted[:, :], bass.IndirectOffsetOnAxis(ap=slot_i[:, t:t + 1], axis=0), xn2_all[:, t, :], None,
                   reads=("slot_i", "xn2_%d" % (t // 4), "xs_sorted"), writes=("xs_sorted_s",), key="scat")
            S.idma("pool", ws_sorted[:, :], bass.IndirectOffsetOnAxis(ap=slot_i[:, t:t + 1], axis=0), wsel_all[:, t, :], None,
                   reads=("slot_i", "wsel_all", "ws_sorted"), writes=("ws_sorted_s",), key="scat")
        S.barrier()
        c1.close()

        c2 = ExitStack()
        xst = [sb("xst%d" % i, [128, 4, 1024], BF16, c2) for i in range(2)]
        wst = [sb("wst%d" % i, [128, 4, 4], F32, c2) for i in range(2)]
        hmTs = [sb("hmT%d" % i, [128, 8, NB], BF16, c2) for i in range(2)]
        cbc = [sb("cbc%d" % i, [128, 4, NB], BF16, c2) for i in range(2)]
        dgm = sb("dgm", [128, 4, 128], BF16, c2)
        actb = [sb("actb%d" % i, [128, NB], BF16, c2) for i in range(16)]
        wgu = [sb("wgu%d" % i, [128, 2, 8, 512], BF16, c2) for i in range(3)]
        NWD = 5
        wd = [sb("wd%d" % i, [128, 4, 1024], BF16, c2) for i in range(NWD)]
        sg = [sb("sg%d" % i, [128, NB], F32, c2) for i in range(2)]
        tt = [sb("tt%d" % i, [128, NB], BF16, c2) for i in range(2)]
        ysb = [sb("ysb%d" % i, [128, 4, 1024], F32, c2) for i in range(2)]
        ectr = [0]
        for sgi in range(NSEG):
            pb2 = sgi % 2
            hmT, hk = hmTs[pb2], "hmT%d" % pb2
            xk2, wk2, ck2, yk2 = "xst%d" % pb2, "wst%d" % pb2, "cbc%d" % pb2, "ysb%d" % pb2
            S.dma("sp", xst[pb2][:], xs_sorted[NB * sgi:NB * sgi + NB, :].rearrange("(j p) d -> p j d", p=128), writes=(xk2,), key=xk2)
            S.dma("sp", wst[pb2][:], ws_sorted[NB * sgi:NB * sgi + NB, :].rearrange("(j p) c -> p j c", p=128), writes=(wk2,), key=wk2)
            for j in range(4):
                S.group("pe", [lambda e, j=j, c=c, pb2=pb2: e.transpose(out=tpv[:, c, 128 * j:128 * j + 128],
                                                                        in_=xst[pb2][:, j, 128 * c:128 * c + 128], identity=identb[:])
                               for c in range(8)], reads=(xk2, "identb"), writes=TPK)
            for c in range(8):
                S.op("act", lambda e, c=c, hmT=hmT: e.activation(out=hmT[:, c, :], in_=tpv[:, c, :], func=AF.Identity,
                                                                 scale=s2[:, c:c + 1], bias=mods[:, 24 + c, 0:1]),
                     reads=TPK + ("sc", "mods"), writes=(hk,))
            for j in range(4):
                S.op("dve", lambda e, j=j, pb2=pb2: e.tensor_tensor(out=dgm[:], in0=bc(identb[:].unsqueeze(1), [128, 4, 128]),
                                                                    in1=bc(wst[pb2][:, j, :].unsqueeze(2), [128, 4, 128]), op=ALU.mult),
                     reads=("identb", wk2), writes=("dgm",))
                S.group("pe", [lambda e: e.matmul(bank(4 + (j % 2)), lhsT=ones1[:], rhs=dgm[:], start=True, stop=True)],
                        reads=("dgm", "ones1"), writes=(pk(4 + (j % 2)),))
                S.op("act", lambda e, j=j, pb2=pb2: e.activation(out=cbc[pb2][:, :, 128 * j:128 * j + 128],
                                                                 in_=bank(4 + (j % 2)).rearrange("p (x t) -> p x t", t=128), func=AF.Copy),
                     reads=(pk(4 + (j % 2)),), writes=(ck2,))
            for el in range(4):
                si = ectr[0] % 3
                di = ectr[0] % NWD
                ectr[0] += 1
                gk, dk_ = "wgu%d" % si, "wd%d" % di
                ofs = bass.IndirectOffsetOnAxis(ap=offE_i[:, sgi, el:el + 1], axis=0)
                S.idma("pool", wgu[si][:, 0].rearrange("p k n -> p (k n)"), None, w_gate[:, :], ofs, reads=("offE_i",), writes=(gk,), key=gk)
                S.idma("pool", wgu[si][:, 1].rearrange("p k n -> p (k n)"), None, w_up[:, :], ofs, reads=("offE_i",), writes=(gk,), key=gk)
                S.idma("pool", wd[di][:].rearrange("p k n -> p (k n)"), None, w_down[:, :], ofs, reads=("offE_i",), writes=(dk_,), key=dk_)
                for f in range(4):
                    u = 4 * el + f
                    pp = u % 2
                    S.group("pe", [lambda e, k=k, f=f, si=si, pp=pp, hmT=hmT: e.matmul(
                        bank(2 * pp), lhsT=wgu[si][:, 0, k, 128 * f:128 * f + 128], rhs=hmT[:, k, :],
                        start=(k == 0), stop=(k == 7)) for k in range(8)], reads=(hk, gk), writes=(pk(2 * pp),))
                    S.group("pe", [lambda e, k=k, f=f, si=si, pp=pp, hmT=hmT: e.matmul(
                        bank(2 * pp + 1), lhsT=wgu[si][:, 1, k, 128 * f:128 * f + 128], rhs=hmT[:, k, :],
                        start=(k == 0), stop=(k == 7)) for k in range(8)], reads=(hk, gk), writes=(pk(2 * pp + 1),))
                    S.op("act", lambda e, pp=pp: e.activation(out=sg[pp][:], in_=bank(2 * pp), func=AF.Silu),
                         reads=(pk(2 * pp),), writes=("sg%d" % pp,))
                    S.op("dve", lambda e, pp=pp: e.tensor_tensor(out=tt[pp][:], in0=bank(2 * pp + 1), in1=sg[pp][:], op=ALU.mult),
                         reads=(pk(2 * pp + 1), "sg%d" % pp), writes=("tt%d" % pp,))
                    S.op("dve", lambda e, pp=pp, u=u, el=el, pb2=pb2: e.tensor_tensor(out=actb[u][:], in0=tt[pp][:], in1=cbc[pb2][:, el, :],
                                                                                      op=ALU.mult),
                         reads=("tt%d" % pp, ck2), writes=("actb%d" % u,))
            dbase = ectr[0] - 4
            for tp_ in range(2):
                fns = []
                for u in range(16):
                    el, f = divmod(u, 4)
                    di = (dbase + el) % NWD
                    for jj in range(2):
                        j = 2 * tp_ + jj
                        for hh in range(2):
                            fns.append(lambda e, u=u, f=f, di=di, j=j, jj=jj, hh=hh: e.matmul(
                                bank(4 + 2 * jj + hh), lhsT=actb[u][:, 128 * j:128 * j + 128],
                                rhs=wd[di][:, f, 512 * hh:512 * hh + 512], start=(u == 0), stop=(u == 15)))
                S.group("pe", fns, reads=tuple("actb%d" % u for u in range(16)) + tuple("wd%d" % ((dbase + el) % NWD) for el in range(4)),
                        writes=("ps4", "ps5", "ps6", "ps7"))
                for jj in range(2):
                    j = 2 * tp_ + jj
                    for hh in range(2):
                        bk = 4 + 2 * jj + hh
                        S.op("dve", lambda e, bk=bk, hh=hh, j=j, pb2=pb2: e.tensor_tensor(
                            out=ysb[pb2][:, j, 512 * hh:512 * hh + 512], in0=bank(bk), in1=gt2b[:, 512 * hh:512 * hh + 512], op=ALU.mult),
                            reads=(pk(bk), "gt2b"), writes=(yk2,))
            S.dma("sp", y_sorted[NB * sgi:NB * sgi + NB, :].rearrange("(j p) d -> p j d", p=128), ysb[pb2][:],
                  reads=(yk2,), writes=("y_sorted",), key="y_sorted")
        S.barrier()
        c2.close()

        c3 = ExitStack()
        yg = [sb("yg%d" % i, [128, 4, 1024], F32, c3) for i in range(2)]
        x1b = [sb("x1b%d" % i, [128, 4, 1024], F32, c3) for i in range(2)]
        junkF = sb("junkF", [128, 1024], BF16, c3)
        ssF = sb("ssF", [128, 8, 4], F32, c3)
        rsF = sb("rsF", [128, 8, 4], F32, c3)
        for blk in range(8):
            pb2 = blk % 2
            yk3, xk3, sfk = "yg%d" % pb2, "x1b%d" % pb2, "ssF%d" % blk
            S.dma("sp", x1b[pb2][:], x1_scr[NB * blk:NB * blk + NB, :].rearrange("(j p) d -> p j d", p=128), writes=(xk3,), key=xk3)
            for j in range(4):
                S.idma("pool", yg[pb2][:, j, :], None, y_sorted[:, :], bass.IndirectOffsetOnAxis(ap=slot_i[:, 4 * blk + j:4 * blk + j + 1], axis=0),
                       reads=("slot_i",), writes=(yk3,), key=yk3)
            S.op("pool", lambda e, blk=blk: e.memset(ssF[:, blk, :], 0.0), writes=(sfk,))
            for j in range(4):
                S.op("dve", lambda e, j=j, pb2=pb2: e.tensor_tensor(out=x1b[pb2][:, j, :], in0=x1b[pb2][:, j, :], in1=yg[pb2][:, j, :], op=ALU.add),
                     reads=(xk3, yk3), writes=(xk3,))
                S.op("act", lambda e, j=j, pb2=pb2, blk=blk: e.activation(out=junkF[:], in_=x1b[pb2][:, j, :], func=AF.Square,
                                                                          accum_out=ssF[:, blk, j:j + 1]),
                     reads=(xk3,), writes=("junkF", sfk))
            S.op("act", lambda e, blk=blk: e.activation(out=rsF[:, blk, :], in_=ssF[:, blk, :], func=AF.Sqrt, bias=qtr[:, 1:2]),
                 reads=(sfk, "qtr"), writes=(sfk + "r",))
            S.op("dve", lambda e, blk=blk: e.reciprocal(out=rsF[:, blk, :], in_=rsF[:, blk, :]), reads=(sfk + "r",), writes=(sfk + "r",))
            for j in range(4):
                S.op("dve", lambda e, j=j, pb2=pb2, blk=blk: e.scalar_tensor_tensor(out=x1b[pb2][:, j, :], in0=x1b[pb2][:, j, :],
                                                                                    scalar=rsF[:, blk, j:j + 1], in1=gf32[:],
                                                                                    op0=ALU.mult, op1=ALU.mult),
                     reads=(xk3, sfk + "r", "gf32"), writes=(xk3,))
            S.dma("sp", out[NB * blk:NB * blk + NB, :].rearrange("(j p) d -> p j d", p=128), x1b[pb2][:],
                  reads=(xk3,), writes=("out%d" % blk,), key="out")
        S.barrier()
        c3.close()
        pcg.close()
    return nc


_NC_CACHE = {}


def _fm(v):
    v = np.asarray(v, np.float32).reshape(-1, 128)
    return np.ascontiguousarray(v.T)


def kernel(x, c, ctx, c_ctx, w_ada, b_ada, g_mix, w_in, b_in, conv_w, conv_b, ln_g, ln_b, w_pa,
           lru_conv_w, lru_conv_b, w_r_f, b_r_f, w_i_f, b_i_f, lam_f, w_r_b, b_r_b, w_i_b, b_i_b, lam_b,
           w_pb, w_o, g_ffn, w_grp, b_grp, w_er, b_er, w_gate, w_up, w_down, g_final):
    f = lambda a: np.ascontiguousarray(np.asarray(a, np.float32))
    x, c, ctx, c_ctx = f(x), f(c), f(ctx), f(c_ctx)
    B = x.shape[0]
    if "nc" not in _NC_CACHE:
        _NC_CACHE["nc"] = build_program()
    nc = _NC_CACHE["nc"]

    common = {
        "w_ada": f(w_ada[0]), "b_ada_fm": _fm(b_ada[0]),
        "b_ada_gt": f(np.broadcast_to(np.stack([b_ada[0][2048:3072], b_ada[0][5120:6144]])[None], (128, 2, 1024))),
        "w_in": f(w_in[0]), "b_in_fm": _fm(b_in[0]),
        "cb": _fm(conv_b[0]), "lng": _fm(ln_g[0]), "lnb": _fm(ln_b[0]),
        "w_pa": f(w_pa[0]), "w_pb": f(w_pb[0]), "w_o": f(w_o[0]),
        "lb": _fm(lru_conv_b[0]),
        "gmix": _fm(g_mix[0]), "gffn": _fm(g_ffn[0]),
        "gfin": f(np.broadcast_to(np.asarray(g_final, np.float32)[None], (128, 1024))),
        "w_rt": f(np.concatenate([w_grp[0], w_er[0]], axis=1)),
        "b_rt": f(np.broadcast_to(np.concatenate([b_grp[0], b_er[0]])[None], (128, 20))),
        "w_gate": f(np.asarray(w_gate[0], np.float32).reshape(16, 8, 128, 512).transpose(0, 2, 1, 3).reshape(2048, 4096)),
        "w_up": f(np.asarray(w_up[0], np.float32).reshape(16, 8, 128, 512).transpose(0, 2, 1, 3).reshape(2048, 4096)),
        "w_down": f(np.asarray(w_down[0], np.float32).reshape(16, 4, 128, 1024).transpose(0, 2, 1, 3).reshape(2048, 4096)),
        "ident": np.eye(128, dtype=np.float32),
        "tri": np.triu(np.ones((128, 128), np.float32), 1),
        "cE": (np.arange(4)[None, :] * 128 + np.arange(128)[:, None]).astype(np.float32),
        "sidx": np.broadcast_to(np.arange(12, dtype=np.float32)[None], (128, 12)).copy(),
    }
    cwn = np.asarray(conv_w[0], np.float32)
    lwn = np.asarray(lru_conv_w[0], np.float32)
    zero = np.zeros((1, 1024), np.float32)
    lw5_nat = np.concatenate([lwn, zero], axis=0)
    lw5_rev = lw5_nat[::-1]

    def fm3(a):
        T = a.shape[0]
        return np.ascontiguousarray(a.reshape(T, 8, 128).transpose(2, 1, 0))

    pf = (w_r_f[0], b_r_f[0], w_i_f[0], b_i_f[0], lam_f[0])
    pbk = (w_r_b[0], b_r_b[0], w_i_b[0], b_i_b[0], lam_b[0])

    def gates(P, Sd):
        wgs = np.stack([P[0], P[2], Sd[0], Sd[2]]).astype(np.float32)
        bgs = np.stack([np.asarray(t, np.float32) for t in (P[1], P[3], Sd[1], Sd[3])])
        bgs = np.ascontiguousarray(bgs.transpose(2, 0, 1))
        lams = np.stack([np.asarray(P[4], np.float32).reshape(8, 128), np.asarray(Sd[4], np.float32).reshape(8, 128)])
        lams = np.ascontiguousarray(lams.transpose(2, 0, 1))
        return f(wgs), bgs, lams

    per_half = []
    for half in range(2):
        if half == 0:
            wgs, bgs, lams = gates(pf, pbk)
            d = {"cw": fm3(cwn), "lw5": fm3(lw5_nat), "wg": wgs, "bg": bgs, "lam": lams}
        else:
            wgs, bgs, lams = gates(pbk, pf)
            d = {"cw": fm3(cwn[::-1]), "lw5": fm3(lw5_rev), "wg": wgs, "bg": bgs, "lam": lams}
        per_half.append(d)

    in_maps = []
    pad2 = np.zeros((2, 1024), np.float32)
    for b in range(B):
        for half in range(2):
            xs = x[b] if half == 0 else x[b, ::-1]
            cs_ = ctx[b] if half == 0 else ctx[b, ::-1]
            m = dict(common)
            m.update(per_half[half])
            m["xp"] = np.ascontiguousarray(np.concatenate([pad2, xs, pad2], axis=0))
            m["ctxp"] = np.ascontiguousarray(np.concatenate([pad2, cs_, pad2], axis=0))
            m["cvec"] = np.ascontiguousarray(np.stack([_fm(c[b]), _fm(c_ctx)], axis=-1))
            in_maps.append(m)
    res = run_bass_kernel_spmd(nc, in_maps, core_ids=list(range(2 * B)))
    outp = np.empty((B, 2 * NOWN, 1024), np.float32)
    for b in range(B):
        outp[b, :NOWN] = res.results[2 * b]["out"]
        outp[b, NOWN:] = res.results[2 * b + 1]["out"][::-1]
    if DEBUG:
        kernel.last = res
    return outp
```

```python
from contextlib import ExitStack
import os
import numpy as np
import concourse.bass as bass
import concourse.mybir as mybir
from concourse.bass_utils import run_bass_kernel_spmd

F32 = mybir.dt.float32
BF16 = mybir.dt.bfloat16
AF = mybir.ActivationFunctionType
ALU = mybir.AluOpType
AX = mybir.AxisListType
EPS = 1e-6
NB = 512
NOWN = 4096
DEBUG = bool(int(os.environ.get("MK_DEBUG", "0")))


class _Rec:
    def __init__(self):
        self.calls = []

    def __getattr__(self, name):
        def f(*args, **kw):
            self.calls.append((name, args, kw))
            return self
        return f


_TBL = {"Exp": "exp", "Tanh": None, "Identity": None, "Copy": None, "Square": None, "Sqrt": "sqrt", "Silu": "silu",
        "Gelu_apprx_tanh": "gelu", "Ln": "ln"}


def _fsize(ap):
    n = 1
    for d in ap.shape[1:]:
        n *= int(d)
    return n


class Sched:
    REORDER = True
    WINDOW = 600
    ALPHA = 0.05

    def __init__(self, nc, es):
        self.nc = nc
        self.es = es
        self.E = dict(pe=nc.tensor, act=nc.scalar, dve=nc.vector, pool=nc.gpsimd, sp=nc.sync)
        self.sem = {e: es.enter_context(nc.semaphore("c_" + e)) for e in self.E}
        self.cnt = {e: 0 for e in self.E}
        self.seen = {e: {} for e in self.E}
        self.lastw = {}
        self.readers = {}
        self.dsem = {}
        self.dcnt = {}
        self.ops = []

    def op(self, e, fn, reads=(), writes=()):
        r = _Rec()
        fn(r)
        self._add(e, "op", r.calls, tuple(reads), tuple(writes), None)

    def group(self, e, fns, reads=(), writes=()):
        r = _Rec()
        for f in fns:
            f(r)
        self._add(e, "op", r.calls, tuple(reads), tuple(writes), None)

    def dma(self, q, out, in_, reads=(), writes=(), key=None):
        self._add(q, "dma", [("dma_start", (), dict(out=out, in_=in_))], tuple(reads), tuple(writes), key)

    def idma(self, q, out, out_offset, in_, in_offset, reads=(), writes=(), key=None):
        self._add(q, "dma", [("indirect_dma_start", (), dict(out=out, out_offset=out_offset, in_=in_, in_offset=in_offset))],
                  tuple(reads), tuple(writes), key)

    def _add(self, e, kind, calls, reads, writes, key):
        dur = 0.0
        tbl = None
        if kind == "dma":
            kw0 = calls[0][2]
            side = kw0["in_"] if kw0.get("out_offset") is not None else kw0["out"]
            nb = 128 * _fsize(side) * 4
            dur = 1000.0 if e == "pool" else 150.0
            lat = 2000.0 + nb / 300.0
        else:
            lat = 0.0
            for (name, args, kw) in calls:
                if e == "pe":
                    src = kw.get("rhs", kw.get("in_"))
                    dur += 25.0 + 0.5 * max(_fsize(src), 64)
                else:
                    oap = kw.get("out", kw.get("ap", args[0] if args else None))
                    n = _fsize(oap)
                    if e == "act":
                        dur += 250.0 + 0.73 * n
                        fnm = kw.get("func")
                        tbl = _TBL.get(getattr(fnm, "name", str(fnm)), None) if fnm is not None else None
                    elif e == "dve":
                        dur += 160.0 + 1.04 * n
                    else:
                        dur += 300.0 + 3.1 * n
        self.ops.append(dict(e=e, kind=kind, calls=calls, reads=reads, writes=writes, key=key, dur=dur, lat=lat, tbl=tbl))

    def flush(self):
        ops = self.ops
        self.ops = []
        n = len(ops)
        if n == 0:
            return
        lastw, readers = {}, {}
        preds = [None] * n
        succs = [[] for _ in range(n)]
        for i, o in enumerate(ops):
            p = set()
            for k in o["reads"]:
                if k in lastw:
                    p.add(lastw[k])
            for k in o["writes"]:
                if k in lastw:
                    p.add(lastw[k])
                p.update(readers.get(k, ()))
            p.discard(i)
            preds[i] = p
            for j in p:
                succs[j].append(i)
            for k in o["reads"]:
                readers.setdefault(k, []).append(i)
            for k in o["writes"]:
                lastw[k] = i
                readers[k] = []
        if not self.REORDER:
            order = range(n)
        else:
            indeg = [len(p) for p in preds]
            alpha = float(os.environ.get("MK_PRI", self.ALPHA))
            rank = [0.0] * n
            if alpha > 0:
                for i in range(n - 1, -1, -1):
                    m = 0.0
                    for j in succs[i]:
                        if rank[j] > m:
                            m = rank[j]
                    rank[i] = ops[i]["dur"] + ops[i]["lat"] + m
            ready = [i for i in range(n) if indeg[i] == 0]
            finish = [0.0] * n
            efree = {e: 0.0 for e in self.E}
            etbl = [None]
            done = [False] * n
            lo = 0
            order = []
            while len(order) < n:
                while lo < n and done[lo]:
                    lo += 1
                best, bkey = None, None
                for i in ready:
                    if i > lo + self.WINDOW:
                        continue
                    o = ops[i]
                    st = efree[o["e"]]
                    for j in preds[i]:
                        f = finish[j] + (0.0 if ops[j]["e"] == o["e"] else 120.0)
                        if f > st:
                            st = f
                    if o["e"] == "act" and o["tbl"] is not None and o["tbl"] != etbl[0]:
                        st += 1300.0
                    kk = (st - alpha * rank[i], i) if alpha > 0 else (st, i)
                    if bkey is None or kk < bkey:
                        best, bkey = i, kk
                i = best
                o = ops[i]
                st = bkey[0] + (alpha * rank[i] if alpha > 0 else 0.0)
                if os.environ.get("MK_TL") and n > 3000 and len(ops) == int(os.environ.get("MK_TL")):
                    lim = None
                    for j in preds[i]:
                        f = finish[j]
                        if lim is None or f > lim[0]:
                            lim = (f, j)
                    gap = st - efree[o["e"]]
                    if o["e"] == "pe" and gap > 300:
                        print("PE gap %.1fus at t=%.1fus op#%d writes=%s waits for %s op#%d writes=%s" % (
                            gap / 1e3, st / 1e3, i, o["writes"][:2], ops[lim[1]]["e"], lim[1], ops[lim[1]]["writes"][:2]))
                if o["e"] == "act" and o["tbl"] is not None:
                    etbl[0] = o["tbl"]
                efree[o["e"]] = st + o["dur"]
                finish[i] = st + o["dur"] + o["lat"]
                done[i] = True
                ready.remove(i)
                order.append(i)
                for j in succs[i]:
                    indeg[j] -= 1
                    if indeg[j] == 0:
                        ready.append(j)
        if self.REORDER and os.environ.get("MK_STATS"):
            busy = {e: 0.0 for e in self.E}
            for o in ops:
                busy[o["e"]] += o["dur"]
            print("phase: n=%d est_makespan=%.0fus busy(us): %s" % (n, max(finish) / 1e3, {e: int(v / 1e3) for e, v in busy.items()}))
        for i in order:
            self._emit(ops[i])

    def _wait(self, e, tok, same_ok=False):
        if tok is None:
            return
        name, sem, val, src = tok
        if same_ok and src == e:
            return
        d = self.seen[e]
        if d.get(name, 0) >= val:
            return
        self.E[e].wait_ge(sem, val)
        d[name] = val

    def _emit(self, o):
        e, reads, writes = o["e"], o["reads"], o["writes"]
        for k in reads:
            self._wait(e, self.lastw.get(k))
        for k in writes:
            self._wait(e, self.lastw.get(k), same_ok=True)
            for t in self.readers.get(k, {}).values():
                self._wait(e, t, same_ok=True)
        ins = None
        for (name, args, kw) in o["calls"]:
            ins = getattr(self.E[e], name)(*args, **kw)
        if o["kind"] == "dma":
            key = o["key"]
            if key not in self.dsem:
                self.dsem[key] = self.es.enter_context(self.nc.semaphore("d_" + key))
                self.dcnt[key] = 0
            self.dcnt[key] += 16
            ins.then_inc(self.dsem[key], 16)
            tok = ("d_" + key, self.dsem[key], self.dcnt[key], "dma")
        else:
            self.cnt[e] += 1
            ins.then_inc(self.sem[e], 1)
            tok = ("c_" + e, self.sem[e], self.cnt[e], e)
        for k in reads:
            self.readers.setdefault(k, {})[tok[0]] = tok
        for k in writes:
            self.lastw[k] = tok
            self.readers[k] = {}

    def barrier(self):
        self.flush()
        for e in self.E:
            for e2 in self.E:
                if self.cnt[e2] > 0:
                    self._wait(e, ("c_" + e2, self.sem[e2], self.cnt[e2], e2))
            for k, sem in self.dsem.items():
                self._wait(e, ("d_" + k, sem, self.dcnt[k], "dma"))


def build_program():
    nc = bass.Bass("TRN2", target_bir_lowering=False)

    def din(name, shape):
        return nc.dram_tensor(name, list(shape), F32, kind="ExternalInput").ap()

    xp = din("xp", [8196, 1024])
    ctxp = din("ctxp", [260, 1024])
    cvec = din("cvec", [128, 8, 2])
    w_ada = din("w_ada", [1024, 6144])
    b_ada_fm = din("b_ada_fm", [128, 48])
    b_ada_gt = din("b_ada_gt", [128, 2, 1024])
    w_in = din("w_in", [1024, 6144])
    b_in_fm = din("b_in_fm", [128, 48])
    cw = din("cw", [128, 8, 31])
    cb = din("cb", [128, 8])
    lng = din("lng", [128, 8])
    lnb = din("lnb", [128, 8])
    w_pa = din("w_pa", [1024, 1024])
    w_pb = din("w_pb", [1024, 1024])
    w_o = din("w_o", [1024, 1024])
    lw5 = din("lw5", [128, 8, 5])
    lb = din("lb", [128, 8])
    wg = din("wg", [4, 8, 128, 128])
    bg = din("bg", [128, 4, 8])
    lam = din("lam", [128, 2, 8])
    gmix = din("gmix", [128, 8])
    gffn = din("gffn", [128, 8])
    gfin = din("gfin", [128, 1024])
    w_rt = din("w_rt", [1024, 20])
    b_rt = din("b_rt", [128, 20])
    w_gate = din("w_gate", [2048, 4096])
    w_up = din("w_up", [2048, 4096])
    w_down = din("w_down", [2048, 4096])
    ident = din("ident", [128, 128])
    tri = din("tri", [128, 128])
    cE = din("cE", [128, 4])
    sidx = din("sidx", [128, 12])
    out = nc.dram_tensor("out", [NOWN, 1024], F32, kind="ExternalOutput").ap()
    if DEBUG:
        hs_scr = nc.dram_tensor("hs_scr", [8, 128, 8, NB], BF16, kind="ExternalOutput").ap()
        x1_scr = nc.dram_tensor("x1_scr", [NOWN, 1024], F32, kind="ExternalOutput").ap()
    else:
        hs_scr = nc.dram_tensor("hs_scr", [8, 128, 8, NB], BF16, kind="Internal").ap()
        x1_scr = nc.dram_tensor("x1_scr", [NOWN, 1024], F32, kind="Internal").ap()
    gt2_scr = nc.dram_tensor("gt2_scr", [128, 1024], F32, kind="Internal").ap()
    NSEG = 12
    xs_sorted = nc.dram_tensor("xs_sorted", [NSEG * NB, 1024], BF16, kind="Internal").ap()
    ws_sorted = nc.dram_tensor("ws_sorted", [NSEG * NB, 4], F32, kind="Internal").ap()
    y_sorted = nc.dram_tensor("y_sorted", [NSEG * NB, 1024], F32, kind="Internal").ap()
    I32 = mybir.dt.int32

    with ExitStack() as es:
        S = Sched(nc, es)

        def sb(name, shape, dt=F32, stack=es):
            return stack.enter_context(nc.sbuf_tensor(name, list(shape), dt))

        psA = es.enter_context(nc.psum_tensor("psA", [128, 2048], F32))
        psB = es.enter_context(nc.psum_tensor("psB", [128, 2048], F32))

        def bank(i):
            t = psA if i < 4 else psB
            return t[:, 512 * (i % 4):512 * (i % 4) + 512]

        def pk(i):
            return "ps%d" % i

        tpv = psA[:, :].bitcast(BF16).rearrange("p (c t) -> p c t", t=512)
        TPK = ("ps0", "ps1", "ps2", "ps3")

        identb = sb("identb", [128, 128], BF16)
        ones_m = sb("ones_m", [128, 128], BF16)
        ones1 = sb("ones1", [128, 128], BF16)
        b_in_sb = sb("b_in_sb", [128, 48])
        hb_in = sb("hb_in", [128, 48])
        cw_sb = sb("cw_sb", [128, 8, 31])
        cb_sb = sb("cb_sb", [128, 8])
        lng_sb = sb("lng_sb", [128, 8])
        lnb_sb = sb("lnb_sb", [128, 8])
        lw5_sb = sb("lw5_sb", [128, 8, 5])
        lb_sb = sb("lb_sb", [128, 8])
        bg_sb = sb("bg_sb", [128, 4, 8])
        hbg = sb("hbg", [128, 4, 8])
        lam_sb = sb("lam_sb", [128, 2, 8])
        gmix_sb = sb("gmix_sb", [128, 8])
        gffn_sb = sb("gffn_sb", [128, 8])
        b_rt_sb = sb("b_rt_sb", [128, 20])
        b_ada_fm_sb = sb("b_ada_fm_sb", [128, 48])
        cvec_sb = sb("cvec_sb", [128, 8, 2])
        mods = sb("mods", [128, 48, 2])
        s1 = sb("s1", [128, 8])
        s1c = sb("s1c", [128, 8])
        s2 = sb("s2", [128, 8])
        gt1h = sb("gt1h", [128, 1024])
        cl = sb("cl", [128, 2, 8])
        hcl = sb("hcl", [128, 2, 8])
        state = sb("state", [128, 2, 8])
        ss = sb("ss", [128, 8])
        rs = sb("rs", [128, 8])
        w_rt_b = sb("w_rt_b", [128, 8, 20], BF16)
        qtr = sb("qtr", [128, 4], F32)

        def pload(t, src):
            S.dma("sp", t, src, writes=("params",), key="params")

        pload(b_in_sb[:], b_in_fm)
        pload(cw_sb[:], cw)
        pload(cb_sb[:], cb)
        pload(lng_sb[:], lng)
        pload(lnb_sb[:], lnb)
        pload(lw5_sb[:], lw5)
        pload(lb_sb[:], lb)
        pload(bg_sb[:], bg)
        pload(lam_sb[:], lam)
        pload(gmix_sb[:], gmix)
        pload(gffn_sb[:], gffn)
        pload(b_rt_sb[:], b_rt)
        pload(b_ada_fm_sb[:], b_ada_fm)
        pload(cvec_sb[:], cvec)
        S.dma("pool", identb[:], ident, writes=("identb",), key="identb")
        S.dma("pool", w_rt_b[:], w_rt.rearrange("(k p) n -> p k n", p=128), writes=("w_rt_b",), key="w_rt_b")
        S.op("pool", lambda e: e.memset(ones_m[:], 1.0 / 1024.0), writes=("ones_m",))
        S.op("pool", lambda e: e.memset(ones1[:], 1.0), writes=("ones1",))
        S.op("pool", lambda e: e.memset(qtr[:, 0:1], 0.25), writes=("qtr",))
        S.op("pool", lambda e: e.memset(qtr[:, 1:2], 1024.0 * EPS), writes=("qtr",))
        S.op("pool", lambda e: e.memset(qtr[:, 2:3], EPS), writes=("qtr",))
        S.op("pool", lambda e: e.memset(state[:], 0.0), writes=("state",))
        S.op("pool", lambda e: e.memset(ss[:], 0.0), writes=("ss",))

        with ExitStack() as p0:
            cs = sb("cs", [128, 8, 2], BF16, p0)
            cs_rep = sb("cs_rep", [128, 8, 128], BF16, p0)
            b_ada_gt_sb = sb("b_ada_gt_sb", [128, 2, 1024], F32, p0)
            wa = [sb("wa%d" % i, [128, 8, 512], BF16, p0) for i in range(3)]
            e_t = sb("e_t", [128, 16], F32, p0)
            t_t = sb("t_t", [128, 16], F32, p0)
            l_t = sb("l_t", [128, 16], F32, p0)
            m_t = sb("m_t", [128, 16], F32, p0)
            pload(b_ada_gt_sb[:], b_ada_gt)
            gt2b = sb("gt2b0", [128, 1024], F32, p0)

            S.op("act", lambda e: e.activation(out=cs[:], in_=cvec_sb[:], func=AF.Silu), reads=("params",), writes=("cs",))
            S.op("dve", lambda e: e.tensor_copy(out=cs_rep[:], in_=cs[:, :, 0:1].to_broadcast([128, 8, 128])),
                 reads=("cs",), writes=("cs_rep",))
            psm = bank(0)[:, 0:96].rearrange("p (j t) -> p j t", t=2)
            for q in range(12):
                s = q % 3
                S.dma("pool", wa[s][:], w_ada[:, 512 * q:512 * q + 512].rearrange("(k p) n -> p k n", p=128),
                      writes=("wa%d" % s,), key="wa%d" % s)
                fns = []
                for jj in range(4):
                    for k in range(8):
                        fns.append(lambda e, jj=jj, k=k, s=s, q=q: e.matmul(
                            psm[:, 4 * q + jj, :], lhsT=wa[s][:, k, 128 * jj:128 * jj + 128], rhs=cs[:, k, :],
                            start=(k == 0), stop=(k == 7)))
                S.group("pe", fns, reads=("wa%d" % s, "cs"), writes=("ps0",))
                if q in (4, 5, 10, 11):
                    bk = 1 + (q % 2)
                    S.group("pe", [lambda e, k=k, s=s, bk=bk: e.matmul(bank(bk), lhsT=cs_rep[:, k, :], rhs=wa[s][:, k, :],
                                                                      start=(k == 0), stop=(k == 7)) for k in range(8)],
                            reads=("wa%d" % s, "cs_rep"), writes=(pk(bk),))
                    dst = gt1h if q < 6 else gt2b
                    gi = 0 if q < 6 else 1
                    cols = slice(512 * (q % 2), 512 * (q % 2) + 512)
                    S.op("dve", lambda e, dst=dst, gi=gi, cols=cols, bk=bk: e.tensor_tensor(
                        out=dst[:, cols], in0=bank(bk), in1=b_ada_gt_sb[:, gi, cols], op=ALU.add),
                        reads=(pk(bk), "params"), writes=("gt",))
            S.op("dve", lambda e: e.tensor_scalar_mul(out=gt1h[:], in0=gt1h[:], scalar1=0.5), reads=("gt",), writes=("gt",))
            S.dma("sp", gt2_scr, gt2b[:], reads=("gt",), writes=("gt2_scr",), key="gt2_scr")
            S.op("dve", lambda e: e.tensor_tensor(out=mods[:], in0=psm, in1=b_ada_fm_sb[:].unsqueeze(2).to_broadcast([128, 48, 2]),
                                                  op=ALU.add), reads=("ps0", "params"), writes=("mods",))
            for (dst, col, j0, gsb) in ((s1, 0, 8, gmix_sb), (s1c, 1, 8, gmix_sb), (s2, 0, 32, gffn_sb)):
                S.op("dve", lambda e, dst=dst, col=col, j0=j0, gsb=gsb: e.scalar_tensor_tensor(
                    out=dst[:], in0=mods[:, j0:j0 + 8, col], scalar=1.0, in1=gsb[:], op0=ALU.add, op1=ALU.mult),
                    reads=("mods", "params"), writes=("sc",))
                S.op("dve", lambda e, dst=dst: e.tensor_scalar_mul(out=dst[:], in0=dst[:], scalar1=32.0),
                     reads=("sc",), writes=("sc",))
            S.op("dve", lambda e: e.tensor_scalar_mul(out=hb_in[:], in0=b_in_sb[:], scalar1=0.5), reads=("params",), writes=("hb_in",))
            S.op("dve", lambda e: e.tensor_scalar_mul(out=hbg[:], in0=bg_sb[:], scalar1=0.5), reads=("params",), writes=("hbg",))
            lamf = lam_sb[:].rearrange("p a b -> p (a b)")
            S.op("act", lambda e: e.activation(out=e_t[:], in_=lamf, func=AF.Exp, scale=-1.0), reads=("params",), writes=("e_t",))
            S.op("dve", lambda e: e.tensor_scalar(out=t_t[:], in0=e_t[:], scalar1=-0.25, scalar2=1.0 / 3.0, op0=ALU.mult, op1=ALU.add),
                 reads=("e_t",), writes=("t_t",))
            S.op("dve", lambda e: e.tensor_tensor(out=t_t[:], in0=t_t[:], in1=e_t[:], op=ALU.mult), reads=("t_t", "e_t"), writes=("t_t",))
            S.op("dve", lambda e: e.tensor_scalar_add(out=t_t[:], in0=t_t[:], scalar1=-0.5), reads=("t_t",), writes=("t_t",))
            S.op("dve", lambda e: e.tensor_tensor(out=t_t[:], in0=t_t[:], in1=e_t[:], op=ALU.mult), reads=("t_t", "e_t"), writes=("t_t",))
            S.op("dve", lambda e: e.tensor_scalar_add(out=t_t[:], in0=t_t[:], scalar1=1.0), reads=("t_t",), writes=("t_t",))
            S.op("dve", lambda e: e.tensor_tensor(out=t_t[:], in0=t_t[:], in1=e_t[:], op=ALU.mult), reads=("t_t", "e_t"), writes=("t_t",))
            S.op("dve", lambda e: e.tensor_scalar_add(out=l_t[:], in0=e_t[:], scalar1=1.0), reads=("e_t",), writes=("l_t",))
            S.op("act", lambda e: e.activation(out=l_t[:], in_=l_t[:], func=AF.Ln), reads=("l_t",), writes=("l_t",))
            S.op("dve", lambda e: e.tensor_single_scalar(out=m_t[:], in_=e_t[:], scalar=0.1, op=ALU.is_lt), reads=("e_t",), writes=("m_t",))
            S.op("dve", lambda e: e.tensor_tensor(out=t_t[:], in0=t_t[:], in1=l_t[:], op=ALU.subtract), reads=("t_t", "l_t"), writes=("t_t",))
            S.op("dve", lambda e: e.tensor_tensor(out=t_t[:], in0=t_t[:], in1=m_t[:], op=ALU.mult), reads=("t_t", "m_t"), writes=("t_t",))
            S.op("dve", lambda e: e.tensor_tensor(out=t_t[:], in0=t_t[:], in1=l_t[:], op=ALU.add), reads=("t_t", "l_t"), writes=("t_t",))
            clf = cl[:].rearrange("p a b -> p (a b)")
            hclf = hcl[:].rearrange("p a b -> p (a b)")
            S.op("dve", lambda e: e.tensor_scalar_mul(out=clf, in0=t_t[:], scalar1=-8.0), reads=("t_t",), writes=("cl",))
            S.op("dve", lambda e: e.tensor_scalar_mul(out=hclf, in0=t_t[:], scalar1=-4.0), reads=("t_t",), writes=("cl",))
            S.barrier()

        mixer = ExitStack()
        wxr = sb("wxr", [128, 8, 1024], BF16, mixer)
        wgb = sb("wgb", [128, 4, 8, 128], BF16, mixer)
        dg5 = sb("dg5", [128, 8, 5, 128], BF16, mixer)
        S.dma("pool", wxr[:], w_in[:, 3072:4096].rearrange("(k p) n -> p k n", p=128), writes=("wxr",), key="wxr")
        S.dma("pool", wgb[:], wg.rearrange("g h p n -> p g h n"), writes=("wgb",), key="wgb")
        for c in range(8):
            S.op("dve", lambda e, c=c: e.tensor_tensor(
                out=dg5[:, c, :, :], in0=identb[:].unsqueeze(1).to_broadcast([128, 5, 128]),
                in1=lw5_sb[:, c, :].unsqueeze(2).to_broadcast([128, 5, 128]), op=ALU.mult),
                reads=("identb", "params"), writes=("dg5",))

        xt = sb("xt", [128, 4, 1024], F32, mixer)
        xh = sb("xh", [4, 1024], F32, mixer)
        xn = sb("xn", [128, 4, 1024], BF16, mixer)
        xnh = sb("xnh", [4, 1024], BF16, mixer)
        hxTs = [sb("hxT%d" % i, [128, 8, NB + 4], BF16, mixer) for i in range(2)]
        hxT, hxk, hxhk = hxTs[0], "hxT0", "hxTh0"
        hbc = [0]
        xrp = [sb("xrp%d" % i, [128, NB + 4], BF16, mixer) for i in range(2)]
        xcb = [sb("xcb%d" % i, [128, NB], BF16, mixer) for i in range(2)]
        tr = sb("tr", [128, NB], F32, mixer)
        ti = sb("ti", [128, NB], F32, mixer)
        a4 = sb("a4", [128, 4, NB], F32, mixer)
        s4 = sb("s4", [128, 4, NB], F32, mixer)
        t4 = sb("t4", [128, 4, NB], BF16, mixer)
        tmp1 = sb("tmp1", [128, NB], F32, mixer)
        bb_t = sb("bb_t", [128, NB], F32, mixer)
        hf = sb("hf", [128, NB], F32, mixer)
        hsb = sb("hsb", [128, 8, NB], BF16, mixer)

        tph = bank(4).bitcast(BF16)[:, 0:32].rearrange("p (c t) -> p c t", t=4)

        def prep(xsrc, r0, N, sc, bcol, keep_key):
            nt = N // 128
            S.dma("sp", xt[:, 0:nt, :], xsrc[r0:r0 + N, :].rearrange("(j p) d -> p j d", p=128), writes=(keep_key,), key="xt")
            S.dma("sp", xh[0:2, :], xsrc[r0 - 2:r0, :], writes=("xh",), key="xh")
            S.dma("sp", xh[2:4, :], xsrc[r0 + N:r0 + N + 2, :], writes=("xh",), key="xh")
            S.op("pool", lambda e: e.memset(ss[:], 0.0), writes=("ss",))
            for j in range(nt):
                S.op("act", lambda e, j=j: e.activation(out=xn[:, j, :], in_=xt[:, j, :], func=AF.Square, accum_out=ss[:, j:j + 1]),
                     reads=(keep_key,), writes=("xn", "ss"))
            S.op("act", lambda e: e.activation(out=xnh[:], in_=xh[:], func=AF.Square, accum_out=ss[0:4, 4:5]),
                 reads=("xh",), writes=("xnh", "ss"))
            S.op("act", lambda e: e.activation(out=rs[:, 0:5], in_=ss[:, 0:5], func=AF.Sqrt, bias=qtr[:, 1:2]), reads=("ss", "qtr"), writes=("rs",))
            S.op("dve", lambda e: e.reciprocal(out=rs[:, 0:5], in_=rs[:, 0:5]), reads=("rs",), writes=("rs",))
            for j in range(nt):
                S.op("dve", lambda e, j=j: e.tensor_scalar_mul(out=xn[:, j, :], in0=xt[:, j, :], scalar1=rs[:, j:j + 1]),
                     reads=(keep_key, "rs"), writes=("xn",))
            S.op("dve", lambda e: e.tensor_scalar_mul(out=xnh[:], in0=xh[:], scalar1=rs[0:4, 4:5]),
                 reads=("xh", "rs"), writes=("xnh",))
            for j in range(nt):
                S.group("pe", [lambda e, j=j, c=c: e.transpose(out=tpv[:, c, 128 * j:128 * j + 128],
                                                               in_=xn[:, j, 128 * c:128 * c + 128], identity=identb[:])
                               for c in range(8)], reads=("xn", "identb"), writes=TPK)
            S.group("pe", [lambda e, c=c: e.transpose(out=tph[:, c, :], in_=xnh[:, 128 * c:128 * c + 128], identity=identb[0:4, 0:4])
                           for c in range(8)], reads=("xnh", "identb"), writes=("ps4",))
            for c in range(8):
                S.op("act", lambda e, c=c: e.activation(out=hxT[:, c, 0:N], in_=tpv[:, c, 0:N], func=AF.Identity,
                                                        scale=sc[:, c:c + 1], bias=mods[:, c, bcol:bcol + 1]),
                     reads=TPK + ("sc", "mods"), writes=(hxk,))
            S.op("dve", lambda e: e.tensor_tensor(out=hxT[:, :, N:N + 4], in0=tph, in1=sc[:].unsqueeze(2).to_broadcast([128, 8, 4]),
                                                  op=ALU.mult), reads=("ps4", "sc"), writes=(hxhk,))
            S.op("dve", lambda e: e.tensor_tensor(out=hxT[:, :, N:N + 4], in0=hxT[:, :, N:N + 4],
                                                  in1=mods[:, 0:8, bcol:bcol + 1].to_broadcast([128, 8, 4]), op=ALU.add),
                 reads=(hxhk, "mods"), writes=(hxhk,))

        xr_evac_eng = ["dve"]

        def rglru_block(N, d, reverse, has_lo, has_hi, consumer=None):
            def st1(c):
                par = c % 2
                bxr = bank(par)[:, 0:N]
                bxh = bank(2 + par)[:, 0:4]
                S.group("pe", [lambda e, k=k: e.matmul(bxr, lhsT=wxr[:, k, 128 * c:128 * c + 128], rhs=hxT[:, k, 0:N],
                                                       start=(k == 0), stop=(k == 7)) for k in range(8)],
                        reads=(hxk, "wxr"), writes=(pk(par),))
                S.group("pe", [lambda e, k=k: e.matmul(bxh, lhsT=wxr[:, k, 128 * c:128 * c + 128], rhs=hxT[:, k, N:N + 4],
                                                       start=(k == 0), stop=(k == 7)) for k in range(8)],
                        reads=(hxhk, "wxr"), writes=(pk(2 + par),))
                xk = "xrp%d" % par
                bia = b_in_sb[:, 24 + c:25 + c]
                if xr_evac_eng[0] == "act":
                    S.op("act", lambda e: e.activation(out=xrp[par][:, 2:2 + N], in_=bxr, func=AF.Identity, bias=bia),
                         reads=(pk(par), "params"), writes=(xk,))
                else:
                    S.op("dve", lambda e: e.tensor_scalar_add(out=xrp[par][:, 2:2 + N], in0=bxr, scalar1=bia),
                         reads=(pk(par), "params"), writes=(xk,))
                if has_lo:
                    S.op("dve", lambda e: e.tensor_scalar_add(out=xrp[par][:, 0:2], in0=bxh[:, 0:2], scalar1=bia),
                         reads=(pk(2 + par), "params"), writes=(xk,))
                else:
                    S.op("dve", lambda e: e.memset(xrp[par][:, 0:2], 0.0), writes=(xk,))
                if has_hi:
                    S.op("dve", lambda e: e.tensor_scalar_add(out=xrp[par][:, 2 + N:4 + N], in0=bxh[:, 2:4], scalar1=bia),
                         reads=(pk(2 + par), "params"), writes=(xk,))
                else:
                    S.op("dve", lambda e: e.memset(xrp[par][:, 2 + N:4 + N], 0.0), writes=(xk,))

            def st2(c):
                par = c % 2
                xk = "xrp%d" % par
                bcv = bank(4 + par)[:, 0:N]
                S.group("pe", [lambda e, j=j: e.matmul(bcv, lhsT=dg5[:, c, j, :], rhs=xrp[par][:, j:j + N],
                                                       start=(j == 0), stop=(j == 4)) for j in range(5)],
                        reads=(xk, "dg5"), writes=(pk(4 + par),))
                S.op("dve", lambda e: e.tensor_scalar_add(out=xcb[par][:, 0:N], in0=bcv, scalar1=lb_sb[:, c:c + 1]),
                     reads=(pk(4 + par), "params"), writes=("xcb%d" % par,))

            def st3(c):
                par = c % 2
                q = c % 4
                ck = "xcb%d" % par
                br_ = bank(6)[:, 0:N]
                bi_ = bank(7)[:, 0:N]
                S.group("pe", [lambda e: e.matmul(br_, lhsT=wgb[:, 2 * d, c, :], rhs=xcb[par][:, 0:N], start=True, stop=True)],
                        reads=(ck, "wgb"), writes=("ps6",))
                S.group("pe", [lambda e: e.matmul(bi_, lhsT=wgb[:, 2 * d + 1, c, :], rhs=xcb[par][:, 0:N], start=True, stop=True)],
                        reads=(ck, "wgb"), writes=("ps7",))
                S.op("act", lambda e: e.activation(out=tr[:, 0:N], in_=br_, func=AF.Tanh, scale=0.5, bias=hbg[:, 2 * d, c:c + 1]),
                     reads=("ps6", "hbg"), writes=("tr",))
                S.op("act", lambda e: e.activation(out=ti[:, 0:N], in_=bi_, func=AF.Tanh, scale=0.5, bias=hbg[:, 2 * d + 1, c:c + 1]),
                     reads=("ps7", "hbg"), writes=("ti",))
                S.op("act", lambda e: e.activation(out=a4[:, q, 0:N], in_=tr[:, 0:N], func=AF.Exp, scale=hcl[:, d, c:c + 1],
                                                   bias=hcl[:, d, c:c + 1]), reads=("tr", "cl"), writes=("a4_%d" % q,))
                S.op("pool", lambda e: e.tensor_tensor(out=s4[:, q, 0:N], in0=a4[:, q, 0:N], in1=a4[:, q, 0:N], op=ALU.mult),
                     reads=("a4_%d" % q,), writes=("s4_%d" % q,))
                S.op("dve", lambda e: e.scalar_tensor_tensor(out=t4[:, q, 0:N], in0=ti[:, 0:N], scalar=1.0, in1=xcb[par][:, 0:N],
                                                             op0=ALU.add, op1=ALU.mult), reads=("ti", ck), writes=("t4_%d" % q,))

            def st4(c0):
                sk = tuple("s4_%d" % q for q in range(4))
                S.op("act", lambda e: e.activation(out=s4[:, :, 0:N], in_=s4[:, :, 0:N], func=AF.Sqrt, scale=-0.25, bias=qtr[:, 0:1]),
                     reads=sk + ("qtr",), writes=sk)
                for c in range(c0, c0 + 4):
                    q = c % 4
                    S.op("pool", lambda e, q=q: e.tensor_tensor(out=bb_t[:, 0:N], in0=s4[:, q, 0:N], in1=t4[:, q, 0:N], op=ALU.mult),
                         reads=("s4_%d" % q, "t4_%d" % q), writes=("bb_t",))
                    if reverse:
                        S.op("dve", lambda e, q=q, c=c: e.tensor_tensor_scan(
                            out=hf[:, 0:N][:, ::-1], data0=a4[:, q, 0:N][:, ::-1], data1=bb_t[:, 0:N][:, ::-1],
                            initial=state[:, d, c:c + 1], op0=ALU.mult, op1=ALU.add),
                            reads=("a4_%d" % q, "bb_t", "state"), writes=("hf",))
                        S.op("pool", lambda e, c=c: e.tensor_copy(out=state[:, d, c:c + 1], in_=hf[:, 0:1]),
                             reads=("hf",), writes=("state",))
                    else:
                        S.op("dve", lambda e, q=q, c=c: e.tensor_tensor_scan(
                            out=hf[:, 0:N], data0=a4[:, q, 0:N], data1=bb_t[:, 0:N], initial=state[:, d, c:c + 1],
                            op0=ALU.mult, op1=ALU.add), reads=("a4_%d" % q, "bb_t", "state"), writes=("hf",))
                        S.op("pool", lambda e, c=c: e.tensor_copy(out=state[:, d, c:c + 1], in_=hf[:, N - 1:N]),
                             reads=("hf",), writes=("state",))
                    if consumer is not None:
                        consumer(c)

            for s_ in range(10):
                if s_ < 8:
                    st1(s_)
                if 1 <= s_ <= 8:
                    st2(s_ - 1)
                if 2 <= s_ <= 9:
                    st3(s_ - 2)
                    if (s_ - 2) % 4 == 3:
                        st4(s_ - 2 - 3)

        hxT, hxk, hxhk = hxTs[0], "hxT0", "hxTh0"
        prep(ctxp, 2, 256, s1c, 1, "xt")
        hbc[0] = 1
        rglru_block(256, 0, False, False, False)
        rglru_block(256, 1, True, False, False)
        for blk in range(15, -1, -1):
            _i = hbc[0] % 2
            hbc[0] += 1
            hxT, hxk, hxhk = hxTs[_i], "hxT%d" % _i, "hxTh%d" % _i
            prep(xp, 2 + NB * blk, NB, s1, 0, "xt")
            def cons_a(c):
                S.op("dve", lambda e, c=c: e.tensor_copy(out=hsb[:, c, :], in_=hf[:]), reads=("hf",), writes=("hsb",))
            rglru_block(NB, 1, True, blk != 0, blk != 15, cons_a if blk < 8 else None)
            if blk < 8:
                S.dma("sp", hs_scr[blk], hsb[:], reads=("hsb",), writes=("hs_scr%d" % blk,), key="hs_scr")

        pb_ = ExitStack()
        cwh = cw_sb
        S.op("dve", lambda e: e.tensor_scalar_mul(out=cwh[:], in0=cw_sb[:], scalar1=0.5), reads=("params",), writes=("cwh",))
        lnr = sb("lnr", [128, NB], F32, pb_)
        lmr = sb("lmr", [128, NB], F32, pb_)
        wsl = [sb("wsl%d" % i, [128, 8, 512], BF16, pb_) for i in range(3)]
        dgc = [sb("dgc0", [128, 31, 128], BF16, pb_)] * 2
        tv, uu = tr, ti
        zb = [sb("zb0", [128, NB], BF16, pb_)] * 2
        zc = sb("zc", [128, 8, NB], BF16, pb_)
        aa = zc
        zsq = [sb("zsq0", [128, NB], BF16, pb_)] * 2
        A_t = sb("A_t", [128, 8, NB], BF16, pb_)
        gy = sb("gy", [128, 8, NB], BF16, pb_)
        mg = gy
        yb = sb("yb", [128, 8, NB], BF16, pb_)
        hs_in = hsb
        x1 = xt

        xres = [sb("xres%d" % i, [128, 512], F32, pb_) for i in range(3)]
        xrc = [0]
        wctr = [0]

        def wpiece(col0, src=None):
            src = w_in if src is None else src
            i = wctr[0] % 3
            wctr[0] += 1
            S.dma("pool", wsl[i][:], src[:, col0:col0 + 512].rearrange("(k p) n -> p k n", p=128),
                  writes=("wsl%d" % i,), key="wsl%d" % i)
            return wsl[i], "wsl%d" % i

        def inproj(dstbank, wt, wk, j4):
            S.group("pe", [lambda e, k=k: e.matmul(bank(dstbank), lhsT=wt[:, k, 128 * j4:128 * j4 + 128], rhs=hxT[:, k, 0:NB],
                                                   start=(k == 0), stop=(k == 7)) for k in range(8)],
                    reads=(hxk, wk), writes=(pk(dstbank),))

        xr_evac_eng[0] = "act"
        for blk in range(8):
            _i = hbc[0] % 2
            hbc[0] += 1
            hxT, hxk, hxhk = hxTs[_i], "hxT%d" % _i, "hxTh%d" % _i
            prep(xp, 2 + NB * blk, NB, s1, 0, "xt")
            S.dma("sp", hs_in[:], hs_scr[blk], reads=("hs_scr%d" % blk,), writes=("hsb",), key="hs_in")
            for half in range(2):
                wu, wuk = wpiece(512 * half)
                wv, wvk = wpiece(1024 + 512 * half)
                for c4 in range(4):
                    c = 4 * half + c4
                    par = c % 2
                    inproj(par, wu, wuk, c4)
                    inproj(2 + par, wv, wvk, c4)
                    S.op("act", lambda e, c=c, par=par: e.activation(out=tv[:], in_=bank(2 + par), func=AF.Tanh, scale=0.5,
                                                                     bias=hb_in[:, 8 + c:9 + c]),
                         reads=(pk(2 + par), "hb_in"), writes=("tr",))
                    S.op("act", lambda e, c=c, par=par: e.activation(out=uu[:], in_=bank(par), func=AF.Identity,
                                                                     bias=b_in_sb[:, c:c + 1]),
                         reads=(pk(par), "params"), writes=("ti",))
                    zk = "zb0"
                    S.op("dve", lambda e, par=par: e.scalar_tensor_tensor(out=zb[par][:], in0=tv[:], scalar=1.0, in1=uu[:],
                                                                          op0=ALU.add, op1=ALU.mult),
                         reads=("tr", "ti"), writes=(zk,))
                    dk = "dgc0"
                    S.op("dve", lambda e, c=c, par=par: e.tensor_tensor(
                        out=dgc[par][:], in0=identb[:].unsqueeze(1).to_broadcast([128, 31, 128]),
                        in1=cwh[:, c, :].unsqueeze(2).to_broadcast([128, 31, 128]), op=ALU.mult),
                        reads=("identb", "cwh"), writes=(dk,))
                    zv = zb[par][:].rearrange("p (r t) -> p r t", t=64)
                    pcv = bank(4 + par).rearrange("p (r t) -> p r t", t=64)
                    fns = []
                    order = [15] + [k for k in range(31) if k != 15]
                    for idx, k in enumerate(order):
                        o = k - 15
                        t0, t1 = max(0, -o), 64 - max(0, o)
                        fns.append(lambda e, k=k, o=o, t0=t0, t1=t1, idx=idx, par=par, pcv=pcv, zv=zv: e.matmul(
                            pcv[:, :, t0:t1], lhsT=dgc[par][:, k, :], rhs=zv[:, :, t0 + o:t1 + o],
                            start=(idx == 0), stop=(idx == 30)))
                    S.group("pe", fns, reads=(zk, dk), writes=(pk(4 + par),))
                    S.op("act", lambda e, c=c, par=par: e.activation(out=zc[:, c, :], in_=bank(4 + par), func=AF.Identity,
                                                                     bias=cb_sb[:, c:c + 1]),
                         reads=(pk(4 + par), "params"), writes=("zc",))
                    qk = "zsq0"
                    S.op("act", lambda e, c=c, par=par: e.activation(out=zsq[par][:], in_=bank(4 + par), func=AF.Square,
                                                                     bias=cb_sb[:, c:c + 1]),
                         reads=(pk(4 + par), "params"), writes=(qk,))
                    S.group("pe", [lambda e, c=c: e.matmul(bank(6), lhsT=ones_m[:], rhs=zc[:, c, :], start=(c == 0), stop=(c == 7))],
                            reads=("zc", "ones_m"), writes=("ps6",))
                    S.group("pe", [lambda e, c=c, par=par: e.matmul(bank(7), lhsT=ones_m[:], rhs=zsq[par][:], start=(c == 0),
                                                                    stop=(c == 7))], reads=(qk, "ones_m"), writes=("ps7",))
            S.op("act", lambda e: e.activation(out=tv[:], in_=bank(6), func=AF.Copy), reads=("ps6",), writes=("tr",))
            S.op("dve", lambda e: e.tensor_tensor(out=uu[:], in0=tv[:], in1=tv[:], op=ALU.mult), reads=("tr",), writes=("ti",))
            S.op("dve", lambda e: e.tensor_tensor(out=lnr[:], in0=bank(7), in1=uu[:], op=ALU.subtract), reads=("ps7", "ti"),
                 writes=("lnr",))
            S.op("act", lambda e: e.activation(out=lnr[:], in_=lnr[:], func=AF.Sqrt, bias=qtr[:, 2:3]), reads=("lnr", "qtr"), writes=("lnr",))
            S.op("dve", lambda e: e.reciprocal(out=lnr[:], in_=lnr[:]), reads=("lnr",), writes=("lnr",))
            S.op("dve", lambda e: e.tensor_tensor(out=lmr[:], in0=tv[:], in1=lnr[:], op=ALU.mult), reads=("tr", "lnr"),
                 writes=("lmr",))
            for half in range(2):
                wy, wyk = wpiece(2048 + 512 * half)
                for c4 in range(4):
                    c = 4 * half + c4
                    par = c % 2
                    inproj(par, wy, wyk, c4)
                    S.op("act", lambda e, c=c, par=par: e.activation(out=gy[:, c, :], in_=bank(par), func=AF.Gelu_apprx_tanh,
                                                                     bias=b_in_sb[:, 16 + c:17 + c]),
                         reads=(pk(par), "params"), writes=("gy",))
            def cons_b(c):
                S.op("dve", lambda e, c=c: e.tensor_tensor(out=tmp1[:], in0=hf[:], in1=hs_in[:, c, :], op=ALU.add),
                     reads=("hf", "hsb"), writes=("tmp1",))
                S.op("dve", lambda e, c=c: e.tensor_tensor(out=yb[:, c, :], in0=tmp1[:], in1=gy[:, c, :], op=ALU.mult),
                     reads=("tmp1", "gy"), writes=("yb",))
            rglru_block(NB, 0, False, blk != 0, True, cons_b)
            for c in range(8):
                S.op("dve", lambda e, c=c: e.tensor_tensor(out=tv[:], in0=zc[:, c, :], in1=lnr[:], op=ALU.mult),
                     reads=("zc", "lnr"), writes=("tr",))
                S.op("dve", lambda e: e.tensor_tensor(out=uu[:], in0=tv[:], in1=lmr[:], op=ALU.subtract), reads=("tr", "lmr"),
                     writes=("ti",))
                S.op("act", lambda e, c=c: e.activation(out=aa[:, c, :], in_=uu[:], func=AF.Silu, scale=lng_sb[:, c:c + 1],
                                                        bias=lnb_sb[:, c:c + 1]), reads=("ti", "params"), writes=("zc",))
            for half in range(2):
                wga, wgak = wpiece(4096 + 512 * half)
                wpa, wpak = wpiece(512 * half, w_pa)
                for m4 in range(4):
                    m = 4 * half + m4
                    par = m % 2
                    S.group("pe", [lambda e, k=k, m4=m4, par=par, wpa=wpa: e.matmul(bank(par), lhsT=wpa[:, k, 128 * m4:128 * m4 + 128],
                                                                         rhs=aa[:, k, :], start=(k == 0), stop=(k == 7))
                                   for k in range(8)], reads=("zc", wpak), writes=(pk(par),))
                    inproj(2 + par, wga, wgak, m4)
                    S.op("act", lambda e, m=m, par=par: e.activation(out=tv[:], in_=bank(2 + par), func=AF.Tanh, scale=0.5,
                                                                     bias=hb_in[:, 32 + m:33 + m]),
                         reads=(pk(2 + par), "hb_in"), writes=("tr",))
                    S.op("dve", lambda e, m=m, par=par: e.scalar_tensor_tensor(out=A_t[:, m, :], in0=tv[:], scalar=1.0,
                                                                               in1=bank(par), op0=ALU.add, op1=ALU.mult),
                         reads=("tr", pk(par)), writes=("A_t",))
            for half in range(2):
                wgb_, wgbk = wpiece(5120 + 512 * half)
                wpb, wpbk = wpiece(512 * half, w_pb)
                for m4 in range(4):
                    m = 4 * half + m4
                    par = m % 2
                    S.group("pe", [lambda e, k=k, m4=m4, par=par, wpb=wpb: e.matmul(bank(par), lhsT=wpb[:, k, 128 * m4:128 * m4 + 128],
                                                                         rhs=yb[:, k, :], start=(k == 0), stop=(k == 7))
                                   for k in range(8)], reads=("yb", wpbk), writes=(pk(par),))
                    inproj(2 + par, wgb_, wgbk, m4)
                    S.op("act", lambda e, m=m, par=par: e.activation(out=tv[:], in_=bank(2 + par), func=AF.Tanh, scale=0.5,
                                                                     bias=hb_in[:, 40 + m:41 + m]),
                         reads=(pk(2 + par), "hb_in"), writes=("tr",))
                    S.op("dve", lambda e, par=par: e.scalar_tensor_tensor(out=uu[:], in0=tv[:], scalar=1.0, in1=bank(par),
                                                                          op0=ALU.add, op1=ALU.mult),
                         reads=("tr", pk(par)), writes=("ti",))
                    S.op("dve", lambda e, m=m: e.tensor_tensor(out=mg[:, m, :], in0=uu[:], in1=A_t[:, m, :], op=ALU.add),
                         reads=("ti", "A_t"), writes=("gy",))
            for hh in range(2):
                wo, wok = wpiece(512 * hh, w_o)
                for j in range(4):
                    bk = 4 + j
                    xi = xrc[0] % 3
                    xrc[0] += 1
                    xk_ = "xres%d" % xi
                    r0_ = 2 + NB * blk + 128 * j
                    S.dma("sp", xres[xi][:], xp[r0_:r0_ + 128, 512 * hh:512 * hh + 512], writes=(xk_,), key=xk_)
                    S.group("pe", [lambda e, k=k, j=j, bk=bk, wo=wo: e.matmul(bank(bk), lhsT=mg[:, k, 128 * j:128 * j + 128],
                                                                             rhs=wo[:, k, :], start=(k == 0), stop=(k == 7))
                                   for k in range(8)], reads=("gy", wok), writes=(pk(bk),))
                    S.op("dve", lambda e, hh=hh, bk=bk: e.tensor_tensor(out=tmp1[:], in0=bank(bk), in1=gt1h[:, 512 * hh:512 * hh + 512],
                                                                        op=ALU.mult), reads=(pk(bk), "gt"), writes=("tmp1",))
                    S.op("pool", lambda e, xi=xi: e.tensor_tensor(out=xres[xi][:], in0=xres[xi][:], in1=tmp1[:], op=ALU.add),
                         reads=("tmp1", xk_), writes=(xk_,))
                    S.dma("sp", x1_scr[NB * blk + 128 * j:NB * blk + 128 * j + 128, 512 * hh:512 * hh + 512], xres[xi][:],
                          reads=(xk_,), writes=("x1_scr%d" % blk,), key="x1_scr")
        S.barrier()
        pb_.close()
        mixer.close()

        def bc(ap, shape):
            return ap.to_broadcast(shape)

        pcg = ExitStack()
        gf32 = sb("gf32", [128, 1024], F32, pcg)
        gt2b = sb("gt2b", [128, 1024], F32, pcg)
        S.dma("sp", gf32[:], gfin, writes=("gf32",), key="gf32")
        S.dma("sp", gt2b[:], gt2_scr, reads=("gt2_scr",), writes=("gt2b",), key="gt2b")
        S.op("dve", lambda e: e.tensor_scalar_mul(out=gf32[:], in0=gf32[:], scalar1=32.0), reads=("gf32",), writes=("gf32",))
        slot_i = sb("slot_i", [128, 32], I32, pcg)
        offE_i = sb("offE_i", [128, NSEG, 4], I32, pcg)
        trib = sb("trib", [128, 128], BF16, pcg)
        S.dma("pool", trib[:], tri, writes=("trib",), key="trib")

        c1 = ExitStack()
        x1l = [sb("x1l%d" % i, [128, 4, 1024], F32, c1) for i in range(2)]
        xn2_all = sb("xn2_all", [128, 32, 1024], BF16, c1)
        hmT1 = sb("hmTr", [128, 8, NB], BF16, c1)
        zt = sb("zt", [128, 4096], BF16, c1)
        ztf = sb("ztf", [128, 192], F32, c1)
        ssA = sb("ssA", [128, 8, 4], F32, c1)
        rsA = sb("rsA", [128, 8, 4], F32, c1)
        oh_all = sb("oh_all", [128, 32, 4], F32, c1)
        wsel_all = sb("wsel_all", [128, 32, 4], F32, c1)
        oh_bf = sb("oh_bf", [128, 32, 4], BF16, c1)
        R1s = sb("R1s", [128, 32, 4], F32, c1)
        Cs = sb("Cs", [128, 32, 4], F32, c1)
        incl = sb("incl", [128, 4, 32], F32, c1)
        onesf = sb("onesf", [128, 32], F32, c1)
        ng = sb("ng", [128, 4], F32, c1)
        nseg = sb("nseg", [128, 4], F32, c1)
        sst = sb("sst", [128, 4], F32, c1)
        sen = sb("sen", [128, 4], F32, c1)
        slot_f = sb("slot_f", [128, 32], F32, c1)
        Gs = sb("Gs", [128, NSEG], F32, c1)
        sidx_sb = sb("sidx_sb", [128, NSEG], F32, c1)
        cE_sb = sb("cE_sb", [128, 4], F32, c1)
        offE_f = sb("offE_f", [128, NSEG, 4], F32, c1)
        L = sb("L", [128, 4, 20], F32, c1)
        gmax = sb("gmax", [128, 4, 1], F32, c1)
        eg = sb("eg", [128, 4, 4], F32, c1)
        pg = sb("pg", [128, 4, 1], F32, c1)
        tmp16 = sb("tmp16", [128, 4, 16], F32, c1)
        esel = sb("esel", [128, 4, 4], F32, c1)
        m1 = sb("m1", [128, 4, 1], F32, c1)
        m2 = sb("m2", [128, 4, 1], F32, c1)
        k1 = sb("k1", [128, 4, 4], F32, c1)
        k2 = sb("k2", [128, 4, 4], F32, c1)
        e2 = sb("e2", [128, 4, 4], F32, c1)
        w1 = sb("w1", [128, 4, 1], F32, c1)
        w2 = sb("w2", [128, 4, 1], F32, c1)
        S.dma("sp", sidx_sb[:], sidx, writes=("cidx",), key="cidx")
        S.dma("sp", cE_sb[:], cE, writes=("cidx",), key="cidx")
        S.op("pool", lambda e: e.memset(zt[:], 0.0), writes=("zt",))
        S.op("pool", lambda e: e.memset(ztf[:], 0.0), writes=("zt",))
        S.op("pool", lambda e: e.memset(onesf[:], 1.0), writes=("onesf",))
        for sg_ in range(NSEG):
            S.dma("sp", xs_sorted[NB * sg_:NB * sg_ + NB, :].rearrange("(p r) d -> p (r d)", r=4), zt[:],
                  reads=("zt",), writes=("xs_sorted",), key="xs_z")
        S.dma("sp", ws_sorted.rearrange("(p r) c -> p (r c)", r=48), ztf[:], reads=("zt",), writes=("ws_sorted",), key="xs_z")

        for blk in range(8):
            pb2 = blk % 2
            x1t = x1l[pb2]
            ak = "x1l%d" % pb2
            sak = "ssA%d" % blk
            oh = oh_all[:, 4 * blk:4 * blk + 4, :]
            wsel = wsel_all[:, 4 * blk:4 * blk + 4, :]
            S.dma("sp", x1t[:], x1_scr[NB * blk:NB * blk + NB, :].rearrange("(j p) d -> p j d", p=128),
                  reads=("x1_scr%d" % blk,), writes=(ak,), key=ak)
            S.op("pool", lambda e, blk=blk: e.memset(ssA[:, blk, :], 0.0), writes=(sak,))
            for j in range(4):
                S.op("act", lambda e, j=j, x1t=x1t, blk=blk: e.activation(out=xn2_all[:, 4 * blk + j, :], in_=x1t[:, j, :], func=AF.Square,
                                                                          accum_out=ssA[:, blk, j:j + 1]),
                     reads=(ak,), writes=("xn2_%d" % blk, sak))
            S.op("act", lambda e, blk=blk: e.activation(out=rsA[:, blk, :], in_=ssA[:, blk, :], func=AF.Sqrt, bias=qtr[:, 1:2]),
                 reads=(sak, "qtr"), writes=(sak + "r",))
            S.op("dve", lambda e, blk=blk: e.reciprocal(out=rsA[:, blk, :], in_=rsA[:, blk, :]), reads=(sak + "r",), writes=(sak + "r",))
            for j in range(4):
                S.op("dve", lambda e, j=j, x1t=x1t, blk=blk: e.tensor_scalar_mul(out=xn2_all[:, 4 * blk + j, :], in0=x1t[:, j, :],
                                                                                 scalar1=rsA[:, blk, j:j + 1]),
                     reads=(ak, sak + "r"), writes=("xn2_%d" % blk,))
            for j in range(4):
                S.group("pe", [lambda e, j=j, c=c, blk=blk: e.transpose(out=tpv[:, c, 128 * j:128 * j + 128],
                                                                        in_=xn2_all[:, 4 * blk + j, 128 * c:128 * c + 128], identity=identb[:])
                               for c in range(8)], reads=("xn2_%d" % blk, "identb"), writes=TPK)
            for c in range(8):
                S.op("act", lambda e, c=c: e.activation(out=hmT1[:, c, :], in_=tpv[:, c, :], func=AF.Identity,
                                                        scale=s2[:, c:c + 1], bias=mods[:, 24 + c, 0:1]),
                     reads=TPK + ("sc", "mods"), writes=("hmT1",))
            for j in range(4):
                S.group("pe", [lambda e, k=k, j=j: e.matmul(bank(4)[:, 20 * j:20 * j + 20], lhsT=hmT1[:, k, 128 * j:128 * j + 128],
                                                            rhs=w_rt_b[:, k, :], start=(k == 0), stop=(k == 7))
                               for k in range(8)], reads=("hmT1", "w_rt_b"), writes=("ps4",))
            S.op("dve", lambda e: e.tensor_tensor(out=L[:], in0=bank(4)[:, 0:80].rearrange("p (j n) -> p j n", n=20),
                                                  in1=bc(b_rt_sb[:].unsqueeze(1), [128, 4, 20]), op=ALU.add),
                 reads=("ps4", "params"), writes=("L",))
            R = ("rt",)
            OK_ = ("oh_all",)
            S.op("dve", lambda e: e.tensor_reduce(out=gmax[:], in_=L[:, :, 0:4], axis=AX.X, op=ALU.max), reads=("L",), writes=R)
            S.op("dve", lambda e, oh=oh: e.tensor_tensor(out=oh, in0=L[:, :, 0:4], in1=bc(gmax[:], [128, 4, 4]), op=ALU.is_equal),
                 reads=R + ("L",), writes=R + OK_)
            S.op("dve", lambda e: e.tensor_tensor(out=eg[:], in0=L[:, :, 0:4], in1=bc(gmax[:], [128, 4, 4]), op=ALU.subtract),
                 reads=R + ("L",), writes=R)
            S.op("act", lambda e: e.activation(out=eg[:], in_=eg[:], func=AF.Exp), reads=R, writes=R)
            S.op("dve", lambda e: e.tensor_reduce(out=pg[:], in_=eg[:], axis=AX.X, op=ALU.add), reads=R, writes=R)
            S.op("dve", lambda e: e.reciprocal(out=pg[:], in_=pg[:]), reads=R, writes=R)
            S.op("dve", lambda e, oh=oh: e.tensor_tensor(out=tmp16[:].rearrange("p j (g x) -> p j g x", x=4),
                                                         in0=L[:, :, 4:20].rearrange("p j (g x) -> p j g x", x=4),
                                                         in1=bc(oh.unsqueeze(3), [128, 4, 4, 4]), op=ALU.mult),
                 reads=R + ("L",), writes=R)
            S.op("dve", lambda e: e.tensor_reduce(out=esel[:].unsqueeze(3), in_=tmp16[:].rearrange("p j (g x) -> p j x g", x=4),
                                                  axis=AX.X, op=ALU.add), reads=R, writes=R)
            S.op("dve", lambda e: e.tensor_reduce(out=m1[:], in_=esel[:], axis=AX.X, op=ALU.max), reads=R, writes=R)
            S.op("dve", lambda e: e.tensor_tensor(out=k1[:], in0=esel[:], in1=bc(m1[:], [128, 4, 4]), op=ALU.is_equal),
                 reads=R, writes=R)
            S.op("dve", lambda e: e.scalar_tensor_tensor(out=e2[:], in0=k1[:], scalar=-1e30, in1=esel[:], op0=ALU.mult, op1=ALU.add),
                 reads=R, writes=R)
            S.op("dve", lambda e: e.tensor_reduce(out=m2[:], in_=e2[:], axis=AX.X, op=ALU.max), reads=R, writes=R)
            S.op("dve", lambda e: e.tensor_tensor(out=k2[:], in0=e2[:], in1=bc(m2[:], [128, 4, 4]), op=ALU.is_equal),
                 reads=R, writes=R)
            S.op("dve", lambda e: e.tensor_tensor(out=w2[:], in0=m2[:], in1=m1[:], op=ALU.subtract), reads=R, writes=R)
            S.op("act", lambda e: e.activation(out=w2[:], in_=w2[:], func=AF.Exp), reads=R, writes=R)
            S.op("dve", lambda e: e.tensor_scalar_add(out=w1[:], in0=w2[:], scalar1=1.0), reads=R, writes=R)
            S.op("dve", lambda e: e.reciprocal(out=w1[:], in_=w1[:]), reads=R, writes=R)
            S.op("dve", lambda e: e.tensor_tensor(out=w2[:], in0=w2[:], in1=w1[:], op=ALU.mult), reads=R, writes=R)
            S.op("dve", lambda e: e.tensor_tensor(out=w1[:], in0=w1[:], in1=pg[:], op=ALU.mult), reads=R, writes=R)
            S.op("dve", lambda e: e.tensor_tensor(out=w2[:], in0=w2[:], in1=pg[:], op=ALU.mult), reads=R, writes=R)
            S.op("dve", lambda e, wsel=wsel: e.tensor_tensor(out=wsel, in0=k1[:], in1=bc(w1[:], [128, 4, 4]), op=ALU.mult),
                 reads=R, writes=R + ("wsel_all",))
            S.op("dve", lambda e: e.tensor_tensor(out=k2[:], in0=k2[:], in1=bc(w2[:], [128, 4, 4]), op=ALU.mult), reads=R, writes=R)
            S.op("dve", lambda e, wsel=wsel: e.tensor_tensor(out=wsel, in0=wsel, in1=k2[:], op=ALU.add), reads=R + ("wsel_all",),
                 writes=R + ("wsel_all",))

        ohf = oh_all[:].rearrange("p t g -> p (t g)")
        S.op("dve", lambda e: e.tensor_copy(out=oh_bf[:], in_=oh_all[:]), reads=("oh_all",), writes=("oh_bf",))
        S.group("pe", [lambda e: e.matmul(bank(0)[:, 0:128], lhsT=trib[:], rhs=oh_bf[:].rearrange("p t g -> p (t g)"), start=True, stop=True)],
                reads=("oh_bf", "trib"), writes=("ps0",))
        S.group("pe", [lambda e: e.matmul(bank(1)[:, 0:128], lhsT=ones1[:], rhs=oh_bf[:].rearrange("p t g -> p (t g)"), start=True, stop=True)],
                reads=("oh_bf", "ones1"), writes=("ps1",))
        S.op("act", lambda e: e.activation(out=R1s[:].rearrange("p t g -> p (t g)"), in_=bank(0)[:, 0:128], func=AF.Copy),
             reads=("ps0",), writes=("R1s",))
        S.op("act", lambda e: e.activation(out=Cs[:].rearrange("p t g -> p (t g)"), in_=bank(1)[:, 0:128], func=AF.Copy),
             reads=("ps1",), writes=("Cs",))
        for g in range(4):
            S.op("dve", lambda e, g=g: e.tensor_tensor_scan(out=incl[:, g, :], data0=onesf[:], data1=Cs[:, :, g], initial=0.0,
                                                            op0=ALU.mult, op1=ALU.add), reads=("Cs", "onesf"), writes=("incl",))
        S.op("dve", lambda e: e.tensor_copy(out=ng[:], in_=incl[:, :, 31]), reads=("incl",), writes=("ng",))
        S.op("dve", lambda e: e.tensor_tensor(out=incl[:], in0=incl[:], in1=Cs[:].rearrange("p t g -> p g t"), op=ALU.subtract),
             reads=("incl", "Cs"), writes=("incl",))
        S.op("dve", lambda e: e.memset(nseg[:], 0.0), writes=("nseg",))
        for k in range(8):
            S.op("dve", lambda e, k=k: e.scalar_tensor_tensor(out=nseg[:], in0=ng[:], scalar=float(NB * k), in1=nseg[:],
                                                              op0=ALU.is_gt, op1=ALU.add), reads=("ng", "nseg"), writes=("nseg",))
        S.op("dve", lambda e: e.memset(sst[:], 0.0), writes=("sst",))
        for g in range(1, 4):
            S.op("dve", lambda e, g=g: e.tensor_tensor(out=sst[:, g:g + 1], in0=sst[:, g - 1:g], in1=nseg[:, g - 1:g], op=ALU.add),
                 reads=("sst", "nseg"), writes=("sst",))
        S.op("dve", lambda e: e.tensor_tensor(out=sen[:], in0=sst[:], in1=nseg[:], op=ALU.add), reads=("sst", "nseg"), writes=("sen",))
        S.op("dve", lambda e: e.tensor_scalar_mul(out=sst[:], in0=sst[:], scalar1=float(NB)), reads=("sst", "sen"), writes=("sst",))
        S.op("dve", lambda e: e.tensor_tensor(out=R1s[:], in0=R1s[:], in1=incl[:].rearrange("p g t -> p t g"), op=ALU.add),
             reads=("R1s", "incl"), writes=("R1s",))
        S.op("dve", lambda e: e.tensor_tensor(out=R1s[:], in0=R1s[:], in1=bc(sst[:].unsqueeze(1), [128, 32, 4]), op=ALU.add),
             reads=("R1s", "sst"), writes=("R1s",))
        S.op("dve", lambda e: e.tensor_tensor(out=R1s[:], in0=R1s[:], in1=oh_all[:], op=ALU.mult), reads=("R1s", "oh_all"), writes=("R1s",))
        S.op("dve", lambda e: e.tensor_reduce(out=slot_f[:].unsqueeze(2), in_=R1s[:], axis=AX.X, op=ALU.add), reads=("R1s",), writes=("slot_f",))
        S.op("dve", lambda e: e.tensor_copy(out=slot_i[:], in_=slot_f[:]), reads=("slot_f",), writes=("slot_i",))
        S.op("dve", lambda e: e.memset(Gs[:], 0.0), writes=("Gs",))
        for g in range(3):
            S.op("dve", lambda e, g=g: e.scalar_tensor_tensor(out=Gs[:], in0=sidx_sb[:], scalar=sen[:, g:g + 1], in1=Gs[:],
                                                              op0=ALU.is_ge, op1=ALU.add), reads=("cidx", "sen", "Gs"), writes=("Gs",))
        S.op("dve", lambda e: e.tensor_scalar_mul(out=offE_f[:], in0=bc(Gs[:].unsqueeze(2), [128, NSEG, 4]), scalar1=512.0),
             reads=("Gs",), writes=("offE_f",))
        S.op("dve", lambda e: e.tensor_tensor(out=offE_f[:], in0=offE_f[:], in1=bc(cE_sb[:].unsqueeze(1), [128, NSEG, 4]), op=ALU.add),
             reads=("offE_f", "cidx"), writes=("offE_f",))
        S.op("dve", lambda e: e.tensor_copy(out=offE_i[:], in_=offE_f[:]), reads=("offE_f",), writes=("offE_i",))
        for t in range(32):
            S.idma("pool", xs_sorted[:, :], bass.IndirectOffsetOnAxis(ap=slot_i[:, t:t + 1], axis=0), xn2_all[:, t, :], None,
                   reads=("slot_i", "xn2_%d" % (t // 4), "xs_sorted"), writes=("xs_sorted_s",), key="scat")
            S.idma("pool", ws_sorted[:, :], bass.IndirectOffsetOnAxis(ap=slot_i[:, t:t + 1], axis=0), wsel_all[:, t, :], None,
                   reads=("slot_i", "wsel_all", "ws_sorted"), writes=("ws_sorted_s",), key="scat")
        S.barrier()
        c1.close()

        S.ALPHA = 0.0
        c2 = ExitStack()
        xst = [sb("xst%d" % i, [128, 4, 1024], BF16, c2) for i in range(2)]
        wst = [sb("wst%d" % i, [128, 4, 4], F32, c2) for i in range(2)]
        hmTs = [sb("hmT%d" % i, [128, 8, NB], BF16, c2) for i in range(2)]
        cbc = [sb("cbc%d" % i, [128, 4, NB], BF16, c2) for i in range(2)]
        dgm = sb("dgm", [128, 4, 128], BF16, c2)
        actb = [sb("actb%d" % i, [128, NB], BF16, c2) for i in range(16)]
        wgu = [sb("wgu%d" % i, [128, 2, 8, 512], BF16, c2) for i in range(3)]
        NWD = 5
        wd = [sb("wd%d" % i, [128, 4, 1024], BF16, c2) for i in range(NWD)]
        sg = [sb("sg%d" % i, [128, NB], F32, c2) for i in range(2)]
        tt = [sb("tt%d" % i, [128, NB], BF16, c2) for i in range(2)]
        ysb = [sb("ysb%d" % i, [128, 4, 1024], F32, c2) for i in range(2)]
        ectr = [0]
        for sgi in range(NSEG):
            pb2 = sgi % 2
            hmT, hk = hmTs[pb2], "hmT%d" % pb2
            xk2, wk2, ck2, yk2 = "xst%d" % pb2, "wst%d" % pb2, "cbc%d" % pb2, "ysb%d" % pb2
            S.dma("sp", xst[pb2][:], xs_sorted[NB * sgi:NB * sgi + NB, :].rearrange("(j p) d -> p j d", p=128), writes=(xk2,), key=xk2)
            S.dma("sp", wst[pb2][:], ws_sorted[NB * sgi:NB * sgi + NB, :].rearrange("(j p) c -> p j c", p=128), writes=(wk2,), key=wk2)
            for j in range(4):
                S.group("pe", [lambda e, j=j, c=c, pb2=pb2: e.transpose(out=tpv[:, c, 128 * j:128 * j + 128],
                                                                        in_=xst[pb2][:, j, 128 * c:128 * c + 128], identity=identb[:])
                               for c in range(8)], reads=(xk2, "identb"), writes=TPK)
            for c in range(8):
                S.op("act", lambda e, c=c, hmT=hmT: e.activation(out=hmT[:, c, :], in_=tpv[:, c, :], func=AF.Identity,
                                                                 scale=s2[:, c:c + 1], bias=mods[:, 24 + c, 0:1]),
                     reads=TPK + ("sc", "mods"), writes=(hk,))
            for j in range(4):
                S.op("dve", lambda e, j=j, pb2=pb2: e.tensor_tensor(out=dgm[:], in0=bc(identb[:].unsqueeze(1), [128, 4, 128]),
                                                                    in1=bc(wst[pb2][:, j, :].unsqueeze(2), [128, 4, 128]), op=ALU.mult),
                     reads=("identb", wk2), writes=("dgm",))
                S.group("pe", [lambda e: e.matmul(bank(4 + (j % 2)), lhsT=ones1[:], rhs=dgm[:], start=True, stop=True)],
                        reads=("dgm", "ones1"), writes=(pk(4 + (j % 2)),))
                S.op("act", lambda e, j=j, pb2=pb2: e.activation(out=cbc[pb2][:, :, 128 * j:128 * j + 128],
                                                                 in_=bank(4 + (j % 2)).rearrange("p (x t) -> p x t", t=128), func=AF.Copy),
                     reads=(pk(4 + (j % 2)),), writes=(ck2,))
            for el in range(4):
                si = ectr[0] % 3
                di = ectr[0] % NWD
                ectr[0] += 1
                gk, dk_ = "wgu%d" % si, "wd%d" % di
                ofs = bass.IndirectOffsetOnAxis(ap=offE_i[:, sgi, el:el + 1], axis=0)
                S.idma("pool", wgu[si][:, 0].rearrange("p k n -> p (k n)"), None, w_gate[:, :], ofs, reads=("offE_i",), writes=(gk,), key=gk)
                S.idma("pool", wgu[si][:, 1].rearrange("p k n -> p (k n)"), None, w_up[:, :], ofs, reads=("offE_i",), writes=(gk,), key=gk)
                S.idma("pool", wd[di][:].rearrange("p k n -> p (k n)"), None, w_down[:, :], ofs, reads=("offE_i",), writes=(dk_,), key=dk_)
                for f in range(4):
                    u = 4 * el + f
                    pp = u % 2
                    S.group("pe", [lambda e, k=k, f=f, si=si, pp=pp, hmT=hmT: e.matmul(
                        bank(2 * pp), lhsT=wgu[si][:, 0, k, 128 * f:128 * f + 128], rhs=hmT[:, k, :],
                        start=(k == 0), stop=(k == 7)) for k in range(8)], reads=(hk, gk), writes=(pk(2 * pp),))
                    S.group("pe", [lambda e, k=k, f=f, si=si, pp=pp, hmT=hmT: e.matmul(
                        bank(2 * pp + 1), lhsT=wgu[si][:, 1, k, 128 * f:128 * f + 128], rhs=hmT[:, k, :],
                        start=(k == 0), stop=(k == 7)) for k in range(8)], reads=(hk, gk), writes=(pk(2 * pp + 1),))
                    S.op("act", lambda e, pp=pp: e.activation(out=sg[pp][:], in_=bank(2 * pp), func=AF.Silu),
                         reads=(pk(2 * pp),), writes=("sg%d" % pp,))
                    S.op("dve", lambda e, pp=pp: e.tensor_tensor(out=tt[pp][:], in0=bank(2 * pp + 1), in1=sg[pp][:], op=ALU.mult),
                         reads=(pk(2 * pp + 1), "sg%d" % pp), writes=("tt%d" % pp,))
                    S.op("dve", lambda e, pp=pp, u=u, el=el, pb2=pb2: e.tensor_tensor(out=actb[u][:], in0=tt[pp][:], in1=cbc[pb2][:, el, :],
                                                                                      op=ALU.mult),
                         reads=("tt%d" % pp, ck2), writes=("actb%d" % u,))
            dbase = ectr[0] - 4
            for tp_ in range(2):
                fns = []
                for u in range(16):
                    el, f = divmod(u, 4)
                    di = (dbase + el) % NWD
                    for jj in range(2):
                        j = 2 * tp_ + jj
                        for hh in range(2):
                            fns.append(lambda e, u=u, f=f, di=di, j=j, jj=jj, hh=hh: e.matmul(
                                bank(4 + 2 * jj + hh), lhsT=actb[u][:, 128 * j:128 * j + 128],
                                rhs=wd[di][:, f, 512 * hh:512 * hh + 512], start=(u == 0), stop=(u == 15)))
                S.group("pe", fns, reads=tuple("actb%d" % u for u in range(16)) + tuple("wd%d" % ((dbase + el) % NWD) for el in range(4)),
                        writes=("ps4", "ps5", "ps6", "ps7"))
                for jj in range(2):
                    j = 2 * tp_ + jj
                    for hh in range(2):
                        bk = 4 + 2 * jj + hh
                        S.op("dve", lambda e, bk=bk, hh=hh, j=j, pb2=pb2: e.tensor_tensor(
                            out=ysb[pb2][:, j, 512 * hh:512 * hh + 512], in0=bank(bk), in1=gt2b[:, 512 * hh:512 * hh + 512], op=ALU.mult),
                            reads=(pk(bk), "gt2b"), writes=(yk2,))
            S.dma("sp", y_sorted[NB * sgi:NB * sgi + NB, :].rearrange("(j p) d -> p j d", p=128), ysb[pb2][:],
                  reads=(yk2,), writes=("y_sorted",), key="y_sorted")
        S.barrier()
        c2.close()

        S.ALPHA = 0.05
        c3 = ExitStack()
        yg = [sb("yg%d" % i, [128, 4, 1024], F32, c3) for i in range(2)]
        x1b = [sb("x1b%d" % i, [128, 4, 1024], F32, c3) for i in range(2)]
        junkF = sb("junkF", [128, 1024], BF16, c3)
        ssF = sb("ssF", [128, 8, 4], F32, c3)
        rsF = sb("rsF", [128, 8, 4], F32, c3)
        for blk in range(8):
            pb2 = blk % 2
            yk3, xk3, sfk = "yg%d" % pb2, "x1b%d" % pb2, "ssF%d" % blk
            S.dma("sp", x1b[pb2][:], x1_scr[NB * blk:NB * blk + NB, :].rearrange("(j p) d -> p j d", p=128), writes=(xk3,), key=xk3)
            for j in range(4):
                S.idma("pool", yg[pb2][:, j, :], None, y_sorted[:, :], bass.IndirectOffsetOnAxis(ap=slot_i[:, 4 * blk + j:4 * blk + j + 1], axis=0),
                       reads=("slot_i",), writes=(yk3,), key=yk3)
            S.op("pool", lambda e, blk=blk: e.memset(ssF[:, blk, :], 0.0), writes=(sfk,))
            for j in range(4):
                S.op("dve", lambda e, j=j, pb2=pb2: e.tensor_tensor(out=x1b[pb2][:, j, :], in0=x1b[pb2][:, j, :], in1=yg[pb2][:, j, :], op=ALU.add),
                     reads=(xk3, yk3), writes=(xk3,))
                S.op("act", lambda e, j=j, pb2=pb2, blk=blk: e.activation(out=junkF[:], in_=x1b[pb2][:, j, :], func=AF.Square,
                                                                          accum_out=ssF[:, blk, j:j + 1]),
                     reads=(xk3,), writes=("junkF", sfk))
            S.op("act", lambda e, blk=blk: e.activation(out=rsF[:, blk, :], in_=ssF[:, blk, :], func=AF.Sqrt, bias=qtr[:, 1:2]),
                 reads=(sfk, "qtr"), writes=(sfk + "r",))
            S.op("dve", lambda e, blk=blk: e.reciprocal(out=rsF[:, blk, :], in_=rsF[:, blk, :]), reads=(sfk + "r",), writes=(sfk + "r",))
            for j in range(4):
                S.op("dve", lambda e, j=j, pb2=pb2, blk=blk: e.scalar_tensor_tensor(out=x1b[pb2][:, j, :], in0=x1b[pb2][:, j, :],
                                                                                    scalar=rsF[:, blk, j:j + 1], in1=gf32[:],
                                                                                    op0=ALU.mult, op1=ALU.mult),
                     reads=(xk3, sfk + "r", "gf32"), writes=(xk3,))
            S.dma("sp", out[NB * blk:NB * blk + NB, :].rearrange("(j p) d -> p j d", p=128), x1b[pb2][:],
                  reads=(xk3,), writes=("out%d" % blk,), key="out")
        S.barrier()
        c3.close()
        pcg.close()
    return nc


_NC_CACHE = {}


def _fm(v):
    v = np.asarray(v, np.float32).reshape(-1, 128)
    return np.ascontiguousarray(v.T)


def kernel(x, c, ctx, c_ctx, w_ada, b_ada, g_mix, w_in, b_in, conv_w, conv_b, ln_g, ln_b, w_pa,
           lru_conv_w, lru_conv_b, w_r_f, b_r_f, w_i_f, b_i_f, lam_f, w_r_b, b_r_b, w_i_b, b_i_b, lam_b,
           w_pb, w_o, g_ffn, w_grp, b_grp, w_er, b_er, w_gate, w_up, w_down, g_final):
    f = lambda a: np.ascontiguousarray(np.asarray(a, np.float32))
    x, c, ctx, c_ctx = f(x), f(c), f(ctx), f(c_ctx)
    B = x.shape[0]
    if "nc" not in _NC_CACHE:
        _NC_CACHE["nc"] = build_program()
    nc = _NC_CACHE["nc"]

    common = {
        "w_ada": f(w_ada[0]), "b_ada_fm": _fm(b_ada[0]),
        "b_ada_gt": f(np.broadcast_to(np.stack([b_ada[0][2048:3072], b_ada[0][5120:6144]])[None], (128, 2, 1024))),
        "w_in": f(w_in[0]), "b_in_fm": _fm(b_in[0]),
        "cb": _fm(conv_b[0]), "lng": _fm(ln_g[0]), "lnb": _fm(ln_b[0]),
        "w_pa": f(w_pa[0]), "w_pb": f(w_pb[0]), "w_o": f(w_o[0]),
        "lb": _fm(lru_conv_b[0]),
        "gmix": _fm(g_mix[0]), "gffn": _fm(g_ffn[0]),
        "gfin": f(np.broadcast_to(np.asarray(g_final, np.float32)[None], (128, 1024))),
        "w_rt": f(np.concatenate([w_grp[0], w_er[0]], axis=1)),
        "b_rt": f(np.broadcast_to(np.concatenate([b_grp[0], b_er[0]])[None], (128, 20))),
        "w_gate": f(np.asarray(w_gate[0], np.float32).reshape(16, 8, 128, 512).transpose(0, 2, 1, 3).reshape(2048, 4096)),
        "w_up": f(np.asarray(w_up[0], np.float32).reshape(16, 8, 128, 512).transpose(0, 2, 1, 3).reshape(2048, 4096)),
        "w_down": f(np.asarray(w_down[0], np.float32).reshape(16, 4, 128, 1024).transpose(0, 2, 1, 3).reshape(2048, 4096)),
        "ident": np.eye(128, dtype=np.float32),
        "tri": np.triu(np.ones((128, 128), np.float32), 1),
        "cE": (np.arange(4)[None, :] * 128 + np.arange(128)[:, None]).astype(np.float32),
        "sidx": np.broadcast_to(np.arange(12, dtype=np.float32)[None], (128, 12)).copy(),
    }
    cwn = np.asarray(conv_w[0], np.float32)
    lwn = np.asarray(lru_conv_w[0], np.float32)
    zero = np.zeros((1, 1024), np.float32)
    lw5_nat = np.concatenate([lwn, zero], axis=0)
    lw5_rev = lw5_nat[::-1]

    def fm3(a):
        T = a.shape[0]
        return np.ascontiguousarray(a.reshape(T, 8, 128).transpose(2, 1, 0))

    pf = (w_r_f[0], b_r_f[0], w_i_f[0], b_i_f[0], lam_f[0])
    pbk = (w_r_b[0], b_r_b[0], w_i_b[0], b_i_b[0], lam_b[0])

    def gates(P, Sd):
        wgs = np.stack([P[0], P[2], Sd[0], Sd[2]]).astype(np.float32)
        bgs = np.stack([np.asarray(t, np.float32) for t in (P[1], P[3], Sd[1], Sd[3])])
        bgs = np.ascontiguousarray(bgs.transpose(2, 0, 1))
        lams = np.stack([np.asarray(P[4], np.float32).reshape(8, 128), np.asarray(Sd[4], np.float32).reshape(8, 128)])
        lams = np.ascontiguousarray(lams.transpose(2, 0, 1))
        return f(wgs), bgs, lams

    per_half = []
    for half in range(2):
        if half == 0:
            wgs, bgs, lams = gates(pf, pbk)
            d = {"cw": fm3(cwn), "lw5": fm3(lw5_nat), "wg": wgs, "bg": bgs, "lam": lams}
        else:
            wgs, bgs, lams = gates(pbk, pf)
            d = {"cw": fm3(cwn[::-1]), "lw5": fm3(lw5_rev), "wg": wgs, "bg": bgs, "lam": lams}
        per_half.append(d)

    in_maps = []
    pad2 = np.zeros((2, 1024), np.float32)
    for b in range(B):
        for half in range(2):
            xs = x[b] if half == 0 else x[b, ::-1]
            cs_ = ctx[b] if half == 0 else ctx[b, ::-1]
            m = dict(common)
            m.update(per_half[half])
            m["xp"] = np.ascontiguousarray(np.concatenate([pad2, xs, pad2], axis=0))
            m["ctxp"] = np.ascontiguousarray(np.concatenate([pad2, cs_, pad2], axis=0))
            m["cvec"] = np.ascontiguousarray(np.stack([_fm(c[b]), _fm(c_ctx)], axis=-1))
            in_maps.append(m)
    res = run_bass_kernel_spmd(nc, in_maps, core_ids=list(range(2 * B)))
    outp = np.empty((B, 2 * NOWN, 1024), np.float32)
    for b in range(B):
        outp[b, :NOWN] = res.results[2 * b]["out"]
        outp[b, NOWN:] = res.results[2 * b + 1]["out"][::-1]
    if DEBUG:
        kernel.last = res
    return outp
```

```python
from contextlib import ExitStack
import os
import numpy as np
import concourse.bass as bass
import concourse.mybir as mybir
from concourse.bass_utils import run_bass_kernel_spmd

F32 = mybir.dt.float32
BF16 = mybir.dt.bfloat16
AF = mybir.ActivationFunctionType
ALU = mybir.AluOpType
AX = mybir.AxisListType
EPS = 1e-6
NB = 512
NOWN = 4096
DEBUG = bool(int(os.environ.get("MK_DEBUG", "0")))


class _Rec:
    def __init__(self):
        self.calls = []

    def __getattr__(self, name):
        def f(*args, **kw):
            self.calls.append((name, args, kw))
            return self
        return f


_TBL = {"Exp": "exp", "Tanh": None, "Identity": None, "Copy": None, "Square": None, "Sqrt": "sqrt", "Silu": "silu",
        "Gelu_apprx_tanh": "gelu", "Ln": "ln"}


def _fsize(ap):
    n = 1
    for d in ap.shape[1:]:
        n *= int(d)
    return n


class Sched:
    REORDER = True
    WINDOW = 600
    ALPHA = 0.05

    def __init__(self, nc, es):
        self.nc = nc
        self.es = es
        self.E = dict(pe=nc.tensor, act=nc.scalar, dve=nc.vector, pool=nc.gpsimd, sp=nc.sync)
        self.sem = {e: es.enter_context(nc.semaphore("c_" + e)) for e in self.E}
        self.cnt = {e: 0 for e in self.E}
        self.seen = {e: {} for e in self.E}
        self.lastw = {}
        self.readers = {}
        self.dsem = {}
        self.dcnt = {}
        self.ops = []

    def op(self, e, fn, reads=(), writes=()):
        r = _Rec()
        fn(r)
        self._add(e, "op", r.calls, tuple(reads), tuple(writes), None)

    def group(self, e, fns, reads=(), writes=()):
        r = _Rec()
        for f in fns:
            f(r)
        self._add(e, "op", r.calls, tuple(reads), tuple(writes), None)

    def dma(self, q, out, in_, reads=(), writes=(), key=None):
        self._add(q, "dma", [("dma_start", (), dict(out=out, in_=in_))], tuple(reads), tuple(writes), key)

    def idma(self, q, out, out_offset, in_, in_offset, reads=(), writes=(), key=None):
        self._add(q, "dma", [("indirect_dma_start", (), dict(out=out, out_offset=out_offset, in_=in_, in_offset=in_offset))],
                  tuple(reads), tuple(writes), key)

    def _add(self, e, kind, calls, reads, writes, key):
        dur = 0.0
        tbl = None
        if kind == "dma":
            kw0 = calls[0][2]
            side = kw0["in_"] if kw0.get("out_offset") is not None else kw0["out"]
            nb = 128 * _fsize(side) * 4
            dur = 1000.0 if e == "pool" else 150.0
            lat = 2000.0 + nb / 300.0
        else:
            lat = 0.0
            for (name, args, kw) in calls:
                if e == "pe":
                    src = kw.get("rhs", kw.get("in_"))
                    dur += 25.0 + 0.5 * max(_fsize(src), 64)
                else:
                    oap = kw.get("out", kw.get("ap", args[0] if args else None))
                    n = _fsize(oap)
                    if e == "act":
                        dur += 250.0 + 0.73 * n
                        fnm = kw.get("func")
                        tbl = _TBL.get(getattr(fnm, "name", str(fnm)), None) if fnm is not None else None
                    elif e == "dve":
                        dur += 160.0 + 1.04 * n
                    else:
                        dur += 300.0 + 3.1 * n
        self.ops.append(dict(e=e, kind=kind, calls=calls, reads=reads, writes=writes, key=key, dur=dur, lat=lat, tbl=tbl))

    def flush(self):
        ops = self.ops
        self.ops = []
        n = len(ops)
        if n == 0:
            return
        lastw, readers = {}, {}
        preds = [None] * n
        succs = [[] for _ in range(n)]
        for i, o in enumerate(ops):
            p = set()
            for k in o["reads"]:
                if k in lastw:
                    p.add(lastw[k])
            for k in o["writes"]:
                if k in lastw:
                    p.add(lastw[k])
                p.update(readers.get(k, ()))
            p.discard(i)
            preds[i] = p
            for j in p:
                succs[j].append(i)
            for k in o["reads"]:
                readers.setdefault(k, []).append(i)
            for k in o["writes"]:
                lastw[k] = i
                readers[k] = []
        if not self.REORDER:
            order = range(n)
        else:
            indeg = [len(p) for p in preds]
            alpha = float(os.environ.get("MK_PRI", self.ALPHA))
            rank = [0.0] * n
            if alpha > 0:
                for i in range(n - 1, -1, -1):
                    m = 0.0
                    for j in succs[i]:
                        if rank[j] > m:
                            m = rank[j]
                    rank[i] = ops[i]["dur"] + ops[i]["lat"] + m
            ready = [i for i in range(n) if indeg[i] == 0]
            finish = [0.0] * n
            efree = {e: 0.0 for e in self.E}
            etbl = [None]
            done = [False] * n
            lo = 0
            order = []
            while len(order) < n:
                while lo < n and done[lo]:
                    lo += 1
                best, bkey = None, None
                for i in ready:
                    if i > lo + self.WINDOW:
                        continue
                    o = ops[i]
                    st = efree[o["e"]]
                    for j in preds[i]:
                        f = finish[j] + (0.0 if ops[j]["e"] == o["e"] else 120.0)
                        if f > st:
                            st = f
                    if o["e"] == "act" and o["tbl"] is not None and o["tbl"] != etbl[0]:
                        st += 1300.0
                    kk = (st - alpha * rank[i], i) if alpha > 0 else (st, i)
                    if bkey is None or kk < bkey:
                        best, bkey = i, kk
                i = best
                o = ops[i]
                st = bkey[0] + (alpha * rank[i] if alpha > 0 else 0.0)
                if os.environ.get("MK_TL") and n > 3000 and len(ops) == int(os.environ.get("MK_TL")):
                    lim = None
                    for j in preds[i]:
                        f = finish[j]
                        if lim is None or f > lim[0]:
                            lim = (f, j)
                    gap = st - efree[o["e"]]
                    if o["e"] == "pe" and gap > 300:
                        print("PE gap %.1fus at t=%.1fus op#%d writes=%s waits for %s op#%d writes=%s" % (
                            gap / 1e3, st / 1e3, i, o["writes"][:2], ops[lim[1]]["e"], lim[1], ops[lim[1]]["writes"][:2]))
                if o["e"] == "act" and o["tbl"] is not None:
                    etbl[0] = o["tbl"]
                efree[o["e"]] = st + o["dur"]
                finish[i] = st + o["dur"] + o["lat"]
                done[i] = True
                ready.remove(i)
                order.append(i)
                for j in succs[i]:
                    indeg[j] -= 1
                    if indeg[j] == 0:
                        ready.append(j)
        if self.REORDER and os.environ.get("MK_STATS"):
            busy = {e: 0.0 for e in self.E}
            for o in ops:
                busy[o["e"]] += o["dur"]
            print("phase: n=%d est_makespan=%.0fus busy(us): %s" % (n, max(finish) / 1e3, {e: int(v / 1e3) for e, v in busy.items()}))
        for i in order:
            self._emit(ops[i])

    def _wait(self, e, tok, same_ok=False):
        if tok is None:
            return
        name, sem, val, src = tok
        if same_ok and src == e:
            return
        d = self.seen[e]
        if d.get(name, 0) >= val:
            return
        self.E[e].wait_ge(sem, val)
        d[name] = val

    def _emit(self, o):
        e, reads, writes = o["e"], o["reads"], o["writes"]
        for k in reads:
            self._wait(e, self.lastw.get(k))
        for k in writes:
            self._wait(e, self.lastw.get(k), same_ok=True)
            for t in self.readers.get(k, {}).values():
                self._wait(e, t, same_ok=True)
        ins = None
        for (name, args, kw) in o["calls"]:
            ins = getattr(self.E[e], name)(*args, **kw)
        if o["kind"] == "dma":
            key = o["key"]
            if key not in self.dsem:
                self.dsem[key] = self.es.enter_context(self.nc.semaphore("d_" + key))
                self.dcnt[key] = 0
            self.dcnt[key] += 16
            ins.then_inc(self.dsem[key], 16)
            tok = ("d_" + key, self.dsem[key], self.dcnt[key], "dma")
        else:
            self.cnt[e] += 1
            ins.then_inc(self.sem[e], 1)
            tok = ("c_" + e, self.sem[e], self.cnt[e], e)
        for k in reads:
            self.readers.setdefault(k, {})[tok[0]] = tok
        for k in writes:
            self.lastw[k] = tok
            self.readers[k] = {}

    def barrier(self):
        self.flush()
        for e in self.E:
            for e2 in self.E:
                if self.cnt[e2] > 0:
                    self._wait(e, ("c_" + e2, self.sem[e2], self.cnt[e2], e2))
            for k, sem in self.dsem.items():
                self._wait(e, ("d_" + k, sem, self.dcnt[k], "dma"))


def build_program():
    nc = bass.Bass("TRN2", target_bir_lowering=False)

    def din(name, shape):
        return nc.dram_tensor(name, list(shape), F32, kind="ExternalInput").ap()

    xp = din("xp", [8196, 1024])
    ctxp = din("ctxp", [260, 1024])
    cvec = din("cvec", [128, 8, 2])
    w_ada = din("w_ada", [1024, 6144])
    b_ada_fm = din("b_ada_fm", [128, 48])
    b_ada_gt = din("b_ada_gt", [128, 2, 1024])
    w_in = din("w_in", [1024, 6144])
    b_in_fm = din("b_in_fm", [128, 48])
    cw = din("cw", [128, 8, 31])
    cb = din("cb", [128, 8])
    lng = din("lng", [128, 8])
    lnb = din("lnb", [128, 8])
    w_pa = din("w_pa", [1024, 1024])
    w_pb = din("w_pb", [1024, 1024])
    w_o = din("w_o", [1024, 1024])
    lw5 = din("lw5", [128, 8, 5])
    lb = din("lb", [128, 8])
    wg = din("wg", [4, 8, 128, 128])
    bg = din("bg", [128, 4, 8])
    lam = din("lam", [128, 2, 8])
    gmix = din("gmix", [128, 8])
    gffn = din("gffn", [128, 8])
    gfin = din("gfin", [128, 1024])
    w_rt = din("w_rt", [1024, 20])
    b_rt = din("b_rt", [128, 20])
    w_gate = din("w_gate", [2048, 4096])
    w_up = din("w_up", [2048, 4096])
    w_down = din("w_down", [2048, 4096])
    ident = din("ident", [128, 128])
    tri = din("tri", [128, 128])
    cE = din("cE", [128, 4])
    sidx = din("sidx", [128, 12])
    out = nc.dram_tensor("out", [NOWN, 1024], F32, kind="ExternalOutput").ap()
    if DEBUG:
        hs_scr = nc.dram_tensor("hs_scr", [8, 128, 8, NB], BF16, kind="ExternalOutput").ap()
        x1_scr = nc.dram_tensor("x1_scr", [NOWN, 1024], F32, kind="ExternalOutput").ap()
    else:
        hs_scr = nc.dram_tensor("hs_scr", [8, 128, 8, NB], BF16, kind="Internal").ap()
        x1_scr = nc.dram_tensor("x1_scr", [NOWN, 1024], F32, kind="Internal").ap()
    gt2_scr = nc.dram_tensor("gt2_scr", [128, 1024], F32, kind="Internal").ap()
    xc_scr = nc.dram_tensor("xc_scr", [8, 128, 8, NB], BF16, kind="Internal").ap()
    NSEG = 12
    xs_sorted = nc.dram_tensor("xs_sorted", [NSEG * NB, 1024], BF16, kind="Internal").ap()
    ws_sorted = nc.dram_tensor("ws_sorted", [NSEG * NB, 4], F32, kind="Internal").ap()
    y_sorted = nc.dram_tensor("y_sorted", [NSEG * NB, 1024], F32, kind="Internal").ap()
    I32 = mybir.dt.int32

    with ExitStack() as es:
        S = Sched(nc, es)

        def sb(name, shape, dt=F32, stack=es):
            return stack.enter_context(nc.sbuf_tensor(name, list(shape), dt))

        psA = es.enter_context(nc.psum_tensor("psA", [128, 2048], F32))
        psB = es.enter_context(nc.psum_tensor("psB", [128, 2048], F32))

        def bank(i):
            t = psA if i < 4 else psB
            return t[:, 512 * (i % 4):512 * (i % 4) + 512]

        def pk(i):
            return "ps%d" % i

        tpv = psA[:, :].bitcast(BF16).rearrange("p (c t) -> p c t", t=512)
        TPK = ("ps0", "ps1", "ps2", "ps3")

        identb = sb("identb", [128, 128], BF16)
        ones_m = sb("ones_m", [128, 128], BF16)
        ones1 = sb("ones1", [128, 128], BF16)
        b_in_sb = sb("b_in_sb", [128, 48])
        hb_in = sb("hb_in", [128, 48])
        cw_sb = sb("cw_sb", [128, 8, 31])
        cb_sb = sb("cb_sb", [128, 8])
        lng_sb = sb("lng_sb", [128, 8])
        lnb_sb = sb("lnb_sb", [128, 8])
        lw5_sb = sb("lw5_sb", [128, 8, 5])
        lb_sb = sb("lb_sb", [128, 8])
        bg_sb = sb("bg_sb", [128, 4, 8])
        hbg = sb("hbg", [128, 4, 8])
        lam_sb = sb("lam_sb", [128, 2, 8])
        gmix_sb = sb("gmix_sb", [128, 8])
        gffn_sb = sb("gffn_sb", [128, 8])
        b_rt_sb = sb("b_rt_sb", [128, 20])
        b_ada_fm_sb = sb("b_ada_fm_sb", [128, 48])
        cvec_sb = sb("cvec_sb", [128, 8, 2])
        mods = sb("mods", [128, 48, 2])
        s1 = sb("s1", [128, 8])
        s1c = sb("s1c", [128, 8])
        s2 = sb("s2", [128, 8])
        gt1h = sb("gt1h", [128, 1024])
        cl = sb("cl", [128, 2, 8])
        hcl = sb("hcl", [128, 2, 8])
        state = sb("state", [128, 2, 8])
        ss = sb("ss", [128, 8])
        rs = sb("rs", [128, 8])
        w_rt_b = sb("w_rt_b", [128, 8, 20], BF16)
        qtr = sb("qtr", [128, 4], F32)

        def pload(t, src):
            S.dma("sp", t, src, writes=("params",), key="params")

        pload(b_in_sb[:], b_in_fm)
        pload(cw_sb[:], cw)
        pload(cb_sb[:], cb)
        pload(lng_sb[:], lng)
        pload(lnb_sb[:], lnb)
        pload(lw5_sb[:], lw5)
        pload(lb_sb[:], lb)
        pload(bg_sb[:], bg)
        pload(lam_sb[:], lam)
        pload(gmix_sb[:], gmix)
        pload(gffn_sb[:], gffn)
        pload(b_rt_sb[:], b_rt)
        pload(b_ada_fm_sb[:], b_ada_fm)
        pload(cvec_sb[:], cvec)
        S.dma("pool", identb[:], ident, writes=("identb",), key="identb")
        S.dma("pool", w_rt_b[:], w_rt.rearrange("(k p) n -> p k n", p=128), writes=("w_rt_b",), key="w_rt_b")
        S.op("pool", lambda e: e.memset(ones_m[:], 1.0 / 1024.0), writes=("ones_m",))
        S.op("pool", lambda e: e.memset(ones1[:], 1.0), writes=("ones1",))
        S.op("pool", lambda e: e.memset(qtr[:, 0:1], 0.25), writes=("qtr",))
        S.op("pool", lambda e: e.memset(qtr[:, 1:2], 1024.0 * EPS), writes=("qtr",))
        S.op("pool", lambda e: e.memset(qtr[:, 2:3], EPS), writes=("qtr",))
        S.op("pool", lambda e: e.memset(state[:], 0.0), writes=("state",))
        S.op("pool", lambda e: e.memset(ss[:], 0.0), writes=("ss",))

        with ExitStack() as p0:
            cs = sb("cs", [128, 8, 2], BF16, p0)
            cs_rep = sb("cs_rep", [128, 8, 128], BF16, p0)
            b_ada_gt_sb = sb("b_ada_gt_sb", [128, 2, 1024], F32, p0)
            wa = [sb("wa%d" % i, [128, 8, 512], BF16, p0) for i in range(3)]
            e_t = sb("e_t", [128, 16], F32, p0)
            t_t = sb("t_t", [128, 16], F32, p0)
            l_t = sb("l_t", [128, 16], F32, p0)
            m_t = sb("m_t", [128, 16], F32, p0)
            pload(b_ada_gt_sb[:], b_ada_gt)
            gt2b = sb("gt2b0", [128, 1024], F32, p0)

            S.op("act", lambda e: e.activation(out=cs[:], in_=cvec_sb[:], func=AF.Silu), reads=("params",), writes=("cs",))
            S.op("dve", lambda e: e.tensor_copy(out=cs_rep[:], in_=cs[:, :, 0:1].to_broadcast([128, 8, 128])),
                 reads=("cs",), writes=("cs_rep",))
            psm = bank(0)[:, 0:96].rearrange("p (j t) -> p j t", t=2)
            for q in range(12):
                s = q % 3
                S.dma("pool", wa[s][:], w_ada[:, 512 * q:512 * q + 512].rearrange("(k p) n -> p k n", p=128),
                      writes=("wa%d" % s,), key="wa%d" % s)
                fns = []
                for jj in range(4):
                    for k in range(8):
                        fns.append(lambda e, jj=jj, k=k, s=s, q=q: e.matmul(
                            psm[:, 4 * q + jj, :], lhsT=wa[s][:, k, 128 * jj:128 * jj + 128], rhs=cs[:, k, :],
                            start=(k == 0), stop=(k == 7)))
                S.group("pe", fns, reads=("wa%d" % s, "cs"), writes=("ps0",))
                if q in (4, 5, 10, 11):
                    bk = 1 + (q % 2)
                    S.group("pe", [lambda e, k=k, s=s, bk=bk: e.matmul(bank(bk), lhsT=cs_rep[:, k, :], rhs=wa[s][:, k, :],
                                                                      start=(k == 0), stop=(k == 7)) for k in range(8)],
                            reads=("wa%d" % s, "cs_rep"), writes=(pk(bk),))
                    dst = gt1h if q < 6 else gt2b
                    gi = 0 if q < 6 else 1
                    cols = slice(512 * (q % 2), 512 * (q % 2) + 512)
                    S.op("dve", lambda e, dst=dst, gi=gi, cols=cols, bk=bk: e.tensor_tensor(
                        out=dst[:, cols], in0=bank(bk), in1=b_ada_gt_sb[:, gi, cols], op=ALU.add),
                        reads=(pk(bk), "params"), writes=("gt",))
            S.op("dve", lambda e: e.tensor_scalar_mul(out=gt1h[:], in0=gt1h[:], scalar1=0.5), reads=("gt",), writes=("gt",))
            S.dma("sp", gt2_scr, gt2b[:], reads=("gt",), writes=("gt2_scr",), key="gt2_scr")
            S.op("dve", lambda e: e.tensor_tensor(out=mods[:], in0=psm, in1=b_ada_fm_sb[:].unsqueeze(2).to_broadcast([128, 48, 2]),
                                                  op=ALU.add), reads=("ps0", "params"), writes=("mods",))
            for (dst, col, j0, gsb) in ((s1, 0, 8, gmix_sb), (s1c, 1, 8, gmix_sb), (s2, 0, 32, gffn_sb)):
                S.op("dve", lambda e, dst=dst, col=col, j0=j0, gsb=gsb: e.scalar_tensor_tensor(
                    out=dst[:], in0=mods[:, j0:j0 + 8, col], scalar=1.0, in1=gsb[:], op0=ALU.add, op1=ALU.mult),
                    reads=("mods", "params"), writes=("sc",))
                S.op("dve", lambda e, dst=dst: e.tensor_scalar_mul(out=dst[:], in0=dst[:], scalar1=32.0),
                     reads=("sc",), writes=("sc",))
            S.op("dve", lambda e: e.tensor_scalar_mul(out=hb_in[:], in0=b_in_sb[:], scalar1=0.5), reads=("params",), writes=("hb_in",))
            S.op("dve", lambda e: e.tensor_scalar_mul(out=hbg[:], in0=bg_sb[:], scalar1=0.5), reads=("params",), writes=("hbg",))
            lamf = lam_sb[:].rearrange("p a b -> p (a b)")
            S.op("act", lambda e: e.activation(out=e_t[:], in_=lamf, func=AF.Exp, scale=-1.0), reads=("params",), writes=("e_t",))
            S.op("dve", lambda e: e.tensor_scalar(out=t_t[:], in0=e_t[:], scalar1=-0.25, scalar2=1.0 / 3.0, op0=ALU.mult, op1=ALU.add),
                 reads=("e_t",), writes=("t_t",))
            S.op("dve", lambda e: e.tensor_tensor(out=t_t[:], in0=t_t[:], in1=e_t[:], op=ALU.mult), reads=("t_t", "e_t"), writes=("t_t",))
            S.op("dve", lambda e: e.tensor_scalar_add(out=t_t[:], in0=t_t[:], scalar1=-0.5), reads=("t_t",), writes=("t_t",))
            S.op("dve", lambda e: e.tensor_tensor(out=t_t[:], in0=t_t[:], in1=e_t[:], op=ALU.mult), reads=("t_t", "e_t"), writes=("t_t",))
            S.op("dve", lambda e: e.tensor_scalar_add(out=t_t[:], in0=t_t[:], scalar1=1.0), reads=("t_t",), writes=("t_t",))
            S.op("dve", lambda e: e.tensor_tensor(out=t_t[:], in0=t_t[:], in1=e_t[:], op=ALU.mult), reads=("t_t", "e_t"), writes=("t_t",))
            S.op("dve", lambda e: e.tensor_scalar_add(out=l_t[:], in0=e_t[:], scalar1=1.0), reads=("e_t",), writes=("l_t",))
            S.op("act", lambda e: e.activation(out=l_t[:], in_=l_t[:], func=AF.Ln), reads=("l_t",), writes=("l_t",))
            S.op("dve", lambda e: e.tensor_single_scalar(out=m_t[:], in_=e_t[:], scalar=0.1, op=ALU.is_lt), reads=("e_t",), writes=("m_t",))
            S.op("dve", lambda e: e.tensor_tensor(out=t_t[:], in0=t_t[:], in1=l_t[:], op=ALU.subtract), reads=("t_t", "l_t"), writes=("t_t",))
            S.op("dve", lambda e: e.tensor_tensor(out=t_t[:], in0=t_t[:], in1=m_t[:], op=ALU.mult), reads=("t_t", "m_t"), writes=("t_t",))
            S.op("dve", lambda e: e.tensor_tensor(out=t_t[:], in0=t_t[:], in1=l_t[:], op=ALU.add), reads=("t_t", "l_t"), writes=("t_t",))
            clf = cl[:].rearrange("p a b -> p (a b)")
            hclf = hcl[:].rearrange("p a b -> p (a b)")
            S.op("dve", lambda e: e.tensor_scalar_mul(out=clf, in0=t_t[:], scalar1=-8.0), reads=("t_t",), writes=("cl",))
            S.op("dve", lambda e: e.tensor_scalar_mul(out=hclf, in0=t_t[:], scalar1=-4.0), reads=("t_t",), writes=("cl",))
            S.barrier()

        mixer = ExitStack()
        wxr = sb("wxr", [128, 8, 1024], BF16, mixer)
        wgb = sb("wgb", [128, 4, 8, 128], BF16, mixer)
        dg5 = sb("dg5", [128, 8, 5, 128], BF16, mixer)
        S.dma("pool", wxr[:], w_in[:, 3072:4096].rearrange("(k p) n -> p k n", p=128), writes=("wxr",), key="wxr")
        S.dma("pool", wgb[:], wg.rearrange("g h p n -> p g h n"), writes=("wgb",), key="wgb")
        for c in range(8):
            S.op("dve", lambda e, c=c: e.tensor_tensor(
                out=dg5[:, c, :, :], in0=identb[:].unsqueeze(1).to_broadcast([128, 5, 128]),
                in1=lw5_sb[:, c, :].unsqueeze(2).to_broadcast([128, 5, 128]), op=ALU.mult),
                reads=("identb", "params"), writes=("dg5",))

        xt = sb("xt", [128, 4, 1024], F32, mixer)
        xh = sb("xh", [4, 1024], F32, mixer)
        xn = sb("xn", [128, 4, 1024], BF16, mixer)
        xnh = sb("xnh", [4, 1024], BF16, mixer)
        hxTs = [sb("hxT%d" % i, [128, 8, NB + 4], BF16, mixer) for i in range(2)]
        hxT, hxk, hxhk = hxTs[0], "hxT0", "hxTh0"
        hbc = [0]
        xrp = [sb("xrp%d" % i, [128, NB + 4], BF16, mixer) for i in range(2)]
        xcb = [sb("xcb%d" % i, [128, NB], BF16, mixer) for i in range(2)]
        tr = sb("tr", [128, NB], F32, mixer)
        ti = sb("ti", [128, NB], F32, mixer)
        a4 = sb("a4", [128, 4, NB], F32, mixer)
        s4 = sb("s4", [128, 4, NB], F32, mixer)
        t4 = sb("t4", [128, 4, NB], BF16, mixer)
        tmp1 = sb("tmp1", [128, NB], F32, mixer)
        bb_t = sb("bb_t", [128, NB], F32, mixer)
        hf = sb("hf", [128, NB], F32, mixer)
        hsb = sb("hsb", [128, 8, NB], BF16, mixer)

        tph = bank(4).bitcast(BF16)[:, 0:32].rearrange("p (c t) -> p c t", t=4)

        def prep(xsrc, r0, N, sc, bcol, keep_key, halo=True):
            nt = N // 128
            S.dma("sp", xt[:, 0:nt, :], xsrc[r0:r0 + N, :].rearrange("(j p) d -> p j d", p=128), writes=(keep_key,), key="xt")
            if halo:
                S.dma("sp", xh[0:2, :], xsrc[r0 - 2:r0, :], writes=("xh",), key="xh")
                S.dma("sp", xh[2:4, :], xsrc[r0 + N:r0 + N + 2, :], writes=("xh",), key="xh")
            S.op("pool", lambda e: e.memset(ss[:], 0.0), writes=("ss",))
            for j in range(nt):
                S.op("act", lambda e, j=j: e.activation(out=xn[:, j, :], in_=xt[:, j, :], func=AF.Square, accum_out=ss[:, j:j + 1]),
                     reads=(keep_key,), writes=("xn", "ss"))
            if halo:
                S.op("act", lambda e: e.activation(out=xnh[:], in_=xh[:], func=AF.Square, accum_out=ss[0:4, 4:5]),
                     reads=("xh",), writes=("xnh", "ss"))
            S.op("act", lambda e: e.activation(out=rs[:, 0:5], in_=ss[:, 0:5], func=AF.Sqrt, bias=qtr[:, 1:2]), reads=("ss", "qtr"), writes=("rs",))
            S.op("dve", lambda e: e.reciprocal(out=rs[:, 0:5], in_=rs[:, 0:5]), reads=("rs",), writes=("rs",))
            for j in range(nt):
                S.op("dve", lambda e, j=j: e.tensor_scalar_mul(out=xn[:, j, :], in0=xt[:, j, :], scalar1=rs[:, j:j + 1]),
                     reads=(keep_key, "rs"), writes=("xn",))
            if halo:
                S.op("dve", lambda e: e.tensor_scalar_mul(out=xnh[:], in0=xh[:], scalar1=rs[0:4, 4:5]),
                     reads=("xh", "rs"), writes=("xnh",))
            for j in range(nt):
                S.group("pe", [lambda e, j=j, c=c: e.transpose(out=tpv[:, c, 128 * j:128 * j + 128],
                                                               in_=xn[:, j, 128 * c:128 * c + 128], identity=identb[:])
                               for c in range(8)], reads=("xn", "identb"), writes=TPK)
            if halo:
                S.group("pe", [lambda e, c=c: e.transpose(out=tph[:, c, :], in_=xnh[:, 128 * c:128 * c + 128], identity=identb[0:4, 0:4])
                               for c in range(8)], reads=("xnh", "identb"), writes=("ps4",))
            for c in range(8):
                S.op("act", lambda e, c=c: e.activation(out=hxT[:, c, 0:N], in_=tpv[:, c, 0:N], func=AF.Identity,
                                                        scale=sc[:, c:c + 1], bias=mods[:, c, bcol:bcol + 1]),
                     reads=TPK + ("sc", "mods"), writes=(hxk,))
            if halo:
                S.op("dve", lambda e: e.tensor_tensor(out=hxT[:, :, N:N + 4], in0=tph, in1=sc[:].unsqueeze(2).to_broadcast([128, 8, 4]),
                                                      op=ALU.mult), reads=("ps4", "sc"), writes=(hxhk,))
                S.op("dve", lambda e: e.tensor_tensor(out=hxT[:, :, N:N + 4], in0=hxT[:, :, N:N + 4],
                                                      in1=mods[:, 0:8, bcol:bcol + 1].to_broadcast([128, 8, 4]), op=ALU.add),
                     reads=(hxhk, "mods"), writes=(hxhk,))

        xr_evac_eng = ["dve"]

        def rglru_block(N, d, reverse, has_lo, has_hi, consumer=None, xc_store=None, xc_src=None):
            def st1(c):
                par = c % 2
                bxr = bank(par)[:, 0:N]
                bxh = bank(2 + par)[:, 0:4]
                S.group("pe", [lambda e, k=k: e.matmul(bxr, lhsT=wxr[:, k, 128 * c:128 * c + 128], rhs=hxT[:, k, 0:N],
                                                       start=(k == 0), stop=(k == 7)) for k in range(8)],
                        reads=(hxk, "wxr"), writes=(pk(par),))
                S.group("pe", [lambda e, k=k: e.matmul(bxh, lhsT=wxr[:, k, 128 * c:128 * c + 128], rhs=hxT[:, k, N:N + 4],
                                                       start=(k == 0), stop=(k == 7)) for k in range(8)],
                        reads=(hxhk, "wxr"), writes=(pk(2 + par),))
                xk = "xrp%d" % par
                bia = b_in_sb[:, 24 + c:25 + c]
                if xr_evac_eng[0] == "act":
                    S.op("act", lambda e: e.activation(out=xrp[par][:, 2:2 + N], in_=bxr, func=AF.Identity, bias=bia),
                         reads=(pk(par), "params"), writes=(xk,))
                else:
                    S.op("dve", lambda e: e.tensor_scalar_add(out=xrp[par][:, 2:2 + N], in0=bxr, scalar1=bia),
                         reads=(pk(par), "params"), writes=(xk,))
                if has_lo:
                    S.op("dve", lambda e: e.tensor_scalar_add(out=xrp[par][:, 0:2], in0=bxh[:, 0:2], scalar1=bia),
                         reads=(pk(2 + par), "params"), writes=(xk,))
                else:
                    S.op("dve", lambda e: e.memset(xrp[par][:, 0:2], 0.0), writes=(xk,))
                if has_hi:
                    S.op("dve", lambda e: e.tensor_scalar_add(out=xrp[par][:, 2 + N:4 + N], in0=bxh[:, 2:4], scalar1=bia),
                         reads=(pk(2 + par), "params"), writes=(xk,))
                else:
                    S.op("dve", lambda e: e.memset(xrp[par][:, 2 + N:4 + N], 0.0), writes=(xk,))

            def st2(c):
                par = c % 2
                xk = "xrp%d" % par
                bcv = bank(4 + par)[:, 0:N]
                S.group("pe", [lambda e, j=j: e.matmul(bcv, lhsT=dg5[:, c, j, :], rhs=xrp[par][:, j:j + N],
                                                       start=(j == 0), stop=(j == 4)) for j in range(5)],
                        reads=(xk, "dg5"), writes=(pk(4 + par),))
                S.op("dve", lambda e: e.tensor_scalar_add(out=xcb[par][:, 0:N], in0=bcv, scalar1=lb_sb[:, c:c + 1]),
                     reads=(pk(4 + par), "params"), writes=("xcb%d" % par,))
                if xc_store is not None:
                    S.dma("sp", xc_scr[xc_store, :, c, :], xcb[par][:, 0:N], reads=("xcb%d" % par,), writes=("xc_scr%d" % xc_store,), key="xc_scr")

            def st3(c):
                par = c % 2
                q = c % 4
                ck = "xcb%d" % par
                xc_ap = xcb[par][:, 0:N]
                if xc_src is not None:
                    ck = "dg5"
                    xc_ap = xc_src[:, c, :]
                br_ = bank(6)[:, 0:N]
                bi_ = bank(7)[:, 0:N]
                S.group("pe", [lambda e: e.matmul(br_, lhsT=wgb[:, 2 * d, c, :], rhs=xc_ap, start=True, stop=True)],
                        reads=(ck, "wgb"), writes=("ps6",))
                S.group("pe", [lambda e: e.matmul(bi_, lhsT=wgb[:, 2 * d + 1, c, :], rhs=xc_ap, start=True, stop=True)],
                        reads=(ck, "wgb"), writes=("ps7",))
                S.op("act", lambda e: e.activation(out=tr[:, 0:N], in_=br_, func=AF.Tanh, scale=0.5, bias=hbg[:, 2 * d, c:c + 1]),
                     reads=("ps6", "hbg"), writes=("tr",))
                S.op("act", lambda e: e.activation(out=ti[:, 0:N], in_=bi_, func=AF.Tanh, scale=0.5, bias=hbg[:, 2 * d + 1, c:c + 1]),
                     reads=("ps7", "hbg"), writes=("ti",))
                S.op("act", lambda e: e.activation(out=a4[:, q, 0:N], in_=tr[:, 0:N], func=AF.Exp, scale=hcl[:, d, c:c + 1],
                                                   bias=hcl[:, d, c:c + 1]), reads=("tr", "cl"), writes=("a4_%d" % q,))
                S.op("pool", lambda e: e.tensor_tensor(out=s4[:, q, 0:N], in0=a4[:, q, 0:N], in1=a4[:, q, 0:N], op=ALU.mult),
                     reads=("a4_%d" % q,), writes=("s4_%d" % q,))
                S.op("dve", lambda e: e.scalar_tensor_tensor(out=t4[:, q, 0:N], in0=ti[:, 0:N], scalar=1.0, in1=xc_ap,
                                                             op0=ALU.add, op1=ALU.mult), reads=("ti", ck), writes=("t4_%d" % q,))

            def st4(c0):
                sk = tuple("s4_%d" % q for q in range(4))
                S.op("act", lambda e: e.activation(out=s4[:, :, 0:N], in_=s4[:, :, 0:N], func=AF.Sqrt, scale=-0.25, bias=qtr[:, 0:1]),
                     reads=sk + ("qtr",), writes=sk)
                for c in range(c0, c0 + 4):
                    q = c % 4
                    S.op("pool", lambda e, q=q: e.tensor_tensor(out=bb_t[:, 0:N], in0=s4[:, q, 0:N], in1=t4[:, q, 0:N], op=ALU.mult),
                         reads=("s4_%d" % q, "t4_%d" % q), writes=("bb_t",))
                    if reverse:
                        S.op("dve", lambda e, q=q, c=c: e.tensor_tensor_scan(
                            out=hf[:, 0:N][:, ::-1], data0=a4[:, q, 0:N][:, ::-1], data1=bb_t[:, 0:N][:, ::-1],
                            initial=state[:, d, c:c + 1], op0=ALU.mult, op1=ALU.add),
                            reads=("a4_%d" % q, "bb_t", "state"), writes=("hf",))
                        S.op("pool", lambda e, c=c: e.tensor_copy(out=state[:, d, c:c + 1], in_=hf[:, 0:1]),
                             reads=("hf",), writes=("state",))
                    else:
                        S.op("dve", lambda e, q=q, c=c: e.tensor_tensor_scan(
                            out=hf[:, 0:N], data0=a4[:, q, 0:N], data1=bb_t[:, 0:N], initial=state[:, d, c:c + 1],
                            op0=ALU.mult, op1=ALU.add), reads=("a4_%d" % q, "bb_t", "state"), writes=("hf",))
                        S.op("pool", lambda e, c=c: e.tensor_copy(out=state[:, d, c:c + 1], in_=hf[:, N - 1:N]),
                             reads=("hf",), writes=("state",))
                    if consumer is not None:
                        consumer(c)

            for s_ in range(10):
                if s_ < 8 and xc_src is None:
                    st1(s_)
                if 1 <= s_ <= 8 and xc_src is None:
                    st2(s_ - 1)
                if 2 <= s_ <= 9:
                    st3(s_ - 2)
                    if (s_ - 2) % 4 == 3:
                        st4(s_ - 2 - 3)

        hxT, hxk, hxhk = hxTs[0], "hxT0", "hxTh0"
        prep(ctxp, 2, 256, s1c, 1, "xt")
        hbc[0] = 1
        rglru_block(256, 0, False, False, False)
        rglru_block(256, 1, True, False, False)
        for blk in range(15, -1, -1):
            _i = hbc[0] % 2
            hbc[0] += 1
            hxT, hxk, hxhk = hxTs[_i], "hxT%d" % _i, "hxTh%d" % _i
            prep(xp, 2 + NB * blk, NB, s1, 0, "xt")
            def cons_a(c):
                S.op("dve", lambda e, c=c: e.tensor_copy(out=hsb[:, c, :], in_=hf[:]), reads=("hf",), writes=("hsb",))
            rglru_block(NB, 1, True, blk != 0, blk != 15, cons_a if blk < 8 else None, xc_store=(blk if blk < 8 else None))
            if blk < 8:
                S.dma("sp", hs_scr[blk], hsb[:], reads=("hsb",), writes=("hs_scr%d" % blk,), key="hs_scr")

        pb_ = ExitStack()
        cwh = cw_sb
        S.op("dve", lambda e: e.tensor_scalar_mul(out=cwh[:], in0=cw_sb[:], scalar1=0.5), reads=("params",), writes=("cwh",))
        lnr = sb("lnr", [128, NB], F32, pb_)
        lmr = sb("lmr", [128, NB], F32, pb_)
        wsl = [sb("wsl%d" % i, [128, 8, 512], BF16, pb_) for i in range(3)]
        dgc = [sb("dgc0", [128, 31, 128], BF16, pb_)] * 2
        tv, uu = tr, ti
        zb = [sb("zb0", [128, NB], BF16, pb_)] * 2
        zc = sb("zc", [128, 8, NB], BF16, pb_)
        aa = zc
        zsq = [sb("zsq0", [128, NB], BF16, pb_)] * 2
        A_t = sb("A_t", [128, 8, NB], BF16, pb_)
        gy = sb("gy", [128, 8, NB], BF16, pb_)
        mg = gy
        yb = sb("yb", [128, 8, NB], BF16, pb_)
        hs_in = hsb
        xc_in = dg5[:].rearrange("p c j m -> p (c j m)")[:, 0:8 * NB].rearrange("p (c t) -> p c t", t=NB)
        x1 = xt

        xres = [sb("xres%d" % i, [128, 512], F32, pb_) for i in range(3)]
        xrc = [0]
        wctr = [0]

        def wpiece(col0, src=None):
            src = w_in if src is None else src
            i = wctr[0] % 3
            wctr[0] += 1
            S.dma("pool", wsl[i][:], src[:, col0:col0 + 512].rearrange("(k p) n -> p k n", p=128),
                  writes=("wsl%d" % i,), key="wsl%d" % i)
            return wsl[i], "wsl%d" % i

        def inproj(dstbank, wt, wk, j4):
            S.group("pe", [lambda e, k=k: e.matmul(bank(dstbank), lhsT=wt[:, k, 128 * j4:128 * j4 + 128], rhs=hxT[:, k, 0:NB],
                                                   start=(k == 0), stop=(k == 7)) for k in range(8)],
                    reads=(hxk, wk), writes=(pk(dstbank),))

        xr_evac_eng[0] = "act"
        for blk in range(8):
            _i = hbc[0] % 2
            hbc[0] += 1
            hxT, hxk, hxhk = hxTs[_i], "hxT%d" % _i, "hxTh%d" % _i
            prep(xp, 2 + NB * blk, NB, s1, 0, "xt", halo=False)
            S.dma("sp", xc_in[:], xc_scr[blk], reads=("xc_scr%d" % blk,), writes=("dg5",), key="xc_in")
            S.dma("sp", hs_in[:], hs_scr[blk], reads=("hs_scr%d" % blk,), writes=("hsb",), key="hs_in")
            for half in range(2):
                wu, wuk = wpiece(512 * half)
                wv, wvk = wpiece(1024 + 512 * half)
                for c4 in range(4):
                    c = 4 * half + c4
                    par = c % 2
                    inproj(par, wu, wuk, c4)
                    inproj(2 + par, wv, wvk, c4)
                    S.op("act", lambda e, c=c, par=par: e.activation(out=tv[:], in_=bank(2 + par), func=AF.Tanh, scale=0.5,
                                                                     bias=hb_in[:, 8 + c:9 + c]),
                         reads=(pk(2 + par), "hb_in"), writes=("tr",))
                    S.op("act", lambda e, c=c, par=par: e.activation(out=uu[:], in_=bank(par), func=AF.Identity,
                                                                     bias=b_in_sb[:, c:c + 1]),
                         reads=(pk(par), "params"), writes=("ti",))
                    zk = "zb0"
                    S.op("dve", lambda e, par=par: e.scalar_tensor_tensor(out=zb[par][:], in0=tv[:], scalar=1.0, in1=uu[:],
                                                                          op0=ALU.add, op1=ALU.mult),
                         reads=("tr", "ti"), writes=(zk,))
                    dk = "dgc0"
                    S.op("dve", lambda e, c=c, par=par: e.tensor_tensor(
                        out=dgc[par][:], in0=identb[:].unsqueeze(1).to_broadcast([128, 31, 128]),
                        in1=cwh[:, c, :].unsqueeze(2).to_broadcast([128, 31, 128]), op=ALU.mult),
                        reads=("identb", "cwh"), writes=(dk,))
                    zv = zb[par][:].rearrange("p (r t) -> p r t", t=64)
                    pcv = bank(4 + par).rearrange("p (r t) -> p r t", t=64)
                    fns = []
                    order = [15] + [k for k in range(31) if k != 15]
                    for idx, k in enumerate(order):
                        o = k - 15
                        t0, t1 = max(0, -o), 64 - max(0, o)
                        fns.append(lambda e, k=k, o=o, t0=t0, t1=t1, idx=idx, par=par, pcv=pcv, zv=zv: e.matmul(
                            pcv[:, :, t0:t1], lhsT=dgc[par][:, k, :], rhs=zv[:, :, t0 + o:t1 + o],
                            start=(idx == 0), stop=(idx == 30)))
                    S.group("pe", fns, reads=(zk, dk), writes=(pk(4 + par),))
                    S.op("act", lambda e, c=c, par=par: e.activation(out=zc[:, c, :], in_=bank(4 + par), func=AF.Identity,
                                                                     bias=cb_sb[:, c:c + 1]),
                         reads=(pk(4 + par), "params"), writes=("zc",))
                    qk = "zsq0"
                    S.op("act", lambda e, c=c, par=par: e.activation(out=zsq[par][:], in_=bank(4 + par), func=AF.Square,
                                                                     bias=cb_sb[:, c:c + 1]),
                         reads=(pk(4 + par), "params"), writes=(qk,))
                    S.group("pe", [lambda e, c=c: e.matmul(bank(6), lhsT=ones_m[:], rhs=zc[:, c, :], start=(c == 0), stop=(c == 7))],
                            reads=("zc", "ones_m"), writes=("ps6",))
                    S.group("pe", [lambda e, c=c, par=par: e.matmul(bank(7), lhsT=ones_m[:], rhs=zsq[par][:], start=(c == 0),
                                                                    stop=(c == 7))], reads=(qk, "ones_m"), writes=("ps7",))
            S.op("act", lambda e: e.activation(out=tv[:], in_=bank(6), func=AF.Copy), reads=("ps6",), writes=("tr",))
            S.op("dve", lambda e: e.tensor_tensor(out=uu[:], in0=tv[:], in1=tv[:], op=ALU.mult), reads=("tr",), writes=("ti",))
            S.op("dve", lambda e: e.tensor_tensor(out=lnr[:], in0=bank(7), in1=uu[:], op=ALU.subtract), reads=("ps7", "ti"),
                 writes=("lnr",))
            S.op("act", lambda e: e.activation(out=lnr[:], in_=lnr[:], func=AF.Sqrt, bias=qtr[:, 2:3]), reads=("lnr", "qtr"), writes=("lnr",))
            S.op("dve", lambda e: e.reciprocal(out=lnr[:], in_=lnr[:]), reads=("lnr",), writes=("lnr",))
            S.op("dve", lambda e: e.tensor_tensor(out=lmr[:], in0=tv[:], in1=lnr[:], op=ALU.mult), reads=("tr", "lnr"),
                 writes=("lmr",))
            for half in range(2):
                wy, wyk = wpiece(2048 + 512 * half)
                for c4 in range(4):
                    c = 4 * half + c4
                    par = c % 2
                    inproj(par, wy, wyk, c4)
                    S.op("act", lambda e, c=c, par=par: e.activation(out=gy[:, c, :], in_=bank(par), func=AF.Gelu_apprx_tanh,
                                                                     bias=b_in_sb[:, 16 + c:17 + c]),
                         reads=(pk(par), "params"), writes=("gy",))
            def cons_b(c):
                S.op("dve", lambda e, c=c: e.tensor_tensor(out=tmp1[:], in0=hf[:], in1=hs_in[:, c, :], op=ALU.add),
                     reads=("hf", "hsb"), writes=("tmp1",))
                S.op("dve", lambda e, c=c: e.tensor_tensor(out=yb[:, c, :], in0=tmp1[:], in1=gy[:, c, :], op=ALU.mult),
                     reads=("tmp1", "gy"), writes=("yb",))
            rglru_block(NB, 0, False, blk != 0, True, cons_b, xc_src=xc_in)
            for c in range(8):
                S.op("dve", lambda e, c=c: e.tensor_tensor(out=tv[:], in0=zc[:, c, :], in1=lnr[:], op=ALU.mult),
                     reads=("zc", "lnr"), writes=("tr",))
                S.op("dve", lambda e: e.tensor_tensor(out=uu[:], in0=tv[:], in1=lmr[:], op=ALU.subtract), reads=("tr", "lmr"),
                     writes=("ti",))
                S.op("act", lambda e, c=c: e.activation(out=aa[:, c, :], in_=uu[:], func=AF.Silu, scale=lng_sb[:, c:c + 1],
                                                        bias=lnb_sb[:, c:c + 1]), reads=("ti", "params"), writes=("zc",))
            for half in range(2):
                wga, wgak = wpiece(4096 + 512 * half)
                wpa, wpak = wpiece(512 * half, w_pa)
                for m4 in range(4):
                    m = 4 * half + m4
                    par = m % 2
                    S.group("pe", [lambda e, k=k, m4=m4, par=par, wpa=wpa: e.matmul(bank(par), lhsT=wpa[:, k, 128 * m4:128 * m4 + 128],
                                                                         rhs=aa[:, k, :], start=(k == 0), stop=(k == 7))
                                   for k in range(8)], reads=("zc", wpak), writes=(pk(par),))
                    inproj(2 + par, wga, wgak, m4)
                    S.op("act", lambda e, m=m, par=par: e.activation(out=tv[:], in_=bank(2 + par), func=AF.Tanh, scale=0.5,
                                                                     bias=hb_in[:, 32 + m:33 + m]),
                         reads=(pk(2 + par), "hb_in"), writes=("tr",))
                    S.op("dve", lambda e, m=m, par=par: e.scalar_tensor_tensor(out=A_t[:, m, :], in0=tv[:], scalar=1.0,
                                                                               in1=bank(par), op0=ALU.add, op1=ALU.mult),
                         reads=("tr", pk(par)), writes=("A_t",))
            for half in range(2):
                wgb_, wgbk = wpiece(5120 + 512 * half)
                wpb, wpbk = wpiece(512 * half, w_pb)
                for m4 in range(4):
                    m = 4 * half + m4
                    par = m % 2
                    S.group("pe", [lambda e, k=k, m4=m4, par=par, wpb=wpb: e.matmul(bank(par), lhsT=wpb[:, k, 128 * m4:128 * m4 + 128],
                                                                         rhs=yb[:, k, :], start=(k == 0), stop=(k == 7))
                                   for k in range(8)], reads=("yb", wpbk), writes=(pk(par),))
                    inproj(2 + par, wgb_, wgbk, m4)
                    S.op("act", lambda e, m=m, par=par: e.activation(out=tv[:], in_=bank(2 + par), func=AF.Tanh, scale=0.5,
                                                                     bias=hb_in[:, 40 + m:41 + m]),
                         reads=(pk(2 + par), "hb_in"), writes=("tr",))
                    S.op("dve", lambda e, par=par: e.scalar_tensor_tensor(out=uu[:], in0=tv[:], scalar=1.0, in1=bank(par),
                                                                          op0=ALU.add, op1=ALU.mult),
                         reads=("tr", pk(par)), writes=("ti",))
                    S.op("dve", lambda e, m=m: e.tensor_tensor(out=mg[:, m, :], in0=uu[:], in1=A_t[:, m, :], op=ALU.add),
                         reads=("ti", "A_t"), writes=("gy",))
            for hh in range(2):
                wo, wok = wpiece(512 * hh, w_o)
                for j in range(4):
                    bk = 4 + j
                    xi = xrc[0] % 3
                    xrc[0] += 1
                    xk_ = "xres%d" % xi
                    r0_ = 2 + NB * blk + 128 * j
                    S.dma("sp", xres[xi][:], xp[r0_:r0_ + 128, 512 * hh:512 * hh + 512], writes=(xk_,), key=xk_)
                    S.group("pe", [lambda e, k=k, j=j, bk=bk, wo=wo: e.matmul(bank(bk), lhsT=mg[:, k, 128 * j:128 * j + 128],
                                                                             rhs=wo[:, k, :], start=(k == 0), stop=(k == 7))
                                   for k in range(8)], reads=("gy", wok), writes=(pk(bk),))
                    S.op("dve", lambda e, hh=hh, bk=bk: e.tensor_tensor(out=tmp1[:], in0=bank(bk), in1=gt1h[:, 512 * hh:512 * hh + 512],
                                                                        op=ALU.mult), reads=(pk(bk), "gt"), writes=("tmp1",))
                    S.op("pool", lambda e, xi=xi: e.tensor_tensor(out=xres[xi][:], in0=xres[xi][:], in1=tmp1[:], op=ALU.add),
                         reads=("tmp1", xk_), writes=(xk_,))
                    S.dma("sp", x1_scr[NB * blk + 128 * j:NB * blk + 128 * j + 128, 512 * hh:512 * hh + 512], xres[xi][:],
                          reads=(xk_,), writes=("x1_scr%d" % blk,), key="x1_scr")
        S.barrier()
        pb_.close()
        mixer.close()

        def bc(ap, shape):
            return ap.to_broadcast(shape)

        pcg = ExitStack()
        gf32 = sb("gf32", [128, 1024], F32, pcg)
        gt2b = sb("gt2b", [128, 1024], F32, pcg)
        S.dma("sp", gf32[:], gfin, writes=("gf32",), key="gf32")
        S.dma("sp", gt2b[:], gt2_scr, reads=("gt2_scr",), writes=("gt2b",), key="gt2b")
        S.op("dve", lambda e: e.tensor_scalar_mul(out=gf32[:], in0=gf32[:], scalar1=32.0), reads=("gf32",), writes=("gf32",))
        slot_i = sb("slot_i", [128, 32], I32, pcg)
        offE_i = sb("offE_i", [128, NSEG, 4], I32, pcg)
        trib = sb("trib", [128, 128], BF16, pcg)
        S.dma("pool", trib[:], tri, writes=("trib",), key="trib")

        c1 = ExitStack()
        x1l = [sb("x1l%d" % i, [128, 4, 1024], F32, c1) for i in range(2)]
        xn2_all = sb("xn2_all", [128, 32, 1024], BF16, c1)
        hmT1 = sb("hmTr", [128, 8, NB], BF16, c1)
        zt = sb("zt", [128, 4096], BF16, c1)
        ztf = sb("ztf", [128, 192], F32, c1)
        ssA = sb("ssA", [128, 8, 4], F32, c1)
        rsA = sb("rsA", [128, 8, 4], F32, c1)
        oh_all = sb("oh_all", [128, 32, 4], F32, c1)
        wsel_all = sb("wsel_all", [128, 32, 4], F32, c1)
        oh_bf = sb("oh_bf", [128, 32, 4], BF16, c1)
        R1s = sb("R1s", [128, 32, 4], F32, c1)
        Cs = sb("Cs", [128, 32, 4], F32, c1)
        incl = sb("incl", [128, 4, 32], F32, c1)
        onesf = sb("onesf", [128, 32], F32, c1)
        ng = sb("ng", [128, 4], F32, c1)
        nseg = sb("nseg", [128, 4], F32, c1)
        sst = sb("sst", [128, 4], F32, c1)
        sen = sb("sen", [128, 4], F32, c1)
        slot_f = sb("slot_f", [128, 32], F32, c1)
        Gs = sb("Gs", [128, NSEG], F32, c1)
        sidx_sb = sb("sidx_sb", [128, NSEG], F32, c1)
        cE_sb = sb("cE_sb", [128, 4], F32, c1)
        offE_f = sb("offE_f", [128, NSEG, 4], F32, c1)
        L = sb("L", [128, 4, 20], F32, c1)
        gmax = sb("gmax", [128, 4, 1], F32, c1)
        eg = sb("eg", [128, 4, 4], F32, c1)
        pg = sb("pg", [128, 4, 1], F32, c1)
        tmp16 = sb("tmp16", [128, 4, 16], F32, c1)
        esel = sb("esel", [128, 4, 4], F32, c1)
        m1 = sb("m1", [128, 4, 1], F32, c1)
        m2 = sb("m2", [128, 4, 1], F32, c1)
        k1 = sb("k1", [128, 4, 4], F32, c1)
        k2 = sb("k2", [128, 4, 4], F32, c1)
        e2 = sb("e2", [128, 4, 4], F32, c1)
        w1 = sb("w1", [128, 4, 1], F32, c1)
        w2 = sb("w2", [128, 4, 1], F32, c1)
        S.dma("sp", sidx_sb[:], sidx, writes=("cidx",), key="cidx")
        S.dma("sp", cE_sb[:], cE, writes=("cidx",), key="cidx")
        S.op("pool", lambda e: e.memset(zt[:], 0.0), writes=("zt",))
        S.op("pool", lambda e: e.memset(ztf[:], 0.0), writes=("zt",))
        S.op("pool", lambda e: e.memset(onesf[:], 1.0), writes=("onesf",))
        for sg_ in range(NSEG):
            S.dma("sp", xs_sorted[NB * sg_:NB * sg_ + NB, :].rearrange("(p r) d -> p (r d)", r=4), zt[:],
                  reads=("zt",), writes=("xs_sorted",), key="xs_z")
        S.dma("sp", ws_sorted.rearrange("(p r) c -> p (r c)", r=48), ztf[:], reads=("zt",), writes=("ws_sorted",), key="xs_z")

        for blk in range(8):
            pb2 = blk % 2
            x1t = x1l[pb2]
            ak = "x1l%d" % pb2
            sak = "ssA%d" % blk
            oh = oh_all[:, 4 * blk:4 * blk + 4, :]
            wsel = wsel_all[:, 4 * blk:4 * blk + 4, :]
            S.dma("sp", x1t[:], x1_scr[NB * blk:NB * blk + NB, :].rearrange("(j p) d -> p j d", p=128),
                  reads=("x1_scr%d" % blk,), writes=(ak,), key=ak)
            S.op("pool", lambda e, blk=blk: e.memset(ssA[:, blk, :], 0.0), writes=(sak,))
            for j in range(4):
                S.op("act", lambda e, j=j, x1t=x1t, blk=blk: e.activation(out=xn2_all[:, 4 * blk + j, :], in_=x1t[:, j, :], func=AF.Square,
                                                                          accum_out=ssA[:, blk, j:j + 1]),
                     reads=(ak,), writes=("xn2_%d" % blk, sak))
            S.op("act", lambda e, blk=blk: e.activation(out=rsA[:, blk, :], in_=ssA[:, blk, :], func=AF.Sqrt, bias=qtr[:, 1:2]),
                 reads=(sak, "qtr"), writes=(sak + "r",))
            S.op("dve", lambda e, blk=blk: e.reciprocal(out=rsA[:, blk, :], in_=rsA[:, blk, :]), reads=(sak + "r",), writes=(sak + "r",))
            for j in range(4):
                S.op("dve", lambda e, j=j, x1t=x1t, blk=blk: e.tensor_scalar_mul(out=xn2_all[:, 4 * blk + j, :], in0=x1t[:, j, :],
                                                                                 scalar1=rsA[:, blk, j:j + 1]),
                     reads=(ak, sak + "r"), writes=("xn2_%d" % blk,))
            for j in range(4):
                S.group("pe", [lambda e, j=j, c=c, blk=blk: e.transpose(out=tpv[:, c, 128 * j:128 * j + 128],
                                                                        in_=xn2_all[:, 4 * blk + j, 128 * c:128 * c + 128], identity=identb[:])
                               for c in range(8)], reads=("xn2_%d" % blk, "identb"), writes=TPK)
            for c in range(8):
                S.op("act", lambda e, c=c: e.activation(out=hmT1[:, c, :], in_=tpv[:, c, :], func=AF.Identity,
                                                        scale=s2[:, c:c + 1], bias=mods[:, 24 + c, 0:1]),
                     reads=TPK + ("sc", "mods"), writes=("hmT1",))
            for j in range(4):
                S.group("pe", [lambda e, k=k, j=j: e.matmul(bank(4)[:, 20 * j:20 * j + 20], lhsT=hmT1[:, k, 128 * j:128 * j + 128],
                                                            rhs=w_rt_b[:, k, :], start=(k == 0), stop=(k == 7))
                               for k in range(8)], reads=("hmT1", "w_rt_b"), writes=("ps4",))
            S.op("dve", lambda e: e.tensor_tensor(out=L[:], in0=bank(4)[:, 0:80].rearrange("p (j n) -> p j n", n=20),
                                                  in1=bc(b_rt_sb[:].unsqueeze(1), [128, 4, 20]), op=ALU.add),
                 reads=("ps4", "params"), writes=("L",))
            R = ("rt",)
            OK_ = ("oh_all",)
            S.op("dve", lambda e: e.tensor_reduce(out=gmax[:], in_=L[:, :, 0:4], axis=AX.X, op=ALU.max), reads=("L",), writes=R)
            S.op("dve", lambda e, oh=oh: e.tensor_tensor(out=oh, in0=L[:, :, 0:4], in1=bc(gmax[:], [128, 4, 4]), op=ALU.is_equal),
                 reads=R + ("L",), writes=R + OK_)
            S.op("dve", lambda e: e.tensor_tensor(out=eg[:], in0=L[:, :, 0:4], in1=bc(gmax[:], [128, 4, 4]), op=ALU.subtract),
                 reads=R + ("L",), writes=R)
            S.op("act", lambda e: e.activation(out=eg[:], in_=eg[:], func=AF.Exp), reads=R, writes=R)
            S.op("dve", lambda e: e.tensor_reduce(out=pg[:], in_=eg[:], axis=AX.X, op=ALU.add), reads=R, writes=R)
            S.op("dve", lambda e: e.reciprocal(out=pg[:], in_=pg[:]), reads=R, writes=R)
            S.op("dve", lambda e, oh=oh: e.tensor_tensor(out=tmp16[:].rearrange("p j (g x) -> p j g x", x=4),
                                                         in0=L[:, :, 4:20].rearrange("p j (g x) -> p j g x", x=4),
                                                         in1=bc(oh.unsqueeze(3), [128, 4, 4, 4]), op=ALU.mult),
                 reads=R + ("L",), writes=R)
            S.op("dve", lambda e: e.tensor_reduce(out=esel[:].unsqueeze(3), in_=tmp16[:].rearrange("p j (g x) -> p j x g", x=4),
                                                  axis=AX.X, op=ALU.add), reads=R, writes=R)
            S.op("dve", lambda e: e.tensor_reduce(out=m1[:], in_=esel[:], axis=AX.X, op=ALU.max), reads=R, writes=R)
            S.op("dve", lambda e: e.tensor_tensor(out=k1[:], in0=esel[:], in1=bc(m1[:], [128, 4, 4]), op=ALU.is_equal),
                 reads=R, writes=R)
            S.op("dve", lambda e: e.scalar_tensor_tensor(out=e2[:], in0=k1[:], scalar=-1e30, in1=esel[:], op0=ALU.mult, op1=ALU.add),
                 reads=R, writes=R)
            S.op("dve", lambda e: e.tensor_reduce(out=m2[:], in_=e2[:], axis=AX.X, op=ALU.max), reads=R, writes=R)
            S.op("dve", lambda e: e.tensor_tensor(out=k2[:], in0=e2[:], in1=bc(m2[:], [128, 4, 4]), op=ALU.is_equal),
                 reads=R, writes=R)
            S.op("dve", lambda e: e.tensor_tensor(out=w2[:], in0=m2[:], in1=m1[:], op=ALU.subtract), reads=R, writes=R)
            S.op("act", lambda e: e.activation(out=w2[:], in_=w2[:], func=AF.Exp), reads=R, writes=R)
            S.op("dve", lambda e: e.tensor_scalar_add(out=w1[:], in0=w2[:], scalar1=1.0), reads=R, writes=R)
            S.op("dve", lambda e: e.reciprocal(out=w1[:], in_=w1[:]), reads=R, writes=R)
            S.op("dve", lambda e: e.tensor_tensor(out=w2[:], in0=w2[:], in1=w1[:], op=ALU.mult), reads=R, writes=R)
            S.op("dve", lambda e: e.tensor_tensor(out=w1[:], in0=w1[:], in1=pg[:], op=ALU.mult), reads=R, writes=R)
            S.op("dve", lambda e: e.tensor_tensor(out=w2[:], in0=w2[:], in1=pg[:], op=ALU.mult), reads=R, writes=R)
            S.op("dve", lambda e, wsel=wsel: e.tensor_tensor(out=wsel, in0=k1[:], in1=bc(w1[:], [128, 4, 4]), op=ALU.mult),
                 reads=R, writes=R + ("wsel_all",))
            S.op("dve", lambda e: e.tensor_tensor(out=k2[:], in0=k2[:], in1=bc(w2[:], [128, 4, 4]), op=ALU.mult), reads=R, writes=R)
            S.op("dve", lambda e, wsel=wsel: e.tensor_tensor(out=wsel, in0=wsel, in1=k2[:], op=ALU.add), reads=R + ("wsel_all",),
                 writes=R + ("wsel_all",))

        ohf = oh_all[:].rearrange("p t g -> p (t g)")
        S.op("dve", lambda e: e.tensor_copy(out=oh_bf[:], in_=oh_all[:]), reads=("oh_all",), writes=("oh_bf",))
        S.group("pe", [lambda e: e.matmul(bank(0)[:, 0:128], lhsT=trib[:], rhs=oh_bf[:].rearrange("p t g -> p (t g)"), start=True, stop=True)],
                reads=("oh_bf", "trib"), writes=("ps0",))
        S.group("pe", [lambda e: e.matmul(bank(1)[:, 0:128], lhsT=ones1[:], rhs=oh_bf[:].rearrange("p t g -> p (t g)"), start=True, stop=True)],
                reads=("oh_bf", "ones1"), writes=("ps1",))
        S.op("act", lambda e: e.activation(out=R1s[:].rearrange("p t g -> p (t g)"), in_=bank(0)[:, 0:128], func=AF.Copy),
             reads=("ps0",), writes=("R1s",))
        S.op("act", lambda e: e.activation(out=Cs[:].rearrange("p t g -> p (t g)"), in_=bank(1)[:, 0:128], func=AF.Copy),
             reads=("ps1",), writes=("Cs",))
        for g in range(4):
            S.op("dve", lambda e, g=g: e.tensor_tensor_scan(out=incl[:, g, :], data0=onesf[:], data1=Cs[:, :, g], initial=0.0,
                                                            op0=ALU.mult, op1=ALU.add), reads=("Cs", "onesf"), writes=("incl",))
        S.op("dve", lambda e: e.tensor_copy(out=ng[:], in_=incl[:, :, 31]), reads=("incl",), writes=("ng",))
        S.op("dve", lambda e: e.tensor_tensor(out=incl[:], in0=incl[:], in1=Cs[:].rearrange("p t g -> p g t"), op=ALU.subtract),
             reads=("incl", "Cs"), writes=("incl",))
        S.op("dve", lambda e: e.memset(nseg[:], 0.0), writes=("nseg",))
        for k in range(8):
            S.op("dve", lambda e, k=k: e.scalar_tensor_tensor(out=nseg[:], in0=ng[:], scalar=float(NB * k), in1=nseg[:],
                                                              op0=ALU.is_gt, op1=ALU.add), reads=("ng", "nseg"), writes=("nseg",))
        S.op("dve", lambda e: e.memset(sst[:], 0.0), writes=("sst",))
        for g in range(1, 4):
            S.op("dve", lambda e, g=g: e.tensor_tensor(out=sst[:, g:g + 1], in0=sst[:, g - 1:g], in1=nseg[:, g - 1:g], op=ALU.add),
                 reads=("sst", "nseg"), writes=("sst",))
        S.op("dve", lambda e: e.tensor_tensor(out=sen[:], in0=sst[:], in1=nseg[:], op=ALU.add), reads=("sst", "nseg"), writes=("sen",))
        S.op("dve", lambda e: e.tensor_scalar_mul(out=sst[:], in0=sst[:], scalar1=float(NB)), reads=("sst", "sen"), writes=("sst",))
        S.op("dve", lambda e: e.tensor_tensor(out=R1s[:], in0=R1s[:], in1=incl[:].rearrange("p g t -> p t g"), op=ALU.add),
             reads=("R1s", "incl"), writes=("R1s",))
        S.op("dve", lambda e: e.tensor_tensor(out=R1s[:], in0=R1s[:], in1=bc(sst[:].unsqueeze(1), [128, 32, 4]), op=ALU.add),
             reads=("R1s", "sst"), writes=("R1s",))
        S.op("dve", lambda e: e.tensor_tensor(out=R1s[:], in0=R1s[:], in1=oh_all[:], op=ALU.mult), reads=("R1s", "oh_all"), writes=("R1s",))
        S.op("dve", lambda e: e.tensor_reduce(out=slot_f[:].unsqueeze(2), in_=R1s[:], axis=AX.X, op=ALU.add), reads=("R1s",), writes=("slot_f",))
        S.op("dve", lambda e: e.tensor_copy(out=slot_i[:], in_=slot_f[:]), reads=("slot_f",), writes=("slot_i",))
        S.op("dve", lambda e: e.memset(Gs[:], 0.0), writes=("Gs",))
        for g in range(3):
            S.op("dve", lambda e, g=g: e.scalar_tensor_tensor(out=Gs[:], in0=sidx_sb[:], scalar=sen[:, g:g + 1], in1=Gs[:],
                                                              op0=ALU.is_ge, op1=ALU.add), reads=("cidx", "sen", "Gs"), writes=("Gs",))
        S.op("dve", lambda e: e.tensor_scalar_mul(out=offE_f[:], in0=bc(Gs[:].unsqueeze(2), [128, NSEG, 4]), scalar1=512.0),
             reads=("Gs",), writes=("offE_f",))
        S.op("dve", lambda e: e.tensor_tensor(out=offE_f[:], in0=offE_f[:], in1=bc(cE_sb[:].unsqueeze(1), [128, NSEG, 4]), op=ALU.add),
             reads=("offE_f", "cidx"), writes=("offE_f",))
        S.op("dve", lambda e: e.tensor_copy(out=offE_i[:], in_=offE_f[:]), reads=("offE_f",), writes=("offE_i",))
        for t in range(32):
            S.idma("pool", xs_sorted[:, :], bass.IndirectOffsetOnAxis(ap=slot_i[:, t:t + 1], axis=0), xn2_all[:, t, :], None,
                   reads=("slot_i", "xn2_%d" % (t // 4), "xs_sorted"), writes=("xs_sorted_s",), key="scat")
            S.idma("pool", ws_sorted[:, :], bass.IndirectOffsetOnAxis(ap=slot_i[:, t:t + 1], axis=0), wsel_all[:, t, :], None,
                   reads=("slot_i", "wsel_all", "ws_sorted"), writes=("ws_sorted_s",), key="scat")
        S.barrier()
        c1.close()

        S.ALPHA = 0.0
        c2 = ExitStack()
        xst = [sb("xst%d" % i, [128, 4, 1024], BF16, c2) for i in range(2)]
        wst = [sb("wst%d" % i, [128, 4, 4], F32, c2) for i in range(2)]
        hmTs = [sb("hmT%d" % i, [128, 8, NB], BF16, c2) for i in range(2)]
        cbc = [sb("cbc%d" % i, [128, 4, NB], BF16, c2) for i in range(2)]
        dgm = sb("dgm", [128, 4, 128], BF16, c2)
        actb = [sb("actb%d" % i, [128, NB], BF16, c2) for i in range(16)]
        wgu = [sb("wgu%d" % i, [128, 2, 8, 512], BF16, c2) for i in range(3)]
        NWD = 5
        wd = [sb("wd%d" % i, [128, 4, 1024], BF16, c2) for i in range(NWD)]
        sg = [sb("sg%d" % i, [128, NB], F32, c2) for i in range(2)]
        tt = [sb("tt%d" % i, [128, NB], BF16, c2) for i in range(2)]
        ysb = [sb("ysb%d" % i, [128, 4, 1024], F32, c2) for i in range(2)]
        ectr = [0]
        for sgi in range(NSEG):
            pb2 = sgi % 2
            hmT, hk = hmTs[pb2], "hmT%d" % pb2
            xk2, wk2, ck2, yk2 = "xst%d" % pb2, "wst%d" % pb2, "cbc%d" % pb2, "ysb%d" % pb2
            S.dma("sp", xst[pb2][:], xs_sorted[NB * sgi:NB * sgi + NB, :].rearrange("(j p) d -> p j d", p=128), writes=(xk2,), key=xk2)
            S.dma("sp", wst[pb2][:], ws_sorted[NB * sgi:NB * sgi + NB, :].rearrange("(j p) c -> p j c", p=128), writes=(wk2,), key=wk2)
            for j in range(4):
                S.group("pe", [lambda e, j=j, c=c, pb2=pb2: e.transpose(out=tpv[:, c, 128 * j:128 * j + 128],
                                                                        in_=xst[pb2][:, j, 128 * c:128 * c + 128], identity=identb[:])
                               for c in range(8)], reads=(xk2, "identb"), writes=TPK)
            for c in range(8):
                S.op("act", lambda e, c=c, hmT=hmT: e.activation(out=hmT[:, c, :], in_=tpv[:, c, :], func=AF.Identity,
                                                                 scale=s2[:, c:c + 1], bias=mods[:, 24 + c, 0:1]),
                     reads=TPK + ("sc", "mods"), writes=(hk,))
            for j in range(4):
                S.op("dve", lambda e, j=j, pb2=pb2: e.tensor_tensor(out=dgm[:], in0=bc(identb[:].unsqueeze(1), [128, 4, 128]),
                                                                    in1=bc(wst[pb2][:, j, :].unsqueeze(2), [128, 4, 128]), op=ALU.mult),
                     reads=("identb", wk2), writes=("dgm",))
                S.group("pe", [lambda e: e.matmul(bank(4 + (j % 2)), lhsT=ones1[:], rhs=dgm[:], start=True, stop=True)],
                        reads=("dgm", "ones1"), writes=(pk(4 + (j % 2)),))
                S.op("act", lambda e, j=j, pb2=pb2: e.activation(out=cbc[pb2][:, :, 128 * j:128 * j + 128],
                                                                 in_=bank(4 + (j % 2)).rearrange("p (x t) -> p x t", t=128), func=AF.Copy),
                     reads=(pk(4 + (j % 2)),), writes=(ck2,))
            for el in range(4):
                si = ectr[0] % 3
                di = ectr[0] % NWD
                ectr[0] += 1
                gk, dk_ = "wgu%d" % si, "wd%d" % di
                ofs = bass.IndirectOffsetOnAxis(ap=offE_i[:, sgi, el:el + 1], axis=0)
                S.idma("pool", wgu[si][:, 0].rearrange("p k n -> p (k n)"), None, w_gate[:, :], ofs, reads=("offE_i",), writes=(gk,), key=gk)
                S.idma("pool", wgu[si][:, 1].rearrange("p k n -> p (k n)"), None, w_up[:, :], ofs, reads=("offE_i",), writes=(gk,), key=gk)
                S.idma("pool", wd[di][:].rearrange("p k n -> p (k n)"), None, w_down[:, :], ofs, reads=("offE_i",), writes=(dk_,), key=dk_)
                for f in range(4):
                    u = 4 * el + f
                    pp = u % 2
                    S.group("pe", [lambda e, k=k, f=f, si=si, pp=pp, hmT=hmT: e.matmul(
                        bank(2 * pp), lhsT=wgu[si][:, 0, k, 128 * f:128 * f + 128], rhs=hmT[:, k, :],
                        start=(k == 0), stop=(k == 7)) for k in range(8)], reads=(hk, gk), writes=(pk(2 * pp),))
                    S.group("pe", [lambda e, k=k, f=f, si=si, pp=pp, hmT=hmT: e.matmul(
                        bank(2 * pp + 1), lhsT=wgu[si][:, 1, k, 128 * f:128 * f + 128], rhs=hmT[:, k, :],
                        start=(k == 0), stop=(k == 7)) for k in range(8)], reads=(hk, gk), writes=(pk(2 * pp + 1),))
                    S.op("act", lambda e, pp=pp: e.activation(out=sg[pp][:], in_=bank(2 * pp), func=AF.Silu),
                         reads=(pk(2 * pp),), writes=("sg%d" % pp,))
                    S.op("dve", lambda e, pp=pp: e.tensor_tensor(out=tt[pp][:], in0=bank(2 * pp + 1), in1=sg[pp][:], op=ALU.mult),
                         reads=(pk(2 * pp + 1), "sg%d" % pp), writes=("tt%d" % pp,))
                    S.op("dve", lambda e, pp=pp, u=u, el=el, pb2=pb2: e.tensor_tensor(out=actb[u][:], in0=tt[pp][:], in1=cbc[pb2][:, el, :],
                                                                                      op=ALU.mult),
                         reads=("tt%d" % pp, ck2), writes=("actb%d" % u,))
            dbase = ectr[0] - 4
            for tp_ in range(2):
                fns = []
                for u in range(16):
                    el, f = divmod(u, 4)
                    di = (dbase + el) % NWD
                    for jj in range(2):
                        j = 2 * tp_ + jj
                        for hh in range(2):
                            fns.append(lambda e, u=u, f=f, di=di, j=j, jj=jj, hh=hh: e.matmul(
                                bank(4 + 2 * jj + hh), lhsT=actb[u][:, 128 * j:128 * j + 128],
                                rhs=wd[di][:, f, 512 * hh:512 * hh + 512], start=(u == 0), stop=(u == 15)))
                S.group("pe", fns, reads=tuple("actb%d" % u for u in range(16)) + tuple("wd%d" % ((dbase + el) % NWD) for el in range(4)),
                        writes=("ps4", "ps5", "ps6", "ps7"))
                for jj in range(2):
                    j = 2 * tp_ + jj
                    for hh in range(2):
                        bk = 4 + 2 * jj + hh
                        S.op("dve", lambda e, bk=bk, hh=hh, j=j, pb2=pb2: e.tensor_tensor(
                            out=ysb[pb2][:, j, 512 * hh:512 * hh + 512], in0=bank(bk), in1=gt2b[:, 512 * hh:512 * hh + 512], op=ALU.mult),
                            reads=(pk(bk), "gt2b"), writes=(yk2,))
            S.dma("sp", y_sorted[NB * sgi:NB * sgi + NB, :].rearrange("(j p) d -> p j d", p=128), ysb[pb2][:],
                  reads=(yk2,), writes=("y_sorted",), key="y_sorted")
        S.barrier()
        c2.close()

        S.ALPHA = 0.05
        c3 = ExitStack()
        yg = [sb("yg%d" % i, [128, 4, 1024], F32, c3) for i in range(2)]
        x1b = [sb("x1b%d" % i, [128, 4, 1024], F32, c3) for i in range(2)]
        junkF = sb("junkF", [128, 1024], BF16, c3)
        ssF = sb("ssF", [128, 8, 4], F32, c3)
        rsF = sb("rsF", [128, 8, 4], F32, c3)
        for blk in range(8):
            pb2 = blk % 2
            yk3, xk3, sfk = "yg%d" % pb2, "x1b%d" % pb2, "ssF%d" % blk
            S.dma("sp", x1b[pb2][:], x1_scr[NB * blk:NB * blk + NB, :].rearrange("(j p) d -> p j d", p=128), writes=(xk3,), key=xk3)
            for j in range(4):
                S.idma("pool", yg[pb2][:, j, :], None, y_sorted[:, :], bass.IndirectOffsetOnAxis(ap=slot_i[:, 4 * blk + j:4 * blk + j + 1], axis=0),
                       reads=("slot_i",), writes=(yk3,), key=yk3)
            S.op("pool", lambda e, blk=blk: e.memset(ssF[:, blk, :], 0.0), writes=(sfk,))
            for j in range(4):
                S.op("dve", lambda e, j=j, pb2=pb2: e.tensor_tensor(out=x1b[pb2][:, j, :], in0=x1b[pb2][:, j, :], in1=yg[pb2][:, j, :], op=ALU.add),
                     reads=(xk3, yk3), writes=(xk3,))
                S.op("act", lambda e, j=j, pb2=pb2, blk=blk: e.activation(out=junkF[:], in_=x1b[pb2][:, j, :], func=AF.Square,
                                                                          accum_out=ssF[:, blk, j:j + 1]),
                     reads=(xk3,), writes=("junkF", sfk))
            S.op("act", lambda e, blk=blk: e.activation(out=rsF[:, blk, :], in_=ssF[:, blk, :], func=AF.Sqrt, bias=qtr[:, 1:2]),
                 reads=(sfk, "qtr"), writes=(sfk + "r",))
            S.op("dve", lambda e, blk=blk: e.reciprocal(out=rsF[:, blk, :], in_=rsF[:, blk, :]), reads=(sfk + "r",), writes=(sfk + "r",))
            for j in range(4):
                S.op("dve", lambda e, j=j, pb2=pb2, blk=blk: e.scalar_tensor_tensor(out=x1b[pb2][:, j, :], in0=x1b[pb2][:, j, :],
                                                                                    scalar=rsF[:, blk, j:j + 1], in1=gf32[:],
                                                                                    op0=ALU.mult, op1=ALU.mult),
                     reads=(xk3, sfk + "r", "gf32"), writes=(xk3,))
            S.dma("sp", out[NB * blk:NB * blk + NB, :].rearrange("(j p) d -> p j d", p=128), x1b[pb2][:],
                  reads=(xk3,), writes=("out%d" % blk,), key="out")
        S.barrier()
        c3.close()
        pcg.close()
    return nc


_NC_CACHE = {}


def _fm(v):
    v = np.asarray(v, np.float32).reshape(-1, 128)
    return np.ascontiguousarray(v.T)


def kernel(x, c, ctx, c_ctx, w_ada, b_ada, g_mix, w_in, b_in, conv_w, conv_b, ln_g, ln_b, w_pa,
           lru_conv_w, lru_conv_b, w_r_f, b_r_f, w_i_f, b_i_f, lam_f, w_r_b, b_r_b, w_i_b, b_i_b, lam_b,
           w_pb, w_o, g_ffn, w_grp, b_grp, w_er, b_er, w_gate, w_up, w_down, g_final):
    f = lambda a: np.ascontiguousarray(np.asarray(a, np.float32))
    x, c, ctx, c_ctx = f(x), f(c), f(ctx), f(c_ctx)
    B = x.shape[0]
    if "nc" not in _NC_CACHE:
        _NC_CACHE["nc"] = build_program()
    nc = _NC_CACHE["nc"]

    common = {
        "w_ada": f(w_ada[0]), "b_ada_fm": _fm(b_ada[0]),
        "b_ada_gt": f(np.broadcast_to(np.stack([b_ada[0][2048:3072], b_ada[0][5120:6144]])[None], (128, 2, 1024))),
        "w_in": f(w_in[0]), "b_in_fm": _fm(b_in[0]),
        "cb": _fm(conv_b[0]), "lng": _fm(ln_g[0]), "lnb": _fm(ln_b[0]),
        "w_pa": f(w_pa[0]), "w_pb": f(w_pb[0]), "w_o": f(w_o[0]),
        "lb": _fm(lru_conv_b[0]),
        "gmix": _fm(g_mix[0]), "gffn": _fm(g_ffn[0]),
        "gfin": f(np.broadcast_to(np.asarray(g_final, np.float32)[None], (128, 1024))),
        "w_rt": f(np.concatenate([w_grp[0], w_er[0]], axis=1)),
        "b_rt": f(np.broadcast_to(np.concatenate([b_grp[0], b_er[0]])[None], (128, 20))),
        "w_gate": f(np.asarray(w_gate[0], np.float32).reshape(16, 8, 128, 512).transpose(0, 2, 1, 3).reshape(2048, 4096)),
        "w_up": f(np.asarray(w_up[0], np.float32).reshape(16, 8, 128, 512).transpose(0, 2, 1, 3).reshape(2048, 4096)),
        "w_down": f(np.asarray(w_down[0], np.float32).reshape(16, 4, 128, 1024).transpose(0, 2, 1, 3).reshape(2048, 4096)),
        "ident": np.eye(128, dtype=np.float32),
        "tri": np.triu(np.ones((128, 128), np.float32), 1),
        "cE": (np.arange(4)[None, :] * 128 + np.arange(128)[:, None]).astype(np.float32),
        "sidx": np.broadcast_to(np.arange(12, dtype=np.float32)[None], (128, 12)).copy(),
    }
    cwn = np.asarray(conv_w[0], np.float32)
    lwn = np.asarray(lru_conv_w[0], np.float32)
    zero = np.zeros((1, 1024), np.float32)
    lw5_nat = np.concatenate([lwn, zero], axis=0)
    lw5_rev = lw5_nat[::-1]

    def fm3(a):
        T = a.shape[0]
        return np.ascontiguousarray(a.reshape(T, 8, 128).transpose(2, 1, 0))

    pf = (w_r_f[0], b_r_f[0], w_i_f[0], b_i_f[0], lam_f[0])
    pbk = (w_r_b[0], b_r_b[0], w_i_b[0], b_i_b[0], lam_b[0])

    def gates(P, Sd):
        wgs = np.stack([P[0], P[2], Sd[0], Sd[2]]).astype(np.float32)
        bgs = np.stack([np.asarray(t, np.float32) for t in (P[1], P[3], Sd[1], Sd[3])])
        bgs = np.ascontiguousarray(bgs.transpose(2, 0, 1))
        lams = np.stack([np.asarray(P[4], np.float32).reshape(8, 128), np.asarray(Sd[4], np.float32).reshape(8, 128)])
        lams = np.ascontiguousarray(lams.transpose(2, 0, 1))
        return f(wgs), bgs, lams

    per_half = []
    for half in range(2):
        if half == 0:
            wgs, bgs, lams = gates(pf, pbk)
            d = {"cw": fm3(cwn), "lw5": fm3(lw5_nat), "wg": wgs, "bg": bgs, "lam": lams}
        else:
            wgs, bgs, lams = gates(pbk, pf)
            d = {"cw": fm3(cwn[::-1]), "lw5": fm3(lw5_rev), "wg": wgs, "bg": bgs, "lam": lams}
        per_half.append(d)

    in_maps = []
    pad2 = np.zeros((2, 1024), np.float32)
    for b in range(B):
        for half in range(2):
            xs = x[b] if half == 0 else x[b, ::-1]
            cs_ = ctx[b] if half == 0 else ctx[b, ::-1]
            m = dict(common)
            m.update(per_half[half])
            m["xp"] = np.ascontiguousarray(np.concatenate([pad2, xs, pad2], axis=0))
            m["ctxp"] = np.ascontiguousarray(np.concatenate([pad2, cs_, pad2], axis=0))
            m["cvec"] = np.ascontiguousarray(np.stack([_fm(c[b]), _fm(c_ctx)], axis=-1))
            in_maps.append(m)
    res = run_bass_kernel_spmd(nc, in_maps, core_ids=list(range(2 * B)))
    outp = np.empty((B, 2 * NOWN, 1024), np.float32)
    for b in range(B):
        outp[b, :NOWN] = res.results[2 * b]["out"]
        outp[b, NOWN:] = res.results[2 * b + 1]["out"][::-1]
    if DEBUG:
        kernel.last = res
    return outp
```

```python
from contextlib import ExitStack
import os
import numpy as np
import concourse.bass as bass
import concourse.mybir as mybir
from concourse.bass_utils import run_bass_kernel_spmd

F32 = mybir.dt.float32
BF16 = mybir.dt.bfloat16
AF = mybir.ActivationFunctionType
ALU = mybir.AluOpType
AX = mybir.AxisListType
EPS = 1e-6
NB = 512
NOWN = 4096
DEBUG = bool(int(os.environ.get("MK_DEBUG", "0")))


class _Rec:
    def __init__(self):
        self.calls = []

    def __getattr__(self, name):
        def f(*args, **kw):
            self.calls.append((name, args, kw))
            return self
        return f


_TBL = {"Exp": "exp", "Tanh": None, "Identity": None, "Copy": None, "Square": None, "Sqrt": "sqrt", "Silu": "silu",
        "Gelu_apprx_tanh": "gelu", "Ln": "ln"}


def _fsize(ap):
    n = 1
    for d in ap.shape[1:]:
        n *= int(d)
    return n


class Sched:
    REORDER = True
    WINDOW = 600
    ALPHA = 0.05

    def __init__(self, nc, es):
        self.nc = nc
        self.es = es
        self.E = dict(pe=nc.tensor, act=nc.scalar, dve=nc.vector, pool=nc.gpsimd, sp=nc.sync)
        self.sem = {e: es.enter_context(nc.semaphore("c_" + e)) for e in self.E}
        self.cnt = {e: 0 for e in self.E}
        self.seen = {e: {} for e in self.E}
        self.lastw = {}
        self.readers = {}
        self.dsem = {}
        self.dcnt = {}
        self.ops = []
        self.lw_nowaw = {}

    def op(self, e, fn, reads=(), writes=()):
        r = _Rec()
        fn(r)
        self._add(e, "op", r.calls, tuple(reads), tuple(writes), None)

    def group(self, e, fns, reads=(), writes=()):
        r = _Rec()
        for f in fns:
            f(r)
        self._add(e, "op", r.calls, tuple(reads), tuple(writes), None)

    def dma(self, q, out, in_, reads=(), writes=(), key=None, nowaw=False):
        self._add(q, "dma", [("dma_start", (), dict(out=out, in_=in_))], tuple(reads), tuple(writes), key, nowaw)

    def idma(self, q, out, out_offset, in_, in_offset, reads=(), writes=(), key=None, nowaw=False):
        self._add(q, "dma", [("indirect_dma_start", (), dict(out=out, out_offset=out_offset, in_=in_, in_offset=in_offset))],
                  tuple(reads), tuple(writes), key, nowaw)

    def _add(self, e, kind, calls, reads, writes, key, nowaw=False):
        dur = 0.0
        tbl = None
        if kind == "dma":
            kw0 = calls[0][2]
            side = kw0["in_"] if kw0.get("out_offset") is not None else kw0["out"]
            nb = 128 * _fsize(side) * 4
            dur = 1000.0 if e == "pool" else 150.0
            lat = 2000.0 + nb / 300.0
        else:
            lat = 0.0
            for (name, args, kw) in calls:
                if e == "pe":
                    src = kw.get("rhs", kw.get("in_"))
                    dur += 25.0 + 0.5 * max(_fsize(src), 64)
                else:
                    oap = kw.get("out", kw.get("ap", args[0] if args else None))
                    n = _fsize(oap)
                    if e == "act":
                        dur += 250.0 + 0.73 * n
                        fnm = kw.get("func")
                        tbl = _TBL.get(getattr(fnm, "name", str(fnm)), None) if fnm is not None else None
                    elif e == "dve":
                        dur += 160.0 + 1.04 * n
                    else:
                        dur += 300.0 + 3.1 * n
        self.ops.append(dict(e=e, kind=kind, calls=calls, reads=reads, writes=writes, key=key, dur=dur, lat=lat, tbl=tbl, nowaw=nowaw))

    def flush(self):
        ops = self.ops
        self.ops = []
        n = len(ops)
        if n == 0:
            return
        lastw, readers = {}, {}
        preds = [None] * n
        succs = [[] for _ in range(n)]
        wkind = {}
        gdeps = {}
        for i, o in enumerate(ops):
            p = set()
            for k in o["reads"]:
                p.update(lastw.get(k, ()))
            for k in o["writes"]:
                grp = lastw.get(k, ())
                joins = o["nowaw"] and wkind.get(k) == o["key"] and not readers.get(k)
                if not joins:
                    p.update(grp)
                    p.update(readers.get(k, ()))
                    gdeps[k] = tuple(grp) + tuple(readers.get(k, ()))
                else:
                    p.update(gdeps.get(k, ()))
            p.discard(i)
            preds[i] = p
            for j in p:
                succs[j].append(i)
            for k in o["reads"]:
                readers.setdefault(k, []).append(i)
            for k in o["writes"]:
                joins = o["nowaw"] and wkind.get(k) == o["key"] and not readers.get(k)
                if joins:
                    lastw[k] = lastw[k] + (i,)
                else:
                    lastw[k] = (i,)
                    wkind[k] = o["key"] if o["nowaw"] else None
                readers[k] = []
        if not self.REORDER:
            order = range(n)
        else:
            indeg = [len(p) for p in preds]
            alpha = float(os.environ.get("MK_PRI", self.ALPHA))
            rank = [0.0] * n
            if alpha > 0:
                for i in range(n - 1, -1, -1):
                    m = 0.0
                    for j in succs[i]:
                        if rank[j] > m:
                            m = rank[j]
                    rank[i] = ops[i]["dur"] + ops[i]["lat"] + m
            ready = [i for i in range(n) if indeg[i] == 0]
            finish = [0.0] * n
            efree = {e: 0.0 for e in self.E}
            etbl = [None]
            done = [False] * n
            lo = 0
            order = []
            while len(order) < n:
                while lo < n and done[lo]:
                    lo += 1
                best, bkey = None, None
                for i in ready:
                    if i > lo + self.WINDOW:
                        continue
                    o = ops[i]
                    st = efree[o["e"]]
                    for j in preds[i]:
                        f = finish[j] + (0.0 if ops[j]["e"] == o["e"] else 120.0)
                        if f > st:
                            st = f
                    if o["e"] == "act" and o["tbl"] is not None and o["tbl"] != etbl[0]:
                        st += 1300.0
                    kk = (st - alpha * rank[i], i) if alpha > 0 else (st, i)
                    if bkey is None or kk < bkey:
                        best, bkey = i, kk
                i = best
                o = ops[i]
                st = bkey[0] + (alpha * rank[i] if alpha > 0 else 0.0)
                if os.environ.get("MK_TL") and n > 3000 and len(ops) == int(os.environ.get("MK_TL")):
                    lim = None
                    for j in preds[i]:
                        f = finish[j]
                        if lim is None or f > lim[0]:
                            lim = (f, j)
                    gap = st - efree[o["e"]]
                    if o["e"] == "pe" and gap > 300:
                        print("PE gap %.1fus at t=%.1fus op#%d writes=%s waits for %s op#%d writes=%s" % (
                            gap / 1e3, st / 1e3, i, o["writes"][:2], ops[lim[1]]["e"], lim[1], ops[lim[1]]["writes"][:2]))
                if o["e"] == "act" and o["tbl"] is not None:
                    etbl[0] = o["tbl"]
                efree[o["e"]] = st + o["dur"]
                finish[i] = st + o["dur"] + o["lat"]
                done[i] = True
                ready.remove(i)
                order.append(i)
                for j in succs[i]:
                    indeg[j] -= 1
                    if indeg[j] == 0:
                        ready.append(j)
        if self.REORDER and os.environ.get("MK_STATS"):
            busy = {e: 0.0 for e in self.E}
            for o in ops:
                busy[o["e"]] += o["dur"]
            print("phase: n=%d est_makespan=%.0fus busy(us): %s" % (n, max(finish) / 1e3, {e: int(v / 1e3) for e, v in busy.items()}))
        for i in order:
            self._emit(ops[i])

    def _wait(self, e, tok, same_ok=False):
        if tok is None:
            return
        name, sem, val, src = tok
        if same_ok and src == e:
            return
        d = self.seen[e]
        if d.get(name, 0) >= val:
            return
        self.E[e].wait_ge(sem, val)
        d[name] = val

    def _emit(self, o):
        e, reads, writes = o["e"], o["reads"], o["writes"]
        for k in reads:
            self._wait(e, self.lastw.get(k))
        for k in writes:
            lw = self.lastw.get(k)
            if not (o["nowaw"] and lw is not None and lw[0] == "d_" + str(o["key"]) and self.lw_nowaw.get(k) and not self.readers.get(k)):
                self._wait(e, lw, same_ok=True)
            for t in self.readers.get(k, {}).values():
                self._wait(e, t, same_ok=True)
        ins = None
        for (name, args, kw) in o["calls"]:
            ins = getattr(self.E[e], name)(*args, **kw)
        if o["kind"] == "dma":
            key = o["key"]
            if key not in self.dsem:
                self.dsem[key] = self.es.enter_context(self.nc.semaphore("d_" + key))
                self.dcnt[key] = 0
            self.dcnt[key] += 16
            ins.then_inc(self.dsem[key], 16)
            tok = ("d_" + key, self.dsem[key], self.dcnt[key], "dma")
        else:
            self.cnt[e] += 1
            ins.then_inc(self.sem[e], 1)
            tok = ("c_" + e, self.sem[e], self.cnt[e], e)
        for k in reads:
            self.readers.setdefault(k, {})[tok[0]] = tok
        for k in writes:
            self.lastw[k] = tok
            self.lw_nowaw[k] = o["nowaw"]
            self.readers[k] = {}

    def barrier(self):
        self.flush()
        for e in self.E:
            for e2 in self.E:
                if self.cnt[e2] > 0:
                    self._wait(e, ("c_" + e2, self.sem[e2], self.cnt[e2], e2))
            for k, sem in self.dsem.items():
                self._wait(e, ("d_" + k, sem, self.dcnt[k], "dma"))


def build_program():
    nc = bass.Bass("TRN2", target_bir_lowering=False)

    def din(name, shape):
        return nc.dram_tensor(name, list(shape), F32, kind="ExternalInput").ap()

    xp = din("xp", [8196, 1024])
    ctxp = din("ctxp", [260, 1024])
    cvec = din("cvec", [128, 8, 2])
    w_ada = din("w_ada", [1024, 6144])
    b_ada_fm = din("b_ada_fm", [128, 48])
    b_ada_gt = din("b_ada_gt", [128, 2, 1024])
    w_in = din("w_in", [1024, 6144])
    b_in_fm = din("b_in_fm", [128, 48])
    cw = din("cw", [128, 8, 31])
    cb = din("cb", [128, 8])
    lng = din("lng", [128, 8])
    lnb = din("lnb", [128, 8])
    w_pa = din("w_pa", [1024, 1024])
    w_pb = din("w_pb", [1024, 1024])
    w_o = din("w_o", [1024, 1024])
    lw5 = din("lw5", [128, 8, 5])
    lb = din("lb", [128, 8])
    wg = din("wg", [4, 8, 128, 128])
    bg = din("bg", [128, 4, 8])
    lam = din("lam", [128, 2, 8])
    gmix = din("gmix", [128, 8])
    gffn = din("gffn", [128, 8])
    gfin = din("gfin", [128, 1024])
    w_rt = din("w_rt", [1024, 20])
    b_rt = din("b_rt", [128, 20])
    w_gate = din("w_gate", [2048, 4096])
    w_up = din("w_up", [2048, 4096])
    w_down = din("w_down", [2048, 4096])
    ident = din("ident", [128, 128])
    tri = din("tri", [128, 128])
    cE = din("cE", [128, 4])
    sidx = din("sidx", [128, 12])
    out = nc.dram_tensor("out", [NOWN, 1024], F32, kind="ExternalOutput").ap()
    if DEBUG:
        hs_scr = nc.dram_tensor("hs_scr", [8, 128, 8, NB], BF16, kind="ExternalOutput").ap()
        x1_scr = nc.dram_tensor("x1_scr", [NOWN, 1024], F32, kind="ExternalOutput").ap()
    else:
        hs_scr = nc.dram_tensor("hs_scr", [8, 128, 8, NB], BF16, kind="Internal").ap()
        x1_scr = nc.dram_tensor("x1_scr", [NOWN, 1024], F32, kind="Internal").ap()
    gt2_scr = nc.dram_tensor("gt2_scr", [128, 1024], F32, kind="Internal").ap()
    xc_scr = nc.dram_tensor("xc_scr", [8, 128, 8, NB], BF16, kind="Internal").ap()
    NSEG = 12
    xs_sorted = nc.dram_tensor("xs_sorted", [NSEG * NB, 1024], BF16, kind="Internal").ap()
    ws_sorted = nc.dram_tensor("ws_sorted", [NSEG * NB, 4], F32, kind="Internal").ap()
    y_sorted = nc.dram_tensor("y_sorted", [NSEG * NB, 1024], F32, kind="Internal").ap()
    I32 = mybir.dt.int32

    with ExitStack() as es:
        S = Sched(nc, es)

        def sb(name, shape, dt=F32, stack=es):
            return stack.enter_context(nc.sbuf_tensor(name, list(shape), dt))

        psA = es.enter_context(nc.psum_tensor("psA", [128, 2048], F32))
        psB = es.enter_context(nc.psum_tensor("psB", [128, 2048], F32))

        def bank(i):
            t = psA if i < 4 else psB
            return t[:, 512 * (i % 4):512 * (i % 4) + 512]

        def pk(i):
            return "ps%d" % i

        tpv = psA[:, :].bitcast(BF16).rearrange("p (c t) -> p c t", t=512)
        TPK = ("ps0", "ps1", "ps2", "ps3")

        identb = sb("identb", [128, 128], BF16)
        ones_m = sb("ones_m", [128, 128], BF16)
        ones1 = sb("ones1", [128, 128], BF16)
        b_in_sb = sb("b_in_sb", [128, 48])
        hb_in = sb("hb_in", [128, 48])
        cw_sb = sb("cw_sb", [128, 8, 31])
        cb_sb = sb("cb_sb", [128, 8])
        lng_sb = sb("lng_sb", [128, 8])
        lnb_sb = sb("lnb_sb", [128, 8])
        lw5_sb = sb("lw5_sb", [128, 8, 5])
        lb_sb = sb("lb_sb", [128, 8])
        bg_sb = sb("bg_sb", [128, 4, 8])
        hbg = sb("hbg", [128, 4, 8])
        lam_sb = sb("lam_sb", [128, 2, 8])
        gmix_sb = sb("gmix_sb", [128, 8])
        gffn_sb = sb("gffn_sb", [128, 8])
        b_rt_sb = sb("b_rt_sb", [128, 20])
        b_ada_fm_sb = sb("b_ada_fm_sb", [128, 48])
        cvec_sb = sb("cvec_sb", [128, 8, 2])
        mods = sb("mods", [128, 48, 2])
        s1 = sb("s1", [128, 8])
        s1c = sb("s1c", [128, 8])
        s2 = sb("s2", [128, 8])
        gt1h = sb("gt1h", [128, 1024])
        cl = sb("cl", [128, 2, 8])
        hcl = sb("hcl", [128, 2, 8])
        state = sb("state", [128, 2, 8])
        ss = sb("ss", [128, 8])
        rs = sb("rs", [128, 8])
        w_rt_b = sb("w_rt_b", [128, 8, 20], BF16)
        qtr = sb("qtr", [128, 4], F32)

        def pload(t, src):
            S.dma("sp", t, src, writes=("params",), key="params", nowaw=True)

        pload(b_in_sb[:], b_in_fm)
        pload(cw_sb[:], cw)
        pload(cb_sb[:], cb)
        pload(lng_sb[:], lng)
        pload(lnb_sb[:], lnb)
        pload(lw5_sb[:], lw5)
        pload(lb_sb[:], lb)
        pload(bg_sb[:], bg)
        pload(lam_sb[:], lam)
        pload(gmix_sb[:], gmix)
        pload(gffn_sb[:], gffn)
        pload(b_rt_sb[:], b_rt)
        pload(b_ada_fm_sb[:], b_ada_fm)
        pload(cvec_sb[:], cvec)
        S.dma("pool", identb[:], ident, writes=("identb",), key="identb")
        S.dma("pool", w_rt_b[:], w_rt.rearrange("(k p) n -> p k n", p=128), writes=("w_rt_b",), key="w_rt_b")
        S.op("pool", lambda e: e.memset(ones_m[:], 1.0 / 1024.0), writes=("ones_m",))
        S.op("pool", lambda e: e.memset(ones1[:], 1.0), writes=("ones1",))
        S.op("pool", lambda e: e.memset(qtr[:, 0:1], 0.25), writes=("qtr",))
        S.op("pool", lambda e: e.memset(qtr[:, 1:2], 1024.0 * EPS), writes=("qtr",))
        S.op("pool", lambda e: e.memset(qtr[:, 2:3], EPS), writes=("qtr",))
        S.op("pool", lambda e: e.memset(state[:], 0.0), writes=("state",))
        S.op("pool", lambda e: e.memset(ss[:], 0.0), writes=("ss",))

        with ExitStack() as p0:
            cs = sb("cs", [128, 8, 2], BF16, p0)
            cs_rep = sb("cs_rep", [128, 8, 128], BF16, p0)
            b_ada_gt_sb = sb("b_ada_gt_sb", [128, 2, 1024], F32, p0)
            wa = [sb("wa%d" % i, [128, 8, 512], BF16, p0) for i in range(3)]
            e_t = sb("e_t", [128, 16], F32, p0)
            t_t = sb("t_t", [128, 16], F32, p0)
            l_t = sb("l_t", [128, 16], F32, p0)
            m_t = sb("m_t", [128, 16], F32, p0)
            pload(b_ada_gt_sb[:], b_ada_gt)
            gt2b = sb("gt2b0", [128, 1024], F32, p0)

            S.op("act", lambda e: e.activation(out=cs[:], in_=cvec_sb[:], func=AF.Silu), reads=("params",), writes=("cs",))
            S.op("dve", lambda e: e.tensor_copy(out=cs_rep[:], in_=cs[:, :, 0:1].to_broadcast([128, 8, 128])),
                 reads=("cs",), writes=("cs_rep",))
            psm = bank(0)[:, 0:96].rearrange("p (j t) -> p j t", t=2)
            for q in range(12):
                s = q % 3
                S.dma("pool", wa[s][:], w_ada[:, 512 * q:512 * q + 512].rearrange("(k p) n -> p k n", p=128),
                      writes=("wa%d" % s,), key="wa%d" % s)
                fns = []
                for jj in range(4):
                    for k in range(8):
                        fns.append(lambda e, jj=jj, k=k, s=s, q=q: e.matmul(
                            psm[:, 4 * q + jj, :], lhsT=wa[s][:, k, 128 * jj:128 * jj + 128], rhs=cs[:, k, :],
                            start=(k == 0), stop=(k == 7)))
                S.group("pe", fns, reads=("wa%d" % s, "cs"), writes=("ps0",))
                if q in (4, 5, 10, 11):
                    bk = 1 + (q % 2)
                    S.group("pe", [lambda e, k=k, s=s, bk=bk: e.matmul(bank(bk), lhsT=cs_rep[:, k, :], rhs=wa[s][:, k, :],
                                                                      start=(k == 0), stop=(k == 7)) for k in range(8)],
                            reads=("wa%d" % s, "cs_rep"), writes=(pk(bk),))
                    dst = gt1h if q < 6 else gt2b
                    gi = 0 if q < 6 else 1
                    cols = slice(512 * (q % 2), 512 * (q % 2) + 512)
                    S.op("dve", lambda e, dst=dst, gi=gi, cols=cols, bk=bk: e.tensor_tensor(
                        out=dst[:, cols], in0=bank(bk), in1=b_ada_gt_sb[:, gi, cols], op=ALU.add),
                        reads=(pk(bk), "params"), writes=("gt",))
            S.op("dve", lambda e: e.tensor_scalar_mul(out=gt1h[:], in0=gt1h[:], scalar1=0.5), reads=("gt",), writes=("gt",))
            S.dma("sp", gt2_scr, gt2b[:], reads=("gt",), writes=("gt2_scr",), key="gt2_scr")
            S.op("dve", lambda e: e.tensor_tensor(out=mods[:], in0=psm, in1=b_ada_fm_sb[:].unsqueeze(2).to_broadcast([128, 48, 2]),
                                                  op=ALU.add), reads=("ps0", "params"), writes=("mods",))
            for (dst, col, j0, gsb) in ((s1, 0, 8, gmix_sb), (s1c, 1, 8, gmix_sb), (s2, 0, 32, gffn_sb)):
                S.op("dve", lambda e, dst=dst, col=col, j0=j0, gsb=gsb: e.scalar_tensor_tensor(
                    out=dst[:], in0=mods[:, j0:j0 + 8, col], scalar=1.0, in1=gsb[:], op0=ALU.add, op1=ALU.mult),
                    reads=("mods", "params"), writes=("sc",))
                S.op("dve", lambda e, dst=dst: e.tensor_scalar_mul(out=dst[:], in0=dst[:], scalar1=32.0),
                     reads=("sc",), writes=("sc",))
            S.op("dve", lambda e: e.tensor_scalar_mul(out=hb_in[:], in0=b_in_sb[:], scalar1=0.5), reads=("params",), writes=("hb_in",))
            S.op("dve", lambda e: e.tensor_scalar_mul(out=hbg[:], in0=bg_sb[:], scalar1=0.5), reads=("params",), writes=("hbg",))
            lamf = lam_sb[:].rearrange("p a b -> p (a b)")
            S.op("act", lambda e: e.activation(out=e_t[:], in_=lamf, func=AF.Exp, scale=-1.0), reads=("params",), writes=("e_t",))
            S.op("dve", lambda e: e.tensor_scalar(out=t_t[:], in0=e_t[:], scalar1=-0.25, scalar2=1.0 / 3.0, op0=ALU.mult, op1=ALU.add),
                 reads=("e_t",), writes=("t_t",))
            S.op("dve", lambda e: e.tensor_tensor(out=t_t[:], in0=t_t[:], in1=e_t[:], op=ALU.mult), reads=("t_t", "e_t"), writes=("t_t",))
            S.op("dve", lambda e: e.tensor_scalar_add(out=t_t[:], in0=t_t[:], scalar1=-0.5), reads=("t_t",), writes=("t_t",))
            S.op("dve", lambda e: e.tensor_tensor(out=t_t[:], in0=t_t[:], in1=e_t[:], op=ALU.mult), reads=("t_t", "e_t"), writes=("t_t",))
            S.op("dve", lambda e: e.tensor_scalar_add(out=t_t[:], in0=t_t[:], scalar1=1.0), reads=("t_t",), writes=("t_t",))
            S.op("dve", lambda e: e.tensor_tensor(out=t_t[:], in0=t_t[:], in1=e_t[:], op=ALU.mult), reads=("t_t", "e_t"), writes=("t_t",))
            S.op("dve", lambda e: e.tensor_scalar_add(out=l_t[:], in0=e_t[:], scalar1=1.0), reads=("e_t",), writes=("l_t",))
            S.op("act", lambda e: e.activation(out=l_t[:], in_=l_t[:], func=AF.Ln), reads=("l_t",), writes=("l_t",))
            S.op("dve", lambda e: e.tensor_single_scalar(out=m_t[:], in_=e_t[:], scalar=0.1, op=ALU.is_lt), reads=("e_t",), writes=("m_t",))
            S.op("dve", lambda e: e.tensor_tensor(out=t_t[:], in0=t_t[:], in1=l_t[:], op=ALU.subtract), reads=("t_t", "l_t"), writes=("t_t",))
            S.op("dve", lambda e: e.tensor_tensor(out=t_t[:], in0=t_t[:], in1=m_t[:], op=ALU.mult), reads=("t_t", "m_t"), writes=("t_t",))
            S.op("dve", lambda e: e.tensor_tensor(out=t_t[:], in0=t_t[:], in1=l_t[:], op=ALU.add), reads=("t_t", "l_t"), writes=("t_t",))
            clf = cl[:].rearrange("p a b -> p (a b)")
            hclf = hcl[:].rearrange("p a b -> p (a b)")
            S.op("dve", lambda e: e.tensor_scalar_mul(out=clf, in0=t_t[:], scalar1=-8.0), reads=("t_t",), writes=("cl",))
            S.op("dve", lambda e: e.tensor_scalar_mul(out=hclf, in0=t_t[:], scalar1=-4.0), reads=("t_t",), writes=("cl",))
            S.barrier()

        mixer = ExitStack()
        wxr = sb("wxr", [128, 8, 1024], BF16, mixer)
        wgb = sb("wgb", [128, 4, 8, 128], BF16, mixer)
        dg5 = sb("dg5", [128, 8, 5, 128], BF16, mixer)
        S.dma("pool", wxr[:], w_in[:, 3072:4096].rearrange("(k p) n -> p k n", p=128), writes=("wxr",), key="wxr")
        S.dma("pool", wgb[:], wg.rearrange("g h p n -> p g h n"), writes=("wgb",), key="wgb")
        for c in range(8):
            S.op("dve", lambda e, c=c: e.tensor_tensor(
                out=dg5[:, c, :, :], in0=identb[:].unsqueeze(1).to_broadcast([128, 5, 128]),
                in1=lw5_sb[:, c, :].unsqueeze(2).to_broadcast([128, 5, 128]), op=ALU.mult),
                reads=("identb", "params"), writes=("dg5",))

        xt = sb("xt", [128, 4, 1024], F32, mixer)
        xh128 = sb("xh128", [128, 1024], F32, mixer)
        xh = xh128[0:4, :]
        xn = sb("xn", [128, 4, 1024], BF16, mixer)
        xnh128 = sb("xnh128", [128, 1024], BF16, mixer)
        xnh = xnh128[0:4, :]
        hxTs = [sb("hxT%d" % i, [128, 8, NB + 4], BF16, mixer) for i in range(2)]
        hxT, hxk, hxhk = hxTs[0], "hxT0", "hxTh0"
        hbc = [0]
        xrp = [sb("xrp%d" % i, [128, NB + 4], BF16, mixer) for i in range(2)]
        xcb = [sb("xcb%d" % i, [128, NB], BF16, mixer) for i in range(2)]
        tr = sb("tr", [128, NB], F32, mixer)
        ti = sb("ti", [128, NB], F32, mixer)
        a4 = sb("a4", [128, 4, NB], F32, mixer)
        s4 = sb("s4", [128, 4, NB], F32, mixer)
        t4 = sb("t4", [128, 4, NB], BF16, mixer)
        tmp1 = sb("tmp1", [128, NB], F32, mixer)
        bb_t = sb("bb_t", [128, NB], F32, mixer)
        hf = sb("hf", [128, NB], F32, mixer)
        hsb = sb("hsb", [128, 8, NB], BF16, mixer)

        tph = bank(4).bitcast(BF16)[:, 0:32].rearrange("p (c t) -> p c t", t=4)

        def prep(xsrc, r0, N, sc, bcol, keep_key, halo=True):
            nt = N // 128
            S.dma("sp", xt[:, 0:nt, :], xsrc[r0:r0 + N, :].rearrange("(j p) d -> p j d", p=128), writes=(keep_key,), key="xt")
            if halo:
                S.dma("sp", xh[0:2, :], xsrc[r0 - 2:r0, :], writes=("xh",), key="xh", nowaw=True)
                S.dma("sp", xh[2:4, :], xsrc[r0 + N:r0 + N + 2, :], writes=("xh",), key="xh", nowaw=True)
            S.op("pool", lambda e: e.memset(ss[:], 0.0), writes=("ss",))
            for j in range(nt):
                S.op("act", lambda e, j=j: e.activation(out=xn[:, j, :], in_=xt[:, j, :], func=AF.Square, accum_out=ss[:, j:j + 1]),
                     reads=(keep_key,), writes=("xn", "ss"))
            if halo:
                S.op("act", lambda e: e.activation(out=xnh[:], in_=xh[:], func=AF.Square, accum_out=ss[0:4, 4:5]),
                     reads=("xh",), writes=("xnh", "ss"))
            S.op("act", lambda e: e.activation(out=rs[:, 0:5], in_=ss[:, 0:5], func=AF.Sqrt, bias=qtr[:, 1:2]), reads=("ss", "qtr"), writes=("rs",))
            S.op("dve", lambda e: e.reciprocal(out=rs[:, 0:5], in_=rs[:, 0:5]), reads=("rs",), writes=("rs",))
            for j in range(nt):
                S.op("dve", lambda e, j=j: e.tensor_scalar_mul(out=xn[:, j, :], in0=xt[:, j, :], scalar1=rs[:, j:j + 1]),
                     reads=(keep_key, "rs"), writes=("xn",))
            if halo:
                S.op("dve", lambda e: e.tensor_scalar_mul(out=xnh[:], in0=xh[:], scalar1=rs[0:4, 4:5]),
                     reads=("xh", "rs"), writes=("xnh",))
            for j in range(nt):
                S.group("pe", [lambda e, j=j, c=c: e.transpose(out=tpv[:, c, 128 * j:128 * j + 128],
                                                               in_=xn[:, j, 128 * c:128 * c + 128], identity=identb[:])
                               for c in range(8)], reads=("xn", "identb"), writes=TPK)
            if halo:
                S.group("pe", [lambda e, c=c: e.transpose(out=tph[:, c, :], in_=xnh[:, 128 * c:128 * c + 128], identity=identb[0:4, 0:4])
                               for c in range(8)], reads=("xnh", "identb"), writes=("ps4",))
            for c in range(8):
                S.op("act", lambda e, c=c: e.activation(out=hxT[:, c, 0:N], in_=tpv[:, c, 0:N], func=AF.Identity,
                                                        scale=sc[:, c:c + 1], bias=mods[:, c, bcol:bcol + 1]),
                     reads=TPK + ("sc", "mods"), writes=(hxk,))
            if halo:
                S.op("dve", lambda e: e.tensor_tensor(out=hxT[:, :, N:N + 4], in0=tph, in1=sc[:].unsqueeze(2).to_broadcast([128, 8, 4]),
                                                      op=ALU.mult), reads=("ps4", "sc"), writes=(hxhk,))
                S.op("dve", lambda e: e.tensor_tensor(out=hxT[:, :, N:N + 4], in0=hxT[:, :, N:N + 4],
                                                      in1=mods[:, 0:8, bcol:bcol + 1].to_broadcast([128, 8, 4]), op=ALU.add),
                     reads=(hxhk, "mods"), writes=(hxhk,))

        xr_evac_eng = ["dve"]

        def rglru_block(N, d, reverse, has_lo, has_hi, consumer=None, xc_store=None, xc_src=None):
            def st1(c):
                par = c % 2
                bxr = bank(par)[:, 0:N]
                bxh = bank(2 + par)[:, 0:4]
                S.group("pe", [lambda e, k=k: e.matmul(bxr, lhsT=wxr[:, k, 128 * c:128 * c + 128], rhs=hxT[:, k, 0:N],
                                                       start=(k == 0), stop=(k == 7)) for k in range(8)],
                        reads=(hxk, "wxr"), writes=(pk(par),))
                S.group("pe", [lambda e, k=k: e.matmul(bxh, lhsT=wxr[:, k, 128 * c:128 * c + 128], rhs=hxT[:, k, N:N + 4],
                                                       start=(k == 0), stop=(k == 7)) for k in range(8)],
                        reads=(hxhk, "wxr"), writes=(pk(2 + par),))
                xk = "xrp%d" % par
                bia = b_in_sb[:, 24 + c:25 + c]
                if xr_evac_eng[0] == "act":
                    S.op("act", lambda e: e.activation(out=xrp[par][:, 2:2 + N], in_=bxr, func=AF.Identity, bias=bia),
                         reads=(pk(par), "params"), writes=(xk,))
                else:
                    S.op("dve", lambda e: e.tensor_scalar_add(out=xrp[par][:, 2:2 + N], in0=bxr, scalar1=bia),
                         reads=(pk(par), "params"), writes=(xk,))
                if has_lo:
                    S.op("dve", lambda e: e.tensor_scalar_add(out=xrp[par][:, 0:2], in0=bxh[:, 0:2], scalar1=bia),
                         reads=(pk(2 + par), "params"), writes=(xk,))
                else:
                    S.op("dve", lambda e: e.memset(xrp[par][:, 0:2], 0.0), writes=(xk,))
                if has_hi:
                    S.op("dve", lambda e: e.tensor_scalar_add(out=xrp[par][:, 2 + N:4 + N], in0=bxh[:, 2:4], scalar1=bia),
                         reads=(pk(2 + par), "params"), writes=(xk,))
                else:
                    S.op("dve", lambda e: e.memset(xrp[par][:, 2 + N:4 + N], 0.0), writes=(xk,))

            def st2(c):
                par = c % 2
                xk = "xrp%d" % par
                bcv = bank(4 + par)[:, 0:N]
                S.group("pe", [lambda e, j=j: e.matmul(bcv, lhsT=dg5[:, c, j, :], rhs=xrp[par][:, j:j + N],
                                                       start=(j == 0), stop=(j == 4)) for j in range(5)],
                        reads=(xk, "dg5"), writes=(pk(4 + par),))
                S.op("dve", lambda e: e.tensor_scalar_add(out=xcb[par][:, 0:N], in0=bcv, scalar1=lb_sb[:, c:c + 1]),
                     reads=(pk(4 + par), "params"), writes=("xcb%d" % par,))
                if xc_store is not None:
                    S.dma("sp", xc_scr[xc_store, :, c, :], xcb[par][:, 0:N], reads=("xcb%d" % par,), writes=("xc_scr%d" % xc_store,), key="xc_scr", nowaw=True)

            def st3(c):
                par = c % 2
                q = c % 4
                ck = "xcb%d" % par
                xc_ap = xcb[par][:, 0:N]
                if xc_src is not None:
                    ck = "dg5"
                    xc_ap = xc_src[:, c, :]
                br_ = bank(6)[:, 0:N]
                bi_ = bank(7)[:, 0:N]
                S.group("pe", [lambda e: e.matmul(br_, lhsT=wgb[:, 2 * d, c, :], rhs=xc_ap, start=True, stop=True)],
                        reads=(ck, "wgb"), writes=("ps6",))
                S.group("pe", [lambda e: e.matmul(bi_, lhsT=wgb[:, 2 * d + 1, c, :], rhs=xc_ap, start=True, stop=True)],
                        reads=(ck, "wgb"), writes=("ps7",))
                S.op("act", lambda e: e.activation(out=tr[:, 0:N], in_=br_, func=AF.Tanh, scale=0.5, bias=hbg[:, 2 * d, c:c + 1]),
                     reads=("ps6", "hbg"), writes=("tr",))
                S.op("act", lambda e: e.activation(out=ti[:, 0:N], in_=bi_, func=AF.Tanh, scale=0.5, bias=hbg[:, 2 * d + 1, c:c + 1]),
                     reads=("ps7", "hbg"), writes=("ti",))
                S.op("act", lambda e: e.activation(out=a4[:, q, 0:N], in_=tr[:, 0:N], func=AF.Exp, scale=hcl[:, d, c:c + 1],
                                                   bias=hcl[:, d, c:c + 1]), reads=("tr", "cl"), writes=("a4_%d" % q,))
                S.op("pool", lambda e: e.tensor_tensor(out=s4[:, q, 0:N], in0=a4[:, q, 0:N], in1=a4[:, q, 0:N], op=ALU.mult),
                     reads=("a4_%d" % q,), writes=("s4_%d" % q,))
                S.op("dve", lambda e: e.scalar_tensor_tensor(out=t4[:, q, 0:N], in0=ti[:, 0:N], scalar=1.0, in1=xc_ap,
                                                             op0=ALU.add, op1=ALU.mult), reads=("ti", ck), writes=("t4_%d" % q,))

            def st4(c0):
                sk = tuple("s4_%d" % q for q in range(4))
                S.op("act", lambda e: e.activation(out=s4[:, :, 0:N], in_=s4[:, :, 0:N], func=AF.Sqrt, scale=-0.25, bias=qtr[:, 0:1]),
                     reads=sk + ("qtr",), writes=sk)
                for c in range(c0, c0 + 4):
                    q = c % 4
                    S.op("pool", lambda e, q=q: e.tensor_tensor(out=bb_t[:, 0:N], in0=s4[:, q, 0:N], in1=t4[:, q, 0:N], op=ALU.mult),
                         reads=("s4_%d" % q, "t4_%d" % q), writes=("bb_t",))
                    if reverse:
                        S.op("dve", lambda e, q=q, c=c: e.tensor_tensor_scan(
                            out=hf[:, 0:N][:, ::-1], data0=a4[:, q, 0:N][:, ::-1], data1=bb_t[:, 0:N][:, ::-1],
                            initial=state[:, d, c:c + 1], op0=ALU.mult, op1=ALU.add),
                            reads=("a4_%d" % q, "bb_t", "state"), writes=("hf",))
                        S.op("pool", lambda e, c=c: e.tensor_copy(out=state[:, d, c:c + 1], in_=hf[:, 0:1]),
                             reads=("hf",), writes=("state",))
                    else:
                        S.op("dve", lambda e, q=q, c=c: e.tensor_tensor_scan(
                            out=hf[:, 0:N], data0=a4[:, q, 0:N], data1=bb_t[:, 0:N], initial=state[:, d, c:c + 1],
                            op0=ALU.mult, op1=ALU.add), reads=("a4_%d" % q, "bb_t", "state"), writes=("hf",))
                        S.op("pool", lambda e, c=c: e.tensor_copy(out=state[:, d, c:c + 1], in_=hf[:, N - 1:N]),
                             reads=("hf",), writes=("state",))
                    if consumer is not None:
                        consumer(c)

            for s_ in range(10):
                if s_ < 8 and xc_src is None:
                    st1(s_)
                if 1 <= s_ <= 8 and xc_src is None:
                    st2(s_ - 1)
                if 2 <= s_ <= 9:
                    st3(s_ - 2)
                    if (s_ - 2) % 4 == 3:
                        st4(s_ - 2 - 3)

        hxT, hxk, hxhk = hxTs[0], "hxT0", "hxTh0"
        prep(ctxp, 2, 256, s1c, 1, "xt")
        hbc[0] = 1
        rglru_block(256, 0, False, False, False)
        rglru_block(256, 1, True, False, False)
        for blk in range(15, -1, -1):
            _i = hbc[0] % 2
            hbc[0] += 1
            hxT, hxk, hxhk = hxTs[_i], "hxT%d" % _i, "hxTh%d" % _i
            prep(xp, 2 + NB * blk, NB, s1, 0, "xt")
            def cons_a(c):
                S.op("dve", lambda e, c=c: e.tensor_copy(out=hsb[:, c, :], in_=hf[:]), reads=("hf",), writes=("hsb",))
            rglru_block(NB, 1, True, blk != 0, blk != 15, cons_a if blk < 8 else None, xc_store=(blk if blk < 8 else None))
            if blk < 8:
                S.dma("sp", hs_scr[blk], hsb[:], reads=("hsb",), writes=("hs_scr%d" % blk,), key="hs_scr")

        pb_ = ExitStack()
        cwh = cw_sb
        S.op("dve", lambda e: e.tensor_scalar_mul(out=cwh[:], in0=cw_sb[:], scalar1=0.5), reads=("params",), writes=("cwh",))
        lnr = sb("lnr", [128, NB], F32, pb_)
        lmr = sb("lmr", [128, NB], F32, pb_)
        wsl = [sb("wsl%d" % i, [128, 8, 512], BF16, pb_) for i in range(3)]
        dgc = [sb("dgc0", [128, 31, 128], BF16, pb_)] * 2
        tv, uu = tr, ti
        zb = [sb("zb0", [128, NB], BF16, pb_)] * 2
        zc = sb("zc", [128, 8, NB], BF16, pb_)
        aa = zc
        zsq = [sb("zsq0", [128, NB], BF16, pb_)] * 2
        A_t = sb("A_t", [128, 8, NB], BF16, pb_)
        gy = sb("gy", [128, 8, NB], BF16, pb_)
        mg = gy
        yb = sb("yb", [128, 8, NB], BF16, pb_)
        hs_in = hsb
        lnt0, lnt1 = xh128[:, 0:512], xh128[:, 512:1024]
        tg = xnh128[:].bitcast(F32)
        first_ln = [True]
        xc_in = dg5[:].rearrange("p c j m -> p (c j m)")[:, 0:8 * NB].rearrange("p (c t) -> p c t", t=NB)
        x1 = xt

        xres = [sb("xres%d" % i, [128, 512], F32, pb_) for i in range(3)]
        xrc = [0]
        wctr = [0]

        def wpiece(col0, src=None):
            src = w_in if src is None else src
            i = wctr[0] % 3
            wctr[0] += 1
            S.dma("pool", wsl[i][:], src[:, col0:col0 + 512].rearrange("(k p) n -> p k n", p=128),
                  writes=("wsl%d" % i,), key="wsl%d" % i)
            return wsl[i], "wsl%d" % i

        def inproj(dstbank, wt, wk, j4):
            S.group("pe", [lambda e, k=k: e.matmul(bank(dstbank), lhsT=wt[:, k, 128 * j4:128 * j4 + 128], rhs=hxT[:, k, 0:NB],
                                                   start=(k == 0), stop=(k == 7)) for k in range(8)],
                    reads=(hxk, wk), writes=(pk(dstbank),))

        xr_evac_eng[0] = "act"
        for blk in range(8):
            _i = hbc[0] % 2
            hbc[0] += 1
            hxT, hxk, hxhk = hxTs[_i], "hxT%d" % _i, "hxTh%d" % _i
            prep(xp, 2 + NB * blk, NB, s1, 0, "xt", halo=False)
            S.dma("sp", xc_in[:], xc_scr[blk], reads=("xc_scr%d" % blk,), writes=("dg5",), key="xc_in")
            S.dma("sp", hs_in[:], hs_scr[blk], reads=("hs_scr%d" % blk,), writes=("hsb",), key="hs_in")
            for half in range(2):
                wu, wuk = wpiece(512 * half)
                wv, wvk = wpiece(1024 + 512 * half)
                for c4 in range(4):
                    c = 4 * half + c4
                    par = c % 2
                    inproj(par, wu, wuk, c4)
                    inproj(2 + par, wv, wvk, c4)
                    S.op("act", lambda e, c=c, par=par: e.activation(out=tv[:], in_=bank(2 + par), func=AF.Tanh, scale=0.5,
                                                                     bias=hb_in[:, 8 + c:9 + c]),
                         reads=(pk(2 + par), "hb_in"), writes=("tr",))
                    S.op("act", lambda e, c=c, par=par: e.activation(out=uu[:], in_=bank(par), func=AF.Identity,
                                                                     bias=b_in_sb[:, c:c + 1]),
                         reads=(pk(par), "params"), writes=("ti",))
                    zk = "zb0"
                    S.op("dve", lambda e, par=par: e.scalar_tensor_tensor(out=zb[par][:], in0=tv[:], scalar=1.0, in1=uu[:],
                                                                          op0=ALU.add, op1=ALU.mult),
                         reads=("tr", "ti"), writes=(zk,))
                    dk = "dgc0"
                    S.op("dve", lambda e, c=c, par=par: e.tensor_tensor(
                        out=dgc[par][:], in0=identb[:].unsqueeze(1).to_broadcast([128, 31, 128]),
                        in1=cwh[:, c, :].unsqueeze(2).to_broadcast([128, 31, 128]), op=ALU.mult),
                        reads=("identb", "cwh"), writes=(dk,))
                    zv = zb[par][:].rearrange("p (r t) -> p r t", t=64)
                    pcv = bank(4 + par).rearrange("p (r t) -> p r t", t=64)
                    fns = []
                    order = [15] + [k for k in range(31) if k != 15]
                    for idx, k in enumerate(order):
                        o = k - 15
                        t0, t1 = max(0, -o), 64 - max(0, o)
                        fns.append(lambda e, k=k, o=o, t0=t0, t1=t1, idx=idx, par=par, pcv=pcv, zv=zv: e.matmul(
                            pcv[:, :, t0:t1], lhsT=dgc[par][:, k, :], rhs=zv[:, :, t0 + o:t1 + o],
                            start=(idx == 0), stop=(idx == 30)))
                    S.group("pe", fns, reads=(zk, dk), writes=(pk(4 + par),))
                    S.op("act", lambda e, c=c, par=par: e.activation(out=zc[:, c, :], in_=bank(4 + par), func=AF.Identity,
                                                                     bias=cb_sb[:, c:c + 1]),
                         reads=(pk(4 + par), "params"), writes=("zc",))
                    qk = "zsq0"
                    S.op("act", lambda e, c=c, par=par: e.activation(out=zsq[par][:], in_=bank(4 + par), func=AF.Square,
                                                                     bias=cb_sb[:, c:c + 1]),
                         reads=(pk(4 + par), "params"), writes=(qk,))
                    S.group("pe", [lambda e, c=c: e.matmul(bank(6), lhsT=ones_m[:], rhs=zc[:, c, :], start=(c == 0), stop=(c == 7))],
                            reads=("zc", "ones_m"), writes=("ps6",))
                    S.group("pe", [lambda e, c=c, par=par: e.matmul(bank(7), lhsT=ones_m[:], rhs=zsq[par][:], start=(c == 0),
                                                                    stop=(c == 7))], reads=(qk, "ones_m"), writes=("ps7",))
            S.op("act", lambda e: e.activation(out=tv[:], in_=bank(6), func=AF.Copy), reads=("ps6",), writes=("tr",))
            S.op("dve", lambda e: e.tensor_tensor(out=uu[:], in0=tv[:], in1=tv[:], op=ALU.mult), reads=("tr",), writes=("ti",))
            S.op("dve", lambda e: e.tensor_tensor(out=lnr[:], in0=bank(7), in1=uu[:], op=ALU.subtract), reads=("ps7", "ti"),
                 writes=("lnr",))
            S.op("act", lambda e: e.activation(out=lnr[:], in_=lnr[:], func=AF.Sqrt, bias=qtr[:, 2:3]), reads=("lnr", "qtr"), writes=("lnr",))
            S.op("dve", lambda e: e.reciprocal(out=lnr[:], in_=lnr[:]), reads=("lnr",), writes=("lnr",))
            S.op("dve", lambda e: e.tensor_tensor(out=lmr[:], in0=tv[:], in1=lnr[:], op=ALU.mult), reads=("tr", "lnr"),
                 writes=("lmr",))
            for half in range(2):
                wy, wyk = wpiece(2048 + 512 * half)
                for c4 in range(4):
                    c = 4 * half + c4
                    par = c % 2
                    inproj(par, wy, wyk, c4)
                    S.op("act", lambda e, c=c, par=par: e.activation(out=gy[:, c, :], in_=bank(par), func=AF.Gelu_apprx_tanh,
                                                                     bias=b_in_sb[:, 16 + c:17 + c]),
                         reads=(pk(par), "params"), writes=("gy",))
            def cons_b(c):
                S.op("dve", lambda e, c=c: e.tensor_tensor(out=tmp1[:], in0=hf[:], in1=hs_in[:, c, :], op=ALU.add),
                     reads=("hf", "hsb"), writes=("tmp1",))
                S.op("dve", lambda e, c=c: e.tensor_tensor(out=yb[:, c, :], in0=tmp1[:], in1=gy[:, c, :], op=ALU.mult),
                     reads=("tmp1", "gy"), writes=("yb",))
            rglru_block(NB, 0, False, blk != 0, True, cons_b, xc_src=xc_in)
            for c in range(8):
                extra = ("xh", "xnh") if first_ln[0] else ()
                first_ln[0] = False
                S.op("dve", lambda e, c=c: e.tensor_tensor(out=lnt0, in0=zc[:, c, :], in1=lnr[:], op=ALU.mult),
                     reads=("zc", "lnr"), writes=("lnt0",) + extra)
                S.op("dve", lambda e: e.tensor_tensor(out=lnt1, in0=lnt0, in1=lmr[:], op=ALU.subtract), reads=("lnt0", "lmr"),
                     writes=("lnt1",))
                S.op("act", lambda e, c=c: e.activation(out=aa[:, c, :], in_=lnt1, func=AF.Silu, scale=lng_sb[:, c:c + 1],
                                                        bias=lnb_sb[:, c:c + 1]), reads=("lnt1", "params"), writes=("zc",))
            for half in range(2):
                wga, wgak = wpiece(4096 + 512 * half)
                wpa, wpak = wpiece(512 * half, w_pa)
                for m4 in range(4):
                    m = 4 * half + m4
                    par = m % 2
                    S.group("pe", [lambda e, k=k, m4=m4, par=par, wpa=wpa: e.matmul(bank(par), lhsT=wpa[:, k, 128 * m4:128 * m4 + 128],
                                                                         rhs=aa[:, k, :], start=(k == 0), stop=(k == 7))
                                   for k in range(8)], reads=("zc", wpak), writes=(pk(par),))
                    inproj(2 + par, wga, wgak, m4)
                    S.op("act", lambda e, m=m, par=par: e.activation(out=tg, in_=bank(2 + par), func=AF.Tanh, scale=0.5,
                                                                     bias=hb_in[:, 32 + m:33 + m]),
                         reads=(pk(2 + par), "hb_in"), writes=("tg",))
                    S.op("dve", lambda e, m=m, par=par: e.scalar_tensor_tensor(out=A_t[:, m, :], in0=tg, scalar=1.0,
                                                                               in1=bank(par), op0=ALU.add, op1=ALU.mult),
                         reads=("tg", pk(par)), writes=("A_t",))
            for half in range(2):
                wgb_, wgbk = wpiece(5120 + 512 * half)
                wpb, wpbk = wpiece(512 * half, w_pb)
                for m4 in range(4):
                    m = 4 * half + m4
                    par = m % 2
                    S.group("pe", [lambda e, k=k, m4=m4, par=par, wpb=wpb: e.matmul(bank(par), lhsT=wpb[:, k, 128 * m4:128 * m4 + 128],
                                                                         rhs=yb[:, k, :], start=(k == 0), stop=(k == 7))
                                   for k in range(8)], reads=("yb", wpbk), writes=(pk(par),))
                    inproj(2 + par, wgb_, wgbk, m4)
                    S.op("act", lambda e, m=m, par=par: e.activation(out=tv[:], in_=bank(2 + par), func=AF.Tanh, scale=0.5,
                                                                     bias=hb_in[:, 40 + m:41 + m]),
                         reads=(pk(2 + par), "hb_in"), writes=("tr",))
                    S.op("dve", lambda e, par=par: e.scalar_tensor_tensor(out=uu[:], in0=tv[:], scalar=1.0, in1=bank(par),
                                                                          op0=ALU.add, op1=ALU.mult),
                         reads=("tr", pk(par)), writes=("ti",))
                    S.op("dve", lambda e, m=m: e.tensor_tensor(out=mg[:, m, :], in0=uu[:], in1=A_t[:, m, :], op=ALU.add),
                         reads=("ti", "A_t"), writes=("gy",))
            for hh in range(2):
                wo, wok = wpiece(512 * hh, w_o)
                for j in range(4):
                    bk = 4 + j
                    xi = xrc[0] % 3
                    xrc[0] += 1
                    xk_ = "xres%d" % xi
                    r0_ = 2 + NB * blk + 128 * j
                    S.dma("sp", xres[xi][:], xp[r0_:r0_ + 128, 512 * hh:512 * hh + 512], writes=(xk_,), key=xk_)
                    S.group("pe", [lambda e, k=k, j=j, bk=bk, wo=wo: e.matmul(bank(bk), lhsT=mg[:, k, 128 * j:128 * j + 128],
                                                                             rhs=wo[:, k, :], start=(k == 0), stop=(k == 7))
                                   for k in range(8)], reads=("gy", wok), writes=(pk(bk),))
                    S.op("dve", lambda e, hh=hh, bk=bk: e.tensor_tensor(out=tmp1[:], in0=bank(bk), in1=gt1h[:, 512 * hh:512 * hh + 512],
                                                                        op=ALU.mult), reads=(pk(bk), "gt"), writes=("tmp1",))
                    S.op("pool", lambda e, xi=xi: e.tensor_tensor(out=xres[xi][:], in0=xres[xi][:], in1=tmp1[:], op=ALU.add),
                         reads=("tmp1", xk_), writes=(xk_,))
                    S.dma("sp", x1_scr[NB * blk + 128 * j:NB * blk + 128 * j + 128, 512 * hh:512 * hh + 512], xres[xi][:],
                          reads=(xk_,), writes=("x1_scr%d" % blk,), key="x1_scr", nowaw=True)
        S.barrier()
        pb_.close()
        mixer.close()

        def bc(ap, shape):
            return ap.to_broadcast(shape)

        pcg = ExitStack()
        gf32 = sb("gf32", [128, 1024], F32, pcg)
        gt2b = sb("gt2b", [128, 1024], F32, pcg)
        S.dma("sp", gf32[:], gfin, writes=("gf32",), key="gf32")
        S.dma("sp", gt2b[:], gt2_scr, reads=("gt2_scr",), writes=("gt2b",), key="gt2b")
        S.op("dve", lambda e: e.tensor_scalar_mul(out=gf32[:], in0=gf32[:], scalar1=32.0), reads=("gf32",), writes=("gf32",))
        slot_i = sb("slot_i", [128, 32], I32, pcg)
        offE_i = sb("offE_i", [128, NSEG, 4], I32, pcg)
        trib = sb("trib", [128, 128], BF16, pcg)
        S.dma("pool", trib[:], tri, writes=("trib",), key="trib")

        c1 = ExitStack()
        x1l = [sb("x1l%d" % i, [128, 4, 1024], F32, c1) for i in range(2)]
        xn2_all = sb("xn2_all", [128, 32, 1024], BF16, c1)
        hmT1 = sb("hmTr", [128, 8, NB], BF16, c1)
        zt = sb("zt", [128, 4096], BF16, c1)
        ztf = sb("ztf", [128, 192], F32, c1)
        ssA = sb("ssA", [128, 8, 4], F32, c1)
        rsA = sb("rsA", [128, 8, 4], F32, c1)
        oh_all = sb("oh_all", [128, 32, 4], F32, c1)
        wsel_all = sb("wsel_all", [128, 32, 4], F32, c1)
        oh_bf = sb("oh_bf", [128, 32, 4], BF16, c1)
        R1s = sb("R1s", [128, 32, 4], F32, c1)
        Cs = sb("Cs", [128, 32, 4], F32, c1)
        incl = sb("incl", [128, 4, 32], F32, c1)
        onesf = sb("onesf", [128, 32], F32, c1)
        ng = sb("ng", [128, 4], F32, c1)
        nseg = sb("nseg", [128, 4], F32, c1)
        sst = sb("sst", [128, 4], F32, c1)
        sen = sb("sen", [128, 4], F32, c1)
        slot_f = sb("slot_f", [128, 32], F32, c1)
        Gs = sb("Gs", [128, NSEG], F32, c1)
        sidx_sb = sb("sidx_sb", [128, NSEG], F32, c1)
        cE_sb = sb("cE_sb", [128, 4], F32, c1)
        offE_f = sb("offE_f", [128, NSEG, 4], F32, c1)
        L = sb("L", [128, 32, 20], F32, c1)
        gmax = sb("gmax", [128, 32, 1], F32, c1)
        eg = sb("eg", [128, 32, 4], F32, c1)
        pg = sb("pg", [128, 32, 1], F32, c1)
        tmp16 = sb("tmp16", [128, 32, 16], F32, c1)
        esel = sb("esel", [128, 32, 4], F32, c1)
        m1 = sb("m1", [128, 32, 1], F32, c1)
        m2 = sb("m2", [128, 32, 1], F32, c1)
        k1 = sb("k1", [128, 32, 4], F32, c1)
        k2 = sb("k2", [128, 32, 4], F32, c1)
        e2 = sb("e2", [128, 32, 4], F32, c1)
        w1 = sb("w1", [128, 32, 1], F32, c1)
        w2 = sb("w2", [128, 32, 1], F32, c1)
        S.dma("sp", sidx_sb[:], sidx, writes=("cidx",), key="cidx")
        S.dma("sp", cE_sb[:], cE, writes=("cidx",), key="cidx")
        S.op("pool", lambda e: e.memset(zt[:], 0.0), writes=("zt",))
        S.op("pool", lambda e: e.memset(ztf[:], 0.0), writes=("zt",))
        S.op("pool", lambda e: e.memset(onesf[:], 1.0), writes=("onesf",))
        for sg_ in range(NSEG):
            S.dma("sp", xs_sorted[NB * sg_:NB * sg_ + NB, :].rearrange("(p r) d -> p (r d)", r=4), zt[:],
                  reads=("zt",), writes=("xs_sorted",), key="xs_z", nowaw=True)
        S.dma("sp", ws_sorted.rearrange("(p r) c -> p (r c)", r=48), ztf[:], reads=("zt",), writes=("ws_sorted",), key="xs_z")

        for blk in range(8):
            pb2 = blk % 2
            x1t = x1l[pb2]
            ak = "x1l%d" % pb2
            sak = "ssA%d" % blk
            S.dma("sp", x1t[:], x1_scr[NB * blk:NB * blk + NB, :].rearrange("(j p) d -> p j d", p=128),
                  reads=("x1_scr%d" % blk,), writes=(ak,), key=ak)
            S.op("pool", lambda e, blk=blk: e.memset(ssA[:, blk, :], 0.0), writes=(sak,))
            for j in range(4):
                S.op("act", lambda e, j=j, x1t=x1t, blk=blk: e.activation(out=xn2_all[:, 4 * blk + j, :], in_=x1t[:, j, :], func=AF.Square,
                                                                          accum_out=ssA[:, blk, j:j + 1]),
                     reads=(ak,), writes=("xn2_%d" % blk, sak))
            S.op("act", lambda e, blk=blk: e.activation(out=rsA[:, blk, :], in_=ssA[:, blk, :], func=AF.Sqrt, bias=qtr[:, 1:2]),
                 reads=(sak, "qtr"), writes=(sak + "r",))
            S.op("dve", lambda e, blk=blk: e.reciprocal(out=rsA[:, blk, :], in_=rsA[:, blk, :]), reads=(sak + "r",), writes=(sak + "r",))
            for j in range(4):
                S.op("dve", lambda e, j=j, x1t=x1t, blk=blk: e.tensor_scalar_mul(out=xn2_all[:, 4 * blk + j, :], in0=x1t[:, j, :],
                                                                                 scalar1=rsA[:, blk, j:j + 1]),
                     reads=(ak, sak + "r"), writes=("xn2_%d" % blk,))
            for j in range(4):
                S.group("pe", [lambda e, j=j, c=c, blk=blk: e.transpose(out=tpv[:, c, 128 * j:128 * j + 128],
                                                                        in_=xn2_all[:, 4 * blk + j, 128 * c:128 * c + 128], identity=identb[:])
                               for c in range(8)], reads=("xn2_%d" % blk, "identb"), writes=TPK)
            for c in range(8):
                S.op("act", lambda e, c=c: e.activation(out=hmT1[:, c, :], in_=tpv[:, c, :], func=AF.Identity,
                                                        scale=s2[:, c:c + 1], bias=mods[:, 24 + c, 0:1]),
                     reads=TPK + ("sc", "mods"), writes=("hmT1",))
            for j in range(4):
                S.group("pe", [lambda e, k=k, j=j: e.matmul(bank(4)[:, 20 * j:20 * j + 20], lhsT=hmT1[:, k, 128 * j:128 * j + 128],
                                                            rhs=w_rt_b[:, k, :], start=(k == 0), stop=(k == 7))
                               for k in range(8)], reads=("hmT1", "w_rt_b"), writes=("ps4",))
            S.op("dve", lambda e, blk=blk: e.tensor_tensor(out=L[:, 4 * blk:4 * blk + 4, :], in0=bank(4)[:, 0:80].rearrange("p (j n) -> p j n", n=20),
                                                  in1=bc(b_rt_sb[:].unsqueeze(1), [128, 4, 20]), op=ALU.add),
                 reads=("ps4", "params"), writes=("L",))

        oh = oh_all
        R = ("rt",)
        OK_ = ("oh_all",)
        S.op("dve", lambda e: e.tensor_reduce(out=gmax[:], in_=L[:, :, 0:4], axis=AX.X, op=ALU.max), reads=("L",), writes=R)
        S.op("dve", lambda e: e.tensor_tensor(out=oh_all[:], in0=L[:, :, 0:4], in1=bc(gmax[:], [128, 32, 4]), op=ALU.is_equal),
             reads=R + ("L",), writes=R + OK_)
        S.op("dve", lambda e: e.tensor_tensor(out=eg[:], in0=L[:, :, 0:4], in1=bc(gmax[:], [128, 32, 4]), op=ALU.subtract),
             reads=R + ("L",), writes=R)
        S.op("act", lambda e: e.activation(out=eg[:], in_=eg[:], func=AF.Exp), reads=R, writes=R)
        S.op("dve", lambda e: e.tensor_reduce(out=pg[:], in_=eg[:], axis=AX.X, op=ALU.add), reads=R, writes=R)
        S.op("dve", lambda e: e.reciprocal(out=pg[:], in_=pg[:]), reads=R, writes=R)
        S.op("dve", lambda e: e.tensor_tensor(out=tmp16[:].rearrange("p j (g x) -> p j g x", x=4),
                                                     in0=L[:, :, 4:20].rearrange("p j (g x) -> p j g x", x=4),
                                                     in1=bc(oh_all[:].unsqueeze(3), [128, 32, 4, 4]), op=ALU.mult),
             reads=R + ("L",), writes=R)
        S.op("dve", lambda e: e.tensor_reduce(out=esel[:].unsqueeze(3), in_=tmp16[:].rearrange("p j (g x) -> p j x g", x=4),
                                              axis=AX.X, op=ALU.add), reads=R, writes=R)
        S.op("dve", lambda e: e.tensor_reduce(out=m1[:], in_=esel[:], axis=AX.X, op=ALU.max), reads=R, writes=R)
        S.op("dve", lambda e: e.tensor_tensor(out=k1[:], in0=esel[:], in1=bc(m1[:], [128, 32, 4]), op=ALU.is_equal),
             reads=R, writes=R)
        S.op("dve", lambda e: e.scalar_tensor_tensor(out=e2[:], in0=k1[:], scalar=-1e30, in1=esel[:], op0=ALU.mult, op1=ALU.add),
             reads=R, writes=R)
        S.op("dve", lambda e: e.tensor_reduce(out=m2[:], in_=e2[:], axis=AX.X, op=ALU.max), reads=R, writes=R)
        S.op("dve", lambda e: e.tensor_tensor(out=k2[:], in0=e2[:], in1=bc(m2[:], [128, 32, 4]), op=ALU.is_equal),
             reads=R, writes=R)
        S.op("dve", lambda e: e.tensor_tensor(out=w2[:], in0=m2[:], in1=m1[:], op=ALU.subtract), reads=R, writes=R)
        S.op("act", lambda e: e.activation(out=w2[:], in_=w2[:], func=AF.Exp), reads=R, writes=R)
        S.op("dve", lambda e: e.tensor_scalar_add(out=w1[:], in0=w2[:], scalar1=1.0), reads=R, writes=R)
        S.op("dve", lambda e: e.reciprocal(out=w1[:], in_=w1[:]), reads=R, writes=R)
        S.op("dve", lambda e: e.tensor_tensor(out=w2[:], in0=w2[:], in1=w1[:], op=ALU.mult), reads=R, writes=R)
        S.op("dve", lambda e: e.tensor_tensor(out=w1[:], in0=w1[:], in1=pg[:], op=ALU.mult), reads=R, writes=R)
        S.op("dve", lambda e: e.tensor_tensor(out=w2[:], in0=w2[:], in1=pg[:], op=ALU.mult), reads=R, writes=R)
        S.op("dve", lambda e: e.tensor_tensor(out=wsel_all[:], in0=k1[:], in1=bc(w1[:], [128, 32, 4]), op=ALU.mult),
             reads=R, writes=R + ("wsel_all",))
        S.op("dve", lambda e: e.tensor_tensor(out=k2[:], in0=k2[:], in1=bc(w2[:], [128, 32, 4]), op=ALU.mult), reads=R, writes=R)
        S.op("dve", lambda e: e.tensor_tensor(out=wsel_all[:], in0=wsel_all[:], in1=k2[:], op=ALU.add), reads=R + ("wsel_all",),
             writes=R + ("wsel_all",))

        ohf = oh_all[:].rearrange("p t g -> p (t g)")
        S.op("dve", lambda e: e.tensor_copy(out=oh_bf[:], in_=oh_all[:]), reads=("oh_all",), writes=("oh_bf",))
        S.group("pe", [lambda e: e.matmul(bank(0)[:, 0:128], lhsT=trib[:], rhs=oh_bf[:].rearrange("p t g -> p (t g)"), start=True, stop=True)],
                reads=("oh_bf", "trib"), writes=("ps0",))
        S.group("pe", [lambda e: e.matmul(bank(1)[:, 0:128], lhsT=ones1[:], rhs=oh_bf[:].rearrange("p t g -> p (t g)"), start=True, stop=True)],
                reads=("oh_bf", "ones1"), writes=("ps1",))
        S.op("act", lambda e: e.activation(out=R1s[:].rearrange("p t g -> p (t g)"), in_=bank(0)[:, 0:128], func=AF.Copy),
             reads=("ps0",), writes=("R1s",))
        S.op("act", lambda e: e.activation(out=Cs[:].rearrange("p t g -> p (t g)"), in_=bank(1)[:, 0:128], func=AF.Copy),
             reads=("ps1",), writes=("Cs",))
        for g in range(4):
            S.op("dve", lambda e, g=g: e.tensor_tensor_scan(out=incl[:, g, :], data0=onesf[:], data1=Cs[:, :, g], initial=0.0,
                                                            op0=ALU.mult, op1=ALU.add), reads=("Cs", "onesf"), writes=("incl",))
        S.op("dve", lambda e: e.tensor_copy(out=ng[:], in_=incl[:, :, 31]), reads=("incl",), writes=("ng",))
        S.op("dve", lambda e: e.tensor_tensor(out=incl[:], in0=incl[:], in1=Cs[:].rearrange("p t g -> p g t"), op=ALU.subtract),
             reads=("incl", "Cs"), writes=("incl",))
        S.op("dve", lambda e: e.memset(nseg[:], 0.0), writes=("nseg",))
        for k in range(8):
            S.op("dve", lambda e, k=k: e.scalar_tensor_tensor(out=nseg[:], in0=ng[:], scalar=float(NB * k), in1=nseg[:],
                                                              op0=ALU.is_gt, op1=ALU.add), reads=("ng", "nseg"), writes=("nseg",))
        S.op("dve", lambda e: e.memset(sst[:], 0.0), writes=("sst",))
        for g in range(1, 4):
            S.op("dve", lambda e, g=g: e.tensor_tensor(out=sst[:, g:g + 1], in0=sst[:, g - 1:g], in1=nseg[:, g - 1:g], op=ALU.add),
                 reads=("sst", "nseg"), writes=("sst",))
        S.op("dve", lambda e: e.tensor_tensor(out=sen[:], in0=sst[:], in1=nseg[:], op=ALU.add), reads=("sst", "nseg"), writes=("sen",))
        S.op("dve", lambda e: e.tensor_scalar_mul(out=sst[:], in0=sst[:], scalar1=float(NB)), reads=("sst", "sen"), writes=("sst",))
        S.op("dve", lambda e: e.tensor_tensor(out=R1s[:], in0=R1s[:], in1=incl[:].rearrange("p g t -> p t g"), op=ALU.add),
             reads=("R1s", "incl"), writes=("R1s",))
        S.op("dve", lambda e: e.tensor_tensor(out=R1s[:], in0=R1s[:], in1=bc(sst[:].unsqueeze(1), [128, 32, 4]), op=ALU.add),
             reads=("R1s", "sst"), writes=("R1s",))
        S.op("dve", lambda e: e.tensor_tensor(out=R1s[:], in0=R1s[:], in1=oh_all[:], op=ALU.mult), reads=("R1s", "oh_all"), writes=("R1s",))
        S.op("dve", lambda e: e.tensor_reduce(out=slot_f[:].unsqueeze(2), in_=R1s[:], axis=AX.X, op=ALU.add), reads=("R1s",), writes=("slot_f",))
        S.op("dve", lambda e: e.tensor_copy(out=slot_i[:], in_=slot_f[:]), reads=("slot_f",), writes=("slot_i",))
        S.op("dve", lambda e: e.memset(Gs[:], 0.0), writes=("Gs",))
        for g in range(3):
            S.op("dve", lambda e, g=g: e.scalar_tensor_tensor(out=Gs[:], in0=sidx_sb[:], scalar=sen[:, g:g + 1], in1=Gs[:],
                                                              op0=ALU.is_ge, op1=ALU.add), reads=("cidx", "sen", "Gs"), writes=("Gs",))
        S.op("dve", lambda e: e.tensor_scalar_mul(out=offE_f[:], in0=bc(Gs[:].unsqueeze(2), [128, NSEG, 4]), scalar1=512.0),
             reads=("Gs",), writes=("offE_f",))
        S.op("dve", lambda e: e.tensor_tensor(out=offE_f[:], in0=offE_f[:], in1=bc(cE_sb[:].unsqueeze(1), [128, NSEG, 4]), op=ALU.add),
             reads=("offE_f", "cidx"), writes=("offE_f",))
        S.op("dve", lambda e: e.tensor_copy(out=offE_i[:], in_=offE_f[:]), reads=("offE_f",), writes=("offE_i",))
        for t in range(32):
            S.idma("pool", xs_sorted[:, :], bass.IndirectOffsetOnAxis(ap=slot_i[:, t:t + 1], axis=0), xn2_all[:, t, :], None,
                   reads=("slot_i", "xn2_%d" % (t // 4), "xs_sorted"), writes=("xs_sorted_s",), key="scat", nowaw=True)
            S.idma("pool", ws_sorted[:, :], bass.IndirectOffsetOnAxis(ap=slot_i[:, t:t + 1], axis=0), wsel_all[:, t, :], None,
                   reads=("slot_i", "wsel_all", "ws_sorted"), writes=("ws_sorted_s",), key="scat", nowaw=True)
        S.barrier()
        c1.close()

        S.ALPHA = 0.0
        c2 = ExitStack()
        xst = [sb("xst%d" % i, [128, 4, 1024], BF16, c2) for i in range(2)]
        wst = [sb("wst%d" % i, [128, 4, 4], F32, c2) for i in range(2)]
        hmTs = [sb("hmT%d" % i, [128, 8, NB], BF16, c2) for i in range(2)]
        cbc = [sb("cbc%d" % i, [128, 4, NB], BF16, c2) for i in range(2)]
        dgm = sb("dgm", [128, 4, 128], BF16, c2)
        actb = [sb("actb%d" % i, [128, NB], BF16, c2) for i in range(16)]
        wgu = [sb("wgu%d" % i, [128, 2, 8, 512], BF16, c2) for i in range(3)]
        NWD = 5
        wd = [sb("wd%d" % i, [128, 4, 1024], BF16, c2) for i in range(NWD)]
        sg = [sb("sg%d" % i, [128, NB], F32, c2) for i in range(2)]
        tt = [sb("tt%d" % i, [128, NB], BF16, c2) for i in range(2)]
        ysb = [sb("ysb%d" % i, [128, 4, 1024], F32, c2) for i in range(2)]
        ectr = [0]
        for sgi in range(NSEG):
            pb2 = sgi % 2
            hmT, hk = hmTs[pb2], "hmT%d" % pb2
            xk2, wk2, ck2, yk2 = "xst%d" % pb2, "wst%d" % pb2, "cbc%d" % pb2, "ysb%d" % pb2
            S.dma("sp", xst[pb2][:], xs_sorted[NB * sgi:NB * sgi + NB, :].rearrange("(j p) d -> p j d", p=128), writes=(xk2,), key=xk2)
            S.dma("sp", wst[pb2][:], ws_sorted[NB * sgi:NB * sgi + NB, :].rearrange("(j p) c -> p j c", p=128), writes=(wk2,), key=wk2)
            for j in range(4):
                S.group("pe", [lambda e, j=j, c=c, pb2=pb2: e.transpose(out=tpv[:, c, 128 * j:128 * j + 128],
                                                                        in_=xst[pb2][:, j, 128 * c:128 * c + 128], identity=identb[:])
                               for c in range(8)], reads=(xk2, "identb"), writes=TPK)
            for c in range(8):
                S.op("act", lambda e, c=c, hmT=hmT: e.activation(out=hmT[:, c, :], in_=tpv[:, c, :], func=AF.Identity,
                                                                 scale=s2[:, c:c + 1], bias=mods[:, 24 + c, 0:1]),
                     reads=TPK + ("sc", "mods"), writes=(hk,))
            for j in range(4):
                S.op("dve", lambda e, j=j, pb2=pb2: e.tensor_tensor(out=dgm[:], in0=bc(identb[:].unsqueeze(1), [128, 4, 128]),
                                                                    in1=bc(wst[pb2][:, j, :].unsqueeze(2), [128, 4, 128]), op=ALU.mult),
                     reads=("identb", wk2), writes=("dgm",))
                S.group("pe", [lambda e: e.matmul(bank(4 + (j % 2)), lhsT=ones1[:], rhs=dgm[:], start=True, stop=True)],
                        reads=("dgm", "ones1"), writes=(pk(4 + (j % 2)),))
                S.op("act", lambda e, j=j, pb2=pb2: e.activation(out=cbc[pb2][:, :, 128 * j:128 * j + 128],
                                                                 in_=bank(4 + (j % 2)).rearrange("p (x t) -> p x t", t=128), func=AF.Copy),
                     reads=(pk(4 + (j % 2)),), writes=(ck2,))
            for el in range(4):
                si = ectr[0] % 3
                di = ectr[0] % NWD
                ectr[0] += 1
                gk, dk_ = "wgu%d" % si, "wd%d" % di
                ofs = bass.IndirectOffsetOnAxis(ap=offE_i[:, sgi, el:el + 1], axis=0)
                S.idma("pool", wgu[si][:, 0].rearrange("p k n -> p (k n)"), None, w_gate[:, :], ofs, reads=("offE_i",), writes=(gk,), key=gk, nowaw=True)
                S.idma("pool", wgu[si][:, 1].rearrange("p k n -> p (k n)"), None, w_up[:, :], ofs, reads=("offE_i",), writes=(gk,), key=gk, nowaw=True)
                S.idma("pool", wd[di][:].rearrange("p k n -> p (k n)"), None, w_down[:, :], ofs, reads=("offE_i",), writes=(dk_,), key=dk_)
                for f in range(4):
                    u = 4 * el + f
                    pp = u % 2
                    S.group("pe", [lambda e, k=k, f=f, si=si, pp=pp, hmT=hmT: e.matmul(
                        bank(2 * pp), lhsT=wgu[si][:, 0, k, 128 * f:128 * f + 128], rhs=hmT[:, k, :],
                        start=(k == 0), stop=(k == 7)) for k in range(8)], reads=(hk, gk), writes=(pk(2 * pp),))
                    S.group("pe", [lambda e, k=k, f=f, si=si, pp=pp, hmT=hmT: e.matmul(
                        bank(2 * pp + 1), lhsT=wgu[si][:, 1, k, 128 * f:128 * f + 128], rhs=hmT[:, k, :],
                        start=(k == 0), stop=(k == 7)) for k in range(8)], reads=(hk, gk), writes=(pk(2 * pp + 1),))
                    S.op("act", lambda e, pp=pp: e.activation(out=sg[pp][:], in_=bank(2 * pp), func=AF.Silu),
                         reads=(pk(2 * pp),), writes=("sg%d" % pp,))
                    S.op("dve", lambda e, pp=pp: e.tensor_tensor(out=tt[pp][:], in0=bank(2 * pp + 1), in1=sg[pp][:], op=ALU.mult),
                         reads=(pk(2 * pp + 1), "sg%d" % pp), writes=("tt%d" % pp,))
                    S.op("dve", lambda e, pp=pp, u=u, el=el, pb2=pb2: e.tensor_tensor(out=actb[u][:], in0=tt[pp][:], in1=cbc[pb2][:, el, :],
                                                                                      op=ALU.mult),
                         reads=("tt%d" % pp, ck2), writes=("actb%d" % u,))
            dbase = ectr[0] - 4
            for tp_ in range(2):
                fns = []
                for u in range(16):
                    el, f = divmod(u, 4)
                    di = (dbase + el) % NWD
                    for jj in range(2):
                        j = 2 * tp_ + jj
                        for hh in range(2):
                            fns.append(lambda e, u=u, f=f, di=di, j=j, jj=jj, hh=hh: e.matmul(
                                bank(4 + 2 * jj + hh), lhsT=actb[u][:, 128 * j:128 * j + 128],
                                rhs=wd[di][:, f, 512 * hh:512 * hh + 512], start=(u == 0), stop=(u == 15)))
                S.group("pe", fns, reads=tuple("actb%d" % u for u in range(16)) + tuple("wd%d" % ((dbase + el) % NWD) for el in range(4)),
                        writes=("ps4", "ps5", "ps6", "ps7"))
                for jj in range(2):
                    j = 2 * tp_ + jj
                    for hh in range(2):
                        bk = 4 + 2 * jj + hh
                        S.op("dve", lambda e, bk=bk, hh=hh, j=j, pb2=pb2: e.tensor_tensor(
                            out=ysb[pb2][:, j, 512 * hh:512 * hh + 512], in0=bank(bk), in1=gt2b[:, 512 * hh:512 * hh + 512], op=ALU.mult),
                            reads=(pk(bk), "gt2b"), writes=(yk2,))
            S.dma("sp", y_sorted[NB * sgi:NB * sgi + NB, :].rearrange("(j p) d -> p j d", p=128), ysb[pb2][:],
                  reads=(yk2,), writes=("y_sorted",), key="y_sorted")
        S.barrier()
        c2.close()

        S.ALPHA = 0.05
        c3 = ExitStack()
        yg = [sb("yg%d" % i, [128, 4, 1024], F32, c3) for i in range(2)]
        x1b = [sb("x1b%d" % i, [128, 4, 1024], F32, c3) for i in range(2)]
        junkF = sb("junkF", [128, 1024], BF16, c3)
        ssF = sb("ssF", [128, 8, 4], F32, c3)
        rsF = sb("rsF", [128, 8, 4], F32, c3)
        for blk in range(8):
            pb2 = blk % 2
            yk3, xk3, sfk = "yg%d" % pb2, "x1b%d" % pb2, "ssF%d" % blk
            S.dma("sp", x1b[pb2][:], x1_scr[NB * blk:NB * blk + NB, :].rearrange("(j p) d -> p j d", p=128), writes=(xk3,), key=xk3)
            for j in range(4):
                S.idma("pool", yg[pb2][:, j, :], None, y_sorted[:, :], bass.IndirectOffsetOnAxis(ap=slot_i[:, 4 * blk + j:4 * blk + j + 1], axis=0),
                       reads=("slot_i",), writes=(yk3,), key=yk3, nowaw=True)
            S.op("pool", lambda e, blk=blk: e.memset(ssF[:, blk, :], 0.0), writes=(sfk,))
            for j in range(4):
                S.op("dve", lambda e, j=j, pb2=pb2: e.tensor_tensor(out=x1b[pb2][:, j, :], in0=x1b[pb2][:, j, :], in1=yg[pb2][:, j, :], op=ALU.add),
                     reads=(xk3, yk3), writes=(xk3,))
                S.op("act", lambda e, j=j, pb2=pb2, blk=blk: e.activation(out=junkF[:], in_=x1b[pb2][:, j, :], func=AF.Square,
                                                                          accum_out=ssF[:, blk, j:j + 1]),
                     reads=(xk3,), writes=("junkF", sfk))
            S.op("act", lambda e, blk=blk: e.activation(out=rsF[:, blk, :], in_=ssF[:, blk, :], func=AF.Sqrt, bias=qtr[:, 1:2]),
                 reads=(sfk, "qtr"), writes=(sfk + "r",))
            S.op("dve", lambda e, blk=blk: e.reciprocal(out=rsF[:, blk, :], in_=rsF[:, blk, :]), reads=(sfk + "r",), writes=(sfk + "r",))
            for j in range(4):
                S.op("dve", lambda e, j=j, pb2=pb2, blk=blk: e.scalar_tensor_tensor(out=x1b[pb2][:, j, :], in0=x1b[pb2][:, j, :],
                                                                                    scalar=rsF[:, blk, j:j + 1], in1=gf32[:],
                                                                                    op0=ALU.mult, op1=ALU.mult),
                     reads=(xk3, sfk + "r", "gf32"), writes=(xk3,))
            S.dma("sp", out[NB * blk:NB * blk + NB, :].rearrange("(j p) d -> p j d", p=128), x1b[pb2][:],
                  reads=(xk3,), writes=("out%d" % blk,), key="out")
        S.barrier()
        c3.close()
        pcg.close()
    return nc


_NC_CACHE = {}


def _fm(v):
    v = np.asarray(v, np.float32).reshape(-1, 128)
    return np.ascontiguousarray(v.T)


def kernel(x, c, ctx, c_ctx, w_ada, b_ada, g_mix, w_in, b_in, conv_w, conv_b, ln_g, ln_b, w_pa,
           lru_conv_w, lru_conv_b, w_r_f, b_r_f, w_i_f, b_i_f, lam_f, w_r_b, b_r_b, w_i_b, b_i_b, lam_b,
           w_pb, w_o, g_ffn, w_grp, b_grp, w_er, b_er, w_gate, w_up, w_down, g_final):
    f = lambda a: np.ascontiguousarray(np.asarray(a, np.float32))
    x, c, ctx, c_ctx = f(x), f(c), f(ctx), f(c_ctx)
    B = x.shape[0]
    if "nc" not in _NC_CACHE:
        _NC_CACHE["nc"] = build_program()
    nc = _NC_CACHE["nc"]

    common = {
        "w_ada": f(w_ada[0]), "b_ada_fm": _fm(b_ada[0]),
        "b_ada_gt": f(np.broadcast_to(np.stack([b_ada[0][2048:3072], b_ada[0][5120:6144]])[None], (128, 2, 1024))),
        "w_in": f(w_in[0]), "b_in_fm": _fm(b_in[0]),
        "cb": _fm(conv_b[0]), "lng": _fm(ln_g[0]), "lnb": _fm(ln_b[0]),
        "w_pa": f(w_pa[0]), "w_pb": f(w_pb[0]), "w_o": f(w_o[0]),
        "lb": _fm(lru_conv_b[0]),
        "gmix": _fm(g_mix[0]), "gffn": _fm(g_ffn[0]),
        "gfin": f(np.broadcast_to(np.asarray(g_final, np.float32)[None], (128, 1024))),
        "w_rt": f(np.concatenate([w_grp[0], w_er[0]], axis=1)),
        "b_rt": f(np.broadcast_to(np.concatenate([b_grp[0], b_er[0]])[None], (128, 20))),
        "w_gate": f(np.asarray(w_gate[0], np.float32).reshape(16, 8, 128, 512).transpose(0, 2, 1, 3).reshape(2048, 4096)),
        "w_up": f(np.asarray(w_up[0], np.float32).reshape(16, 8, 128, 512).transpose(0, 2, 1, 3).reshape(2048, 4096)),
        "w_down": f(np.asarray(w_down[0], np.float32).reshape(16, 4, 128, 1024).transpose(0, 2, 1, 3).reshape(2048, 4096)),
        "ident": np.eye(128, dtype=np.float32),
        "tri": np.triu(np.ones((128, 128), np.float32), 1),
        "cE": (np.arange(4)[None, :] * 128 + np.arange(128)[:, None]).astype(np.float32),
        "sidx": np.broadcast_to(np.arange(12, dtype=np.float32)[None], (128, 12)).copy(),
    }
    cwn = np.asarray(conv_w[0], np.float32)
    lwn = np.asarray(lru_conv_w[0], np.float32)
    zero = np.zeros((1, 1024), np.float32)
    lw5_nat = np.concatenate([lwn, zero], axis=0)
    lw5_rev = lw5_nat[::-1]

    def fm3(a):
        T = a.shape[0]
        return np.ascontiguousarray(a.reshape(T, 8, 128).transpose(2, 1, 0))

    pf = (w_r_f[0], b_r_f[0], w_i_f[0], b_i_f[0], lam_f[0])
    pbk = (w_r_b[0], b_r_b[0], w_i_b[0], b_i_b[0], lam_b[0])

    def gates(P, Sd):
        wgs = np.stack([P[0], P[2], Sd[0], Sd[2]]).astype(np.float32)
        bgs = np.stack([np.asarray(t, np.float32) for t in (P[1], P[3], Sd[1], Sd[3])])
        bgs = np.ascontiguousarray(bgs.transpose(2, 0, 1))
        lams = np.stack([np.asarray(P[4], np.float32).reshape(8, 128), np.asarray(Sd[4], np.float32).reshape(8, 128)])
        lams = np.ascontiguousarray(lams.transpose(2, 0, 1))
        return f(wgs), bgs, lams

    per_half = []
    for half in range(2):
        if half == 0:
            wgs, bgs, lams = gates(pf, pbk)
            d = {"cw": fm3(cwn), "lw5": fm3(lw5_nat), "wg": wgs, "bg": bgs, "lam": lams}
        else:
            wgs, bgs, lams = gates(pbk, pf)
            d = {"cw": fm3(cwn[::-1]), "lw5": fm3(lw5_rev), "wg": wgs, "bg": bgs, "lam": lams}
        per_half.append(d)

    in_maps = []
    pad2 = np.zeros((2, 1024), np.float32)
    for b in range(B):
        for half in range(2):
            xs = x[b] if half == 0 else x[b, ::-1]
            cs_ = ctx[b] if half == 0 else ctx[b, ::-1]
            m = dict(common)
            m.update(per_half[half])
            m["xp"] = np.ascontiguousarray(np.concatenate([pad2, xs, pad2], axis=0))
            m["ctxp"] = np.ascontiguousarray(np.concatenate([pad2, cs_, pad2], axis=0))
            m["cvec"] = np.ascontiguousarray(np.stack([_fm(c[b]), _fm(c_ctx)], axis=-1))
            in_maps.append(m)
    res = run_bass_kernel_spmd(nc, in_maps, core_ids=list(range(2 * B)))
    outp = np.empty((B, 2 * NOWN, 1024), np.float32)
    for b in range(B):
        outp[b, :NOWN] = res.results[2 * b]["out"]
        outp[b, NOWN:] = res.results[2 * b + 1]["out"][::-1]
    if DEBUG:
        kernel.last = res
    return outp
```

```python
from contextlib import ExitStack
import os
import numpy as np
import concourse.bass as bass
import concourse.mybir as mybir
from concourse.bass_utils import run_bass_kernel_spmd

F32 = mybir.dt.float32
BF16 = mybir.dt.bfloat16
AF = mybir.ActivationFunctionType
ALU = mybir.AluOpType
AX = mybir.AxisListType
EPS = 1e-6
NB = 512
NOWN = 4096
DEBUG = bool(int(os.environ.get("MK_DEBUG", "0")))


class _Rec:
    def __init__(self):
        self.calls = []

    def __getattr__(self, name):
        def f(*args, **kw):
            self.calls.append((name, args, kw))
            return self
        return f


_TBL = {"Exp": "exp", "Tanh": None, "Identity": None, "Copy": None, "Square": None, "Sqrt": "sqrt", "Silu": "silu",
        "Gelu_apprx_tanh": "gelu", "Ln": "ln"}


def _fsize(ap):
    n = 1
    for d in ap.shape[1:]:
        n *= int(d)
    return n


class Sched:
    REORDER = True
    WINDOW = 600
    ALPHA = 0.05

    def __init__(self, nc, es):
        self.nc = nc
        self.es = es
        self.E = dict(pe=nc.tensor, act=nc.scalar, dve=nc.vector, pool=nc.gpsimd, sp=nc.sync)
        self.sem = {e: es.enter_context(nc.semaphore("c_" + e)) for e in self.E}
        self.cnt = {e: 0 for e in self.E}
        self.seen = {e: {} for e in self.E}
        self.lastw = {}
        self.readers = {}
        self.dsem = {}
        self.dcnt = {}
        self.ops = []
        self.lw_nowaw = {}

    def op(self, e, fn, reads=(), writes=()):
        r = _Rec()
        fn(r)
        self._add(e, "op", r.calls, tuple(reads), tuple(writes), None)

    def group(self, e, fns, reads=(), writes=()):
        r = _Rec()
        for f in fns:
            f(r)
        self._add(e, "op", r.calls, tuple(reads), tuple(writes), None)

    def dma(self, q, out, in_, reads=(), writes=(), key=None, nowaw=False):
        self._add(q, "dma", [("dma_start", (), dict(out=out, in_=in_))], tuple(reads), tuple(writes), key, nowaw)

    def idma(self, q, out, out_offset, in_, in_offset, reads=(), writes=(), key=None, nowaw=False):
        self._add(q, "dma", [("indirect_dma_start", (), dict(out=out, out_offset=out_offset, in_=in_, in_offset=in_offset))],
                  tuple(reads), tuple(writes), key, nowaw)

    def _add(self, e, kind, calls, reads, writes, key, nowaw=False):
        dur = 0.0
        tbl = None
        if kind == "dma":
            kw0 = calls[0][2]
            side = kw0["in_"] if kw0.get("out_offset") is not None else kw0["out"]
            nb = 128 * _fsize(side) * 4
            dur = 1000.0 if e == "pool" else 150.0
            lat = 2000.0 + nb / 300.0
        else:
            lat = 0.0
            for (name, args, kw) in calls:
                if e == "pe":
                    src = kw.get("rhs", kw.get("in_"))
                    dur += 12.0 + 0.45 * max(_fsize(src), 110)
                else:
                    oap = kw.get("out", kw.get("ap", args[0] if args else None))
                    n = _fsize(oap)
                    if e == "act":
                        dur += 250.0 + 0.73 * n
                        fnm = kw.get("func")
                        tbl = _TBL.get(getattr(fnm, "name", str(fnm)), None) if fnm is not None else None
                    elif e == "dve":
                        dur += 160.0 + 1.04 * n
                    else:
                        dur += 300.0 + 3.1 * n
        self.ops.append(dict(e=e, kind=kind, calls=calls, reads=reads, writes=writes, key=key, dur=dur, lat=lat, tbl=tbl, nowaw=nowaw))

    def flush(self):
        ops = self.ops
        self.ops = []
        n = len(ops)
        if n == 0:
            return
        lastw, readers = {}, {}
        preds = [None] * n
        succs = [[] for _ in range(n)]
        wkind = {}
        gdeps = {}
        for i, o in enumerate(ops):
            p = set()
            for k in o["reads"]:
                p.update(lastw.get(k, ()))
            for k in o["writes"]:
                grp = lastw.get(k, ())
                joins = o["nowaw"] and wkind.get(k) == o["key"] and not readers.get(k)
                if not joins:
                    p.update(grp)
                    p.update(readers.get(k, ()))
                    gdeps[k] = tuple(grp) + tuple(readers.get(k, ()))
                else:
                    p.update(gdeps.get(k, ()))
            p.discard(i)
            preds[i] = p
            for j in p:
                succs[j].append(i)
            for k in o["reads"]:
                readers.setdefault(k, []).append(i)
            for k in o["writes"]:
                joins = o["nowaw"] and wkind.get(k) == o["key"] and not readers.get(k)
                if joins:
                    lastw[k] = lastw[k] + (i,)
                else:
                    lastw[k] = (i,)
                    wkind[k] = o["key"] if o["nowaw"] else None
                readers[k] = []
        if not self.REORDER:
            order = range(n)
        else:
            indeg = [len(p) for p in preds]
            alpha = float(os.environ.get("MK_PRI", self.ALPHA))
            rank = [0.0] * n
            if alpha > 0:
                for i in range(n - 1, -1, -1):
                    m = 0.0
                    for j in succs[i]:
                        if rank[j] > m:
                            m = rank[j]
                    rank[i] = ops[i]["dur"] + ops[i]["lat"] + m
            ready = [i for i in range(n) if indeg[i] == 0]
            finish = [0.0] * n
            efree = {e: 0.0 for e in self.E}
            etbl = [None]
            done = [False] * n
            lo = 0
            order = []
            while len(order) < n:
                while lo < n and done[lo]:
                    lo += 1
                best, bkey = None, None
                for i in ready:
                    if i > lo + self.WINDOW:
                        continue
                    o = ops[i]
                    st = efree[o["e"]]
                    for j in preds[i]:
                        f = finish[j] + (0.0 if ops[j]["e"] == o["e"] else 120.0)
                        if f > st:
                            st = f
                    if o["e"] == "act" and o["tbl"] is not None and o["tbl"] != etbl[0]:
                        st += 1300.0
                    kk = (st - alpha * rank[i], i) if alpha > 0 else (st, i)
                    if bkey is None or kk < bkey:
                        best, bkey = i, kk
                i = best
                o = ops[i]
                st = bkey[0] + (alpha * rank[i] if alpha > 0 else 0.0)
                if os.environ.get("MK_TL") and n > 3000 and len(ops) == int(os.environ.get("MK_TL")):
                    lim = None
                    for j in preds[i]:
                        f = finish[j]
                        if lim is None or f > lim[0]:
                            lim = (f, j)
                    gap = st - efree[o["e"]]
                    if o["e"] == "pe" and gap > 300:
                        print("PE gap %.1fus at t=%.1fus op#%d writes=%s waits for %s op#%d writes=%s" % (
                            gap / 1e3, st / 1e3, i, o["writes"][:2], ops[lim[1]]["e"], lim[1], ops[lim[1]]["writes"][:2]))
                if o["e"] == "act" and o["tbl"] is not None:
                    etbl[0] = o["tbl"]
                efree[o["e"]] = st + o["dur"]
                finish[i] = st + o["dur"] + o["lat"]
                done[i] = True
                ready.remove(i)
                order.append(i)
                for j in succs[i]:
                    indeg[j] -= 1
                    if indeg[j] == 0:
                        ready.append(j)
        if self.REORDER and os.environ.get("MK_STATS"):
            busy = {e: 0.0 for e in self.E}
            for o in ops:
                busy[o["e"]] += o["dur"]
            print("phase: n=%d est_makespan=%.0fus busy(us): %s" % (n, max(finish) / 1e3, {e: int(v / 1e3) for e, v in busy.items()}))
        for i in order:
            self._emit(ops[i])

    def _wait(self, e, tok, same_ok=False):
        if tok is None:
            return
        name, sem, val, src = tok
        if same_ok and src == e:
            return
        d = self.seen[e]
        if d.get(name, 0) >= val:
            return
        self.E[e].wait_ge(sem, val)
        d[name] = val

    def _emit(self, o):
        e, reads, writes = o["e"], o["reads"], o["writes"]
        for k in reads:
            self._wait(e, self.lastw.get(k))
        for k in writes:
            lw = self.lastw.get(k)
            if not (o["nowaw"] and lw is not None and lw[0] == "d_" + str(o["key"]) and self.lw_nowaw.get(k) and not self.readers.get(k)):
                self._wait(e, lw, same_ok=True)
            for t in self.readers.get(k, {}).values():
                self._wait(e, t, same_ok=True)
        ins = None
        for (name, args, kw) in o["calls"]:
            ins = getattr(self.E[e], name)(*args, **kw)
        if o["kind"] == "dma":
            key = o["key"]
            if key not in self.dsem:
                self.dsem[key] = self.es.enter_context(self.nc.semaphore("d_" + key))
                self.dcnt[key] = 0
            self.dcnt[key] += 16
            ins.then_inc(self.dsem[key], 16)
            tok = ("d_" + key, self.dsem[key], self.dcnt[key], "dma")
        else:
            self.cnt[e] += 1
            ins.then_inc(self.sem[e], 1)
            tok = ("c_" + e, self.sem[e], self.cnt[e], e)
        for k in reads:
            self.readers.setdefault(k, {})[tok[0]] = tok
        for k in writes:
            self.lastw[k] = tok
            self.lw_nowaw[k] = o["nowaw"]
            self.readers[k] = {}

    def barrier(self):
        self.flush()
        for e in self.E:
            for e2 in self.E:
                if self.cnt[e2] > 0:
                    self._wait(e, ("c_" + e2, self.sem[e2], self.cnt[e2], e2))
            for k, sem in self.dsem.items():
                self._wait(e, ("d_" + k, sem, self.dcnt[k], "dma"))


def build_program():
    nc = bass.Bass("TRN2", target_bir_lowering=False)

    def din(name, shape):
        return nc.dram_tensor(name, list(shape), F32, kind="ExternalInput").ap()

    xp = din("xp", [8196, 1024])
    ctxp = din("ctxp", [260, 1024])
    cvec = din("cvec", [128, 8, 2])
    w_ada = din("w_ada", [1024, 6144])
    b_ada_fm = din("b_ada_fm", [128, 48])
    b_ada_gt = din("b_ada_gt", [128, 2, 1024])
    w_in = din("w_in", [1024, 6144])
    b_in_fm = din("b_in_fm", [128, 48])
    cw = din("cw", [128, 8, 31])
    cb = din("cb", [128, 8])
    lng = din("lng", [128, 8])
    lnb = din("lnb", [128, 8])
    w_pa = din("w_pa", [1024, 1024])
    w_pb = din("w_pb", [1024, 1024])
    w_o = din("w_o", [1024, 1024])
    lw5 = din("lw5", [128, 8, 5])
    lb = din("lb", [128, 8])
    wg = din("wg", [4, 8, 128, 128])
    bg = din("bg", [128, 4, 8])
    lam = din("lam", [128, 2, 8])
    gmix = din("gmix", [128, 8])
    gffn = din("gffn", [128, 8])
    gfin = din("gfin", [128, 1024])
    w_rt = din("w_rt", [1024, 20])
    b_rt = din("b_rt", [128, 20])
    w_gate = din("w_gate", [2048, 4096])
    w_up = din("w_up", [2048, 4096])
    w_down = din("w_down", [2048, 4096])
    ident = din("ident", [128, 128])
    tri = din("tri", [128, 128])
    cE = din("cE", [128, 4])
    sidx = din("sidx", [128, 11])
    out = nc.dram_tensor("out", [NOWN, 1024], F32, kind="ExternalOutput").ap()
    if DEBUG:
        hs_scr = nc.dram_tensor("hs_scr", [8, 128, 8, NB], BF16, kind="ExternalOutput").ap()
        x1_scr = nc.dram_tensor("x1_scr", [NOWN, 1024], F32, kind="ExternalOutput").ap()
    else:
        hs_scr = nc.dram_tensor("hs_scr", [8, 128, 8, NB], BF16, kind="Internal").ap()
        x1_scr = nc.dram_tensor("x1_scr", [NOWN, 1024], F32, kind="Internal").ap()
    gt2_scr = nc.dram_tensor("gt2_scr", [128, 1024], F32, kind="Internal").ap()
    xc_scr = nc.dram_tensor("xc_scr", [8, 128, 8, NB], BF16, kind="Internal").ap()
    NSEG = 11
    xs_sorted = nc.dram_tensor("xs_sorted", [NSEG * NB, 1024], BF16, kind="Internal").ap()
    ws_sorted = nc.dram_tensor("ws_sorted", [NSEG * NB, 4], F32, kind="Internal").ap()
    y_sorted = nc.dram_tensor("y_sorted", [NSEG * NB, 1024], F32, kind="Internal").ap()
    I32 = mybir.dt.int32

    with ExitStack() as es:
        S = Sched(nc, es)

        def sb(name, shape, dt=F32, stack=es):
            return stack.enter_context(nc.sbuf_tensor(name, list(shape), dt))

        psA = es.enter_context(nc.psum_tensor("psA", [128, 2048], F32))
        psB = es.enter_context(nc.psum_tensor("psB", [128, 2048], F32))

        def bank(i):
            t = psA if i < 4 else psB
            return t[:, 512 * (i % 4):512 * (i % 4) + 512]

        def pk(i):
            return "ps%d" % i

        tpv = psA[:, :].bitcast(BF16).rearrange("p (c t) -> p c t", t=512)
        TPK = ("ps0", "ps1", "ps2", "ps3")

        identb = sb("identb", [128, 128], BF16)
        ones_m = sb("ones_m", [128, 128], BF16)
        ones1 = sb("ones1", [128, 128], BF16)
        b_in_sb = sb("b_in_sb", [128, 48])
        hb_in = sb("hb_in", [128, 48])
        cw_sb = sb("cw_sb", [128, 8, 31])
        cb_sb = sb("cb_sb", [128, 8])
        lng_sb = sb("lng_sb", [128, 8])
        lnb_sb = sb("lnb_sb", [128, 8])
        lw5_sb = sb("lw5_sb", [128, 8, 5])
        lb_sb = sb("lb_sb", [128, 8])
        bg_sb = sb("bg_sb", [128, 4, 8])
        hbg = sb("hbg", [128, 4, 8])
        lam_sb = sb("lam_sb", [128, 2, 8])
        gmix_sb = sb("gmix_sb", [128, 8])
        gffn_sb = sb("gffn_sb", [128, 8])
        b_rt_sb = sb("b_rt_sb", [128, 20])
        b_ada_fm_sb = sb("b_ada_fm_sb", [128, 48])
        cvec_sb = sb("cvec_sb", [128, 8, 2])
        mods = sb("mods", [128, 48, 2])
        s1 = sb("s1", [128, 8])
        s1c = sb("s1c", [128, 8])
        s2 = sb("s2", [128, 8])
        gt1h = sb("gt1h", [128, 1024])
        cl = sb("cl", [128, 2, 8])
        hcl = sb("hcl", [128, 2, 8])
        state = sb("state", [128, 2, 8])
        ss = sb("ss", [128, 8])
        rs = sb("rs", [128, 8])
        w_rt_b = sb("w_rt_b", [128, 8, 20], BF16)
        qtr = sb("qtr", [128, 4], F32)

        def pload(t, src):
            S.dma("sp", t, src, writes=("params",), key="params", nowaw=True)

        pload(b_in_sb[:], b_in_fm)
        pload(cw_sb[:], cw)
        pload(cb_sb[:], cb)
        pload(lng_sb[:], lng)
        pload(lnb_sb[:], lnb)
        pload(lw5_sb[:], lw5)
        pload(lb_sb[:], lb)
        pload(bg_sb[:], bg)
        pload(lam_sb[:], lam)
        pload(gmix_sb[:], gmix)
        pload(gffn_sb[:], gffn)
        pload(b_rt_sb[:], b_rt)
        pload(b_ada_fm_sb[:], b_ada_fm)
        pload(cvec_sb[:], cvec)
        S.dma("pool", identb[:], ident, writes=("identb",), key="identb")
        S.dma("pool", w_rt_b[:], w_rt.rearrange("(k p) n -> p k n", p=128), writes=("w_rt_b",), key="w_rt_b")
        S.op("pool", lambda e: e.memset(ones_m[:], 1.0 / 1024.0), writes=("ones_m",))
        S.op("pool", lambda e: e.memset(ones1[:], 1.0), writes=("ones1",))
        S.op("pool", lambda e: e.memset(qtr[:, 0:1], 0.25), writes=("qtr",))
        S.op("pool", lambda e: e.memset(qtr[:, 1:2], 1024.0 * EPS), writes=("qtr",))
        S.op("pool", lambda e: e.memset(qtr[:, 2:3], EPS), writes=("qtr",))
        S.op("pool", lambda e: e.memset(state[:], 0.0), writes=("state",))
        S.op("pool", lambda e: e.memset(ss[:], 0.0), writes=("ss",))

        with ExitStack() as p0:
            cs = sb("cs", [128, 8, 2], BF16, p0)
            cs_rep = sb("cs_rep", [128, 8, 128], BF16, p0)
            b_ada_gt_sb = sb("b_ada_gt_sb", [128, 2, 1024], F32, p0)
            wa = [sb("wa%d" % i, [128, 8, 512], BF16, p0) for i in range(3)]
            e_t = sb("e_t", [128, 16], F32, p0)
            t_t = sb("t_t", [128, 16], F32, p0)
            l_t = sb("l_t", [128, 16], F32, p0)
            m_t = sb("m_t", [128, 16], F32, p0)
            pload(b_ada_gt_sb[:], b_ada_gt)
            gt2b = sb("gt2b0", [128, 1024], F32, p0)

            S.op("act", lambda e: e.activation(out=cs[:], in_=cvec_sb[:], func=AF.Silu), reads=("params",), writes=("cs",))
            S.op("dve", lambda e: e.tensor_copy(out=cs_rep[:], in_=cs[:, :, 0:1].to_broadcast([128, 8, 128])),
                 reads=("cs",), writes=("cs_rep",))
            psm = bank(0)[:, 0:96].rearrange("p (j t) -> p j t", t=2)
            for q in range(12):
                s = q % 3
                S.dma("pool", wa[s][:], w_ada[:, 512 * q:512 * q + 512].rearrange("(k p) n -> p k n", p=128),
                      writes=("wa%d" % s,), key="wa%d" % s)
                fns = []
                for jj in range(4):
                    for k in range(8):
                        fns.append(lambda e, jj=jj, k=k, s=s, q=q: e.matmul(
                            psm[:, 4 * q + jj, :], lhsT=wa[s][:, k, 128 * jj:128 * jj + 128], rhs=cs[:, k, :],
                            start=(k == 0), stop=(k == 7)))
                S.group("pe", fns, reads=("wa%d" % s, "cs"), writes=("ps0",))
                if q in (4, 5, 10, 11):
                    bk = 1 + (q % 2)
                    S.group("pe", [lambda e, k=k, s=s, bk=bk: e.matmul(bank(bk), lhsT=cs_rep[:, k, :], rhs=wa[s][:, k, :],
                                                                      start=(k == 0), stop=(k == 7)) for k in range(8)],
                            reads=("wa%d" % s, "cs_rep"), writes=(pk(bk),))
                    dst = gt1h if q < 6 else gt2b
                    gi = 0 if q < 6 else 1
                    cols = slice(512 * (q % 2), 512 * (q % 2) + 512)
                    S.op("dve", lambda e, dst=dst, gi=gi, cols=cols, bk=bk: e.tensor_tensor(
                        out=dst[:, cols], in0=bank(bk), in1=b_ada_gt_sb[:, gi, cols], op=ALU.add),
                        reads=(pk(bk), "params"), writes=("gt",))
            S.op("dve", lambda e: e.tensor_scalar_mul(out=gt1h[:], in0=gt1h[:], scalar1=0.5), reads=("gt",), writes=("gt",))
            S.dma("sp", gt2_scr, gt2b[:], reads=("gt",), writes=("gt2_scr",), key="gt2_scr")
            S.op("dve", lambda e: e.tensor_tensor(out=mods[:], in0=psm, in1=b_ada_fm_sb[:].unsqueeze(2).to_broadcast([128, 48, 2]),
                                                  op=ALU.add), reads=("ps0", "params"), writes=("mods",))
            for (dst, col, j0, gsb) in ((s1, 0, 8, gmix_sb), (s1c, 1, 8, gmix_sb), (s2, 0, 32, gffn_sb)):
                S.op("dve", lambda e, dst=dst, col=col, j0=j0, gsb=gsb: e.scalar_tensor_tensor(
                    out=dst[:], in0=mods[:, j0:j0 + 8, col], scalar=1.0, in1=gsb[:], op0=ALU.add, op1=ALU.mult),
                    reads=("mods", "params"), writes=("sc",))
                S.op("dve", lambda e, dst=dst: e.tensor_scalar_mul(out=dst[:], in0=dst[:], scalar1=32.0),
                     reads=("sc",), writes=("sc",))
            S.op("dve", lambda e: e.tensor_scalar_mul(out=hb_in[:], in0=b_in_sb[:], scalar1=0.5), reads=("params",), writes=("hb_in",))
            S.op("dve", lambda e: e.tensor_scalar_mul(out=hbg[:], in0=bg_sb[:], scalar1=0.5), reads=("params",), writes=("hbg",))
            lamf = lam_sb[:].rearrange("p a b -> p (a b)")
            S.op("act", lambda e: e.activation(out=e_t[:], in_=lamf, func=AF.Exp, scale=-1.0), reads=("params",), writes=("e_t",))
            S.op("dve", lambda e: e.tensor_scalar(out=t_t[:], in0=e_t[:], scalar1=-0.25, scalar2=1.0 / 3.0, op0=ALU.mult, op1=ALU.add),
                 reads=("e_t",), writes=("t_t",))
            S.op("dve", lambda e: e.tensor_tensor(out=t_t[:], in0=t_t[:], in1=e_t[:], op=ALU.mult), reads=("t_t", "e_t"), writes=("t_t",))
            S.op("dve", lambda e: e.tensor_scalar_add(out=t_t[:], in0=t_t[:], scalar1=-0.5), reads=("t_t",), writes=("t_t",))
            S.op("dve", lambda e: e.tensor_tensor(out=t_t[:], in0=t_t[:], in1=e_t[:], op=ALU.mult), reads=("t_t", "e_t"), writes=("t_t",))
            S.op("dve", lambda e: e.tensor_scalar_add(out=t_t[:], in0=t_t[:], scalar1=1.0), reads=("t_t",), writes=("t_t",))
            S.op("dve", lambda e: e.tensor_tensor(out=t_t[:], in0=t_t[:], in1=e_t[:], op=ALU.mult), reads=("t_t", "e_t"), writes=("t_t",))
            S.op("dve", lambda e: e.tensor_scalar_add(out=l_t[:], in0=e_t[:], scalar1=1.0), reads=("e_t",), writes=("l_t",))
            S.op("act", lambda e: e.activation(out=l_t[:], in_=l_t[:], func=AF.Ln), reads=("l_t",), writes=("l_t",))
            S.op("dve", lambda e: e.tensor_single_scalar(out=m_t[:], in_=e_t[:], scalar=0.1, op=ALU.is_lt), reads=("e_t",), writes=("m_t",))
            S.op("dve", lambda e: e.tensor_tensor(out=t_t[:], in0=t_t[:], in1=l_t[:], op=ALU.subtract), reads=("t_t", "l_t"), writes=("t_t",))
            S.op("dve", lambda e: e.tensor_tensor(out=t_t[:], in0=t_t[:], in1=m_t[:], op=ALU.mult), reads=("t_t", "m_t"), writes=("t_t",))
            S.op("dve", lambda e: e.tensor_tensor(out=t_t[:], in0=t_t[:], in1=l_t[:], op=ALU.add), reads=("t_t", "l_t"), writes=("t_t",))
            clf = cl[:].rearrange("p a b -> p (a b)")
            hclf = hcl[:].rearrange("p a b -> p (a b)")
            S.op("dve", lambda e: e.tensor_scalar_mul(out=clf, in0=t_t[:], scalar1=-8.0), reads=("t_t",), writes=("cl",))
            S.op("dve", lambda e: e.tensor_scalar_mul(out=hclf, in0=t_t[:], scalar1=-4.0), reads=("t_t",), writes=("cl",))
            S.barrier()

        mixer = ExitStack()
        wxr = sb("wxr", [128, 8, 1024], BF16, mixer)
        wgb = sb("wgb", [128, 4, 8, 128], BF16, mixer)
        dg5 = sb("dg5", [128, 8, 5, 128], BF16, mixer)
        S.dma("pool", wxr[:], w_in[:, 3072:4096].rearrange("(k p) n -> p k n", p=128), writes=("wxr",), key="wxr")
        S.dma("pool", wgb[:], wg.rearrange("g h p n -> p g h n"), writes=("wgb",), key="wgb")
        for c in range(8):
            S.op("dve", lambda e, c=c: e.tensor_tensor(
                out=dg5[:, c, :, :], in0=identb[:].unsqueeze(1).to_broadcast([128, 5, 128]),
                in1=lw5_sb[:, c, :].unsqueeze(2).to_broadcast([128, 5, 128]), op=ALU.mult),
                reads=("identb", "params"), writes=("dg5",))

        xt = sb("xt", [128, 4, 1024], F32, mixer)
        xh128 = sb("xh128", [128, 1024], F32, mixer)
        xh = xh128[0:4, :]
        xn = sb("xn", [128, 4, 1024], BF16, mixer)
        xnh128 = sb("xnh128", [128, 1024], BF16, mixer)
        xnh = xnh128[0:4, :]
        hxTs = [sb("hxT%d" % i, [128, 8, NB + 4], BF16, mixer) for i in range(2)]
        hxT, hxk, hxhk = hxTs[0], "hxT0", "hxTh0"
        hbc = [0]
        xrp = [sb("xrp%d" % i, [128, NB + 4], BF16, mixer) for i in range(2)]
        xcb = [sb("xcb%d" % i, [128, NB], BF16, mixer) for i in range(2)]
        tr = sb("tr", [128, NB], F32, mixer)
        ti = sb("ti", [128, NB], F32, mixer)
        a4 = sb("a4", [128, 4, NB], F32, mixer)
        s4 = sb("s4", [128, 4, NB], F32, mixer)
        t4 = sb("t4", [128, 4, NB], BF16, mixer)
        tmp1 = sb("tmp1", [128, NB], F32, mixer)
        bb_t = sb("bb_t", [128, NB], F32, mixer)
        hf = sb("hf", [128, NB], F32, mixer)
        hsb = sb("hsb", [128, 8, NB], BF16, mixer)

        tph = bank(4).bitcast(BF16)[:, 0:32].rearrange("p (c t) -> p c t", t=4)

        def prep(xsrc, r0, N, sc, bcol, keep_key, halo=True):
            nt = N // 128
            S.dma("sp", xt[:, 0:nt, :], xsrc[r0:r0 + N, :].rearrange("(j p) d -> p j d", p=128), writes=(keep_key,), key="xt")
            if halo:
                S.dma("sp", xh[0:2, :], xsrc[r0 - 2:r0, :], writes=("xh",), key="xh", nowaw=True)
                S.dma("sp", xh[2:4, :], xsrc[r0 + N:r0 + N + 2, :], writes=("xh",), key="xh", nowaw=True)
            S.op("pool", lambda e: e.memset(ss[:], 0.0), writes=("ss",))
            for j in range(nt):
                S.op("act", lambda e, j=j: e.activation(out=xn[:, j, :], in_=xt[:, j, :], func=AF.Square, accum_out=ss[:, j:j + 1]),
                     reads=(keep_key,), writes=("xn", "ss"))
            if halo:
                S.op("act", lambda e: e.activation(out=xnh[:], in_=xh[:], func=AF.Square, accum_out=ss[0:4, 4:5]),
                     reads=("xh",), writes=("xnh", "ss"))
            S.op("act", lambda e: e.activation(out=rs[:, 0:5], in_=ss[:, 0:5], func=AF.Sqrt, bias=qtr[:, 1:2]), reads=("ss", "qtr"), writes=("rs",))
            S.op("dve", lambda e: e.reciprocal(out=rs[:, 0:5], in_=rs[:, 0:5]), reads=("rs",), writes=("rs",))
            for j in range(nt):
                S.op("dve", lambda e, j=j: e.tensor_scalar_mul(out=xn[:, j, :], in0=xt[:, j, :], scalar1=rs[:, j:j + 1]),
                     reads=(keep_key, "rs"), writes=("xn",))
            if halo:
                S.op("dve", lambda e: e.tensor_scalar_mul(out=xnh[:], in0=xh[:], scalar1=rs[0:4, 4:5]),
                     reads=("xh", "rs"), writes=("xnh",))
            for j in range(nt):
                S.group("pe", [lambda e, j=j, c=c: e.transpose(out=tpv[:, c, 128 * j:128 * j + 128],
                                                               in_=xn[:, j, 128 * c:128 * c + 128], identity=identb[:])
                               for c in range(8)], reads=("xn", "identb"), writes=TPK)
            if halo:
                S.group("pe", [lambda e, c=c: e.transpose(out=tph[:, c, :], in_=xnh[:, 128 * c:128 * c + 128], identity=identb[0:4, 0:4])
                               for c in range(8)], reads=("xnh", "identb"), writes=("ps4",))
            for c in range(8):
                S.op("act", lambda e, c=c: e.activation(out=hxT[:, c, 0:N], in_=tpv[:, c, 0:N], func=AF.Identity,
                                                        scale=sc[:, c:c + 1], bias=mods[:, c, bcol:bcol + 1]),
                     reads=TPK + ("sc", "mods"), writes=(hxk,))
            if halo:
                S.op("dve", lambda e: e.tensor_tensor(out=hxT[:, :, N:N + 4], in0=tph, in1=sc[:].unsqueeze(2).to_broadcast([128, 8, 4]),
                                                      op=ALU.mult), reads=("ps4", "sc"), writes=(hxhk,))
                S.op("dve", lambda e: e.tensor_tensor(out=hxT[:, :, N:N + 4], in0=hxT[:, :, N:N + 4],
                                                      in1=mods[:, 0:8, bcol:bcol + 1].to_broadcast([128, 8, 4]), op=ALU.add),
                     reads=(hxhk, "mods"), writes=(hxhk,))

        xr_evac_eng = ["dve"]

        def rglru_block(N, d, reverse, has_lo, has_hi, consumer=None, xc_store=None, xc_src=None):
            def st1(c):
                par = c % 2
                bxr = bank(par)[:, 0:N]
                bxh = bank(2 + par)[:, 0:4]
                S.group("pe", [lambda e, k=k: e.matmul(bxr, lhsT=wxr[:, k, 128 * c:128 * c + 128], rhs=hxT[:, k, 0:N],
                                                       start=(k == 0), stop=(k == 7)) for k in range(8)],
                        reads=(hxk, "wxr"), writes=(pk(par),))
                S.group("pe", [lambda e, k=k: e.matmul(bxh, lhsT=wxr[:, k, 128 * c:128 * c + 128], rhs=hxT[:, k, N:N + 4],
                                                       start=(k == 0), stop=(k == 7)) for k in range(8)],
                        reads=(hxhk, "wxr"), writes=(pk(2 + par),))
                xk = "xrp%d" % par
                bia = b_in_sb[:, 24 + c:25 + c]
                if xr_evac_eng[0] == "act":
                    S.op("act", lambda e: e.activation(out=xrp[par][:, 2:2 + N], in_=bxr, func=AF.Identity, bias=bia),
                         reads=(pk(par), "params"), writes=(xk,))
                else:
                    S.op("dve", lambda e: e.tensor_scalar_add(out=xrp[par][:, 2:2 + N], in0=bxr, scalar1=bia),
                         reads=(pk(par), "params"), writes=(xk,))
                if has_lo:
                    S.op("dve", lambda e: e.tensor_scalar_add(out=xrp[par][:, 0:2], in0=bxh[:, 0:2], scalar1=bia),
                         reads=(pk(2 + par), "params"), writes=(xk,))
                else:
                    S.op("dve", lambda e: e.memset(xrp[par][:, 0:2], 0.0), writes=(xk,))
                if has_hi:
                    S.op("dve", lambda e: e.tensor_scalar_add(out=xrp[par][:, 2 + N:4 + N], in0=bxh[:, 2:4], scalar1=bia),
                         reads=(pk(2 + par), "params"), writes=(xk,))
                else:
                    S.op("dve", lambda e: e.memset(xrp[par][:, 2 + N:4 + N], 0.0), writes=(xk,))

            def st2(c):
                par = c % 2
                xk = "xrp%d" % par
                bcv = bank(4 + par)[:, 0:N]
                S.group("pe", [lambda e, j=j: e.matmul(bcv, lhsT=dg5[:, c, j, :], rhs=xrp[par][:, j:j + N],
                                                       start=(j == 0), stop=(j == 4)) for j in range(5)],
                        reads=(xk, "dg5"), writes=(pk(4 + par),))
                S.op("dve", lambda e: e.tensor_scalar_add(out=xcb[par][:, 0:N], in0=bcv, scalar1=lb_sb[:, c:c + 1]),
                     reads=(pk(4 + par), "params"), writes=("xcb%d" % par,))
                if xc_store is not None:
                    S.dma("sp", xc_scr[xc_store, :, c, :], xcb[par][:, 0:N], reads=("xcb%d" % par,), writes=("xc_scr%d" % xc_store,), key="xc_scr", nowaw=True)

            def st3(c):
                par = c % 2
                q = c % 4
                ck = "xcb%d" % par
                xc_ap = xcb[par][:, 0:N]
                if xc_src is not None:
                    ck = "dg5"
                    xc_ap = xc_src[:, c, :]
                br_ = bank(6)[:, 0:N]
                bi_ = bank(7)[:, 0:N]
                S.group("pe", [lambda e: e.matmul(br_, lhsT=wgb[:, 2 * d, c, :], rhs=xc_ap, start=True, stop=True)],
                        reads=(ck, "wgb"), writes=("ps6",))
                S.group("pe", [lambda e: e.matmul(bi_, lhsT=wgb[:, 2 * d + 1, c, :], rhs=xc_ap, start=True, stop=True)],
                        reads=(ck, "wgb"), writes=("ps7",))
                S.op("act", lambda e: e.activation(out=tr[:, 0:N], in_=br_, func=AF.Tanh, scale=0.5, bias=hbg[:, 2 * d, c:c + 1]),
                     reads=("ps6", "hbg"), writes=("tr",))
                S.op("act", lambda e: e.activation(out=ti[:, 0:N], in_=bi_, func=AF.Tanh, scale=0.5, bias=hbg[:, 2 * d + 1, c:c + 1]),
                     reads=("ps7", "hbg"), writes=("ti",))
                S.op("act", lambda e: e.activation(out=a4[:, q, 0:N], in_=tr[:, 0:N], func=AF.Exp, scale=hcl[:, d, c:c + 1],
                                                   bias=hcl[:, d, c:c + 1]), reads=("tr", "cl"), writes=("a4_%d" % q,))
                S.op("pool", lambda e: e.tensor_tensor(out=s4[:, q, 0:N], in0=a4[:, q, 0:N], in1=a4[:, q, 0:N], op=ALU.mult),
                     reads=("a4_%d" % q,), writes=("s4_%d" % q,))
                S.op("dve", lambda e: e.scalar_tensor_tensor(out=t4[:, q, 0:N], in0=ti[:, 0:N], scalar=1.0, in1=xc_ap,
                                                             op0=ALU.add, op1=ALU.mult), reads=("ti", ck), writes=("t4_%d" % q,))

            def st4(c0):
                sk = tuple("s4_%d" % q for q in range(4))
                S.op("act", lambda e: e.activation(out=s4[:, :, 0:N], in_=s4[:, :, 0:N], func=AF.Sqrt, scale=-0.25, bias=qtr[:, 0:1]),
                     reads=sk + ("qtr",), writes=sk)
                for c in range(c0, c0 + 4):
                    q = c % 4
                    S.op("pool", lambda e, q=q: e.tensor_tensor(out=bb_t[:, 0:N], in0=s4[:, q, 0:N], in1=t4[:, q, 0:N], op=ALU.mult),
                         reads=("s4_%d" % q, "t4_%d" % q), writes=("bb_t",))
                    if reverse:
                        S.op("dve", lambda e, q=q, c=c: e.tensor_tensor_scan(
                            out=hf[:, 0:N][:, ::-1], data0=a4[:, q, 0:N][:, ::-1], data1=bb_t[:, 0:N][:, ::-1],
                            initial=state[:, d, c:c + 1], op0=ALU.mult, op1=ALU.add),
                            reads=("a4_%d" % q, "bb_t", "state"), writes=("hf",))
                        S.op("pool", lambda e, c=c: e.tensor_copy(out=state[:, d, c:c + 1], in_=hf[:, 0:1]),
                             reads=("hf",), writes=("state",))
                    else:
                        S.op("dve", lambda e, q=q, c=c: e.tensor_tensor_scan(
                            out=hf[:, 0:N], data0=a4[:, q, 0:N], data1=bb_t[:, 0:N], initial=state[:, d, c:c + 1],
                            op0=ALU.mult, op1=ALU.add), reads=("a4_%d" % q, "bb_t", "state"), writes=("hf",))
                        S.op("pool", lambda e, c=c: e.tensor_copy(out=state[:, d, c:c + 1], in_=hf[:, N - 1:N]),
                             reads=("hf",), writes=("state",))
                    if consumer is not None:
                        consumer(c)

            for s_ in range(10):
                if s_ < 8 and xc_src is None:
                    st1(s_)
                if 1 <= s_ <= 8 and xc_src is None:
                    st2(s_ - 1)
                if 2 <= s_ <= 9:
                    st3(s_ - 2)
                    if (s_ - 2) % 4 == 3:
                        st4(s_ - 2 - 3)

        hxT, hxk, hxhk = hxTs[0], "hxT0", "hxTh0"
        prep(ctxp, 2, 256, s1c, 1, "xt")
        hbc[0] = 1
        rglru_block(256, 0, False, False, False)
        rglru_block(256, 1, True, False, False)
        for blk in range(15, -1, -1):
            _i = hbc[0] % 2
            hbc[0] += 1
            hxT, hxk, hxhk = hxTs[_i], "hxT%d" % _i, "hxTh%d" % _i
            prep(xp, 2 + NB * blk, NB, s1, 0, "xt")
            def cons_a(c):
                S.op("dve", lambda e, c=c: e.tensor_copy(out=hsb[:, c, :], in_=hf[:]), reads=("hf",), writes=("hsb",))
            rglru_block(NB, 1, True, blk != 0, blk != 15, cons_a if blk < 8 else None, xc_store=(blk if blk < 8 else None))
            if blk < 8:
                S.dma("sp", hs_scr[blk], hsb[:], reads=("hsb",), writes=("hs_scr%d" % blk,), key="hs_scr")

        pb_ = ExitStack()
        cwh = cw_sb
        S.op("dve", lambda e: e.tensor_scalar_mul(out=cwh[:], in0=cw_sb[:], scalar1=0.5), reads=("params",), writes=("cwh",))
        lnr = sb("lnr", [128, NB], F32, pb_)
        lmr = sb("lmr", [128, NB], F32, pb_)
        wsl = [sb("wsl%d" % i, [128, 8, 512], BF16, pb_) for i in range(3)]
        dgc = [sb("dgc0", [128, 31, 128], BF16, pb_)] * 2
        tv, uu = tr, ti
        zb = [sb("zb0", [128, NB], BF16, pb_)] * 2
        zc = sb("zc", [128, 8, NB], BF16, pb_)
        aa = zc
        zsq = [sb("zsq0", [128, NB], BF16, pb_)] * 2
        A_t = sb("A_t", [128, 8, NB], BF16, pb_)
        gy = sb("gy", [128, 8, NB], BF16, pb_)
        mg = gy
        yb = sb("yb", [128, 8, NB], BF16, pb_)
        hs_in = hsb
        lnt0, lnt1 = xh128[:, 0:512], xh128[:, 512:1024]
        tg = xnh128[:].bitcast(F32)
        first_ln = [True]
        xc_in = dg5[:].rearrange("p c j m -> p (c j m)")[:, 0:8 * NB].rearrange("p (c t) -> p c t", t=NB)
        x1 = xt

        xres = [sb("xres%d" % i, [128, 512], F32, pb_) for i in range(3)]
        xrc = [0]
        wctr = [0]

        def wpiece(col0, src=None):
            src = w_in if src is None else src
            i = wctr[0] % 3
            wctr[0] += 1
            S.dma("pool", wsl[i][:], src[:, col0:col0 + 512].rearrange("(k p) n -> p k n", p=128),
                  writes=("wsl%d" % i,), key="wsl%d" % i)
            return wsl[i], "wsl%d" % i

        def inproj(dstbank, wt, wk, j4):
            S.group("pe", [lambda e, k=k: e.matmul(bank(dstbank), lhsT=wt[:, k, 128 * j4:128 * j4 + 128], rhs=hxT[:, k, 0:NB],
                                                   start=(k == 0), stop=(k == 7)) for k in range(8)],
                    reads=(hxk, wk), writes=(pk(dstbank),))

        xr_evac_eng[0] = "act"
        for blk in range(8):
            _i = hbc[0] % 2
            hbc[0] += 1
            hxT, hxk, hxhk = hxTs[_i], "hxT%d" % _i, "hxTh%d" % _i
            prep(xp, 2 + NB * blk, NB, s1, 0, "xt", halo=False)
            S.dma("sp", xc_in[:], xc_scr[blk], reads=("xc_scr%d" % blk,), writes=("dg5",), key="xc_in")
            S.dma("sp", hs_in[:], hs_scr[blk], reads=("hs_scr%d" % blk,), writes=("hsb",), key="hs_in")
            for half in range(2):
                wu, wuk = wpiece(512 * half)
                wv, wvk = wpiece(1024 + 512 * half)
                for c4 in range(4):
                    c = 4 * half + c4
                    par = c % 2
                    inproj(par, wu, wuk, c4)
                    inproj(2 + par, wv, wvk, c4)
                    S.op("act", lambda e, c=c, par=par: e.activation(out=tv[:], in_=bank(2 + par), func=AF.Tanh, scale=0.5,
                                                                     bias=hb_in[:, 8 + c:9 + c]),
                         reads=(pk(2 + par), "hb_in"), writes=("tr",))
                    S.op("act", lambda e, c=c, par=par: e.activation(out=uu[:], in_=bank(par), func=AF.Identity,
                                                                     bias=b_in_sb[:, c:c + 1]),
                         reads=(pk(par), "params"), writes=("ti",))
                    zk = "zb0"
                    S.op("dve", lambda e, par=par: e.scalar_tensor_tensor(out=zb[par][:], in0=tv[:], scalar=1.0, in1=uu[:],
                                                                          op0=ALU.add, op1=ALU.mult),
                         reads=("tr", "ti"), writes=(zk,))
                    dk = "dgc0"
                    S.op("dve", lambda e, c=c, par=par: e.tensor_tensor(
                        out=dgc[par][:], in0=identb[:].unsqueeze(1).to_broadcast([128, 31, 128]),
                        in1=cwh[:, c, :].unsqueeze(2).to_broadcast([128, 31, 128]), op=ALU.mult),
                        reads=("identb", "cwh"), writes=(dk,))
                    zv = zb[par][:].rearrange("p (r t) -> p r t", t=64)
                    pcv = bank(4 + par).rearrange("p (r t) -> p r t", t=64)
                    fns = []
                    order = [15] + [k for k in range(31) if k != 15]
                    for idx, k in enumerate(order):
                        o = k - 15
                        t0, t1 = max(0, -o), 64 - max(0, o)
                        fns.append(lambda e, k=k, o=o, t0=t0, t1=t1, idx=idx, par=par, pcv=pcv, zv=zv: e.matmul(
                            pcv[:, :, t0:t1], lhsT=dgc[par][:, k, :], rhs=zv[:, :, t0 + o:t1 + o],
                            start=(idx == 0), stop=(idx == 30)))
                    S.group("pe", fns, reads=(zk, dk), writes=(pk(4 + par),))
                    S.op("act", lambda e, c=c, par=par: e.activation(out=zc[:, c, :], in_=bank(4 + par), func=AF.Identity,
                                                                     bias=cb_sb[:, c:c + 1]),
                         reads=(pk(4 + par), "params"), writes=("zc",))
                    qk = "zsq0"
                    S.op("act", lambda e, c=c, par=par: e.activation(out=zsq[par][:], in_=bank(4 + par), func=AF.Square,
                                                                     bias=cb_sb[:, c:c + 1]),
                         reads=(pk(4 + par), "params"), writes=(qk,))
                    S.group("pe", [lambda e, c=c: e.matmul(bank(6), lhsT=ones_m[:], rhs=zc[:, c, :], start=(c == 0), stop=(c == 7))],
                            reads=("zc", "ones_m"), writes=("ps6",))
                    S.group("pe", [lambda e, c=c, par=par: e.matmul(bank(7), lhsT=ones_m[:], rhs=zsq[par][:], start=(c == 0),
                                                                    stop=(c == 7))], reads=(qk, "ones_m"), writes=("ps7",))
            S.op("act", lambda e: e.activation(out=tv[:], in_=bank(6), func=AF.Copy), reads=("ps6",), writes=("tr",))
            S.op("dve", lambda e: e.tensor_tensor(out=uu[:], in0=tv[:], in1=tv[:], op=ALU.mult), reads=("tr",), writes=("ti",))
            S.op("dve", lambda e: e.tensor_tensor(out=lnr[:], in0=bank(7), in1=uu[:], op=ALU.subtract), reads=("ps7", "ti"),
                 writes=("lnr",))
            S.op("act", lambda e: e.activation(out=lnr[:], in_=lnr[:], func=AF.Sqrt, bias=qtr[:, 2:3]), reads=("lnr", "qtr"), writes=("lnr",))
            S.op("dve", lambda e: e.reciprocal(out=lnr[:], in_=lnr[:]), reads=("lnr",), writes=("lnr",))
            S.op("dve", lambda e: e.tensor_tensor(out=lmr[:], in0=tv[:], in1=lnr[:], op=ALU.mult), reads=("tr", "lnr"),
                 writes=("lmr",))
            for half in range(2):
                wy, wyk = wpiece(2048 + 512 * half)
                for c4 in range(4):
                    c = 4 * half + c4
                    par = c % 2
                    inproj(par, wy, wyk, c4)
                    S.op("act", lambda e, c=c, par=par: e.activation(out=gy[:, c, :], in_=bank(par), func=AF.Gelu_apprx_tanh,
                                                                     bias=b_in_sb[:, 16 + c:17 + c]),
                         reads=(pk(par), "params"), writes=("gy",))
            def cons_b(c):
                S.op("dve", lambda e, c=c: e.tensor_tensor(out=tmp1[:], in0=hf[:], in1=hs_in[:, c, :], op=ALU.add),
                     reads=("hf", "hsb"), writes=("tmp1",))
                S.op("dve", lambda e, c=c: e.tensor_tensor(out=yb[:, c, :], in0=tmp1[:], in1=gy[:, c, :], op=ALU.mult),
                     reads=("tmp1", "gy"), writes=("yb",))
            rglru_block(NB, 0, False, blk != 0, True, cons_b, xc_src=xc_in)
            for c in range(8):
                extra = ("xh", "xnh") if first_ln[0] else ()
                first_ln[0] = False
                S.op("dve", lambda e, c=c: e.tensor_tensor(out=lnt0, in0=zc[:, c, :], in1=lnr[:], op=ALU.mult),
                     reads=("zc", "lnr"), writes=("lnt0",) + extra)
                S.op("dve", lambda e: e.tensor_tensor(out=lnt1, in0=lnt0, in1=lmr[:], op=ALU.subtract), reads=("lnt0", "lmr"),
                     writes=("lnt1",))
                S.op("act", lambda e, c=c: e.activation(out=aa[:, c, :], in_=lnt1, func=AF.Silu, scale=lng_sb[:, c:c + 1],
                                                        bias=lnb_sb[:, c:c + 1]), reads=("lnt1", "params"), writes=("zc",))
            for half in range(2):
                wga, wgak = wpiece(4096 + 512 * half)
                wpa, wpak = wpiece(512 * half, w_pa)
                for m4 in range(4):
                    m = 4 * half + m4
                    par = m % 2
                    S.group("pe", [lambda e, k=k, m4=m4, par=par, wpa=wpa: e.matmul(bank(par), lhsT=wpa[:, k, 128 * m4:128 * m4 + 128],
                                                                         rhs=aa[:, k, :], start=(k == 0), stop=(k == 7))
                                   for k in range(8)], reads=("zc", wpak), writes=(pk(par),))
                    inproj(2 + par, wga, wgak, m4)
                    S.op("act", lambda e, m=m, par=par: e.activation(out=tg, in_=bank(2 + par), func=AF.Tanh, scale=0.5,
                                                                     bias=hb_in[:, 32 + m:33 + m]),
                         reads=(pk(2 + par), "hb_in"), writes=("tg",))
                    S.op("dve", lambda e, m=m, par=par: e.scalar_tensor_tensor(out=A_t[:, m, :], in0=tg, scalar=1.0,
                                                                               in1=bank(par), op0=ALU.add, op1=ALU.mult),
                         reads=("tg", pk(par)), writes=("A_t",))
            for half in range(2):
                wgb_, wgbk = wpiece(5120 + 512 * half)
                wpb, wpbk = wpiece(512 * half, w_pb)
                for m4 in range(4):
                    m = 4 * half + m4
                    par = m % 2
                    S.group("pe", [lambda e, k=k, m4=m4, par=par, wpb=wpb: e.matmul(bank(par), lhsT=wpb[:, k, 128 * m4:128 * m4 + 128],
                                                                         rhs=yb[:, k, :], start=(k == 0), stop=(k == 7))
                                   for k in range(8)], reads=("yb", wpbk), writes=(pk(par),))
                    inproj(2 + par, wgb_, wgbk, m4)
                    S.op("act", lambda e, m=m, par=par: e.activation(out=tv[:], in_=bank(2 + par), func=AF.Tanh, scale=0.5,
                                                                     bias=hb_in[:, 40 + m:41 + m]),
                         reads=(pk(2 + par), "hb_in"), writes=("tr",))
                    S.op("dve", lambda e, par=par: e.scalar_tensor_tensor(out=uu[:], in0=tv[:], scalar=1.0, in1=bank(par),
                                                                          op0=ALU.add, op1=ALU.mult),
                         reads=("tr", pk(par)), writes=("ti",))
                    S.op("dve", lambda e, m=m: e.tensor_tensor(out=mg[:, m, :], in0=uu[:], in1=A_t[:, m, :], op=ALU.add),
                         reads=("ti", "A_t"), writes=("gy",))
            for hh in range(2):
                wo, wok = wpiece(512 * hh, w_o)
                for j in range(4):
                    bk = 4 + j
                    xi = xrc[0] % 3
                    xrc[0] += 1
                    xk_ = "xres%d" % xi
                    r0_ = 2 + NB * blk + 128 * j
                    S.dma("sp", xres[xi][:], xp[r0_:r0_ + 128, 512 * hh:512 * hh + 512], writes=(xk_,), key=xk_)
                    S.group("pe", [lambda e, k=k, j=j, bk=bk, wo=wo: e.matmul(bank(bk), lhsT=mg[:, k, 128 * j:128 * j + 128],
                                                                             rhs=wo[:, k, :], start=(k == 0), stop=(k == 7))
                                   for k in range(8)], reads=("gy", wok), writes=(pk(bk),))
                    S.op("dve", lambda e, hh=hh, bk=bk: e.tensor_tensor(out=tmp1[:], in0=bank(bk), in1=gt1h[:, 512 * hh:512 * hh + 512],
                                                                        op=ALU.mult), reads=(pk(bk), "gt"), writes=("tmp1",))
                    S.op("pool", lambda e, xi=xi: e.tensor_tensor(out=xres[xi][:], in0=xres[xi][:], in1=tmp1[:], op=ALU.add),
                         reads=("tmp1", xk_), writes=(xk_,))
                    S.dma("sp", x1_scr[NB * blk + 128 * j:NB * blk + 128 * j + 128, 512 * hh:512 * hh + 512], xres[xi][:],
                          reads=(xk_,), writes=("x1_scr%d" % blk,), key="x1_scr", nowaw=True)
        S.barrier()
        pb_.close()
        mixer.close()

        def bc(ap, shape):
            return ap.to_broadcast(shape)

        pcg = ExitStack()
        gf32 = sb("gf32", [128, 1024], F32, pcg)
        gt2b = sb("gt2b", [128, 1024], F32, pcg)
        S.dma("sp", gf32[:], gfin, writes=("gf32",), key="gf32")
        S.dma("sp", gt2b[:], gt2_scr, reads=("gt2_scr",), writes=("gt2b",), key="gt2b")
        S.op("dve", lambda e: e.tensor_scalar_mul(out=gf32[:], in0=gf32[:], scalar1=32.0), reads=("gf32",), writes=("gf32",))
        slot_i = sb("slot_i", [128, 32], I32, pcg)
        offE_i = sb("offE_i", [128, NSEG, 4], I32, pcg)
        trib = sb("trib", [128, 128], BF16, pcg)
        S.dma("pool", trib[:], tri, writes=("trib",), key="trib")

        c1 = ExitStack()
        x1l = [sb("x1l%d" % i, [128, 4, 1024], F32, c1) for i in range(2)]
        xn2_all = sb("xn2_all", [128, 32, 1024], BF16, c1)
        hmT1 = sb("hmTr", [128, 8, NB], BF16, c1)
        zt = sb("zt", [128, 4096], BF16, c1)
        ztf = sb("ztf", [128, 192], F32, c1)
        ssA = sb("ssA", [128, 8, 4], F32, c1)
        rsA = sb("rsA", [128, 8, 4], F32, c1)
        oh_all = sb("oh_all", [128, 32, 4], F32, c1)
        wsel_all = sb("wsel_all", [128, 32, 4], F32, c1)
        oh_bf = sb("oh_bf", [128, 32, 4], BF16, c1)
        R1s = sb("R1s", [128, 32, 4], F32, c1)
        Cs = sb("Cs", [128, 32, 4], F32, c1)
        incl = sb("incl", [128, 4, 32], F32, c1)
        onesf = sb("onesf", [128, 32], F32, c1)
        ng = sb("ng", [128, 4], F32, c1)
        nseg = sb("nseg", [128, 4], F32, c1)
        sst = sb("sst", [128, 4], F32, c1)
        sen = sb("sen", [128, 4], F32, c1)
        slot_f = sb("slot_f", [128, 32], F32, c1)
        Gs = sb("Gs", [128, NSEG], F32, c1)
        sidx_sb = sb("sidx_sb", [128, NSEG], F32, c1)
        cE_sb = sb("cE_sb", [128, 4], F32, c1)
        offE_f = sb("offE_f", [128, NSEG, 4], F32, c1)
        L = sb("L", [128, 32, 20], F32, c1)
        gmax = sb("gmax", [128, 32, 1], F32, c1)
        eg = sb("eg", [128, 32, 4], F32, c1)
        pg = sb("pg", [128, 32, 1], F32, c1)
        tmp16 = sb("tmp16", [128, 32, 16], F32, c1)
        esel = sb("esel", [128, 32, 4], F32, c1)
        m1 = sb("m1", [128, 32, 1], F32, c1)
        m2 = sb("m2", [128, 32, 1], F32, c1)
        k1 = sb("k1", [128, 32, 4], F32, c1)
        k2 = sb("k2", [128, 32, 4], F32, c1)
        e2 = sb("e2", [128, 32, 4], F32, c1)
        w1 = sb("w1", [128, 32, 1], F32, c1)
        w2 = sb("w2", [128, 32, 1], F32, c1)
        S.dma("sp", sidx_sb[:], sidx, writes=("cidx",), key="cidx")
        S.dma("sp", cE_sb[:], cE, writes=("cidx",), key="cidx")
        S.op("pool", lambda e: e.memset(zt[:], 0.0), writes=("zt",))
        S.op("pool", lambda e: e.memset(ztf[:], 0.0), writes=("zt",))
        S.op("pool", lambda e: e.memset(onesf[:], 1.0), writes=("onesf",))
        for sg_ in range(NSEG):
            S.dma("sp", xs_sorted[NB * sg_:NB * sg_ + NB, :].rearrange("(p r) d -> p (r d)", r=4), zt[:],
                  reads=("zt",), writes=("xs_sorted",), key="xs_z", nowaw=True)
        S.dma("sp", ws_sorted.rearrange("(p r) c -> p (r c)", r=NSEG * NB // 128), ztf[:, 0:4 * (NSEG * NB // 128)], reads=("zt",), writes=("ws_sorted",), key="xs_z")

        for blk in range(8):
            pb2 = blk % 2
            x1t = x1l[pb2]
            ak = "x1l%d" % pb2
            sak = "ssA%d" % blk
            S.dma("sp", x1t[:], x1_scr[NB * blk:NB * blk + NB, :].rearrange("(j p) d -> p j d", p=128),
                  reads=("x1_scr%d" % blk,), writes=(ak,), key=ak)
            S.op("pool", lambda e, blk=blk: e.memset(ssA[:, blk, :], 0.0), writes=(sak,))
            for j in range(4):
                S.op("act", lambda e, j=j, x1t=x1t, blk=blk: e.activation(out=xn2_all[:, 4 * blk + j, :], in_=x1t[:, j, :], func=AF.Square,
                                                                          accum_out=ssA[:, blk, j:j + 1]),
                     reads=(ak,), writes=("xn2_%d" % blk, sak))
            S.op("act", lambda e, blk=blk: e.activation(out=rsA[:, blk, :], in_=ssA[:, blk, :], func=AF.Sqrt, bias=qtr[:, 1:2]),
                 reads=(sak, "qtr"), writes=(sak + "r",))
            S.op("dve", lambda e, blk=blk: e.reciprocal(out=rsA[:, blk, :], in_=rsA[:, blk, :]), reads=(sak + "r",), writes=(sak + "r",))
            for j in range(4):
                S.op("dve", lambda e, j=j, x1t=x1t, blk=blk: e.tensor_scalar_mul(out=xn2_all[:, 4 * blk + j, :], in0=x1t[:, j, :],
                                                                                 scalar1=rsA[:, blk, j:j + 1]),
                     reads=(ak, sak + "r"), writes=("xn2_%d" % blk,))
            for j in range(4):
                S.group("pe", [lambda e, j=j, c=c, blk=blk: e.transpose(out=tpv[:, c, 128 * j:128 * j + 128],
                                                                        in_=xn2_all[:, 4 * blk + j, 128 * c:128 * c + 128], identity=identb[:])
                               for c in range(8)], reads=("xn2_%d" % blk, "identb"), writes=TPK)
            for c in range(8):
                S.op("act", lambda e, c=c: e.activation(out=hmT1[:, c, :], in_=tpv[:, c, :], func=AF.Identity,
                                                        scale=s2[:, c:c + 1], bias=mods[:, 24 + c, 0:1]),
                     reads=TPK + ("sc", "mods"), writes=("hmT1",))
            for j in range(4):
                S.group("pe", [lambda e, k=k, j=j: e.matmul(bank(4)[:, 20 * j:20 * j + 20], lhsT=hmT1[:, k, 128 * j:128 * j + 128],
                                                            rhs=w_rt_b[:, k, :], start=(k == 0), stop=(k == 7))
                               for k in range(8)], reads=("hmT1", "w_rt_b"), writes=("ps4",))
            S.op("dve", lambda e, blk=blk: e.tensor_tensor(out=L[:, 4 * blk:4 * blk + 4, :], in0=bank(4)[:, 0:80].rearrange("p (j n) -> p j n", n=20),
                                                  in1=bc(b_rt_sb[:].unsqueeze(1), [128, 4, 20]), op=ALU.add),
                 reads=("ps4", "params"), writes=("L",))

        oh = oh_all
        R = ("rt",)
        OK_ = ("oh_all",)
        S.op("dve", lambda e: e.tensor_reduce(out=gmax[:], in_=L[:, :, 0:4], axis=AX.X, op=ALU.max), reads=("L",), writes=R)
        S.op("dve", lambda e: e.tensor_tensor(out=oh_all[:], in0=L[:, :, 0:4], in1=bc(gmax[:], [128, 32, 4]), op=ALU.is_equal),
             reads=R + ("L",), writes=R + OK_)
        S.op("dve", lambda e: e.tensor_tensor(out=eg[:], in0=L[:, :, 0:4], in1=bc(gmax[:], [128, 32, 4]), op=ALU.subtract),
             reads=R + ("L",), writes=R)
        S.op("act", lambda e: e.activation(out=eg[:], in_=eg[:], func=AF.Exp), reads=R, writes=R)
        S.op("dve", lambda e: e.tensor_reduce(out=pg[:], in_=eg[:], axis=AX.X, op=ALU.add), reads=R, writes=R)
        S.op("dve", lambda e: e.reciprocal(out=pg[:], in_=pg[:]), reads=R, writes=R)
        S.op("dve", lambda e: e.tensor_tensor(out=tmp16[:].rearrange("p j (g x) -> p j g x", x=4),
                                                     in0=L[:, :, 4:20].rearrange("p j (g x) -> p j g x", x=4),
                                                     in1=bc(oh_all[:].unsqueeze(3), [128, 32, 4, 4]), op=ALU.mult),
             reads=R + ("L",), writes=R)
        S.op("dve", lambda e: e.tensor_reduce(out=esel[:].unsqueeze(3), in_=tmp16[:].rearrange("p j (g x) -> p j x g", x=4),
                                              axis=AX.X, op=ALU.add), reads=R, writes=R)
        S.op("dve", lambda e: e.tensor_reduce(out=m1[:], in_=esel[:], axis=AX.X, op=ALU.max), reads=R, writes=R)
        S.op("dve", lambda e: e.tensor_tensor(out=k1[:], in0=esel[:], in1=bc(m1[:], [128, 32, 4]), op=ALU.is_equal),
             reads=R, writes=R)
        S.op("dve", lambda e: e.scalar_tensor_tensor(out=e2[:], in0=k1[:], scalar=-1e30, in1=esel[:], op0=ALU.mult, op1=ALU.add),
             reads=R, writes=R)
        S.op("dve", lambda e: e.tensor_reduce(out=m2[:], in_=e2[:], axis=AX.X, op=ALU.max), reads=R, writes=R)
        S.op("dve", lambda e: e.tensor_tensor(out=k2[:], in0=e2[:], in1=bc(m2[:], [128, 32, 4]), op=ALU.is_equal),
             reads=R, writes=R)
        S.op("dve", lambda e: e.tensor_tensor(out=w2[:], in0=m2[:], in1=m1[:], op=ALU.subtract), reads=R, writes=R)
        S.op("act", lambda e: e.activation(out=w2[:], in_=w2[:], func=AF.Exp), reads=R, writes=R)
        S.op("dve", lambda e: e.tensor_scalar_add(out=w1[:], in0=w2[:], scalar1=1.0), reads=R, writes=R)
        S.op("dve", lambda e: e.reciprocal(out=w1[:], in_=w1[:]), reads=R, writes=R)
        S.op("dve", lambda e: e.tensor_tensor(out=w2[:], in0=w2[:], in1=w1[:], op=ALU.mult), reads=R, writes=R)
        S.op("dve", lambda e: e.tensor_tensor(out=w1[:], in0=w1[:], in1=pg[:], op=ALU.mult), reads=R, writes=R)
        S.op("dve", lambda e: e.tensor_tensor(out=w2[:], in0=w2[:], in1=pg[:], op=ALU.mult), reads=R, writes=R)
        S.op("dve", lambda e: e.tensor_tensor(out=wsel_all[:], in0=k1[:], in1=bc(w1[:], [128, 32, 4]), op=ALU.mult),
             reads=R, writes=R + ("wsel_all",))
        S.op("dve", lambda e: e.tensor_tensor(out=k2[:], in0=k2[:], in1=bc(w2[:], [128, 32, 4]), op=ALU.mult), reads=R, writes=R)
        S.op("dve", lambda e: e.tensor_tensor(out=wsel_all[:], in0=wsel_all[:], in1=k2[:], op=ALU.add), reads=R + ("wsel_all",),
             writes=R + ("wsel_all",))

        ohf = oh_all[:].rearrange("p t g -> p (t g)")
        S.op("dve", lambda e: e.tensor_copy(out=oh_bf[:], in_=oh_all[:]), reads=("oh_all",), writes=("oh_bf",))
        S.group("pe", [lambda e: e.matmul(bank(0)[:, 0:128], lhsT=trib[:], rhs=oh_bf[:].rearrange("p t g -> p (t g)"), start=True, stop=True)],
                reads=("oh_bf", "trib"), writes=("ps0",))
        S.group("pe", [lambda e: e.matmul(bank(1)[:, 0:128], lhsT=ones1[:], rhs=oh_bf[:].rearrange("p t g -> p (t g)"), start=True, stop=True)],
                reads=("oh_bf", "ones1"), writes=("ps1",))
        S.op("act", lambda e: e.activation(out=R1s[:].rearrange("p t g -> p (t g)"), in_=bank(0)[:, 0:128], func=AF.Copy),
             reads=("ps0",), writes=("R1s",))
        S.op("act", lambda e: e.activation(out=Cs[:].rearrange("p t g -> p (t g)"), in_=bank(1)[:, 0:128], func=AF.Copy),
             reads=("ps1",), writes=("Cs",))
        for g in range(4):
            S.op("dve", lambda e, g=g: e.tensor_tensor_scan(out=incl[:, g, :], data0=onesf[:], data1=Cs[:, :, g], initial=0.0,
                                                            op0=ALU.mult, op1=ALU.add), reads=("Cs", "onesf"), writes=("incl",))
        S.op("dve", lambda e: e.tensor_copy(out=ng[:], in_=incl[:, :, 31]), reads=("incl",), writes=("ng",))
        S.op("dve", lambda e: e.tensor_tensor(out=incl[:], in0=incl[:], in1=Cs[:].rearrange("p t g -> p g t"), op=ALU.subtract),
             reads=("incl", "Cs"), writes=("incl",))
        S.op("dve", lambda e: e.memset(nseg[:], 0.0), writes=("nseg",))
        for k in range(8):
            S.op("dve", lambda e, k=k: e.scalar_tensor_tensor(out=nseg[:], in0=ng[:], scalar=float(NB * k), in1=nseg[:],
                                                              op0=ALU.is_gt, op1=ALU.add), reads=("ng", "nseg"), writes=("nseg",))
        S.op("dve", lambda e: e.memset(sst[:], 0.0), writes=("sst",))
        for g in range(1, 4):
            S.op("dve", lambda e, g=g: e.tensor_tensor(out=sst[:, g:g + 1], in0=sst[:, g - 1:g], in1=nseg[:, g - 1:g], op=ALU.add),
                 reads=("sst", "nseg"), writes=("sst",))
        S.op("dve", lambda e: e.tensor_tensor(out=sen[:], in0=sst[:], in1=nseg[:], op=ALU.add), reads=("sst", "nseg"), writes=("sen",))
        S.op("dve", lambda e: e.tensor_scalar_mul(out=sst[:], in0=sst[:], scalar1=float(NB)), reads=("sst", "sen"), writes=("sst",))
        S.op("dve", lambda e: e.tensor_tensor(out=R1s[:], in0=R1s[:], in1=incl[:].rearrange("p g t -> p t g"), op=ALU.add),
             reads=("R1s", "incl"), writes=("R1s",))
        S.op("dve", lambda e: e.tensor_tensor(out=R1s[:], in0=R1s[:], in1=bc(sst[:].unsqueeze(1), [128, 32, 4]), op=ALU.add),
             reads=("R1s", "sst"), writes=("R1s",))
        S.op("dve", lambda e: e.tensor_tensor(out=R1s[:], in0=R1s[:], in1=oh_all[:], op=ALU.mult), reads=("R1s", "oh_all"), writes=("R1s",))
        S.op("dve", lambda e: e.tensor_reduce(out=slot_f[:].unsqueeze(2), in_=R1s[:], axis=AX.X, op=ALU.add), reads=("R1s",), writes=("slot_f",))
        S.op("dve", lambda e: e.tensor_copy(out=slot_i[:], in_=slot_f[:]), reads=("slot_f",), writes=("slot_i",))
        S.op("dve", lambda e: e.memset(Gs[:], 0.0), writes=("Gs",))
        for g in range(3):
            S.op("dve", lambda e, g=g: e.scalar_tensor_tensor(out=Gs[:], in0=sidx_sb[:], scalar=sen[:, g:g + 1], in1=Gs[:],
                                                              op0=ALU.is_ge, op1=ALU.add), reads=("cidx", "sen", "Gs"), writes=("Gs",))
        S.op("dve", lambda e: e.tensor_scalar_mul(out=offE_f[:], in0=bc(Gs[:].unsqueeze(2), [128, NSEG, 4]), scalar1=512.0),
             reads=("Gs",), writes=("offE_f",))
        S.op("dve", lambda e: e.tensor_tensor(out=offE_f[:], in0=offE_f[:], in1=bc(cE_sb[:].unsqueeze(1), [128, NSEG, 4]), op=ALU.add),
             reads=("offE_f", "cidx"), writes=("offE_f",))
        S.op("dve", lambda e: e.tensor_copy(out=offE_i[:], in_=offE_f[:]), reads=("offE_f",), writes=("offE_i",))
        for t in range(32):
            S.idma("pool", xs_sorted[:, :], bass.IndirectOffsetOnAxis(ap=slot_i[:, t:t + 1], axis=0), xn2_all[:, t, :], None,
                   reads=("slot_i", "xn2_%d" % (t // 4), "xs_sorted"), writes=("xs_sorted_s",), key="scat", nowaw=True)
            S.idma("pool", ws_sorted[:, :], bass.IndirectOffsetOnAxis(ap=slot_i[:, t:t + 1], axis=0), wsel_all[:, t, :], None,
                   reads=("slot_i", "wsel_all", "ws_sorted"), writes=("ws_sorted_s",), key="scat", nowaw=True)
        S.barrier()
        c1.close()

        S.ALPHA = 0.0
        c2 = ExitStack()
        xst = [sb("xst%d" % i, [128, 4, 1024], BF16, c2) for i in range(2)]
        wst = [sb("wst%d" % i, [128, 4, 4], F32, c2) for i in range(2)]
        hmTs = [sb("hmT%d" % i, [128, 8, NB], BF16, c2) for i in range(2)]
        cbc = [sb("cbc%d" % i, [128, 4, NB], BF16, c2) for i in range(2)]
        dgm = sb("dgm", [128, 4, 128], BF16, c2)
        actb = [sb("actb%d" % i, [128, NB], BF16, c2) for i in range(16)]
        wgu = [sb("wgu%d" % i, [128, 2, 8, 512], BF16, c2) for i in range(3)]
        NWD = 5
        wd = [sb("wd%d" % i, [128, 4, 1024], BF16, c2) for i in range(NWD)]
        sg = [sb("sg%d" % i, [128, NB], F32, c2) for i in range(2)]
        tt = [sb("tt%d" % i, [128, NB], BF16, c2) for i in range(2)]
        ysb = [sb("ysb%d" % i, [128, 4, 1024], F32, c2) for i in range(2)]
        ectr = [0]
        for sgi in range(NSEG):
            pb2 = sgi % 2
            hmT, hk = hmTs[pb2], "hmT%d" % pb2
            xk2, wk2, ck2, yk2 = "xst%d" % pb2, "wst%d" % pb2, "cbc%d" % pb2, "ysb%d" % pb2
            S.dma("sp", xst[pb2][:], xs_sorted[NB * sgi:NB * sgi + NB, :].rearrange("(j p) d -> p j d", p=128), writes=(xk2,), key=xk2)
            S.dma("sp", wst[pb2][:], ws_sorted[NB * sgi:NB * sgi + NB, :].rearrange("(j p) c -> p j c", p=128), writes=(wk2,), key=wk2)
            for j in range(4):
                S.group("pe", [lambda e, j=j, c=c, pb2=pb2: e.transpose(out=tpv[:, c, 128 * j:128 * j + 128],
                                                                        in_=xst[pb2][:, j, 128 * c:128 * c + 128], identity=identb[:])
                               for c in range(8)], reads=(xk2, "identb"), writes=TPK)
            for c in range(8):
                S.op("act", lambda e, c=c, hmT=hmT: e.activation(out=hmT[:, c, :], in_=tpv[:, c, :], func=AF.Identity,
                                                                 scale=s2[:, c:c + 1], bias=mods[:, 24 + c, 0:1]),
                     reads=TPK + ("sc", "mods"), writes=(hk,))
            for j in range(4):
                S.op("dve", lambda e, j=j, pb2=pb2: e.tensor_tensor(out=dgm[:], in0=bc(identb[:].unsqueeze(1), [128, 4, 128]),
                                                                    in1=bc(wst[pb2][:, j, :].unsqueeze(2), [128, 4, 128]), op=ALU.mult),
                     reads=("identb", wk2), writes=("dgm",))
                S.group("pe", [lambda e: e.matmul(bank(4 + (j % 2)), lhsT=ones1[:], rhs=dgm[:], start=True, stop=True)],
                        reads=("dgm", "ones1"), writes=(pk(4 + (j % 2)),))
                S.op("act", lambda e, j=j, pb2=pb2: e.activation(out=cbc[pb2][:, :, 128 * j:128 * j + 128],
                                                                 in_=bank(4 + (j % 2)).rearrange("p (x t) -> p x t", t=128), func=AF.Copy),
                     reads=(pk(4 + (j % 2)),), writes=(ck2,))
            for el in range(4):
                si = ectr[0] % 3
                di = ectr[0] % NWD
                ectr[0] += 1
                gk, dk_ = "wgu%d" % si, "wd%d" % di
                ofs = bass.IndirectOffsetOnAxis(ap=offE_i[:, sgi, el:el + 1], axis=0)
                S.idma("pool", wgu[si][:, 0].rearrange("p k n -> p (k n)"), None, w_gate[:, :], ofs, reads=("offE_i",), writes=(gk,), key=gk, nowaw=True)
                S.idma("pool", wgu[si][:, 1].rearrange("p k n -> p (k n)"), None, w_up[:, :], ofs, reads=("offE_i",), writes=(gk,), key=gk, nowaw=True)
                S.idma("pool", wd[di][:].rearrange("p k n -> p (k n)"), None, w_down[:, :], ofs, reads=("offE_i",), writes=(dk_,), key=dk_)
                for f in range(4):
                    u = 4 * el + f
                    pp = u % 2
                    S.group("pe", [lambda e, k=k, f=f, si=si, pp=pp, hmT=hmT: e.matmul(
                        bank(2 * pp), lhsT=wgu[si][:, 0, k, 128 * f:128 * f + 128], rhs=hmT[:, k, :],
                        start=(k == 0), stop=(k == 7)) for k in range(8)], reads=(hk, gk), writes=(pk(2 * pp),))
                    S.group("pe", [lambda e, k=k, f=f, si=si, pp=pp, hmT=hmT: e.matmul(
                        bank(2 * pp + 1), lhsT=wgu[si][:, 1, k, 128 * f:128 * f + 128], rhs=hmT[:, k, :],
                        start=(k == 0), stop=(k == 7)) for k in range(8)], reads=(hk, gk), writes=(pk(2 * pp + 1),))
                    S.op("act", lambda e, pp=pp: e.activation(out=sg[pp][:], in_=bank(2 * pp), func=AF.Silu),
                         reads=(pk(2 * pp),), writes=("sg%d" % pp,))
                    S.op("dve", lambda e, pp=pp: e.tensor_tensor(out=tt[pp][:], in0=bank(2 * pp + 1), in1=sg[pp][:], op=ALU.mult),
                         reads=(pk(2 * pp + 1), "sg%d" % pp), writes=("tt%d" % pp,))
                    S.op("dve", lambda e, pp=pp, u=u, el=el, pb2=pb2: e.tensor_tensor(out=actb[u][:], in0=tt[pp][:], in1=cbc[pb2][:, el, :],
                                                                                      op=ALU.mult),
                         reads=("tt%d" % pp, ck2), writes=("actb%d" % u,))
            dbase = ectr[0] - 4
            for tp_ in range(2):
                fns = []
                for u in range(16):
                    el, f = divmod(u, 4)
                    di = (dbase + el) % NWD
                    for jj in range(2):
                        j = 2 * tp_ + jj
                        for hh in range(2):
                            fns.append(lambda e, u=u, f=f, di=di, j=j, jj=jj, hh=hh: e.matmul(
                                bank(4 + 2 * jj + hh), lhsT=actb[u][:, 128 * j:128 * j + 128],
                                rhs=wd[di][:, f, 512 * hh:512 * hh + 512], start=(u == 0), stop=(u == 15)))
                S.group("pe", fns, reads=tuple("actb%d" % u for u in range(16)) + tuple("wd%d" % ((dbase + el) % NWD) for el in range(4)),
                        writes=("ps4", "ps5", "ps6", "ps7"))
                for jj in range(2):
                    j = 2 * tp_ + jj
                    for hh in range(2):
                        bk = 4 + 2 * jj + hh
                        S.op("dve", lambda e, bk=bk, hh=hh, j=j, pb2=pb2: e.tensor_tensor(
                            out=ysb[pb2][:, j, 512 * hh:512 * hh + 512], in0=bank(bk), in1=gt2b[:, 512 * hh:512 * hh + 512], op=ALU.mult),
                            reads=(pk(bk), "gt2b"), writes=(yk2,))
            S.dma("sp", y_sorted[NB * sgi:NB * sgi + NB, :].rearrange("(j p) d -> p j d", p=128), ysb[pb2][:],
                  reads=(yk2,), writes=("y_sorted",), key="y_sorted")
        S.barrier()
        c2.close()

        S.ALPHA = 0.05
        c3 = ExitStack()
        yg = [sb("yg%d" % i, [128, 4, 1024], F32, c3) for i in range(2)]
        x1b = [sb("x1b%d" % i, [128, 4, 1024], F32, c3) for i in range(2)]
        junkF = sb("junkF", [128, 1024], BF16, c3)
        ssF = sb("ssF", [128, 8, 4], F32, c3)
        rsF = sb("rsF", [128, 8, 4], F32, c3)
        for blk in range(8):
            pb2 = blk % 2
            yk3, xk3, sfk = "yg%d" % pb2, "x1b%d" % pb2, "ssF%d" % blk
            S.dma("sp", x1b[pb2][:], x1_scr[NB * blk:NB * blk + NB, :].rearrange("(j p) d -> p j d", p=128), writes=(xk3,), key=xk3)
            for j in range(4):
                S.idma("pool", yg[pb2][:, j, :], None, y_sorted[:, :], bass.IndirectOffsetOnAxis(ap=slot_i[:, 4 * blk + j:4 * blk + j + 1], axis=0),
                       reads=("slot_i",), writes=(yk3,), key=yk3, nowaw=True)
            S.op("pool", lambda e, blk=blk: e.memset(ssF[:, blk, :], 0.0), writes=(sfk,))
            for j in range(4):
                S.op("dve", lambda e, j=j, pb2=pb2: e.tensor_tensor(out=x1b[pb2][:, j, :], in0=x1b[pb2][:, j, :], in1=yg[pb2][:, j, :], op=ALU.add),
                     reads=(xk3, yk3), writes=(xk3,))
                S.op("act", lambda e, j=j, pb2=pb2, blk=blk: e.activation(out=junkF[:], in_=x1b[pb2][:, j, :], func=AF.Square,
                                                                          accum_out=ssF[:, blk, j:j + 1]),
                     reads=(xk3,), writes=("junkF", sfk))
            S.op("act", lambda e, blk=blk: e.activation(out=rsF[:, blk, :], in_=ssF[:, blk, :], func=AF.Sqrt, bias=qtr[:, 1:2]),
                 reads=(sfk, "qtr"), writes=(sfk + "r",))
            S.op("dve", lambda e, blk=blk: e.reciprocal(out=rsF[:, blk, :], in_=rsF[:, blk, :]), reads=(sfk + "r",), writes=(sfk + "r",))
            for j in range(4):
                S.op("dve", lambda e, j=j, pb2=pb2, blk=blk: e.scalar_tensor_tensor(out=x1b[pb2][:, j, :], in0=x1b[pb2][:, j, :],
                                                                                    scalar=rsF[:, blk, j:j + 1], in1=gf32[:],
                                                                                    op0=ALU.mult, op1=ALU.mult),
                     reads=(xk3, sfk + "r", "gf32"), writes=(xk3,))
            S.dma("sp", out[NB * blk:NB * blk + NB, :].rearrange("(j p) d -> p j d", p=128), x1b[pb2][:],
                  reads=(xk3,), writes=("out%d" % blk,), key="out")
        S.barrier()
        c3.close()
        pcg.close()
    return nc


_NC_CACHE = {}


def _fm(v):
    v = np.asarray(v, np.float32).reshape(-1, 128)
    return np.ascontiguousarray(v.T)


def kernel(x, c, ctx, c_ctx, w_ada, b_ada, g_mix, w_in, b_in, conv_w, conv_b, ln_g, ln_b, w_pa,
           lru_conv_w, lru_conv_b, w_r_f, b_r_f, w_i_f, b_i_f, lam_f, w_r_b, b_r_b, w_i_b, b_i_b, lam_b,
           w_pb, w_o, g_ffn, w_grp, b_grp, w_er, b_er, w_gate, w_up, w_down, g_final):
    f = lambda a: np.ascontiguousarray(np.asarray(a, np.float32))
    x, c, ctx, c_ctx = f(x), f(c), f(ctx), f(c_ctx)
    B = x.shape[0]
    if "nc" not in _NC_CACHE:
        _NC_CACHE["nc"] = build_program()
    nc = _NC_CACHE["nc"]

    common = {
        "w_ada": f(w_ada[0]), "b_ada_fm": _fm(b_ada[0]),
        "b_ada_gt": f(np.broadcast_to(np.stack([b_ada[0][2048:3072], b_ada[0][5120:6144]])[None], (128, 2, 1024))),
        "w_in": f(w_in[0]), "b_in_fm": _fm(b_in[0]),
        "cb": _fm(conv_b[0]), "lng": _fm(ln_g[0]), "lnb": _fm(ln_b[0]),
        "w_pa": f(w_pa[0]), "w_pb": f(w_pb[0]), "w_o": f(w_o[0]),
        "lb": _fm(lru_conv_b[0]),
        "gmix": _fm(g_mix[0]), "gffn": _fm(g_ffn[0]),
        "gfin": f(np.broadcast_to(np.asarray(g_final, np.float32)[None], (128, 1024))),
        "w_rt": f(np.concatenate([w_grp[0], w_er[0]], axis=1)),
        "b_rt": f(np.broadcast_to(np.concatenate([b_grp[0], b_er[0]])[None], (128, 20))),
        "w_gate": f(np.asarray(w_gate[0], np.float32).reshape(16, 8, 128, 512).transpose(0, 2, 1, 3).reshape(2048, 4096)),
        "w_up": f(np.asarray(w_up[0], np.float32).reshape(16, 8, 128, 512).transpose(0, 2, 1, 3).reshape(2048, 4096)),
        "w_down": f(np.asarray(w_down[0], np.float32).reshape(16, 4, 128, 1024).transpose(0, 2, 1, 3).reshape(2048, 4096)),
        "ident": np.eye(128, dtype=np.float32),
        "tri": np.triu(np.ones((128, 128), np.float32), 1),
        "cE": (np.arange(4)[None, :] * 128 + np.arange(128)[:, None]).astype(np.float32),
        "sidx": np.broadcast_to(np.arange(11, dtype=np.float32)[None], (128, 11)).copy(),
    }
    cwn = np.asarray(conv_w[0], np.float32)
    lwn = np.asarray(lru_conv_w[0], np.float32)
    zero = np.zeros((1, 1024), np.float32)
    lw5_nat = np.concatenate([lwn, zero], axis=0)
    lw5_rev = lw5_nat[::-1]

    def fm3(a):
        T = a.shape[0]
        return np.ascontiguousarray(a.reshape(T, 8, 128).transpose(2, 1, 0))

    pf = (w_r_f[0], b_r_f[0], w_i_f[0], b_i_f[0], lam_f[0])
    pbk = (w_r_b[0], b_r_b[0], w_i_b[0], b_i_b[0], lam_b[0])

    def gates(P, Sd):
        wgs = np.stack([P[0], P[2], Sd[0], Sd[2]]).astype(np.float32)
        bgs = np.stack([np.asarray(t, np.float32) for t in (P[1], P[3], Sd[1], Sd[3])])
        bgs = np.ascontiguousarray(bgs.transpose(2, 0, 1))
        lams = np.stack([np.asarray(P[4], np.float32).reshape(8, 128), np.asarray(Sd[4], np.float32).reshape(8, 128)])
        lams = np.ascontiguousarray(lams.transpose(2, 0, 1))
        return f(wgs), bgs, lams

    per_half = []
    for half in range(2):
        if half == 0:
            wgs, bgs, lams = gates(pf, pbk)
            d = {"cw": fm3(cwn), "lw5": fm3(lw5_nat), "wg": wgs, "bg": bgs, "lam": lams}
        else:
            wgs, bgs, lams = gates(pbk, pf)
            d = {"cw": fm3(cwn[::-1]), "lw5": fm3(lw5_rev), "wg": wgs, "bg": bgs, "lam": lams}
        per_half.append(d)

    in_maps = []
    pad2 = np.zeros((2, 1024), np.float32)
    for b in range(B):
        for half in range(2):
            xs = x[b] if half == 0 else x[b, ::-1]
            cs_ = ctx[b] if half == 0 else ctx[b, ::-1]
            m = dict(common)
            m.update(per_half[half])
            m["xp"] = np.ascontiguousarray(np.concatenate([pad2, xs, pad2], axis=0))
            m["ctxp"] = np.ascontiguousarray(np.concatenate([pad2, cs_, pad2], axis=0))
            m["cvec"] = np.ascontiguousarray(np.stack([_fm(c[b]), _fm(c_ctx)], axis=-1))
            in_maps.append(m)
    res = run_bass_kernel_spmd(nc, in_maps, core_ids=list(range(2 * B)))
    outp = np.empty((B, 2 * NOWN, 1024), np.float32)
    for b in range(B):
        outp[b, :NOWN] = res.results[2 * b]["out"]
        outp[b, NOWN:] = res.results[2 * b + 1]["out"][::-1]
    if DEBUG:
        kernel.last = res
    return outp
```
